# Optimizing a Trainium2 kernel written in Bass

```python
import math
import jax
import jax.numpy as jnp
from jax import lax
import numpy as np

D_MODEL = 1024
BATCH = 8
SEQ = 4096
DEPTH = 1

CTX_LEN = 256
GRID_W = 64
N_BRANCHES = 2
EPS = 1e-6
F32 = jnp.float32

DN_HEADS = 8
DN_DK = 128
DN_DV = 128
DN_CONV = 5
DN_CHUNK = 64
MLA_HEADS = 8
MLA_Q_RANK = 384
MLA_KV_RANK = 256
MLA_NOPE = 128
MLA_ROPE = 64
MLA_QK_DIM = MLA_NOPE + MLA_ROPE
MLA_V = 128
Q_BLOCK = 128
ROPE_BASE = 10000.0
N_EXPERTS = 16
EC_CAPACITY = 2
EXPERT_FF = 1408

DN_QKV_DIM = 2 * DN_HEADS * DN_DK + DN_HEADS * DN_DV
OFF_Z = DN_QKV_DIM
OFF_ALPHA = OFF_Z + DN_HEADS * DN_DV
OFF_BETA = OFF_ALPHA + 2 * DN_HEADS
OFF_CQ = OFF_BETA + 2 * DN_HEADS
OFF_CKV = OFF_CQ + MLA_Q_RANK
OFF_KROPE = OFF_CKV + MLA_KV_RANK
OFF_GATE = OFF_KROPE + MLA_ROPE
D_IN = OFF_GATE + N_BRANCHES * D_MODEL
IN_OFFSETS = (OFF_Z, OFF_ALPHA, OFF_BETA, OFF_CQ, OFF_CKV, OFF_KROPE, OFF_GATE)

kernel_name = "hybrid_deltanet_mla_ec_moe_dit_block"


def rms_norm(x, g):
    xf = x.astype(F32)
    y = xf * lax.rsqrt(jnp.mean(xf * xf, axis=-1, keepdims=True) + EPS)
    return y.astype(x.dtype) * g


def l2_norm(x):
    return x * lax.rsqrt(jnp.sum(x * x, axis=-1, keepdims=True) + EPS)


def adaln(cond, ada_w, ada_b):
    mod = jax.nn.silu(cond) @ ada_w + ada_b
    return jnp.split(mod[:, None, :], 6, axis=-1)


def modulate(x, g, shift, scale):
    return rms_norm(x, g) * (1.0 + scale) + shift


def short_conv(x, w):
    pad = DN_CONV // 2
    y = lax.conv_general_dilated(x, w[:, None, :], window_strides=(1,), padding=[(pad, pad)],
                                 dimension_numbers=('NWC', 'WIO', 'NWC'), feature_group_count=x.shape[-1])
    return jax.nn.silu(y)


def gated_delta_chunked(q, k, v, g, beta, s0):
    b_, n_tok, n_h, dk = q.shape
    dv = v.shape[-1]
    n_chunk = n_tok // DN_CHUNK

    def to_chunks(t):
        return jnp.moveaxis(t.reshape(b_, n_chunk, DN_CHUNK, n_h, *t.shape[3:]), 3, 1)

    q, k, v, g, beta = (to_chunks(t) for t in (q, k, v, g, beta))
    q = q * dk ** -0.5
    g = jnp.cumsum(g, axis=-1)
    incl = jnp.tril(jnp.ones((DN_CHUNK, DN_CHUNK), dtype=bool))
    strict = jnp.tril(jnp.ones((DN_CHUNK, DN_CHUNK), dtype=bool), -1)
    decay = jnp.exp(jnp.where(incl, g[..., :, None] - g[..., None, :], -jnp.inf))
    k_beta = k * beta[..., None]
    m = jnp.where(strict, jnp.einsum('bhnid,bhnjd->bhnij', k_beta, k) * decay, 0.0)
    eye = jnp.eye(DN_CHUNK, dtype=q.dtype)
    t_inv = lax.linalg.triangular_solve(m + eye, jnp.broadcast_to(eye, m.shape), left_side=True,
                                        lower=True, unit_diagonal=True)
    u = jnp.einsum('bhnij,bhnjd->bhnid', t_inv, v * beta[..., None])
    w = jnp.einsum('bhnij,bhnjd->bhnid', t_inv, k_beta * jnp.exp(g)[..., None])
    qk = jnp.where(incl, jnp.einsum('bhnid,bhnjd->bhnij', q, k) * decay, 0.0)
    q_dec = q * jnp.exp(g)[..., None]
    k_dec = k * jnp.exp(g[..., -1:] - g)[..., None]
    chunk_decay = jnp.exp(g[..., -1])

    def chunk_step(s, xs):
        qk_c, q_c, k_c, u_c, w_c, a_c = xs
        v_new = u_c - jnp.einsum('bhik,bhkv->bhiv', w_c, s)
        o_c = jnp.einsum('bhik,bhkv->bhiv', q_c, s) + jnp.einsum('bhij,bhjv->bhiv', qk_c, v_new)
        s = s * a_c[..., None, None] + jnp.einsum('bhik,bhiv->bhkv', k_c, v_new)
        return s, o_c

    xs = tuple(jnp.moveaxis(t, 2, 0) for t in (qk, q_dec, k_dec, u, w, chunk_decay))
    s_final, o = lax.scan(chunk_step, s0, xs)
    o = jnp.transpose(o, (1, 0, 3, 2, 4)).reshape(b_, n_tok, n_h, dv)
    return o, s_final


def deltanet_inputs(qkv, alpha, beta_logit, conv_w, a_log, dt_bias):
    b_, n_tok, _ = qkv.shape
    qkv = short_conv(qkv, conv_w)
    q, k, v = jnp.split(qkv, [DN_HEADS * DN_DK, 2 * DN_HEADS * DN_DK], axis=-1)
    q = l2_norm(q.astype(F32).reshape(b_, n_tok, DN_HEADS, DN_DK))
    k = l2_norm(k.astype(F32).reshape(b_, n_tok, DN_HEADS, DN_DK))
    v = v.astype(F32).reshape(b_, n_tok, DN_HEADS, DN_DV)
    alpha = alpha.astype(F32).reshape(b_, n_tok, 2, DN_HEADS)
    g = -jnp.exp(a_log.astype(F32)) * jax.nn.softplus(alpha + dt_bias.astype(F32))
    beta = jax.nn.sigmoid(beta_logit.astype(F32).reshape(b_, n_tok, 2, DN_HEADS))
    return q, k, v, g, beta


def deltanet_bidir(q, k, v, g, beta, s0_fwd, s0_bwd):
    o_f, s_f = gated_delta_chunked(q, k, v, g[:, :, 0], beta[:, :, 0], s0_fwd)
    flip = lambda t: jnp.flip(t, axis=1)
    o_b, s_b = gated_delta_chunked(flip(q), flip(k), flip(v), flip(g[:, :, 1]), flip(beta[:, :, 1]), s0_bwd)
    return o_f + flip(o_b), s_f, s_b


def deltanet_out(o, z, norm_g):
    b_, n_tok = o.shape[:2]
    zf = z.astype(F32).reshape(b_, n_tok, DN_HEADS, DN_DV)
    y = rms_norm(o, norm_g.astype(F32)) * jax.nn.silu(zf)
    return y.reshape(b_, n_tok, DN_HEADS * DN_DV).astype(z.dtype)


def axial_rope_tables(rows, dtype):
    row = jnp.repeat(jnp.arange(rows), GRID_W).astype(F32)
    col = jnp.broadcast_to(jnp.arange(GRID_W), (rows, GRID_W)).reshape(-1).astype(F32)
    n_freq = MLA_ROPE // 4
    inv_freq = ROPE_BASE ** (-jnp.arange(n_freq, dtype=F32) / n_freq)
    ang_r = row[:, None] * inv_freq
    ang_c = col[:, None] * inv_freq
    ang = jnp.concatenate([ang_r, ang_r, ang_c, ang_c], axis=-1)[:, None, :]
    return jnp.cos(ang).astype(dtype), jnp.sin(ang).astype(dtype)


def apply_rope_tail(t, rope):
    cos, sin = rope
    nope, pe = jnp.split(t, [MLA_NOPE], axis=-1)
    x1, x2, x3, x4 = jnp.split(pe, 4, axis=-1)
    rot = jnp.concatenate([-x2, x1, -x4, x3], axis=-1)
    return jnp.concatenate([nope, pe * cos + rot * sin], axis=-1)


def mla_queries(c_q, q_a_norm_g, w_uq, q_norm_g, rope):
    b_, n_tok, _ = c_q.shape
    q = (rms_norm(c_q, q_a_norm_g) @ w_uq).reshape(b_, n_tok, MLA_HEADS, MLA_QK_DIM)
    q = rms_norm(q, q_norm_g)
    if rope is not None:
        q = apply_rope_tail(q, rope)
    return jnp.transpose(q, (0, 2, 1, 3))


def mla_keys_values(c_kv, k_rope, kv_a_norm_g, w_ukv, k_norm_g, rope):
    b_, n_tok, _ = c_kv.shape
    kv = (rms_norm(c_kv, kv_a_norm_g) @ w_ukv).reshape(b_, n_tok, MLA_HEADS, MLA_NOPE + MLA_V)
    k_nope, v = jnp.split(kv, [MLA_NOPE], axis=-1)
    k_pe = jnp.broadcast_to(k_rope[:, :, None, :], (b_, n_tok, MLA_HEADS, MLA_ROPE))
    k = rms_norm(jnp.concatenate([k_nope, k_pe], axis=-1), k_norm_g)
    if rope is not None:
        k = apply_rope_tail(k, rope)
    return jnp.transpose(k, (0, 2, 1, 3)), jnp.transpose(v, (0, 2, 1, 3))


def softmax_attention(q, k, v):
    b_, n_h, n_tok, dh = q.shape
    n_blk = n_tok // Q_BLOCK
    q_blocks = jnp.moveaxis(q.reshape(b_, n_h, n_blk, Q_BLOCK, dh), 2, 0)
    scale = dh ** -0.5

    def attend_block(qb):
        s = jnp.einsum('bhqd,bhkd->bhqk', qb, k, preferred_element_type=F32) * scale
        p = jax.nn.softmax(s, axis=-1).astype(v.dtype)
        return jnp.einsum('bhqk,bhkd->bhqd', p, v)

    o = lax.map(attend_block, q_blocks)
    o = jnp.moveaxis(o, 0, 2).reshape(b_, n_h, n_tok, -1)
    return jnp.transpose(o, (0, 2, 1, 3)).reshape(b_, n_tok, -1)


def merge_branches(y_a, y_b, gate_logits, w_out_a, w_out_b, w_o):
    g_a, g_b = jnp.split(jax.nn.sigmoid(gate_logits), N_BRANCHES, axis=-1)
    return (g_a * (y_a @ w_out_a) + g_b * (y_b @ w_out_b)) @ w_o


def expert_choice_moe(h, router_w, w_gate, w_up, w_down):
    b_, n_tok, d = h.shape
    cap = EC_CAPACITY * n_tok // N_EXPERTS
    affinity = jax.nn.softmax((h @ router_w).astype(F32), axis=-1)
    gate, idx = lax.top_k(jnp.swapaxes(affinity, 1, 2), cap)
    xe = jax.vmap(lambda hb, ib: hb[ib])(h, idx)
    hid = jax.nn.silu(jnp.einsum('becd,edf->becf', xe, w_gate)) * jnp.einsum('becd,edf->becf', xe, w_up)
    ye = jnp.einsum('becf,efd->becd', hid, w_down) * gate[..., None].astype(h.dtype)
    return jax.vmap(lambda ib, yb: jnp.zeros((n_tok, d), yb.dtype).at[ib.reshape(-1)].add(yb.reshape(-1, d)))(idx, ye)


def setup_inputs(seed: int = 0) -> dict:
    key = jax.random.key(seed)
    ks = jax.random.split(key, 26)
    d = D_MODEL
    nrm = lambda k, shape, scale: jax.random.normal(k, shape, F32) * scale
    gain = lambda k, n: 1.0 + 0.02 * jax.random.normal(k, (DEPTH, n), F32)
    dt = jnp.exp(jax.random.uniform(ks[10], (DEPTH, 2, DN_HEADS), F32, math.log(1e-3), math.log(1e-1)))
    return {
        'x': nrm(ks[0], (BATCH, SEQ, d), 1.0),
        'c': nrm(ks[1], (BATCH, d), 1.0),
        'ctx': nrm(ks[2], (BATCH, CTX_LEN, d), 1.0),
        'c_ctx': nrm(ks[3], (d,), 1.0),
        'ada_w': nrm(ks[4], (DEPTH, d, 6 * d), d ** -0.5),
        'ada_b': nrm(ks[5], (DEPTH, 6 * d), 0.02),
        'norm1_g': gain(ks[6], d),
        'norm2_g': gain(ks[7], d),
        'w_in': nrm(ks[8], (DEPTH, d, D_IN), d ** -0.5),
        'conv_w': nrm(ks[9], (DEPTH, DN_CONV, DN_QKV_DIM), DN_CONV ** -0.5),
        'a_log': jnp.log(jax.random.uniform(ks[11], (DEPTH, 2, DN_HEADS), F32, 1.0, 16.0)),
        'dt_bias': dt + jnp.log(-jnp.expm1(-dt)),
        'dn_norm_g': gain(ks[12], DN_DV),
        'w_out_a': nrm(ks[13], (DEPTH, DN_HEADS * DN_DV, d), (DN_HEADS * DN_DV) ** -0.5),
        'q_a_norm_g': gain(ks[14], MLA_Q_RANK),
        'w_uq': nrm(ks[15], (DEPTH, MLA_Q_RANK, MLA_HEADS * MLA_QK_DIM), MLA_Q_RANK ** -0.5),
        'kv_a_norm_g': gain(ks[16], MLA_KV_RANK),
        'w_ukv': nrm(ks[17], (DEPTH, MLA_KV_RANK, MLA_HEADS * (MLA_NOPE + MLA_V)), MLA_KV_RANK ** -0.5),
        'q_norm_g': gain(ks[18], MLA_QK_DIM),
        'k_norm_g': gain(ks[19], MLA_QK_DIM),
        'w_out_b': nrm(ks[20], (DEPTH, MLA_HEADS * MLA_V, d), (MLA_HEADS * MLA_V) ** -0.5),
        'w_o': nrm(ks[21], (DEPTH, d, d), d ** -0.5),
        'router_w': nrm(ks[22], (DEPTH, d, N_EXPERTS), d ** -0.5),
        'w_gate': nrm(ks[23], (DEPTH, N_EXPERTS, d, EXPERT_FF), d ** -0.5),
        'w_up': nrm(ks[24], (DEPTH, N_EXPERTS, d, EXPERT_FF), d ** -0.5),
        'w_down': nrm(ks[25], (DEPTH, N_EXPERTS, EXPERT_FF, d), EXPERT_FF ** -0.5),
    }


def reference(x, c, ctx, c_ctx, ada_w, ada_b, norm1_g, norm2_g, w_in, conv_w, a_log, dt_bias, dn_norm_g,
              w_out_a, q_a_norm_g, w_uq, kv_a_norm_g, w_ukv, q_norm_g, k_norm_g, w_out_b, w_o, router_w,
              w_gate, w_up, w_down):
    b_, n_lat, _ = x.shape
    rows = n_lat // GRID_W
    rope = axial_rope_tables(rows, x.dtype)
    zero_state = jnp.zeros((b_, DN_HEADS, DN_DK, DN_DV), F32)
    for i in range(DEPTH):
        last = i == DEPTH - 1
        mod_x = adaln(c, ada_w[i], ada_b[i])
        mod_c = adaln(c_ctx[None], ada_w[i], ada_b[i])

        h_c = modulate(ctx, norm1_g[i], mod_c[0], mod_c[1])
        h_x = modulate(x, norm1_g[i], mod_x[0], mod_x[1])
        qkv_c, z_c, al_c, be_c, cq_c, ckv_c, kr_c, gt_c = jnp.split(h_c @ w_in[i], IN_OFFSETS, axis=-1)
        qkv_x, z_x, al_x, be_x, cq_x, ckv_x, kr_x, gt_x = jnp.split(h_x @ w_in[i], IN_OFFSETS, axis=-1)

        o_dn_c, s_fwd, s_bwd = deltanet_bidir(*deltanet_inputs(qkv_c, al_c, be_c, conv_w[i], a_log[i], dt_bias[i]),
                                              zero_state, zero_state)
        o_dn_x, _, _ = deltanet_bidir(*deltanet_inputs(qkv_x, al_x, be_x, conv_w[i], a_log[i], dt_bias[i]),
                                      s_fwd, s_bwd)

        k_c, v_c = mla_keys_values(ckv_c, kr_c, kv_a_norm_g[i], w_ukv[i], k_norm_g[i], None)
        k_x, v_x = mla_keys_values(ckv_x, kr_x, kv_a_norm_g[i], w_ukv[i], k_norm_g[i], rope)
        q_x = mla_queries(cq_x, q_a_norm_g[i], w_uq[i], q_norm_g[i], rope)
        o_mla_x = softmax_attention(q_x, jnp.concatenate([k_c, k_x], axis=2), jnp.concatenate([v_c, v_x], axis=2))

        mix_x = merge_branches(deltanet_out(o_dn_x, z_x, dn_norm_g[i]), o_mla_x, gt_x, w_out_a[i], w_out_b[i], w_o[i])
        x_mid = x + mod_x[2] * mix_x

        if not last:
            q_c = mla_queries(cq_c, q_a_norm_g[i], w_uq[i], q_norm_g[i], None)
            o_mla_c = softmax_attention(q_c, k_c, v_c)
            mix_c = merge_branches(deltanet_out(o_dn_c, z_c, dn_norm_g[i]), o_mla_c, gt_c,
                                   w_out_a[i], w_out_b[i], w_o[i])
            ctx = ctx + mod_c[2] * mix_c
            ctx = ctx + mod_c[5] * expert_choice_moe(modulate(ctx, norm2_g[i], mod_c[3], mod_c[4]),
                                                     router_w[i], w_gate[i], w_up[i], w_down[i])

        h2 = modulate(x_mid, norm2_g[i], mod_x[3], mod_x[4])
        x = x_mid + mod_x[5] * expert_choice_moe(h2, router_w[i], w_gate[i], w_up[i], w_down[i])
    return x
```

```python
import numpy as np
import concourse.bass as bass
import concourse.mybir as mybir
from concourse.bass_utils import run_bass_kernel_spmd

F32 = mybir.dt.float32
BF16 = mybir.dt.bfloat16
I32 = mybir.dt.int32
AF = mybir.ActivationFunctionType
ALU = mybir.AluOpType
AX = mybir.AxisListType

ENGS = ("pe", "act", "dve", "pool", "sp")
EPOCH = 12000


class Res:
    __slots__ = ("name", "w", "rs", "multi", "ws", "excl")

    def __init__(self, name="", multi=False, excl=False):
        self.excl = excl
        self.name = name
        self.w = None
        self.rs = []
        self.multi = multi
        self.ws = []


class Tok:
    __slots__ = ("key", "val", "eng")

    def __init__(self, key, val, eng):
        self.key = key
        self.val = val
        self.eng = eng


class DmaPool:
    def __init__(self, sched, name, n):
        self.s = sched
        self.name = name
        self.n = n
        self.i = 0
        self.count = [0] * n
        self.last = [None] * n

    def keys(self):
        return [("dma", self.name, j) for j in range(self.n)]


class Sched:
    def __init__(self, nc):
        self.nc = nc
        self.ops = {e: [] for e in ENGS}
        self.cnt = {e: 0 for e in ENGS}
        self.pools = []
        self.last_tok = {e: None for e in ENGS}
        self.n_instr = 0

    def pool(self, name, n):
        p = DmaPool(self, name, n)
        self.pools.append(p)
        return p

    def _deps(self, eng, reads, writes):
        deps = []
        for r in reads:
            if r.multi:
                deps.extend(r.ws)
            elif r.w is not None:
                deps.append(r.w)
            if r.excl:
                deps.extend(t for t in r.rs if t.eng != eng)
        for w in writes:
            if w.multi:
                pass
            elif w.w is not None and w.w.eng != eng:
                deps.append(w.w)
            for t in w.rs:
                if t.eng != eng:
                    deps.append(t)
        return deps

    def _mark_w(self, writes, tok):
        for w in writes:
            if w.multi:
                w.ws.append(tok)
            else:
                w.w = tok
                w.rs = []

    def op(self, eng, fn, reads=(), writes=(), extra=()):
        deps = self._deps(eng, reads, writes) + list(extra)
        c = self.cnt[eng]
        tok = Tok(("eng", eng, c // EPOCH), c % EPOCH + 1, eng)
        self.cnt[eng] = c + 1
        for r in reads:
            r.rs.append(tok)
        self._mark_w(writes, tok)
        self.ops[eng].append((deps, fn, tok, 1))
        self.last_tok[eng] = tok
        return tok

    def dma(self, eng, pool, fn, reads=(), writes=(), extra=()):
        deps = self._deps('__dma__', reads, writes) + list(extra)
        j = pool.i
        pool.i = (pool.i + 1) % pool.n
        if pool.last[j] is not None:
            deps.append(pool.last[j])
        pool.count[j] += 16
        tok = Tok(("dma", pool.name, j), pool.count[j], None)
        pool.last[j] = tok
        for r in reads:
            r.rs.append(tok)
        self._mark_w(writes, tok)
        self.ops[eng].append((deps, fn, tok, 16))
        return tok

    def barrier(self):
        toks = [t for t in self.last_tok.values() if t is not None]
        for p in self.pools:
            toks += [t for t in p.last if t is not None]
        for e in ENGS:
            self.ops[e].append((list(toks), None, None, 0))

    def emit(self, final_waits_eng="sp"):
        nc = self.nc
        sems = {}

        def sem_of(key):
            if key not in sems:
                sems[key] = nc.alloc_semaphore("s_" + "_".join(str(k) for k in key))
            return sems[key]

        for e in ENGS:
            for ep in range((self.cnt[e] + EPOCH - 1) // EPOCH):
                sem_of(("eng", e, ep))
        for p in self.pools:
            for k in p.keys():
                sem_of(k)

        toks = [t for t in self.last_tok.values() if t is not None]
        for p in self.pools:
            toks += [t for t in p.last if t is not None]
        self.ops[final_waits_eng].append((list(toks), None, None, 0))

        engobj = {"pe": "tensor", "act": "scalar", "dve": "vector", "pool": "gpsimd", "sp": "sync"}
        sched = self

        def run(ename):
            def body(eng):
                seen = {}
                for deps, fn, tok, inc in sched.ops[ename]:
                    need = {}
                    for t in deps:
                        if t.val > need.get(t.key, 0):
                            need[t.key] = t.val
                    for k, v in need.items():
                        if seen.get(k, 0) >= v:
                            continue
                        seen[k] = v
                        eng.wait_ge(sem_of(k), v)
                        sched.n_instr += 1
                    if fn is not None:
                        ins = fn(eng)
                        ins.then_inc(sem_of(tok.key), inc)
                        sched.n_instr += 1
            return body

        with nc.Block() as block:
            for ename in ENGS:
                getattr(block, engobj[ename])(run(ename))


class Arena:
    def __init__(self, nc, kbytes=198):
        self.nc = nc
        self.words = kbytes * 256
        self.t = nc.alloc_sbuf_tensor("arena", [128, self.words], F32)
        self.ap = self.t.ap()
        self.off = 0
        self.marks = []
        self.peak = 0

    def tile(self, shape, dtype, name=None):
        esz = {F32: 4, BF16: 2, I32: 4}[dtype]
        n = int(np.prod(shape[1:]))
        nw = (n * esz + 3) // 4
        off = (self.off + 15) // 16 * 16
        assert off + nw <= self.words, f"SBUF overflow {off}+{nw} > {self.words}"
        self.off = off + nw
        self.peak = max(self.peak, self.off)
        a = self.ap[0:shape[0], off:off + nw]
        if dtype != F32:
            a = a.bitcast(dtype)
        a = a[:, 0:n]
        if len(shape) == 3:
            a = a.rearrange("p (a b) -> p a b", a=shape[1])
        elif len(shape) == 4:
            a = a.rearrange("p (a b c) -> p a b c", a=shape[1], b=shape[2])
        return a

    def mark(self):
        self.marks.append(self.off)

    def release(self):
        self.off = self.marks.pop()

D = 1024
T = 4352
NT = 34
TX = 4096
NCTX = 256
H = 8
OFF_Z = 3072
OFF_GATE = 4832
D_IN = 6880
NMID = 1760
EPS = 1e-6
NEG = -30000.0


class K:
    def __init__(self, dbg=()):
        self.nc = bass.Bass("TRN2", target_bir_lowering=False)
        self.S = Sched(self.nc)
        self.A = Arena(self.nc)
        self.dbg = set(dbg)
        self.ins = {}
        self.scr = {}
        nc = self.nc
        self.ps = nc.alloc_psum_tensor("ps", [128, 8, 512], F32).ap()
        self.psr = [Res(f"ps{b}", excl=True) for b in range(8)]
        self.ld = self.S.pool("ld", 8)
        self.st = self.S.pool("st", 8)
        self.wl = self.S.pool("wl", 6)

    def inp(self, name, shape, dtype=F32):
        t = self.nc.dram_tensor(name, list(shape), dtype, kind="ExternalInput").ap()
        self.ins[name] = t
        return t

    def scratch(self, name, shape, dtype):
        kind = "ExternalOutput" if name in self.dbg else "Internal"
        t = self.nc.dram_tensor(name, list(shape), dtype, kind=kind).ap()
        self.scr[name] = (t, Res(name, multi=True))
        return t, self.scr[name][1]

    def bank(self, b, dtype=F32):
        a = self.ps[:, b, :]
        if dtype == BF16:
            a = a.bitcast(BF16)
        return a, self.psr[b]

    def tile(self, shape, dtype, name=None):
        return self.A.tile(shape, dtype, name), Res(name or "t")

    def load(self, dst, dres, src, sres=None, q="sp", pool=None):
        return self.S.dma(q, pool or self.ld, lambda e: e.dma_start(out=dst, in_=src),
                          reads=[sres] if sres is not None else [], writes=[dres])

    def store(self, dst, dres, src, sres, q="sp", pool=None):
        return self.S.dma(q, pool or self.st, lambda e: e.dma_start(out=dst, in_=src),
                          reads=[sres], writes=[dres] if dres is not None else [])

    def wload(self, dst, dres, src):
        return self.S.dma("pool", self.wl, lambda e: e.dma_start(out=dst, in_=src), writes=[dres])


def declare_inputs(k):
    i = k.inp
    i("x", [TX, D]); i("ctx", [NCTX, D]); i("c2", [128, 8, 2])
    i("ada_w", [D, 6 * D]); i("ada_b_row", [1, 6 * D]); i("ada_bT", [128, 48])
    i("g1T", [128, 8]); i("g2T", [128, 8]); i("g2_rep", [128, D])
    i("w_in", [D, D_IN]); i("convT", [128, 24, 5])
    i("alog_rep", [128, 16]); i("dtb_rep", [128, 16]); i("dng_rep", [128, 128])
    i("gqaT", [128, 3]); i("w_uq", [384, 1536]); i("gkvaT", [128, 2]); i("w_ukv", [256, 2048])
    i("gq_rep", [128, 192]); i("gk_rep", [128, 192])
    i("w_out_a", [D, D]); i("w_out_b", [D, D]); i("w_o", [D, D])
    i("router_w", [D, 16]); i("w_gate", [16, D, 1408]); i("w_up", [16, D, 1408]); i("w_down", [16, 1408, D])
    i("rope_cs", [TX, 128])
    i("ident", [128, 128]); i("consts", [128, 1024])
    i("moe_blk", [128, 128]); i("moe_sel8", [128, 16]); i("moe_tris", [128, 128])
    i("dn_masks", [128, 9, 128]); i("dn_esel", [64, 2, 8, 128]); i("dn_linit", [64, 128])
    k.out = k.nc.dram_tensor("out", [TX, D], F32, kind="ExternalOutput").ap()
    k.out_res = Res("out", multi=True)


def setup_consts(k):
    S = k.S
    k.ident_f, k.r_ident_f = k.tile([128, 128], F32, "identf")
    k.ident_b, k.r_ident_b = k.tile([128, 128], BF16, "identb")
    k.load(k.ident_f, k.r_ident_f, k.ins["ident"])
    S.op("dve", lambda e: e.tensor_copy(out=k.ident_b, in_=k.ident_f), reads=[k.r_ident_f], writes=[k.r_ident_b])
    k.ones_f, k.r_ones_f = k.tile([128, 128], F32, "onesf")
    k.ones_b, k.r_ones_b = k.tile([128, 128], BF16, "onesb")
    S.op("pool", lambda e: e.memset(k.ones_f, 1.0), writes=[k.r_ones_f])
    S.op("pool", lambda e: e.memset(k.ones_b, 1.0), writes=[k.r_ones_b])


def phase_a0(k):
    S, A, nc = k.S, k.A, k.nc
    ins = k.ins
    k.modT, k.r_modT = k.tile([128, 48, 2], F32, "modT")
    k.s1, k.r_s1 = k.tile([128, 8, 2], F32, "s1")
    k.s2, k.r_s2 = k.tile([128, 8], F32, "s2")
    k.gate1_row, k.r_gate1 = k.tile([128, D], F32, "gate1row")
    k.gate2_row, k.r_gate2 = k.tile([128, D], F32, "gate2row")
    k.off_after_gate2 = A.off
    k.shift2_row, k.r_shift2 = k.tile([128, D], F32, "shift2row")
    k.s2_row, k.r_s2row = k.tile([128, D], F32, "s2row")
    A.mark()
    c2, r_c2 = k.tile([128, 8, 2], F32, "c2")
    sc, r_sc = k.tile([128, 8, 2], F32, "sc")
    screp, r_screp = k.tile([128, 8, 128], F32, "screp")
    abT, r_abT = k.tile([128, 48], F32, "abT")
    abrow, r_abrow = k.tile([1, 6 * D], F32, "abrow")
    g1T, r_g1T = k.tile([128, 8], F32, "g1T")
    g2T, r_g2T = k.tile([128, 8], F32, "g2T")
    wbuf = [k.tile([128, 8, D], F32, f"adaw{j}") for j in range(2)]
    k.load(c2, r_c2, ins["c2"])
    k.load(abT, r_abT, ins["ada_bT"])
    k.load(abrow, r_abrow, ins["ada_b_row"])
    k.load(g1T, r_g1T, ins["g1T"])
    k.load(g2T, r_g2T, ins["g2T"])
    S.op("act", lambda e: e.activation(out=sc, in_=c2, func=AF.Silu), reads=[r_c2], writes=[r_sc])
    S.op("dve", lambda e: e.tensor_copy(out=screp, in_=sc[:, :, 0:1].to_broadcast([128, 8, 128])),
         reads=[r_sc], writes=[r_screp])
    pm, r_pm = k.bank(0)
    aw = ins["ada_w"].rearrange("(kc p) n -> p kc n", p=128)
    for sec in range(6):
        wt, r_wt = wbuf[sec % 2]
        q = "sp" if sec % 2 == 0 else "act"
        S.dma(q, k.ld, lambda e, wt=wt, sec=sec: e.dma_start(out=wt, in_=aw[:, :, sec * D:(sec + 1) * D]), writes=[r_wt])

        def mm(e, wt=wt, sec=sec):
            for fc in range(8):
                for kc in range(8):
                    ins_ = e.matmul(pm[:, (sec * 8 + fc) * 2:(sec * 8 + fc) * 2 + 2], lhsT=wt[:, kc, fc * 128:(fc + 1) * 128],
                                    rhs=sc[:, kc, :], start=(kc == 0), stop=(kc == 7))
            return ins_
        S.op("pe", mm, reads=[r_wt, r_sc], writes=[r_pm])
        if sec in (2, 3, 4, 5):
            dst, r_dst = {2: (k.gate1_row, k.r_gate1), 5: (k.gate2_row, k.r_gate2), 3: (k.shift2_row, k.r_shift2), 4: (k.s2_row, k.r_s2row)}[sec]
            for hf in range(2):
                pb, r_pb = k.bank(1 + hf)

                def mmr(e, wt=wt, sec=sec, hf=hf, pb=pb):
                    for kc in range(8):
                        e.matmul(pb, lhsT=screp[:, kc, :], rhs=wt[:, kc, hf * 512:(hf + 1) * 512], start=(kc == 0), stop=False)
                    return e.matmul(pb, lhsT=k.ones_f[0:1, :], rhs=abrow[0:1, sec * D + hf * 512: sec * D + (hf + 1) * 512],
                                    start=False, stop=True)
                S.op("pe", mmr, reads=[r_wt, r_screp, k.r_ones_f, r_abrow], writes=[r_pb])
                S.op("act", lambda e, dst=dst, hf=hf, pb=pb: e.activation(out=dst[:, hf * 512:(hf + 1) * 512], in_=pb, func=AF.Copy),
                     reads=[r_pb], writes=[r_dst])
    S.op("dve", lambda e: e.tensor_tensor(out=k.modT, in0=pm[:, 0:96].rearrange("p (a b) -> p a b", b=2),
                                          in1=abT.unsqueeze(2).to_broadcast([128, 48, 2]), op=ALU.add),
         reads=[r_pm, r_abT], writes=[k.r_modT])
    S.op("dve", lambda e: e.scalar_tensor_tensor(out=k.s1, in0=k.modT[:, 8:16, :], scalar=1.0,
                                                 in1=g1T.unsqueeze(2).to_broadcast([128, 8, 2]), op0=ALU.add, op1=ALU.mult),
         reads=[k.r_modT, r_g1T], writes=[k.r_s1])
    S.op("dve", lambda e: e.scalar_tensor_tensor(out=k.s2, in0=k.modT[:, 32:40, 0], scalar=1.0,
                                                 in1=g2T, op0=ALU.add, op1=ALU.mult),
         reads=[k.r_modT, r_g2T], writes=[k.r_s2])
    g2rep, r_g2rep = k.tile([128, D], F32, "g2rep")
    k.load(g2rep, r_g2rep, ins["g2_rep"])
    S.op("dve", lambda e: e.scalar_tensor_tensor(out=k.s2_row, in0=k.s2_row, scalar=1.0, in1=g2rep, op0=ALU.add, op1=ALU.mult),
         reads=[k.r_s2row, r_g2rep], writes=[k.r_s2row])
    S.barrier()
    A.release()

def _nb(k, dtype=F32):
    b = getattr(k, "_bank_i", 0)
    k._bank_i = (b + 1) % 8
    return k.bank(b, dtype)


def _rstd(k, ss, r_ss, n, out, r_out):
    S = k.S
    S.op("act", lambda e: e.activation(out=out, in_=ss, func=AF.Sqrt, scale=1.0 / n, bias=EPS), reads=[r_ss], writes=[r_out])
    S.op("dve", lambda e: e.reciprocal(out=out, in_=out), reads=[r_out], writes=[r_out])


def _rope(k, pe, r_pe, cos_t, sin_t, r_tab, t1, t2, r_t1, r_t2):
    S = k.S
    cb = cos_t.unsqueeze(1).to_broadcast([128, 8, 64])
    S.op("pool", lambda e: e.tensor_tensor(out=t1, in0=pe, in1=cb, op=ALU.mult), reads=[r_pe, r_tab], writes=[r_t1])
    pe5 = pe.rearrange("p h (a s c) -> p h a s c", a=2, s=2)
    t25 = t2.rearrange("p h (a s c) -> p h a s c", a=2, s=2)
    sn5 = sin_t.rearrange("p (a s c) -> p a s c", a=2, s=2)

    def f(e):
        for s in range(2):
            ins_ = e.tensor_tensor(out=t25[:, :, :, s, :], in0=pe5[:, :, :, 1 - s, :],
                                   in1=sn5[:, :, s, :].unsqueeze(1).to_broadcast([128, 8, 2, 16]), op=ALU.mult)
        return ins_
    S.op("dve", f, reads=[r_pe, r_tab], writes=[r_t2])
    S.op("pool", lambda e: e.tensor_tensor(out=pe, in0=t1, in1=t2, op=ALU.add), reads=[r_t1, r_t2], writes=[r_pe])


def phase_a1(k):
    S, A, nc, ins = k.S, k.A, k.nc, k.ins
    zs_s, r_zs_s = k.scratch("zs_s", [TX, D], BF16)
    gb_s, r_gb_s = k.scratch("gb_s", [T, 48], F32)
    qmT_s, r_qmT_s = k.scratch("qmT_s", [H, 192, TX], BF16)
    kmT_s, r_kmT_s = k.scratch("kmT_s", [H, 192, T], BF16)
    vm_s, r_vm_s = k.scratch("vm_s", [T, D], BF16)
    A.mark()
    k.hT, _ = k.tile([128, 8, T], BF16, "hT")
    k.r_hT = [Res(f"hT{i}") for i in range(NT)]
    A.mark()
    wmid, r_wmid = k.tile([128, 8, NMID], BF16, "wmid")
    wuq, r_wuq = k.tile([128, 3, 1536], BF16, "wuq")
    wukv, r_wukv = k.tile([128, 2, 2048], BF16, "wukv")
    dtb, r_dtb = k.tile([128, 16], F32, "dtb")
    negA, r_negA = k.tile([128, 16], F32, "negA")
    gq, r_gq = k.tile([128, 192], F32, "gq")
    gk, r_gk = k.tile([128, 192], F32, "gk")
    gqaT, r_gqaT = k.tile([128, 3], F32, "gqaT")
    gkvaT, r_gkvaT = k.tile([128, 2], F32, "gkvaT")
    win = ins["w_in"].rearrange("(kc p) n -> p kc n", p=128)
    for j in range(4):
        k.wload(wmid[:, 2 * j:2 * j + 2, :], r_wmid, win[:, 2 * j:2 * j + 2, OFF_Z:OFF_GATE])
    k.wload(wuq, r_wuq, ins["w_uq"].rearrange("(kc p) n -> p kc n", p=128))
    k.wload(wukv, r_wukv, ins["w_ukv"].rearrange("(kc p) n -> p kc n", p=128))
    k.load(dtb, r_dtb, ins["dtb_rep"])
    k.load(negA, r_negA, ins["alog_rep"])
    k.load(gq, r_gq, ins["gq_rep"])
    k.load(gk, r_gk, ins["gk_rep"])
    k.load(gqaT, r_gqaT, ins["gqaT"])
    k.load(gkvaT, r_gkvaT, ins["gkvaT"])
    S.op("act", lambda e: e.activation(out=negA, in_=negA, func=AF.Exp), reads=[r_negA], writes=[r_negA])
    S.op("dve", lambda e: e.tensor_scalar(out=negA, in0=negA, scalar1=-1.0, scalar2=None, op0=ALU.mult), reads=[r_negA], writes=[r_negA])
    S.op("dve", lambda e: e.tensor_scalar(out=gq, in0=gq, scalar1=192.0 ** -0.5, scalar2=None, op0=ALU.mult), reads=[r_gq], writes=[r_gq])

    NB = 2
    xt = [k.tile([128, D], F32, "xt") for _ in range(NB)]
    junk, r_junk = k.tile([128, D], BF16, "junk")
    ss = [k.tile([128, 8], F32, "ss") for _ in range(NB)]
    xn = [k.tile([128, D], BF16, "xn") for _ in range(NB)]
    zs = [k.tile([128, D], BF16, "zs") for _ in range(NB)]
    gb = [k.tile([128, 48], F32, "gb") for _ in range(NB)]
    t16, r_t16 = k.tile([128, 16], F32, "t16")
    cqn, r_cqn = k.tile([128, 384], BF16, "cqn")
    ckvn, r_ckvn = k.tile([128, 256], BF16, "ckvn")
    cqnT, r_cqnT = k.tile([128, 3, 128], BF16, "cqnT")
    ckvnT, r_ckvnT = k.tile([128, 2, 128], BF16, "ckvnT")
    kr, r_kr = k.tile([128, 64], F32, "kr")
    qsb, r_qsb = k.tile([128, 8, 192], F32, "qsb")
    sq, r_sq = k.tile([128, 8, 192], F32, "sq")
    r8, r_r8 = k.tile([128, 8], F32, "r8")
    kvsb, r_kvsb = k.tile([128, 8, 2, 128], F32, "kvsb")
    tmpf, r_tmpf = kvsb.rearrange("p a b c -> p (a b c)")[:, 0:1024].rearrange("p (a b) -> p a b", a=8), r_kvsb
    kf, r_kf = sq, r_sq
    rk8, r_rk8 = k.tile([128, 8], F32, "rk8")
    sskr, r_sskr = k.tile([128, 1], F32, "sskr")
    rt1, r_rt1 = k.tile([128, 8, 64], F32, "rt1")
    rt2, r_rt2 = k.tile([128, 8, 64], F32, "rt2")
    cs_t = [k.tile([128, 128], F32, "cs") for _ in range(NB)]
    qf, r_qf = k.tile([128, 8, 192], BF16, "qf")
    kfb, r_kfb = k.tile([128, 8, 192], BF16, "kfb")
    vb = [k.tile([128, 8, 128], BF16, "vb") for _ in range(1)]
    qTn = [k.tile([128, 8, 128], BF16, "qTn") for _ in range(1)]
    qTr = [k.tile([64, 8, 128], BF16, "qTr") for _ in range(1)]
    kTn = [k.tile([128, 8, 128], BF16, "kTn") for _ in range(1)]
    kTr = [k.tile([64, 8, 128], BF16, "kTr") for _ in range(1)]

    def src_rows(i):
        return ins["ctx"][i * 128:(i + 1) * 128, :] if i < 2 else ins["x"][(i - 2) * 128:(i - 1) * 128, :]

    def prefetch(i):
        b = i % NB
        k.load(xt[b][0], xt[b][1], src_rows(i))
        if i >= 2:
            xi = i - 2
            S.dma("act", k.ld, lambda e: e.dma_start(out=cs_t[b][0], in_=ins["rope_cs"][xi * 128:(xi + 1) * 128, :]), writes=[cs_t[b][1]])

    def transposes(src, r_src, n, width=128, rows=128):
        pb, r_pb = _nb(k, BF16)
        pv = pb.rearrange("p (a b) -> p a b", b=128)[0:width, 0:n, :]

        def f(e):
            for j in range(n):
                ins_ = e.transpose(out=pv[:, j, :], in_=src(j), identity=k.ident_b)
            return ins_
        S.op("pe", f, reads=[r_src, k.r_ident_b], writes=[r_pb])
        return pv, r_pb

    prefetch(0)
    for i in range(NT):
        b = i % NB
        is_x = i >= 2
        xi = i - 2
        col = 0 if is_x else 1
        tok = slice(i * 128, (i + 1) * 128)
        if i + 1 < NT:
            prefetch(i + 1)
        x_t, r_x = xt[b]
        ss_t, r_ss = ss[b]
        xn_t, r_xn = xn[b]
        S.op("act", lambda e, x_t=x_t, ss_t=ss_t: e.activation(out=junk, in_=x_t, func=AF.Square, accum_out=ss_t[:, 0:1]),
             reads=[r_x], writes=[r_junk, r_ss])
        _rstd(k, ss_t[:, 0:1], r_ss, D, ss_t[:, 1:2], r_ss)
        S.op("act", lambda e, x_t=x_t, ss_t=ss_t, xn_t=xn_t: e.activation(out=xn_t, in_=x_t, func=AF.Copy, scale=ss_t[:, 1:2]),
             reads=[r_x, r_ss], writes=[r_xn])
        pv, r_pv = transposes(lambda j, xn_t=xn_t: xn_t[:, j * 128:(j + 1) * 128], r_xn, 8)
        S.op("dve", lambda e, pv=pv, col=col: e.tensor_tensor(out=tmpf, in0=pv, in1=k.s1[:, :, col:col + 1].to_broadcast([128, 8, 128]), op=ALU.mult),
             reads=[r_pv, k.r_s1], writes=[r_tmpf])
        S.op("pool", lambda e, col=col, tok=tok: e.tensor_tensor(out=k.hT[:, :, tok], in0=tmpf,
                                                                in1=k.modT[:, 0:8, col:col + 1].to_broadcast([128, 8, 128]), op=ALU.add),
             reads=[r_tmpf, k.r_modT], writes=[k.r_hT[i]])
        groups = [(0, 512), (512, 1024), (1024, 1440), (1440, 1760)]
        banks = []
        for g, (c0, c1) in enumerate(groups):
            if g < 2 and not is_x:
                banks.append(None)
                continue
            pb, r_pb = _nb(k)

            def mm(e, pb=pb, c0=c0, c1=c1, tok=tok):
                for kc in range(8):
                    ins_ = e.matmul(pb[:, 0:c1 - c0], lhsT=k.hT[:, kc, tok], rhs=wmid[:, kc, c0:c1], start=(kc == 0), stop=(kc == 7))
                return ins_
            S.op("pe", mm, reads=[k.r_hT[i], r_wmid], writes=[r_pb])
            banks.append((pb, r_pb))
        if is_x:
            z_t, r_z = zs[b]
            for g in range(2):
                pb, r_pb = banks[g]
                S.op("act", lambda e, pb=pb, g=g, z_t=z_t: e.activation(out=z_t[:, g * 512:(g + 1) * 512], in_=pb, func=AF.Silu),
                     reads=[r_pb], writes=[r_z])
            k.store(zs_s[xi * 128:(xi + 1) * 128, :], r_zs_s, z_t, r_z)
        p2, r_p2 = banks[2]
        p3, r_p3 = banks[3]
        gb_t, r_gb = gb[b]
        S.op("dve", lambda e, p2=p2: e.tensor_tensor(out=t16, in0=p2[:, 0:16], in1=dtb, op=ALU.add), reads=[r_p2, r_dtb], writes=[r_t16])
        S.op("act", lambda e: e.activation(out=t16, in_=t16, func=AF.Exp), reads=[r_t16], writes=[r_t16])
        S.op("act", lambda e: e.activation(out=t16, in_=t16, func=AF.Ln, bias=1.0), reads=[r_t16], writes=[r_t16])
        S.op("dve", lambda e, gb_t=gb_t: e.tensor_tensor(out=gb_t[:, 0:16], in0=t16, in1=negA, op=ALU.mult), reads=[r_t16, r_negA], writes=[r_gb])
        S.op("act", lambda e, gb_t=gb_t, p2=p2: e.activation(out=gb_t[:, 16:32], in_=p2[:, 16:32], func=AF.Sigmoid), reads=[r_p2], writes=[r_gb])
        S.op("act", lambda e, gb_t=gb_t: e.activation(out=gb_t[:, 32:48], in_=gb_t[:, 16:32], func=AF.Ln), reads=[r_gb], writes=[r_gb])
        k.store(gb_s[tok, :], r_gb_s, gb_t, r_gb)
        if is_x:
            S.op("act", lambda e, p2=p2, ss_t=ss_t: e.activation(out=junk[:, 0:384], in_=p2[:, 32:416], func=AF.Square, accum_out=ss_t[:, 4:5]),
                 reads=[r_p2], writes=[r_junk, r_ss])
            _rstd(k, ss_t[:, 4:5], r_ss, 384, ss_t[:, 5:6], r_ss)
            S.op("act", lambda e, p2=p2, ss_t=ss_t: e.activation(out=cqn, in_=p2[:, 32:416], func=AF.Copy, scale=ss_t[:, 5:6]),
                 reads=[r_p2, r_ss], writes=[r_cqn])

        S.op("act", lambda e, p3=p3, ss_t=ss_t: e.activation(out=junk[:, 0:256], in_=p3[:, 0:256], func=AF.Square, accum_out=ss_t[:, 2:3]),
             reads=[r_p3], writes=[r_junk, r_ss])
        _rstd(k, ss_t[:, 2:3], r_ss, 256, ss_t[:, 3:4], r_ss)
        S.op("act", lambda e, p3=p3, ss_t=ss_t: e.activation(out=ckvn, in_=p3[:, 0:256], func=AF.Copy, scale=ss_t[:, 3:4]),
             reads=[r_p3, r_ss], writes=[r_ckvn])
        S.op("dve", lambda e, p3=p3: e.tensor_copy(out=kr, in_=p3[:, 256:320]), reads=[r_p3], writes=[r_kr])
        pv, r_pv = transposes(lambda j: ckvn[:, j * 128:(j + 1) * 128], r_ckvn, 2)
        S.op("dve", lambda e, pv=pv: e.tensor_tensor(out=ckvnT, in0=pv, in1=gkvaT.unsqueeze(2).to_broadcast([128, 2, 128]), op=ALU.mult),
             reads=[r_pv, r_gkvaT], writes=[r_ckvnT])
        for b4 in range(4):
            pb, r_pb = _nb(k)

            def mmkv(e, pb=pb, b4=b4):
                for kc in range(2):
                    ins_ = e.matmul(pb, lhsT=ckvnT[:, kc, :], rhs=wukv[:, kc, b4 * 512:(b4 + 1) * 512], start=(kc == 0), stop=(kc == 1))
                return ins_
            S.op("pe", mmkv, reads=[r_ckvnT, r_wukv], writes=[r_pb])
            eng = "act" if b4 % 2 == 0 else "dve"
            dst = kvsb[:, 2 * b4:2 * b4 + 2, :, :].rearrange("p a b c -> p (a b c)")
            if eng == "act":
                S.op("act", lambda e, dst=dst, pb=pb: e.activation(out=dst, in_=pb, func=AF.Copy), reads=[r_pb], writes=[r_kvsb])
            else:
                S.op("dve", lambda e, dst=dst, pb=pb: e.tensor_copy(out=dst, in_=pb), reads=[r_pb], writes=[r_kvsb])
        vb_t, r_vb = vb[0]
        S.op("pool", lambda e, vb_t=vb_t: e.tensor_copy(out=vb_t, in_=kvsb[:, :, 1, :]), reads=[r_kvsb], writes=[r_vb])
        k.store(vm_s[tok, :], r_vm_s, vb_t.rearrange("p h d -> p (h d)"), r_vb)
        S.op("pool", lambda e: e.tensor_tensor(out=sq[:, :, 0:128], in0=kvsb[:, :, 0, :], in1=kvsb[:, :, 0, :], op=ALU.mult),
             reads=[r_kvsb], writes=[r_sq])
        S.op("dve", lambda e: e.tensor_reduce(out=rk8, in_=sq[:, :, 0:128], axis=AX.X, op=ALU.add), reads=[r_sq], writes=[r_rk8])
        S.op("act", lambda e: e.activation(out=junk[:, 0:64], in_=kr, func=AF.Square, accum_out=sskr), reads=[r_kr], writes=[r_junk, r_sskr])
        S.op("dve", lambda e: e.tensor_scalar(out=rk8, in0=rk8, scalar1=sskr, scalar2=None, op0=ALU.add), reads=[r_rk8, r_sskr], writes=[r_rk8])
        _rstd(k, rk8, r_rk8, 192, rk8, r_rk8)
        S.op("dve", lambda e: e.tensor_tensor(out=kf[:, :, 0:128], in0=kvsb[:, :, 0, :], in1=rk8.unsqueeze(2).to_broadcast([128, 8, 128]), op=ALU.mult),
             reads=[r_kvsb, r_rk8], writes=[r_kf])
        S.op("dve", lambda e: e.tensor_tensor(out=kf[:, :, 128:192], in0=kr.unsqueeze(1).to_broadcast([128, 8, 64]),
                                              in1=rk8.unsqueeze(2).to_broadcast([128, 8, 64]), op=ALU.mult),
             reads=[r_kr, r_rk8, r_kf], writes=[r_kf])
        S.op("pool", lambda e: e.tensor_tensor(out=kf, in0=kf, in1=gk.unsqueeze(1).to_broadcast([128, 8, 192]), op=ALU.mult),
             reads=[r_kf, r_gk], writes=[r_kf])
        if is_x:
            _rope(k, kf[:, :, 128:192], r_kf, cs_t[b][0][:, 0:64], cs_t[b][0][:, 64:128], cs_t[b][1], rt1, rt2, r_rt1, r_rt2)
        S.op("act", lambda e: e.activation(out=kfb, in_=kf, func=AF.Copy), reads=[r_kf], writes=[r_kfb])
        kTn_t, r_kTn = kTn[0]
        kTr_t, r_kTr = kTr[0]
        pv, r_pv = transposes(lambda j: kfb[:, j, 0:128], r_kfb, 8)
        S.op("dve", lambda e, pv=pv, kTn_t=kTn_t: e.tensor_copy(out=kTn_t, in_=pv), reads=[r_pv], writes=[r_kTn])
        pv, r_pv = transposes(lambda j: kfb[:, j, 128:192], r_kfb, 8, width=64)
        S.op("act", lambda e, pv=pv, kTr_t=kTr_t: e.activation(out=kTr_t, in_=pv, func=AF.Copy), reads=[r_pv], writes=[r_kTr])
        k.store(kmT_s[:, 0:128, tok].rearrange("h d t -> d h t"), r_kmT_s, kTn_t, r_kTn)
        k.store(kmT_s[:, 128:192, tok].rearrange("h d t -> d h t"), r_kmT_s, kTr_t, r_kTr)
        if not is_x:
            continue
        xtok = slice(xi * 128, (xi + 1) * 128)
        pv, r_pv = transposes(lambda j: cqn[:, j * 128:(j + 1) * 128], r_cqn, 3)
        S.op("dve", lambda e, pv=pv: e.tensor_tensor(out=cqnT, in0=pv, in1=gqaT.unsqueeze(2).to_broadcast([128, 3, 128]), op=ALU.mult),
             reads=[r_pv, r_gqaT], writes=[r_cqnT])
        qflat = qsb.rearrange("p h d -> p (h d)")
        for b3 in range(3):
            pb, r_pb = _nb(k)

            def mmq(e, pb=pb, b3=b3):
                for kc in range(3):
                    ins_ = e.matmul(pb, lhsT=cqnT[:, kc, :], rhs=wuq[:, kc, b3 * 512:(b3 + 1) * 512], start=(kc == 0), stop=(kc == 2))
                return ins_
            S.op("pe", mmq, reads=[r_cqnT, r_wuq], writes=[r_pb])
            if b3 % 2 == 0:
                S.op("act", lambda e, pb=pb, b3=b3: e.activation(out=qflat[:, b3 * 512:(b3 + 1) * 512], in_=pb, func=AF.Copy), reads=[r_pb], writes=[r_qsb])
            else:
                S.op("dve", lambda e, pb=pb, b3=b3: e.tensor_copy(out=qflat[:, b3 * 512:(b3 + 1) * 512], in_=pb), reads=[r_pb], writes=[r_qsb])
        S.op("pool", lambda e: e.tensor_tensor(out=sq, in0=qsb, in1=qsb, op=ALU.mult), reads=[r_qsb], writes=[r_sq])
        S.op("dve", lambda e: e.tensor_reduce(out=r8, in_=sq, axis=AX.X, op=ALU.add), reads=[r_sq], writes=[r_r8])
        _rstd(k, r8, r_r8, 192, r8, r_r8)
        S.op("dve", lambda e: e.tensor_tensor(out=qsb, in0=qsb, in1=r8.unsqueeze(2).to_broadcast([128, 8, 192]), op=ALU.mult),
             reads=[r_qsb, r_r8], writes=[r_qsb])
        S.op("pool", lambda e: e.tensor_tensor(out=qsb, in0=qsb, in1=gq.unsqueeze(1).to_broadcast([128, 8, 192]), op=ALU.mult),
             reads=[r_qsb, r_gq], writes=[r_qsb])
        _rope(k, qsb[:, :, 128:192], r_qsb, cs_t[b][0][:, 0:64], cs_t[b][0][:, 64:128], cs_t[b][1], rt1, rt2, r_rt1, r_rt2)
        S.op("act", lambda e: e.activation(out=qf, in_=qsb, func=AF.Copy), reads=[r_qsb], writes=[r_qf])
        qTn_t, r_qTn = qTn[0]
        qTr_t, r_qTr = qTr[0]
        pv, r_pv = transposes(lambda j: qf[:, j, 0:128], r_qf, 8)
        S.op("dve", lambda e, pv=pv, qTn_t=qTn_t: e.tensor_copy(out=qTn_t, in_=pv), reads=[r_pv], writes=[r_qTn])
        pv, r_pv = transposes(lambda j: qf[:, j, 128:192], r_qf, 8, width=64)
        S.op("act", lambda e, pv=pv, qTr_t=qTr_t: e.activation(out=qTr_t, in_=pv, func=AF.Copy), reads=[r_pv], writes=[r_qTr])
        k.store(qmT_s[:, 0:128, xtok].rearrange("h d t -> d h t"), r_qmT_s, qTn_t, r_qTn)
        k.store(qmT_s[:, 128:192, xtok].rearrange("h d t -> d h t"), r_qmT_s, qTr_t, r_qTr)
    S.barrier()
    A.release()

RW = 4364
NU = 4356


def phase_a2(k):
    S, A, nc, ins = k.S, k.A, k.nc, k.ins
    qdT_s, r_qdT_s = k.scratch("qdT_s", [H, 128, TX], BF16)
    kdT_s, r_kdT_s = k.scratch("kdT_s", [H, 128, T], BF16)
    kd_s, r_kd_s = k.scratch("kd_s", [T, H, 128], BF16)
    vd_s, r_vd_s = k.scratch("vd_s", [T, H, 128], BF16)
    sgT_s, r_sgT_s = k.scratch("sgT_s", [16, 128, TX], BF16)
    A.mark()
    convw, r_convw = k.tile([128, 24, 5], F32, "convw")
    k.load(convw, r_convw, ins["convT"])
    wc = [k.tile([128, 8, 512], BF16, "wc") for _ in range(2)]
    R = [k.tile([128, RW], F32, "R") for _ in range(2)]
    acc, r_acc = k.tile([128, NU], F32, "acc")
    sq, r_sq = k.tile([128, NU], BF16, "sq")
    Yb = [k.tile([128, NU], BF16, "Yb") for _ in range(2)]
    rn, r_rn = k.tile([128, 512], F32, "rn")
    tm = [k.tile([128, NT, 128], BF16, "tm") for _ in range(1)]
    sg = [k.tile([128, 512], BF16, "sg") for _ in range(2)]
    for j in range(2):
        S.op("pool", lambda e, j=j: e.memset(R[j][0], 0.0), writes=[R[j][1]])
    win = ins["w_in"].rearrange("(kc p) n -> p kc n", p=128)
    blocks = [(c * 512, "qkv", c * 4) for c in range(6)] + [(OFF_GATE + c * 512, "gate", c * 4) for c in range(4)]
    tgroups = [(0, 256, 2)] + [(256 + g * 512, 512, 262 + g * 512) for g in range(8)]
    all_hT = list(k.r_hT)

    def load_block(bi):
        c0, kind, _ = blocks[bi]
        w_t, r_w = wc[bi % 2]
        for hf in range(2):
            k.wload(w_t[:, 4 * hf:4 * hf + 4, :], r_w, win[:, 4 * hf:4 * hf + 4, c0:c0 + 512])

    load_block(0)
    ci = 0
    for bi, (c0, kind, chunk0) in enumerate(blocks):
        if bi + 1 < len(blocks):
            load_block(bi + 1)
        w_t, r_w = wc[bi % 2]
        for sub in range(4):
            cc = chunk0 + sub
            if kind == "gate":
                for g in range(8):
                    pb, r_pb = _nb(k)

                    def mm(e, pb=pb, g=g, sub=sub, w_t=w_t):
                        for kc in range(8):
                            ins_ = e.matmul(pb, lhsT=w_t[:, kc, sub * 128:(sub + 1) * 128], rhs=k.hT[:, kc, 256 + g * 512:256 + (g + 1) * 512],
                                            start=(kc == 0), stop=(kc == 7))
                        return ins_
                    S.op("pe", mm, reads=[r_w] + all_hT, writes=[r_pb])
                    s_t, r_s = sg[g % 2]
                    S.op("act", lambda e, pb=pb, s_t=s_t: e.activation(out=s_t, in_=pb, func=AF.Sigmoid), reads=[r_pb], writes=[r_s])
                    k.store(sgT_s[cc, :, g * 512:(g + 1) * 512], r_sgT_s, s_t, r_s)
                continue
            R_t, r_R = R[ci % 2]
            Y_t, r_Y = Yb[ci % 2]
            ci += 1
            for gi, (h0, n, ro) in enumerate(tgroups):
                pb, r_pb = _nb(k)

                def mm(e, pb=pb, h0=h0, n=n, sub=sub, w_t=w_t):
                    for kc in range(8):
                        ins_ = e.matmul(pb[:, 0:n], lhsT=w_t[:, kc, sub * 128:(sub + 1) * 128], rhs=k.hT[:, kc, h0:h0 + n],
                                        start=(kc == 0), stop=(kc == 7))
                    return ins_
                S.op("pe", mm, reads=[r_w] + all_hT, writes=[r_pb])
                if gi % 2 == 0:
                    S.op("act", lambda e, pb=pb, n=n, ro=ro, R_t=R_t: e.activation(out=R_t[:, ro:ro + n], in_=pb[:, 0:n], func=AF.Copy),
                         reads=[r_pb], writes=[r_R])
                else:
                    S.op("dve", lambda e, pb=pb, n=n, ro=ro, R_t=R_t: e.tensor_copy(out=R_t[:, ro:ro + n], in_=pb[:, 0:n]),
                         reads=[r_pb], writes=[r_R])
            ceng = "dve"

            def conv(e, R_t=R_t, cc=cc):
                e.tensor_scalar(out=acc, in0=R_t[:, 0:NU], scalar1=convw[:, cc, 0:1], scalar2=None, op0=ALU.mult)
                for j in range(1, 5):
                    ins_ = e.scalar_tensor_tensor(out=acc, in0=R_t[:, j:j + NU], scalar=convw[:, cc, j:j + 1], in1=acc,
                                                  op0=ALU.mult, op1=ALU.add)
                return ins_
            S.op(ceng, conv, reads=[r_R, r_convw], writes=[r_acc])
            head = cc % 8
            if cc >= 16:
                S.op("act", lambda e, Y_t=Y_t: e.activation(out=Y_t, in_=acc, func=AF.Silu), reads=[r_acc], writes=[r_Y])
            else:
                S.op("act", lambda e: e.activation(out=acc, in_=acc, func=AF.Silu), reads=[r_acc], writes=[r_acc])
                S.op("pool" if ceng == "dve" else "dve", lambda e: e.tensor_tensor(out=sq, in0=acc, in1=acc, op=ALU.mult), reads=[r_acc], writes=[r_sq])
                scale = (128.0 ** -0.5) if cc < 8 else 1.0
                for g in range(9):
                    u0 = g * 512
                    n = min(512, NU - u0)
                    pb, r_pb = _nb(k)
                    S.op("pe", lambda e, pb=pb, u0=u0, n=n: e.matmul(pb[:, 0:n], lhsT=k.ones_b, rhs=sq[:, u0:u0 + n], start=True, stop=True),
                         reads=[r_sq, k.r_ones_b], writes=[r_pb])
                    S.op("act", lambda e, pb=pb, n=n, scale=scale: e.activation(out=rn[:, 0:n], in_=pb[:, 0:n], func=AF.Sqrt,
                                                                              scale=1.0 / (scale * scale), bias=EPS / (scale * scale)),
                         reads=[r_pb], writes=[r_rn])
                    S.op("dve", lambda e, n=n: e.reciprocal(out=rn[:, 0:n], in_=rn[:, 0:n]), reads=[r_rn], writes=[r_rn])
                    S.op("dve", lambda e, u0=u0, n=n, Y_t=Y_t: e.tensor_tensor(out=Y_t[:, u0:u0 + n], in0=acc[:, u0:u0 + n], in1=rn[:, 0:n], op=ALU.mult),
                         reads=[r_acc, r_rn], writes=[r_Y])
            if cc < 8:
                k.store(qdT_s[head, :, :], r_qdT_s, Y_t[:, 260:260 + TX], r_Y)
            elif cc < 16:
                k.store(kdT_s[head, :, 0:256], r_kdT_s, Y_t[:, 0:256], r_Y)
                k.store(kdT_s[head, :, 256:T], r_kdT_s, Y_t[:, 260:260 + TX], r_Y)
            if cc >= 8:
                tm_t, r_tm = tm[0]
                for tb in range(5):
                    t0 = tb * 8
                    nt = min(8, NT - t0)
                    pb, r_pb = _nb(k, BF16)
                    pv = pb.rearrange("p (a b) -> p a b", b=128)[:, 0:nt, :]

                    def tr(e, pv=pv, t0=t0, nt=nt, Y_t=Y_t):
                        for j in range(nt):
                            ti = t0 + j
                            u = ti * 128 if ti < 2 else 260 + (ti - 2) * 128
                            ins_ = e.transpose(out=pv[:, j, :], in_=Y_t[:, u:u + 128], identity=k.ident_b)
                        return ins_
                    S.op("pe", tr, reads=[r_Y, k.r_ident_b], writes=[r_pb])
                    if tb % 2 == 0:
                        S.op("act", lambda e, pv=pv, t0=t0, nt=nt, tm_t=tm_t: e.activation(out=tm_t[:, t0:t0 + nt, :], in_=pv, func=AF.Copy),
                             reads=[r_pb], writes=[r_tm])
                    else:
                        S.op("dve", lambda e, pv=pv, t0=t0, nt=nt, tm_t=tm_t: e.tensor_copy(out=tm_t[:, t0:t0 + nt, :], in_=pv),
                             reads=[r_pb], writes=[r_tm])
                dst_s, r_dst = (kd_s, r_kd_s) if cc < 16 else (vd_s, r_vd_s)
                k.store(dst_s.rearrange("(n p) h d -> p n h d", p=128)[:, :, head, :], r_dst, tm_t, r_tm)
    S.barrier()
    A.release()
    A.release()

def _drive(*gens):
    gens = [g for g in gens if g is not None]
    while gens:
        for g in list(gens):
            try:
                next(g)
            except StopIteration:
                gens.remove(g)


def phase_b(k):
    S, A, nc, ins = k.S, k.A, k.nc, k.ins
    qdT_s, r_qdT_s = k.scr["qdT_s"]
    kdT_s, r_kdT_s = k.scr["kdT_s"]
    kd_s, r_kd_s = k.scr["kd_s"]
    vd_s, r_vd_s = k.scr["vd_s"]
    gb_s, r_gb_s = k.scr["gb_s"]
    o_s = [k.scratch("of_s", [TX, D], F32), k.scratch("ob_s", [TX, D], F32)]
    A.mark()
    msk, r_msk = k.tile([128, 9, 128], F32, "msk")
    k.load(msk, r_msk, ins["dn_masks"])
    EC, r_EC = k.tile([64, 2, 8, 128], F32, "EC")
    k.load(EC, r_EC, ins["dn_esel"])
    L1, r_L1 = k.tile([64, 128], F32, "L1")
    L2, r_L2 = k.tile([64, 128], F32, "L2")
    R1, r_R1 = k.tile([64, 8, 128], F32, "R1")
    R2, r_R2 = k.tile([64, 8, 128], F32, "R2")
    X, r_X = k.tile([128, 2, 64], F32, "X")
    k.load(L1, r_L1, ins["dn_linit"])
    k.load(L2, r_L2, ins["dn_linit"])
    S.op("dve", lambda e: e.tensor_copy(out=R1, in_=EC[:, 0, :, :]), reads=[r_EC], writes=[r_R1])
    S.op("dve", lambda e: e.tensor_copy(out=R2, in_=EC[:, 1, :, :]), reads=[r_EC], writes=[r_R2])
    S.op("pool", lambda e: e.memset(X, 0.0), writes=[r_X])
    S32 = [k.tile([128, 8, 128], F32, f"S32_{d}") for d in range(2)]
    Sbf = [k.tile([128, 8, 128], BF16, f"Sbf_{d}") for d in range(2)]
    for d in range(2):
        S.op("pool", lambda e, d=d: e.memset(S32[d][0], 0.0), writes=[S32[d][1]])
        S.op("pool", lambda e, d=d: e.memset(Sbf[d][0], 0.0), writes=[Sbf[d][1]])
    NB = 2
    gbt = [k.tile([128, 48], F32, "gbt") for _ in range(NB)]
    kT = [k.tile([128, 8, 128], BF16, "kT") for _ in range(NB)]
    qT = [k.tile([128, 8, 128], BF16, "qT") for _ in range(NB)]
    ktm = [k.tile([128, 8, 128], BF16, "ktm") for _ in range(NB)]
    vtm = [k.tile([128, 8, 128], BF16, "vtm") for _ in range(NB)]
    sm = [k.tile([128, 4, 8], F32, "sm") for _ in range(NB)]
    E1, r_E1 = k.tile([128, 8, 128], F32, "E1")
    M, r_M = k.tile([128, 8, 128], F32, "M")
    Mt, r_Mt = k.tile([128, 8, 128], F32, "Mt")
    Md, r_Md = k.tile([128, 8, 128], F32, "Md")
    Mo1, r_Mo1 = k.tile([128, 8, 128], F32, "Mo1")
    Mo2, r_Mo2 = k.tile([128, 8, 128], F32, "Mo2")
    PP = [k.tile([128, 8, 128], F32, f"PP{j}") for j in range(2)]
    PT = [k.tile([128, 8, 128], F32, f"PT{j}") for j in range(2)]
    Tt, r_Tt = k.tile([128, 8, 128], F32, "Tt")
    TtB, r_TtB = k.tile([128, 8, 128], BF16, "TtB")
    kg, r_kg = k.tile([128, 8, 128], BF16, "kg")
    kdec = [k.tile([128, 8, 128], BF16, "kdec") for _ in range(NB)]
    wT = [k.tile([128, 8, 128], BF16, "wT") for _ in range(NB)]
    u = [k.tile([128, 8, 128], F32, "u") for _ in range(NB)]
    qkT = [k.tile([128, 8, 128], BF16, "qkT") for _ in range(NB)]
    vnew, r_vnew = k.tile([128, 8, 128], BF16, "vnew")
    tmpo, r_tmpo = k.tile([128, 8, 128], F32, "tmpo")
    o_t = [k.tile([128, 8, 128], F32, "o") for _ in range(NB)]

    pre_i = [0]

    def nbp(dtype=F32):
        b = pre_i[0]
        pre_i[0] = (b + 1) % 4
        return k.bank(b, dtype)

    def b4(pb):
        return pb.rearrange("p (a b) -> p a b", b=128)

    units = []
    fwd = list(range(NT))
    bwd = [1, 0] + list(range(NT - 1, 1, -1))
    for s in range(NT):
        units.append((0, fwd[s]))
        units.append((1, bwd[s]))

    def loads(ui):
        d, ti = units[ui]
        b = ui % NB
        tok = slice(ti * 128, (ti + 1) * 128)
        k.load(gbt[b][0], gbt[b][1], gb_s[tok, :], r_gb_s)
        k.load(kT[b][0], kT[b][1], kdT_s[:, :, tok].rearrange("h d t -> d h t"), r_kdT_s)
        k.load(ktm[b][0], ktm[b][1], kd_s[tok, :, :], r_kd_s, q="act")
        k.load(vtm[b][0], vtm[b][1], vd_s[tok, :, :], r_vd_s, q="act")
        if ti >= 2:
            xt_ = slice((ti - 2) * 128, (ti - 1) * 128)
            k.load(qT[b][0], qT[b][1], qdT_s[:, :, xt_].rearrange("h d t -> d h t"), r_qdT_s)

    def pre(ui):
        d, ti = units[ui]
        b = ui % NB
        is_x = ti >= 2
        g_t, r_g = gbt[b]
        kT_t, r_kT = kT[b]
        qT_t, r_qT = qT[b]
        ktm_t, r_ktm = ktm[b]
        vtm_t, r_vtm = vtm[b]
        sm_t, r_sm = sm[b]
        gcol = g_t[:, d * 8:(d + 1) * 8]
        beta = g_t[:, 16 + d * 8:16 + (d + 1) * 8]
        lnb = g_t[:, 32 + d * 8:32 + (d + 1) * 8]
        pb, r_pb = nbp()

        def mm0(e):
            e.matmul(pb[:, 0:8], lhsT=msk[:, d, :], rhs=gcol, start=True, stop=True)
            return e.matmul(pb[:, 8:16], lhsT=k.ones_f, rhs=gcol, start=True, stop=True)
        S.op("pe", mm0, reads=[r_msk, r_g, k.r_ones_f], writes=[r_pb])
        S.op("dve", lambda e: e.tensor_copy(out=X[:, :, 32:40], in_=pb[:, 0:8].unsqueeze(1).to_broadcast([128, 2, 8])), reads=[r_pb], writes=[r_X])
        S.op("dve", lambda e: e.tensor_copy(out=X[:, 1, 0:8], in_=pb[:, 0:8]), reads=[r_pb], writes=[r_X])
        S.op("dve", lambda e: e.tensor_tensor(out=X[:, 0, 0:8], in0=pb[:, 0:8], in1=lnb, op=ALU.add), reads=[r_pb, r_g], writes=[r_X])
        S.op("act", lambda e: e.activation(out=sm_t[:, 0, :], in_=pb[:, 0:8], func=AF.Exp), reads=[r_pb], writes=[r_sm])
        S.op("act", lambda e: e.activation(out=sm_t[:, 1, :], in_=pb[:, 8:16], func=AF.Exp), reads=[r_pb], writes=[r_sm])
        S.op("dve", lambda e: e.tensor_tensor(out=sm_t[:, 3, :], in0=pb[:, 8:16], in1=X[:, 1, 0:8], op=ALU.subtract), reads=[r_pb, r_X], writes=[r_sm])
        S.op("act", lambda e: e.activation(out=sm_t[:, 2, :], in_=sm_t[:, 3, :], func=AF.Exp), reads=[r_sm], writes=[r_sm])
        pt, r_pt = nbp()

        def tr0(e):
            e.transpose(out=pt[0:64, 0:128], in_=X[:, 0, :], identity=k.ident_f)
            return e.transpose(out=pt[0:64, 128:256], in_=X[:, 1, :], identity=k.ident_f)
        S.op("pe", tr0, reads=[r_X, k.r_ident_f], writes=[r_pt])
        S.op("act", lambda e: e.activation(out=L1[0:8, :], in_=pt[0:8, 0:128], func=AF.Copy), reads=[r_pt], writes=[r_L1])
        S.op("act", lambda e: e.activation(out=L2[0:8, :], in_=pt[0:8, 128:256], func=AF.Copy), reads=[r_pt], writes=[r_L2])
        S.op("dve", lambda e: e.tensor_tensor(out=R1[32:40, :, :], in0=EC[32:40, 1, :, :], in1=pt[32:40, 0:128].unsqueeze(1).to_broadcast([8, 8, 128]), op=ALU.mult),
             reads=[r_pt, r_EC], writes=[r_R1])
        S.op("dve", lambda e: e.tensor_tensor(out=R2[32:40, :, :], in0=EC[32:40, 0, :, :], in1=pt[32:40, 128:256].unsqueeze(1).to_broadcast([8, 8, 128]), op=ALU.mult),
             reads=[r_pt, r_EC], writes=[r_R2])
        kd_t, r_kd = kdec[b]
        S.op("pool", lambda e: e.tensor_tensor(out=kg, in0=ktm_t, in1=sm_t[:, 0, :].unsqueeze(2).to_broadcast([128, 8, 128]), op=ALU.mult),
             reads=[r_ktm, r_sm], writes=[r_kg])
        S.op("pool", lambda e: e.tensor_tensor(out=kd_t, in0=ktm_t, in1=sm_t[:, 2, :].unsqueeze(2).to_broadcast([128, 8, 128]), op=ALU.mult),
             reads=[r_ktm, r_sm], writes=[r_kd])
        yield
        def do_group(grp):
            hs = range(4 * grp, 4 * grp + 4)
            gs = slice(4 * grp, 4 * grp + 4)
            pk, r_pk = nbp()
            pd, r_pd = nbp()

            def mmk(e):
                for hh, h in enumerate(hs):
                    ins_ = e.matmul(b4(pk)[:, hh, :], lhsT=kT_t[:, h, :], rhs=kT_t[:, h, :], start=True, stop=True)
                return ins_
            S.op("pe", mmk, reads=[r_kT], writes=[r_pk])

            def mmd(e):
                for hh, h in enumerate(hs):
                    ins_ = e.matmul(b4(pd)[:, hh, :], lhsT=L1, rhs=R1[:, h, :], start=True, stop=True)
                return ins_
            S.op("pe", mmd, reads=[r_L1, r_R1], writes=[r_pd])
            S.op("dve", lambda e: e.scalar_tensor_tensor(out=E1[:, gs, :], in0=b4(pd), scalar=0.0, in1=msk[:, 2 + d, :].unsqueeze(1).to_broadcast([128, 4, 128]),
                                                         op0=ALU.min, op1=ALU.add), reads=[r_pd, r_msk], writes=[r_E1])
            S.op("act", lambda e: e.activation(out=E1[:, gs, :], in_=E1[:, gs, :], func=AF.Exp), reads=[r_E1], writes=[r_E1])
            S.op("dve", lambda e: e.tensor_tensor(out=M[:, gs, :], in0=b4(pk), in1=E1[:, gs, :], op=ALU.mult), reads=[r_pk, r_E1], writes=[r_M])
            for (dst_, rdst_, mi_) in ((Md, r_Md, 6), (Mo1, r_Mo1, 7), (Mo2, r_Mo2, 8)):
                S.op("pool", lambda e, dst_=dst_, mi_=mi_: e.tensor_tensor(out=dst_[:, gs, :], in0=M[:, gs, :],
                                                                        in1=msk[:, mi_, :].unsqueeze(1).to_broadcast([128, 4, 128]), op=ALU.mult),
                     reads=[r_M, r_msk], writes=[rdst_])
            pm, r_pm = nbp()

            def trm(e):
                for hh, h in enumerate(hs):
                    ins_ = e.transpose(out=b4(pm)[:, hh, :], in_=Md[:, h, :], identity=k.ident_f)
                return ins_
            S.op("pe", trm, reads=[r_Md, k.r_ident_f], writes=[r_pm])
            S.op("act", lambda e: e.activation(out=Mt[:, gs, :], in_=b4(pm), func=AF.Copy), reads=[r_pm], writes=[r_Mt])
            S.op("dve", lambda e: e.scalar_tensor_tensor(out=Tt[:, gs, :], in0=b4(pm), scalar=-1.0, in1=k.ident_f.unsqueeze(1).to_broadcast([128, 4, 128]),
                                                         op0=ALU.mult, op1=ALU.add), reads=[r_pm, k.r_ident_f], writes=[r_Tt])
            yield
            P_prev, rP_prev, Pt_prev, rPt_prev = Md, r_Md, Mt, r_Mt
            for lvl in range(1, 5):
                P_new, rP_new = PP[lvl % 2]
                Pt_new, rPt_new = PT[lvl % 2]
                pa, r_pa = nbp()

                def mma(e, P_prev=P_prev, Pt_prev=Pt_prev, pa=pa):
                    for hh, h in enumerate(hs):
                        ins_ = e.matmul(b4(pa)[:, hh, :], lhsT=Pt_prev[:, h, :], rhs=P_prev[:, h, :], start=True, stop=True)
                    return ins_
                S.op("pe", mma, reads=[rP_prev, rPt_prev], writes=[r_pa])
                S.op("act", lambda e, P_new=P_new, pa=pa: e.activation(out=P_new[:, gs, :], in_=b4(pa), func=AF.Copy), reads=[r_pa], writes=[rP_new])
                if lvl < 4:
                    pbk, r_pbk = nbp()

                    def mmb(e, P_prev=P_prev, Pt_prev=Pt_prev, pbk=pbk):
                        for hh, h in enumerate(hs):
                            ins_ = e.matmul(b4(pbk)[:, hh, :], lhsT=P_prev[:, h, :], rhs=Pt_prev[:, h, :], start=True, stop=True)
                        return ins_
                    S.op("pe", mmb, reads=[rP_prev, rPt_prev], writes=[r_pbk])
                    S.op("dve", lambda e, Pt_new=Pt_new, pbk=pbk: e.tensor_copy(out=Pt_new[:, gs, :], in_=b4(pbk)), reads=[r_pbk], writes=[rPt_new])
                pc, r_pc = nbp()

                def mmc(e, P_new=P_new, pc=pc):
                    for hh, h in enumerate(hs):
                        ins_ = e.matmul(b4(pc)[:, hh, :], lhsT=P_new[:, h, :], rhs=Tt[:, h, :], start=True, stop=True)
                    return ins_
                S.op("pe", mmc, reads=[rP_new, r_Tt], writes=[r_pc])
                S.op("dve", lambda e, pc=pc: e.tensor_tensor(out=Tt[:, gs, :], in0=Tt[:, gs, :], in1=b4(pc), op=ALU.add), reads=[r_Tt, r_pc], writes=[r_Tt])
                P_prev, rP_prev, Pt_prev, rPt_prev = P_new, rP_new, Pt_new, rPt_new
                yield
            for (Mo_, rMo_) in ((Mo1, r_Mo1), (Mo2, r_Mo2)):
                ptd, r_ptd = nbp()

                def trt(e, ptd=ptd):
                    for hh, h in enumerate(hs):
                        ins_ = e.transpose(out=b4(ptd)[:, hh, :], in_=Tt[:, h, :], identity=k.ident_f)
                    return ins_
                S.op("pe", trt, reads=[r_Tt, k.r_ident_f], writes=[r_ptd])
                S.op("act", lambda e, ptd=ptd: e.activation(out=Mt[:, gs, :], in_=b4(ptd), func=AF.Copy), reads=[r_ptd], writes=[r_Mt])
                pa2, r_pa2 = nbp()

                def mma2(e, pa2=pa2, Mo_=Mo_):
                    for hh, h in enumerate(hs):
                        ins_ = e.matmul(b4(pa2)[:, hh, :], lhsT=Mo_[:, h, :], rhs=Tt[:, h, :], start=True, stop=True)
                    return ins_
                S.op("pe", mma2, reads=[rMo_, r_Tt], writes=[r_pa2])
                S.op("dve", lambda e, pa2=pa2: e.tensor_copy(out=E1[:, gs, :], in_=b4(pa2)), reads=[r_pa2], writes=[r_E1])
                pc2, r_pc2 = nbp()

                def mmc2(e, pc2=pc2):
                    for hh, h in enumerate(hs):
                        ins_ = e.matmul(b4(pc2)[:, hh, :], lhsT=Mt[:, h, :], rhs=E1[:, h, :], start=True, stop=True)
                    return ins_
                S.op("pe", mmc2, reads=[r_Mt, r_E1], writes=[r_pc2])
                S.op("dve", lambda e, pc2=pc2: e.tensor_tensor(out=Tt[:, gs, :], in0=Tt[:, gs, :], in1=b4(pc2), op=ALU.subtract), reads=[r_Tt, r_pc2], writes=[r_Tt])
                yield
            S.op("pool", lambda e: e.tensor_tensor(out=TtB[:, gs, :], in0=Tt[:, gs, :], in1=beta[:, gs].unsqueeze(2).to_broadcast([128, 4, 128]), op=ALU.mult),
                 reads=[r_Tt, r_g], writes=[r_TtB])
            pw, r_pw = nbp()

            def mmw(e):
                for hh, h in enumerate(hs):
                    ins_ = e.matmul(b4(pw)[:, hh, :], lhsT=kg[:, h, :], rhs=TtB[:, h, :], start=True, stop=True)
                return ins_
            S.op("pe", mmw, reads=[r_kg, r_TtB], writes=[r_pw])
            S.op("act", lambda e: e.activation(out=wT[b][0][:, gs, :], in_=b4(pw), func=AF.Copy), reads=[r_pw], writes=[wT[b][1]])
            pu, r_pu = nbp()

            def mmu(e):
                for hh, h in enumerate(hs):
                    ins_ = e.matmul(b4(pu)[:, hh, :], lhsT=TtB[:, h, :], rhs=vtm_t[:, h, :], start=True, stop=True)
                return ins_
            S.op("pe", mmu, reads=[r_TtB, r_vtm], writes=[r_pu])
            S.op("dve", lambda e: e.tensor_copy(out=u[b][0][:, gs, :], in_=b4(pu)), reads=[r_pu], writes=[u[b][1]])
            yield
            if is_x:
                pq, r_pq = nbp()
                pd2, r_pd2 = nbp()

                def mmq(e):
                    for hh, h in enumerate(hs):
                        ins_ = e.matmul(b4(pq)[:, hh, :], lhsT=kT_t[:, h, :], rhs=qT_t[:, h, :], start=True, stop=True)
                    return ins_
                S.op("pe", mmq, reads=[r_kT, r_qT], writes=[r_pq])

                def mmd2(e):
                    for hh, h in enumerate(hs):
                        ins_ = e.matmul(b4(pd2)[:, hh, :], lhsT=L2, rhs=R2[:, h, :], start=True, stop=True)
                    return ins_
                S.op("pe", mmd2, reads=[r_L2, r_R2], writes=[r_pd2])
                S.op("dve", lambda e: e.scalar_tensor_tensor(out=E1[:, gs, :], in0=b4(pd2), scalar=0.0, in1=msk[:, 4 + d, :].unsqueeze(1).to_broadcast([128, 4, 128]),
                                                             op0=ALU.min, op1=ALU.add), reads=[r_pd2, r_msk], writes=[r_E1])
                S.op("act", lambda e: e.activation(out=E1[:, gs, :], in_=E1[:, gs, :], func=AF.Exp), reads=[r_E1], writes=[r_E1])
                S.op("dve", lambda e: e.tensor_tensor(out=qkT[b][0][:, gs, :], in0=b4(pq), in1=E1[:, gs, :], op=ALU.mult), reads=[r_pq, r_E1], writes=[qkT[b][1]])
                yield
        for grp in range(2):
            yield from do_group(grp)

    def seq(ui):
        d, ti = units[ui]
        b = ui % NB
        is_x = ti >= 2
        S32_t, r_S32 = S32[d]
        Sbf_t, r_Sbf = Sbf[d]
        sm_t, r_sm = sm[b]
        wT_t, r_wT = wT[b]
        u_t, r_u = u[b]
        qT_t, r_qT = qT[b]
        qk_t, r_qk = qkT[b]
        kd_t, r_kd = kdec[b]
        banks = [k.bank(4 + j) for j in range(4)]

        def grp_mm(bank2, lhs_fn, rhs_fn, reads):
            for grp in range(2):
                pb, r_pb = bank2[grp]

                def f(e, grp=grp, pb=pb):
                    for hh in range(4):
                        h = 4 * grp + hh
                        ins_ = e.matmul(b4(pb)[:, hh, :], lhsT=lhs_fn(h), rhs=rhs_fn(h), start=True, stop=True)
                    return ins_
                S.op("pe", f, reads=reads, writes=[r_pb])
        grp_mm(banks[0:2], lambda h: wT_t[:, h, :], lambda h: Sbf_t[:, h, :], [r_wT, r_Sbf])
        S.op("pool", lambda e: e.tensor_tensor(out=S32_t, in0=S32_t, in1=sm_t[:, 1, :].unsqueeze(2).to_broadcast([128, 8, 128]), op=ALU.mult),
             reads=[r_S32, r_sm], writes=[r_S32])
        yield
        for grp in range(2):
            gs = slice(4 * grp, 4 * grp + 4)
            pb, r_pb = banks[grp]
            S.op("dve", lambda e, pb=pb, gs=gs: e.tensor_tensor(out=vnew[:, gs, :], in0=u_t[:, gs, :], in1=b4(pb), op=ALU.subtract),
                 reads=[r_u, r_pb], writes=[r_vnew])
        yield
        if is_x:
            grp_mm(banks[2:4], lambda h: qT_t[:, h, :], lambda h: Sbf_t[:, h, :], [r_qT, r_Sbf])
            grp_mm(banks[0:2], lambda h: qk_t[:, h, :], lambda h: vnew[:, h, :], [r_qk, r_vnew])
            yield
            o_tt, r_o = o_t[b]
            for grp in range(2):
                gs = slice(4 * grp, 4 * grp + 4)
                pbq, r_pbq = banks[2 + grp]
                pbc, r_pbc = banks[grp]
                S.op("dve", lambda e, pbq=pbq, gs=gs: e.tensor_tensor(out=tmpo[:, gs, :], in0=b4(pbq), in1=sm_t[:, 0, gs].unsqueeze(2).to_broadcast([128, 4, 128]), op=ALU.mult),
                     reads=[r_pbq, r_sm], writes=[r_tmpo])
                S.op("dve", lambda e, pbc=pbc, gs=gs, o_tt=o_tt: e.tensor_tensor(out=o_tt[:, gs, :], in0=tmpo[:, gs, :], in1=b4(pbc), op=ALU.add),
                     reads=[r_tmpo, r_pbc], writes=[r_o])
            xi = ti - 2
            k.store(o_s[d][0][xi * 128:(xi + 1) * 128, :], o_s[d][1], o_tt.rearrange("p h d -> p (h d)"), r_o)
            yield
        grp_mm(banks[2:4], lambda h: kd_t[:, h, :], lambda h: vnew[:, h, :], [r_kd, r_vnew])
        yield
        for grp in range(2):
            gs = slice(4 * grp, 4 * grp + 4)
            pb, r_pb = banks[2 + grp]
            S.op("dve", lambda e, pb=pb, gs=gs: e.tensor_tensor(out=S32_t[:, gs, :], in0=S32_t[:, gs, :], in1=b4(pb), op=ALU.add),
                 reads=[r_S32, r_pb], writes=[r_S32])
        S.op("act", lambda e: e.activation(out=Sbf_t, in_=S32_t, func=AF.Copy), reads=[r_S32], writes=[r_Sbf])
        yield

    import os as _os
    stop_after = int(_os.environ.get("B_UNITS", len(units)))
    pre_cut = int(_os.environ.get("B_PRE_CUT", 10000))
    do_seq = int(_os.environ.get("B_SEQ", 1))
    _pre = pre
    _seq = seq

    def pre(ui):
        for n_, _ in enumerate(_pre(ui)):
            if n_ + 1 >= pre_cut:
                return
            yield

    def seq(ui):
        if not do_seq:
            return
        yield from _seq(ui)
    loads(0)
    _drive(pre(0))
    for ui in range(stop_after):
        if ui + 1 < stop_after:
            loads(ui + 1)
            _drive(pre(ui + 1), seq(ui))
        else:
            _drive(seq(ui))
    k.b_state = (S32, Sbf)
    S.barrier()
    A.release()

def phase_c(k):
    S, A, nc, ins = k.S, k.A, k.nc, k.ins
    of_s, r_of_s = k.scr["of_s"]
    ob_s, r_ob_s = k.scr["ob_s"]
    zs_s, r_zs_s = k.scr["zs_s"]
    qmT_s, r_qmT_s = k.scr["qmT_s"]
    kmT_s, r_kmT_s = k.scr["kmT_s"]
    vm_s, r_vm_s = k.scr["vm_s"]
    yaT_s, r_yaT_s = k.scratch("yaT_s", [H, 128, TX], BF16)
    ybT_s, r_ybT_s = k.scratch("ybT_s", [H, 128, TX], BF16)
    A.mark()
    dng, r_dng = k.tile([128, 128], F32, "dng")
    k.load(dng, r_dng, ins["dng_rep"])
    NB = 2
    of_t = [k.tile([128, 8, 128], F32, "of") for _ in range(NB)]
    ob_t = [k.tile([128, 8, 128], F32, "ob") for _ in range(NB)]
    z_t = [k.tile([128, 8, 128], BF16, "z") for _ in range(NB)]
    sq, r_sq = k.tile([128, 8, 128], F32, "sq")
    ss8, r_ss8 = k.tile([128, 8], F32, "ss8")
    yb, r_yb = k.tile([128, 8, 128], BF16, "yb")
    yT = [k.tile([128, 8, 128], BF16, "yT") for _ in range(NB)]

    def c1_loads(xi):
        b = xi % NB
        rows = slice(xi * 128, (xi + 1) * 128)
        k.load(of_t[b][0], of_t[b][1], of_s[rows, :].rearrange("p (h d) -> p h d", h=8), r_of_s)
        k.load(ob_t[b][0], ob_t[b][1], ob_s[rows, :].rearrange("p (h d) -> p h d", h=8), r_ob_s, q="act")
        k.load(z_t[b][0], z_t[b][1], zs_s[rows, :].rearrange("p (h d) -> p h d", h=8), r_zs_s)

    c1_loads(0)
    for xi in range(32):
        b = xi % NB
        if xi + 1 < 32:
            c1_loads(xi + 1)
        o_, r_o = of_t[b]
        ob_, r_ob = ob_t[b]
        z_, r_z = z_t[b]
        S.op("dve", lambda e, o_=o_, ob_=ob_: e.tensor_tensor(out=o_, in0=o_, in1=ob_, op=ALU.add), reads=[r_o, r_ob], writes=[r_o])
        S.op("pool", lambda e, o_=o_: e.tensor_tensor(out=sq, in0=o_, in1=o_, op=ALU.mult), reads=[r_o], writes=[r_sq])
        S.op("dve", lambda e: e.tensor_reduce(out=ss8, in_=sq, axis=AX.X, op=ALU.add), reads=[r_sq], writes=[r_ss8])
        _rstd(k, ss8, r_ss8, 128, ss8, r_ss8)
        S.op("dve", lambda e, o_=o_: e.tensor_tensor(out=o_, in0=o_, in1=ss8.unsqueeze(2).to_broadcast([128, 8, 128]), op=ALU.mult),
             reads=[r_o, r_ss8], writes=[r_o])
        S.op("pool", lambda e, o_=o_: e.tensor_tensor(out=o_, in0=o_, in1=dng.unsqueeze(1).to_broadcast([128, 8, 128]), op=ALU.mult),
             reads=[r_o, r_dng], writes=[r_o])
        S.op("pool", lambda e, o_=o_, z_=z_: e.tensor_tensor(out=yb, in0=o_, in1=z_, op=ALU.mult), reads=[r_o, r_z], writes=[r_yb])
        pb, r_pb = _nb(k, BF16)
        pv = pb.rearrange("p (a b) -> p a b", b=128)

        def tr(e, pv=pv):
            for j in range(8):
                ins_ = e.transpose(out=pv[:, j, :], in_=yb[:, j, :], identity=k.ident_b)
            return ins_
        S.op("pe", tr, reads=[r_yb, k.r_ident_b], writes=[r_pb])
        yT_t, r_yT = yT[b]
        S.op("act", lambda e, pv=pv, yT_t=yT_t: e.activation(out=yT_t, in_=pv, func=AF.Copy), reads=[r_pb], writes=[r_yT])
        k.store(yaT_s[:, :, xi * 128:(xi + 1) * 128].rearrange("h d t -> d h t"), r_yaT_s, yT_t, r_yT)
    S.barrier()
    A.release()
    A.mark()
    Kn = [k.tile([128, T], BF16, "Kn") for _ in range(2)]
    Kr = [k.tile([64, T], BF16, "Kr") for _ in range(2)]
    Vh = [k.tile([128, NT, 128], BF16, "Vh") for _ in range(2)]
    Qn = [k.tile([128, 512], BF16, "Qn") for _ in range(2)]
    Qr = [k.tile([64, 512], BF16, "Qr") for _ in range(2)]
    NP = 4
    PT = [k.tile([128, 512], BF16, "PT") for _ in range(NP)]
    rinv, r_rinv = k.tile([128, 512], F32, "rinv")
    yo = [k.tile([128, 512], BF16, "yo") for _ in range(2)]
    vmv = vm_s.rearrange("(n p) c -> p n c", p=128)

    def head_loads(h):
        b = h % 2
        k.load(Kn[b][0], Kn[b][1], kmT_s[h, 0:128, :], r_kmT_s)
        k.load(Kr[b][0], Kr[b][1], kmT_s[h, 128:192, :], r_kmT_s, q="act")
        k.load(Vh[b][0], Vh[b][1], vmv[:, :, h * 128:(h + 1) * 128], r_vm_s)

    def q_loads(h, qg):
        b = (h * 8 + qg) % 2
        k.load(Qn[b][0], Qn[b][1], qmT_s[h, 0:128, qg * 512:(qg + 1) * 512], r_qmT_s)
        k.load(Qr[b][0], Qr[b][1], qmT_s[h, 128:192, qg * 512:(qg + 1) * 512], r_qmT_s, q="act")

    head_loads(0)
    q_loads(0, 0)
    sbank = [0]
    it = 0
    for h in range(H):
        if h + 1 < H:
            head_loads(h + 1)
        Kn_t, r_Kn = Kn[h % 2]
        Kr_t, r_Kr = Kr[h % 2]
        V_t, r_V = Vh[h % 2]
        for qg in range(8):
            gi = h * 8 + qg
            nxt = gi + 1
            if nxt < H * 8:
                q_loads(nxt // 8, nxt % 8)
            Qn_t, r_Qn = Qn[gi % 2]
            Qr_t, r_Qr = Qr[gi % 2]
            po, r_po = k.bank(4 + gi % 2)
            pr, r_pr = k.bank(6 + gi % 2)
            sb = {}

            def emit_s(kt):
                bnk = sbank[0]
                sbank[0] = (bnk + 1) % 4
                ps_, r_ps = k.bank(bnk)

                def f(e, ps_=ps_, kt=kt, Kn_t=Kn_t, Kr_t=Kr_t, Qn_t=Qn_t, Qr_t=Qr_t):
                    e.matmul(ps_, lhsT=Kn_t[:, kt * 128:(kt + 1) * 128], rhs=Qn_t, start=True, stop=False)
                    return e.matmul(ps_, lhsT=Kr_t[:, kt * 128:(kt + 1) * 128], rhs=Qr_t, start=False, stop=True)
                S.op("pe", f, reads=[r_Kn, r_Kr, r_Qn, r_Qr], writes=[r_ps])
                pt_, r_pt = PT[kt % NP]
                S.op("act", lambda e, ps_=ps_, pt_=pt_: e.activation(out=pt_, in_=ps_, func=AF.Exp), reads=[r_ps], writes=[r_pt])
                sb[kt] = (pt_, r_pt)

            def emit_pv(kt):
                pt_, r_pt = sb.pop(kt)

                def f(e, pt_=pt_, kt=kt, po=po, pr=pr, V_t=V_t):
                    e.matmul(po, lhsT=V_t[:, kt, :], rhs=pt_, start=(kt == 0), stop=(kt == NT - 1))
                    return e.matmul(pr, lhsT=k.ones_b, rhs=pt_, start=(kt == 0), stop=(kt == NT - 1))
                S.op("pe", f, reads=[r_V, r_pt, k.r_ones_b], writes=[r_po, r_pr])

            LOOK = 2
            for kt in range(min(LOOK, NT)):
                emit_s(kt)
            for kt in range(NT):
                if kt + LOOK < NT:
                    emit_s(kt + LOOK)
                emit_pv(kt)
            S.op("dve", lambda e, pr=pr: e.reciprocal(out=rinv, in_=pr), reads=[r_pr], writes=[r_rinv])
            yo_t, r_yo = yo[gi % 2]
            S.op("dve", lambda e, po=po, yo_t=yo_t: e.tensor_tensor(out=yo_t, in0=po, in1=rinv, op=ALU.mult), reads=[r_po, r_rinv], writes=[r_yo])
            k.store(ybT_s[h, :, qg * 512:(qg + 1) * 512], r_ybT_s, yo_t, r_yo)
    S.barrier()
    A.release()

def phase_d(k):
    S, A, nc, ins = k.S, k.A, k.nc, k.ins
    yaT_s, r_yaT_s = k.scr["yaT_s"]
    ybT_s, r_ybT_s = k.scr["ybT_s"]
    sgT_s, r_sgT_s = k.scr["sgT_s"]
    xmid_s, r_xmid_s = k.scratch("xmid_s", [TX, D], F32)
    h2_s, r_h2_s = k.scratch("h2_s", [TX, D], BF16)
    aff_s, r_aff_s = k.scratch("aff_s", [TX, 16], F32)
    affT_s, r_affT_s = k.scratch("affT_s", [16, TX], F32)
    A.mark()
    woa, r_woa = k.tile([128, 8, D], BF16, "woa")
    wob, r_wob = k.tile([128, 8, D], BF16, "wob")
    wo, r_wo = k.tile([128, 8, D], BF16, "wo")
    rw, r_rw = k.tile([128, 8, 16], F32, "rw")
    for (dst, rdst, nm) in ((woa, r_woa, "w_out_a"), (wob, r_wob, "w_out_b"), (wo, r_wo, "w_o")):
        src = ins[nm].rearrange("(kc p) n -> p kc n", p=128)
        for hf in range(2):
            k.wload(dst[:, 4 * hf:4 * hf + 4, :], rdst, src[:, 4 * hf:4 * hf + 4, :])
    k.load(rw, r_rw, ins["router_w"].rearrange("(kc p) n -> p kc n", p=128))
    NB = 2
    yaT = [k.tile([128, 8, 512], BF16, "yaT") for _ in range(NB)]
    ybT = [k.tile([128, 8, 512], BF16, "ybT") for _ in range(NB)]
    gA = [k.tile([128, 8, 512], BF16, "gA") for _ in range(NB)]
    gB = [k.tile([128, 8, 512], BF16, "gB") for _ in range(NB)]
    mg, r_mg = k.tile([128, 8, 512], BF16, "mg")
    t1 = [k.tile([128, 512], F32, "t1") for _ in range(2)]
    t2 = [k.tile([128, 512], F32, "t2") for _ in range(2)]
    xt = [k.tile([128, D], F32, "xt") for _ in range(NB)]
    xm = [k.tile([128, D], F32, "xm") for _ in range(NB)]
    junk, r_junk = k.tile([128, D], BF16, "junk")
    ssd = [k.tile([128, 8], F32, "ssd") for _ in range(NB)]
    h2f, r_h2f = k.tile([128, D], F32, "h2f")
    h2b = [k.tile([128, D], BF16, "h2b") for _ in range(NB)]
    h2T, r_h2T = k.tile([128, 8, 128], F32, "h2T")
    ex = [k.tile([128, 16], F32, "ex") for _ in range(NB)]
    affT = [k.tile([16, 128], F32, "affT") for _ in range(NB)]

    def g_loads(g):
        b = g % NB
        cols = slice(g * 512, (g + 1) * 512)
        k.load(yaT[b][0], yaT[b][1], yaT_s[:, :, cols].rearrange("h d t -> d h t"), r_yaT_s)
        k.load(ybT[b][0], ybT[b][1], ybT_s[:, :, cols].rearrange("h d t -> d h t"), r_ybT_s, q="act")
        k.load(gA[b][0], gA[b][1], sgT_s[0:8, :, cols].rearrange("h d t -> d h t"), r_sgT_s)
        k.load(gB[b][0], gB[b][1], sgT_s[8:16, :, cols].rearrange("h d t -> d h t"), r_sgT_s, q="act")

    g_loads(0)
    for g in range(8):
        b = g % NB
        if g + 1 < 8:
            g_loads(g + 1)
        ya_, r_ya = yaT[b]
        yb_, r_yb = ybT[b]
        gA_, r_gA = gA[b]
        gB_, r_gB = gB[b]
        for oc in range(8):
            pa, r_pa = _nb(k)
            pbk, r_pbk = _nb(k)

            def mma(e, pa=pa, oc=oc, ya_=ya_):
                for kc in range(8):
                    ins_ = e.matmul(pa, lhsT=woa[:, kc, oc * 128:(oc + 1) * 128], rhs=ya_[:, kc, :], start=(kc == 0), stop=(kc == 7))
                return ins_
            S.op("pe", mma, reads=[r_woa, r_ya], writes=[r_pa])

            def mmb(e, pbk=pbk, oc=oc, yb_=yb_):
                for kc in range(8):
                    ins_ = e.matmul(pbk, lhsT=wob[:, kc, oc * 128:(oc + 1) * 128], rhs=yb_[:, kc, :], start=(kc == 0), stop=(kc == 7))
                return ins_
            S.op("pe", mmb, reads=[r_wob, r_yb], writes=[r_pbk])
            t1_, r_t1 = t1[oc % 2]
            t2_, r_t2 = t2[oc % 2]
            S.op("dve", lambda e, pa=pa, oc=oc, t1_=t1_, gA_=gA_: e.tensor_tensor(out=t1_, in0=pa, in1=gA_[:, oc, :], op=ALU.mult), reads=[r_pa, r_gA], writes=[r_t1])
            S.op("dve", lambda e, pbk=pbk, oc=oc, t2_=t2_, gB_=gB_: e.tensor_tensor(out=t2_, in0=pbk, in1=gB_[:, oc, :], op=ALU.mult), reads=[r_pbk, r_gB], writes=[r_t2])
            S.op("pool", lambda e, oc=oc, t1_=t1_, t2_=t2_: e.tensor_tensor(out=mg[:, oc, :], in0=t1_, in1=t2_, op=ALU.add), reads=[r_t1, r_t2], writes=[r_mg])
        for tt in range(4):
            ti = g * 4 + tt
            tb = ti % NB
            rows = slice(ti * 128, (ti + 1) * 128)
            x_, r_x = xt[tb]
            xm_, r_xm = xm[tb]
            ss_, r_ss = ssd[tb]
            k.load(x_, r_x, ins["x"][rows, :])
            for hf in range(2):
                pm, r_pm = _nb(k)

                def mmo(e, pm=pm, hf=hf, tt=tt):
                    for kc in range(8):
                        ins_ = e.matmul(pm, lhsT=mg[:, kc, tt * 128:(tt + 1) * 128], rhs=wo[:, kc, hf * 512:(hf + 1) * 512], start=(kc == 0), stop=(kc == 7))
                    return ins_
                S.op("pe", mmo, reads=[r_mg, r_wo], writes=[r_pm])
                S.op("dve", lambda e, pm=pm, hf=hf, xm_=xm_: e.tensor_tensor(out=xm_[:, hf * 512:(hf + 1) * 512], in0=pm, in1=k.gate1_row[:, hf * 512:(hf + 1) * 512], op=ALU.mult),
                     reads=[r_pm, k.r_gate1], writes=[r_xm])
            S.op("pool", lambda e, xm_=xm_, x_=x_: e.tensor_tensor(out=xm_, in0=xm_, in1=x_, op=ALU.add), reads=[r_xm, r_x], writes=[r_xm])
            k.store(xmid_s[rows, :], r_xmid_s, xm_, r_xm)
            S.op("act", lambda e, xm_=xm_, ss_=ss_: e.activation(out=junk, in_=xm_, func=AF.Square, accum_out=ss_[:, 0:1]), reads=[r_xm], writes=[r_junk, r_ss])
            _rstd(k, ss_[:, 0:1], r_ss, D, ss_[:, 1:2], r_ss)
            S.op("act", lambda e, xm_=xm_, ss_=ss_: e.activation(out=h2f, in_=xm_, func=AF.Copy, scale=ss_[:, 1:2]), reads=[r_xm, r_ss], writes=[r_h2f])
            S.op("pool", lambda e: e.tensor_tensor(out=h2f, in0=h2f, in1=k.s2_row, op=ALU.mult), reads=[r_h2f, k.r_s2row], writes=[r_h2f])
            S.op("pool", lambda e: e.tensor_tensor(out=h2f, in0=h2f, in1=k.shift2_row, op=ALU.add), reads=[r_h2f, k.r_shift2], writes=[r_h2f])
            h2b_, r_h2b = h2b[tb]
            S.op("act", lambda e, h2b_=h2b_: e.activation(out=h2b_, in_=h2f, func=AF.Copy), reads=[r_h2f], writes=[r_h2b])
            k.store(h2_s[rows, :], r_h2_s, h2b_, r_h2b)
            for hf in range(2):
                pt, r_pt = _nb(k)
                pv = pt.rearrange("p (a b) -> p a b", b=128)

                def trh(e, pv=pv, hf=hf):
                    for j in range(4):
                        ins_ = e.transpose(out=pv[:, j, :], in_=h2f[:, (hf * 4 + j) * 128:(hf * 4 + j + 1) * 128], identity=k.ident_f)
                    return ins_
                S.op("pe", trh, reads=[r_h2f, k.r_ident_f], writes=[r_pt])
                if hf == 0:
                    S.op("act", lambda e, pv=pv: e.activation(out=h2T[:, 0:4, :], in_=pv, func=AF.Copy), reads=[r_pt], writes=[r_h2T])
                else:
                    S.op("dve", lambda e, pv=pv: e.tensor_copy(out=h2T[:, 4:8, :], in_=pv), reads=[r_pt], writes=[r_h2T])
            pl, r_pl = _nb(k)

            def mml(e, pl=pl):
                for kc in range(8):
                    ins_ = e.matmul(pl[:, 0:16], lhsT=h2T[:, kc, :], rhs=rw[:, kc, :], start=(kc == 0), stop=(kc == 7))
                return ins_
            S.op("pe", mml, reads=[r_h2T, r_rw], writes=[r_pl])
            ex_, r_ex = ex[tb]
            S.op("dve", lambda e, pl=pl, ss_=ss_: e.tensor_reduce(out=ss_[:, 2:3], in_=pl[:, 0:16], axis=AX.X, op=ALU.max), reads=[r_pl], writes=[r_ss])
            S.op("dve", lambda e, ss_=ss_: e.tensor_scalar(out=ss_[:, 3:4], in0=ss_[:, 2:3], scalar1=-1.0, scalar2=None, op0=ALU.mult), reads=[r_ss], writes=[r_ss])
            S.op("act", lambda e, pl=pl, ss_=ss_, ex_=ex_: e.activation(out=ex_, in_=pl[:, 0:16], func=AF.Exp, bias=ss_[:, 3:4], accum_out=ss_[:, 4:5]),
                 reads=[r_pl, r_ss], writes=[r_ex, r_ss])
            S.op("dve", lambda e, ss_=ss_: e.reciprocal(out=ss_[:, 5:6], in_=ss_[:, 4:5]), reads=[r_ss], writes=[r_ss])
            S.op("dve", lambda e, ss_=ss_, ex_=ex_: e.tensor_scalar(out=ex_, in0=ex_, scalar1=ss_[:, 5:6], scalar2=None, op0=ALU.mult), reads=[r_ex, r_ss], writes=[r_ex])
            k.store(aff_s[rows, :], r_aff_s, ex_, r_ex)
            pt2, r_pt2 = _nb(k)
            S.op("pe", lambda e, pt2=pt2, ex_=ex_: e.transpose(out=pt2[0:16, 0:128], in_=ex_, identity=k.ident_f), reads=[r_ex, k.r_ident_f], writes=[r_pt2])
            aT_, r_aT = affT[tb]
            S.op("act", lambda e, pt2=pt2, aT_=aT_: e.activation(out=aT_, in_=pt2[0:16, 0:128], func=AF.Copy), reads=[r_pt2], writes=[r_aT])
            k.store(affT_s[:, rows], r_affT_s, aT_, r_aT)
    S.barrier()
    A.release()

NE = 16
CAP = 512
FF = 1408
NFC = 11


def phase_e(k):
    S, A, nc, ins = k.S, k.A, k.nc, k.ins
    aff_s, r_aff_s = k.scr["aff_s"]
    affT_s, r_affT_s = k.scr["affT_s"]
    h2_s, r_h2_s = k.scr["h2_s"]
    xmid_s, r_xmid_s = k.scr["xmid_s"]
    posmT_s, r_posmT_s = k.scratch("posmT_s", [NE, TX], F32)
    xeT_s, r_xeT_s = k.scratch("xeT_s", [NE, 8, 128, CAP], BF16)
    gc_s, r_gc_s = k.scratch("gc_s", [NE, 128, 4], F32)
    ye_s, r_ye_s = k.scratch("ye_s", [NE, CAP, D], BF16)
    A.off = k.off_after_gate2
    A.mark()
    cst, r_cst = k.tile([128, 1024], F32, "cst")
    k.load(cst, r_cst, ins["consts"])
    blk, r_blk = k.tile([128, 128], F32, "blk")
    k.load(blk, r_blk, ins["moe_blk"])
    sel8, r_sel8 = k.tile([128, 16], F32, "sel8")
    k.load(sel8, r_sel8, ins["moe_sel8"])
    tris, r_tris = k.tile([128, 128], BF16, "tris")
    k.wload(tris, r_tris, ins["moe_tris"])
    iota_c = cst[:, 0:512]
    A.mark()
    A8, r_A8 = k.tile([128, 512], F32, "A8")
    k.load(A8, r_A8, affT_s.rearrange("e (s t) -> (e s) t", s=8), r_affT_s)
    junk, r_junk = k.tile([128, 512], F32, "junk")
    sc, r_sc = k.tile([128, 16], F32, "sc")
    S.op("pool", lambda e: e.memset(sc, 0.0), writes=[r_sc])
    S.op("pool", lambda e: e.memset(sc[:, 1:2], 1.0), reads=[r_sc], writes=[r_sc])
    for it in range(30):
        S.op("dve", lambda e: e.tensor_tensor(out=sc[:, 2:3], in0=sc[:, 0:1], in1=sc[:, 1:2], op=ALU.add), reads=[r_sc], writes=[r_sc])
        S.op("dve", lambda e: e.tensor_scalar(out=sc[:, 2:3], in0=sc[:, 2:3], scalar1=0.5, scalar2=None, op0=ALU.mult), reads=[r_sc], writes=[r_sc])
        S.op("dve", lambda e: e.tensor_scalar(out=junk, in0=A8, scalar1=sc[:, 2:3], scalar2=0.0, op0=ALU.is_ge, op1=ALU.add, accum_out=sc[:, 3:4]),
             reads=[r_A8, r_sc], writes=[r_junk, r_sc])
        pb, r_pb = _nb(k)
        S.op("pe", lambda e, pb=pb: e.matmul(pb[:, 0:1], lhsT=blk, rhs=sc[:, 3:4], start=True, stop=True), reads=[r_blk, r_sc], writes=[r_pb])
        S.op("dve", lambda e, pb=pb: e.tensor_scalar(out=sc[:, 4:5], in0=pb[:, 0:1], scalar1=CAP - 0.5, scalar2=None, op0=ALU.is_ge), reads=[r_pb], writes=[r_sc])
        S.op("dve", lambda e: e.tensor_scalar(out=sc[:, 5:6], in0=sc[:, 4:5], scalar1=-1.0, scalar2=1.0, op0=ALU.mult, op1=ALU.add), reads=[r_sc], writes=[r_sc])
        S.op("dve", lambda e: e.tensor_tensor(out=sc[:, 6:7], in0=sc[:, 2:3], in1=sc[:, 0:1], op=ALU.subtract), reads=[r_sc], writes=[r_sc])
        S.op("dve", lambda e: e.tensor_tensor(out=sc[:, 7:8], in0=sc[:, 1:2], in1=sc[:, 2:3], op=ALU.subtract), reads=[r_sc], writes=[r_sc])
        S.op("dve", lambda e: e.scalar_tensor_tensor(out=sc[:, 0:1], in0=sc[:, 6:7], scalar=sc[:, 4:5], in1=sc[:, 0:1], op0=ALU.mult, op1=ALU.add), reads=[r_sc], writes=[r_sc])
        S.op("dve", lambda e: e.scalar_tensor_tensor(out=sc[:, 1:2], in0=sc[:, 7:8], scalar=sc[:, 4:5], in1=sc[:, 2:3], op0=ALU.mult, op1=ALU.add), reads=[r_sc], writes=[r_sc])
        S.op("dve", lambda e: e.memset(sc[:, 3:4], 0.0), reads=[r_sc], writes=[r_sc])
    thrrep, r_thrrep = k.tile([128, 128], F32, "thrrep")
    S.op("dve", lambda e: e.tensor_copy(out=thrrep, in_=sc[:, 0:1].to_broadcast([128, 128])), reads=[r_sc], writes=[r_thrrep])
    pb, r_pb = _nb(k)
    S.op("pe", lambda e, pb=pb: e.matmul(pb[:, 0:16], lhsT=thrrep, rhs=sel8, start=True, stop=True), reads=[r_thrrep, r_sel8], writes=[r_pb])
    thr_row, r_thr = k.tile([128, 16], F32, "thr_row")
    S.op("act", lambda e, pb=pb: e.activation(out=thr_row, in_=pb[:, 0:16], func=AF.Copy), reads=[r_pb], writes=[r_thr])
    import os as _os
    if _os.environ.get("E_DBG"):
        k.dump("sc", sc, r_sc, [128, 16])
        k.dump("thr_row", thr_row, r_thr, [128, 16])
        k.dump("A8", A8, r_A8, [128, 512])
        S.barrier()
        A.release()
        A.release()
        return
    aff, r_aff = k.tile([128, 32, 16], F32, "aff")
    k.load(aff, r_aff, aff_s.rearrange("(n p) e -> p n e", p=128), r_aff_s)
    maskf, r_maskf = k.tile([128, 32, 16], F32, "maskf")
    maskb, r_maskb = k.tile([128, 32, 16], BF16, "maskb")
    posm, r_posm = k.tile([128, 32, 16], F32, "posm")
    parts, r_parts = k.tile([128, 32, 16, 3], BF16, "parts")
    rem, r_rem = k.tile([128, 32, 16], F32, "rem")
    S.op("dve", lambda e: e.tensor_tensor(out=maskf, in0=aff, in1=thr_row.unsqueeze(1).to_broadcast([128, 32, 16]), op=ALU.is_ge),
         reads=[r_aff, r_thr], writes=[r_maskf])
    S.op("act", lambda e: e.activation(out=maskb, in_=maskf, func=AF.Copy), reads=[r_maskf], writes=[r_maskb])
    pp, r_pp = _nb(k)
    ppv = pp.rearrange("p (n e) -> p n e", e=16)

    def mmpos(e):
        for n in range(32):
            for m in range(n):
                e.matmul(ppv[:, n, :], lhsT=k.ones_b, rhs=maskb[:, m, :], start=(m == 0), stop=False)
            ins_ = e.matmul(ppv[:, n, :], lhsT=tris, rhs=maskb[:, n, :], start=(n == 0), stop=True)
        return ins_
    S.op("pe", mmpos, reads=[r_maskb, k.r_ones_b, r_tris], writes=[r_pp])
    S.op("dve", lambda e: e.scalar_tensor_tensor(out=posm, in0=ppv, scalar=1.0, in1=maskf, op0=ALU.add, op1=ALU.mult), reads=[r_pp, r_maskf], writes=[r_posm])
    S.op("dve", lambda e: e.tensor_scalar(out=posm, in0=posm, scalar1=-1.0, scalar2=None, op0=ALU.add), reads=[r_posm], writes=[r_posm])
    S.op("act", lambda e: e.activation(out=parts[:, :, :, 0], in_=aff, func=AF.Copy), reads=[r_aff], writes=[r_parts])
    S.op("dve", lambda e: e.tensor_tensor(out=rem, in0=aff, in1=parts[:, :, :, 0], op=ALU.subtract), reads=[r_aff, r_parts], writes=[r_rem])
    S.op("act", lambda e: e.activation(out=parts[:, :, :, 1], in_=rem, func=AF.Copy), reads=[r_rem], writes=[r_parts])
    S.op("dve", lambda e: e.tensor_tensor(out=rem, in0=rem, in1=parts[:, :, :, 1], op=ALU.subtract), reads=[r_rem, r_parts], writes=[r_rem])
    S.op("act", lambda e: e.activation(out=parts[:, :, :, 2], in_=rem, func=AF.Copy), reads=[r_rem], writes=[r_parts])
    pmTs = [k.tile([16, 512], F32, "pmT") for _ in range(2)]
    for g in range(8):
        pt, r_pt = _nb(k)

        def trp(e, pt=pt, g=g):
            for j in range(4):
                ins_ = e.transpose(out=pt[0:16, j * 128:(j + 1) * 128], in_=posm[:, g * 4 + j, :], identity=k.ident_f)
            return ins_
        S.op("pe", trp, reads=[r_posm, k.r_ident_f], writes=[r_pt])
        pmT, r_pmT = pmTs[g % 2]
        S.op("act", lambda e, pt=pt, pmT=pmT: e.activation(out=pmT, in_=pt[0:16, :], func=AF.Copy), reads=[r_pt], writes=[r_pmT])
        k.store(posmT_s[:, g * 512:(g + 1) * 512], r_posmT_s, pmT, r_pmT)
    h2, r_h2 = k.tile([128, 32, D], BF16, "h2")
    for q4 in range(4):
        k.load(h2[:, q4 * 8:(q4 + 1) * 8, :], r_h2, h2_s.rearrange("(n p) d -> p n d", p=128)[:, q4 * 8:(q4 + 1) * 8, :], r_h2_s, q=("sp" if q4 % 2 == 0 else "act"))
    Sel = [k.tile([128, 32, CAP], BF16, "Sel") for _ in range(2)]
    xeT = [k.tile([128, 8, CAP], BF16, "xeT") for _ in range(2)]
    gc3, r_gc3 = k.tile([128, 4, 3], F32, "gc3")
    gcs = [k.tile([128, 4], F32, "gcs") for _ in range(2)]
    for ex in range(NE):
        Sel_t, r_Sel = Sel[ex % 2]
        xe_t, r_xe = xeT[ex % 2]
        gcs_t, r_gcs = gcs[ex % 2]

        def bsel(e, Sel_t=Sel_t, ex=ex, par=0):
            for n in range(par, 32, 2):
                ins_ = e.tensor_scalar(out=Sel_t[:, n, :], in0=iota_c, scalar1=posm[:, n, ex:ex + 1], scalar2=None, op0=ALU.is_equal)
            return ins_
        S.op("dve", lambda e, f=bsel: f(e, par=0), reads=[r_cst, r_posm], writes=[r_Sel])
        S.op("pool", lambda e, f=bsel: f(e, par=1), reads=[r_cst, r_posm], writes=[r_Sel])
        for kc in range(8):
            pg, r_pg = _nb(k)

            def mmg(e, pg=pg, kc=kc, Sel_t=Sel_t):
                for n in range(32):
                    ins_ = e.matmul(pg, lhsT=h2[:, n, kc * 128:(kc + 1) * 128], rhs=Sel_t[:, n, :], start=(n == 0), stop=(n == 31))
                return ins_
            S.op("pe", mmg, reads=[r_h2, r_Sel], writes=[r_pg])
            if kc % 2 == 0:
                S.op("act", lambda e, pg=pg, kc=kc, xe_t=xe_t: e.activation(out=xe_t[:, kc, :], in_=pg, func=AF.Copy), reads=[r_pg], writes=[r_xe])
            else:
                S.op("dve", lambda e, pg=pg, kc=kc, xe_t=xe_t: e.tensor_copy(out=xe_t[:, kc, :], in_=pg), reads=[r_pg], writes=[r_xe])
        k.store(xeT_s[ex].rearrange("kc p c -> p kc c"), r_xeT_s, xe_t, r_xe)
        pq, r_pq = _nb(k)

        def mmgate(e, pq=pq, Sel_t=Sel_t, ex=ex):
            for cc in range(4):
                for n in range(32):
                    ins_ = e.matmul(pq[:, cc * 4:cc * 4 + 3], lhsT=Sel_t[:, n, cc * 128:(cc + 1) * 128], rhs=parts[:, n, ex, :], start=(n == 0), stop=(n == 31))
            return ins_
        S.op("pe", mmgate, reads=[r_Sel, r_parts], writes=[r_pq])
        S.op("dve", lambda e, pq=pq, gcs_t=gcs_t: e.tensor_reduce(out=gcs_t, in_=pq[:, 0:16].rearrange("p (a b) -> p a b", b=4)[:, :, 0:3], axis=AX.X, op=ALU.add),
             reads=[r_pq], writes=[r_gcs])
        k.store(gc_s[ex], r_gc_s, gcs_t, r_gcs)
    S.barrier()
    A.release()
    A.mark()
    wg = [k.tile([128, 8, FF], BF16, "wg") for _ in range(2)]
    wu = [k.tile([128, 8, FF], BF16, "wu") for _ in range(2)]
    wd = [k.tile([128, NFC, D], BF16, "wd") for _ in range(2)]
    xe2 = [k.tile([128, 8, CAP], BF16, "xe2") for _ in range(2)]
    gc2 = [k.tile([128, 4], F32, "gc2") for _ in range(2)]
    hid, r_hid = k.tile([128, NFC, CAP], BF16, "hid")
    sg = [k.tile([128, CAP], F32, "sg") for _ in range(2)]
    ye = [k.tile([128, 4, D], BF16, "ye") for _ in range(2)]

    def w_loads(ex):
        b = ex % 2
        srcg = ins["w_gate"][ex].rearrange("(kc p) f -> p kc f", p=128)
        srcu = ins["w_up"][ex].rearrange("(kc p) f -> p kc f", p=128)
        srcd = ins["w_down"][ex].rearrange("(fc p) d -> p fc d", p=128)
        for j in range(4):
            k.wload(wg[b][0][:, 2 * j:2 * j + 2, :], wg[b][1], srcg[:, 2 * j:2 * j + 2, :])
        for j in range(4):
            k.wload(wu[b][0][:, 2 * j:2 * j + 2, :], wu[b][1], srcu[:, 2 * j:2 * j + 2, :])
        for (a0, a1) in ((0, 3), (3, 6), (6, 9), (9, 11)):
            k.wload(wd[b][0][:, a0:a1, :], wd[b][1], srcd[:, a0:a1, :])
        k.load(xe2[b][0], xe2[b][1], xeT_s[ex].rearrange("kc p c -> p kc c"), r_xeT_s)
        k.load(gc2[b][0], gc2[b][1], gc_s[ex], r_gc_s, q="act")

    w_loads(0)
    for ex in range(NE):
        b = ex % 2
        if ex + 1 < NE:
            w_loads(ex + 1)
        wg_t, r_wg = wg[b]
        wu_t, r_wu = wu[b]
        wd_t, r_wd = wd[b]
        xe_t, r_xe = xe2[b]
        gc_t, r_gc = gc2[b]
        ye_t, r_ye = ye[b]
        for fc in range(NFC):
            pg, r_pg = _nb(k)
            pu, r_pu = _nb(k)

            def mmG(e, pg=pg, fc=fc, wg_t=wg_t, xe_t=xe_t):
                for kc in range(8):
                    ins_ = e.matmul(pg, lhsT=wg_t[:, kc, fc * 128:(fc + 1) * 128], rhs=xe_t[:, kc, :], start=(kc == 0), stop=(kc == 7))
                return ins_
            S.op("pe", mmG, reads=[r_wg, r_xe], writes=[r_pg])

            def mmU(e, pu=pu, fc=fc, wu_t=wu_t, xe_t=xe_t):
                for kc in range(8):
                    ins_ = e.matmul(pu, lhsT=wu_t[:, kc, fc * 128:(fc + 1) * 128], rhs=xe_t[:, kc, :], start=(kc == 0), stop=(kc == 7))
                return ins_
            S.op("pe", mmU, reads=[r_wu, r_xe], writes=[r_pu])
            sg_t, r_sg = sg[fc % 2]
            S.op("act", lambda e, pg=pg, sg_t=sg_t: e.activation(out=sg_t, in_=pg, func=AF.Silu), reads=[r_pg], writes=[r_sg])
            S.op("dve", lambda e, pu=pu, sg_t=sg_t, fc=fc: e.tensor_tensor(out=hid[:, fc, :], in0=pu, in1=sg_t, op=ALU.mult), reads=[r_pu, r_sg], writes=[r_hid])
        for cc in range(4):
            for hf in range(2):
                pd, r_pd = _nb(k)

                def mmD(e, pd=pd, cc=cc, hf=hf, wd_t=wd_t):
                    for fc in range(NFC):
                        ins_ = e.matmul(pd, lhsT=hid[:, fc, cc * 128:(cc + 1) * 128], rhs=wd_t[:, fc, hf * 512:(hf + 1) * 512], start=(fc == 0), stop=(fc == NFC - 1))
                    return ins_
                S.op("pe", mmD, reads=[r_hid, r_wd], writes=[r_pd])
                S.op("act", lambda e, pd=pd, cc=cc, hf=hf, ye_t=ye_t, gc_t=gc_t: e.activation(out=ye_t[:, cc, hf * 512:(hf + 1) * 512], in_=pd, func=AF.Copy, scale=gc_t[:, cc:cc + 1]),
                     reads=[r_pd, r_gc], writes=[r_ye])
        k.store(ye_s[ex].rearrange("(cc p) d -> p cc d", p=128), r_ye_s, ye_t, r_ye)
    S.barrier()
    A.release()
    A.mark()
    acc, r_acc = k.tile([128, 32, 512], F32, "acc")
    posrow = [k.tile([128, TX], F32, "posrow") for _ in range(2)]
    SelT, r_SelT = k.tile([128, 4, TX], BF16, "SelT")
    yeb = [k.tile([128, 4, 512], BF16, "yeb") for _ in range(2)]
    iop, r_iop = k.tile([128, 4], F32, "iop")
    for cc in range(4):
        S.op("dve", lambda e, cc=cc: e.tensor_scalar(out=iop[:, cc:cc + 1], in0=cst[:, 512:513], scalar1=float(128 * cc), scalar2=None, op0=ALU.add),
             reads=[r_cst], writes=[r_iop])
    xm = [k.tile([128, 512], F32, "xm") for _ in range(2)]
    ot = [k.tile([128, 512], F32, "ot") for _ in range(2)]

    def eb_loads(dh, ex):
        b = (dh * NE + ex) % 2
        k.load(posrow[b][0], posrow[b][1], posmT_s[ex:ex + 1, :].broadcast_to([128, TX]), r_posmT_s)
        k.load(yeb[b][0], yeb[b][1], ye_s[ex].rearrange("(cc p) d -> p cc d", p=128)[:, :, dh * 512:(dh + 1) * 512], r_ye_s, q="act")

    eb_loads(0, 0)
    for dh in range(2):
        for ex in range(NE):
            gi = dh * NE + ex
            b = gi % 2
            if gi + 1 < 2 * NE:
                eb_loads((gi + 1) // NE, (gi + 1) % NE)
            pr_t, r_pr = posrow[b]
            ye_t, r_ye = yeb[b]
            for cc in range(4):
                eng = "dve" if cc % 2 == 0 else "pool"
                S.op(eng, lambda e, cc=cc, pr_t=pr_t: e.tensor_scalar(out=SelT[:, cc, :], in0=pr_t, scalar1=iop[:, cc:cc + 1], scalar2=None, op0=ALU.is_equal),
                     reads=[r_pr, r_iop], writes=[r_SelT])
            for n in range(32):
                ps_, r_ps = _nb(k)

                def mms(e, ps_=ps_, n=n, ye_t=ye_t):
                    for cc in range(4):
                        ins_ = e.matmul(ps_, lhsT=SelT[:, cc, n * 128:(n + 1) * 128], rhs=ye_t[:, cc, :], start=(cc == 0), stop=(cc == 3))
                    return ins_
                S.op("pe", mms, reads=[r_SelT, r_ye], writes=[r_ps])
                if ex == 0:
                    S.op("act", lambda e, ps_=ps_, n=n: e.activation(out=acc[:, n, :], in_=ps_, func=AF.Copy), reads=[r_ps], writes=[r_acc])
                else:
                    S.op("dve", lambda e, ps_=ps_, n=n: e.tensor_tensor(out=acc[:, n, :], in0=acc[:, n, :], in1=ps_, op=ALU.add), reads=[r_acc, r_ps], writes=[r_acc])
        for n in range(32):
            xm_t, r_xm = xm[n % 2]
            o_t, r_o = ot[n % 2]
            k.load(xm_t, r_xm, xmid_s[n * 128:(n + 1) * 128, dh * 512:(dh + 1) * 512], r_xmid_s)
            S.op("pool", lambda e, n=n, o_t=o_t, dh=dh: e.tensor_tensor(out=o_t, in0=acc[:, n, :], in1=k.gate2_row[:, dh * 512:(dh + 1) * 512], op=ALU.mult),
                 reads=[r_acc, k.r_gate2], writes=[r_o])
            S.op("pool", lambda e, o_t=o_t, xm_t=xm_t: e.tensor_tensor(out=o_t, in0=o_t, in1=xm_t, op=ALU.add), reads=[r_o, r_xm], writes=[r_o])
            k.store(k.out[n * 128:(n + 1) * 128, dh * 512:(dh + 1) * 512], k.out_res, o_t, r_o)
    S.barrier()
    A.release()
    A.release()

def _rope_tables():
    rows, gw = 64, 64
    row = np.repeat(np.arange(rows), gw).astype(np.float32)
    col = np.tile(np.arange(gw), rows).astype(np.float32)
    n_freq = 16
    inv_freq = (10000.0 ** (-np.arange(n_freq, dtype=np.float32) / n_freq)).astype(np.float32)
    ang_r = row[:, None] * inv_freq
    ang_c = col[:, None] * inv_freq
    ang = np.concatenate([ang_r, ang_r, ang_c, ang_c], axis=-1).astype(np.float32)
    cos = np.cos(ang).astype(np.float32)
    sin = np.sin(ang).astype(np.float32)
    sgn = np.concatenate([-np.ones(16), np.ones(16), -np.ones(16), np.ones(16)]).astype(np.float32)
    return cos, (sin * sgn).astype(np.float32)


def prep_shared(inp):
    f = np.float32
    sh = {}
    sh["ada_w"] = np.ascontiguousarray(inp["ada_w"][0])
    sh["ada_b_row"] = np.ascontiguousarray(inp["ada_b"][0][None, :])
    sh["ada_bT"] = np.ascontiguousarray(inp["ada_b"][0].reshape(48, 128).T)
    sh["g1T"] = np.ascontiguousarray(inp["norm1_g"][0].reshape(8, 128).T)
    sh["g2T"] = np.ascontiguousarray(inp["norm2_g"][0].reshape(8, 128).T)
    sh["g2_rep"] = np.ascontiguousarray(np.broadcast_to(inp["norm2_g"][0].reshape(1, 1024), (128, 1024)))
    sh["w_in"] = np.ascontiguousarray(inp["w_in"][0])
    sh["convT"] = np.ascontiguousarray(inp["conv_w"][0].T.reshape(24, 128, 5).transpose(1, 0, 2))
    sh["alog_rep"] = np.ascontiguousarray(np.broadcast_to(inp["a_log"][0].reshape(1, 16), (128, 16)))
    sh["dtb_rep"] = np.ascontiguousarray(np.broadcast_to(inp["dt_bias"][0].reshape(1, 16), (128, 16)))
    sh["dng_rep"] = np.ascontiguousarray(np.broadcast_to(inp["dn_norm_g"][0].reshape(1, 128), (128, 128)))
    sh["gqaT"] = np.ascontiguousarray(inp["q_a_norm_g"][0].reshape(3, 128).T)
    sh["w_uq"] = np.ascontiguousarray(inp["w_uq"][0])
    sh["gkvaT"] = np.ascontiguousarray(inp["kv_a_norm_g"][0].reshape(2, 128).T)
    sh["w_ukv"] = np.ascontiguousarray(inp["w_ukv"][0])
    sh["gq_rep"] = np.ascontiguousarray(np.broadcast_to(inp["q_norm_g"][0].reshape(1, 192), (128, 192)))
    sh["gk_rep"] = np.ascontiguousarray(np.broadcast_to(inp["k_norm_g"][0].reshape(1, 192), (128, 192)))
    sh["w_out_a"] = np.ascontiguousarray(inp["w_out_a"][0])
    sh["w_out_b"] = np.ascontiguousarray(inp["w_out_b"][0])
    sh["w_o"] = np.ascontiguousarray(inp["w_o"][0])
    sh["router_w"] = np.ascontiguousarray(inp["router_w"][0])
    sh["w_gate"] = np.ascontiguousarray(inp["w_gate"][0])
    sh["w_up"] = np.ascontiguousarray(inp["w_up"][0])
    sh["w_down"] = np.ascontiguousarray(inp["w_down"][0])
    cos, sinS = _rope_tables()
    sh["rope_cs"] = np.ascontiguousarray(np.concatenate([cos, sinS], axis=1))
    sh["ident"] = np.eye(128, dtype=f)
    consts = np.zeros((128, 1024), f)
    consts[:, 0:512] = np.arange(512, dtype=f)[None, :]
    consts[:, 512] = np.arange(128, dtype=f)
    sh["consts"] = consts
    pp_ = np.arange(128)
    sh["moe_blk"] = (pp_[:, None] // 8 == pp_[None, :] // 8).astype(f)
    s8 = np.zeros((128, 16), f)
    s8[np.arange(16) * 8, np.arange(16)] = 1.0
    sh["moe_sel8"] = s8
    sh["moe_tris"] = (pp_[:, None] < pp_[None, :]).astype(f)
    ii = np.arange(128)
    P, Fr = ii[:, None], ii[None, :]
    NEGV = -30000.0
    mk = np.zeros((128, 9, 128), f)
    mk[:, 0] = (P <= Fr)
    mk[:, 1] = (P >= Fr)
    mk[:, 2] = np.where(P > Fr, 0.0, NEGV)
    mk[:, 3] = np.where(P < Fr, 0.0, NEGV)
    mk[:, 4] = np.where(Fr >= P, 0.0, NEGV)
    mk[:, 5] = np.where(Fr <= P, 0.0, NEGV)
    mk[:, 6] = (P // 32 == Fr // 32)
    mk[:, 7] = (P // 64 == Fr // 64) & (P // 32 != Fr // 32)
    mk[:, 8] = (P // 64 != Fr // 64)
    sh["dn_masks"] = mk
    es = np.zeros((64, 2, 8, 128), f)
    for hh in range(8):
        es[hh, 0, hh, :] = 1.0
        es[32 + hh, 0, hh, :] = 1.0
    es[:, 1] = -es[:, 0]
    sh["dn_esel"] = es
    li = np.zeros((64, 128), f)
    li[32:40] = 1.0
    sh["dn_linit"] = li
    return sh


def prep_core(inp, sh, b):
    m = dict(sh)
    m["x"] = np.ascontiguousarray(inp["x"][b])
    m["ctx"] = np.ascontiguousarray(inp["ctx"][b])
    cc = np.stack([inp["c"][b], inp["c_ctx"]], axis=-1).astype(np.float32)
    m["c2"] = np.ascontiguousarray(cc.reshape(8, 128, 2).transpose(1, 0, 2))
    return m

PHASES = ["a0", "a1", "a2", "b", "c", "d", "e"]


def build(upto="e", dbg=(), dumps=()):
    k = K(dbg=dbg)
    declare_inputs(k)
    setup_consts(k)
    k.dump_list = []

    def dump(name, ap, res, shape):
        t = k.nc.dram_tensor("dbg_" + name, list(shape), ap.dtype, kind="ExternalOutput").ap()
        k.store(t, None, ap, res)
        k.dump_list.append("dbg_" + name)
    k.dump = dump
    k.dumps = set(dumps)
    fns = {"a0": phase_a0}
    for nm in ("a1", "a2", "b", "c", "d", "e"):
        f = globals().get("phase_" + nm)
        if f is not None:
            fns[nm] = f
    for ph in PHASES:
        if ph in fns:
            fns[ph](k)
        if ph == upto:
            break
    k.S.emit()
    return k


_CACHE = {}


def kernel(**inputs):
    inp = {kk: np.asarray(v) for kk, v in inputs.items()}
    sh = prep_shared(inp)
    in_maps = [prep_core(inp, sh, b) for b in range(8)]
    k = build()
    res = run_bass_kernel_spmd(k.nc, in_maps, core_ids=list(range(8)))
    out = np.stack([np.asarray(r["out"]) for r in res.results], axis=0).astype(np.float32)
    return out
```

```python
import numpy as np
import concourse.bass as bass
import concourse.mybir as mybir
from concourse.bass_utils import run_bass_kernel_spmd

F32 = mybir.dt.float32
BF16 = mybir.dt.bfloat16
I32 = mybir.dt.int32
AF = mybir.ActivationFunctionType
ALU = mybir.AluOpType
AX = mybir.AxisListType

ENGS = ("pe", "act", "dve", "pool", "sp")
EPOCH = 12000


class Res:
    __slots__ = ("name", "w", "rs", "multi", "ws", "excl")

    def __init__(self, name="", multi=False, excl=False):
        self.excl = excl
        self.name = name
        self.w = None
        self.rs = []
        self.multi = multi
        self.ws = []


class Tok:
    __slots__ = ("key", "val", "eng")

    def __init__(self, key, val, eng):
        self.key = key
        self.val = val
        self.eng = eng


class DmaPool:
    def __init__(self, sched, name, n):
        self.s = sched
        self.name = name
        self.n = n
        self.i = 0
        self.count = [0] * n
        self.last = [None] * n

    def keys(self):
        return [("dma", self.name, j) for j in range(self.n)]


class Sched:
    def __init__(self, nc):
        self.nc = nc
        self.ops = {e: [] for e in ENGS}
        self.cnt = {e: 0 for e in ENGS}
        self.pools = []
        self.last_tok = {e: None for e in ENGS}
        self.n_instr = 0

    def pool(self, name, n):
        p = DmaPool(self, name, n)
        self.pools.append(p)
        return p

    def _deps(self, eng, reads, writes):
        deps = []
        for r in reads:
            if r.multi:
                deps.extend(r.ws)
            elif r.w is not None:
                deps.append(r.w)
            if r.excl:
                deps.extend(t for t in r.rs if t.eng != eng)
        for w in writes:
            if w.multi:
                pass
            elif w.w is not None and w.w.eng != eng:
                deps.append(w.w)
            for t in w.rs:
                if t.eng != eng:
                    deps.append(t)
        return deps

    def _mark_w(self, writes, tok):
        for w in writes:
            if w.multi:
                w.ws.append(tok)
            else:
                w.w = tok
                w.rs = []

    def op(self, eng, fn, reads=(), writes=(), extra=()):
        deps = self._deps(eng, reads, writes) + list(extra)
        c = self.cnt[eng]
        tok = Tok(("eng", eng, c // EPOCH), c % EPOCH + 1, eng)
        self.cnt[eng] = c + 1
        for r in reads:
            r.rs.append(tok)
        self._mark_w(writes, tok)
        self.ops[eng].append((deps, fn, tok, 1))
        self.last_tok[eng] = tok
        return tok

    def dma(self, eng, pool, fn, reads=(), writes=(), extra=()):
        deps = self._deps('__dma__', reads, writes) + list(extra)
        j = pool.i
        pool.i = (pool.i + 1) % pool.n
        if pool.last[j] is not None:
            deps.append(pool.last[j])
        pool.count[j] += 16
        tok = Tok(("dma", pool.name, j), pool.count[j], None)
        pool.last[j] = tok
        for r in reads:
            r.rs.append(tok)
        self._mark_w(writes, tok)
        self.ops[eng].append((deps, fn, tok, 16))
        return tok

    def barrier(self):
        toks = [t for t in self.last_tok.values() if t is not None]
        for p in self.pools:
            toks += [t for t in p.last if t is not None]
        for e in ENGS:
            self.ops[e].append((list(toks), None, None, 0))

    def emit(self, final_waits_eng="sp"):
        nc = self.nc
        sems = {}

        def sem_of(key):
            if key not in sems:
                sems[key] = nc.alloc_semaphore("s_" + "_".join(str(k) for k in key))
            return sems[key]

        for e in ENGS:
            for ep in range((self.cnt[e] + EPOCH - 1) // EPOCH):
                sem_of(("eng", e, ep))
        for p in self.pools:
            for k in p.keys():
                sem_of(k)

        toks = [t for t in self.last_tok.values() if t is not None]
        for p in self.pools:
            toks += [t for t in p.last if t is not None]
        self.ops[final_waits_eng].append((list(toks), None, None, 0))

        engobj = {"pe": "tensor", "act": "scalar", "dve": "vector", "pool": "gpsimd", "sp": "sync"}
        sched = self

        def run(ename):
            def body(eng):
                seen = {}
                for deps, fn, tok, inc in sched.ops[ename]:
                    need = {}
                    for t in deps:
                        if t.val > need.get(t.key, 0):
                            need[t.key] = t.val
                    for k, v in need.items():
                        if seen.get(k, 0) >= v:
                            continue
                        seen[k] = v
                        eng.wait_ge(sem_of(k), v)
                        sched.n_instr += 1
                    if fn is not None:
                        ins = fn(eng)
                        ins.then_inc(sem_of(tok.key), inc)
                        sched.n_instr += 1
            return body

        with nc.Block() as block:
            for ename in ENGS:
                getattr(block, engobj[ename])(run(ename))


class Arena:
    def __init__(self, nc, kbytes=198):
        self.nc = nc
        self.words = kbytes * 256
        self.t = nc.alloc_sbuf_tensor("arena", [128, self.words], F32)
        self.ap = self.t.ap()
        self.off = 0
        self.marks = []
        self.peak = 0

    def tile(self, shape, dtype, name=None):
        esz = {F32: 4, BF16: 2, I32: 4}[dtype]
        n = int(np.prod(shape[1:]))
        nw = (n * esz + 3) // 4
        off = (self.off + 15) // 16 * 16
        assert off + nw <= self.words, f"SBUF overflow {off}+{nw} > {self.words}"
        self.off = off + nw
        self.peak = max(self.peak, self.off)
        a = self.ap[0:shape[0], off:off + nw]
        if dtype != F32:
            a = a.bitcast(dtype)
        a = a[:, 0:n]
        if len(shape) == 3:
            a = a.rearrange("p (a b) -> p a b", a=shape[1])
        elif len(shape) == 4:
            a = a.rearrange("p (a b c) -> p a b c", a=shape[1], b=shape[2])
        return a

    def mark(self):
        self.marks.append(self.off)

    def release(self):
        self.off = self.marks.pop()

D = 1024
T = 4352
NT = 34
TX = 4096
NCTX = 256
H = 8
OFF_Z = 3072
OFF_GATE = 4832
D_IN = 6880
NMID = 1760
EPS = 1e-6
NEG = -30000.0


class K:
    def __init__(self, dbg=()):
        self.nc = bass.Bass("TRN2", target_bir_lowering=False)
        self.S = Sched(self.nc)
        self.A = Arena(self.nc)
        self.dbg = set(dbg)
        self.ins = {}
        self.scr = {}
        nc = self.nc
        self.ps = nc.alloc_psum_tensor("ps", [128, 8, 512], F32).ap()
        self.psr = [Res(f"ps{b}", excl=True) for b in range(8)]
        self.ld = self.S.pool("ld", 8)
        self.st = self.S.pool("st", 8)
        self.wl = self.S.pool("wl", 6)

    def inp(self, name, shape, dtype=F32):
        t = self.nc.dram_tensor(name, list(shape), dtype, kind="ExternalInput").ap()
        self.ins[name] = t
        return t

    def scratch(self, name, shape, dtype):
        kind = "ExternalOutput" if name in self.dbg else "Internal"
        t = self.nc.dram_tensor(name, list(shape), dtype, kind=kind).ap()
        self.scr[name] = (t, Res(name, multi=True))
        return t, self.scr[name][1]

    def bank(self, b, dtype=F32):
        a = self.ps[:, b, :]
        if dtype == BF16:
            a = a.bitcast(BF16)
        return a, self.psr[b]

    def tile(self, shape, dtype, name=None):
        return self.A.tile(shape, dtype, name), Res(name or "t")

    def load(self, dst, dres, src, sres=None, q="sp", pool=None):
        return self.S.dma(q, pool or self.ld, lambda e: e.dma_start(out=dst, in_=src),
                          reads=[sres] if sres is not None else [], writes=[dres])

    def store(self, dst, dres, src, sres, q="sp", pool=None):
        return self.S.dma(q, pool or self.st, lambda e: e.dma_start(out=dst, in_=src),
                          reads=[sres], writes=[dres] if dres is not None else [])

    def wload(self, dst, dres, src):
        return self.S.dma("pool", self.wl, lambda e: e.dma_start(out=dst, in_=src), writes=[dres])


def declare_inputs(k):
    i = k.inp
    i("x", [TX, D]); i("ctx", [NCTX, D]); i("c2", [128, 8, 2])
    i("ada_w", [D, 6 * D]); i("ada_b_row", [1, 6 * D]); i("ada_bT", [128, 48])
    i("g1T", [128, 8]); i("g2T", [128, 8]); i("g2_rep", [128, D])
    i("w_in", [D, D_IN]); i("convT", [128, 24, 5])
    i("alog_rep", [128, 16]); i("dtb_rep", [128, 16]); i("dng_rep", [128, 128])
    i("gqaT", [128, 3]); i("w_uq", [384, 1536]); i("gkvaT", [128, 2]); i("w_ukv", [256, 2048])
    i("gq_rep", [128, 192]); i("gk_rep", [128, 192])
    i("w_out_a", [D, D]); i("w_out_b", [D, D]); i("w_o", [D, D])
    i("router_w", [D, 16]); i("w_gate", [16, D, 1408]); i("w_up", [16, D, 1408]); i("w_down", [16, 1408, D])
    i("rope_cs", [TX, 128])
    i("ident", [128, 128]); i("consts", [128, 1024])
    i("moe_tok", [128, 32, 2]); i("moe_blk", [128, 128]); i("moe_sel8", [128, 16]); i("moe_tris", [128, 128])
    i("dn_masks", [128, 9, 128]); i("dn_esel", [64, 2, 8, 128]); i("dn_linit", [64, 128])
    k.out = k.nc.dram_tensor("out", [TX, D], F32, kind="ExternalOutput").ap()
    k.out_res = Res("out", multi=True)


def setup_consts(k):
    S = k.S
    k.ident_f, k.r_ident_f = k.tile([128, 128], F32, "identf")
    k.ident_b, k.r_ident_b = k.tile([128, 128], BF16, "identb")
    k.load(k.ident_f, k.r_ident_f, k.ins["ident"])
    S.op("dve", lambda e: e.tensor_copy(out=k.ident_b, in_=k.ident_f), reads=[k.r_ident_f], writes=[k.r_ident_b])
    k.ones_f, k.r_ones_f = k.tile([128, 128], F32, "onesf")
    k.ones_b, k.r_ones_b = k.tile([128, 128], BF16, "onesb")
    S.op("pool", lambda e: e.memset(k.ones_f, 1.0), writes=[k.r_ones_f])
    S.op("pool", lambda e: e.memset(k.ones_b, 1.0), writes=[k.r_ones_b])


def phase_a0(k):
    S, A, nc = k.S, k.A, k.nc
    ins = k.ins
    k.modT, k.r_modT = k.tile([128, 48, 2], F32, "modT")
    k.s1, k.r_s1 = k.tile([128, 8, 2], F32, "s1")
    k.s2, k.r_s2 = k.tile([128, 8], F32, "s2")
    k.gate1_row, k.r_gate1 = k.tile([128, D], F32, "gate1row")
    k.gate2_row, k.r_gate2 = k.tile([128, D], F32, "gate2row")
    k.off_after_gate2 = A.off
    k.shift2_row, k.r_shift2 = k.tile([128, D], F32, "shift2row")
    k.s2_row, k.r_s2row = k.tile([128, D], F32, "s2row")
    A.mark()
    c2, r_c2 = k.tile([128, 8, 2], F32, "c2")
    sc, r_sc = k.tile([128, 8, 2], F32, "sc")
    screp, r_screp = k.tile([128, 8, 128], F32, "screp")
    abT, r_abT = k.tile([128, 48], F32, "abT")
    abrow, r_abrow = k.tile([1, 6 * D], F32, "abrow")
    g1T, r_g1T = k.tile([128, 8], F32, "g1T")
    g2T, r_g2T = k.tile([128, 8], F32, "g2T")
    wbuf = [k.tile([128, 8, D], F32, f"adaw{j}") for j in range(2)]
    k.load(c2, r_c2, ins["c2"])
    k.load(abT, r_abT, ins["ada_bT"])
    k.load(abrow, r_abrow, ins["ada_b_row"])
    k.load(g1T, r_g1T, ins["g1T"])
    k.load(g2T, r_g2T, ins["g2T"])
    S.op("act", lambda e: e.activation(out=sc, in_=c2, func=AF.Silu), reads=[r_c2], writes=[r_sc])
    S.op("dve", lambda e: e.tensor_copy(out=screp, in_=sc[:, :, 0:1].to_broadcast([128, 8, 128])),
         reads=[r_sc], writes=[r_screp])
    pm, r_pm = k.bank(0)
    aw = ins["ada_w"].rearrange("(kc p) n -> p kc n", p=128)
    for sec in range(6):
        wt, r_wt = wbuf[sec % 2]
        q = "sp" if sec % 2 == 0 else "act"
        S.dma(q, k.ld, lambda e, wt=wt, sec=sec: e.dma_start(out=wt, in_=aw[:, :, sec * D:(sec + 1) * D]), writes=[r_wt])

        def mm(e, wt=wt, sec=sec):
            for fc in range(8):
                for kc in range(8):
                    ins_ = e.matmul(pm[:, (sec * 8 + fc) * 2:(sec * 8 + fc) * 2 + 2], lhsT=wt[:, kc, fc * 128:(fc + 1) * 128],
                                    rhs=sc[:, kc, :], start=(kc == 0), stop=(kc == 7))
            return ins_
        S.op("pe", mm, reads=[r_wt, r_sc], writes=[r_pm])
        if sec in (2, 3, 4, 5):
            dst, r_dst = {2: (k.gate1_row, k.r_gate1), 5: (k.gate2_row, k.r_gate2), 3: (k.shift2_row, k.r_shift2), 4: (k.s2_row, k.r_s2row)}[sec]
            for hf in range(2):
                pb, r_pb = k.bank(1 + hf)

                def mmr(e, wt=wt, sec=sec, hf=hf, pb=pb):
                    for kc in range(8):
                        e.matmul(pb, lhsT=screp[:, kc, :], rhs=wt[:, kc, hf * 512:(hf + 1) * 512], start=(kc == 0), stop=False)
                    return e.matmul(pb, lhsT=k.ones_f[0:1, :], rhs=abrow[0:1, sec * D + hf * 512: sec * D + (hf + 1) * 512],
                                    start=False, stop=True)
                S.op("pe", mmr, reads=[r_wt, r_screp, k.r_ones_f, r_abrow], writes=[r_pb])
                S.op("act", lambda e, dst=dst, hf=hf, pb=pb: e.activation(out=dst[:, hf * 512:(hf + 1) * 512], in_=pb, func=AF.Copy),
                     reads=[r_pb], writes=[r_dst])
    S.op("dve", lambda e: e.tensor_tensor(out=k.modT, in0=pm[:, 0:96].rearrange("p (a b) -> p a b", b=2),
                                          in1=abT.unsqueeze(2).to_broadcast([128, 48, 2]), op=ALU.add),
         reads=[r_pm, r_abT], writes=[k.r_modT])
    S.op("dve", lambda e: e.scalar_tensor_tensor(out=k.s1, in0=k.modT[:, 8:16, :], scalar=1.0,
                                                 in1=g1T.unsqueeze(2).to_broadcast([128, 8, 2]), op0=ALU.add, op1=ALU.mult),
         reads=[k.r_modT, r_g1T], writes=[k.r_s1])
    S.op("dve", lambda e: e.scalar_tensor_tensor(out=k.s2, in0=k.modT[:, 32:40, 0], scalar=1.0,
                                                 in1=g2T, op0=ALU.add, op1=ALU.mult),
         reads=[k.r_modT, r_g2T], writes=[k.r_s2])
    g2rep, r_g2rep = k.tile([128, D], F32, "g2rep")
    k.load(g2rep, r_g2rep, ins["g2_rep"])
    S.op("dve", lambda e: e.scalar_tensor_tensor(out=k.s2_row, in0=k.s2_row, scalar=1.0, in1=g2rep, op0=ALU.add, op1=ALU.mult),
         reads=[k.r_s2row, r_g2rep], writes=[k.r_s2row])
    S.barrier()
    A.release()

def _nb(k, dtype=F32):
    b = getattr(k, "_bank_i", 0)
    k._bank_i = (b + 1) % 8
    return k.bank(b, dtype)


def _rstd(k, ss, r_ss, n, out, r_out):
    S = k.S
    S.op("act", lambda e: e.activation(out=out, in_=ss, func=AF.Sqrt, scale=1.0 / n, bias=EPS), reads=[r_ss], writes=[r_out])
    S.op("dve", lambda e: e.reciprocal(out=out, in_=out), reads=[r_out], writes=[r_out])


def _rope(k, pe, r_pe, cos_t, sin_t, r_tab, t1, t2, r_t1, r_t2):
    S = k.S
    cb = cos_t.unsqueeze(1).to_broadcast([128, 8, 64])
    S.op("pool", lambda e: e.tensor_tensor(out=t1, in0=pe, in1=cb, op=ALU.mult), reads=[r_pe, r_tab], writes=[r_t1])
    pe5 = pe.rearrange("p h (a s c) -> p h a s c", a=2, s=2)
    t25 = t2.rearrange("p h (a s c) -> p h a s c", a=2, s=2)
    sn5 = sin_t.rearrange("p (a s c) -> p a s c", a=2, s=2)

    def f(e):
        for s in range(2):
            ins_ = e.tensor_tensor(out=t25[:, :, :, s, :], in0=pe5[:, :, :, 1 - s, :],
                                   in1=sn5[:, :, s, :].unsqueeze(1).to_broadcast([128, 8, 2, 16]), op=ALU.mult)
        return ins_
    S.op("dve", f, reads=[r_pe, r_tab], writes=[r_t2])
    S.op("pool", lambda e: e.tensor_tensor(out=pe, in0=t1, in1=t2, op=ALU.add), reads=[r_t1, r_t2], writes=[r_pe])


def phase_a1(k):
    S, A, nc, ins = k.S, k.A, k.nc, k.ins
    zs_s, r_zs_s = k.scratch("zs_s", [TX, D], BF16)
    gb_s, r_gb_s = k.scratch("gb_s", [T, 48], F32)
    qmT_s, r_qmT_s = k.scratch("qmT_s", [H, 192, TX], BF16)
    kmT_s, r_kmT_s = k.scratch("kmT_s", [H, 192, T], BF16)
    vm_s, r_vm_s = k.scratch("vm_s", [T, D], BF16)
    A.mark()
    k.hT, _ = k.tile([128, 8, T], BF16, "hT")
    k.r_hT = [Res(f"hT{i}") for i in range(NT)]
    A.mark()
    wmid, r_wmid = k.tile([128, 8, NMID], BF16, "wmid")
    wuq, r_wuq = k.tile([128, 3, 1536], BF16, "wuq")
    wukv, r_wukv = k.tile([128, 2, 2048], BF16, "wukv")
    dtb, r_dtb = k.tile([128, 16], F32, "dtb")
    negA, r_negA = k.tile([128, 16], F32, "negA")
    gq, r_gq = k.tile([128, 192], F32, "gq")
    gk, r_gk = k.tile([128, 192], F32, "gk")
    gqaT, r_gqaT = k.tile([128, 3], F32, "gqaT")
    gkvaT, r_gkvaT = k.tile([128, 2], F32, "gkvaT")
    win = ins["w_in"].rearrange("(kc p) n -> p kc n", p=128)
    for j in range(4):
        k.wload(wmid[:, 2 * j:2 * j + 2, :], r_wmid, win[:, 2 * j:2 * j + 2, OFF_Z:OFF_GATE])
    k.wload(wuq, r_wuq, ins["w_uq"].rearrange("(kc p) n -> p kc n", p=128))
    k.wload(wukv, r_wukv, ins["w_ukv"].rearrange("(kc p) n -> p kc n", p=128))
    k.load(dtb, r_dtb, ins["dtb_rep"])
    k.load(negA, r_negA, ins["alog_rep"])
    k.load(gq, r_gq, ins["gq_rep"])
    k.load(gk, r_gk, ins["gk_rep"])
    k.load(gqaT, r_gqaT, ins["gqaT"])
    k.load(gkvaT, r_gkvaT, ins["gkvaT"])
    S.op("act", lambda e: e.activation(out=negA, in_=negA, func=AF.Exp), reads=[r_negA], writes=[r_negA])
    S.op("dve", lambda e: e.tensor_scalar(out=negA, in0=negA, scalar1=-1.0, scalar2=None, op0=ALU.mult), reads=[r_negA], writes=[r_negA])
    S.op("dve", lambda e: e.tensor_scalar(out=gq, in0=gq, scalar1=192.0 ** -0.5, scalar2=None, op0=ALU.mult), reads=[r_gq], writes=[r_gq])

    NB = 2
    xt = [k.tile([128, D], F32, "xt") for _ in range(NB)]
    junk, r_junk = k.tile([128, D], BF16, "junk")
    ss = [k.tile([128, 8], F32, "ss") for _ in range(NB)]
    xn = [k.tile([128, D], BF16, "xn") for _ in range(NB)]
    zs = [k.tile([128, D], BF16, "zs") for _ in range(NB)]
    gb = [k.tile([128, 48], F32, "gb") for _ in range(NB)]
    t16, r_t16 = k.tile([128, 16], F32, "t16")
    cqn, r_cqn = k.tile([128, 384], BF16, "cqn")
    ckvn, r_ckvn = k.tile([128, 256], BF16, "ckvn")
    cqnT, r_cqnT = k.tile([128, 3, 128], BF16, "cqnT")
    ckvnT, r_ckvnT = k.tile([128, 2, 128], BF16, "ckvnT")
    kr, r_kr = k.tile([128, 64], F32, "kr")
    qsb, r_qsb = k.tile([128, 8, 192], F32, "qsb")
    sq, r_sq = k.tile([128, 8, 192], F32, "sq")
    r8, r_r8 = k.tile([128, 8], F32, "r8")
    kvsb, r_kvsb = k.tile([128, 8, 2, 128], F32, "kvsb")
    tmpf, r_tmpf = kvsb.rearrange("p a b c -> p (a b c)")[:, 0:1024].rearrange("p (a b) -> p a b", a=8), r_kvsb
    kf, r_kf = sq, r_sq
    rk8, r_rk8 = k.tile([128, 8], F32, "rk8")
    sskr, r_sskr = k.tile([128, 1], F32, "sskr")
    rt1, r_rt1 = k.tile([128, 8, 64], F32, "rt1")
    rt2, r_rt2 = k.tile([128, 8, 64], F32, "rt2")
    cs_t = [k.tile([128, 128], F32, "cs") for _ in range(NB)]
    qf, r_qf = k.tile([128, 8, 192], BF16, "qf")
    kfb, r_kfb = k.tile([128, 8, 192], BF16, "kfb")
    vb = [k.tile([128, 8, 128], BF16, "vb") for _ in range(1)]
    qTn = [k.tile([128, 8, 128], BF16, "qTn") for _ in range(1)]
    qTr = [k.tile([64, 8, 128], BF16, "qTr") for _ in range(1)]
    kTn = [k.tile([128, 8, 128], BF16, "kTn") for _ in range(1)]
    kTr = [k.tile([64, 8, 128], BF16, "kTr") for _ in range(1)]

    def src_rows(i):
        return ins["ctx"][i * 128:(i + 1) * 128, :] if i < 2 else ins["x"][(i - 2) * 128:(i - 1) * 128, :]

    def prefetch(i):
        b = i % NB
        k.load(xt[b][0], xt[b][1], src_rows(i))
        if i >= 2:
            xi = i - 2
            S.dma("act", k.ld, lambda e: e.dma_start(out=cs_t[b][0], in_=ins["rope_cs"][xi * 128:(xi + 1) * 128, :]), writes=[cs_t[b][1]])

    def transposes(src, r_src, n, width=128, rows=128):
        pb, r_pb = _nb(k, BF16)
        pv = pb.rearrange("p (a b) -> p a b", b=128)[0:width, 0:n, :]

        def f(e):
            for j in range(n):
                ins_ = e.transpose(out=pv[:, j, :], in_=src(j), identity=k.ident_b)
            return ins_
        S.op("pe", f, reads=[r_src, k.r_ident_b], writes=[r_pb])
        return pv, r_pb

    prefetch(0)
    for i in range(NT):
        b = i % NB
        is_x = i >= 2
        xi = i - 2
        col = 0 if is_x else 1
        tok = slice(i * 128, (i + 1) * 128)
        if i + 1 < NT:
            prefetch(i + 1)
        x_t, r_x = xt[b]
        ss_t, r_ss = ss[b]
        xn_t, r_xn = xn[b]
        S.op("act", lambda e, x_t=x_t, ss_t=ss_t: e.activation(out=junk, in_=x_t, func=AF.Square, accum_out=ss_t[:, 0:1]),
             reads=[r_x], writes=[r_junk, r_ss])
        _rstd(k, ss_t[:, 0:1], r_ss, D, ss_t[:, 1:2], r_ss)
        S.op("act", lambda e, x_t=x_t, ss_t=ss_t, xn_t=xn_t: e.activation(out=xn_t, in_=x_t, func=AF.Copy, scale=ss_t[:, 1:2]),
             reads=[r_x, r_ss], writes=[r_xn])
        pv, r_pv = transposes(lambda j, xn_t=xn_t: xn_t[:, j * 128:(j + 1) * 128], r_xn, 8)
        S.op("dve", lambda e, pv=pv, col=col: e.tensor_tensor(out=tmpf, in0=pv, in1=k.s1[:, :, col:col + 1].to_broadcast([128, 8, 128]), op=ALU.mult),
             reads=[r_pv, k.r_s1], writes=[r_tmpf])
        S.op("pool", lambda e, col=col, tok=tok: e.tensor_tensor(out=k.hT[:, :, tok], in0=tmpf,
                                                                in1=k.modT[:, 0:8, col:col + 1].to_broadcast([128, 8, 128]), op=ALU.add),
             reads=[r_tmpf, k.r_modT], writes=[k.r_hT[i]])
        groups = [(0, 512), (512, 1024), (1024, 1440), (1440, 1760)]
        banks = []
        for g, (c0, c1) in enumerate(groups):
            if g < 2 and not is_x:
                banks.append(None)
                continue
            pb, r_pb = _nb(k)

            def mm(e, pb=pb, c0=c0, c1=c1, tok=tok):
                for kc in range(8):
                    ins_ = e.matmul(pb[:, 0:c1 - c0], lhsT=k.hT[:, kc, tok], rhs=wmid[:, kc, c0:c1], start=(kc == 0), stop=(kc == 7))
                return ins_
            S.op("pe", mm, reads=[k.r_hT[i], r_wmid], writes=[r_pb])
            banks.append((pb, r_pb))
        if is_x:
            z_t, r_z = zs[b]
            for g in range(2):
                pb, r_pb = banks[g]
                S.op("act", lambda e, pb=pb, g=g, z_t=z_t: e.activation(out=z_t[:, g * 512:(g + 1) * 512], in_=pb, func=AF.Silu),
                     reads=[r_pb], writes=[r_z])
            k.store(zs_s[xi * 128:(xi + 1) * 128, :], r_zs_s, z_t, r_z)
        p2, r_p2 = banks[2]
        p3, r_p3 = banks[3]
        gb_t, r_gb = gb[b]
        S.op("dve", lambda e, p2=p2: e.tensor_tensor(out=t16, in0=p2[:, 0:16], in1=dtb, op=ALU.add), reads=[r_p2, r_dtb], writes=[r_t16])
        S.op("act", lambda e: e.activation(out=t16, in_=t16, func=AF.Exp), reads=[r_t16], writes=[r_t16])
        S.op("act", lambda e: e.activation(out=t16, in_=t16, func=AF.Ln, bias=1.0), reads=[r_t16], writes=[r_t16])
        S.op("dve", lambda e, gb_t=gb_t: e.tensor_tensor(out=gb_t[:, 0:16], in0=t16, in1=negA, op=ALU.mult), reads=[r_t16, r_negA], writes=[r_gb])
        S.op("act", lambda e, gb_t=gb_t, p2=p2: e.activation(out=gb_t[:, 16:32], in_=p2[:, 16:32], func=AF.Sigmoid), reads=[r_p2], writes=[r_gb])
        S.op("act", lambda e, gb_t=gb_t: e.activation(out=gb_t[:, 32:48], in_=gb_t[:, 16:32], func=AF.Ln), reads=[r_gb], writes=[r_gb])
        k.store(gb_s[tok, :], r_gb_s, gb_t, r_gb)
        if is_x:
            S.op("act", lambda e, p2=p2, ss_t=ss_t: e.activation(out=junk[:, 0:384], in_=p2[:, 32:416], func=AF.Square, accum_out=ss_t[:, 4:5]),
                 reads=[r_p2], writes=[r_junk, r_ss])
            _rstd(k, ss_t[:, 4:5], r_ss, 384, ss_t[:, 5:6], r_ss)
            S.op("act", lambda e, p2=p2, ss_t=ss_t: e.activation(out=cqn, in_=p2[:, 32:416], func=AF.Copy, scale=ss_t[:, 5:6]),
                 reads=[r_p2, r_ss], writes=[r_cqn])

        S.op("act", lambda e, p3=p3, ss_t=ss_t: e.activation(out=junk[:, 0:256], in_=p3[:, 0:256], func=AF.Square, accum_out=ss_t[:, 2:3]),
             reads=[r_p3], writes=[r_junk, r_ss])
        _rstd(k, ss_t[:, 2:3], r_ss, 256, ss_t[:, 3:4], r_ss)
        S.op("act", lambda e, p3=p3, ss_t=ss_t: e.activation(out=ckvn, in_=p3[:, 0:256], func=AF.Copy, scale=ss_t[:, 3:4]),
             reads=[r_p3, r_ss], writes=[r_ckvn])
        S.op("dve", lambda e, p3=p3: e.tensor_copy(out=kr, in_=p3[:, 256:320]), reads=[r_p3], writes=[r_kr])
        pv, r_pv = transposes(lambda j: ckvn[:, j * 128:(j + 1) * 128], r_ckvn, 2)
        S.op("dve", lambda e, pv=pv: e.tensor_tensor(out=ckvnT, in0=pv, in1=gkvaT.unsqueeze(2).to_broadcast([128, 2, 128]), op=ALU.mult),
             reads=[r_pv, r_gkvaT], writes=[r_ckvnT])
        for b4 in range(4):
            pb, r_pb = _nb(k)

            def mmkv(e, pb=pb, b4=b4):
                for kc in range(2):
                    ins_ = e.matmul(pb, lhsT=ckvnT[:, kc, :], rhs=wukv[:, kc, b4 * 512:(b4 + 1) * 512], start=(kc == 0), stop=(kc == 1))
                return ins_
            S.op("pe", mmkv, reads=[r_ckvnT, r_wukv], writes=[r_pb])
            eng = "act" if b4 % 2 == 0 else "dve"
            dst = kvsb[:, 2 * b4:2 * b4 + 2, :, :].rearrange("p a b c -> p (a b c)")
            if eng == "act":
                S.op("act", lambda e, dst=dst, pb=pb: e.activation(out=dst, in_=pb, func=AF.Copy), reads=[r_pb], writes=[r_kvsb])
            else:
                S.op("dve", lambda e, dst=dst, pb=pb: e.tensor_copy(out=dst, in_=pb), reads=[r_pb], writes=[r_kvsb])
        vb_t, r_vb = vb[0]
        S.op("pool", lambda e, vb_t=vb_t: e.tensor_copy(out=vb_t, in_=kvsb[:, :, 1, :]), reads=[r_kvsb], writes=[r_vb])
        k.store(vm_s[tok, :], r_vm_s, vb_t.rearrange("p h d -> p (h d)"), r_vb)
        S.op("pool", lambda e: e.tensor_tensor(out=sq[:, :, 0:128], in0=kvsb[:, :, 0, :], in1=kvsb[:, :, 0, :], op=ALU.mult),
             reads=[r_kvsb], writes=[r_sq])
        S.op("dve", lambda e: e.tensor_reduce(out=rk8, in_=sq[:, :, 0:128], axis=AX.X, op=ALU.add), reads=[r_sq], writes=[r_rk8])
        S.op("act", lambda e: e.activation(out=junk[:, 0:64], in_=kr, func=AF.Square, accum_out=sskr), reads=[r_kr], writes=[r_junk, r_sskr])
        S.op("dve", lambda e: e.tensor_scalar(out=rk8, in0=rk8, scalar1=sskr, scalar2=None, op0=ALU.add), reads=[r_rk8, r_sskr], writes=[r_rk8])
        _rstd(k, rk8, r_rk8, 192, rk8, r_rk8)
        S.op("dve", lambda e: e.tensor_tensor(out=kf[:, :, 0:128], in0=kvsb[:, :, 0, :], in1=rk8.unsqueeze(2).to_broadcast([128, 8, 128]), op=ALU.mult),
             reads=[r_kvsb, r_rk8], writes=[r_kf])
        S.op("dve", lambda e: e.tensor_tensor(out=kf[:, :, 128:192], in0=kr.unsqueeze(1).to_broadcast([128, 8, 64]),
                                              in1=rk8.unsqueeze(2).to_broadcast([128, 8, 64]), op=ALU.mult),
             reads=[r_kr, r_rk8, r_kf], writes=[r_kf])
        S.op("pool", lambda e: e.tensor_tensor(out=kf, in0=kf, in1=gk.unsqueeze(1).to_broadcast([128, 8, 192]), op=ALU.mult),
             reads=[r_kf, r_gk], writes=[r_kf])
        if is_x:
            _rope(k, kf[:, :, 128:192], r_kf, cs_t[b][0][:, 0:64], cs_t[b][0][:, 64:128], cs_t[b][1], rt1, rt2, r_rt1, r_rt2)
        S.op("act", lambda e: e.activation(out=kfb, in_=kf, func=AF.Copy), reads=[r_kf], writes=[r_kfb])
        kTn_t, r_kTn = kTn[0]
        kTr_t, r_kTr = kTr[0]
        pv, r_pv = transposes(lambda j: kfb[:, j, 0:128], r_kfb, 8)
        S.op("dve", lambda e, pv=pv, kTn_t=kTn_t: e.tensor_copy(out=kTn_t, in_=pv), reads=[r_pv], writes=[r_kTn])
        pv, r_pv = transposes(lambda j: kfb[:, j, 128:192], r_kfb, 8, width=64)
        S.op("act", lambda e, pv=pv, kTr_t=kTr_t: e.activation(out=kTr_t, in_=pv, func=AF.Copy), reads=[r_pv], writes=[r_kTr])
        k.store(kmT_s[:, 0:128, tok].rearrange("h d t -> d h t"), r_kmT_s, kTn_t, r_kTn)
        k.store(kmT_s[:, 128:192, tok].rearrange("h d t -> d h t"), r_kmT_s, kTr_t, r_kTr)
        if not is_x:
            continue
        xtok = slice(xi * 128, (xi + 1) * 128)
        pv, r_pv = transposes(lambda j: cqn[:, j * 128:(j + 1) * 128], r_cqn, 3)
        S.op("dve", lambda e, pv=pv: e.tensor_tensor(out=cqnT, in0=pv, in1=gqaT.unsqueeze(2).to_broadcast([128, 3, 128]), op=ALU.mult),
             reads=[r_pv, r_gqaT], writes=[r_cqnT])
        qflat = qsb.rearrange("p h d -> p (h d)")
        for b3 in range(3):
            pb, r_pb = _nb(k)

            def mmq(e, pb=pb, b3=b3):
                for kc in range(3):
                    ins_ = e.matmul(pb, lhsT=cqnT[:, kc, :], rhs=wuq[:, kc, b3 * 512:(b3 + 1) * 512], start=(kc == 0), stop=(kc == 2))
                return ins_
            S.op("pe", mmq, reads=[r_cqnT, r_wuq], writes=[r_pb])
            if b3 % 2 == 0:
                S.op("act", lambda e, pb=pb, b3=b3: e.activation(out=qflat[:, b3 * 512:(b3 + 1) * 512], in_=pb, func=AF.Copy), reads=[r_pb], writes=[r_qsb])
            else:
                S.op("dve", lambda e, pb=pb, b3=b3: e.tensor_copy(out=qflat[:, b3 * 512:(b3 + 1) * 512], in_=pb), reads=[r_pb], writes=[r_qsb])
        S.op("pool", lambda e: e.tensor_tensor(out=sq, in0=qsb, in1=qsb, op=ALU.mult), reads=[r_qsb], writes=[r_sq])
        S.op("dve", lambda e: e.tensor_reduce(out=r8, in_=sq, axis=AX.X, op=ALU.add), reads=[r_sq], writes=[r_r8])
        _rstd(k, r8, r_r8, 192, r8, r_r8)
        S.op("dve", lambda e: e.tensor_tensor(out=qsb, in0=qsb, in1=r8.unsqueeze(2).to_broadcast([128, 8, 192]), op=ALU.mult),
             reads=[r_qsb, r_r8], writes=[r_qsb])
        S.op("pool", lambda e: e.tensor_tensor(out=qsb, in0=qsb, in1=gq.unsqueeze(1).to_broadcast([128, 8, 192]), op=ALU.mult),
             reads=[r_qsb, r_gq], writes=[r_qsb])
        _rope(k, qsb[:, :, 128:192], r_qsb, cs_t[b][0][:, 0:64], cs_t[b][0][:, 64:128], cs_t[b][1], rt1, rt2, r_rt1, r_rt2)
        S.op("act", lambda e: e.activation(out=qf, in_=qsb, func=AF.Copy), reads=[r_qsb], writes=[r_qf])
        qTn_t, r_qTn = qTn[0]
        qTr_t, r_qTr = qTr[0]
        pv, r_pv = transposes(lambda j: qf[:, j, 0:128], r_qf, 8)
        S.op("dve", lambda e, pv=pv, qTn_t=qTn_t: e.tensor_copy(out=qTn_t, in_=pv), reads=[r_pv], writes=[r_qTn])
        pv, r_pv = transposes(lambda j: qf[:, j, 128:192], r_qf, 8, width=64)
        S.op("act", lambda e, pv=pv, qTr_t=qTr_t: e.activation(out=qTr_t, in_=pv, func=AF.Copy), reads=[r_pv], writes=[r_qTr])
        k.store(qmT_s[:, 0:128, xtok].rearrange("h d t -> d h t"), r_qmT_s, qTn_t, r_qTn)
        k.store(qmT_s[:, 128:192, xtok].rearrange("h d t -> d h t"), r_qmT_s, qTr_t, r_qTr)
    S.barrier()
    A.release()

RW = 4364
NU = 4356


def phase_a2(k):
    S, A, nc, ins = k.S, k.A, k.nc, k.ins
    qdT_s, r_qdT_s = k.scratch("qdT_s", [H, 128, TX], BF16)
    kdT_s, r_kdT_s = k.scratch("kdT_s", [H, 128, T], BF16)
    kd_s, r_kd_s = k.scratch("kd_s", [T, H, 128], BF16)
    vd_s, r_vd_s = k.scratch("vd_s", [T, H, 128], BF16)
    sgT_s, r_sgT_s = k.scratch("sgT_s", [16, 128, TX], BF16)
    A.mark()
    convw, r_convw = k.tile([128, 24, 5], F32, "convw")
    k.load(convw, r_convw, ins["convT"])
    wc = [k.tile([128, 8, 512], BF16, "wc") for _ in range(2)]
    R = [k.tile([128, RW], F32, "R") for _ in range(2)]
    acc, r_acc = k.tile([128, NU], F32, "acc")
    sq, r_sq = k.tile([128, NU], BF16, "sq")
    Yb = [k.tile([128, NU], BF16, "Yb") for _ in range(2)]
    rn, r_rn = k.tile([128, 512], F32, "rn")
    tm = [k.tile([128, NT, 128], BF16, "tm") for _ in range(1)]
    sg = [k.tile([128, 512], BF16, "sg") for _ in range(2)]
    for j in range(2):
        S.op("pool", lambda e, j=j: e.memset(R[j][0], 0.0), writes=[R[j][1]])
    win = ins["w_in"].rearrange("(kc p) n -> p kc n", p=128)
    blocks = [(c * 512, "qkv", c * 4) for c in range(6)] + [(OFF_GATE + c * 512, "gate", c * 4) for c in range(4)]
    tgroups = [(0, 256, 2)] + [(256 + g * 512, 512, 262 + g * 512) for g in range(8)]
    all_hT = list(k.r_hT)

    def load_block(bi):
        c0, kind, _ = blocks[bi]
        w_t, r_w = wc[bi % 2]
        for hf in range(2):
            k.wload(w_t[:, 4 * hf:4 * hf + 4, :], r_w, win[:, 4 * hf:4 * hf + 4, c0:c0 + 512])

    load_block(0)
    ci = 0
    for bi, (c0, kind, chunk0) in enumerate(blocks):
        if bi + 1 < len(blocks):
            load_block(bi + 1)
        w_t, r_w = wc[bi % 2]
        for sub in range(4):
            cc = chunk0 + sub
            if kind == "gate":
                for g in range(8):
                    pb, r_pb = _nb(k)

                    def mm(e, pb=pb, g=g, sub=sub, w_t=w_t):
                        for kc in range(8):
                            ins_ = e.matmul(pb, lhsT=w_t[:, kc, sub * 128:(sub + 1) * 128], rhs=k.hT[:, kc, 256 + g * 512:256 + (g + 1) * 512],
                                            start=(kc == 0), stop=(kc == 7))
                        return ins_
                    S.op("pe", mm, reads=[r_w] + all_hT, writes=[r_pb])
                    s_t, r_s = sg[g % 2]
                    S.op("act", lambda e, pb=pb, s_t=s_t: e.activation(out=s_t, in_=pb, func=AF.Sigmoid), reads=[r_pb], writes=[r_s])
                    k.store(sgT_s[cc, :, g * 512:(g + 1) * 512], r_sgT_s, s_t, r_s)
                continue
            R_t, r_R = R[ci % 2]
            Y_t, r_Y = Yb[ci % 2]
            ci += 1
            for gi, (h0, n, ro) in enumerate(tgroups):
                pb, r_pb = _nb(k)

                def mm(e, pb=pb, h0=h0, n=n, sub=sub, w_t=w_t):
                    for kc in range(8):
                        ins_ = e.matmul(pb[:, 0:n], lhsT=w_t[:, kc, sub * 128:(sub + 1) * 128], rhs=k.hT[:, kc, h0:h0 + n],
                                        start=(kc == 0), stop=(kc == 7))
                    return ins_
                S.op("pe", mm, reads=[r_w] + all_hT, writes=[r_pb])
                if gi % 2 == 0:
                    S.op("act", lambda e, pb=pb, n=n, ro=ro, R_t=R_t: e.activation(out=R_t[:, ro:ro + n], in_=pb[:, 0:n], func=AF.Copy),
                         reads=[r_pb], writes=[r_R])
                else:
                    S.op("dve", lambda e, pb=pb, n=n, ro=ro, R_t=R_t: e.tensor_copy(out=R_t[:, ro:ro + n], in_=pb[:, 0:n]),
                         reads=[r_pb], writes=[r_R])
            ceng = "dve"

            def conv(e, R_t=R_t, cc=cc):
                e.tensor_scalar(out=acc, in0=R_t[:, 0:NU], scalar1=convw[:, cc, 0:1], scalar2=None, op0=ALU.mult)
                for j in range(1, 5):
                    ins_ = e.scalar_tensor_tensor(out=acc, in0=R_t[:, j:j + NU], scalar=convw[:, cc, j:j + 1], in1=acc,
                                                  op0=ALU.mult, op1=ALU.add)
                return ins_
            S.op(ceng, conv, reads=[r_R, r_convw], writes=[r_acc])
            head = cc % 8
            if cc >= 16:
                S.op("act", lambda e, Y_t=Y_t: e.activation(out=Y_t, in_=acc, func=AF.Silu), reads=[r_acc], writes=[r_Y])
            else:
                S.op("act", lambda e: e.activation(out=acc, in_=acc, func=AF.Silu), reads=[r_acc], writes=[r_acc])
                S.op("pool" if ceng == "dve" else "dve", lambda e: e.tensor_tensor(out=sq, in0=acc, in1=acc, op=ALU.mult), reads=[r_acc], writes=[r_sq])
                scale = (128.0 ** -0.5) if cc < 8 else 1.0
                for g in range(9):
                    u0 = g * 512
                    n = min(512, NU - u0)
                    pb, r_pb = _nb(k)
                    S.op("pe", lambda e, pb=pb, u0=u0, n=n: e.matmul(pb[:, 0:n], lhsT=k.ones_b, rhs=sq[:, u0:u0 + n], start=True, stop=True),
                         reads=[r_sq, k.r_ones_b], writes=[r_pb])
                    S.op("act", lambda e, pb=pb, n=n, scale=scale: e.activation(out=rn[:, 0:n], in_=pb[:, 0:n], func=AF.Sqrt,
                                                                              scale=1.0 / (scale * scale), bias=EPS / (scale * scale)),
                         reads=[r_pb], writes=[r_rn])
                    S.op("dve", lambda e, n=n: e.reciprocal(out=rn[:, 0:n], in_=rn[:, 0:n]), reads=[r_rn], writes=[r_rn])
                    S.op("dve", lambda e, u0=u0, n=n, Y_t=Y_t: e.tensor_tensor(out=Y_t[:, u0:u0 + n], in0=acc[:, u0:u0 + n], in1=rn[:, 0:n], op=ALU.mult),
                         reads=[r_acc, r_rn], writes=[r_Y])
            if cc < 8:
                k.store(qdT_s[head, :, :], r_qdT_s, Y_t[:, 260:260 + TX], r_Y)
            elif cc < 16:
                k.store(kdT_s[head, :, 0:256], r_kdT_s, Y_t[:, 0:256], r_Y)
                k.store(kdT_s[head, :, 256:T], r_kdT_s, Y_t[:, 260:260 + TX], r_Y)
            if cc >= 8:
                tm_t, r_tm = tm[0]
                for tb in range(5):
                    t0 = tb * 8
                    nt = min(8, NT - t0)
                    pb, r_pb = _nb(k, BF16)
                    pv = pb.rearrange("p (a b) -> p a b", b=128)[:, 0:nt, :]

                    def tr(e, pv=pv, t0=t0, nt=nt, Y_t=Y_t):
                        for j in range(nt):
                            ti = t0 + j
                            u = ti * 128 if ti < 2 else 260 + (ti - 2) * 128
                            ins_ = e.transpose(out=pv[:, j, :], in_=Y_t[:, u:u + 128], identity=k.ident_b)
                        return ins_
                    S.op("pe", tr, reads=[r_Y, k.r_ident_b], writes=[r_pb])
                    if tb % 2 == 0:
                        S.op("act", lambda e, pv=pv, t0=t0, nt=nt, tm_t=tm_t: e.activation(out=tm_t[:, t0:t0 + nt, :], in_=pv, func=AF.Copy),
                             reads=[r_pb], writes=[r_tm])
                    else:
                        S.op("dve", lambda e, pv=pv, t0=t0, nt=nt, tm_t=tm_t: e.tensor_copy(out=tm_t[:, t0:t0 + nt, :], in_=pv),
                             reads=[r_pb], writes=[r_tm])
                dst_s, r_dst = (kd_s, r_kd_s) if cc < 16 else (vd_s, r_vd_s)
                k.store(dst_s.rearrange("(n p) h d -> p n h d", p=128)[:, :, head, :], r_dst, tm_t, r_tm)
    S.barrier()
    A.release()
    A.release()

def _drive(*gens):
    gens = [g for g in gens if g is not None]
    while gens:
        for g in list(gens):
            try:
                next(g)
            except StopIteration:
                gens.remove(g)


def phase_b(k):
    S, A, nc, ins = k.S, k.A, k.nc, k.ins
    qdT_s, r_qdT_s = k.scr["qdT_s"]
    kdT_s, r_kdT_s = k.scr["kdT_s"]
    kd_s, r_kd_s = k.scr["kd_s"]
    vd_s, r_vd_s = k.scr["vd_s"]
    gb_s, r_gb_s = k.scr["gb_s"]
    o_s = [k.scratch("of_s", [TX, D], F32), k.scratch("ob_s", [TX, D], F32)]
    A.mark()
    msk, r_msk = k.tile([128, 9, 128], F32, "msk")
    k.load(msk, r_msk, ins["dn_masks"])
    EC, r_EC = k.tile([64, 2, 8, 128], F32, "EC")
    k.load(EC, r_EC, ins["dn_esel"])
    L1, r_L1 = k.tile([64, 128], F32, "L1")
    L2, r_L2 = k.tile([64, 128], F32, "L2")
    R1, r_R1 = k.tile([64, 8, 128], F32, "R1")
    R2, r_R2 = k.tile([64, 8, 128], F32, "R2")
    X, r_X = k.tile([128, 2, 64], F32, "X")
    k.load(L1, r_L1, ins["dn_linit"])
    k.load(L2, r_L2, ins["dn_linit"])
    S.op("dve", lambda e: e.tensor_copy(out=R1, in_=EC[:, 0, :, :]), reads=[r_EC], writes=[r_R1])
    S.op("dve", lambda e: e.tensor_copy(out=R2, in_=EC[:, 1, :, :]), reads=[r_EC], writes=[r_R2])
    S.op("pool", lambda e: e.memset(X, 0.0), writes=[r_X])
    S32 = [k.tile([128, 8, 128], F32, f"S32_{d}") for d in range(2)]
    Sbf = [k.tile([128, 8, 128], BF16, f"Sbf_{d}") for d in range(2)]
    for d in range(2):
        S.op("pool", lambda e, d=d: e.memset(S32[d][0], 0.0), writes=[S32[d][1]])
        S.op("pool", lambda e, d=d: e.memset(Sbf[d][0], 0.0), writes=[Sbf[d][1]])
    NB = 2
    gbt = [k.tile([128, 48], F32, "gbt") for _ in range(NB)]
    kT = [k.tile([128, 8, 128], BF16, "kT") for _ in range(NB)]
    qT = [k.tile([128, 8, 128], BF16, "qT") for _ in range(NB)]
    ktm = [k.tile([128, 8, 128], BF16, "ktm") for _ in range(NB)]
    vtm = [k.tile([128, 8, 128], BF16, "vtm") for _ in range(NB)]
    sm = [k.tile([128, 4, 8], F32, "sm") for _ in range(NB)]
    E1, r_E1 = k.tile([128, 8, 128], F32, "E1")
    M, r_M = k.tile([128, 8, 128], F32, "M")
    Mt, r_Mt = k.tile([128, 8, 128], F32, "Mt")
    Md, r_Md = k.tile([128, 8, 128], F32, "Md")
    Mo1, r_Mo1 = k.tile([128, 8, 128], F32, "Mo1")
    Mo2, r_Mo2 = k.tile([128, 8, 128], F32, "Mo2")
    PP = [k.tile([128, 8, 128], F32, f"PP{j}") for j in range(2)]
    PT = [k.tile([128, 8, 128], F32, f"PT{j}") for j in range(2)]
    Tt, r_Tt = k.tile([128, 8, 128], F32, "Tt")
    TtB, r_TtB = k.tile([128, 8, 128], BF16, "TtB")
    kg, r_kg = k.tile([128, 8, 128], BF16, "kg")
    kdec = [k.tile([128, 8, 128], BF16, "kdec") for _ in range(NB)]
    wT = [k.tile([128, 8, 128], BF16, "wT") for _ in range(NB)]
    u = [k.tile([128, 8, 128], F32, "u") for _ in range(NB)]
    qkT = [k.tile([128, 8, 128], BF16, "qkT") for _ in range(NB)]
    vnew, r_vnew = k.tile([128, 8, 128], BF16, "vnew")
    tmpo, r_tmpo = k.tile([128, 8, 128], F32, "tmpo")
    o_t = [k.tile([128, 8, 128], F32, "o") for _ in range(NB)]

    pre_i = [0]

    def nbp(dtype=F32):
        b = pre_i[0]
        pre_i[0] = (b + 1) % 4
        return k.bank(b, dtype)

    def b4(pb):
        return pb.rearrange("p (a b) -> p a b", b=128)

    units = []
    fwd = list(range(NT))
    bwd = [1, 0] + list(range(NT - 1, 1, -1))
    for s in range(NT):
        units.append((0, fwd[s]))
        units.append((1, bwd[s]))

    def loads(ui):
        d, ti = units[ui]
        b = ui % NB
        tok = slice(ti * 128, (ti + 1) * 128)
        k.load(gbt[b][0], gbt[b][1], gb_s[tok, :], r_gb_s)
        k.load(kT[b][0], kT[b][1], kdT_s[:, :, tok].rearrange("h d t -> d h t"), r_kdT_s)
        k.load(ktm[b][0], ktm[b][1], kd_s[tok, :, :], r_kd_s, q="act")
        k.load(vtm[b][0], vtm[b][1], vd_s[tok, :, :], r_vd_s, q="act")
        if ti >= 2:
            xt_ = slice((ti - 2) * 128, (ti - 1) * 128)
            k.load(qT[b][0], qT[b][1], qdT_s[:, :, xt_].rearrange("h d t -> d h t"), r_qdT_s)

    def pre(ui):
        d, ti = units[ui]
        b = ui % NB
        is_x = ti >= 2
        g_t, r_g = gbt[b]
        kT_t, r_kT = kT[b]
        qT_t, r_qT = qT[b]
        ktm_t, r_ktm = ktm[b]
        vtm_t, r_vtm = vtm[b]
        sm_t, r_sm = sm[b]
        gcol = g_t[:, d * 8:(d + 1) * 8]
        beta = g_t[:, 16 + d * 8:16 + (d + 1) * 8]
        lnb = g_t[:, 32 + d * 8:32 + (d + 1) * 8]
        pb, r_pb = nbp()

        def mm0(e):
            e.matmul(pb[:, 0:8], lhsT=msk[:, d, :], rhs=gcol, start=True, stop=True)
            return e.matmul(pb[:, 8:16], lhsT=k.ones_f, rhs=gcol, start=True, stop=True)
        S.op("pe", mm0, reads=[r_msk, r_g, k.r_ones_f], writes=[r_pb])
        S.op("dve", lambda e: e.tensor_copy(out=X[:, :, 32:40], in_=pb[:, 0:8].unsqueeze(1).to_broadcast([128, 2, 8])), reads=[r_pb], writes=[r_X])
        S.op("dve", lambda e: e.tensor_copy(out=X[:, 1, 0:8], in_=pb[:, 0:8]), reads=[r_pb], writes=[r_X])
        S.op("dve", lambda e: e.tensor_tensor(out=X[:, 0, 0:8], in0=pb[:, 0:8], in1=lnb, op=ALU.add), reads=[r_pb, r_g], writes=[r_X])
        S.op("act", lambda e: e.activation(out=sm_t[:, 0, :], in_=pb[:, 0:8], func=AF.Exp), reads=[r_pb], writes=[r_sm])
        S.op("act", lambda e: e.activation(out=sm_t[:, 1, :], in_=pb[:, 8:16], func=AF.Exp), reads=[r_pb], writes=[r_sm])
        S.op("dve", lambda e: e.tensor_tensor(out=sm_t[:, 3, :], in0=pb[:, 8:16], in1=X[:, 1, 0:8], op=ALU.subtract), reads=[r_pb, r_X], writes=[r_sm])
        S.op("act", lambda e: e.activation(out=sm_t[:, 2, :], in_=sm_t[:, 3, :], func=AF.Exp), reads=[r_sm], writes=[r_sm])
        pt, r_pt = nbp()

        def tr0(e):
            e.transpose(out=pt[0:64, 0:128], in_=X[:, 0, :], identity=k.ident_f)
            return e.transpose(out=pt[0:64, 128:256], in_=X[:, 1, :], identity=k.ident_f)
        S.op("pe", tr0, reads=[r_X, k.r_ident_f], writes=[r_pt])
        S.op("act", lambda e: e.activation(out=L1[0:8, :], in_=pt[0:8, 0:128], func=AF.Copy), reads=[r_pt], writes=[r_L1])
        S.op("act", lambda e: e.activation(out=L2[0:8, :], in_=pt[0:8, 128:256], func=AF.Copy), reads=[r_pt], writes=[r_L2])
        S.op("dve", lambda e: e.tensor_tensor(out=R1[32:40, :, :], in0=EC[32:40, 1, :, :], in1=pt[32:40, 0:128].unsqueeze(1).to_broadcast([8, 8, 128]), op=ALU.mult),
             reads=[r_pt, r_EC], writes=[r_R1])
        S.op("dve", lambda e: e.tensor_tensor(out=R2[32:40, :, :], in0=EC[32:40, 0, :, :], in1=pt[32:40, 128:256].unsqueeze(1).to_broadcast([8, 8, 128]), op=ALU.mult),
             reads=[r_pt, r_EC], writes=[r_R2])
        kd_t, r_kd = kdec[b]
        S.op("pool", lambda e: e.tensor_tensor(out=kg, in0=ktm_t, in1=sm_t[:, 0, :].unsqueeze(2).to_broadcast([128, 8, 128]), op=ALU.mult),
             reads=[r_ktm, r_sm], writes=[r_kg])
        S.op("pool", lambda e: e.tensor_tensor(out=kd_t, in0=ktm_t, in1=sm_t[:, 2, :].unsqueeze(2).to_broadcast([128, 8, 128]), op=ALU.mult),
             reads=[r_ktm, r_sm], writes=[r_kd])
        yield
        def do_group(grp):
            hs = range(4 * grp, 4 * grp + 4)
            gs = slice(4 * grp, 4 * grp + 4)
            pk, r_pk = nbp()
            pd, r_pd = nbp()

            def mmk(e):
                for hh, h in enumerate(hs):
                    ins_ = e.matmul(b4(pk)[:, hh, :], lhsT=kT_t[:, h, :], rhs=kT_t[:, h, :], start=True, stop=True)
                return ins_
            S.op("pe", mmk, reads=[r_kT], writes=[r_pk])

            def mmd(e):
                for hh, h in enumerate(hs):
                    ins_ = e.matmul(b4(pd)[:, hh, :], lhsT=L1, rhs=R1[:, h, :], start=True, stop=True)
                return ins_
            S.op("pe", mmd, reads=[r_L1, r_R1], writes=[r_pd])
            S.op("dve", lambda e: e.scalar_tensor_tensor(out=E1[:, gs, :], in0=b4(pd), scalar=0.0, in1=msk[:, 2 + d, :].unsqueeze(1).to_broadcast([128, 4, 128]),
                                                         op0=ALU.min, op1=ALU.add), reads=[r_pd, r_msk], writes=[r_E1])
            S.op("act", lambda e: e.activation(out=E1[:, gs, :], in_=E1[:, gs, :], func=AF.Exp), reads=[r_E1], writes=[r_E1])
            S.op("dve", lambda e: e.tensor_tensor(out=M[:, gs, :], in0=b4(pk), in1=E1[:, gs, :], op=ALU.mult), reads=[r_pk, r_E1], writes=[r_M])
            for (dst_, rdst_, mi_) in ((Md, r_Md, 6), (Mo1, r_Mo1, 7), (Mo2, r_Mo2, 8)):
                S.op("pool", lambda e, dst_=dst_, mi_=mi_: e.tensor_tensor(out=dst_[:, gs, :], in0=M[:, gs, :],
                                                                        in1=msk[:, mi_, :].unsqueeze(1).to_broadcast([128, 4, 128]), op=ALU.mult),
                     reads=[r_M, r_msk], writes=[rdst_])
            pm, r_pm = nbp()

            def trm(e):
                for hh, h in enumerate(hs):
                    ins_ = e.transpose(out=b4(pm)[:, hh, :], in_=Md[:, h, :], identity=k.ident_f)
                return ins_
            S.op("pe", trm, reads=[r_Md, k.r_ident_f], writes=[r_pm])
            S.op("act", lambda e: e.activation(out=Mt[:, gs, :], in_=b4(pm), func=AF.Copy), reads=[r_pm], writes=[r_Mt])
            S.op("dve", lambda e: e.scalar_tensor_tensor(out=Tt[:, gs, :], in0=b4(pm), scalar=-1.0, in1=k.ident_f.unsqueeze(1).to_broadcast([128, 4, 128]),
                                                         op0=ALU.mult, op1=ALU.add), reads=[r_pm, k.r_ident_f], writes=[r_Tt])
            yield
            P_prev, rP_prev, Pt_prev, rPt_prev = Md, r_Md, Mt, r_Mt
            for lvl in range(1, 5):
                P_new, rP_new = PP[lvl % 2]
                Pt_new, rPt_new = PT[lvl % 2]
                pa, r_pa = nbp()

                def mma(e, P_prev=P_prev, Pt_prev=Pt_prev, pa=pa):
                    for hh, h in enumerate(hs):
                        ins_ = e.matmul(b4(pa)[:, hh, :], lhsT=Pt_prev[:, h, :], rhs=P_prev[:, h, :], start=True, stop=True)
                    return ins_
                S.op("pe", mma, reads=[rP_prev, rPt_prev], writes=[r_pa])
                S.op("act", lambda e, P_new=P_new, pa=pa: e.activation(out=P_new[:, gs, :], in_=b4(pa), func=AF.Copy), reads=[r_pa], writes=[rP_new])
                if lvl < 4:
                    pbk, r_pbk = nbp()

                    def mmb(e, P_prev=P_prev, Pt_prev=Pt_prev, pbk=pbk):
                        for hh, h in enumerate(hs):
                            ins_ = e.matmul(b4(pbk)[:, hh, :], lhsT=P_prev[:, h, :], rhs=Pt_prev[:, h, :], start=True, stop=True)
                        return ins_
                    S.op("pe", mmb, reads=[rP_prev, rPt_prev], writes=[r_pbk])
                    S.op("dve", lambda e, Pt_new=Pt_new, pbk=pbk: e.tensor_copy(out=Pt_new[:, gs, :], in_=b4(pbk)), reads=[r_pbk], writes=[rPt_new])
                pc, r_pc = nbp()

                def mmc(e, P_new=P_new, pc=pc):
                    for hh, h in enumerate(hs):
                        ins_ = e.matmul(b4(pc)[:, hh, :], lhsT=P_new[:, h, :], rhs=Tt[:, h, :], start=True, stop=True)
                    return ins_
                S.op("pe", mmc, reads=[rP_new, r_Tt], writes=[r_pc])
                S.op("dve", lambda e, pc=pc: e.tensor_tensor(out=Tt[:, gs, :], in0=Tt[:, gs, :], in1=b4(pc), op=ALU.add), reads=[r_Tt, r_pc], writes=[r_Tt])
                P_prev, rP_prev, Pt_prev, rPt_prev = P_new, rP_new, Pt_new, rPt_new
                yield
            for (Mo_, rMo_) in ((Mo1, r_Mo1), (Mo2, r_Mo2)):
                ptd, r_ptd = nbp()

                def trt(e, ptd=ptd):
                    for hh, h in enumerate(hs):
                        ins_ = e.transpose(out=b4(ptd)[:, hh, :], in_=Tt[:, h, :], identity=k.ident_f)
                    return ins_
                S.op("pe", trt, reads=[r_Tt, k.r_ident_f], writes=[r_ptd])
                S.op("act", lambda e, ptd=ptd: e.activation(out=Mt[:, gs, :], in_=b4(ptd), func=AF.Copy), reads=[r_ptd], writes=[r_Mt])
                pa2, r_pa2 = nbp()

                def mma2(e, pa2=pa2, Mo_=Mo_):
                    for hh, h in enumerate(hs):
                        ins_ = e.matmul(b4(pa2)[:, hh, :], lhsT=Mo_[:, h, :], rhs=Tt[:, h, :], start=True, stop=True)
                    return ins_
                S.op("pe", mma2, reads=[rMo_, r_Tt], writes=[r_pa2])
                S.op("dve", lambda e, pa2=pa2: e.tensor_copy(out=E1[:, gs, :], in_=b4(pa2)), reads=[r_pa2], writes=[r_E1])
                pc2, r_pc2 = nbp()

                def mmc2(e, pc2=pc2):
                    for hh, h in enumerate(hs):
                        ins_ = e.matmul(b4(pc2)[:, hh, :], lhsT=Mt[:, h, :], rhs=E1[:, h, :], start=True, stop=True)
                    return ins_
                S.op("pe", mmc2, reads=[r_Mt, r_E1], writes=[r_pc2])
                S.op("dve", lambda e, pc2=pc2: e.tensor_tensor(out=Tt[:, gs, :], in0=Tt[:, gs, :], in1=b4(pc2), op=ALU.subtract), reads=[r_Tt, r_pc2], writes=[r_Tt])
                yield
            S.op("pool", lambda e: e.tensor_tensor(out=TtB[:, gs, :], in0=Tt[:, gs, :], in1=beta[:, gs].unsqueeze(2).to_broadcast([128, 4, 128]), op=ALU.mult),
                 reads=[r_Tt, r_g], writes=[r_TtB])
            pw, r_pw = nbp()

            def mmw(e):
                for hh, h in enumerate(hs):
                    ins_ = e.matmul(b4(pw)[:, hh, :], lhsT=kg[:, h, :], rhs=TtB[:, h, :], start=True, stop=True)
                return ins_
            S.op("pe", mmw, reads=[r_kg, r_TtB], writes=[r_pw])
            S.op("act", lambda e: e.activation(out=wT[b][0][:, gs, :], in_=b4(pw), func=AF.Copy), reads=[r_pw], writes=[wT[b][1]])
            pu, r_pu = nbp()

            def mmu(e):
                for hh, h in enumerate(hs):
                    ins_ = e.matmul(b4(pu)[:, hh, :], lhsT=TtB[:, h, :], rhs=vtm_t[:, h, :], start=True, stop=True)
                return ins_
            S.op("pe", mmu, reads=[r_TtB, r_vtm], writes=[r_pu])
            S.op("dve", lambda e: e.tensor_copy(out=u[b][0][:, gs, :], in_=b4(pu)), reads=[r_pu], writes=[u[b][1]])
            yield
            if is_x:
                pq, r_pq = nbp()
                pd2, r_pd2 = nbp()

                def mmq(e):
                    for hh, h in enumerate(hs):
                        ins_ = e.matmul(b4(pq)[:, hh, :], lhsT=kT_t[:, h, :], rhs=qT_t[:, h, :], start=True, stop=True)
                    return ins_
                S.op("pe", mmq, reads=[r_kT, r_qT], writes=[r_pq])

                def mmd2(e):
                    for hh, h in enumerate(hs):
                        ins_ = e.matmul(b4(pd2)[:, hh, :], lhsT=L2, rhs=R2[:, h, :], start=True, stop=True)
                    return ins_
                S.op("pe", mmd2, reads=[r_L2, r_R2], writes=[r_pd2])
                S.op("dve", lambda e: e.scalar_tensor_tensor(out=E1[:, gs, :], in0=b4(pd2), scalar=0.0, in1=msk[:, 4 + d, :].unsqueeze(1).to_broadcast([128, 4, 128]),
                                                             op0=ALU.min, op1=ALU.add), reads=[r_pd2, r_msk], writes=[r_E1])
                S.op("act", lambda e: e.activation(out=E1[:, gs, :], in_=E1[:, gs, :], func=AF.Exp), reads=[r_E1], writes=[r_E1])
                S.op("dve", lambda e: e.tensor_tensor(out=qkT[b][0][:, gs, :], in0=b4(pq), in1=E1[:, gs, :], op=ALU.mult), reads=[r_pq, r_E1], writes=[qkT[b][1]])
                yield
        for grp in range(2):
            yield from do_group(grp)

    def seq(ui):
        d, ti = units[ui]
        b = ui % NB
        is_x = ti >= 2
        S32_t, r_S32 = S32[d]
        Sbf_t, r_Sbf = Sbf[d]
        sm_t, r_sm = sm[b]
        wT_t, r_wT = wT[b]
        u_t, r_u = u[b]
        qT_t, r_qT = qT[b]
        qk_t, r_qk = qkT[b]
        kd_t, r_kd = kdec[b]
        banks = [k.bank(4 + j) for j in range(4)]

        def grp_mm(bank2, lhs_fn, rhs_fn, reads):
            for grp in range(2):
                pb, r_pb = bank2[grp]

                def f(e, grp=grp, pb=pb):
                    for hh in range(4):
                        h = 4 * grp + hh
                        ins_ = e.matmul(b4(pb)[:, hh, :], lhsT=lhs_fn(h), rhs=rhs_fn(h), start=True, stop=True)
                    return ins_
                S.op("pe", f, reads=reads, writes=[r_pb])
        grp_mm(banks[0:2], lambda h: wT_t[:, h, :], lambda h: Sbf_t[:, h, :], [r_wT, r_Sbf])
        S.op("pool", lambda e: e.tensor_tensor(out=S32_t, in0=S32_t, in1=sm_t[:, 1, :].unsqueeze(2).to_broadcast([128, 8, 128]), op=ALU.mult),
             reads=[r_S32, r_sm], writes=[r_S32])
        yield
        for grp in range(2):
            gs = slice(4 * grp, 4 * grp + 4)
            pb, r_pb = banks[grp]
            S.op("dve", lambda e, pb=pb, gs=gs: e.tensor_tensor(out=vnew[:, gs, :], in0=u_t[:, gs, :], in1=b4(pb), op=ALU.subtract),
                 reads=[r_u, r_pb], writes=[r_vnew])
        yield
        if is_x:
            grp_mm(banks[2:4], lambda h: qT_t[:, h, :], lambda h: Sbf_t[:, h, :], [r_qT, r_Sbf])
            grp_mm(banks[0:2], lambda h: qk_t[:, h, :], lambda h: vnew[:, h, :], [r_qk, r_vnew])
            yield
            o_tt, r_o = o_t[b]
            for grp in range(2):
                gs = slice(4 * grp, 4 * grp + 4)
                pbq, r_pbq = banks[2 + grp]
                pbc, r_pbc = banks[grp]
                S.op("dve", lambda e, pbq=pbq, gs=gs: e.tensor_tensor(out=tmpo[:, gs, :], in0=b4(pbq), in1=sm_t[:, 0, gs].unsqueeze(2).to_broadcast([128, 4, 128]), op=ALU.mult),
                     reads=[r_pbq, r_sm], writes=[r_tmpo])
                S.op("dve", lambda e, pbc=pbc, gs=gs, o_tt=o_tt: e.tensor_tensor(out=o_tt[:, gs, :], in0=tmpo[:, gs, :], in1=b4(pbc), op=ALU.add),
                     reads=[r_tmpo, r_pbc], writes=[r_o])
            xi = ti - 2
            k.store(o_s[d][0][xi * 128:(xi + 1) * 128, :], o_s[d][1], o_tt.rearrange("p h d -> p (h d)"), r_o)
            yield
        grp_mm(banks[2:4], lambda h: kd_t[:, h, :], lambda h: vnew[:, h, :], [r_kd, r_vnew])
        yield
        for grp in range(2):
            gs = slice(4 * grp, 4 * grp + 4)
            pb, r_pb = banks[2 + grp]
            S.op("dve", lambda e, pb=pb, gs=gs: e.tensor_tensor(out=S32_t[:, gs, :], in0=S32_t[:, gs, :], in1=b4(pb), op=ALU.add),
                 reads=[r_S32, r_pb], writes=[r_S32])
        S.op("act", lambda e: e.activation(out=Sbf_t, in_=S32_t, func=AF.Copy), reads=[r_S32], writes=[r_Sbf])
        yield

    import os as _os
    stop_after = int(_os.environ.get("B_UNITS", len(units)))
    pre_cut = int(_os.environ.get("B_PRE_CUT", 10000))
    do_seq = int(_os.environ.get("B_SEQ", 1))
    _pre = pre
    _seq = seq

    def pre(ui):
        for n_, _ in enumerate(_pre(ui)):
            if n_ + 1 >= pre_cut:
                return
            yield

    def seq(ui):
        if not do_seq:
            return
        yield from _seq(ui)
    loads(0)
    _drive(pre(0))
    for ui in range(stop_after):
        if ui + 1 < stop_after:
            loads(ui + 1)
            _drive(pre(ui + 1), seq(ui))
        else:
            _drive(seq(ui))
    k.b_state = (S32, Sbf)
    S.barrier()
    A.release()

def phase_c(k):
    S, A, nc, ins = k.S, k.A, k.nc, k.ins
    of_s, r_of_s = k.scr["of_s"]
    ob_s, r_ob_s = k.scr["ob_s"]
    zs_s, r_zs_s = k.scr["zs_s"]
    qmT_s, r_qmT_s = k.scr["qmT_s"]
    kmT_s, r_kmT_s = k.scr["kmT_s"]
    vm_s, r_vm_s = k.scr["vm_s"]
    yaT_s, r_yaT_s = k.scratch("yaT_s", [H, 128, TX], BF16)
    ybT_s, r_ybT_s = k.scratch("ybT_s", [H, 128, TX], BF16)
    A.mark()
    dng, r_dng = k.tile([128, 128], F32, "dng")
    k.load(dng, r_dng, ins["dng_rep"])
    NB = 2
    of_t = [k.tile([128, 8, 128], F32, "of") for _ in range(NB)]
    ob_t = [k.tile([128, 8, 128], F32, "ob") for _ in range(NB)]
    z_t = [k.tile([128, 8, 128], BF16, "z") for _ in range(NB)]
    sq, r_sq = k.tile([128, 8, 128], F32, "sq")
    ss8, r_ss8 = k.tile([128, 8], F32, "ss8")
    yb, r_yb = k.tile([128, 8, 128], BF16, "yb")
    yT = [k.tile([128, 8, 128], BF16, "yT") for _ in range(NB)]

    def c1_loads(xi):
        b = xi % NB
        rows = slice(xi * 128, (xi + 1) * 128)
        k.load(of_t[b][0], of_t[b][1], of_s[rows, :].rearrange("p (h d) -> p h d", h=8), r_of_s)
        k.load(ob_t[b][0], ob_t[b][1], ob_s[rows, :].rearrange("p (h d) -> p h d", h=8), r_ob_s, q="act")
        k.load(z_t[b][0], z_t[b][1], zs_s[rows, :].rearrange("p (h d) -> p h d", h=8), r_zs_s)

    c1_loads(0)
    for xi in range(32):
        b = xi % NB
        if xi + 1 < 32:
            c1_loads(xi + 1)
        o_, r_o = of_t[b]
        ob_, r_ob = ob_t[b]
        z_, r_z = z_t[b]
        S.op("dve", lambda e, o_=o_, ob_=ob_: e.tensor_tensor(out=o_, in0=o_, in1=ob_, op=ALU.add), reads=[r_o, r_ob], writes=[r_o])
        S.op("pool", lambda e, o_=o_: e.tensor_tensor(out=sq, in0=o_, in1=o_, op=ALU.mult), reads=[r_o], writes=[r_sq])
        S.op("dve", lambda e: e.tensor_reduce(out=ss8, in_=sq, axis=AX.X, op=ALU.add), reads=[r_sq], writes=[r_ss8])
        _rstd(k, ss8, r_ss8, 128, ss8, r_ss8)
        S.op("dve", lambda e, o_=o_: e.tensor_tensor(out=o_, in0=o_, in1=ss8.unsqueeze(2).to_broadcast([128, 8, 128]), op=ALU.mult),
             reads=[r_o, r_ss8], writes=[r_o])
        S.op("pool", lambda e, o_=o_: e.tensor_tensor(out=o_, in0=o_, in1=dng.unsqueeze(1).to_broadcast([128, 8, 128]), op=ALU.mult),
             reads=[r_o, r_dng], writes=[r_o])
        S.op("pool", lambda e, o_=o_, z_=z_: e.tensor_tensor(out=yb, in0=o_, in1=z_, op=ALU.mult), reads=[r_o, r_z], writes=[r_yb])
        pb, r_pb = _nb(k, BF16)
        pv = pb.rearrange("p (a b) -> p a b", b=128)

        def tr(e, pv=pv):
            for j in range(8):
                ins_ = e.transpose(out=pv[:, j, :], in_=yb[:, j, :], identity=k.ident_b)
            return ins_
        S.op("pe", tr, reads=[r_yb, k.r_ident_b], writes=[r_pb])
        yT_t, r_yT = yT[b]
        S.op("act", lambda e, pv=pv, yT_t=yT_t: e.activation(out=yT_t, in_=pv, func=AF.Copy), reads=[r_pb], writes=[r_yT])
        k.store(yaT_s[:, :, xi * 128:(xi + 1) * 128].rearrange("h d t -> d h t"), r_yaT_s, yT_t, r_yT)
    S.barrier()
    A.release()
    A.mark()
    Kn = [k.tile([128, T], BF16, "Kn") for _ in range(2)]
    Kr = [k.tile([64, T], BF16, "Kr") for _ in range(2)]
    Vh = [k.tile([128, NT, 128], BF16, "Vh") for _ in range(2)]
    Qn = [k.tile([128, 512], BF16, "Qn") for _ in range(2)]
    Qr = [k.tile([64, 512], BF16, "Qr") for _ in range(2)]
    NP = 4
    PT = [k.tile([128, 512], BF16, "PT") for _ in range(NP)]
    rinv, r_rinv = k.tile([128, 512], F32, "rinv")
    yo = [k.tile([128, 512], BF16, "yo") for _ in range(2)]
    vmv = vm_s.rearrange("(n p) c -> p n c", p=128)

    def head_loads(h):
        b = h % 2
        k.load(Kn[b][0], Kn[b][1], kmT_s[h, 0:128, :], r_kmT_s)
        k.load(Kr[b][0], Kr[b][1], kmT_s[h, 128:192, :], r_kmT_s, q="act")
        k.load(Vh[b][0], Vh[b][1], vmv[:, :, h * 128:(h + 1) * 128], r_vm_s)

    def q_loads(h, qg):
        b = (h * 8 + qg) % 2
        k.load(Qn[b][0], Qn[b][1], qmT_s[h, 0:128, qg * 512:(qg + 1) * 512], r_qmT_s)
        k.load(Qr[b][0], Qr[b][1], qmT_s[h, 128:192, qg * 512:(qg + 1) * 512], r_qmT_s, q="act")

    head_loads(0)
    q_loads(0, 0)
    sbank = [0]
    it = 0
    for h in range(H):
        if h + 1 < H:
            head_loads(h + 1)
        Kn_t, r_Kn = Kn[h % 2]
        Kr_t, r_Kr = Kr[h % 2]
        V_t, r_V = Vh[h % 2]
        for qg in range(8):
            gi = h * 8 + qg
            nxt = gi + 1
            if nxt < H * 8:
                q_loads(nxt // 8, nxt % 8)
            Qn_t, r_Qn = Qn[gi % 2]
            Qr_t, r_Qr = Qr[gi % 2]
            po, r_po = k.bank(4 + gi % 2)
            pr, r_pr = k.bank(6 + gi % 2)
            sb = {}

            def emit_s(kt):
                bnk = sbank[0]
                sbank[0] = (bnk + 1) % 4
                ps_, r_ps = k.bank(bnk)

                def f(e, ps_=ps_, kt=kt, Kn_t=Kn_t, Kr_t=Kr_t, Qn_t=Qn_t, Qr_t=Qr_t):
                    e.matmul(ps_, lhsT=Kn_t[:, kt * 128:(kt + 1) * 128], rhs=Qn_t, start=True, stop=False)
                    return e.matmul(ps_, lhsT=Kr_t[:, kt * 128:(kt + 1) * 128], rhs=Qr_t, start=False, stop=True)
                S.op("pe", f, reads=[r_Kn, r_Kr, r_Qn, r_Qr], writes=[r_ps])
                pt_, r_pt = PT[kt % NP]
                S.op("act", lambda e, ps_=ps_, pt_=pt_: e.activation(out=pt_, in_=ps_, func=AF.Exp), reads=[r_ps], writes=[r_pt])
                sb[kt] = (pt_, r_pt)

            def emit_pv(kt):
                pt_, r_pt = sb.pop(kt)

                def f(e, pt_=pt_, kt=kt, po=po, pr=pr, V_t=V_t):
                    e.matmul(po, lhsT=V_t[:, kt, :], rhs=pt_, start=(kt == 0), stop=(kt == NT - 1))
                    return e.matmul(pr, lhsT=k.ones_b, rhs=pt_, start=(kt == 0), stop=(kt == NT - 1))
                S.op("pe", f, reads=[r_V, r_pt, k.r_ones_b], writes=[r_po, r_pr])

            LOOK = 2
            for kt in range(min(LOOK, NT)):
                emit_s(kt)
            for kt in range(NT):
                if kt + LOOK < NT:
                    emit_s(kt + LOOK)
                emit_pv(kt)
            S.op("dve", lambda e, pr=pr: e.reciprocal(out=rinv, in_=pr), reads=[r_pr], writes=[r_rinv])
            yo_t, r_yo = yo[gi % 2]
            S.op("dve", lambda e, po=po, yo_t=yo_t: e.tensor_tensor(out=yo_t, in0=po, in1=rinv, op=ALU.mult), reads=[r_po, r_rinv], writes=[r_yo])
            k.store(ybT_s[h, :, qg * 512:(qg + 1) * 512], r_ybT_s, yo_t, r_yo)
    S.barrier()
    A.release()

def phase_d(k):
    S, A, nc, ins = k.S, k.A, k.nc, k.ins
    yaT_s, r_yaT_s = k.scr["yaT_s"]
    ybT_s, r_ybT_s = k.scr["ybT_s"]
    sgT_s, r_sgT_s = k.scr["sgT_s"]
    xmid_s, r_xmid_s = k.scratch("xmid_s", [TX, D], F32)
    h2_s, r_h2_s = k.scratch("h2_s", [TX, D], BF16)
    aff_s, r_aff_s = k.scratch("aff_s", [TX, 16], F32)
    affT_s, r_affT_s = k.scratch("affT_s", [16, TX], F32)
    A.mark()
    woa, r_woa = k.tile([128, 8, D], BF16, "woa")
    wob, r_wob = k.tile([128, 8, D], BF16, "wob")
    wo, r_wo = k.tile([128, 8, D], BF16, "wo")
    rw, r_rw = k.tile([128, 8, 16], F32, "rw")
    for (dst, rdst, nm) in ((woa, r_woa, "w_out_a"), (wob, r_wob, "w_out_b"), (wo, r_wo, "w_o")):
        src = ins[nm].rearrange("(kc p) n -> p kc n", p=128)
        for hf in range(2):
            k.wload(dst[:, 4 * hf:4 * hf + 4, :], rdst, src[:, 4 * hf:4 * hf + 4, :])
    k.load(rw, r_rw, ins["router_w"].rearrange("(kc p) n -> p kc n", p=128))
    NB = 2
    yaT = [k.tile([128, 8, 512], BF16, "yaT") for _ in range(NB)]
    ybT = [k.tile([128, 8, 512], BF16, "ybT") for _ in range(NB)]
    gA = [k.tile([128, 8, 512], BF16, "gA") for _ in range(NB)]
    gB = [k.tile([128, 8, 512], BF16, "gB") for _ in range(NB)]
    mg, r_mg = k.tile([128, 8, 512], BF16, "mg")
    t1 = [k.tile([128, 512], F32, "t1") for _ in range(2)]
    t2 = [k.tile([128, 512], F32, "t2") for _ in range(2)]
    xt = [k.tile([128, D], F32, "xt") for _ in range(NB)]
    xm = [k.tile([128, D], F32, "xm") for _ in range(NB)]
    junk, r_junk = k.tile([128, D], BF16, "junk")
    ssd = [k.tile([128, 8], F32, "ssd") for _ in range(NB)]
    h2f, r_h2f = k.tile([128, D], F32, "h2f")
    h2b = [k.tile([128, D], BF16, "h2b") for _ in range(NB)]
    h2T, r_h2T = k.tile([128, 8, 128], F32, "h2T")
    ex = [k.tile([128, 16], F32, "ex") for _ in range(NB)]
    affT = [k.tile([16, 128], F32, "affT") for _ in range(NB)]

    def g_loads(g):
        b = g % NB
        cols = slice(g * 512, (g + 1) * 512)
        k.load(yaT[b][0], yaT[b][1], yaT_s[:, :, cols].rearrange("h d t -> d h t"), r_yaT_s)
        k.load(ybT[b][0], ybT[b][1], ybT_s[:, :, cols].rearrange("h d t -> d h t"), r_ybT_s, q="act")
        k.load(gA[b][0], gA[b][1], sgT_s[0:8, :, cols].rearrange("h d t -> d h t"), r_sgT_s)
        k.load(gB[b][0], gB[b][1], sgT_s[8:16, :, cols].rearrange("h d t -> d h t"), r_sgT_s, q="act")

    g_loads(0)
    for g in range(8):
        b = g % NB
        if g + 1 < 8:
            g_loads(g + 1)
        ya_, r_ya = yaT[b]
        yb_, r_yb = ybT[b]
        gA_, r_gA = gA[b]
        gB_, r_gB = gB[b]
        for oc in range(8):
            pa, r_pa = _nb(k)
            pbk, r_pbk = _nb(k)

            def mma(e, pa=pa, oc=oc, ya_=ya_):
                for kc in range(8):
                    ins_ = e.matmul(pa, lhsT=woa[:, kc, oc * 128:(oc + 1) * 128], rhs=ya_[:, kc, :], start=(kc == 0), stop=(kc == 7))
                return ins_
            S.op("pe", mma, reads=[r_woa, r_ya], writes=[r_pa])

            def mmb(e, pbk=pbk, oc=oc, yb_=yb_):
                for kc in range(8):
                    ins_ = e.matmul(pbk, lhsT=wob[:, kc, oc * 128:(oc + 1) * 128], rhs=yb_[:, kc, :], start=(kc == 0), stop=(kc == 7))
                return ins_
            S.op("pe", mmb, reads=[r_wob, r_yb], writes=[r_pbk])
            t1_, r_t1 = t1[oc % 2]
            t2_, r_t2 = t2[oc % 2]
            S.op("dve", lambda e, pa=pa, oc=oc, t1_=t1_, gA_=gA_: e.tensor_tensor(out=t1_, in0=pa, in1=gA_[:, oc, :], op=ALU.mult), reads=[r_pa, r_gA], writes=[r_t1])
            S.op("dve", lambda e, pbk=pbk, oc=oc, t2_=t2_, gB_=gB_: e.tensor_tensor(out=t2_, in0=pbk, in1=gB_[:, oc, :], op=ALU.mult), reads=[r_pbk, r_gB], writes=[r_t2])
            S.op("pool", lambda e, oc=oc, t1_=t1_, t2_=t2_: e.tensor_tensor(out=mg[:, oc, :], in0=t1_, in1=t2_, op=ALU.add), reads=[r_t1, r_t2], writes=[r_mg])
        for tt in range(4):
            ti = g * 4 + tt
            tb = ti % NB
            rows = slice(ti * 128, (ti + 1) * 128)
            x_, r_x = xt[tb]
            xm_, r_xm = xm[tb]
            ss_, r_ss = ssd[tb]
            k.load(x_, r_x, ins["x"][rows, :])
            for hf in range(2):
                pm, r_pm = _nb(k)

                def mmo(e, pm=pm, hf=hf, tt=tt):
                    for kc in range(8):
                        ins_ = e.matmul(pm, lhsT=mg[:, kc, tt * 128:(tt + 1) * 128], rhs=wo[:, kc, hf * 512:(hf + 1) * 512], start=(kc == 0), stop=(kc == 7))
                    return ins_
                S.op("pe", mmo, reads=[r_mg, r_wo], writes=[r_pm])
                S.op("dve", lambda e, pm=pm, hf=hf, xm_=xm_: e.tensor_tensor(out=xm_[:, hf * 512:(hf + 1) * 512], in0=pm, in1=k.gate1_row[:, hf * 512:(hf + 1) * 512], op=ALU.mult),
                     reads=[r_pm, k.r_gate1], writes=[r_xm])
            S.op("pool", lambda e, xm_=xm_, x_=x_: e.tensor_tensor(out=xm_, in0=xm_, in1=x_, op=ALU.add), reads=[r_xm, r_x], writes=[r_xm])
            k.store(xmid_s[rows, :], r_xmid_s, xm_, r_xm)
            k.store(k.out[rows, :], k.out_res, xm_, r_xm)
            S.op("act", lambda e, xm_=xm_, ss_=ss_: e.activation(out=junk, in_=xm_, func=AF.Square, accum_out=ss_[:, 0:1]), reads=[r_xm], writes=[r_junk, r_ss])
            _rstd(k, ss_[:, 0:1], r_ss, D, ss_[:, 1:2], r_ss)
            S.op("act", lambda e, xm_=xm_, ss_=ss_: e.activation(out=h2f, in_=xm_, func=AF.Copy, scale=ss_[:, 1:2]), reads=[r_xm, r_ss], writes=[r_h2f])
            S.op("pool", lambda e: e.tensor_tensor(out=h2f, in0=h2f, in1=k.s2_row, op=ALU.mult), reads=[r_h2f, k.r_s2row], writes=[r_h2f])
            S.op("pool", lambda e: e.tensor_tensor(out=h2f, in0=h2f, in1=k.shift2_row, op=ALU.add), reads=[r_h2f, k.r_shift2], writes=[r_h2f])
            h2b_, r_h2b = h2b[tb]
            S.op("act", lambda e, h2b_=h2b_: e.activation(out=h2b_, in_=h2f, func=AF.Copy), reads=[r_h2f], writes=[r_h2b])
            k.store(h2_s[rows, :], r_h2_s, h2b_, r_h2b)
            for hf in range(2):
                pt, r_pt = _nb(k)
                pv = pt.rearrange("p (a b) -> p a b", b=128)

                def trh(e, pv=pv, hf=hf):
                    for j in range(4):
                        ins_ = e.transpose(out=pv[:, j, :], in_=h2f[:, (hf * 4 + j) * 128:(hf * 4 + j + 1) * 128], identity=k.ident_f)
                    return ins_
                S.op("pe", trh, reads=[r_h2f, k.r_ident_f], writes=[r_pt])
                if hf == 0:
                    S.op("act", lambda e, pv=pv: e.activation(out=h2T[:, 0:4, :], in_=pv, func=AF.Copy), reads=[r_pt], writes=[r_h2T])
                else:
                    S.op("dve", lambda e, pv=pv: e.tensor_copy(out=h2T[:, 4:8, :], in_=pv), reads=[r_pt], writes=[r_h2T])
            pl, r_pl = _nb(k)

            def mml(e, pl=pl):
                for kc in range(8):
                    ins_ = e.matmul(pl[:, 0:16], lhsT=h2T[:, kc, :], rhs=rw[:, kc, :], start=(kc == 0), stop=(kc == 7))
                return ins_
            S.op("pe", mml, reads=[r_h2T, r_rw], writes=[r_pl])
            ex_, r_ex = ex[tb]
            S.op("dve", lambda e, pl=pl, ss_=ss_: e.tensor_reduce(out=ss_[:, 2:3], in_=pl[:, 0:16], axis=AX.X, op=ALU.max), reads=[r_pl], writes=[r_ss])
            S.op("dve", lambda e, ss_=ss_: e.tensor_scalar(out=ss_[:, 3:4], in0=ss_[:, 2:3], scalar1=-1.0, scalar2=None, op0=ALU.mult), reads=[r_ss], writes=[r_ss])
            S.op("act", lambda e, pl=pl, ss_=ss_, ex_=ex_: e.activation(out=ex_, in_=pl[:, 0:16], func=AF.Exp, bias=ss_[:, 3:4], accum_out=ss_[:, 4:5]),
                 reads=[r_pl, r_ss], writes=[r_ex, r_ss])
            S.op("dve", lambda e, ss_=ss_: e.reciprocal(out=ss_[:, 5:6], in_=ss_[:, 4:5]), reads=[r_ss], writes=[r_ss])
            S.op("dve", lambda e, ss_=ss_, ex_=ex_: e.tensor_scalar(out=ex_, in0=ex_, scalar1=ss_[:, 5:6], scalar2=None, op0=ALU.mult), reads=[r_ex, r_ss], writes=[r_ex])
            k.store(aff_s[rows, :], r_aff_s, ex_, r_ex)
            pt2, r_pt2 = _nb(k)
            S.op("pe", lambda e, pt2=pt2, ex_=ex_: e.transpose(out=pt2[0:16, 0:128], in_=ex_, identity=k.ident_f), reads=[r_ex, k.r_ident_f], writes=[r_pt2])
            aT_, r_aT = affT[tb]
            S.op("act", lambda e, pt2=pt2, aT_=aT_: e.activation(out=aT_, in_=pt2[0:16, 0:128], func=AF.Copy), reads=[r_pt2], writes=[r_aT])
            k.store(affT_s[:, rows], r_affT_s, aT_, r_aT)
    S.barrier()
    A.release()

NE = 16
CAP = 512
FF = 1408
NFC = 11


def phase_e(k):
    S, A, nc, ins = k.S, k.A, k.nc, k.ins
    aff_s, r_aff_s = k.scr["aff_s"]
    affT_s, r_affT_s = k.scr["affT_s"]
    h2_s, r_h2_s = k.scr["h2_s"]
    xmid_s, r_xmid_s = k.scr["xmid_s"]
    posmT_s, r_posmT_s = k.scratch("posmT_s", [NE, TX], F32)
    gc_s, r_gc_s = k.scratch("gc_s", [NE, 128, 4], F32)
    idx_s, r_idx_s = k.scratch("idx_s", [NE, 128, 4], I32)
    A.off = k.off_after_gate2
    A.mark()
    cst, r_cst = k.tile([128, 1024], F32, "cst")
    k.load(cst, r_cst, ins["consts"])
    blk, r_blk = k.tile([128, 128], F32, "blk")
    k.load(blk, r_blk, ins["moe_blk"])
    sel8, r_sel8 = k.tile([128, 16], F32, "sel8")
    k.load(sel8, r_sel8, ins["moe_sel8"])
    tris, r_tris = k.tile([128, 128], BF16, "tris")
    k.wload(tris, r_tris, ins["moe_tris"])
    iota_c = cst[:, 0:512]
    A.mark()
    A8, r_A8 = k.tile([128, 512], F32, "A8")
    k.load(A8, r_A8, affT_s.rearrange("e (s t) -> (e s) t", s=8), r_affT_s)
    junk, r_junk = k.tile([128, 512], F32, "junk")
    sc, r_sc = k.tile([128, 16], F32, "sc")
    S.op("pool", lambda e: e.memset(sc, 0.0), writes=[r_sc])
    S.op("pool", lambda e: e.memset(sc[:, 1:2], 1.0), reads=[r_sc], writes=[r_sc])
    for it in range(30):
        S.op("dve", lambda e: e.tensor_tensor(out=sc[:, 2:3], in0=sc[:, 0:1], in1=sc[:, 1:2], op=ALU.add), reads=[r_sc], writes=[r_sc])
        S.op("dve", lambda e: e.tensor_scalar(out=sc[:, 2:3], in0=sc[:, 2:3], scalar1=0.5, scalar2=None, op0=ALU.mult), reads=[r_sc], writes=[r_sc])
        S.op("dve", lambda e: e.tensor_scalar(out=junk, in0=A8, scalar1=sc[:, 2:3], scalar2=0.0, op0=ALU.is_ge, op1=ALU.add, accum_out=sc[:, 3:4]),
             reads=[r_A8, r_sc], writes=[r_junk, r_sc])
        pb, r_pb = _nb(k)
        S.op("pe", lambda e, pb=pb: e.matmul(pb[:, 0:1], lhsT=blk, rhs=sc[:, 3:4], start=True, stop=True), reads=[r_blk, r_sc], writes=[r_pb])
        S.op("dve", lambda e, pb=pb: e.tensor_scalar(out=sc[:, 4:5], in0=pb[:, 0:1], scalar1=CAP - 0.5, scalar2=None, op0=ALU.is_ge), reads=[r_pb], writes=[r_sc])
        S.op("dve", lambda e: e.tensor_scalar(out=sc[:, 5:6], in0=sc[:, 4:5], scalar1=-1.0, scalar2=1.0, op0=ALU.mult, op1=ALU.add), reads=[r_sc], writes=[r_sc])
        S.op("dve", lambda e: e.tensor_tensor(out=sc[:, 6:7], in0=sc[:, 2:3], in1=sc[:, 0:1], op=ALU.subtract), reads=[r_sc], writes=[r_sc])
        S.op("dve", lambda e: e.tensor_tensor(out=sc[:, 7:8], in0=sc[:, 1:2], in1=sc[:, 2:3], op=ALU.subtract), reads=[r_sc], writes=[r_sc])
        S.op("dve", lambda e: e.scalar_tensor_tensor(out=sc[:, 0:1], in0=sc[:, 6:7], scalar=sc[:, 4:5], in1=sc[:, 0:1], op0=ALU.mult, op1=ALU.add), reads=[r_sc], writes=[r_sc])
        S.op("dve", lambda e: e.scalar_tensor_tensor(out=sc[:, 1:2], in0=sc[:, 7:8], scalar=sc[:, 4:5], in1=sc[:, 2:3], op0=ALU.mult, op1=ALU.add), reads=[r_sc], writes=[r_sc])
        S.op("dve", lambda e: e.memset(sc[:, 3:4], 0.0), reads=[r_sc], writes=[r_sc])
    thrrep, r_thrrep = k.tile([128, 128], F32, "thrrep")
    S.op("dve", lambda e: e.tensor_copy(out=thrrep, in_=sc[:, 0:1].to_broadcast([128, 128])), reads=[r_sc], writes=[r_thrrep])
    pb, r_pb = _nb(k)
    S.op("pe", lambda e, pb=pb: e.matmul(pb[:, 0:16], lhsT=thrrep, rhs=sel8, start=True, stop=True), reads=[r_thrrep, r_sel8], writes=[r_pb])
    thr_row, r_thr = k.tile([128, 16], F32, "thr_row")
    S.op("act", lambda e, pb=pb: e.activation(out=thr_row, in_=pb[:, 0:16], func=AF.Copy), reads=[r_pb], writes=[r_thr])
    import os as _os
    if _os.environ.get("E_DBG"):
        k.dump("sc", sc, r_sc, [128, 16])
        k.dump("thr_row", thr_row, r_thr, [128, 16])
        k.dump("A8", A8, r_A8, [128, 512])
        S.barrier()
        A.release()
        A.release()
        return
    aff, r_aff = k.tile([128, 32, 16], F32, "aff")
    k.load(aff, r_aff, aff_s.rearrange("(n p) e -> p n e", p=128), r_aff_s)
    maskf, r_maskf = k.tile([128, 32, 16], F32, "maskf")
    maskb, r_maskb = k.tile([128, 32, 16], BF16, "maskb")
    posm, r_posm = k.tile([128, 32, 16], F32, "posm")
    parts, r_parts = k.tile([128, 32, 16, 5], BF16, "parts")
    tokp, r_tokp = k.tile([128, 32, 2], F32, "tokp")
    k.load(tokp, r_tokp, ins["moe_tok"])
    rem, r_rem = k.tile([128, 32, 16], F32, "rem")
    S.op("dve", lambda e: e.tensor_tensor(out=maskf, in0=aff, in1=thr_row.unsqueeze(1).to_broadcast([128, 32, 16]), op=ALU.is_ge),
         reads=[r_aff, r_thr], writes=[r_maskf])
    S.op("act", lambda e: e.activation(out=maskb, in_=maskf, func=AF.Copy), reads=[r_maskf], writes=[r_maskb])
    pp, r_pp = _nb(k)
    ppv = pp.rearrange("p (n e) -> p n e", e=16)

    def mmpos(e):
        for n in range(32):
            for m in range(n):
                e.matmul(ppv[:, n, :], lhsT=k.ones_b, rhs=maskb[:, m, :], start=(m == 0), stop=False)
            ins_ = e.matmul(ppv[:, n, :], lhsT=tris, rhs=maskb[:, n, :], start=(n == 0), stop=True)
        return ins_
    S.op("pe", mmpos, reads=[r_maskb, k.r_ones_b, r_tris], writes=[r_pp])
    S.op("dve", lambda e: e.scalar_tensor_tensor(out=posm, in0=ppv, scalar=1.0, in1=maskf, op0=ALU.add, op1=ALU.mult), reads=[r_pp, r_maskf], writes=[r_posm])
    S.op("dve", lambda e: e.tensor_scalar(out=posm, in0=posm, scalar1=-1.0, scalar2=None, op0=ALU.add), reads=[r_posm], writes=[r_posm])
    S.op("act", lambda e: e.activation(out=parts[:, :, :, 0], in_=aff, func=AF.Copy), reads=[r_aff], writes=[r_parts])
    S.op("dve", lambda e: e.tensor_tensor(out=rem, in0=aff, in1=parts[:, :, :, 0], op=ALU.subtract), reads=[r_aff, r_parts], writes=[r_rem])
    S.op("act", lambda e: e.activation(out=parts[:, :, :, 1], in_=rem, func=AF.Copy), reads=[r_rem], writes=[r_parts])
    S.op("dve", lambda e: e.tensor_tensor(out=rem, in0=rem, in1=parts[:, :, :, 1], op=ALU.subtract), reads=[r_rem, r_parts], writes=[r_rem])
    S.op("act", lambda e: e.activation(out=parts[:, :, :, 2], in_=rem, func=AF.Copy), reads=[r_rem], writes=[r_parts])
    S.op("dve", lambda e: e.tensor_copy(out=parts[:, :, :, 3:5], in_=tokp.unsqueeze(2).to_broadcast([128, 32, 16, 2])), reads=[r_tokp, r_parts], writes=[r_parts])
    pmTs = [k.tile([16, 512], F32, "pmT") for _ in range(2)]
    for g in range(8):
        pt, r_pt = _nb(k)

        def trp(e, pt=pt, g=g):
            for j in range(4):
                ins_ = e.transpose(out=pt[0:16, j * 128:(j + 1) * 128], in_=posm[:, g * 4 + j, :], identity=k.ident_f)
            return ins_
        S.op("pe", trp, reads=[r_posm, k.r_ident_f], writes=[r_pt])
        pmT, r_pmT = pmTs[g % 2]
        S.op("act", lambda e, pt=pt, pmT=pmT: e.activation(out=pmT, in_=pt[0:16, :], func=AF.Copy), reads=[r_pt], writes=[r_pmT])
        k.store(posmT_s[:, g * 512:(g + 1) * 512], r_posmT_s, pmT, r_pmT)
    if _os.environ.get("E_STOP") == "e1":
        S.barrier(); A.release(); A.release(); return
    Sel = [k.tile([128, 32, CAP], BF16, "Sel") for _ in range(2)]
    Sel_res = [(Res("sel0"), Res("sel1")) for _ in range(2)]
    gcs = [k.tile([128, 4], F32, "gcs") for _ in range(2)]
    idf = [k.tile([128, 4], F32, "idf") for _ in range(2)]
    idi = [k.tile([128, 4], I32, "idi") for _ in range(2)]
    for ex in range(NE):
        Sel_t, _ = Sel[ex % 2]
        r_Sel0, r_Sel1 = Sel_res[ex % 2]
        gcs_t, r_gcs = gcs[ex % 2]
        idf_t, r_idf = idf[ex % 2]
        idi_t, r_idi = idi[ex % 2]

        def bsel(e, Sel_t=Sel_t, ex=ex, par=0):
            for n in range(par, 32, 2):
                ins_ = e.tensor_scalar(out=Sel_t[:, n, :], in0=iota_c, scalar1=posm[:, n, ex:ex + 1], scalar2=None, op0=ALU.is_equal)
            return ins_
        S.op("dve", lambda e, f=bsel: f(e, par=0), reads=[r_cst, r_posm], writes=[r_Sel0])
        S.op("pool", lambda e, f=bsel: f(e, par=1), reads=[r_cst, r_posm], writes=[r_Sel1])
        pq, r_pq = _nb(k)

        def mmgate(e, pq=pq, Sel_t=Sel_t, ex=ex):
            for cc in range(4):
                for n in range(32):
                    ins_ = e.matmul(pq[:, cc * 8:cc * 8 + 5], lhsT=Sel_t[:, n, cc * 128:(cc + 1) * 128], rhs=parts[:, n, ex, :], start=(n == 0), stop=(n == 31))
            return ins_
        S.op("pe", mmgate, reads=[r_Sel0, r_Sel1, r_parts], writes=[r_pq])
        pq3 = pq[:, 0:32].rearrange("p (a b) -> p a b", b=8)
        S.op("dve", lambda e, pq3=pq3, gcs_t=gcs_t: e.tensor_reduce(out=gcs_t, in_=pq3[:, :, 0:3], axis=AX.X, op=ALU.add), reads=[r_pq], writes=[r_gcs])
        S.op("dve", lambda e, pq3=pq3, idf_t=idf_t: e.tensor_scalar(out=idf_t, in0=pq3[:, :, 3], scalar1=128.0, scalar2=None, op0=ALU.mult), reads=[r_pq], writes=[r_idf])
        S.op("dve", lambda e, pq3=pq3, idf_t=idf_t: e.tensor_tensor(out=idf_t, in0=idf_t, in1=pq3[:, :, 4], op=ALU.add), reads=[r_pq, r_idf], writes=[r_idf])
        S.op("dve", lambda e, idf_t=idf_t, idi_t=idi_t: e.tensor_copy(out=idi_t, in_=idf_t), reads=[r_idf], writes=[r_idi])
        k.store(gc_s[ex], r_gc_s, gcs_t, r_gcs)
        k.store(idx_s[ex], r_idx_s, idi_t, r_idi)
    S.barrier()
    A.release()
    if _os.environ.get("E_STOP") == "ea1":
        A.release(); return
    A.mark()
    U32 = mybir.dt.uint32
    ig = S.pool("ig", 4)
    wg = [k.tile([128, 8, FF], BF16, "wg") for _ in range(2)]
    wu = [k.tile([128, 8, FF], BF16, "wu") for _ in range(2)]
    wd = [k.tile([128, NFC, D], BF16, "wd") for _ in range(1)]
    xg = [k.tile([128, 4, D], BF16, "xg") for _ in range(2)]
    idx2 = [k.tile([128, 4], I32, "idx2") for _ in range(2)]
    gc2 = [k.tile([128, 4], F32, "gc2") for _ in range(2)]
    xeT_t, r_xeT = k.tile([128, 8, CAP], BF16, "xeT")
    hid, r_hid = k.tile([128, NFC, CAP], BF16, "hid")
    sg = [k.tile([128, CAP], F32, "sg") for _ in range(2)]
    yef = [k.tile([128, D], F32, "yef") for _ in range(4)]
    r_outacc = Res("outacc")
    h2_rows = h2_s

    def w_loads(ex):
        b = ex % 2
        srcg = ins["w_gate"][ex].rearrange("(kc p) f -> p kc f", p=128)
        srcu = ins["w_up"][ex].rearrange("(kc p) f -> p kc f", p=128)
        for j in range(4):
            k.wload(wg[b][0][:, 2 * j:2 * j + 2, :], wg[b][1], srcg[:, 2 * j:2 * j + 2, :])
        for j in range(4):
            k.wload(wu[b][0][:, 2 * j:2 * j + 2, :], wu[b][1], srcu[:, 2 * j:2 * j + 2, :])

    def wd_loads(ex):
        srcd = ins["w_down"][ex].rearrange("(fc p) d -> p fc d", p=128)
        for (a0, a1) in ((0, 3), (3, 6), (6, 9), (9, 11)):
            k.wload(wd[0][0][:, a0:a1, :], wd[0][1], srcd[:, a0:a1, :])

    def g_loads(ex):
        b = ex % 2
        k.load(idx2[b][0], idx2[b][1], idx_s[ex], r_idx_s)
        k.load(gc2[b][0], gc2[b][1], gc_s[ex], r_gc_s, q="act")
        for cc in range(4):
            S.dma("pool", ig, lambda e, b=b, cc=cc: e.indirect_dma_start(out=xg[b][0][:, cc, :], out_offset=None, in_=h2_rows,
                                                                       in_offset=bass.IndirectOffsetOnAxis(idx2[b][0].bitcast(U32)[:, cc:cc + 1], 0)),
                  reads=[idx2[b][1], r_h2_s], writes=[xg[b][1]])

    g_loads(0)
    w_loads(0)
    for ex in range(NE):
        b = ex % 2
        if ex + 1 < NE:
            g_loads(ex + 1)
            w_loads(ex + 1)
        wd_loads(ex)
        wg_t, r_wg = wg[b]
        wu_t, r_wu = wu[b]
        wd_t, r_wd = wd[0]
        xg_t, r_xg = xg[b]
        gc_t, r_gc = gc2[b]
        id_t, r_id = idx2[b]
        for kc in range(8):
            pt, r_pt = _nb(k, BF16)

            def trx(e, pt=pt, kc=kc, xg_t=xg_t):
                for cc in range(4):
                    ins_ = e.transpose(out=pt[:, cc * 128:(cc + 1) * 128], in_=xg_t[:, cc, kc * 128:(kc + 1) * 128], identity=k.ident_b)
                return ins_
            S.op("pe", trx, reads=[r_xg, k.r_ident_b], writes=[r_pt])
            if kc % 2 == 0:
                S.op("act", lambda e, pt=pt, kc=kc: e.activation(out=xeT_t[:, kc, :], in_=pt[:, 0:512], func=AF.Copy), reads=[r_pt], writes=[r_xeT])
            else:
                S.op("dve", lambda e, pt=pt, kc=kc: e.tensor_copy(out=xeT_t[:, kc, :], in_=pt[:, 0:512]), reads=[r_pt], writes=[r_xeT])
        for fc in range(NFC):
            pg, r_pg = _nb(k)
            pu, r_pu = _nb(k)

            def mmG(e, pg=pg, fc=fc, wg_t=wg_t):
                for kc in range(8):
                    ins_ = e.matmul(pg, lhsT=wg_t[:, kc, fc * 128:(fc + 1) * 128], rhs=xeT_t[:, kc, :], start=(kc == 0), stop=(kc == 7))
                return ins_
            S.op("pe", mmG, reads=[r_wg, r_xeT], writes=[r_pg])

            def mmU(e, pu=pu, fc=fc, wu_t=wu_t):
                for kc in range(8):
                    ins_ = e.matmul(pu, lhsT=wu_t[:, kc, fc * 128:(fc + 1) * 128], rhs=xeT_t[:, kc, :], start=(kc == 0), stop=(kc == 7))
                return ins_
            S.op("pe", mmU, reads=[r_wu, r_xeT], writes=[r_pu])
            sg_t, r_sg = sg[fc % 2]
            S.op("act", lambda e, pg=pg, sg_t=sg_t: e.activation(out=sg_t, in_=pg, func=AF.Silu), reads=[r_pg], writes=[r_sg])
            S.op("dve", lambda e, pu=pu, sg_t=sg_t, fc=fc: e.tensor_tensor(out=hid[:, fc, :], in0=pu, in1=sg_t, op=ALU.mult), reads=[r_pu, r_sg], writes=[r_hid])
        for cc in range(4):
            y_t, r_y = yef[cc]
            for hf in range(2):
                pd, r_pd = _nb(k)

                def mmD(e, pd=pd, cc=cc, hf=hf):
                    for fc in range(NFC):
                        ins_ = e.matmul(pd, lhsT=hid[:, fc, cc * 128:(cc + 1) * 128], rhs=wd_t[:, fc, hf * 512:(hf + 1) * 512], start=(fc == 0), stop=(fc == NFC - 1))
                    return ins_
                S.op("pe", mmD, reads=[r_hid, r_wd], writes=[r_pd])
                S.op("act", lambda e, pd=pd, cc=cc, hf=hf, y_t=y_t, gc_t=gc_t: e.activation(out=y_t[:, hf * 512:(hf + 1) * 512], in_=pd, func=AF.Copy, scale=gc_t[:, cc:cc + 1]),
                     reads=[r_pd, r_gc], writes=[r_y])
            S.op("pool", lambda e, y_t=y_t: e.tensor_tensor(out=y_t, in0=y_t, in1=k.gate2_row, op=ALU.mult), reads=[r_y, k.r_gate2], writes=[r_y])
            S.dma("pool", ig, lambda e, y_t=y_t, id_t=id_t, cc=cc: e.indirect_dma_start(out=k.out, out_offset=bass.IndirectOffsetOnAxis(id_t.bitcast(U32)[:, cc:cc + 1], 0),
                                                                                   in_=y_t, in_offset=None, compute_op=ALU.add),
                  reads=[r_y, r_id, k.out_res], writes=[r_outacc])
    S.barrier()
    A.release()
    A.release()

def _rope_tables():
    rows, gw = 64, 64
    row = np.repeat(np.arange(rows), gw).astype(np.float32)
    col = np.tile(np.arange(gw), rows).astype(np.float32)
    n_freq = 16
    inv_freq = (10000.0 ** (-np.arange(n_freq, dtype=np.float32) / n_freq)).astype(np.float32)
    ang_r = row[:, None] * inv_freq
    ang_c = col[:, None] * inv_freq
    ang = np.concatenate([ang_r, ang_r, ang_c, ang_c], axis=-1).astype(np.float32)
    cos = np.cos(ang).astype(np.float32)
    sin = np.sin(ang).astype(np.float32)
    sgn = np.concatenate([-np.ones(16), np.ones(16), -np.ones(16), np.ones(16)]).astype(np.float32)
    return cos, (sin * sgn).astype(np.float32)


def prep_shared(inp):
    f = np.float32
    sh = {}
    sh["ada_w"] = np.ascontiguousarray(inp["ada_w"][0])
    sh["ada_b_row"] = np.ascontiguousarray(inp["ada_b"][0][None, :])
    sh["ada_bT"] = np.ascontiguousarray(inp["ada_b"][0].reshape(48, 128).T)
    sh["g1T"] = np.ascontiguousarray(inp["norm1_g"][0].reshape(8, 128).T)
    sh["g2T"] = np.ascontiguousarray(inp["norm2_g"][0].reshape(8, 128).T)
    sh["g2_rep"] = np.ascontiguousarray(np.broadcast_to(inp["norm2_g"][0].reshape(1, 1024), (128, 1024)))
    sh["w_in"] = np.ascontiguousarray(inp["w_in"][0])
    sh["convT"] = np.ascontiguousarray(inp["conv_w"][0].T.reshape(24, 128, 5).transpose(1, 0, 2))
    sh["alog_rep"] = np.ascontiguousarray(np.broadcast_to(inp["a_log"][0].reshape(1, 16), (128, 16)))
    sh["dtb_rep"] = np.ascontiguousarray(np.broadcast_to(inp["dt_bias"][0].reshape(1, 16), (128, 16)))
    sh["dng_rep"] = np.ascontiguousarray(np.broadcast_to(inp["dn_norm_g"][0].reshape(1, 128), (128, 128)))
    sh["gqaT"] = np.ascontiguousarray(inp["q_a_norm_g"][0].reshape(3, 128).T)
    sh["w_uq"] = np.ascontiguousarray(inp["w_uq"][0])
    sh["gkvaT"] = np.ascontiguousarray(inp["kv_a_norm_g"][0].reshape(2, 128).T)
    sh["w_ukv"] = np.ascontiguousarray(inp["w_ukv"][0])
    sh["gq_rep"] = np.ascontiguousarray(np.broadcast_to(inp["q_norm_g"][0].reshape(1, 192), (128, 192)))
    sh["gk_rep"] = np.ascontiguousarray(np.broadcast_to(inp["k_norm_g"][0].reshape(1, 192), (128, 192)))
    sh["w_out_a"] = np.ascontiguousarray(inp["w_out_a"][0])
    sh["w_out_b"] = np.ascontiguousarray(inp["w_out_b"][0])
    sh["w_o"] = np.ascontiguousarray(inp["w_o"][0])
    sh["router_w"] = np.ascontiguousarray(inp["router_w"][0])
    sh["w_gate"] = np.ascontiguousarray(inp["w_gate"][0])
    sh["w_up"] = np.ascontiguousarray(inp["w_up"][0])
    sh["w_down"] = np.ascontiguousarray(inp["w_down"][0])
    cos, sinS = _rope_tables()
    sh["rope_cs"] = np.ascontiguousarray(np.concatenate([cos, sinS], axis=1))
    sh["ident"] = np.eye(128, dtype=f)
    consts = np.zeros((128, 1024), f)
    consts[:, 0:512] = np.arange(512, dtype=f)[None, :]
    consts[:, 512] = np.arange(128, dtype=f)
    sh["consts"] = consts
    pp_ = np.arange(128)
    sh["moe_blk"] = (pp_[:, None] // 8 == pp_[None, :] // 8).astype(f)
    s8 = np.zeros((128, 16), f)
    s8[np.arange(16) * 8, np.arange(16)] = 1.0
    sh["moe_sel8"] = s8
    tk = np.zeros((128, 32, 2), f)
    tk[:, :, 0] = np.arange(32, dtype=f)[None, :]
    tk[:, :, 1] = np.arange(128, dtype=f)[:, None]
    sh["moe_tok"] = tk
    sh["moe_tris"] = (pp_[:, None] < pp_[None, :]).astype(f)
    ii = np.arange(128)
    P, Fr = ii[:, None], ii[None, :]
    NEGV = -30000.0
    mk = np.zeros((128, 9, 128), f)
    mk[:, 0] = (P <= Fr)
    mk[:, 1] = (P >= Fr)
    mk[:, 2] = np.where(P > Fr, 0.0, NEGV)
    mk[:, 3] = np.where(P < Fr, 0.0, NEGV)
    mk[:, 4] = np.where(Fr >= P, 0.0, NEGV)
    mk[:, 5] = np.where(Fr <= P, 0.0, NEGV)
    mk[:, 6] = (P // 32 == Fr // 32)
    mk[:, 7] = (P // 64 == Fr // 64) & (P // 32 != Fr // 32)
    mk[:, 8] = (P // 64 != Fr // 64)
    sh["dn_masks"] = mk
    es = np.zeros((64, 2, 8, 128), f)
    for hh in range(8):
        es[hh, 0, hh, :] = 1.0
        es[32 + hh, 0, hh, :] = 1.0
    es[:, 1] = -es[:, 0]
    sh["dn_esel"] = es
    li = np.zeros((64, 128), f)
    li[32:40] = 1.0
    sh["dn_linit"] = li
    return sh


def prep_core(inp, sh, b):
    m = dict(sh)
    m["x"] = np.ascontiguousarray(inp["x"][b])
    m["ctx"] = np.ascontiguousarray(inp["ctx"][b])
    cc = np.stack([inp["c"][b], inp["c_ctx"]], axis=-1).astype(np.float32)
    m["c2"] = np.ascontiguousarray(cc.reshape(8, 128, 2).transpose(1, 0, 2))
    return m

PHASES = ["a0", "a1", "a2", "b", "c", "d", "e"]


def build(upto="e", dbg=(), dumps=()):
    k = K(dbg=dbg)
    declare_inputs(k)
    setup_consts(k)
    k.dump_list = []

    def dump(name, ap, res, shape):
        t = k.nc.dram_tensor("dbg_" + name, list(shape), ap.dtype, kind="ExternalOutput").ap()
        k.store(t, None, ap, res)
        k.dump_list.append("dbg_" + name)
    k.dump = dump
    k.dumps = set(dumps)
    fns = {"a0": phase_a0}
    for nm in ("a1", "a2", "b", "c", "d", "e"):
        f = globals().get("phase_" + nm)
        if f is not None:
            fns[nm] = f
    for ph in PHASES:
        if ph in fns:
            fns[ph](k)
        if ph == upto:
            break
    k.S.emit()
    return k


_CACHE = {}


def kernel(**inputs):
    inp = {kk: np.asarray(v) for kk, v in inputs.items()}
    sh = prep_shared(inp)
    in_maps = [prep_core(inp, sh, b) for b in range(8)]
    k = build()
    res = run_bass_kernel_spmd(k.nc, in_maps, core_ids=list(range(8)))
    out = np.stack([np.asarray(r["out"]) for r in res.results], axis=0).astype(np.float32)
    return out
```

```python
import numpy as np
import concourse.bass as bass
import concourse.mybir as mybir
from concourse.bass_utils import run_bass_kernel_spmd

F32 = mybir.dt.float32
BF16 = mybir.dt.bfloat16
F32R = mybir.dt.float32r
I32 = mybir.dt.int32
AF = mybir.ActivationFunctionType
ALU = mybir.AluOpType
AX = mybir.AxisListType

ENGS = ("pe", "act", "dve", "pool", "sp")
EPOCH = 12000


class Res:
    __slots__ = ("name", "w", "rs", "multi", "ws", "excl")

    def __init__(self, name="", multi=False, excl=False):
        self.excl = excl
        self.name = name
        self.w = None
        self.rs = []
        self.multi = multi
        self.ws = []


class Tok:
    __slots__ = ("key", "val", "eng")

    def __init__(self, key, val, eng):
        self.key = key
        self.val = val
        self.eng = eng


class DmaPool:
    def __init__(self, sched, name, n):
        self.s = sched
        self.name = name
        self.n = n
        self.i = 0
        self.count = [0] * n
        self.last = [None] * n

    def keys(self):
        return [("dma", self.name, j) for j in range(self.n)]


class Sched:
    def __init__(self, nc):
        self.nc = nc
        self.ops = {e: [] for e in ENGS}
        self.cnt = {e: 0 for e in ENGS}
        self.pools = []
        self.last_tok = {e: None for e in ENGS}
        self.n_instr = 0

    def pool(self, name, n):
        p = DmaPool(self, name, n)
        self.pools.append(p)
        return p

    def _deps(self, eng, reads, writes):
        deps = []
        for r in reads:
            if r.multi:
                deps.extend(r.ws)
            elif r.w is not None:
                deps.append(r.w)
            if r.excl:
                deps.extend(t for t in r.rs if t.eng != eng)
        for w in writes:
            if w.multi:
                pass
            elif w.w is not None and w.w.eng != eng:
                deps.append(w.w)
            for t in w.rs:
                if t.eng != eng:
                    deps.append(t)
        return deps

    def _mark_w(self, writes, tok):
        for w in writes:
            if w.multi:
                w.ws.append(tok)
            else:
                w.w = tok
                w.rs = []

    def op(self, eng, fn, reads=(), writes=(), extra=()):
        deps = self._deps(eng, reads, writes) + list(extra)
        c = self.cnt[eng]
        tok = Tok(("eng", eng, c // EPOCH), c % EPOCH + 1, eng)
        self.cnt[eng] = c + 1
        for r in reads:
            r.rs.append(tok)
        self._mark_w(writes, tok)
        self.ops[eng].append((deps, fn, tok, 1))
        self.last_tok[eng] = tok
        return tok

    def dma(self, eng, pool, fn, reads=(), writes=(), extra=()):
        deps = self._deps('__dma__', reads, writes) + list(extra)
        j = pool.i
        pool.i = (pool.i + 1) % pool.n
        if pool.last[j] is not None:
            deps.append(pool.last[j])
        pool.count[j] += 16
        tok = Tok(("dma", pool.name, j), pool.count[j], None)
        pool.last[j] = tok
        for r in reads:
            r.rs.append(tok)
        self._mark_w(writes, tok)
        self.ops[eng].append((deps, fn, tok, 16))
        return tok

    def barrier(self):
        toks = [t for t in self.last_tok.values() if t is not None]
        for p in self.pools:
            toks += [t for t in p.last if t is not None]
        for e in ENGS:
            self.ops[e].append((list(toks), None, None, 0))

    def emit(self, final_waits_eng="sp"):
        nc = self.nc
        sems = {}

        def sem_of(key):
            if key not in sems:
                sems[key] = nc.alloc_semaphore("s_" + "_".join(str(k) for k in key))
            return sems[key]

        for e in ENGS:
            for ep in range((self.cnt[e] + EPOCH - 1) // EPOCH):
                sem_of(("eng", e, ep))
        for p in self.pools:
            for k in p.keys():
                sem_of(k)

        toks = [t for t in self.last_tok.values() if t is not None]
        for p in self.pools:
            toks += [t for t in p.last if t is not None]
        self.ops[final_waits_eng].append((list(toks), None, None, 0))

        engobj = {"pe": "tensor", "act": "scalar", "dve": "vector", "pool": "gpsimd", "sp": "sync"}
        sched = self

        def run(ename):
            def body(eng):
                seen = {}
                for deps, fn, tok, inc in sched.ops[ename]:
                    need = {}
                    for t in deps:
                        if t.val > need.get(t.key, 0):
                            need[t.key] = t.val
                    for k, v in need.items():
                        if seen.get(k, 0) >= v:
                            continue
                        seen[k] = v
                        eng.wait_ge(sem_of(k), v)
                        sched.n_instr += 1
                    if fn is not None:
                        ins = fn(eng)
                        ins.then_inc(sem_of(tok.key), inc)
                        sched.n_instr += 1
            return body

        with nc.Block() as block:
            for ename in ENGS:
                getattr(block, engobj[ename])(run(ename))


class Arena:
    def __init__(self, nc, kbytes=198):
        self.nc = nc
        self.words = kbytes * 256
        self.t = nc.alloc_sbuf_tensor("arena", [128, self.words], F32)
        self.ap = self.t.ap()
        self.off = 0
        self.marks = []
        self.peak = 0

    def tile(self, shape, dtype, name=None):
        esz = {F32: 4, BF16: 2, I32: 4}[dtype]
        n = int(np.prod(shape[1:]))
        nw = (n * esz + 3) // 4
        off = (self.off + 15) // 16 * 16
        assert off + nw <= self.words, f"SBUF overflow {off}+{nw} > {self.words}"
        self.off = off + nw
        self.peak = max(self.peak, self.off)
        a = self.ap[0:shape[0], off:off + nw]
        if dtype != F32:
            a = a.bitcast(dtype)
        a = a[:, 0:n]
        if len(shape) == 3:
            a = a.rearrange("p (a b) -> p a b", a=shape[1])
        elif len(shape) == 4:
            a = a.rearrange("p (a b c) -> p a b c", a=shape[1], b=shape[2])
        return a

    def mark(self):
        self.marks.append(self.off)

    def release(self):
        self.off = self.marks.pop()

D = 1024
T = 4352
NT = 34
TX = 4096
NCTX = 256
H = 8
OFF_Z = 3072
OFF_GATE = 4832
D_IN = 6880
NMID = 1760
EPS = 1e-6
NEG = -30000.0


class K:
    def __init__(self, dbg=()):
        self.nc = bass.Bass("TRN2", target_bir_lowering=False)
        self.S = Sched(self.nc)
        self.A = Arena(self.nc)
        self.dbg = set(dbg)
        self.ins = {}
        self.scr = {}
        nc = self.nc
        self.ps = nc.alloc_psum_tensor("ps", [128, 8, 512], F32).ap()
        self.psr = [Res(f"ps{b}", excl=True) for b in range(8)]
        self.ld = self.S.pool("ld", 8)
        self.st = self.S.pool("st", 8)
        self.wl = self.S.pool("wl", 6)

    def inp(self, name, shape, dtype=F32):
        t = self.nc.dram_tensor(name, list(shape), dtype, kind="ExternalInput").ap()
        self.ins[name] = t
        return t

    def scratch(self, name, shape, dtype):
        kind = "ExternalOutput" if name in self.dbg else "Internal"
        t = self.nc.dram_tensor(name, list(shape), dtype, kind=kind).ap()
        self.scr[name] = (t, Res(name, multi=True))
        return t, self.scr[name][1]

    def bank(self, b, dtype=F32):
        a = self.ps[:, b, :]
        if dtype == BF16:
            a = a.bitcast(BF16)
        return a, self.psr[b]

    def tile(self, shape, dtype, name=None):
        return self.A.tile(shape, dtype, name), Res(name or "t")

    def load(self, dst, dres, src, sres=None, q="sp", pool=None):
        return self.S.dma(q, pool or self.ld, lambda e: e.dma_start(out=dst, in_=src),
                          reads=[sres] if sres is not None else [], writes=[dres])

    def store(self, dst, dres, src, sres, q="sp", pool=None):
        return self.S.dma(q, pool or self.st, lambda e: e.dma_start(out=dst, in_=src),
                          reads=[sres], writes=[dres] if dres is not None else [])

    def wload(self, dst, dres, src):
        return self.S.dma("pool", self.wl, lambda e: e.dma_start(out=dst, in_=src), writes=[dres])


def declare_inputs(k):
    i = k.inp
    i("x", [TX, D]); i("ctx", [NCTX, D]); i("c2", [128, 8, 2])
    i("ada_w", [D, 6 * D]); i("ada_b_row", [1, 6 * D]); i("ada_bT", [128, 48])
    i("g1T", [128, 8]); i("g2T", [128, 8]); i("g2_rep", [128, D])
    i("w_in", [D, D_IN]); i("convT", [128, 24, 5])
    i("alog_rep", [128, 16]); i("dtb_rep", [128, 16]); i("dng_rep", [128, 128])
    i("gqaT", [128, 3]); i("w_uq", [384, 1536]); i("gkvaT", [128, 2]); i("w_ukv", [256, 2048])
    i("gq_rep", [128, 192]); i("gk_rep", [128, 192])
    i("w_out_a", [D, D]); i("w_out_b", [D, D]); i("w_o", [D, D])
    i("router_w", [D, 16]); i("w_gate", [16, D, 1408]); i("w_up", [16, D, 1408]); i("w_down", [16, 1408, D])
    i("rope_cs", [TX, 128])
    i("ident", [128, 128]); i("consts", [128, 1024])
    i("moe_tok", [128, 32, 2]); i("moe_blk", [128, 128]); i("moe_sel8", [128, 16]); i("moe_tris", [128, 128])
    i("dn_masks", [128, 9, 128]); i("dn_esel", [64, 2, 8, 128]); i("dn_linit", [64, 128])
    k.out = k.nc.dram_tensor("out", [TX, D], F32, kind="ExternalOutput").ap()
    k.out_res = Res("out", multi=True)


def setup_consts(k):
    S = k.S
    k.ident_f, k.r_ident_f = k.tile([128, 128], F32, "identf")
    k.ident_b, k.r_ident_b = k.tile([128, 128], BF16, "identb")
    k.load(k.ident_f, k.r_ident_f, k.ins["ident"])
    S.op("dve", lambda e: e.tensor_copy(out=k.ident_b, in_=k.ident_f), reads=[k.r_ident_f], writes=[k.r_ident_b])
    k.ones_f, k.r_ones_f = k.tile([128, 128], F32, "onesf")
    k.ones_b, k.r_ones_b = k.tile([128, 128], BF16, "onesb")
    S.op("pool", lambda e: e.memset(k.ones_f, 1.0), writes=[k.r_ones_f])
    S.op("pool", lambda e: e.memset(k.ones_b, 1.0), writes=[k.r_ones_b])


def phase_a0(k):
    S, A, nc = k.S, k.A, k.nc
    ins = k.ins
    k.modT, k.r_modT = k.tile([128, 48, 2], F32, "modT")
    k.s1, k.r_s1 = k.tile([128, 8, 2], F32, "s1")
    k.s2, k.r_s2 = k.tile([128, 8], F32, "s2")
    k.gate1_row, k.r_gate1 = k.tile([128, D], F32, "gate1row")
    k.gate2_row, k.r_gate2 = k.tile([128, D], F32, "gate2row")
    k.off_after_gate2 = A.off
    k.shift2_row, k.r_shift2 = k.tile([128, D], F32, "shift2row")
    k.s2_row, k.r_s2row = k.tile([128, D], F32, "s2row")
    A.mark()
    c2, r_c2 = k.tile([128, 8, 2], F32, "c2")
    sc, r_sc = k.tile([128, 8, 2], F32, "sc")
    screp, r_screp = k.tile([128, 8, 128], F32, "screp")
    abT, r_abT = k.tile([128, 48], F32, "abT")
    abrow, r_abrow = k.tile([1, 6 * D], F32, "abrow")
    g1T, r_g1T = k.tile([128, 8], F32, "g1T")
    g2T, r_g2T = k.tile([128, 8], F32, "g2T")
    wbuf = [k.tile([128, 8, D], F32, f"adaw{j}") for j in range(2)]
    k.load(c2, r_c2, ins["c2"])
    k.load(abT, r_abT, ins["ada_bT"])
    k.load(abrow, r_abrow, ins["ada_b_row"])
    k.load(g1T, r_g1T, ins["g1T"])
    k.load(g2T, r_g2T, ins["g2T"])
    S.op("act", lambda e: e.activation(out=sc, in_=c2, func=AF.Silu), reads=[r_c2], writes=[r_sc])
    S.op("dve", lambda e: e.tensor_copy(out=screp, in_=sc[:, :, 0:1].to_broadcast([128, 8, 128])),
         reads=[r_sc], writes=[r_screp])
    pm, r_pm = k.bank(0)
    aw = ins["ada_w"].rearrange("(kc p) n -> p kc n", p=128)
    for sec in range(6):
        wt, r_wt = wbuf[sec % 2]
        q = "sp" if sec % 2 == 0 else "act"
        S.dma(q, k.ld, lambda e, wt=wt, sec=sec: e.dma_start(out=wt, in_=aw[:, :, sec * D:(sec + 1) * D]), writes=[r_wt])

        def mm(e, wt=wt, sec=sec):
            for fc in range(8):
                for kc in range(8):
                    ins_ = e.matmul(pm[:, (sec * 8 + fc) * 2:(sec * 8 + fc) * 2 + 2], lhsT=wt[:, kc, fc * 128:(fc + 1) * 128],
                                    rhs=sc[:, kc, :], start=(kc == 0), stop=(kc == 7))
            return ins_
        S.op("pe", mm, reads=[r_wt, r_sc], writes=[r_pm])
        if sec in (2, 3, 4, 5):
            dst, r_dst = {2: (k.gate1_row, k.r_gate1), 5: (k.gate2_row, k.r_gate2), 3: (k.shift2_row, k.r_shift2), 4: (k.s2_row, k.r_s2row)}[sec]
            for hf in range(2):
                pb, r_pb = k.bank(1 + hf)

                def mmr(e, wt=wt, sec=sec, hf=hf, pb=pb):
                    for kc in range(8):
                        e.matmul(pb, lhsT=screp[:, kc, :], rhs=wt[:, kc, hf * 512:(hf + 1) * 512], start=(kc == 0), stop=False)
                    return e.matmul(pb, lhsT=k.ones_f[0:1, :], rhs=abrow[0:1, sec * D + hf * 512: sec * D + (hf + 1) * 512],
                                    start=False, stop=True)
                S.op("pe", mmr, reads=[r_wt, r_screp, k.r_ones_f, r_abrow], writes=[r_pb])
                S.op("act", lambda e, dst=dst, hf=hf, pb=pb: e.activation(out=dst[:, hf * 512:(hf + 1) * 512], in_=pb, func=AF.Copy),
                     reads=[r_pb], writes=[r_dst])
    S.op("dve", lambda e: e.tensor_tensor(out=k.modT, in0=pm[:, 0:96].rearrange("p (a b) -> p a b", b=2),
                                          in1=abT.unsqueeze(2).to_broadcast([128, 48, 2]), op=ALU.add),
         reads=[r_pm, r_abT], writes=[k.r_modT])
    S.op("dve", lambda e: e.scalar_tensor_tensor(out=k.s1, in0=k.modT[:, 8:16, :], scalar=1.0,
                                                 in1=g1T.unsqueeze(2).to_broadcast([128, 8, 2]), op0=ALU.add, op1=ALU.mult),
         reads=[k.r_modT, r_g1T], writes=[k.r_s1])
    S.op("dve", lambda e: e.scalar_tensor_tensor(out=k.s2, in0=k.modT[:, 32:40, 0], scalar=1.0,
                                                 in1=g2T, op0=ALU.add, op1=ALU.mult),
         reads=[k.r_modT, r_g2T], writes=[k.r_s2])
    g2rep, r_g2rep = k.tile([128, D], F32, "g2rep")
    k.load(g2rep, r_g2rep, ins["g2_rep"])
    S.op("dve", lambda e: e.scalar_tensor_tensor(out=k.s2_row, in0=k.s2_row, scalar=1.0, in1=g2rep, op0=ALU.add, op1=ALU.mult),
         reads=[k.r_s2row, r_g2rep], writes=[k.r_s2row])
    S.barrier()
    A.release()

def _nb(k, dtype=F32):
    b = getattr(k, "_bank_i", 0)
    k._bank_i = (b + 1) % 8
    return k.bank(b, dtype)


def _rstd(k, ss, r_ss, n, out, r_out):
    S = k.S
    S.op("act", lambda e: e.activation(out=out, in_=ss, func=AF.Sqrt, scale=1.0 / n, bias=EPS), reads=[r_ss], writes=[r_out])
    S.op("dve", lambda e: e.reciprocal(out=out, in_=out), reads=[r_out], writes=[r_out])


def _rope(k, pe, r_pe, cos_t, sin_t, r_tab, t1, t2, r_t1, r_t2):
    S = k.S
    cb = cos_t.unsqueeze(1).to_broadcast([128, 8, 64])
    S.op("pool", lambda e: e.tensor_tensor(out=t1, in0=pe, in1=cb, op=ALU.mult), reads=[r_pe, r_tab], writes=[r_t1])
    pe5 = pe.rearrange("p h (a s c) -> p h a s c", a=2, s=2)
    t25 = t2.rearrange("p h (a s c) -> p h a s c", a=2, s=2)
    sn5 = sin_t.rearrange("p (a s c) -> p a s c", a=2, s=2)

    def f(e):
        for s in range(2):
            ins_ = e.tensor_tensor(out=t25[:, :, :, s, :], in0=pe5[:, :, :, 1 - s, :],
                                   in1=sn5[:, :, s, :].unsqueeze(1).to_broadcast([128, 8, 2, 16]), op=ALU.mult)
        return ins_
    S.op("dve", f, reads=[r_pe, r_tab], writes=[r_t2])
    S.op("pool", lambda e: e.tensor_tensor(out=pe, in0=t1, in1=t2, op=ALU.add), reads=[r_t1, r_t2], writes=[r_pe])


def phase_a1(k):
    S, A, nc, ins = k.S, k.A, k.nc, k.ins
    zs_s, r_zs_s = k.scratch("zs_s", [TX, D], BF16)
    gb_s, r_gb_s = k.scratch("gb_s", [T, 48], F32)
    qmT_s, r_qmT_s = k.scratch("qmT_s", [H, 192, TX], BF16)
    kmT_s, r_kmT_s = k.scratch("kmT_s", [H, 192, T], BF16)
    vm_s, r_vm_s = k.scratch("vm_s", [T, D], BF16)
    A.mark()
    k.hT, _ = k.tile([128, 8, T], BF16, "hT")
    k.r_hT = [Res(f"hT{i}") for i in range(NT)]
    A.mark()
    wmid, r_wmid = k.tile([128, 8, NMID], BF16, "wmid")
    wuq, r_wuq = k.tile([128, 3, 1536], BF16, "wuq")
    wukv, r_wukv = k.tile([128, 2, 2048], BF16, "wukv")
    dtb, r_dtb = k.tile([128, 16], F32, "dtb")
    negA, r_negA = k.tile([128, 16], F32, "negA")
    gq, r_gq = k.tile([128, 192], F32, "gq")
    gk, r_gk = k.tile([128, 192], F32, "gk")
    gqaT, r_gqaT = k.tile([128, 3], F32, "gqaT")
    gkvaT, r_gkvaT = k.tile([128, 2], F32, "gkvaT")
    win = ins["w_in"].rearrange("(kc p) n -> p kc n", p=128)
    for j in range(4):
        k.wload(wmid[:, 2 * j:2 * j + 2, :], r_wmid, win[:, 2 * j:2 * j + 2, OFF_Z:OFF_GATE])
    k.wload(wuq, r_wuq, ins["w_uq"].rearrange("(kc p) n -> p kc n", p=128))
    k.wload(wukv, r_wukv, ins["w_ukv"].rearrange("(kc p) n -> p kc n", p=128))
    k.load(dtb, r_dtb, ins["dtb_rep"])
    k.load(negA, r_negA, ins["alog_rep"])
    k.load(gq, r_gq, ins["gq_rep"])
    k.load(gk, r_gk, ins["gk_rep"])
    k.load(gqaT, r_gqaT, ins["gqaT"])
    k.load(gkvaT, r_gkvaT, ins["gkvaT"])
    S.op("act", lambda e: e.activation(out=negA, in_=negA, func=AF.Exp), reads=[r_negA], writes=[r_negA])
    S.op("dve", lambda e: e.tensor_scalar(out=negA, in0=negA, scalar1=-1.0, scalar2=None, op0=ALU.mult), reads=[r_negA], writes=[r_negA])
    S.op("dve", lambda e: e.tensor_scalar(out=gq, in0=gq, scalar1=192.0 ** -0.5, scalar2=None, op0=ALU.mult), reads=[r_gq], writes=[r_gq])

    NB = 2
    xt = [k.tile([128, D], F32, "xt") for _ in range(NB)]
    junk, r_junk = k.tile([128, D], BF16, "junk")
    ss = [k.tile([128, 8], F32, "ss") for _ in range(NB)]
    xn = [k.tile([128, D], BF16, "xn") for _ in range(NB)]
    zs = [k.tile([128, D], BF16, "zs") for _ in range(NB)]
    gb = [k.tile([128, 48], F32, "gb") for _ in range(NB)]
    t16, r_t16 = k.tile([128, 16], F32, "t16")
    cqn, r_cqn = k.tile([128, 384], BF16, "cqn")
    ckvn, r_ckvn = k.tile([128, 256], BF16, "ckvn")
    cqnT, r_cqnT = k.tile([128, 3, 128], BF16, "cqnT")
    ckvnT, r_ckvnT = k.tile([128, 2, 128], BF16, "ckvnT")
    kr, r_kr = k.tile([128, 64], F32, "kr")
    qsb, r_qsb = k.tile([128, 8, 192], F32, "qsb")
    sq, r_sq = k.tile([128, 8, 192], F32, "sq")
    r8, r_r8 = k.tile([128, 8], F32, "r8")
    kvsb, r_kvsb = k.tile([128, 8, 2, 128], F32, "kvsb")
    tmpf, r_tmpf = kvsb.rearrange("p a b c -> p (a b c)")[:, 0:1024].rearrange("p (a b) -> p a b", a=8), r_kvsb
    kf, r_kf = sq, r_sq
    rk8, r_rk8 = k.tile([128, 8], F32, "rk8")
    sskr, r_sskr = k.tile([128, 1], F32, "sskr")
    rt1, r_rt1 = k.tile([128, 8, 64], F32, "rt1")
    rt2, r_rt2 = k.tile([128, 8, 64], F32, "rt2")
    cs_t = [k.tile([128, 128], F32, "cs") for _ in range(NB)]
    qf, r_qf = k.tile([128, 8, 192], BF16, "qf")
    kfb, r_kfb = k.tile([128, 8, 192], BF16, "kfb")
    vb = [k.tile([128, 8, 128], BF16, "vb") for _ in range(1)]
    qTn = [k.tile([128, 8, 128], BF16, "qTn") for _ in range(1)]
    qTr = [k.tile([64, 8, 128], BF16, "qTr") for _ in range(1)]
    kTn = [k.tile([128, 8, 128], BF16, "kTn") for _ in range(1)]
    kTr = [k.tile([64, 8, 128], BF16, "kTr") for _ in range(1)]

    def src_rows(i):
        return ins["ctx"][i * 128:(i + 1) * 128, :] if i < 2 else ins["x"][(i - 2) * 128:(i - 1) * 128, :]

    def prefetch(i):
        b = i % NB
        k.load(xt[b][0], xt[b][1], src_rows(i))
        if i >= 2:
            xi = i - 2
            S.dma("act", k.ld, lambda e: e.dma_start(out=cs_t[b][0], in_=ins["rope_cs"][xi * 128:(xi + 1) * 128, :]), writes=[cs_t[b][1]])

    def transposes(src, r_src, n, width=128, rows=128):
        pb, r_pb = _nb(k, BF16)
        pv = pb.rearrange("p (a b) -> p a b", b=128)[0:width, 0:n, :]

        def f(e):
            for j in range(n):
                ins_ = e.transpose(out=pv[:, j, :], in_=src(j), identity=k.ident_b)
            return ins_
        S.op("pe", f, reads=[r_src, k.r_ident_b], writes=[r_pb])
        return pv, r_pb

    prefetch(0)
    for i in range(NT):
        b = i % NB
        is_x = i >= 2
        xi = i - 2
        col = 0 if is_x else 1
        tok = slice(i * 128, (i + 1) * 128)
        if i + 1 < NT:
            prefetch(i + 1)
        x_t, r_x = xt[b]
        ss_t, r_ss = ss[b]
        xn_t, r_xn = xn[b]
        S.op("act", lambda e, x_t=x_t, ss_t=ss_t: e.activation(out=junk, in_=x_t, func=AF.Square, accum_out=ss_t[:, 0:1]),
             reads=[r_x], writes=[r_junk, r_ss])
        _rstd(k, ss_t[:, 0:1], r_ss, D, ss_t[:, 1:2], r_ss)
        S.op("act", lambda e, x_t=x_t, ss_t=ss_t, xn_t=xn_t: e.activation(out=xn_t, in_=x_t, func=AF.Copy, scale=ss_t[:, 1:2]),
             reads=[r_x, r_ss], writes=[r_xn])
        pv, r_pv = transposes(lambda j, xn_t=xn_t: xn_t[:, j * 128:(j + 1) * 128], r_xn, 8)
        S.op("dve", lambda e, pv=pv, col=col: e.tensor_tensor(out=tmpf, in0=pv, in1=k.s1[:, :, col:col + 1].to_broadcast([128, 8, 128]), op=ALU.mult),
             reads=[r_pv, k.r_s1], writes=[r_tmpf])
        S.op("pool", lambda e, col=col, tok=tok: e.tensor_tensor(out=k.hT[:, :, tok], in0=tmpf,
                                                                in1=k.modT[:, 0:8, col:col + 1].to_broadcast([128, 8, 128]), op=ALU.add),
             reads=[r_tmpf, k.r_modT], writes=[k.r_hT[i]])
        groups = [(0, 512), (512, 1024), (1024, 1440), (1440, 1760)]
        banks = []
        for g, (c0, c1) in enumerate(groups):
            if g < 2 and not is_x:
                banks.append(None)
                continue
            pb, r_pb = _nb(k)

            def mm(e, pb=pb, c0=c0, c1=c1, tok=tok):
                for kc in range(8):
                    ins_ = e.matmul(pb[:, 0:c1 - c0], lhsT=k.hT[:, kc, tok], rhs=wmid[:, kc, c0:c1], start=(kc == 0), stop=(kc == 7))
                return ins_
            S.op("pe", mm, reads=[k.r_hT[i], r_wmid], writes=[r_pb])
            banks.append((pb, r_pb))
        if is_x:
            z_t, r_z = zs[b]
            for g in range(2):
                pb, r_pb = banks[g]
                S.op("act", lambda e, pb=pb, g=g, z_t=z_t: e.activation(out=z_t[:, g * 512:(g + 1) * 512], in_=pb, func=AF.Silu),
                     reads=[r_pb], writes=[r_z])
            k.store(zs_s[xi * 128:(xi + 1) * 128, :], r_zs_s, z_t, r_z)
        p2, r_p2 = banks[2]
        p3, r_p3 = banks[3]
        gb_t, r_gb = gb[b]
        S.op("dve", lambda e, p2=p2: e.tensor_tensor(out=t16, in0=p2[:, 0:16], in1=dtb, op=ALU.add), reads=[r_p2, r_dtb], writes=[r_t16])
        S.op("act", lambda e: e.activation(out=t16, in_=t16, func=AF.Exp), reads=[r_t16], writes=[r_t16])
        S.op("act", lambda e: e.activation(out=t16, in_=t16, func=AF.Ln, bias=1.0), reads=[r_t16], writes=[r_t16])
        S.op("dve", lambda e, gb_t=gb_t: e.tensor_tensor(out=gb_t[:, 0:16], in0=t16, in1=negA, op=ALU.mult), reads=[r_t16, r_negA], writes=[r_gb])
        S.op("act", lambda e, gb_t=gb_t, p2=p2: e.activation(out=gb_t[:, 16:32], in_=p2[:, 16:32], func=AF.Sigmoid), reads=[r_p2], writes=[r_gb])
        S.op("act", lambda e, gb_t=gb_t: e.activation(out=gb_t[:, 32:48], in_=gb_t[:, 16:32], func=AF.Ln), reads=[r_gb], writes=[r_gb])
        k.store(gb_s[tok, :], r_gb_s, gb_t, r_gb)
        if is_x:
            S.op("act", lambda e, p2=p2, ss_t=ss_t: e.activation(out=junk[:, 0:384], in_=p2[:, 32:416], func=AF.Square, accum_out=ss_t[:, 4:5]),
                 reads=[r_p2], writes=[r_junk, r_ss])
            _rstd(k, ss_t[:, 4:5], r_ss, 384, ss_t[:, 5:6], r_ss)
            S.op("act", lambda e, p2=p2, ss_t=ss_t: e.activation(out=cqn, in_=p2[:, 32:416], func=AF.Copy, scale=ss_t[:, 5:6]),
                 reads=[r_p2, r_ss], writes=[r_cqn])

        S.op("act", lambda e, p3=p3, ss_t=ss_t: e.activation(out=junk[:, 0:256], in_=p3[:, 0:256], func=AF.Square, accum_out=ss_t[:, 2:3]),
             reads=[r_p3], writes=[r_junk, r_ss])
        _rstd(k, ss_t[:, 2:3], r_ss, 256, ss_t[:, 3:4], r_ss)
        S.op("act", lambda e, p3=p3, ss_t=ss_t: e.activation(out=ckvn, in_=p3[:, 0:256], func=AF.Copy, scale=ss_t[:, 3:4]),
             reads=[r_p3, r_ss], writes=[r_ckvn])
        S.op("dve", lambda e, p3=p3: e.tensor_copy(out=kr, in_=p3[:, 256:320]), reads=[r_p3], writes=[r_kr])
        pv, r_pv = transposes(lambda j: ckvn[:, j * 128:(j + 1) * 128], r_ckvn, 2)
        S.op("dve", lambda e, pv=pv: e.tensor_tensor(out=ckvnT, in0=pv, in1=gkvaT.unsqueeze(2).to_broadcast([128, 2, 128]), op=ALU.mult),
             reads=[r_pv, r_gkvaT], writes=[r_ckvnT])
        for b4 in range(4):
            pb, r_pb = _nb(k)

            def mmkv(e, pb=pb, b4=b4):
                for kc in range(2):
                    ins_ = e.matmul(pb, lhsT=ckvnT[:, kc, :], rhs=wukv[:, kc, b4 * 512:(b4 + 1) * 512], start=(kc == 0), stop=(kc == 1))
                return ins_
            S.op("pe", mmkv, reads=[r_ckvnT, r_wukv], writes=[r_pb])
            eng = "act" if b4 % 2 == 0 else "dve"
            dst = kvsb[:, 2 * b4:2 * b4 + 2, :, :].rearrange("p a b c -> p (a b c)")
            if eng == "act":
                S.op("act", lambda e, dst=dst, pb=pb: e.activation(out=dst, in_=pb, func=AF.Copy), reads=[r_pb], writes=[r_kvsb])
            else:
                S.op("dve", lambda e, dst=dst, pb=pb: e.tensor_copy(out=dst, in_=pb), reads=[r_pb], writes=[r_kvsb])
        vb_t, r_vb = vb[0]
        S.op("pool", lambda e, vb_t=vb_t: e.tensor_copy(out=vb_t, in_=kvsb[:, :, 1, :]), reads=[r_kvsb], writes=[r_vb])
        k.store(vm_s[tok, :], r_vm_s, vb_t.rearrange("p h d -> p (h d)"), r_vb)
        S.op("pool", lambda e: e.tensor_tensor(out=sq[:, :, 0:128], in0=kvsb[:, :, 0, :], in1=kvsb[:, :, 0, :], op=ALU.mult),
             reads=[r_kvsb], writes=[r_sq])
        S.op("dve", lambda e: e.tensor_reduce(out=rk8, in_=sq[:, :, 0:128], axis=AX.X, op=ALU.add), reads=[r_sq], writes=[r_rk8])
        S.op("act", lambda e: e.activation(out=junk[:, 0:64], in_=kr, func=AF.Square, accum_out=sskr), reads=[r_kr], writes=[r_junk, r_sskr])
        S.op("dve", lambda e: e.tensor_scalar(out=rk8, in0=rk8, scalar1=sskr, scalar2=None, op0=ALU.add), reads=[r_rk8, r_sskr], writes=[r_rk8])
        _rstd(k, rk8, r_rk8, 192, rk8, r_rk8)
        S.op("dve", lambda e: e.tensor_tensor(out=kf[:, :, 0:128], in0=kvsb[:, :, 0, :], in1=rk8.unsqueeze(2).to_broadcast([128, 8, 128]), op=ALU.mult),
             reads=[r_kvsb, r_rk8], writes=[r_kf])
        S.op("dve", lambda e: e.tensor_tensor(out=kf[:, :, 128:192], in0=kr.unsqueeze(1).to_broadcast([128, 8, 64]),
                                              in1=rk8.unsqueeze(2).to_broadcast([128, 8, 64]), op=ALU.mult),
             reads=[r_kr, r_rk8, r_kf], writes=[r_kf])
        S.op("pool", lambda e: e.tensor_tensor(out=kf, in0=kf, in1=gk.unsqueeze(1).to_broadcast([128, 8, 192]), op=ALU.mult),
             reads=[r_kf, r_gk], writes=[r_kf])
        if is_x:
            _rope(k, kf[:, :, 128:192], r_kf, cs_t[b][0][:, 0:64], cs_t[b][0][:, 64:128], cs_t[b][1], rt1, rt2, r_rt1, r_rt2)
        S.op("act", lambda e: e.activation(out=kfb, in_=kf, func=AF.Copy), reads=[r_kf], writes=[r_kfb])
        kTn_t, r_kTn = kTn[0]
        kTr_t, r_kTr = kTr[0]
        pv, r_pv = transposes(lambda j: kfb[:, j, 0:128], r_kfb, 8)
        S.op("dve", lambda e, pv=pv, kTn_t=kTn_t: e.tensor_copy(out=kTn_t, in_=pv), reads=[r_pv], writes=[r_kTn])
        pv, r_pv = transposes(lambda j: kfb[:, j, 128:192], r_kfb, 8, width=64)
        S.op("act", lambda e, pv=pv, kTr_t=kTr_t: e.activation(out=kTr_t, in_=pv, func=AF.Copy), reads=[r_pv], writes=[r_kTr])
        k.store(kmT_s[:, 0:128, tok].rearrange("h d t -> d h t"), r_kmT_s, kTn_t, r_kTn)
        k.store(kmT_s[:, 128:192, tok].rearrange("h d t -> d h t"), r_kmT_s, kTr_t, r_kTr)
        if not is_x:
            continue
        xtok = slice(xi * 128, (xi + 1) * 128)
        pv, r_pv = transposes(lambda j: cqn[:, j * 128:(j + 1) * 128], r_cqn, 3)
        S.op("dve", lambda e, pv=pv: e.tensor_tensor(out=cqnT, in0=pv, in1=gqaT.unsqueeze(2).to_broadcast([128, 3, 128]), op=ALU.mult),
             reads=[r_pv, r_gqaT], writes=[r_cqnT])
        qflat = qsb.rearrange("p h d -> p (h d)")
        for b3 in range(3):
            pb, r_pb = _nb(k)

            def mmq(e, pb=pb, b3=b3):
                for kc in range(3):
                    ins_ = e.matmul(pb, lhsT=cqnT[:, kc, :], rhs=wuq[:, kc, b3 * 512:(b3 + 1) * 512], start=(kc == 0), stop=(kc == 2))
                return ins_
            S.op("pe", mmq, reads=[r_cqnT, r_wuq], writes=[r_pb])
            if b3 % 2 == 0:
                S.op("act", lambda e, pb=pb, b3=b3: e.activation(out=qflat[:, b3 * 512:(b3 + 1) * 512], in_=pb, func=AF.Copy), reads=[r_pb], writes=[r_qsb])
            else:
                S.op("dve", lambda e, pb=pb, b3=b3: e.tensor_copy(out=qflat[:, b3 * 512:(b3 + 1) * 512], in_=pb), reads=[r_pb], writes=[r_qsb])
        S.op("pool", lambda e: e.tensor_tensor(out=sq, in0=qsb, in1=qsb, op=ALU.mult), reads=[r_qsb], writes=[r_sq])
        S.op("dve", lambda e: e.tensor_reduce(out=r8, in_=sq, axis=AX.X, op=ALU.add), reads=[r_sq], writes=[r_r8])
        _rstd(k, r8, r_r8, 192, r8, r_r8)
        S.op("dve", lambda e: e.tensor_tensor(out=qsb, in0=qsb, in1=r8.unsqueeze(2).to_broadcast([128, 8, 192]), op=ALU.mult),
             reads=[r_qsb, r_r8], writes=[r_qsb])
        S.op("pool", lambda e: e.tensor_tensor(out=qsb, in0=qsb, in1=gq.unsqueeze(1).to_broadcast([128, 8, 192]), op=ALU.mult),
             reads=[r_qsb, r_gq], writes=[r_qsb])
        _rope(k, qsb[:, :, 128:192], r_qsb, cs_t[b][0][:, 0:64], cs_t[b][0][:, 64:128], cs_t[b][1], rt1, rt2, r_rt1, r_rt2)
        S.op("act", lambda e: e.activation(out=qf, in_=qsb, func=AF.Copy), reads=[r_qsb], writes=[r_qf])
        qTn_t, r_qTn = qTn[0]
        qTr_t, r_qTr = qTr[0]
        pv, r_pv = transposes(lambda j: qf[:, j, 0:128], r_qf, 8)
        S.op("dve", lambda e, pv=pv, qTn_t=qTn_t: e.tensor_copy(out=qTn_t, in_=pv), reads=[r_pv], writes=[r_qTn])
        pv, r_pv = transposes(lambda j: qf[:, j, 128:192], r_qf, 8, width=64)
        S.op("act", lambda e, pv=pv, qTr_t=qTr_t: e.activation(out=qTr_t, in_=pv, func=AF.Copy), reads=[r_pv], writes=[r_qTr])
        k.store(qmT_s[:, 0:128, xtok].rearrange("h d t -> d h t"), r_qmT_s, qTn_t, r_qTn)
        k.store(qmT_s[:, 128:192, xtok].rearrange("h d t -> d h t"), r_qmT_s, qTr_t, r_qTr)
    S.barrier()
    A.release()

RW = 4364
NU = 4356


def phase_a2(k):
    S, A, nc, ins = k.S, k.A, k.nc, k.ins
    qdT_s, r_qdT_s = k.scratch("qdT_s", [H, 128, TX], BF16)
    kdT_s, r_kdT_s = k.scratch("kdT_s", [H, 128, T], BF16)
    kd_s, r_kd_s = k.scratch("kd_s", [T, H, 128], BF16)
    vd_s, r_vd_s = k.scratch("vd_s", [T, H, 128], BF16)
    sgT_s, r_sgT_s = k.scratch("sgT_s", [16, 128, TX], BF16)
    A.mark()
    convw, r_convw = k.tile([128, 24, 5], F32, "convw")
    k.load(convw, r_convw, ins["convT"])
    wc = [k.tile([128, 8, 512], BF16, "wc") for _ in range(2)]
    R = [k.tile([128, RW], F32, "R") for _ in range(2)]
    acc, r_acc = k.tile([128, NU], F32, "acc")
    sq, r_sq = k.tile([128, NU], BF16, "sq")
    Yb = [k.tile([128, NU], BF16, "Yb") for _ in range(2)]
    rn, r_rn = k.tile([128, 512], F32, "rn")
    tm = [k.tile([128, NT, 128], BF16, "tm") for _ in range(1)]
    sg = [k.tile([128, 512], BF16, "sg") for _ in range(2)]
    for j in range(2):
        S.op("pool", lambda e, j=j: e.memset(R[j][0], 0.0), writes=[R[j][1]])
    win = ins["w_in"].rearrange("(kc p) n -> p kc n", p=128)
    blocks = [(c * 512, "qkv", c * 4) for c in range(6)] + [(OFF_GATE + c * 512, "gate", c * 4) for c in range(4)]
    tgroups = [(0, 256, 2)] + [(256 + g * 512, 512, 262 + g * 512) for g in range(8)]
    all_hT = list(k.r_hT)

    def load_block(bi):
        c0, kind, _ = blocks[bi]
        w_t, r_w = wc[bi % 2]
        for hf in range(2):
            k.wload(w_t[:, 4 * hf:4 * hf + 4, :], r_w, win[:, 4 * hf:4 * hf + 4, c0:c0 + 512])

    load_block(0)
    ci = 0
    for bi, (c0, kind, chunk0) in enumerate(blocks):
        if bi + 1 < len(blocks):
            load_block(bi + 1)
        w_t, r_w = wc[bi % 2]
        for sub in range(4):
            cc = chunk0 + sub
            if kind == "gate":
                for g in range(8):
                    pb, r_pb = _nb(k)

                    def mm(e, pb=pb, g=g, sub=sub, w_t=w_t):
                        for kc in range(8):
                            ins_ = e.matmul(pb, lhsT=w_t[:, kc, sub * 128:(sub + 1) * 128], rhs=k.hT[:, kc, 256 + g * 512:256 + (g + 1) * 512],
                                            start=(kc == 0), stop=(kc == 7))
                        return ins_
                    S.op("pe", mm, reads=[r_w] + all_hT, writes=[r_pb])
                    s_t, r_s = sg[g % 2]
                    S.op("act", lambda e, pb=pb, s_t=s_t: e.activation(out=s_t, in_=pb, func=AF.Sigmoid), reads=[r_pb], writes=[r_s])
                    k.store(sgT_s[cc, :, g * 512:(g + 1) * 512], r_sgT_s, s_t, r_s)
                continue
            R_t, r_R = R[ci % 2]
            Y_t, r_Y = Yb[ci % 2]
            ci += 1
            for gi, (h0, n, ro) in enumerate(tgroups):
                pb, r_pb = _nb(k)

                def mm(e, pb=pb, h0=h0, n=n, sub=sub, w_t=w_t):
                    for kc in range(8):
                        ins_ = e.matmul(pb[:, 0:n], lhsT=w_t[:, kc, sub * 128:(sub + 1) * 128], rhs=k.hT[:, kc, h0:h0 + n],
                                        start=(kc == 0), stop=(kc == 7))
                    return ins_
                S.op("pe", mm, reads=[r_w] + all_hT, writes=[r_pb])
                if gi % 2 == 0:
                    S.op("act", lambda e, pb=pb, n=n, ro=ro, R_t=R_t: e.activation(out=R_t[:, ro:ro + n], in_=pb[:, 0:n], func=AF.Copy),
                         reads=[r_pb], writes=[r_R])
                else:
                    S.op("dve", lambda e, pb=pb, n=n, ro=ro, R_t=R_t: e.tensor_copy(out=R_t[:, ro:ro + n], in_=pb[:, 0:n]),
                         reads=[r_pb], writes=[r_R])
            ceng = "dve"

            def conv(e, R_t=R_t, cc=cc):
                e.tensor_scalar(out=acc, in0=R_t[:, 0:NU], scalar1=convw[:, cc, 0:1], scalar2=None, op0=ALU.mult)
                for j in range(1, 5):
                    ins_ = e.scalar_tensor_tensor(out=acc, in0=R_t[:, j:j + NU], scalar=convw[:, cc, j:j + 1], in1=acc,
                                                  op0=ALU.mult, op1=ALU.add)
                return ins_
            S.op(ceng, conv, reads=[r_R, r_convw], writes=[r_acc])
            head = cc % 8
            if cc >= 16:
                S.op("act", lambda e, Y_t=Y_t: e.activation(out=Y_t, in_=acc, func=AF.Silu), reads=[r_acc], writes=[r_Y])
            else:
                S.op("act", lambda e: e.activation(out=acc, in_=acc, func=AF.Silu), reads=[r_acc], writes=[r_acc])
                S.op("pool" if ceng == "dve" else "dve", lambda e: e.tensor_tensor(out=sq, in0=acc, in1=acc, op=ALU.mult), reads=[r_acc], writes=[r_sq])
                scale = (128.0 ** -0.5) if cc < 8 else 1.0
                for g in range(9):
                    u0 = g * 512
                    n = min(512, NU - u0)
                    pb, r_pb = _nb(k)
                    S.op("pe", lambda e, pb=pb, u0=u0, n=n: e.matmul(pb[:, 0:n], lhsT=k.ones_b, rhs=sq[:, u0:u0 + n], start=True, stop=True),
                         reads=[r_sq, k.r_ones_b], writes=[r_pb])
                    S.op("act", lambda e, pb=pb, n=n, scale=scale: e.activation(out=rn[:, 0:n], in_=pb[:, 0:n], func=AF.Sqrt,
                                                                              scale=1.0 / (scale * scale), bias=EPS / (scale * scale)),
                         reads=[r_pb], writes=[r_rn])
                    S.op("dve", lambda e, n=n: e.reciprocal(out=rn[:, 0:n], in_=rn[:, 0:n]), reads=[r_rn], writes=[r_rn])
                    S.op("dve", lambda e, u0=u0, n=n, Y_t=Y_t: e.tensor_tensor(out=Y_t[:, u0:u0 + n], in0=acc[:, u0:u0 + n], in1=rn[:, 0:n], op=ALU.mult),
                         reads=[r_acc, r_rn], writes=[r_Y])
            if cc < 8:
                k.store(qdT_s[head, :, :], r_qdT_s, Y_t[:, 260:260 + TX], r_Y)
            elif cc < 16:
                k.store(kdT_s[head, :, 0:256], r_kdT_s, Y_t[:, 0:256], r_Y)
                k.store(kdT_s[head, :, 256:T], r_kdT_s, Y_t[:, 260:260 + TX], r_Y)
            if cc >= 8:
                tm_t, r_tm = tm[0]
                for tb in range(5):
                    t0 = tb * 8
                    nt = min(8, NT - t0)
                    pb, r_pb = _nb(k, BF16)
                    pv = pb.rearrange("p (a b) -> p a b", b=128)[:, 0:nt, :]

                    def tr(e, pv=pv, t0=t0, nt=nt, Y_t=Y_t):
                        for j in range(nt):
                            ti = t0 + j
                            u = ti * 128 if ti < 2 else 260 + (ti - 2) * 128
                            ins_ = e.transpose(out=pv[:, j, :], in_=Y_t[:, u:u + 128], identity=k.ident_b)
                        return ins_
                    S.op("pe", tr, reads=[r_Y, k.r_ident_b], writes=[r_pb])
                    if tb % 2 == 0:
                        S.op("act", lambda e, pv=pv, t0=t0, nt=nt, tm_t=tm_t: e.activation(out=tm_t[:, t0:t0 + nt, :], in_=pv, func=AF.Copy),
                             reads=[r_pb], writes=[r_tm])
                    else:
                        S.op("dve", lambda e, pv=pv, t0=t0, nt=nt, tm_t=tm_t: e.tensor_copy(out=tm_t[:, t0:t0 + nt, :], in_=pv),
                             reads=[r_pb], writes=[r_tm])
                dst_s, r_dst = (kd_s, r_kd_s) if cc < 16 else (vd_s, r_vd_s)
                k.store(dst_s.rearrange("(n p) h d -> p n h d", p=128)[:, :, head, :], r_dst, tm_t, r_tm)
    S.barrier()
    A.release()
    A.release()

import os as _os


def _drive(*gens):
    gens = [g for g in gens if g is not None]
    while gens:
        for g in list(gens):
            try:
                next(g)
            except StopIteration:
                gens.remove(g)


def phase_b(k):
    S, A, nc, ins = k.S, k.A, k.nc, k.ins
    qdT_s, r_qdT_s = k.scr["qdT_s"]
    kdT_s, r_kdT_s = k.scr["kdT_s"]
    kd_s, r_kd_s = k.scr["kd_s"]
    vd_s, r_vd_s = k.scr["vd_s"]
    gb_s, r_gb_s = k.scr["gb_s"]
    o_s = [k.scratch("of_s", [TX, D], F32), k.scratch("ob_s", [TX, D], F32)]
    A.mark()
    msk, r_msk = k.tile([128, 9, 128], F32, "msk")
    k.load(msk, r_msk, ins["dn_masks"])
    EC, r_EC = k.tile([64, 2, 8, 128], F32, "EC")
    k.load(EC, r_EC, ins["dn_esel"])
    L1, r_L1 = k.tile([64, 128], F32, "L1")
    L2, r_L2 = k.tile([64, 128], F32, "L2")
    R1, r_R1 = k.tile([64, 8, 128], F32, "R1")
    R2, r_R2 = k.tile([64, 8, 128], F32, "R2")
    X, r_X = k.tile([128, 2, 64], F32, "X")
    k.load(L1, r_L1, ins["dn_linit"])
    k.load(L2, r_L2, ins["dn_linit"])
    S.op("dve", lambda e: e.tensor_copy(out=R1, in_=EC[:, 0, :, :]), reads=[r_EC], writes=[r_R1])
    S.op("dve", lambda e: e.tensor_copy(out=R2, in_=EC[:, 1, :, :]), reads=[r_EC], writes=[r_R2])
    S.op("pool", lambda e: e.memset(X, 0.0), writes=[r_X])
    S32 = [k.tile([128, 8, 128], F32, f"S32_{d}") for d in range(2)]
    Sbf = [k.tile([128, 8, 128], BF16, f"Sbf_{d}") for d in range(2)]
    for d in range(2):
        S.op("pool", lambda e, d=d: e.memset(S32[d][0], 0.0), writes=[S32[d][1]])
        S.op("pool", lambda e, d=d: e.memset(Sbf[d][0], 0.0), writes=[Sbf[d][1]])
    NB = 2
    gbt = [k.tile([128, 48], F32, "gbt") for _ in range(NB)]
    kT = [k.tile([128, 8, 128], BF16, "kT") for _ in range(NB)]
    qT = [k.tile([128, 8, 128], BF16, "qT") for _ in range(NB)]
    ktm = [k.tile([128, 8, 128], BF16, "ktm") for _ in range(NB)]
    vtm = [k.tile([128, 8, 128], BF16, "vtm") for _ in range(NB)]
    sm = [k.tile([128, 4, 8], F32, "sm") for _ in range(NB)]
    E1, r_E1 = k.tile([128, 8, 128], F32, "E1")
    M, r_M = k.tile([128, 8, 128], F32, "M")
    Mt, r_Mt = k.tile([128, 8, 128], F32, "Mt")
    Md, r_Md = k.tile([128, 8, 128], F32, "Md")
    Mo1, r_Mo1 = k.tile([128, 8, 128], F32, "Mo1")
    Mo2, r_Mo2 = k.tile([128, 8, 128], F32, "Mo2")
    PP = [k.tile([128, 8, 128], F32, f"PP{j}") for j in range(2)]
    PT = [k.tile([128, 8, 128], F32, f"PT{j}") for j in range(2)]
    Tt, r_Tt = k.tile([128, 8, 128], F32, "Tt")
    TtB, r_TtB = k.tile([128, 8, 128], BF16, "TtB")
    Abuf, _ = k.tile([128, 8, 128], F32, "Abuf")
    E1r, Mdr, Mo1r, Mo2r, Mtr, Ttr = (t_ for t_ in (Abuf, Md, Mo1, Mo2, Mt, Tt))
    PPr = [PP[j][0] for j in range(2)]
    PTr = [PT[j][0] for j in range(2)]
    kg, r_kg = k.tile([128, 8, 128], BF16, "kg")
    kdec = [k.tile([128, 8, 128], BF16, "kdec") for _ in range(NB)]
    wT = [k.tile([128, 8, 128], BF16, "wT") for _ in range(NB)]
    u = [k.tile([128, 8, 128], F32, "u") for _ in range(NB)]
    qkT = [k.tile([128, 8, 128], BF16, "qkT") for _ in range(NB)]
    vnew, r_vnew = k.tile([128, 8, 128], BF16, "vnew")
    tmpo, r_tmpo = k.tile([128, 8, 128], F32, "tmpo")
    o_t = [k.tile([128, 8, 128], F32, "o") for _ in range(NB)]

    RG = {nm: [Res(nm + "0"), Res(nm + "1")] for nm in ("Ab", "E1", "M", "Md", "Mo1", "Mo2", "Mt", "Tt", "TtB", "PP0", "PP1", "PT0", "PT1")}
    wT_res = [[Res("wT"), Res("wT")] for _ in range(NB)]
    u_res = [[Res("u"), Res("u")] for _ in range(NB)]
    qk_res = [[Res("qk"), Res("qk")] for _ in range(NB)]
    pre_i = [0]

    def nbp(dtype=F32):
        b = pre_i[0]
        pre_i[0] = (b + 1) % 4
        return k.bank(b, dtype)

    def b4(pb):
        return pb.rearrange("p (a b) -> p a b", b=128)

    units = []
    fwd = list(range(NT))
    bwd = [1, 0] + list(range(NT - 1, 1, -1))
    for s in range(NT):
        units.append((0, fwd[s]))
        units.append((1, bwd[s]))

    def loads(ui):
        d, ti = units[ui]
        b = ui % NB
        tok = slice(ti * 128, (ti + 1) * 128)
        k.load(gbt[b][0], gbt[b][1], gb_s[tok, :], r_gb_s)
        k.load(kT[b][0], kT[b][1], kdT_s[:, :, tok].rearrange("h d t -> d h t"), r_kdT_s)
        k.load(ktm[b][0], ktm[b][1], kd_s[tok, :, :], r_kd_s, q="act")
        k.load(vtm[b][0], vtm[b][1], vd_s[tok, :, :], r_vd_s, q="act")
        if ti >= 2:
            xt_ = slice((ti - 2) * 128, (ti - 1) * 128)
            k.load(qT[b][0], qT[b][1], qdT_s[:, :, xt_].rearrange("h d t -> d h t"), r_qdT_s)

    def pre(ui):
        d, ti = units[ui]
        b = ui % NB
        is_x = ti >= 2
        g_t, r_g = gbt[b]
        kT_t, r_kT = kT[b]
        qT_t, r_qT = qT[b]
        ktm_t, r_ktm = ktm[b]
        vtm_t, r_vtm = vtm[b]
        sm_t, r_sm = sm[b]
        gcol = g_t[:, d * 8:(d + 1) * 8]
        beta = g_t[:, 16 + d * 8:16 + (d + 1) * 8]
        lnb = g_t[:, 32 + d * 8:32 + (d + 1) * 8]
        pb, r_pb = nbp()

        def mm0(e):
            e.matmul(pb[:, 0:8], lhsT=msk[:, d, :], rhs=gcol, start=True, stop=True)
            return e.matmul(pb[:, 8:16], lhsT=k.ones_f, rhs=gcol, start=True, stop=True)
        S.op("pe", mm0, reads=[r_msk, r_g, k.r_ones_f], writes=[r_pb])
        S.op("dve", lambda e: e.tensor_copy(out=X[:, :, 32:40], in_=pb[:, 0:8].unsqueeze(1).to_broadcast([128, 2, 8])), reads=[r_pb], writes=[r_X])
        S.op("dve", lambda e: e.tensor_copy(out=X[:, 1, 0:8], in_=pb[:, 0:8]), reads=[r_pb], writes=[r_X])
        S.op("dve", lambda e: e.tensor_tensor(out=X[:, 0, 0:8], in0=pb[:, 0:8], in1=lnb, op=ALU.add), reads=[r_pb, r_g], writes=[r_X])
        S.op("act", lambda e: e.activation(out=sm_t[:, 0, :], in_=pb[:, 0:8], func=AF.Exp), reads=[r_pb], writes=[r_sm])
        S.op("act", lambda e: e.activation(out=sm_t[:, 1, :], in_=pb[:, 8:16], func=AF.Exp), reads=[r_pb], writes=[r_sm])
        S.op("dve", lambda e: e.tensor_tensor(out=sm_t[:, 3, :], in0=pb[:, 8:16], in1=X[:, 1, 0:8], op=ALU.subtract), reads=[r_pb, r_X], writes=[r_sm])
        S.op("act", lambda e: e.activation(out=sm_t[:, 2, :], in_=sm_t[:, 3, :], func=AF.Exp), reads=[r_sm], writes=[r_sm])
        pt, r_pt = nbp()

        def tr0(e):
            e.transpose(out=pt[0:64, 0:128], in_=X[:, 0, :], identity=k.ident_f)
            return e.transpose(out=pt[0:64, 128:256], in_=X[:, 1, :], identity=k.ident_f)
        S.op("pe", tr0, reads=[r_X, k.r_ident_f], writes=[r_pt])
        S.op("act", lambda e: e.activation(out=L1[0:8, :], in_=pt[0:8, 0:128], func=AF.Copy), reads=[r_pt], writes=[r_L1])
        S.op("act", lambda e: e.activation(out=L2[0:8, :], in_=pt[0:8, 128:256], func=AF.Copy), reads=[r_pt], writes=[r_L2])
        S.op("dve", lambda e: e.tensor_tensor(out=R1[32:40, :, :], in0=EC[32:40, 1, :, :], in1=pt[32:40, 0:128].unsqueeze(1).to_broadcast([8, 8, 128]), op=ALU.mult),
             reads=[r_pt, r_EC], writes=[r_R1])
        S.op("dve", lambda e: e.tensor_tensor(out=R2[32:40, :, :], in0=EC[32:40, 0, :, :], in1=pt[32:40, 128:256].unsqueeze(1).to_broadcast([8, 8, 128]), op=ALU.mult),
             reads=[r_pt, r_EC], writes=[r_R2])
        kd_t, r_kd = kdec[b]
        S.op("pool", lambda e: e.tensor_tensor(out=kg, in0=ktm_t, in1=sm_t[:, 0, :].unsqueeze(2).to_broadcast([128, 8, 128]), op=ALU.mult),
             reads=[r_ktm, r_sm], writes=[r_kg])
        S.op("pool", lambda e: e.tensor_tensor(out=kd_t, in0=ktm_t, in1=sm_t[:, 2, :].unsqueeze(2).to_broadcast([128, 8, 128]), op=ALU.mult),
             reads=[r_ktm, r_sm], writes=[r_kd])
        yield
        def do_group(grp):
            hs = range(4 * grp, 4 * grp + 4)
            gs = slice(4 * grp, 4 * grp + 4)
            r_E1, r_M, r_Md, r_Mo1, r_Mo2, r_Mt, r_Tt, r_TtB = (RG[nm][grp] for nm in ("E1", "M", "Md", "Mo1", "Mo2", "Mt", "Tt", "TtB"))
            r_Ab = RG["Ab"][grp]
            rPP = [RG["PP0"][grp], RG["PP1"][grp]]
            rPT = [RG["PT0"][grp], RG["PT1"][grp]]
            Mo_list = ((Mo1r, r_Mo1), (Mo2r, r_Mo2))
            Md_list = ((Mdr, r_Md, 6), (Mo1r, r_Mo1, 7), (Mo2r, r_Mo2, 8))
            pk, r_pk = nbp()
            pd, r_pd = nbp()

            def mmk(e):
                for hh, h in enumerate(hs):
                    ins_ = e.matmul(b4(pk)[:, hh, :], lhsT=kT_t[:, h, :], rhs=kT_t[:, h, :], start=True, stop=True)
                return ins_
            S.op("pe", mmk, reads=[r_kT], writes=[r_pk])

            def mmd(e):
                for hh, h in enumerate(hs):
                    ins_ = e.matmul(b4(pd)[:, hh, :], lhsT=L1, rhs=R1[:, h, :], start=True, stop=True)
                return ins_
            S.op("pe", mmd, reads=[r_L1, r_R1], writes=[r_pd])
            S.op("dve", lambda e: e.scalar_tensor_tensor(out=E1[:, gs, :], in0=b4(pd), scalar=0.0, in1=msk[:, 2 + d, :].unsqueeze(1).to_broadcast([128, 4, 128]),
                                                         op0=ALU.min, op1=ALU.add), reads=[r_pd, r_msk], writes=[r_E1])
            S.op("act", lambda e: e.activation(out=E1[:, gs, :], in_=E1[:, gs, :], func=AF.Exp), reads=[r_E1], writes=[r_E1])
            S.op("dve", lambda e: e.tensor_tensor(out=M[:, gs, :], in0=b4(pk), in1=E1[:, gs, :], op=ALU.mult), reads=[r_pk, r_E1], writes=[r_M])
            for (dst_, rdst_, mi_) in Md_list:
                S.op("pool", lambda e, dst_=dst_, mi_=mi_: e.tensor_tensor(out=dst_[:, gs, :], in0=M[:, gs, :],
                                                                        in1=msk[:, mi_, :].unsqueeze(1).to_broadcast([128, 4, 128]), op=ALU.mult),
                     reads=[r_M, r_msk], writes=[rdst_])
            pm, r_pm = nbp()

            def trm(e):
                for hh, h in enumerate(hs):
                    ins_ = e.transpose(out=b4(pm)[:, hh, :], in_=Md[:, h, :], identity=k.ident_f)
                return ins_
            S.op("pe", trm, reads=[r_Md, k.r_ident_f], writes=[r_pm])
            S.op("act", lambda e: e.activation(out=Mtr[:, gs, :], in_=b4(pm), func=AF.Copy), reads=[r_pm], writes=[r_Mt])
            S.op("dve", lambda e: e.scalar_tensor_tensor(out=Ttr[:, gs, :], in0=b4(pm), scalar=-1.0, in1=k.ident_f.unsqueeze(1).to_broadcast([128, 4, 128]),
                                                         op0=ALU.mult, op1=ALU.add), reads=[r_pm, k.r_ident_f], writes=[r_Tt])
            yield
            P_prev, rP_prev, Pt_prev, rPt_prev = Mdr, r_Md, Mtr, r_Mt
            for lvl in range(1, 1 + int(_os.environ.get('B_LEVELS', 4))):
                P_new, rP_new = PPr[lvl % 2], rPP[lvl % 2]
                Pt_new, rPt_new = PTr[lvl % 2], rPT[lvl % 2]
                pa, r_pa = nbp()

                def mma(e, P_prev=P_prev, Pt_prev=Pt_prev, pa=pa):
                    for hh, h in enumerate(hs):
                        ins_ = e.matmul(b4(pa)[:, hh, :], lhsT=Pt_prev[:, h, :], rhs=P_prev[:, h, :], start=True, stop=True)
                    return ins_
                S.op("pe", mma, reads=[rP_prev, rPt_prev], writes=[r_pa])
                S.op("act", lambda e, P_new=P_new, pa=pa: e.activation(out=P_new[:, gs, :], in_=b4(pa), func=AF.Copy), reads=[r_pa], writes=[rP_new])
                if lvl < int(_os.environ.get('B_LEVELS', 4)):
                    pbk, r_pbk = nbp()

                    def mmb(e, P_prev=P_prev, Pt_prev=Pt_prev, pbk=pbk):
                        for hh, h in enumerate(hs):
                            ins_ = e.matmul(b4(pbk)[:, hh, :], lhsT=P_prev[:, h, :], rhs=Pt_prev[:, h, :], start=True, stop=True)
                        return ins_
                    S.op("pe", mmb, reads=[rP_prev, rPt_prev], writes=[r_pbk])
                    S.op("act", lambda e, Pt_new=Pt_new, pbk=pbk: e.activation(out=Pt_new[:, gs, :], in_=b4(pbk), func=AF.Copy), reads=[r_pbk], writes=[rPt_new])
                pc, r_pc = nbp()

                def mmc(e, P_new=P_new, pc=pc):
                    for hh, h in enumerate(hs):
                        ins_ = e.matmul(b4(pc)[:, hh, :], lhsT=P_new[:, h, :], rhs=Ttr[:, h, :], start=True, stop=True)
                    return ins_
                S.op("pe", mmc, reads=[rP_new, r_Tt], writes=[r_pc])
                S.op("dve", lambda e, pc=pc: e.tensor_tensor(out=Ttr[:, gs, :], in0=Tt[:, gs, :], in1=b4(pc), op=ALU.add), reads=[r_Tt, r_pc], writes=[r_Tt])
                P_prev, rP_prev, Pt_prev, rPt_prev = P_new, rP_new, Pt_new, rPt_new
                yield
            for (Mo_, rMo_) in Mo_list:
                ptd, r_ptd = nbp()

                def trt(e, ptd=ptd):
                    for hh, h in enumerate(hs):
                        ins_ = e.transpose(out=b4(ptd)[:, hh, :], in_=Tt[:, h, :], identity=k.ident_f)
                    return ins_
                S.op("pe", trt, reads=[r_Tt, k.r_ident_f], writes=[r_ptd])
                S.op("act", lambda e, ptd=ptd: e.activation(out=Mtr[:, gs, :], in_=b4(ptd), func=AF.Copy), reads=[r_ptd], writes=[r_Mt])
                pa2, r_pa2 = nbp()

                def mma2(e, pa2=pa2, Mo_=Mo_):
                    for hh, h in enumerate(hs):
                        ins_ = e.matmul(b4(pa2)[:, hh, :], lhsT=Mo_[:, h, :], rhs=Ttr[:, h, :], start=True, stop=True)
                    return ins_
                S.op("pe", mma2, reads=[rMo_, r_Tt], writes=[r_pa2])
                S.op("act", lambda e, pa2=pa2: e.activation(out=E1r[:, gs, :], in_=b4(pa2), func=AF.Copy), reads=[r_pa2], writes=[r_Ab])
                pc2, r_pc2 = nbp()

                def mmc2(e, pc2=pc2):
                    for hh, h in enumerate(hs):
                        ins_ = e.matmul(b4(pc2)[:, hh, :], lhsT=Mtr[:, h, :], rhs=E1r[:, h, :], start=True, stop=True)
                    return ins_
                S.op("pe", mmc2, reads=[r_Mt, r_Ab], writes=[r_pc2])
                S.op("dve", lambda e, pc2=pc2: e.tensor_tensor(out=Ttr[:, gs, :], in0=Tt[:, gs, :], in1=b4(pc2), op=ALU.subtract), reads=[r_Tt, r_pc2], writes=[r_Tt])
                yield
            S.op("pool", lambda e: e.tensor_tensor(out=TtB[:, gs, :], in0=Tt[:, gs, :], in1=beta[:, gs].unsqueeze(2).to_broadcast([128, 4, 128]), op=ALU.mult),
                 reads=[r_Tt, r_g], writes=[r_TtB])
            pw, r_pw = nbp()

            def mmw(e):
                for hh, h in enumerate(hs):
                    ins_ = e.matmul(b4(pw)[:, hh, :], lhsT=kg[:, h, :], rhs=TtB[:, h, :], start=True, stop=True)
                return ins_
            S.op("pe", mmw, reads=[r_kg, r_TtB], writes=[r_pw])
            S.op("act", lambda e: e.activation(out=wT[b][0][:, gs, :], in_=b4(pw), func=AF.Copy), reads=[r_pw], writes=[wT_res[b][grp]])
            pu, r_pu = nbp()

            def mmu(e):
                for hh, h in enumerate(hs):
                    ins_ = e.matmul(b4(pu)[:, hh, :], lhsT=TtB[:, h, :], rhs=vtm_t[:, h, :], start=True, stop=True)
                return ins_
            S.op("pe", mmu, reads=[r_TtB, r_vtm], writes=[r_pu])
            S.op("act", lambda e: e.activation(out=u[b][0][:, gs, :], in_=b4(pu), func=AF.Copy), reads=[r_pu], writes=[u_res[b][grp]])
            yield
            if is_x:
                pq, r_pq = nbp()
                pd2, r_pd2 = nbp()

                def mmq(e):
                    for hh, h in enumerate(hs):
                        ins_ = e.matmul(b4(pq)[:, hh, :], lhsT=kT_t[:, h, :], rhs=qT_t[:, h, :], start=True, stop=True)
                    return ins_
                S.op("pe", mmq, reads=[r_kT, r_qT], writes=[r_pq])

                def mmd2(e):
                    for hh, h in enumerate(hs):
                        ins_ = e.matmul(b4(pd2)[:, hh, :], lhsT=L2, rhs=R2[:, h, :], start=True, stop=True)
                    return ins_
                S.op("pe", mmd2, reads=[r_L2, r_R2], writes=[r_pd2])
                S.op("dve", lambda e: e.scalar_tensor_tensor(out=E1[:, gs, :], in0=b4(pd2), scalar=0.0, in1=msk[:, 4 + d, :].unsqueeze(1).to_broadcast([128, 4, 128]),
                                                             op0=ALU.min, op1=ALU.add), reads=[r_pd2, r_msk], writes=[r_E1])
                S.op("act", lambda e: e.activation(out=E1[:, gs, :], in_=E1[:, gs, :], func=AF.Exp), reads=[r_E1], writes=[r_E1])
                S.op("dve", lambda e: e.tensor_tensor(out=qkT[b][0][:, gs, :], in0=b4(pq), in1=E1[:, gs, :], op=ALU.mult), reads=[r_pq, r_E1], writes=[qk_res[b][grp]])
                yield
        gens_ = [do_group(0), do_group(1)]
        while gens_:
            for g_ in list(gens_):
                try:
                    next(g_)
                    yield
                except StopIteration:
                    gens_.remove(g_)

    def seq(ui):
        d, ti = units[ui]
        b = ui % NB
        is_x = ti >= 2
        S32_t, r_S32 = S32[d]
        Sbf_t, r_Sbf = Sbf[d]
        sm_t, r_sm = sm[b]
        wT_t, u_t, qk_t = wT[b][0], u[b][0], qkT[b][0]
        qT_t, r_qT = qT[b]
        kd_t, r_kd = kdec[b]
        banks = [k.bank(4 + j) for j in range(4)]

        def grp_mm(bank2, lhs_fn, rhs_fn, reads):
            for grp in range(2):
                pb, r_pb = bank2[grp]

                def f(e, grp=grp, pb=pb):
                    for hh in range(4):
                        h = 4 * grp + hh
                        ins_ = e.matmul(b4(pb)[:, hh, :], lhsT=lhs_fn(h), rhs=rhs_fn(h), start=True, stop=True)
                    return ins_
                S.op("pe", f, reads=reads, writes=[r_pb])
        grp_mm(banks[0:2], lambda h: wT_t[:, h, :], lambda h: Sbf_t[:, h, :], wT_res[b] + [r_Sbf])
        S.op("pool", lambda e: e.tensor_tensor(out=S32_t, in0=S32_t, in1=sm_t[:, 1, :].unsqueeze(2).to_broadcast([128, 8, 128]), op=ALU.mult),
             reads=[r_S32, r_sm], writes=[r_S32])
        yield
        for grp in range(2):
            gs = slice(4 * grp, 4 * grp + 4)
            pb, r_pb = banks[grp]
            S.op("dve", lambda e, pb=pb, gs=gs: e.tensor_tensor(out=vnew[:, gs, :], in0=u_t[:, gs, :], in1=b4(pb), op=ALU.subtract),
                 reads=u_res[b] + [r_pb], writes=[r_vnew])
        yield
        if is_x:
            grp_mm(banks[2:4], lambda h: qT_t[:, h, :], lambda h: Sbf_t[:, h, :], [r_qT, r_Sbf])
            grp_mm(banks[0:2], lambda h: qk_t[:, h, :], lambda h: vnew[:, h, :], qk_res[b] + [r_vnew])
            yield
            o_tt, r_o = o_t[b]
            for grp in range(2):
                gs = slice(4 * grp, 4 * grp + 4)
                pbq, r_pbq = banks[2 + grp]
                pbc, r_pbc = banks[grp]
                S.op("dve", lambda e, pbq=pbq, gs=gs: e.tensor_tensor(out=tmpo[:, gs, :], in0=b4(pbq), in1=sm_t[:, 0, gs].unsqueeze(2).to_broadcast([128, 4, 128]), op=ALU.mult),
                     reads=[r_pbq, r_sm], writes=[r_tmpo])
                S.op("dve", lambda e, pbc=pbc, gs=gs, o_tt=o_tt: e.tensor_tensor(out=o_tt[:, gs, :], in0=tmpo[:, gs, :], in1=b4(pbc), op=ALU.add),
                     reads=[r_tmpo, r_pbc], writes=[r_o])
            xi = ti - 2
            k.store(o_s[d][0][xi * 128:(xi + 1) * 128, :], o_s[d][1], o_tt.rearrange("p h d -> p (h d)"), r_o)
            yield
        grp_mm(banks[2:4], lambda h: kd_t[:, h, :], lambda h: vnew[:, h, :], [r_kd, r_vnew])
        yield
        for grp in range(2):
            gs = slice(4 * grp, 4 * grp + 4)
            pb, r_pb = banks[2 + grp]
            S.op("dve", lambda e, pb=pb, gs=gs: e.tensor_tensor(out=S32_t[:, gs, :], in0=S32_t[:, gs, :], in1=b4(pb), op=ALU.add),
                 reads=[r_S32, r_pb], writes=[r_S32])
        S.op("act", lambda e: e.activation(out=Sbf_t, in_=S32_t, func=AF.Copy), reads=[r_S32], writes=[r_Sbf])
        yield

    import os as _os
    stop_after = int(_os.environ.get("B_UNITS", len(units)))
    pre_cut = int(_os.environ.get("B_PRE_CUT", 10000))
    do_seq = int(_os.environ.get("B_SEQ", 1))
    _pre = pre
    _seq = seq

    def pre(ui):
        for n_, _ in enumerate(_pre(ui)):
            if n_ + 1 >= pre_cut:
                return
            yield

    def seq(ui):
        if not do_seq:
            return
        yield from _seq(ui)
    loads(0)
    _drive(pre(0))
    for ui in range(stop_after):
        if ui + 1 < stop_after:
            loads(ui + 1)
            _drive(pre(ui + 1), seq(ui))
        else:
            _drive(seq(ui))
    k.b_state = (S32, Sbf)
    S.barrier()
    A.release()

def phase_c(k):
    S, A, nc, ins = k.S, k.A, k.nc, k.ins
    of_s, r_of_s = k.scr["of_s"]
    ob_s, r_ob_s = k.scr["ob_s"]
    zs_s, r_zs_s = k.scr["zs_s"]
    qmT_s, r_qmT_s = k.scr["qmT_s"]
    kmT_s, r_kmT_s = k.scr["kmT_s"]
    vm_s, r_vm_s = k.scr["vm_s"]
    yaT_s, r_yaT_s = k.scratch("yaT_s", [H, 128, TX], BF16)
    ybT_s, r_ybT_s = k.scratch("ybT_s", [H, 128, TX], BF16)
    A.mark()
    dng, r_dng = k.tile([128, 128], F32, "dng")
    k.load(dng, r_dng, ins["dng_rep"])
    NB = 2
    of_t = [k.tile([128, 8, 128], F32, "of") for _ in range(NB)]
    ob_t = [k.tile([128, 8, 128], F32, "ob") for _ in range(NB)]
    z_t = [k.tile([128, 8, 128], BF16, "z") for _ in range(NB)]
    sq, r_sq = k.tile([128, 8, 128], F32, "sq")
    ss8, r_ss8 = k.tile([128, 8], F32, "ss8")
    yb, r_yb = k.tile([128, 8, 128], BF16, "yb")
    yT = [k.tile([128, 8, 128], BF16, "yT") for _ in range(NB)]

    def c1_loads(xi):
        b = xi % NB
        rows = slice(xi * 128, (xi + 1) * 128)
        k.load(of_t[b][0], of_t[b][1], of_s[rows, :].rearrange("p (h d) -> p h d", h=8), r_of_s)
        k.load(ob_t[b][0], ob_t[b][1], ob_s[rows, :].rearrange("p (h d) -> p h d", h=8), r_ob_s, q="act")
        k.load(z_t[b][0], z_t[b][1], zs_s[rows, :].rearrange("p (h d) -> p h d", h=8), r_zs_s)

    c1_loads(0)
    for xi in range(32):
        b = xi % NB
        if xi + 1 < 32:
            c1_loads(xi + 1)
        o_, r_o = of_t[b]
        ob_, r_ob = ob_t[b]
        z_, r_z = z_t[b]
        S.op("dve", lambda e, o_=o_, ob_=ob_: e.tensor_tensor(out=o_, in0=o_, in1=ob_, op=ALU.add), reads=[r_o, r_ob], writes=[r_o])
        S.op("pool", lambda e, o_=o_: e.tensor_tensor(out=sq, in0=o_, in1=o_, op=ALU.mult), reads=[r_o], writes=[r_sq])
        S.op("dve", lambda e: e.tensor_reduce(out=ss8, in_=sq, axis=AX.X, op=ALU.add), reads=[r_sq], writes=[r_ss8])
        _rstd(k, ss8, r_ss8, 128, ss8, r_ss8)
        S.op("dve", lambda e, o_=o_: e.tensor_tensor(out=o_, in0=o_, in1=ss8.unsqueeze(2).to_broadcast([128, 8, 128]), op=ALU.mult),
             reads=[r_o, r_ss8], writes=[r_o])
        S.op("pool", lambda e, o_=o_: e.tensor_tensor(out=o_, in0=o_, in1=dng.unsqueeze(1).to_broadcast([128, 8, 128]), op=ALU.mult),
             reads=[r_o, r_dng], writes=[r_o])
        S.op("pool", lambda e, o_=o_, z_=z_: e.tensor_tensor(out=yb, in0=o_, in1=z_, op=ALU.mult), reads=[r_o, r_z], writes=[r_yb])
        pb, r_pb = _nb(k, BF16)
        pv = pb.rearrange("p (a b) -> p a b", b=128)

        def tr(e, pv=pv):
            for j in range(8):
                ins_ = e.transpose(out=pv[:, j, :], in_=yb[:, j, :], identity=k.ident_b)
            return ins_
        S.op("pe", tr, reads=[r_yb, k.r_ident_b], writes=[r_pb])
        yT_t, r_yT = yT[b]
        S.op("act", lambda e, pv=pv, yT_t=yT_t: e.activation(out=yT_t, in_=pv, func=AF.Copy), reads=[r_pb], writes=[r_yT])
        k.store(yaT_s[:, :, xi * 128:(xi + 1) * 128].rearrange("h d t -> d h t"), r_yaT_s, yT_t, r_yT)
    S.barrier()
    A.release()
    A.mark()
    Kn = [k.tile([128, T], BF16, "Kn") for _ in range(2)]
    Kr = [k.tile([64, T], BF16, "Kr") for _ in range(2)]
    Vh = [k.tile([128, NT, 128], BF16, "Vh") for _ in range(2)]
    Qn = [k.tile([128, 512], BF16, "Qn") for _ in range(2)]
    Qr = [k.tile([64, 512], BF16, "Qr") for _ in range(2)]
    NP = 4
    PT = [k.tile([128, 512], BF16, "PT") for _ in range(NP)]
    rinv, r_rinv = k.tile([128, 512], F32, "rinv")
    yo = [k.tile([128, 512], BF16, "yo") for _ in range(2)]
    vmv = vm_s.rearrange("(n p) c -> p n c", p=128)

    def head_loads(h):
        b = h % 2
        k.load(Kn[b][0], Kn[b][1], kmT_s[h, 0:128, :], r_kmT_s)
        k.load(Kr[b][0], Kr[b][1], kmT_s[h, 128:192, :], r_kmT_s, q="act")
        k.load(Vh[b][0], Vh[b][1], vmv[:, :, h * 128:(h + 1) * 128], r_vm_s)

    def q_loads(h, qg):
        b = (h * 8 + qg) % 2
        k.load(Qn[b][0], Qn[b][1], qmT_s[h, 0:128, qg * 512:(qg + 1) * 512], r_qmT_s)
        k.load(Qr[b][0], Qr[b][1], qmT_s[h, 128:192, qg * 512:(qg + 1) * 512], r_qmT_s, q="act")

    head_loads(0)
    q_loads(0, 0)
    sbank = [0]
    it = 0
    for h in range(H):
        if h + 1 < H:
            head_loads(h + 1)
        Kn_t, r_Kn = Kn[h % 2]
        Kr_t, r_Kr = Kr[h % 2]
        V_t, r_V = Vh[h % 2]
        for qg in range(8):
            gi = h * 8 + qg
            nxt = gi + 1
            if nxt < H * 8:
                q_loads(nxt // 8, nxt % 8)
            Qn_t, r_Qn = Qn[gi % 2]
            Qr_t, r_Qr = Qr[gi % 2]
            po, r_po = k.bank(4 + gi % 2)
            pr, r_pr = k.bank(6 + gi % 2)
            sb = {}

            def emit_s(kt):
                bnk = sbank[0]
                sbank[0] = (bnk + 1) % 4
                ps_, r_ps = k.bank(bnk)

                def f(e, ps_=ps_, kt=kt, Kn_t=Kn_t, Kr_t=Kr_t, Qn_t=Qn_t, Qr_t=Qr_t):
                    e.matmul(ps_, lhsT=Kn_t[:, kt * 128:(kt + 1) * 128], rhs=Qn_t, start=True, stop=False)
                    return e.matmul(ps_, lhsT=Kr_t[:, kt * 128:(kt + 1) * 128], rhs=Qr_t, start=False, stop=True)
                S.op("pe", f, reads=[r_Kn, r_Kr, r_Qn, r_Qr], writes=[r_ps])
                pt_, r_pt = PT[kt % NP]
                S.op("act", lambda e, ps_=ps_, pt_=pt_: e.activation(out=pt_, in_=ps_, func=AF.Exp), reads=[r_ps], writes=[r_pt])
                sb[kt] = (pt_, r_pt)

            def emit_pv(kt):
                pt_, r_pt = sb.pop(kt)

                def f(e, pt_=pt_, kt=kt, po=po, pr=pr, V_t=V_t):
                    e.matmul(po, lhsT=V_t[:, kt, :], rhs=pt_, start=(kt == 0), stop=(kt == NT - 1))
                    return e.matmul(pr, lhsT=k.ones_b, rhs=pt_, start=(kt == 0), stop=(kt == NT - 1))
                S.op("pe", f, reads=[r_V, r_pt, k.r_ones_b], writes=[r_po, r_pr])

            LOOK = 2
            for kt in range(min(LOOK, NT)):
                emit_s(kt)
            for kt in range(NT):
                if kt + LOOK < NT:
                    emit_s(kt + LOOK)
                emit_pv(kt)
            S.op("dve", lambda e, pr=pr: e.reciprocal(out=rinv, in_=pr), reads=[r_pr], writes=[r_rinv])
            yo_t, r_yo = yo[gi % 2]
            S.op("dve", lambda e, po=po, yo_t=yo_t: e.tensor_tensor(out=yo_t, in0=po, in1=rinv, op=ALU.mult), reads=[r_po, r_rinv], writes=[r_yo])
            k.store(ybT_s[h, :, qg * 512:(qg + 1) * 512], r_ybT_s, yo_t, r_yo)
    S.barrier()
    A.release()

def phase_d(k):
    S, A, nc, ins = k.S, k.A, k.nc, k.ins
    yaT_s, r_yaT_s = k.scr["yaT_s"]
    ybT_s, r_ybT_s = k.scr["ybT_s"]
    sgT_s, r_sgT_s = k.scr["sgT_s"]
    xmid_s, r_xmid_s = k.scratch("xmid_s", [TX, D], F32)
    h2_s, r_h2_s = k.scratch("h2_s", [TX, D], BF16)
    aff_s, r_aff_s = k.scratch("aff_s", [TX, 16], F32)
    affT_s, r_affT_s = k.scratch("affT_s", [16, TX], F32)
    A.mark()
    woa, r_woa = k.tile([128, 8, D], BF16, "woa")
    wob, r_wob = k.tile([128, 8, D], BF16, "wob")
    wo, r_wo = k.tile([128, 8, D], BF16, "wo")
    rw, r_rw = k.tile([128, 8, 16], F32, "rw")
    for (dst, rdst, nm) in ((woa, r_woa, "w_out_a"), (wob, r_wob, "w_out_b"), (wo, r_wo, "w_o")):
        src = ins[nm].rearrange("(kc p) n -> p kc n", p=128)
        for hf in range(2):
            k.wload(dst[:, 4 * hf:4 * hf + 4, :], rdst, src[:, 4 * hf:4 * hf + 4, :])
    k.load(rw, r_rw, ins["router_w"].rearrange("(kc p) n -> p kc n", p=128))
    NB = 2
    yaT = [k.tile([128, 8, 512], BF16, "yaT") for _ in range(NB)]
    ybT = [k.tile([128, 8, 512], BF16, "ybT") for _ in range(NB)]
    gA = [k.tile([128, 8, 512], BF16, "gA") for _ in range(NB)]
    gB = [k.tile([128, 8, 512], BF16, "gB") for _ in range(NB)]
    mg, r_mg = k.tile([128, 8, 512], BF16, "mg")
    t1 = [k.tile([128, 512], F32, "t1") for _ in range(2)]
    t2 = [k.tile([128, 512], F32, "t2") for _ in range(2)]
    xt = [k.tile([128, D], F32, "xt") for _ in range(NB)]
    xm = [k.tile([128, D], F32, "xm") for _ in range(NB)]
    junk, r_junk = k.tile([128, D], BF16, "junk")
    ssd = [k.tile([128, 8], F32, "ssd") for _ in range(NB)]
    h2f, r_h2f = k.tile([128, D], F32, "h2f")
    h2b = [k.tile([128, D], BF16, "h2b") for _ in range(NB)]
    h2T, r_h2T = k.tile([128, 8, 128], F32, "h2T")
    ex = [k.tile([128, 16], F32, "ex") for _ in range(NB)]
    affT = [k.tile([16, 128], F32, "affT") for _ in range(NB)]

    def g_loads(g):
        b = g % NB
        cols = slice(g * 512, (g + 1) * 512)
        k.load(yaT[b][0], yaT[b][1], yaT_s[:, :, cols].rearrange("h d t -> d h t"), r_yaT_s)
        k.load(ybT[b][0], ybT[b][1], ybT_s[:, :, cols].rearrange("h d t -> d h t"), r_ybT_s, q="act")
        k.load(gA[b][0], gA[b][1], sgT_s[0:8, :, cols].rearrange("h d t -> d h t"), r_sgT_s)
        k.load(gB[b][0], gB[b][1], sgT_s[8:16, :, cols].rearrange("h d t -> d h t"), r_sgT_s, q="act")

    g_loads(0)
    for g in range(8):
        b = g % NB
        if g + 1 < 8:
            g_loads(g + 1)
        ya_, r_ya = yaT[b]
        yb_, r_yb = ybT[b]
        gA_, r_gA = gA[b]
        gB_, r_gB = gB[b]
        for oc in range(8):
            pa, r_pa = _nb(k)
            pbk, r_pbk = _nb(k)

            def mma(e, pa=pa, oc=oc, ya_=ya_):
                for kc in range(8):
                    ins_ = e.matmul(pa, lhsT=woa[:, kc, oc * 128:(oc + 1) * 128], rhs=ya_[:, kc, :], start=(kc == 0), stop=(kc == 7))
                return ins_
            S.op("pe", mma, reads=[r_woa, r_ya], writes=[r_pa])

            def mmb(e, pbk=pbk, oc=oc, yb_=yb_):
                for kc in range(8):
                    ins_ = e.matmul(pbk, lhsT=wob[:, kc, oc * 128:(oc + 1) * 128], rhs=yb_[:, kc, :], start=(kc == 0), stop=(kc == 7))
                return ins_
            S.op("pe", mmb, reads=[r_wob, r_yb], writes=[r_pbk])
            t1_, r_t1 = t1[oc % 2]
            t2_, r_t2 = t2[oc % 2]
            S.op("dve", lambda e, pa=pa, oc=oc, t1_=t1_, gA_=gA_: e.tensor_tensor(out=t1_, in0=pa, in1=gA_[:, oc, :], op=ALU.mult), reads=[r_pa, r_gA], writes=[r_t1])
            S.op("dve", lambda e, pbk=pbk, oc=oc, t2_=t2_, gB_=gB_: e.tensor_tensor(out=t2_, in0=pbk, in1=gB_[:, oc, :], op=ALU.mult), reads=[r_pbk, r_gB], writes=[r_t2])
            S.op("pool", lambda e, oc=oc, t1_=t1_, t2_=t2_: e.tensor_tensor(out=mg[:, oc, :], in0=t1_, in1=t2_, op=ALU.add), reads=[r_t1, r_t2], writes=[r_mg])
        for tt in range(4):
            ti = g * 4 + tt
            tb = ti % NB
            rows = slice(ti * 128, (ti + 1) * 128)
            x_, r_x = xt[tb]
            xm_, r_xm = xm[tb]
            ss_, r_ss = ssd[tb]
            k.load(x_, r_x, ins["x"][rows, :])
            for hf in range(2):
                pm, r_pm = _nb(k)

                def mmo(e, pm=pm, hf=hf, tt=tt):
                    for kc in range(8):
                        ins_ = e.matmul(pm, lhsT=mg[:, kc, tt * 128:(tt + 1) * 128], rhs=wo[:, kc, hf * 512:(hf + 1) * 512], start=(kc == 0), stop=(kc == 7))
                    return ins_
                S.op("pe", mmo, reads=[r_mg, r_wo], writes=[r_pm])
                S.op("dve", lambda e, pm=pm, hf=hf, xm_=xm_: e.tensor_tensor(out=xm_[:, hf * 512:(hf + 1) * 512], in0=pm, in1=k.gate1_row[:, hf * 512:(hf + 1) * 512], op=ALU.mult),
                     reads=[r_pm, k.r_gate1], writes=[r_xm])
            S.op("pool", lambda e, xm_=xm_, x_=x_: e.tensor_tensor(out=xm_, in0=xm_, in1=x_, op=ALU.add), reads=[r_xm, r_x], writes=[r_xm])
            k.store(xmid_s[rows, :], r_xmid_s, xm_, r_xm)
            k.store(k.out[rows, :], k.out_res, xm_, r_xm)
            S.op("act", lambda e, xm_=xm_, ss_=ss_: e.activation(out=junk, in_=xm_, func=AF.Square, accum_out=ss_[:, 0:1]), reads=[r_xm], writes=[r_junk, r_ss])
            _rstd(k, ss_[:, 0:1], r_ss, D, ss_[:, 1:2], r_ss)
            S.op("act", lambda e, xm_=xm_, ss_=ss_: e.activation(out=h2f, in_=xm_, func=AF.Copy, scale=ss_[:, 1:2]), reads=[r_xm, r_ss], writes=[r_h2f])
            S.op("pool", lambda e: e.tensor_tensor(out=h2f, in0=h2f, in1=k.s2_row, op=ALU.mult), reads=[r_h2f, k.r_s2row], writes=[r_h2f])
            S.op("pool", lambda e: e.tensor_tensor(out=h2f, in0=h2f, in1=k.shift2_row, op=ALU.add), reads=[r_h2f, k.r_shift2], writes=[r_h2f])
            h2b_, r_h2b = h2b[tb]
            S.op("act", lambda e, h2b_=h2b_: e.activation(out=h2b_, in_=h2f, func=AF.Copy), reads=[r_h2f], writes=[r_h2b])
            k.store(h2_s[rows, :], r_h2_s, h2b_, r_h2b)
            for hf in range(2):
                pt, r_pt = _nb(k)
                pv = pt.rearrange("p (a b) -> p a b", b=128)

                def trh(e, pv=pv, hf=hf):
                    for j in range(4):
                        ins_ = e.transpose(out=pv[:, j, :], in_=h2f[:, (hf * 4 + j) * 128:(hf * 4 + j + 1) * 128], identity=k.ident_f)
                    return ins_
                S.op("pe", trh, reads=[r_h2f, k.r_ident_f], writes=[r_pt])
                if hf == 0:
                    S.op("act", lambda e, pv=pv: e.activation(out=h2T[:, 0:4, :], in_=pv, func=AF.Copy), reads=[r_pt], writes=[r_h2T])
                else:
                    S.op("dve", lambda e, pv=pv: e.tensor_copy(out=h2T[:, 4:8, :], in_=pv), reads=[r_pt], writes=[r_h2T])
            pl, r_pl = _nb(k)

            def mml(e, pl=pl):
                for kc in range(8):
                    ins_ = e.matmul(pl[:, 0:16], lhsT=h2T[:, kc, :], rhs=rw[:, kc, :], start=(kc == 0), stop=(kc == 7))
                return ins_
            S.op("pe", mml, reads=[r_h2T, r_rw], writes=[r_pl])
            ex_, r_ex = ex[tb]
            S.op("dve", lambda e, pl=pl, ss_=ss_: e.tensor_reduce(out=ss_[:, 2:3], in_=pl[:, 0:16], axis=AX.X, op=ALU.max), reads=[r_pl], writes=[r_ss])
            S.op("dve", lambda e, ss_=ss_: e.tensor_scalar(out=ss_[:, 3:4], in0=ss_[:, 2:3], scalar1=-1.0, scalar2=None, op0=ALU.mult), reads=[r_ss], writes=[r_ss])
            S.op("act", lambda e, pl=pl, ss_=ss_, ex_=ex_: e.activation(out=ex_, in_=pl[:, 0:16], func=AF.Exp, bias=ss_[:, 3:4], accum_out=ss_[:, 4:5]),
                 reads=[r_pl, r_ss], writes=[r_ex, r_ss])
            S.op("dve", lambda e, ss_=ss_: e.reciprocal(out=ss_[:, 5:6], in_=ss_[:, 4:5]), reads=[r_ss], writes=[r_ss])
            S.op("dve", lambda e, ss_=ss_, ex_=ex_: e.tensor_scalar(out=ex_, in0=ex_, scalar1=ss_[:, 5:6], scalar2=None, op0=ALU.mult), reads=[r_ex, r_ss], writes=[r_ex])
            k.store(aff_s[rows, :], r_aff_s, ex_, r_ex)
            pt2, r_pt2 = _nb(k)
            S.op("pe", lambda e, pt2=pt2, ex_=ex_: e.transpose(out=pt2[0:16, 0:128], in_=ex_, identity=k.ident_f), reads=[r_ex, k.r_ident_f], writes=[r_pt2])
            aT_, r_aT = affT[tb]
            S.op("act", lambda e, pt2=pt2, aT_=aT_: e.activation(out=aT_, in_=pt2[0:16, 0:128], func=AF.Copy), reads=[r_pt2], writes=[r_aT])
            k.store(affT_s[:, rows], r_affT_s, aT_, r_aT)
    S.barrier()
    A.release()

NE = 16
CAP = 512
FF = 1408
NFC = 11


def phase_e(k):
    S, A, nc, ins = k.S, k.A, k.nc, k.ins
    aff_s, r_aff_s = k.scr["aff_s"]
    affT_s, r_affT_s = k.scr["affT_s"]
    h2_s, r_h2_s = k.scr["h2_s"]
    xmid_s, r_xmid_s = k.scr["xmid_s"]
    posmT_s, r_posmT_s = k.scratch("posmT_s", [NE, TX], F32)
    gc_s, r_gc_s = k.scratch("gc_s", [NE, 128, 4], F32)
    idx_s, r_idx_s = k.scratch("idx_s", [NE, 128, 4], I32)
    A.off = k.off_after_gate2
    A.mark()
    cst, r_cst = k.tile([128, 1024], F32, "cst")
    k.load(cst, r_cst, ins["consts"])
    blk, r_blk = k.tile([128, 128], F32, "blk")
    k.load(blk, r_blk, ins["moe_blk"])
    sel8, r_sel8 = k.tile([128, 16], F32, "sel8")
    k.load(sel8, r_sel8, ins["moe_sel8"])
    tris, r_tris = k.tile([128, 128], BF16, "tris")
    k.wload(tris, r_tris, ins["moe_tris"])
    iota_c = cst[:, 0:512]
    A.mark()
    A8, r_A8 = k.tile([128, 512], F32, "A8")
    k.load(A8, r_A8, affT_s.rearrange("e (s t) -> (e s) t", s=8), r_affT_s)
    junk, r_junk = k.tile([128, 512], F32, "junk")
    sc, r_sc = k.tile([128, 16], F32, "sc")
    S.op("pool", lambda e: e.memset(sc, 0.0), writes=[r_sc])
    S.op("pool", lambda e: e.memset(sc[:, 1:2], 1.0), reads=[r_sc], writes=[r_sc])
    for it in range(30):
        S.op("dve", lambda e: e.tensor_tensor(out=sc[:, 2:3], in0=sc[:, 0:1], in1=sc[:, 1:2], op=ALU.add), reads=[r_sc], writes=[r_sc])
        S.op("dve", lambda e: e.tensor_scalar(out=sc[:, 2:3], in0=sc[:, 2:3], scalar1=0.5, scalar2=None, op0=ALU.mult), reads=[r_sc], writes=[r_sc])
        S.op("dve", lambda e: e.tensor_scalar(out=junk, in0=A8, scalar1=sc[:, 2:3], scalar2=0.0, op0=ALU.is_ge, op1=ALU.add, accum_out=sc[:, 3:4]),
             reads=[r_A8, r_sc], writes=[r_junk, r_sc])
        pb, r_pb = _nb(k)
        S.op("pe", lambda e, pb=pb: e.matmul(pb[:, 0:1], lhsT=blk, rhs=sc[:, 3:4], start=True, stop=True), reads=[r_blk, r_sc], writes=[r_pb])
        S.op("dve", lambda e, pb=pb: e.tensor_scalar(out=sc[:, 4:5], in0=pb[:, 0:1], scalar1=CAP - 0.5, scalar2=None, op0=ALU.is_ge), reads=[r_pb], writes=[r_sc])
        S.op("dve", lambda e: e.tensor_scalar(out=sc[:, 5:6], in0=sc[:, 4:5], scalar1=-1.0, scalar2=1.0, op0=ALU.mult, op1=ALU.add), reads=[r_sc], writes=[r_sc])
        S.op("dve", lambda e: e.tensor_tensor(out=sc[:, 6:7], in0=sc[:, 2:3], in1=sc[:, 0:1], op=ALU.subtract), reads=[r_sc], writes=[r_sc])
        S.op("dve", lambda e: e.tensor_tensor(out=sc[:, 7:8], in0=sc[:, 1:2], in1=sc[:, 2:3], op=ALU.subtract), reads=[r_sc], writes=[r_sc])
        S.op("dve", lambda e: e.scalar_tensor_tensor(out=sc[:, 0:1], in0=sc[:, 6:7], scalar=sc[:, 4:5], in1=sc[:, 0:1], op0=ALU.mult, op1=ALU.add), reads=[r_sc], writes=[r_sc])
        S.op("dve", lambda e: e.scalar_tensor_tensor(out=sc[:, 1:2], in0=sc[:, 7:8], scalar=sc[:, 4:5], in1=sc[:, 2:3], op0=ALU.mult, op1=ALU.add), reads=[r_sc], writes=[r_sc])
        S.op("dve", lambda e: e.memset(sc[:, 3:4], 0.0), reads=[r_sc], writes=[r_sc])
    thrrep, r_thrrep = k.tile([128, 128], F32, "thrrep")
    S.op("dve", lambda e: e.tensor_copy(out=thrrep, in_=sc[:, 0:1].to_broadcast([128, 128])), reads=[r_sc], writes=[r_thrrep])
    pb, r_pb = _nb(k)
    S.op("pe", lambda e, pb=pb: e.matmul(pb[:, 0:16], lhsT=thrrep, rhs=sel8, start=True, stop=True), reads=[r_thrrep, r_sel8], writes=[r_pb])
    thr_row, r_thr = k.tile([128, 16], F32, "thr_row")
    S.op("act", lambda e, pb=pb: e.activation(out=thr_row, in_=pb[:, 0:16], func=AF.Copy), reads=[r_pb], writes=[r_thr])
    import os as _os
    if _os.environ.get("E_DBG"):
        k.dump("sc", sc, r_sc, [128, 16])
        k.dump("thr_row", thr_row, r_thr, [128, 16])
        k.dump("A8", A8, r_A8, [128, 512])
        S.barrier()
        A.release()
        A.release()
        return
    aff, r_aff = k.tile([128, 32, 16], F32, "aff")
    k.load(aff, r_aff, aff_s.rearrange("(n p) e -> p n e", p=128), r_aff_s)
    maskf, r_maskf = k.tile([128, 32, 16], F32, "maskf")
    maskb, r_maskb = k.tile([128, 32, 16], BF16, "maskb")
    posm, r_posm = k.tile([128, 32, 16], F32, "posm")
    parts, r_parts = k.tile([128, 32, 16, 5], BF16, "parts")
    tokp, r_tokp = k.tile([128, 32, 2], F32, "tokp")
    k.load(tokp, r_tokp, ins["moe_tok"])
    rem, r_rem = k.tile([128, 32, 16], F32, "rem")
    S.op("dve", lambda e: e.tensor_tensor(out=maskf, in0=aff, in1=thr_row.unsqueeze(1).to_broadcast([128, 32, 16]), op=ALU.is_ge),
         reads=[r_aff, r_thr], writes=[r_maskf])
    S.op("act", lambda e: e.activation(out=maskb, in_=maskf, func=AF.Copy), reads=[r_maskf], writes=[r_maskb])
    pp, r_pp = _nb(k)
    ppv = pp.rearrange("p (n e) -> p n e", e=16)

    def mmpos(e):
        for n in range(32):
            for m in range(n):
                e.matmul(ppv[:, n, :], lhsT=k.ones_b, rhs=maskb[:, m, :], start=(m == 0), stop=False)
            ins_ = e.matmul(ppv[:, n, :], lhsT=tris, rhs=maskb[:, n, :], start=(n == 0), stop=True)
        return ins_
    S.op("pe", mmpos, reads=[r_maskb, k.r_ones_b, r_tris], writes=[r_pp])
    S.op("dve", lambda e: e.scalar_tensor_tensor(out=posm, in0=ppv, scalar=1.0, in1=maskf, op0=ALU.add, op1=ALU.mult), reads=[r_pp, r_maskf], writes=[r_posm])
    S.op("dve", lambda e: e.tensor_scalar(out=posm, in0=posm, scalar1=-1.0, scalar2=None, op0=ALU.add), reads=[r_posm], writes=[r_posm])
    S.op("act", lambda e: e.activation(out=parts[:, :, :, 0], in_=aff, func=AF.Copy), reads=[r_aff], writes=[r_parts])
    S.op("dve", lambda e: e.tensor_tensor(out=rem, in0=aff, in1=parts[:, :, :, 0], op=ALU.subtract), reads=[r_aff, r_parts], writes=[r_rem])
    S.op("act", lambda e: e.activation(out=parts[:, :, :, 1], in_=rem, func=AF.Copy), reads=[r_rem], writes=[r_parts])
    S.op("dve", lambda e: e.tensor_tensor(out=rem, in0=rem, in1=parts[:, :, :, 1], op=ALU.subtract), reads=[r_rem, r_parts], writes=[r_rem])
    S.op("act", lambda e: e.activation(out=parts[:, :, :, 2], in_=rem, func=AF.Copy), reads=[r_rem], writes=[r_parts])
    S.op("dve", lambda e: e.tensor_copy(out=parts[:, :, :, 3:5], in_=tokp.unsqueeze(2).to_broadcast([128, 32, 16, 2])), reads=[r_tokp, r_parts], writes=[r_parts])
    pmTs = [k.tile([16, 512], F32, "pmT") for _ in range(2)]
    for g in range(8):
        pt, r_pt = _nb(k)

        def trp(e, pt=pt, g=g):
            for j in range(4):
                ins_ = e.transpose(out=pt[0:16, j * 128:(j + 1) * 128], in_=posm[:, g * 4 + j, :], identity=k.ident_f)
            return ins_
        S.op("pe", trp, reads=[r_posm, k.r_ident_f], writes=[r_pt])
        pmT, r_pmT = pmTs[g % 2]
        S.op("act", lambda e, pt=pt, pmT=pmT: e.activation(out=pmT, in_=pt[0:16, :], func=AF.Copy), reads=[r_pt], writes=[r_pmT])
        k.store(posmT_s[:, g * 512:(g + 1) * 512], r_posmT_s, pmT, r_pmT)
    if _os.environ.get("E_STOP") == "e1":
        S.barrier(); A.release(); A.release(); return
    Sel = [k.tile([128, 32, CAP], BF16, "Sel") for _ in range(2)]
    Sel_res = [(Res("sel0"), Res("sel1")) for _ in range(2)]
    gcs = [k.tile([128, 4], F32, "gcs") for _ in range(2)]
    idf = [k.tile([128, 4], F32, "idf") for _ in range(2)]
    idi = [k.tile([128, 4], I32, "idi") for _ in range(2)]
    for ex in range(NE):
        Sel_t, _ = Sel[ex % 2]
        r_Sel0, r_Sel1 = Sel_res[ex % 2]
        gcs_t, r_gcs = gcs[ex % 2]
        idf_t, r_idf = idf[ex % 2]
        idi_t, r_idi = idi[ex % 2]

        def bsel(e, Sel_t=Sel_t, ex=ex, par=0):
            for n in range(par, 32, 2):
                ins_ = e.tensor_scalar(out=Sel_t[:, n, :], in0=iota_c, scalar1=posm[:, n, ex:ex + 1], scalar2=None, op0=ALU.is_equal)
            return ins_
        S.op("dve", lambda e, f=bsel: f(e, par=0), reads=[r_cst, r_posm], writes=[r_Sel0])
        S.op("pool", lambda e, f=bsel: f(e, par=1), reads=[r_cst, r_posm], writes=[r_Sel1])
        pq, r_pq = _nb(k)

        def mmgate(e, pq=pq, Sel_t=Sel_t, ex=ex):
            for cc in range(4):
                for n in range(32):
                    ins_ = e.matmul(pq[:, cc * 8:cc * 8 + 5], lhsT=Sel_t[:, n, cc * 128:(cc + 1) * 128], rhs=parts[:, n, ex, :], start=(n == 0), stop=(n == 31))
            return ins_
        S.op("pe", mmgate, reads=[r_Sel0, r_Sel1, r_parts], writes=[r_pq])
        pq3 = pq[:, 0:32].rearrange("p (a b) -> p a b", b=8)
        S.op("dve", lambda e, pq3=pq3, gcs_t=gcs_t: e.tensor_reduce(out=gcs_t, in_=pq3[:, :, 0:3], axis=AX.X, op=ALU.add), reads=[r_pq], writes=[r_gcs])
        S.op("dve", lambda e, pq3=pq3, idf_t=idf_t: e.tensor_scalar(out=idf_t, in0=pq3[:, :, 3], scalar1=128.0, scalar2=None, op0=ALU.mult), reads=[r_pq], writes=[r_idf])
        S.op("dve", lambda e, pq3=pq3, idf_t=idf_t: e.tensor_tensor(out=idf_t, in0=idf_t, in1=pq3[:, :, 4], op=ALU.add), reads=[r_pq, r_idf], writes=[r_idf])
        S.op("dve", lambda e, idf_t=idf_t, idi_t=idi_t: e.tensor_copy(out=idi_t, in_=idf_t), reads=[r_idf], writes=[r_idi])
        k.store(gc_s[ex], r_gc_s, gcs_t, r_gcs)
        k.store(idx_s[ex], r_idx_s, idi_t, r_idi)
    S.barrier()
    A.release()
    if _os.environ.get("E_STOP") == "ea1":
        A.release(); return
    A.mark()
    U32 = mybir.dt.uint32
    ig = S.pool("ig", 4)
    wg = [k.tile([128, 8, FF], BF16, "wg") for _ in range(2)]
    wu = [k.tile([128, 8, FF], BF16, "wu") for _ in range(2)]
    wd = [k.tile([128, NFC, D], BF16, "wd") for _ in range(1)]
    xg = [k.tile([128, 4, D], BF16, "xg") for _ in range(2)]
    idx2 = [k.tile([128, 4], I32, "idx2") for _ in range(2)]
    gc2 = [k.tile([128, 4], F32, "gc2") for _ in range(2)]
    xeT_t, r_xeT = k.tile([128, 8, CAP], BF16, "xeT")
    hid, r_hid = k.tile([128, NFC, CAP], BF16, "hid")
    sg = [k.tile([128, CAP], F32, "sg") for _ in range(2)]
    yef = [k.tile([128, D], F32, "yef") for _ in range(4)]
    r_outacc = Res("outacc")
    h2_rows = h2_s

    def w_loads(ex):
        b = ex % 2
        srcg = ins["w_gate"][ex].rearrange("(kc p) f -> p kc f", p=128)
        srcu = ins["w_up"][ex].rearrange("(kc p) f -> p kc f", p=128)
        for j in range(4):
            k.wload(wg[b][0][:, 2 * j:2 * j + 2, :], wg[b][1], srcg[:, 2 * j:2 * j + 2, :])
        for j in range(4):
            k.wload(wu[b][0][:, 2 * j:2 * j + 2, :], wu[b][1], srcu[:, 2 * j:2 * j + 2, :])

    def wd_loads(ex):
        srcd = ins["w_down"][ex].rearrange("(fc p) d -> p fc d", p=128)
        for (a0, a1) in ((0, 3), (3, 6), (6, 9), (9, 11)):
            k.wload(wd[0][0][:, a0:a1, :], wd[0][1], srcd[:, a0:a1, :])

    def g_loads(ex):
        b = ex % 2
        k.load(idx2[b][0], idx2[b][1], idx_s[ex], r_idx_s)
        k.load(gc2[b][0], gc2[b][1], gc_s[ex], r_gc_s, q="act")
        for cc in range(4):
            S.dma("pool", ig, lambda e, b=b, cc=cc: e.indirect_dma_start(out=xg[b][0][:, cc, :], out_offset=None, in_=h2_rows,
                                                                       in_offset=bass.IndirectOffsetOnAxis(idx2[b][0].bitcast(U32)[:, cc:cc + 1], 0)),
                  reads=[idx2[b][1], r_h2_s], writes=[xg[b][1]])

    g_loads(0)
    w_loads(0)
    for ex in range(NE):
        b = ex % 2
        if ex + 1 < NE:
            g_loads(ex + 1)
            w_loads(ex + 1)
        wd_loads(ex)
        wg_t, r_wg = wg[b]
        wu_t, r_wu = wu[b]
        wd_t, r_wd = wd[0]
        xg_t, r_xg = xg[b]
        gc_t, r_gc = gc2[b]
        id_t, r_id = idx2[b]
        for kc in range(8):
            pt, r_pt = _nb(k, BF16)

            def trx(e, pt=pt, kc=kc, xg_t=xg_t):
                for cc in range(4):
                    ins_ = e.transpose(out=pt[:, cc * 128:(cc + 1) * 128], in_=xg_t[:, cc, kc * 128:(kc + 1) * 128], identity=k.ident_b)
                return ins_
            S.op("pe", trx, reads=[r_xg, k.r_ident_b], writes=[r_pt])
            if kc % 2 == 0:
                S.op("act", lambda e, pt=pt, kc=kc: e.activation(out=xeT_t[:, kc, :], in_=pt[:, 0:512], func=AF.Copy), reads=[r_pt], writes=[r_xeT])
            else:
                S.op("dve", lambda e, pt=pt, kc=kc: e.tensor_copy(out=xeT_t[:, kc, :], in_=pt[:, 0:512]), reads=[r_pt], writes=[r_xeT])
        for fc in range(NFC):
            pg, r_pg = _nb(k)
            pu, r_pu = _nb(k)

            def mmG(e, pg=pg, fc=fc, wg_t=wg_t):
                for kc in range(8):
                    ins_ = e.matmul(pg, lhsT=wg_t[:, kc, fc * 128:(fc + 1) * 128], rhs=xeT_t[:, kc, :], start=(kc == 0), stop=(kc == 7))
                return ins_
            S.op("pe", mmG, reads=[r_wg, r_xeT], writes=[r_pg])

            def mmU(e, pu=pu, fc=fc, wu_t=wu_t):
                for kc in range(8):
                    ins_ = e.matmul(pu, lhsT=wu_t[:, kc, fc * 128:(fc + 1) * 128], rhs=xeT_t[:, kc, :], start=(kc == 0), stop=(kc == 7))
                return ins_
            S.op("pe", mmU, reads=[r_wu, r_xeT], writes=[r_pu])
            sg_t, r_sg = sg[fc % 2]
            S.op("act", lambda e, pg=pg, sg_t=sg_t: e.activation(out=sg_t, in_=pg, func=AF.Silu), reads=[r_pg], writes=[r_sg])
            S.op("dve", lambda e, pu=pu, sg_t=sg_t, fc=fc: e.tensor_tensor(out=hid[:, fc, :], in0=pu, in1=sg_t, op=ALU.mult), reads=[r_pu, r_sg], writes=[r_hid])
        for cc in range(4):
            y_t, r_y = yef[cc]
            for hf in range(2):
                pd, r_pd = _nb(k)

                def mmD(e, pd=pd, cc=cc, hf=hf):
                    for fc in range(NFC):
                        ins_ = e.matmul(pd, lhsT=hid[:, fc, cc * 128:(cc + 1) * 128], rhs=wd_t[:, fc, hf * 512:(hf + 1) * 512], start=(fc == 0), stop=(fc == NFC - 1))
                    return ins_
                S.op("pe", mmD, reads=[r_hid, r_wd], writes=[r_pd])
                S.op("act", lambda e, pd=pd, cc=cc, hf=hf, y_t=y_t, gc_t=gc_t: e.activation(out=y_t[:, hf * 512:(hf + 1) * 512], in_=pd, func=AF.Copy, scale=gc_t[:, cc:cc + 1]),
                     reads=[r_pd, r_gc], writes=[r_y])
            S.op("pool", lambda e, y_t=y_t: e.tensor_tensor(out=y_t, in0=y_t, in1=k.gate2_row, op=ALU.mult), reads=[r_y, k.r_gate2], writes=[r_y])
            S.dma("pool", ig, lambda e, y_t=y_t, id_t=id_t, cc=cc: e.indirect_dma_start(out=k.out, out_offset=bass.IndirectOffsetOnAxis(id_t.bitcast(U32)[:, cc:cc + 1], 0),
                                                                                   in_=y_t, in_offset=None, compute_op=ALU.add),
                  reads=[r_y, r_id, k.out_res], writes=[r_outacc])
    S.barrier()
    A.release()
    A.release()

def _rope_tables():
    rows, gw = 64, 64
    row = np.repeat(np.arange(rows), gw).astype(np.float32)
    col = np.tile(np.arange(gw), rows).astype(np.float32)
    n_freq = 16
    inv_freq = (10000.0 ** (-np.arange(n_freq, dtype=np.float32) / n_freq)).astype(np.float32)
    ang_r = row[:, None] * inv_freq
    ang_c = col[:, None] * inv_freq
    ang = np.concatenate([ang_r, ang_r, ang_c, ang_c], axis=-1).astype(np.float32)
    cos = np.cos(ang).astype(np.float32)
    sin = np.sin(ang).astype(np.float32)
    sgn = np.concatenate([-np.ones(16), np.ones(16), -np.ones(16), np.ones(16)]).astype(np.float32)
    return cos, (sin * sgn).astype(np.float32)


def prep_shared(inp):
    f = np.float32
    sh = {}
    sh["ada_w"] = np.ascontiguousarray(inp["ada_w"][0])
    sh["ada_b_row"] = np.ascontiguousarray(inp["ada_b"][0][None, :])
    sh["ada_bT"] = np.ascontiguousarray(inp["ada_b"][0].reshape(48, 128).T)
    sh["g1T"] = np.ascontiguousarray(inp["norm1_g"][0].reshape(8, 128).T)
    sh["g2T"] = np.ascontiguousarray(inp["norm2_g"][0].reshape(8, 128).T)
    sh["g2_rep"] = np.ascontiguousarray(np.broadcast_to(inp["norm2_g"][0].reshape(1, 1024), (128, 1024)))
    sh["w_in"] = np.ascontiguousarray(inp["w_in"][0])
    sh["convT"] = np.ascontiguousarray(inp["conv_w"][0].T.reshape(24, 128, 5).transpose(1, 0, 2))
    sh["alog_rep"] = np.ascontiguousarray(np.broadcast_to(inp["a_log"][0].reshape(1, 16), (128, 16)))
    sh["dtb_rep"] = np.ascontiguousarray(np.broadcast_to(inp["dt_bias"][0].reshape(1, 16), (128, 16)))
    sh["dng_rep"] = np.ascontiguousarray(np.broadcast_to(inp["dn_norm_g"][0].reshape(1, 128), (128, 128)))
    sh["gqaT"] = np.ascontiguousarray(inp["q_a_norm_g"][0].reshape(3, 128).T)
    sh["w_uq"] = np.ascontiguousarray(inp["w_uq"][0])
    sh["gkvaT"] = np.ascontiguousarray(inp["kv_a_norm_g"][0].reshape(2, 128).T)
    sh["w_ukv"] = np.ascontiguousarray(inp["w_ukv"][0])
    sh["gq_rep"] = np.ascontiguousarray(np.broadcast_to(inp["q_norm_g"][0].reshape(1, 192), (128, 192)))
    sh["gk_rep"] = np.ascontiguousarray(np.broadcast_to(inp["k_norm_g"][0].reshape(1, 192), (128, 192)))
    sh["w_out_a"] = np.ascontiguousarray(inp["w_out_a"][0])
    sh["w_out_b"] = np.ascontiguousarray(inp["w_out_b"][0])
    sh["w_o"] = np.ascontiguousarray(inp["w_o"][0])
    sh["router_w"] = np.ascontiguousarray(inp["router_w"][0])
    sh["w_gate"] = np.ascontiguousarray(inp["w_gate"][0])
    sh["w_up"] = np.ascontiguousarray(inp["w_up"][0])
    sh["w_down"] = np.ascontiguousarray(inp["w_down"][0])
    cos, sinS = _rope_tables()
    sh["rope_cs"] = np.ascontiguousarray(np.concatenate([cos, sinS], axis=1))
    sh["ident"] = np.eye(128, dtype=f)
    consts = np.zeros((128, 1024), f)
    consts[:, 0:512] = np.arange(512, dtype=f)[None, :]
    consts[:, 512] = np.arange(128, dtype=f)
    sh["consts"] = consts
    pp_ = np.arange(128)
    sh["moe_blk"] = (pp_[:, None] // 8 == pp_[None, :] // 8).astype(f)
    s8 = np.zeros((128, 16), f)
    s8[np.arange(16) * 8, np.arange(16)] = 1.0
    sh["moe_sel8"] = s8
    tk = np.zeros((128, 32, 2), f)
    tk[:, :, 0] = np.arange(32, dtype=f)[None, :]
    tk[:, :, 1] = np.arange(128, dtype=f)[:, None]
    sh["moe_tok"] = tk
    sh["moe_tris"] = (pp_[:, None] < pp_[None, :]).astype(f)
    ii = np.arange(128)
    P, Fr = ii[:, None], ii[None, :]
    NEGV = -30000.0
    mk = np.zeros((128, 9, 128), f)
    mk[:, 0] = (P <= Fr)
    mk[:, 1] = (P >= Fr)
    mk[:, 2] = np.where(P > Fr, 0.0, NEGV)
    mk[:, 3] = np.where(P < Fr, 0.0, NEGV)
    mk[:, 4] = np.where(Fr >= P, 0.0, NEGV)
    mk[:, 5] = np.where(Fr <= P, 0.0, NEGV)
    mk[:, 6] = (P // 32 == Fr // 32)
    mk[:, 7] = (P // 64 == Fr // 64) & (P // 32 != Fr // 32)
    mk[:, 8] = (P // 64 != Fr // 64)
    sh["dn_masks"] = mk
    es = np.zeros((64, 2, 8, 128), f)
    for hh in range(8):
        es[hh, 0, hh, :] = 1.0
        es[32 + hh, 0, hh, :] = 1.0
    es[:, 1] = -es[:, 0]
    sh["dn_esel"] = es
    li = np.zeros((64, 128), f)
    li[32:40] = 1.0
    sh["dn_linit"] = li
    return sh


def prep_core(inp, sh, b):
    m = dict(sh)
    m["x"] = np.ascontiguousarray(inp["x"][b])
    m["ctx"] = np.ascontiguousarray(inp["ctx"][b])
    cc = np.stack([inp["c"][b], inp["c_ctx"]], axis=-1).astype(np.float32)
    m["c2"] = np.ascontiguousarray(cc.reshape(8, 128, 2).transpose(1, 0, 2))
    return m

PHASES = ["a0", "a1", "a2", "b", "c", "d", "e"]


def build(upto="e", dbg=(), dumps=()):
    k = K(dbg=dbg)
    declare_inputs(k)
    setup_consts(k)
    k.dump_list = []

    def dump(name, ap, res, shape):
        t = k.nc.dram_tensor("dbg_" + name, list(shape), ap.dtype, kind="ExternalOutput").ap()
        k.store(t, None, ap, res)
        k.dump_list.append("dbg_" + name)
    k.dump = dump
    k.dumps = set(dumps)
    fns = {"a0": phase_a0}
    for nm in ("a1", "a2", "b", "c", "d", "e"):
        f = globals().get("phase_" + nm)
        if f is not None:
            fns[nm] = f
    for ph in PHASES:
        if ph in fns:
            fns[ph](k)
        if ph == upto:
            break
    k.S.emit()
    return k


_CACHE = {}


def kernel(**inputs):
    inp = {kk: np.asarray(v) for kk, v in inputs.items()}
    sh = prep_shared(inp)
    in_maps = [prep_core(inp, sh, b) for b in range(8)]
    k = build()
    res = run_bass_kernel_spmd(k.nc, in_maps, core_ids=list(range(8)))
    out = np.stack([np.asarray(r["out"]) for r in res.results], axis=0).astype(np.float32)
    return out
```

```python
import numpy as np
import concourse.bass as bass
import concourse.mybir as mybir
from concourse.bass_utils import run_bass_kernel_spmd

F32 = mybir.dt.float32
BF16 = mybir.dt.bfloat16
F32R = mybir.dt.float32r
I32 = mybir.dt.int32
AF = mybir.ActivationFunctionType
ALU = mybir.AluOpType
AX = mybir.AxisListType

ENGS = ("pe", "act", "dve", "pool", "sp")
EPOCH = 12000


class Res:
    __slots__ = ("name", "w", "rs", "multi", "ws", "excl")

    def __init__(self, name="", multi=False, excl=False):
        self.excl = excl
        self.name = name
        self.w = None
        self.rs = []
        self.multi = multi
        self.ws = []


class Tok:
    __slots__ = ("key", "val", "eng")

    def __init__(self, key, val, eng):
        self.key = key
        self.val = val
        self.eng = eng


class DmaPool:
    def __init__(self, sched, name, n):
        self.s = sched
        self.name = name
        self.n = n
        self.i = 0
        self.count = [0] * n
        self.last = [None] * n

    def keys(self):
        return [("dma", self.name, j) for j in range(self.n)]


class Sched:
    def __init__(self, nc):
        self.nc = nc
        self.ops = {e: [] for e in ENGS}
        self.cnt = {e: 0 for e in ENGS}
        self.pools = []
        self.last_tok = {e: None for e in ENGS}
        self.n_instr = 0

    def pool(self, name, n):
        p = DmaPool(self, name, n)
        self.pools.append(p)
        return p

    def _deps(self, eng, reads, writes):
        deps = []
        for r in reads:
            if r.multi:
                deps.extend(r.ws)
            elif r.w is not None:
                deps.append(r.w)
            if r.excl:
                deps.extend(t for t in r.rs if t.eng != eng)
        for w in writes:
            if w.multi:
                pass
            elif w.w is not None and w.w.eng != eng:
                deps.append(w.w)
            for t in w.rs:
                if t.eng != eng:
                    deps.append(t)
        return deps

    def _mark_w(self, writes, tok):
        for w in writes:
            if w.multi:
                w.ws.append(tok)
            else:
                w.w = tok
                w.rs = []

    def op(self, eng, fn, reads=(), writes=(), extra=()):
        deps = self._deps(eng, reads, writes) + list(extra)
        c = self.cnt[eng]
        tok = Tok(("eng", eng, c // EPOCH), c % EPOCH + 1, eng)
        self.cnt[eng] = c + 1
        for r in reads:
            r.rs.append(tok)
        self._mark_w(writes, tok)
        self.ops[eng].append((deps, fn, tok, 1))
        self.last_tok[eng] = tok
        return tok

    def dma(self, eng, pool, fn, reads=(), writes=(), extra=()):
        deps = self._deps('__dma__', reads, writes) + list(extra)
        j = pool.i
        pool.i = (pool.i + 1) % pool.n
        if pool.last[j] is not None:
            deps.append(pool.last[j])
        pool.count[j] += 16
        tok = Tok(("dma", pool.name, j), pool.count[j], None)
        pool.last[j] = tok
        for r in reads:
            r.rs.append(tok)
        self._mark_w(writes, tok)
        self.ops[eng].append((deps, fn, tok, 16))
        return tok

    def barrier(self):
        toks = [t for t in self.last_tok.values() if t is not None]
        for p in self.pools:
            toks += [t for t in p.last if t is not None]
        for e in ENGS:
            self.ops[e].append((list(toks), None, None, 0))

    def emit(self, final_waits_eng="sp"):
        nc = self.nc
        sems = {}

        def sem_of(key):
            if key not in sems:
                sems[key] = nc.alloc_semaphore("s_" + "_".join(str(k) for k in key))
            return sems[key]

        for e in ENGS:
            for ep in range((self.cnt[e] + EPOCH - 1) // EPOCH):
                sem_of(("eng", e, ep))
        for p in self.pools:
            for k in p.keys():
                sem_of(k)

        toks = [t for t in self.last_tok.values() if t is not None]
        for p in self.pools:
            toks += [t for t in p.last if t is not None]
        self.ops[final_waits_eng].append((list(toks), None, None, 0))

        engobj = {"pe": "tensor", "act": "scalar", "dve": "vector", "pool": "gpsimd", "sp": "sync"}
        sched = self

        def run(ename):
            def body(eng):
                seen = {}
                for deps, fn, tok, inc in sched.ops[ename]:
                    need = {}
                    for t in deps:
                        if t.val > need.get(t.key, 0):
                            need[t.key] = t.val
                    for k, v in need.items():
                        if seen.get(k, 0) >= v:
                            continue
                        seen[k] = v
                        eng.wait_ge(sem_of(k), v)
                        sched.n_instr += 1
                    if fn is not None:
                        ins = fn(eng)
                        ins.then_inc(sem_of(tok.key), inc)
                        sched.n_instr += 1
            return body

        with nc.Block() as block:
            for ename in ENGS:
                getattr(block, engobj[ename])(run(ename))


class Arena:
    def __init__(self, nc, kbytes=198):
        self.nc = nc
        self.words = kbytes * 256
        self.t = nc.alloc_sbuf_tensor("arena", [128, self.words], F32)
        self.ap = self.t.ap()
        self.off = 0
        self.marks = []
        self.peak = 0

    def tile(self, shape, dtype, name=None):
        esz = {F32: 4, BF16: 2, I32: 4}[dtype]
        n = int(np.prod(shape[1:]))
        nw = (n * esz + 3) // 4
        off = (self.off + 15) // 16 * 16
        assert off + nw <= self.words, f"SBUF overflow {off}+{nw} > {self.words}"
        self.off = off + nw
        self.peak = max(self.peak, self.off)
        a = self.ap[0:shape[0], off:off + nw]
        if dtype != F32:
            a = a.bitcast(dtype)
        a = a[:, 0:n]
        if len(shape) == 3:
            a = a.rearrange("p (a b) -> p a b", a=shape[1])
        elif len(shape) == 4:
            a = a.rearrange("p (a b c) -> p a b c", a=shape[1], b=shape[2])
        return a

    def mark(self):
        self.marks.append(self.off)

    def release(self):
        self.off = self.marks.pop()

D = 1024
T = 4352
NT = 34
TX = 4096
NCTX = 256
H = 8
OFF_Z = 3072
OFF_GATE = 4832
D_IN = 6880
NMID = 1760
EPS = 1e-6
NEG = -30000.0


class K:
    def __init__(self, dbg=()):
        self.nc = bass.Bass("TRN2", target_bir_lowering=False)
        self.S = Sched(self.nc)
        self.A = Arena(self.nc)
        self.dbg = set(dbg)
        self.ins = {}
        self.scr = {}
        nc = self.nc
        self.ps = nc.alloc_psum_tensor("ps", [128, 8, 512], F32).ap()
        self.psr = [Res(f"ps{b}", excl=True) for b in range(8)]
        self.ld = self.S.pool("ld", 8)
        self.st = self.S.pool("st", 8)
        self.wl = self.S.pool("wl", 6)

    def inp(self, name, shape, dtype=F32):
        t = self.nc.dram_tensor(name, list(shape), dtype, kind="ExternalInput").ap()
        self.ins[name] = t
        return t

    def scratch(self, name, shape, dtype):
        kind = "ExternalOutput" if name in self.dbg else "Internal"
        t = self.nc.dram_tensor(name, list(shape), dtype, kind=kind).ap()
        self.scr[name] = (t, Res(name, multi=True))
        return t, self.scr[name][1]

    def bank(self, b, dtype=F32):
        a = self.ps[:, b, :]
        if dtype == BF16:
            a = a.bitcast(BF16)
        return a, self.psr[b]

    def tile(self, shape, dtype, name=None):
        return self.A.tile(shape, dtype, name), Res(name or "t")

    def load(self, dst, dres, src, sres=None, q="sp", pool=None):
        return self.S.dma(q, pool or self.ld, lambda e: e.dma_start(out=dst, in_=src),
                          reads=[sres] if sres is not None else [], writes=[dres])

    def store(self, dst, dres, src, sres, q="sp", pool=None):
        return self.S.dma(q, pool or self.st, lambda e: e.dma_start(out=dst, in_=src),
                          reads=[sres], writes=[dres] if dres is not None else [])

    def wload(self, dst, dres, src):
        return self.S.dma("pool", self.wl, lambda e: e.dma_start(out=dst, in_=src), writes=[dres])


def declare_inputs(k):
    i = k.inp
    i("x", [TX, D]); i("ctx", [NCTX, D]); i("c2", [128, 8, 2])
    i("ada_w", [D, 6 * D]); i("ada_b_row", [1, 6 * D]); i("ada_bT", [128, 48])
    i("g1T", [128, 8]); i("g2T", [128, 8]); i("g2_rep", [128, D])
    i("w_in", [D, D_IN]); i("convT", [128, 24, 5])
    i("alog_rep", [128, 16]); i("dtb_rep", [128, 16]); i("dng_rep", [128, 128])
    i("gqaT", [128, 3]); i("w_uq", [384, 1536]); i("gkvaT", [128, 2]); i("w_ukv", [256, 2048])
    i("gq_rep", [128, 192]); i("gk_rep", [128, 192])
    i("w_out_a", [D, D]); i("w_out_b", [D, D]); i("w_o", [D, D])
    i("router_w", [D, 16]); i("w_gate", [16, D, 1408]); i("w_up", [16, D, 1408]); i("w_down", [16, 1408, D])
    i("rope_cs", [TX, 128])
    i("ident", [128, 128]); i("consts", [128, 1024])
    i("moe_tok", [128, 32, 2]); i("moe_blk", [128, 128]); i("moe_sel8", [128, 16]); i("moe_tris", [128, 128])
    i("dn_masks", [128, 9, 128]); i("dn_esel", [64, 2, 8, 128]); i("dn_linit", [64, 128])
    k.out = k.nc.dram_tensor("out", [TX, D], F32, kind="ExternalOutput").ap()
    k.out_res = Res("out", multi=True)


def setup_consts(k):
    S = k.S
    k.ident_f, k.r_ident_f = k.tile([128, 128], F32, "identf")
    k.ident_b, k.r_ident_b = k.tile([128, 128], BF16, "identb")
    k.load(k.ident_f, k.r_ident_f, k.ins["ident"])
    S.op("dve", lambda e: e.tensor_copy(out=k.ident_b, in_=k.ident_f), reads=[k.r_ident_f], writes=[k.r_ident_b])
    k.ones_f, k.r_ones_f = k.tile([128, 128], F32, "onesf")
    k.ones_b, k.r_ones_b = k.tile([128, 128], BF16, "onesb")
    S.op("pool", lambda e: e.memset(k.ones_f, 1.0), writes=[k.r_ones_f])
    S.op("pool", lambda e: e.memset(k.ones_b, 1.0), writes=[k.r_ones_b])


def phase_a0(k):
    S, A, nc = k.S, k.A, k.nc
    ins = k.ins
    k.modT, k.r_modT = k.tile([128, 48, 2], F32, "modT")
    k.s1, k.r_s1 = k.tile([128, 8, 2], F32, "s1")
    k.s2, k.r_s2 = k.tile([128, 8], F32, "s2")
    k.gate1_row, k.r_gate1 = k.tile([128, D], F32, "gate1row")
    k.gate2_row, k.r_gate2 = k.tile([128, D], F32, "gate2row")
    k.off_after_gate2 = A.off
    k.shift2_row, k.r_shift2 = k.tile([128, D], F32, "shift2row")
    k.s2_row, k.r_s2row = k.tile([128, D], F32, "s2row")
    A.mark()
    c2, r_c2 = k.tile([128, 8, 2], F32, "c2")
    sc, r_sc = k.tile([128, 8, 2], F32, "sc")
    screp, r_screp = k.tile([128, 8, 128], F32, "screp")
    abT, r_abT = k.tile([128, 48], F32, "abT")
    abrow, r_abrow = k.tile([1, 6 * D], F32, "abrow")
    g1T, r_g1T = k.tile([128, 8], F32, "g1T")
    g2T, r_g2T = k.tile([128, 8], F32, "g2T")
    wbuf = [k.tile([128, 8, D], F32, f"adaw{j}") for j in range(2)]
    k.load(c2, r_c2, ins["c2"])
    k.load(abT, r_abT, ins["ada_bT"])
    k.load(abrow, r_abrow, ins["ada_b_row"])
    k.load(g1T, r_g1T, ins["g1T"])
    k.load(g2T, r_g2T, ins["g2T"])
    S.op("act", lambda e: e.activation(out=sc, in_=c2, func=AF.Silu), reads=[r_c2], writes=[r_sc])
    S.op("dve", lambda e: e.tensor_copy(out=screp, in_=sc[:, :, 0:1].to_broadcast([128, 8, 128])),
         reads=[r_sc], writes=[r_screp])
    pm, r_pm = k.bank(0)
    aw = ins["ada_w"].rearrange("(kc p) n -> p kc n", p=128)
    for sec in range(6):
        wt, r_wt = wbuf[sec % 2]
        q = "sp" if sec % 2 == 0 else "act"
        S.dma(q, k.ld, lambda e, wt=wt, sec=sec: e.dma_start(out=wt, in_=aw[:, :, sec * D:(sec + 1) * D]), writes=[r_wt])

        def mm(e, wt=wt, sec=sec):
            for fc in range(8):
                for kc in range(8):
                    ins_ = e.matmul(pm[:, (sec * 8 + fc) * 2:(sec * 8 + fc) * 2 + 2], lhsT=wt[:, kc, fc * 128:(fc + 1) * 128],
                                    rhs=sc[:, kc, :], start=(kc == 0), stop=(kc == 7))
            return ins_
        S.op("pe", mm, reads=[r_wt, r_sc], writes=[r_pm])
        if sec in (2, 3, 4, 5):
            dst, r_dst = {2: (k.gate1_row, k.r_gate1), 5: (k.gate2_row, k.r_gate2), 3: (k.shift2_row, k.r_shift2), 4: (k.s2_row, k.r_s2row)}[sec]
            for hf in range(2):
                pb, r_pb = k.bank(1 + hf)

                def mmr(e, wt=wt, sec=sec, hf=hf, pb=pb):
                    for kc in range(8):
                        e.matmul(pb, lhsT=screp[:, kc, :], rhs=wt[:, kc, hf * 512:(hf + 1) * 512], start=(kc == 0), stop=False)
                    return e.matmul(pb, lhsT=k.ones_f[0:1, :], rhs=abrow[0:1, sec * D + hf * 512: sec * D + (hf + 1) * 512],
                                    start=False, stop=True)
                S.op("pe", mmr, reads=[r_wt, r_screp, k.r_ones_f, r_abrow], writes=[r_pb])
                S.op("act", lambda e, dst=dst, hf=hf, pb=pb: e.activation(out=dst[:, hf * 512:(hf + 1) * 512], in_=pb, func=AF.Copy),
                     reads=[r_pb], writes=[r_dst])
    S.op("dve", lambda e: e.tensor_tensor(out=k.modT, in0=pm[:, 0:96].rearrange("p (a b) -> p a b", b=2),
                                          in1=abT.unsqueeze(2).to_broadcast([128, 48, 2]), op=ALU.add),
         reads=[r_pm, r_abT], writes=[k.r_modT])
    S.op("dve", lambda e: e.scalar_tensor_tensor(out=k.s1, in0=k.modT[:, 8:16, :], scalar=1.0,
                                                 in1=g1T.unsqueeze(2).to_broadcast([128, 8, 2]), op0=ALU.add, op1=ALU.mult),
         reads=[k.r_modT, r_g1T], writes=[k.r_s1])
    S.op("dve", lambda e: e.scalar_tensor_tensor(out=k.s2, in0=k.modT[:, 32:40, 0], scalar=1.0,
                                                 in1=g2T, op0=ALU.add, op1=ALU.mult),
         reads=[k.r_modT, r_g2T], writes=[k.r_s2])
    g2rep, r_g2rep = k.tile([128, D], F32, "g2rep")
    k.load(g2rep, r_g2rep, ins["g2_rep"])
    S.op("dve", lambda e: e.scalar_tensor_tensor(out=k.s2_row, in0=k.s2_row, scalar=1.0, in1=g2rep, op0=ALU.add, op1=ALU.mult),
         reads=[k.r_s2row, r_g2rep], writes=[k.r_s2row])
    S.barrier()
    A.release()

def _nb(k, dtype=F32):
    b = getattr(k, "_bank_i", 0)
    k._bank_i = (b + 1) % 8
    return k.bank(b, dtype)


def _rstd(k, ss, r_ss, n, out, r_out):
    S = k.S
    S.op("act", lambda e: e.activation(out=out, in_=ss, func=AF.Sqrt, scale=1.0 / n, bias=EPS), reads=[r_ss], writes=[r_out])
    S.op("dve", lambda e: e.reciprocal(out=out, in_=out), reads=[r_out], writes=[r_out])


def _rope(k, pe, r_pe, cos_t, sin_t, r_tab, t1, t2, r_t1, r_t2):
    S = k.S
    cb = cos_t.unsqueeze(1).to_broadcast([128, 8, 64])
    S.op("pool", lambda e: e.tensor_tensor(out=t1, in0=pe, in1=cb, op=ALU.mult), reads=[r_pe, r_tab], writes=[r_t1])
    pe5 = pe.rearrange("p h (a s c) -> p h a s c", a=2, s=2)
    t25 = t2.rearrange("p h (a s c) -> p h a s c", a=2, s=2)
    sn5 = sin_t.rearrange("p (a s c) -> p a s c", a=2, s=2)

    def f(e):
        for s in range(2):
            ins_ = e.tensor_tensor(out=t25[:, :, :, s, :], in0=pe5[:, :, :, 1 - s, :],
                                   in1=sn5[:, :, s, :].unsqueeze(1).to_broadcast([128, 8, 2, 16]), op=ALU.mult)
        return ins_
    S.op("dve", f, reads=[r_pe, r_tab], writes=[r_t2])
    S.op("pool", lambda e: e.tensor_tensor(out=pe, in0=t1, in1=t2, op=ALU.add), reads=[r_t1, r_t2], writes=[r_pe])


def phase_a1(k):
    S, A, nc, ins = k.S, k.A, k.nc, k.ins
    zs_s, r_zs_s = k.scratch("zs_s", [TX, D], BF16)
    gb_s, r_gb_s = k.scratch("gb_s", [T, 48], F32)
    qmT_s, r_qmT_s = k.scratch("qmT_s", [H, 192, TX], BF16)
    kmT_s, r_kmT_s = k.scratch("kmT_s", [H, 192, T], BF16)
    vm_s, r_vm_s = k.scratch("vm_s", [T, D], BF16)
    A.mark()
    k.hT, _ = k.tile([128, 8, T], BF16, "hT")
    k.r_hT = [Res(f"hT{i}") for i in range(NT)]
    A.mark()
    wmid, r_wmid = k.tile([128, 8, NMID], BF16, "wmid")
    wuq, r_wuq = k.tile([128, 3, 1536], BF16, "wuq")
    wukv, r_wukv = k.tile([128, 2, 2048], BF16, "wukv")
    dtb, r_dtb = k.tile([128, 16], F32, "dtb")
    negA, r_negA = k.tile([128, 16], F32, "negA")
    gq, r_gq = k.tile([128, 192], F32, "gq")
    gk, r_gk = k.tile([128, 192], F32, "gk")
    gqaT, r_gqaT = k.tile([128, 3], F32, "gqaT")
    gkvaT, r_gkvaT = k.tile([128, 2], F32, "gkvaT")
    win = ins["w_in"].rearrange("(kc p) n -> p kc n", p=128)
    for j in range(4):
        k.wload(wmid[:, 2 * j:2 * j + 2, :], r_wmid, win[:, 2 * j:2 * j + 2, OFF_Z:OFF_GATE])
    k.wload(wuq, r_wuq, ins["w_uq"].rearrange("(kc p) n -> p kc n", p=128))
    k.wload(wukv, r_wukv, ins["w_ukv"].rearrange("(kc p) n -> p kc n", p=128))
    k.load(dtb, r_dtb, ins["dtb_rep"])
    k.load(negA, r_negA, ins["alog_rep"])
    k.load(gq, r_gq, ins["gq_rep"])
    k.load(gk, r_gk, ins["gk_rep"])
    k.load(gqaT, r_gqaT, ins["gqaT"])
    k.load(gkvaT, r_gkvaT, ins["gkvaT"])
    S.op("act", lambda e: e.activation(out=negA, in_=negA, func=AF.Exp), reads=[r_negA], writes=[r_negA])
    S.op("dve", lambda e: e.tensor_scalar(out=negA, in0=negA, scalar1=-1.0, scalar2=None, op0=ALU.mult), reads=[r_negA], writes=[r_negA])
    S.op("dve", lambda e: e.tensor_scalar(out=gq, in0=gq, scalar1=192.0 ** -0.5, scalar2=None, op0=ALU.mult), reads=[r_gq], writes=[r_gq])

    NB = 2
    xt = [k.tile([128, D], F32, "xt") for _ in range(NB)]
    junk, r_junk = k.tile([128, D], BF16, "junk")
    ss = [k.tile([128, 8], F32, "ss") for _ in range(NB)]
    xn = [k.tile([128, D], BF16, "xn") for _ in range(NB)]
    zs = [k.tile([128, D], BF16, "zs") for _ in range(NB)]
    gb = [k.tile([128, 48], F32, "gb") for _ in range(NB)]
    t16, r_t16 = k.tile([128, 16], F32, "t16")
    cqn, r_cqn = k.tile([128, 384], BF16, "cqn")
    ckvn, r_ckvn = k.tile([128, 256], BF16, "ckvn")
    cqnT, r_cqnT = k.tile([128, 3, 128], BF16, "cqnT")
    ckvnT, r_ckvnT = k.tile([128, 2, 128], BF16, "ckvnT")
    kr, r_kr = k.tile([128, 64], F32, "kr")
    qsb, r_qsb = k.tile([128, 8, 192], F32, "qsb")
    sq, r_sq = k.tile([128, 8, 192], F32, "sq")
    r8, r_r8 = k.tile([128, 8], F32, "r8")
    kvsb, r_kvsb = k.tile([128, 8, 2, 128], F32, "kvsb")
    tmpf, r_tmpf = kvsb.rearrange("p a b c -> p (a b c)")[:, 0:1024].rearrange("p (a b) -> p a b", a=8), r_kvsb
    kf, r_kf = sq, r_sq
    rk8, r_rk8 = k.tile([128, 8], F32, "rk8")
    sskr, r_sskr = k.tile([128, 1], F32, "sskr")
    rt1, r_rt1 = k.tile([128, 8, 64], F32, "rt1")
    rt2, r_rt2 = k.tile([128, 8, 64], F32, "rt2")
    cs_t = [k.tile([128, 128], F32, "cs") for _ in range(NB)]
    qf, r_qf = k.tile([128, 8, 192], BF16, "qf")
    kfb, r_kfb = k.tile([128, 8, 192], BF16, "kfb")
    vb = [k.tile([128, 8, 128], BF16, "vb") for _ in range(1)]
    qTn = [k.tile([128, 8, 128], BF16, "qTn") for _ in range(1)]
    qTr = [k.tile([64, 8, 128], BF16, "qTr") for _ in range(1)]
    kTn = [k.tile([128, 8, 128], BF16, "kTn") for _ in range(1)]
    kTr = [k.tile([64, 8, 128], BF16, "kTr") for _ in range(1)]

    def src_rows(i):
        return ins["ctx"][i * 128:(i + 1) * 128, :] if i < 2 else ins["x"][(i - 2) * 128:(i - 1) * 128, :]

    def prefetch(i):
        b = i % NB
        k.load(xt[b][0], xt[b][1], src_rows(i))
        if i >= 2:
            xi = i - 2
            S.dma("act", k.ld, lambda e: e.dma_start(out=cs_t[b][0], in_=ins["rope_cs"][xi * 128:(xi + 1) * 128, :]), writes=[cs_t[b][1]])

    def transposes(src, r_src, n, width=128, rows=128):
        pb, r_pb = _nb(k, BF16)
        pv = pb.rearrange("p (a b) -> p a b", b=128)[0:width, 0:n, :]

        def f(e):
            for j in range(n):
                ins_ = e.transpose(out=pv[:, j, :], in_=src(j), identity=k.ident_b)
            return ins_
        S.op("pe", f, reads=[r_src, k.r_ident_b], writes=[r_pb])
        return pv, r_pb

    prefetch(0)
    for i in range(NT):
        b = i % NB
        is_x = i >= 2
        xi = i - 2
        col = 0 if is_x else 1
        tok = slice(i * 128, (i + 1) * 128)
        if i + 1 < NT:
            prefetch(i + 1)
        x_t, r_x = xt[b]
        ss_t, r_ss = ss[b]
        xn_t, r_xn = xn[b]
        S.op("act", lambda e, x_t=x_t, ss_t=ss_t: e.activation(out=junk, in_=x_t, func=AF.Square, accum_out=ss_t[:, 0:1]),
             reads=[r_x], writes=[r_junk, r_ss])
        _rstd(k, ss_t[:, 0:1], r_ss, D, ss_t[:, 1:2], r_ss)
        S.op("act", lambda e, x_t=x_t, ss_t=ss_t, xn_t=xn_t: e.activation(out=xn_t, in_=x_t, func=AF.Copy, scale=ss_t[:, 1:2]),
             reads=[r_x, r_ss], writes=[r_xn])
        pv, r_pv = transposes(lambda j, xn_t=xn_t: xn_t[:, j * 128:(j + 1) * 128], r_xn, 8)
        S.op("dve", lambda e, pv=pv, col=col: e.tensor_tensor(out=tmpf, in0=pv, in1=k.s1[:, :, col:col + 1].to_broadcast([128, 8, 128]), op=ALU.mult),
             reads=[r_pv, k.r_s1], writes=[r_tmpf])
        S.op("pool", lambda e, col=col, tok=tok: e.tensor_tensor(out=k.hT[:, :, tok], in0=tmpf,
                                                                in1=k.modT[:, 0:8, col:col + 1].to_broadcast([128, 8, 128]), op=ALU.add),
             reads=[r_tmpf, k.r_modT], writes=[k.r_hT[i]])
        groups = [(0, 512), (512, 1024), (1024, 1440), (1440, 1760)]
        banks = []
        for g, (c0, c1) in enumerate(groups):
            if g < 2 and not is_x:
                banks.append(None)
                continue
            pb, r_pb = _nb(k)

            def mm(e, pb=pb, c0=c0, c1=c1, tok=tok):
                for kc in range(8):
                    ins_ = e.matmul(pb[:, 0:c1 - c0], lhsT=k.hT[:, kc, tok], rhs=wmid[:, kc, c0:c1], start=(kc == 0), stop=(kc == 7))
                return ins_
            S.op("pe", mm, reads=[k.r_hT[i], r_wmid], writes=[r_pb])
            banks.append((pb, r_pb))
        if is_x:
            z_t, r_z = zs[b]
            for g in range(2):
                pb, r_pb = banks[g]
                S.op("act", lambda e, pb=pb, g=g, z_t=z_t: e.activation(out=z_t[:, g * 512:(g + 1) * 512], in_=pb, func=AF.Silu),
                     reads=[r_pb], writes=[r_z])
            k.store(zs_s[xi * 128:(xi + 1) * 128, :], r_zs_s, z_t, r_z)
        p2, r_p2 = banks[2]
        p3, r_p3 = banks[3]
        gb_t, r_gb = gb[b]
        S.op("dve", lambda e, p2=p2: e.tensor_tensor(out=t16, in0=p2[:, 0:16], in1=dtb, op=ALU.add), reads=[r_p2, r_dtb], writes=[r_t16])
        S.op("act", lambda e: e.activation(out=t16, in_=t16, func=AF.Exp), reads=[r_t16], writes=[r_t16])
        S.op("act", lambda e: e.activation(out=t16, in_=t16, func=AF.Ln, bias=1.0), reads=[r_t16], writes=[r_t16])
        S.op("dve", lambda e, gb_t=gb_t: e.tensor_tensor(out=gb_t[:, 0:16], in0=t16, in1=negA, op=ALU.mult), reads=[r_t16, r_negA], writes=[r_gb])
        S.op("act", lambda e, gb_t=gb_t, p2=p2: e.activation(out=gb_t[:, 16:32], in_=p2[:, 16:32], func=AF.Sigmoid), reads=[r_p2], writes=[r_gb])
        S.op("act", lambda e, gb_t=gb_t: e.activation(out=gb_t[:, 32:48], in_=gb_t[:, 16:32], func=AF.Ln), reads=[r_gb], writes=[r_gb])
        k.store(gb_s[tok, :], r_gb_s, gb_t, r_gb)
        if is_x:
            S.op("act", lambda e, p2=p2, ss_t=ss_t: e.activation(out=junk[:, 0:384], in_=p2[:, 32:416], func=AF.Square, accum_out=ss_t[:, 4:5]),
                 reads=[r_p2], writes=[r_junk, r_ss])
            _rstd(k, ss_t[:, 4:5], r_ss, 384, ss_t[:, 5:6], r_ss)
            S.op("act", lambda e, p2=p2, ss_t=ss_t: e.activation(out=cqn, in_=p2[:, 32:416], func=AF.Copy, scale=ss_t[:, 5:6]),
                 reads=[r_p2, r_ss], writes=[r_cqn])

        S.op("act", lambda e, p3=p3, ss_t=ss_t: e.activation(out=junk[:, 0:256], in_=p3[:, 0:256], func=AF.Square, accum_out=ss_t[:, 2:3]),
             reads=[r_p3], writes=[r_junk, r_ss])
        _rstd(k, ss_t[:, 2:3], r_ss, 256, ss_t[:, 3:4], r_ss)
        S.op("act", lambda e, p3=p3, ss_t=ss_t: e.activation(out=ckvn, in_=p3[:, 0:256], func=AF.Copy, scale=ss_t[:, 3:4]),
             reads=[r_p3, r_ss], writes=[r_ckvn])
        S.op("dve", lambda e, p3=p3: e.tensor_copy(out=kr, in_=p3[:, 256:320]), reads=[r_p3], writes=[r_kr])
        pv, r_pv = transposes(lambda j: ckvn[:, j * 128:(j + 1) * 128], r_ckvn, 2)
        S.op("dve", lambda e, pv=pv: e.tensor_tensor(out=ckvnT, in0=pv, in1=gkvaT.unsqueeze(2).to_broadcast([128, 2, 128]), op=ALU.mult),
             reads=[r_pv, r_gkvaT], writes=[r_ckvnT])
        for b4 in range(4):
            pb, r_pb = _nb(k)

            def mmkv(e, pb=pb, b4=b4):
                for kc in range(2):
                    ins_ = e.matmul(pb, lhsT=ckvnT[:, kc, :], rhs=wukv[:, kc, b4 * 512:(b4 + 1) * 512], start=(kc == 0), stop=(kc == 1))
                return ins_
            S.op("pe", mmkv, reads=[r_ckvnT, r_wukv], writes=[r_pb])
            eng = "act" if b4 % 2 == 0 else "dve"
            dst = kvsb[:, 2 * b4:2 * b4 + 2, :, :].rearrange("p a b c -> p (a b c)")
            if eng == "act":
                S.op("act", lambda e, dst=dst, pb=pb: e.activation(out=dst, in_=pb, func=AF.Copy), reads=[r_pb], writes=[r_kvsb])
            else:
                S.op("dve", lambda e, dst=dst, pb=pb: e.tensor_copy(out=dst, in_=pb), reads=[r_pb], writes=[r_kvsb])
        vb_t, r_vb = vb[0]
        S.op("pool", lambda e, vb_t=vb_t: e.tensor_copy(out=vb_t, in_=kvsb[:, :, 1, :]), reads=[r_kvsb], writes=[r_vb])
        k.store(vm_s[tok, :], r_vm_s, vb_t.rearrange("p h d -> p (h d)"), r_vb)
        S.op("pool", lambda e: e.tensor_tensor(out=sq[:, :, 0:128], in0=kvsb[:, :, 0, :], in1=kvsb[:, :, 0, :], op=ALU.mult),
             reads=[r_kvsb], writes=[r_sq])
        S.op("dve", lambda e: e.tensor_reduce(out=rk8, in_=sq[:, :, 0:128], axis=AX.X, op=ALU.add), reads=[r_sq], writes=[r_rk8])
        S.op("act", lambda e: e.activation(out=junk[:, 0:64], in_=kr, func=AF.Square, accum_out=sskr), reads=[r_kr], writes=[r_junk, r_sskr])
        S.op("dve", lambda e: e.tensor_scalar(out=rk8, in0=rk8, scalar1=sskr, scalar2=None, op0=ALU.add), reads=[r_rk8, r_sskr], writes=[r_rk8])
        _rstd(k, rk8, r_rk8, 192, rk8, r_rk8)
        S.op("dve", lambda e: e.tensor_tensor(out=kf[:, :, 0:128], in0=kvsb[:, :, 0, :], in1=rk8.unsqueeze(2).to_broadcast([128, 8, 128]), op=ALU.mult),
             reads=[r_kvsb, r_rk8], writes=[r_kf])
        S.op("dve", lambda e: e.tensor_tensor(out=kf[:, :, 128:192], in0=kr.unsqueeze(1).to_broadcast([128, 8, 64]),
                                              in1=rk8.unsqueeze(2).to_broadcast([128, 8, 64]), op=ALU.mult),
             reads=[r_kr, r_rk8, r_kf], writes=[r_kf])
        S.op("pool", lambda e: e.tensor_tensor(out=kf, in0=kf, in1=gk.unsqueeze(1).to_broadcast([128, 8, 192]), op=ALU.mult),
             reads=[r_kf, r_gk], writes=[r_kf])
        if is_x:
            _rope(k, kf[:, :, 128:192], r_kf, cs_t[b][0][:, 0:64], cs_t[b][0][:, 64:128], cs_t[b][1], rt1, rt2, r_rt1, r_rt2)
        S.op("act", lambda e: e.activation(out=kfb, in_=kf, func=AF.Copy), reads=[r_kf], writes=[r_kfb])
        kTn_t, r_kTn = kTn[0]
        kTr_t, r_kTr = kTr[0]
        pv, r_pv = transposes(lambda j: kfb[:, j, 0:128], r_kfb, 8)
        S.op("dve", lambda e, pv=pv, kTn_t=kTn_t: e.tensor_copy(out=kTn_t, in_=pv), reads=[r_pv], writes=[r_kTn])
        pv, r_pv = transposes(lambda j: kfb[:, j, 128:192], r_kfb, 8, width=64)
        S.op("act", lambda e, pv=pv, kTr_t=kTr_t: e.activation(out=kTr_t, in_=pv, func=AF.Copy), reads=[r_pv], writes=[r_kTr])
        k.store(kmT_s[:, 0:128, tok].rearrange("h d t -> d h t"), r_kmT_s, kTn_t, r_kTn)
        k.store(kmT_s[:, 128:192, tok].rearrange("h d t -> d h t"), r_kmT_s, kTr_t, r_kTr)
        if not is_x:
            continue
        xtok = slice(xi * 128, (xi + 1) * 128)
        pv, r_pv = transposes(lambda j: cqn[:, j * 128:(j + 1) * 128], r_cqn, 3)
        S.op("dve", lambda e, pv=pv: e.tensor_tensor(out=cqnT, in0=pv, in1=gqaT.unsqueeze(2).to_broadcast([128, 3, 128]), op=ALU.mult),
             reads=[r_pv, r_gqaT], writes=[r_cqnT])
        qflat = qsb.rearrange("p h d -> p (h d)")
        for b3 in range(3):
            pb, r_pb = _nb(k)

            def mmq(e, pb=pb, b3=b3):
                for kc in range(3):
                    ins_ = e.matmul(pb, lhsT=cqnT[:, kc, :], rhs=wuq[:, kc, b3 * 512:(b3 + 1) * 512], start=(kc == 0), stop=(kc == 2))
                return ins_
            S.op("pe", mmq, reads=[r_cqnT, r_wuq], writes=[r_pb])
            if b3 % 2 == 0:
                S.op("act", lambda e, pb=pb, b3=b3: e.activation(out=qflat[:, b3 * 512:(b3 + 1) * 512], in_=pb, func=AF.Copy), reads=[r_pb], writes=[r_qsb])
            else:
                S.op("dve", lambda e, pb=pb, b3=b3: e.tensor_copy(out=qflat[:, b3 * 512:(b3 + 1) * 512], in_=pb), reads=[r_pb], writes=[r_qsb])
        S.op("pool", lambda e: e.tensor_tensor(out=sq, in0=qsb, in1=qsb, op=ALU.mult), reads=[r_qsb], writes=[r_sq])
        S.op("dve", lambda e: e.tensor_reduce(out=r8, in_=sq, axis=AX.X, op=ALU.add), reads=[r_sq], writes=[r_r8])
        _rstd(k, r8, r_r8, 192, r8, r_r8)
        S.op("dve", lambda e: e.tensor_tensor(out=qsb, in0=qsb, in1=r8.unsqueeze(2).to_broadcast([128, 8, 192]), op=ALU.mult),
             reads=[r_qsb, r_r8], writes=[r_qsb])
        S.op("pool", lambda e: e.tensor_tensor(out=qsb, in0=qsb, in1=gq.unsqueeze(1).to_broadcast([128, 8, 192]), op=ALU.mult),
             reads=[r_qsb, r_gq], writes=[r_qsb])
        _rope(k, qsb[:, :, 128:192], r_qsb, cs_t[b][0][:, 0:64], cs_t[b][0][:, 64:128], cs_t[b][1], rt1, rt2, r_rt1, r_rt2)
        S.op("act", lambda e: e.activation(out=qf, in_=qsb, func=AF.Copy), reads=[r_qsb], writes=[r_qf])
        qTn_t, r_qTn = qTn[0]
        qTr_t, r_qTr = qTr[0]
        pv, r_pv = transposes(lambda j: qf[:, j, 0:128], r_qf, 8)
        S.op("dve", lambda e, pv=pv, qTn_t=qTn_t: e.tensor_copy(out=qTn_t, in_=pv), reads=[r_pv], writes=[r_qTn])
        pv, r_pv = transposes(lambda j: qf[:, j, 128:192], r_qf, 8, width=64)
        S.op("act", lambda e, pv=pv, qTr_t=qTr_t: e.activation(out=qTr_t, in_=pv, func=AF.Copy), reads=[r_pv], writes=[r_qTr])
        k.store(qmT_s[:, 0:128, xtok].rearrange("h d t -> d h t"), r_qmT_s, qTn_t, r_qTn)
        k.store(qmT_s[:, 128:192, xtok].rearrange("h d t -> d h t"), r_qmT_s, qTr_t, r_qTr)
    S.barrier()
    A.release()

RW = 4364
NU = 4356


def phase_a2(k):
    S, A, nc, ins = k.S, k.A, k.nc, k.ins
    qdT_s, r_qdT_s = k.scratch("qdT_s", [H, 128, TX], BF16)
    kdT_s, r_kdT_s = k.scratch("kdT_s", [H, 128, T], BF16)
    kd_s, r_kd_s = k.scratch("kd_s", [T, H, 128], BF16)
    vd_s, r_vd_s = k.scratch("vd_s", [T, H, 128], BF16)
    sgT_s, r_sgT_s = k.scratch("sgT_s", [16, 128, TX], BF16)
    A.mark()
    convw, r_convw = k.tile([128, 24, 5], F32, "convw")
    k.load(convw, r_convw, ins["convT"])
    wc = [k.tile([128, 8, 512], BF16, "wc") for _ in range(2)]
    R = [k.tile([128, RW], F32, "R") for _ in range(2)]
    acc, r_acc = k.tile([128, NU], F32, "acc")
    sq, r_sq = k.tile([128, NU], BF16, "sq")
    Yb = [k.tile([128, NU], BF16, "Yb") for _ in range(2)]
    rn, r_rn = k.tile([128, 512], F32, "rn")
    tm = [k.tile([128, NT, 128], BF16, "tm") for _ in range(1)]
    sg = [k.tile([128, 512], BF16, "sg") for _ in range(2)]
    for j in range(2):
        S.op("pool", lambda e, j=j: e.memset(R[j][0], 0.0), writes=[R[j][1]])
    win = ins["w_in"].rearrange("(kc p) n -> p kc n", p=128)
    blocks = [(c * 512, "qkv", c * 4) for c in range(6)] + [(OFF_GATE + c * 512, "gate", c * 4) for c in range(4)]
    tgroups = [(0, 256, 2)] + [(256 + g * 512, 512, 262 + g * 512) for g in range(8)]
    all_hT = list(k.r_hT)

    def load_block(bi):
        c0, kind, _ = blocks[bi]
        w_t, r_w = wc[bi % 2]
        for hf in range(2):
            k.wload(w_t[:, 4 * hf:4 * hf + 4, :], r_w, win[:, 4 * hf:4 * hf + 4, c0:c0 + 512])

    load_block(0)
    ci = 0
    for bi, (c0, kind, chunk0) in enumerate(blocks):
        if bi + 1 < len(blocks):
            load_block(bi + 1)
        w_t, r_w = wc[bi % 2]
        for sub in range(4):
            cc = chunk0 + sub
            if kind == "gate":
                for g in range(8):
                    pb, r_pb = _nb(k)

                    def mm(e, pb=pb, g=g, sub=sub, w_t=w_t):
                        for kc in range(8):
                            ins_ = e.matmul(pb, lhsT=w_t[:, kc, sub * 128:(sub + 1) * 128], rhs=k.hT[:, kc, 256 + g * 512:256 + (g + 1) * 512],
                                            start=(kc == 0), stop=(kc == 7))
                        return ins_
                    S.op("pe", mm, reads=[r_w] + all_hT, writes=[r_pb])
                    s_t, r_s = sg[g % 2]
                    S.op("act", lambda e, pb=pb, s_t=s_t: e.activation(out=s_t, in_=pb, func=AF.Sigmoid), reads=[r_pb], writes=[r_s])
                    k.store(sgT_s[cc, :, g * 512:(g + 1) * 512], r_sgT_s, s_t, r_s)
                continue
            R_t, r_R = R[ci % 2]
            Y_t, r_Y = Yb[ci % 2]
            ci += 1
            for gi, (h0, n, ro) in enumerate(tgroups):
                pb, r_pb = _nb(k)

                def mm(e, pb=pb, h0=h0, n=n, sub=sub, w_t=w_t):
                    for kc in range(8):
                        ins_ = e.matmul(pb[:, 0:n], lhsT=w_t[:, kc, sub * 128:(sub + 1) * 128], rhs=k.hT[:, kc, h0:h0 + n],
                                        start=(kc == 0), stop=(kc == 7))
                    return ins_
                S.op("pe", mm, reads=[r_w] + all_hT, writes=[r_pb])
                if gi % 2 == 0:
                    S.op("act", lambda e, pb=pb, n=n, ro=ro, R_t=R_t: e.activation(out=R_t[:, ro:ro + n], in_=pb[:, 0:n], func=AF.Copy),
                         reads=[r_pb], writes=[r_R])
                else:
                    S.op("dve", lambda e, pb=pb, n=n, ro=ro, R_t=R_t: e.tensor_copy(out=R_t[:, ro:ro + n], in_=pb[:, 0:n]),
                         reads=[r_pb], writes=[r_R])
            ceng = "dve"

            def conv(e, R_t=R_t, cc=cc):
                e.tensor_scalar(out=acc, in0=R_t[:, 0:NU], scalar1=convw[:, cc, 0:1], scalar2=None, op0=ALU.mult)
                for j in range(1, 5):
                    ins_ = e.scalar_tensor_tensor(out=acc, in0=R_t[:, j:j + NU], scalar=convw[:, cc, j:j + 1], in1=acc,
                                                  op0=ALU.mult, op1=ALU.add)
                return ins_
            S.op(ceng, conv, reads=[r_R, r_convw], writes=[r_acc])
            head = cc % 8
            if cc >= 16:
                S.op("act", lambda e, Y_t=Y_t: e.activation(out=Y_t, in_=acc, func=AF.Silu), reads=[r_acc], writes=[r_Y])
            else:
                S.op("act", lambda e: e.activation(out=acc, in_=acc, func=AF.Silu), reads=[r_acc], writes=[r_acc])
                S.op("pool" if ceng == "dve" else "dve", lambda e: e.tensor_tensor(out=sq, in0=acc, in1=acc, op=ALU.mult), reads=[r_acc], writes=[r_sq])
                scale = (128.0 ** -0.5) if cc < 8 else 1.0
                for g in range(9):
                    u0 = g * 512
                    n = min(512, NU - u0)
                    pb, r_pb = _nb(k)
                    S.op("pe", lambda e, pb=pb, u0=u0, n=n: e.matmul(pb[:, 0:n], lhsT=k.ones_b, rhs=sq[:, u0:u0 + n], start=True, stop=True),
                         reads=[r_sq, k.r_ones_b], writes=[r_pb])
                    S.op("act", lambda e, pb=pb, n=n, scale=scale: e.activation(out=rn[:, 0:n], in_=pb[:, 0:n], func=AF.Sqrt,
                                                                              scale=1.0 / (scale * scale), bias=EPS / (scale * scale)),
                         reads=[r_pb], writes=[r_rn])
                    S.op("dve", lambda e, n=n: e.reciprocal(out=rn[:, 0:n], in_=rn[:, 0:n]), reads=[r_rn], writes=[r_rn])
                    S.op("dve", lambda e, u0=u0, n=n, Y_t=Y_t: e.tensor_tensor(out=Y_t[:, u0:u0 + n], in0=acc[:, u0:u0 + n], in1=rn[:, 0:n], op=ALU.mult),
                         reads=[r_acc, r_rn], writes=[r_Y])
            if cc < 8:
                k.store(qdT_s[head, :, :], r_qdT_s, Y_t[:, 260:260 + TX], r_Y)
            elif cc < 16:
                k.store(kdT_s[head, :, 0:256], r_kdT_s, Y_t[:, 0:256], r_Y)
                k.store(kdT_s[head, :, 256:T], r_kdT_s, Y_t[:, 260:260 + TX], r_Y)
            if cc >= 8:
                tm_t, r_tm = tm[0]
                for tb in range(5):
                    t0 = tb * 8
                    nt = min(8, NT - t0)
                    pb, r_pb = _nb(k, BF16)
                    pv = pb.rearrange("p (a b) -> p a b", b=128)[:, 0:nt, :]

                    def tr(e, pv=pv, t0=t0, nt=nt, Y_t=Y_t):
                        for j in range(nt):
                            ti = t0 + j
                            u = ti * 128 if ti < 2 else 260 + (ti - 2) * 128
                            ins_ = e.transpose(out=pv[:, j, :], in_=Y_t[:, u:u + 128], identity=k.ident_b)
                        return ins_
                    S.op("pe", tr, reads=[r_Y, k.r_ident_b], writes=[r_pb])
                    if tb % 2 == 0:
                        S.op("act", lambda e, pv=pv, t0=t0, nt=nt, tm_t=tm_t: e.activation(out=tm_t[:, t0:t0 + nt, :], in_=pv, func=AF.Copy),
                             reads=[r_pb], writes=[r_tm])
                    else:
                        S.op("dve", lambda e, pv=pv, t0=t0, nt=nt, tm_t=tm_t: e.tensor_copy(out=tm_t[:, t0:t0 + nt, :], in_=pv),
                             reads=[r_pb], writes=[r_tm])
                dst_s, r_dst = (kd_s, r_kd_s) if cc < 16 else (vd_s, r_vd_s)
                k.store(dst_s.rearrange("(n p) h d -> p n h d", p=128)[:, :, head, :], r_dst, tm_t, r_tm)
    S.barrier()
    A.release()
    A.release()

import os as _os


def _drive(*gens):
    gens = [g for g in gens if g is not None]
    while gens:
        for g in list(gens):
            try:
                next(g)
            except StopIteration:
                gens.remove(g)


def phase_b(k):
    S, A, nc, ins = k.S, k.A, k.nc, k.ins
    qdT_s, r_qdT_s = k.scr["qdT_s"]
    kdT_s, r_kdT_s = k.scr["kdT_s"]
    kd_s, r_kd_s = k.scr["kd_s"]
    vd_s, r_vd_s = k.scr["vd_s"]
    gb_s, r_gb_s = k.scr["gb_s"]
    o_s = [k.scratch("of_s", [TX, D], F32), k.scratch("ob_s", [TX, D], F32)]
    A.mark()
    msk, r_msk = k.tile([128, 9, 128], F32, "msk")
    k.load(msk, r_msk, ins["dn_masks"])
    EC, r_EC = k.tile([64, 2, 8, 128], F32, "EC")
    k.load(EC, r_EC, ins["dn_esel"])
    L1, r_L1 = k.tile([64, 128], F32, "L1")
    L2, r_L2 = k.tile([64, 128], F32, "L2")
    R1, r_R1 = k.tile([64, 8, 128], F32, "R1")
    R2, r_R2 = k.tile([64, 8, 128], F32, "R2")
    X, r_X = k.tile([128, 2, 64], F32, "X")
    k.load(L1, r_L1, ins["dn_linit"])
    k.load(L2, r_L2, ins["dn_linit"])
    S.op("dve", lambda e: e.tensor_copy(out=R1, in_=EC[:, 0, :, :]), reads=[r_EC], writes=[r_R1])
    S.op("dve", lambda e: e.tensor_copy(out=R2, in_=EC[:, 1, :, :]), reads=[r_EC], writes=[r_R2])
    S.op("pool", lambda e: e.memset(X, 0.0), writes=[r_X])
    S32 = [k.tile([128, 8, 128], F32, f"S32_{d}") for d in range(2)]
    Sbf = [k.tile([128, 8, 128], BF16, f"Sbf_{d}") for d in range(2)]
    for d in range(2):
        S.op("pool", lambda e, d=d: e.memset(S32[d][0], 0.0), writes=[S32[d][1]])
        S.op("pool", lambda e, d=d: e.memset(Sbf[d][0], 0.0), writes=[Sbf[d][1]])
    NB = 2
    gbt = [k.tile([128, 48], F32, "gbt") for _ in range(NB)]
    kT = [k.tile([128, 8, 128], BF16, "kT") for _ in range(NB)]
    qT = [k.tile([128, 8, 128], BF16, "qT") for _ in range(NB)]
    ktm = [k.tile([128, 8, 128], BF16, "ktm") for _ in range(NB)]
    vtm = [k.tile([128, 8, 128], BF16, "vtm") for _ in range(NB)]
    sm = [k.tile([128, 4, 8], F32, "sm") for _ in range(NB)]
    E1, r_E1 = k.tile([128, 8, 128], F32, "E1")
    M, r_M = k.tile([128, 8, 128], F32, "M")
    Mt, r_Mt = k.tile([128, 8, 128], F32, "Mt")
    Md, r_Md = k.tile([128, 8, 128], F32, "Md")
    Mo1, r_Mo1 = k.tile([128, 8, 128], F32, "Mo1")
    Mo2, r_Mo2 = k.tile([128, 8, 128], F32, "Mo2")
    PP = [k.tile([128, 8, 128], F32, f"PP{j}") for j in range(2)]
    PT = [k.tile([128, 8, 128], F32, f"PT{j}") for j in range(2)]
    Tt, r_Tt = k.tile([128, 8, 128], F32, "Tt")
    TtB, r_TtB = k.tile([128, 8, 128], BF16, "TtB")
    Abuf, _ = k.tile([128, 8, 128], F32, "Abuf")
    E1r, Mdr, Mo1r, Mo2r, Mtr, Ttr = (t_ for t_ in (Abuf, Md, Mo1, Mo2, Mt, Tt))
    PPr = [PP[j][0] for j in range(2)]
    PTr = [PT[j][0] for j in range(2)]
    kg, r_kg = k.tile([128, 8, 128], BF16, "kg")
    kdec = [k.tile([128, 8, 128], BF16, "kdec") for _ in range(NB)]
    wT = [k.tile([128, 8, 128], BF16, "wT") for _ in range(NB)]
    u = [k.tile([128, 8, 128], F32, "u") for _ in range(NB)]
    qkT = [k.tile([128, 8, 128], BF16, "qkT") for _ in range(NB)]
    vnew, r_vnew = k.tile([128, 8, 128], BF16, "vnew")
    tmpo, r_tmpo = k.tile([128, 8, 128], F32, "tmpo")
    o_t = [k.tile([128, 8, 128], F32, "o") for _ in range(NB)]

    RG = {nm: [Res(nm + "0"), Res(nm + "1")] for nm in ("Ab", "E1", "M", "Md", "Mo1", "Mo2", "Mt", "Tt", "TtB", "PP0", "PP1", "PT0", "PT1")}
    wT_res = [[Res("wT"), Res("wT")] for _ in range(NB)]
    u_res = [[Res("u"), Res("u")] for _ in range(NB)]
    qk_res = [[Res("qk"), Res("qk")] for _ in range(NB)]
    pre_i = [0]

    def nbp(dtype=F32):
        b = pre_i[0]
        pre_i[0] = (b + 1) % 4
        return k.bank(b, dtype)

    def b4(pb):
        return pb.rearrange("p (a b) -> p a b", b=128)

    units = []
    fwd = list(range(NT))
    bwd = [1, 0] + list(range(NT - 1, 1, -1))
    for s in range(NT):
        units.append((0, fwd[s]))
        units.append((1, bwd[s]))

    def loads(ui):
        d, ti = units[ui]
        b = ui % NB
        tok = slice(ti * 128, (ti + 1) * 128)
        k.load(gbt[b][0], gbt[b][1], gb_s[tok, :], r_gb_s)
        k.load(kT[b][0], kT[b][1], kdT_s[:, :, tok].rearrange("h d t -> d h t"), r_kdT_s)
        k.load(ktm[b][0], ktm[b][1], kd_s[tok, :, :], r_kd_s, q="act")
        k.load(vtm[b][0], vtm[b][1], vd_s[tok, :, :], r_vd_s, q="act")
        if ti >= 2:
            xt_ = slice((ti - 2) * 128, (ti - 1) * 128)
            k.load(qT[b][0], qT[b][1], qdT_s[:, :, xt_].rearrange("h d t -> d h t"), r_qdT_s)

    def pre(ui):
        d, ti = units[ui]
        b = ui % NB
        is_x = ti >= 2
        g_t, r_g = gbt[b]
        kT_t, r_kT = kT[b]
        qT_t, r_qT = qT[b]
        ktm_t, r_ktm = ktm[b]
        vtm_t, r_vtm = vtm[b]
        sm_t, r_sm = sm[b]
        gcol = g_t[:, d * 8:(d + 1) * 8]
        beta = g_t[:, 16 + d * 8:16 + (d + 1) * 8]
        lnb = g_t[:, 32 + d * 8:32 + (d + 1) * 8]
        pb, r_pb = nbp()

        def mm0(e):
            e.matmul(pb[:, 0:8], lhsT=msk[:, d, :], rhs=gcol, start=True, stop=True)
            return e.matmul(pb[:, 8:16], lhsT=k.ones_f, rhs=gcol, start=True, stop=True)
        S.op("pe", mm0, reads=[r_msk, r_g, k.r_ones_f], writes=[r_pb])
        S.op("dve", lambda e: e.tensor_copy(out=X[:, :, 32:40], in_=pb[:, 0:8].unsqueeze(1).to_broadcast([128, 2, 8])), reads=[r_pb], writes=[r_X])
        S.op("dve", lambda e: e.tensor_copy(out=X[:, 1, 0:8], in_=pb[:, 0:8]), reads=[r_pb], writes=[r_X])
        S.op("dve", lambda e: e.tensor_tensor(out=X[:, 0, 0:8], in0=pb[:, 0:8], in1=lnb, op=ALU.add), reads=[r_pb, r_g], writes=[r_X])
        S.op("act", lambda e: e.activation(out=sm_t[:, 0, :], in_=pb[:, 0:8], func=AF.Exp), reads=[r_pb], writes=[r_sm])
        S.op("act", lambda e: e.activation(out=sm_t[:, 1, :], in_=pb[:, 8:16], func=AF.Exp), reads=[r_pb], writes=[r_sm])
        S.op("dve", lambda e: e.tensor_tensor(out=sm_t[:, 3, :], in0=pb[:, 8:16], in1=X[:, 1, 0:8], op=ALU.subtract), reads=[r_pb, r_X], writes=[r_sm])
        S.op("act", lambda e: e.activation(out=sm_t[:, 2, :], in_=sm_t[:, 3, :], func=AF.Exp), reads=[r_sm], writes=[r_sm])
        pt, r_pt = nbp()

        def tr0(e):
            e.transpose(out=pt[0:64, 0:128], in_=X[:, 0, :], identity=k.ident_f)
            return e.transpose(out=pt[0:64, 128:256], in_=X[:, 1, :], identity=k.ident_f)
        S.op("pe", tr0, reads=[r_X, k.r_ident_f], writes=[r_pt])
        S.op("act", lambda e: e.activation(out=L1[0:8, :], in_=pt[0:8, 0:128], func=AF.Copy), reads=[r_pt], writes=[r_L1])
        S.op("act", lambda e: e.activation(out=L2[0:8, :], in_=pt[0:8, 128:256], func=AF.Copy), reads=[r_pt], writes=[r_L2])
        S.op("dve", lambda e: e.tensor_tensor(out=R1[32:40, :, :], in0=EC[32:40, 1, :, :], in1=pt[32:40, 0:128].unsqueeze(1).to_broadcast([8, 8, 128]), op=ALU.mult),
             reads=[r_pt, r_EC], writes=[r_R1])
        S.op("dve", lambda e: e.tensor_tensor(out=R2[32:40, :, :], in0=EC[32:40, 0, :, :], in1=pt[32:40, 128:256].unsqueeze(1).to_broadcast([8, 8, 128]), op=ALU.mult),
             reads=[r_pt, r_EC], writes=[r_R2])
        kd_t, r_kd = kdec[b]
        S.op("pool", lambda e: e.tensor_tensor(out=kg, in0=ktm_t, in1=sm_t[:, 0, :].unsqueeze(2).to_broadcast([128, 8, 128]), op=ALU.mult),
             reads=[r_ktm, r_sm], writes=[r_kg])
        S.op("pool", lambda e: e.tensor_tensor(out=kd_t, in0=ktm_t, in1=sm_t[:, 2, :].unsqueeze(2).to_broadcast([128, 8, 128]), op=ALU.mult),
             reads=[r_ktm, r_sm], writes=[r_kd])
        yield
        def do_group(grp):
            hs = range(4 * grp, 4 * grp + 4)
            gs = slice(4 * grp, 4 * grp + 4)
            r_E1, r_M, r_Md, r_Mo1, r_Mo2, r_Mt, r_Tt, r_TtB = (RG[nm][grp] for nm in ("E1", "M", "Md", "Mo1", "Mo2", "Mt", "Tt", "TtB"))
            r_Ab = RG["Ab"][grp]
            rPP = [RG["PP0"][grp], RG["PP1"][grp]]
            rPT = [RG["PT0"][grp], RG["PT1"][grp]]
            Mo_list = ((Mo1r, r_Mo1), (Mo2r, r_Mo2))
            Md_list = ((Mdr, r_Md, 6), (Mo1r, r_Mo1, 7), (Mo2r, r_Mo2, 8))
            pk, r_pk = nbp()
            pd, r_pd = nbp()

            def mmk(e):
                for hh, h in enumerate(hs):
                    ins_ = e.matmul(b4(pk)[:, hh, :], lhsT=kT_t[:, h, :], rhs=kT_t[:, h, :], start=True, stop=True)
                return ins_
            S.op("pe", mmk, reads=[r_kT], writes=[r_pk])

            def mmd(e):
                for hh, h in enumerate(hs):
                    ins_ = e.matmul(b4(pd)[:, hh, :], lhsT=L1, rhs=R1[:, h, :], start=True, stop=True)
                return ins_
            S.op("pe", mmd, reads=[r_L1, r_R1], writes=[r_pd])
            S.op("dve", lambda e: e.scalar_tensor_tensor(out=E1[:, gs, :], in0=b4(pd), scalar=0.0, in1=msk[:, 2 + d, :].unsqueeze(1).to_broadcast([128, 4, 128]),
                                                         op0=ALU.min, op1=ALU.add), reads=[r_pd, r_msk], writes=[r_E1])
            S.op("act", lambda e: e.activation(out=E1[:, gs, :], in_=E1[:, gs, :], func=AF.Exp), reads=[r_E1], writes=[r_E1])
            S.op("dve", lambda e: e.tensor_tensor(out=M[:, gs, :], in0=b4(pk), in1=E1[:, gs, :], op=ALU.mult), reads=[r_pk, r_E1], writes=[r_M])
            for (dst_, rdst_, mi_) in Md_list:
                S.op("pool", lambda e, dst_=dst_, mi_=mi_: e.tensor_tensor(out=dst_[:, gs, :], in0=M[:, gs, :],
                                                                        in1=msk[:, mi_, :].unsqueeze(1).to_broadcast([128, 4, 128]), op=ALU.mult),
                     reads=[r_M, r_msk], writes=[rdst_])
            pm, r_pm = nbp()

            def trm(e):
                for hh, h in enumerate(hs):
                    ins_ = e.transpose(out=b4(pm)[:, hh, :], in_=Md[:, h, :], identity=k.ident_f)
                return ins_
            S.op("pe", trm, reads=[r_Md, k.r_ident_f], writes=[r_pm])
            S.op("act", lambda e: e.activation(out=Mtr[:, gs, :], in_=b4(pm), func=AF.Copy), reads=[r_pm], writes=[r_Mt])
            S.op("dve", lambda e: e.scalar_tensor_tensor(out=Ttr[:, gs, :], in0=b4(pm), scalar=-1.0, in1=k.ident_f.unsqueeze(1).to_broadcast([128, 4, 128]),
                                                         op0=ALU.mult, op1=ALU.add), reads=[r_pm, k.r_ident_f], writes=[r_Tt])
            yield
            P_prev, rP_prev, Pt_prev, rPt_prev = Mdr, r_Md, Mtr, r_Mt
            for lvl in range(1, 1 + int(_os.environ.get('B_LEVELS', 4))):
                P_new, rP_new = PPr[lvl % 2], rPP[lvl % 2]
                Pt_new, rPt_new = PTr[lvl % 2], rPT[lvl % 2]
                pa, r_pa = nbp()

                def mma(e, P_prev=P_prev, Pt_prev=Pt_prev, pa=pa):
                    for hh, h in enumerate(hs):
                        ins_ = e.matmul(b4(pa)[:, hh, :], lhsT=Pt_prev[:, h, :], rhs=P_prev[:, h, :], start=True, stop=True)
                    return ins_
                S.op("pe", mma, reads=[rP_prev, rPt_prev], writes=[r_pa])
                S.op("act", lambda e, P_new=P_new, pa=pa: e.activation(out=P_new[:, gs, :], in_=b4(pa), func=AF.Copy), reads=[r_pa], writes=[rP_new])
                if lvl < int(_os.environ.get('B_LEVELS', 4)):
                    pbk, r_pbk = nbp()

                    def mmb(e, P_prev=P_prev, Pt_prev=Pt_prev, pbk=pbk):
                        for hh, h in enumerate(hs):
                            ins_ = e.matmul(b4(pbk)[:, hh, :], lhsT=P_prev[:, h, :], rhs=Pt_prev[:, h, :], start=True, stop=True)
                        return ins_
                    S.op("pe", mmb, reads=[rP_prev, rPt_prev], writes=[r_pbk])
                    S.op("act", lambda e, Pt_new=Pt_new, pbk=pbk: e.activation(out=Pt_new[:, gs, :], in_=b4(pbk), func=AF.Copy), reads=[r_pbk], writes=[rPt_new])
                pc, r_pc = nbp()

                def mmc(e, P_new=P_new, pc=pc):
                    for hh, h in enumerate(hs):
                        ins_ = e.matmul(b4(pc)[:, hh, :], lhsT=P_new[:, h, :], rhs=Ttr[:, h, :], start=True, stop=True)
                    return ins_
                S.op("pe", mmc, reads=[rP_new, r_Tt], writes=[r_pc])
                S.op("dve", lambda e, pc=pc: e.tensor_tensor(out=Ttr[:, gs, :], in0=Tt[:, gs, :], in1=b4(pc), op=ALU.add), reads=[r_Tt, r_pc], writes=[r_Tt])
                P_prev, rP_prev, Pt_prev, rPt_prev = P_new, rP_new, Pt_new, rPt_new
                yield
            for (Mo_, rMo_) in Mo_list:
                ptd, r_ptd = nbp()

                def trt(e, ptd=ptd):
                    for hh, h in enumerate(hs):
                        ins_ = e.transpose(out=b4(ptd)[:, hh, :], in_=Tt[:, h, :], identity=k.ident_f)
                    return ins_
                S.op("pe", trt, reads=[r_Tt, k.r_ident_f], writes=[r_ptd])
                S.op("act", lambda e, ptd=ptd: e.activation(out=Mtr[:, gs, :], in_=b4(ptd), func=AF.Copy), reads=[r_ptd], writes=[r_Mt])
                pa2, r_pa2 = nbp()

                def mma2(e, pa2=pa2, Mo_=Mo_):
                    for hh, h in enumerate(hs):
                        ins_ = e.matmul(b4(pa2)[:, hh, :], lhsT=Mo_[:, h, :], rhs=Ttr[:, h, :], start=True, stop=True)
                    return ins_
                S.op("pe", mma2, reads=[rMo_, r_Tt], writes=[r_pa2])
                S.op("act", lambda e, pa2=pa2: e.activation(out=E1r[:, gs, :], in_=b4(pa2), func=AF.Copy), reads=[r_pa2], writes=[r_Ab])
                pc2, r_pc2 = nbp()

                def mmc2(e, pc2=pc2):
                    for hh, h in enumerate(hs):
                        ins_ = e.matmul(b4(pc2)[:, hh, :], lhsT=Mtr[:, h, :], rhs=E1r[:, h, :], start=True, stop=True)
                    return ins_
                S.op("pe", mmc2, reads=[r_Mt, r_Ab], writes=[r_pc2])
                S.op("dve", lambda e, pc2=pc2: e.tensor_tensor(out=Ttr[:, gs, :], in0=Tt[:, gs, :], in1=b4(pc2), op=ALU.subtract), reads=[r_Tt, r_pc2], writes=[r_Tt])
                yield
            S.op("pool", lambda e: e.tensor_tensor(out=TtB[:, gs, :], in0=Tt[:, gs, :], in1=beta[:, gs].unsqueeze(2).to_broadcast([128, 4, 128]), op=ALU.mult),
                 reads=[r_Tt, r_g], writes=[r_TtB])
            pw, r_pw = nbp()

            def mmw(e):
                for hh, h in enumerate(hs):
                    ins_ = e.matmul(b4(pw)[:, hh, :], lhsT=kg[:, h, :], rhs=TtB[:, h, :], start=True, stop=True)
                return ins_
            S.op("pe", mmw, reads=[r_kg, r_TtB], writes=[r_pw])
            S.op("act", lambda e: e.activation(out=wT[b][0][:, gs, :], in_=b4(pw), func=AF.Copy), reads=[r_pw], writes=[wT_res[b][grp]])
            pu, r_pu = nbp()

            def mmu(e):
                for hh, h in enumerate(hs):
                    ins_ = e.matmul(b4(pu)[:, hh, :], lhsT=TtB[:, h, :], rhs=vtm_t[:, h, :], start=True, stop=True)
                return ins_
            S.op("pe", mmu, reads=[r_TtB, r_vtm], writes=[r_pu])
            S.op("act", lambda e: e.activation(out=u[b][0][:, gs, :], in_=b4(pu), func=AF.Copy), reads=[r_pu], writes=[u_res[b][grp]])
            yield
            if is_x:
                pq, r_pq = nbp()
                pd2, r_pd2 = nbp()

                def mmq(e):
                    for hh, h in enumerate(hs):
                        ins_ = e.matmul(b4(pq)[:, hh, :], lhsT=kT_t[:, h, :], rhs=qT_t[:, h, :], start=True, stop=True)
                    return ins_
                S.op("pe", mmq, reads=[r_kT, r_qT], writes=[r_pq])

                def mmd2(e):
                    for hh, h in enumerate(hs):
                        ins_ = e.matmul(b4(pd2)[:, hh, :], lhsT=L2, rhs=R2[:, h, :], start=True, stop=True)
                    return ins_
                S.op("pe", mmd2, reads=[r_L2, r_R2], writes=[r_pd2])
                S.op("dve", lambda e: e.scalar_tensor_tensor(out=E1[:, gs, :], in0=b4(pd2), scalar=0.0, in1=msk[:, 4 + d, :].unsqueeze(1).to_broadcast([128, 4, 128]),
                                                             op0=ALU.min, op1=ALU.add), reads=[r_pd2, r_msk], writes=[r_E1])
                S.op("act", lambda e: e.activation(out=E1[:, gs, :], in_=E1[:, gs, :], func=AF.Exp), reads=[r_E1], writes=[r_E1])
                S.op("dve", lambda e: e.tensor_tensor(out=qkT[b][0][:, gs, :], in0=b4(pq), in1=E1[:, gs, :], op=ALU.mult), reads=[r_pq, r_E1], writes=[qk_res[b][grp]])
                yield
        gens_ = [do_group(0), do_group(1)]
        while gens_:
            for g_ in list(gens_):
                try:
                    next(g_)
                    yield
                except StopIteration:
                    gens_.remove(g_)

    def seq(ui):
        d, ti = units[ui]
        b = ui % NB
        is_x = ti >= 2
        S32_t, r_S32 = S32[d]
        Sbf_t, r_Sbf = Sbf[d]
        sm_t, r_sm = sm[b]
        wT_t, u_t, qk_t = wT[b][0], u[b][0], qkT[b][0]
        qT_t, r_qT = qT[b]
        kd_t, r_kd = kdec[b]
        banks = [k.bank(4 + j) for j in range(4)]

        def grp_mm(bank2, lhs_fn, rhs_fn, reads):
            for grp in range(2):
                pb, r_pb = bank2[grp]

                def f(e, grp=grp, pb=pb):
                    for hh in range(4):
                        h = 4 * grp + hh
                        ins_ = e.matmul(b4(pb)[:, hh, :], lhsT=lhs_fn(h), rhs=rhs_fn(h), start=True, stop=True)
                    return ins_
                S.op("pe", f, reads=reads, writes=[r_pb])
        grp_mm(banks[0:2], lambda h: wT_t[:, h, :], lambda h: Sbf_t[:, h, :], wT_res[b] + [r_Sbf])
        S.op("pool", lambda e: e.tensor_tensor(out=S32_t, in0=S32_t, in1=sm_t[:, 1, :].unsqueeze(2).to_broadcast([128, 8, 128]), op=ALU.mult),
             reads=[r_S32, r_sm], writes=[r_S32])
        yield
        for grp in range(2):
            gs = slice(4 * grp, 4 * grp + 4)
            pb, r_pb = banks[grp]
            S.op("dve", lambda e, pb=pb, gs=gs: e.tensor_tensor(out=vnew[:, gs, :], in0=u_t[:, gs, :], in1=b4(pb), op=ALU.subtract),
                 reads=u_res[b] + [r_pb], writes=[r_vnew])
        yield
        if is_x:
            grp_mm(banks[2:4], lambda h: qT_t[:, h, :], lambda h: Sbf_t[:, h, :], [r_qT, r_Sbf])
            grp_mm(banks[0:2], lambda h: qk_t[:, h, :], lambda h: vnew[:, h, :], qk_res[b] + [r_vnew])
            yield
            o_tt, r_o = o_t[b]
            for grp in range(2):
                gs = slice(4 * grp, 4 * grp + 4)
                pbq, r_pbq = banks[2 + grp]
                pbc, r_pbc = banks[grp]
                S.op("dve", lambda e, pbq=pbq, gs=gs: e.tensor_tensor(out=tmpo[:, gs, :], in0=b4(pbq), in1=sm_t[:, 0, gs].unsqueeze(2).to_broadcast([128, 4, 128]), op=ALU.mult),
                     reads=[r_pbq, r_sm], writes=[r_tmpo])
                S.op("dve", lambda e, pbc=pbc, gs=gs, o_tt=o_tt: e.tensor_tensor(out=o_tt[:, gs, :], in0=tmpo[:, gs, :], in1=b4(pbc), op=ALU.add),
                     reads=[r_tmpo, r_pbc], writes=[r_o])
            xi = ti - 2
            k.store(o_s[d][0][xi * 128:(xi + 1) * 128, :], o_s[d][1], o_tt.rearrange("p h d -> p (h d)"), r_o)
            yield
        grp_mm(banks[2:4], lambda h: kd_t[:, h, :], lambda h: vnew[:, h, :], [r_kd, r_vnew])
        yield
        for grp in range(2):
            gs = slice(4 * grp, 4 * grp + 4)
            pb, r_pb = banks[2 + grp]
            S.op("dve", lambda e, pb=pb, gs=gs: e.tensor_tensor(out=S32_t[:, gs, :], in0=S32_t[:, gs, :], in1=b4(pb), op=ALU.add),
                 reads=[r_S32, r_pb], writes=[r_S32])
        S.op("act", lambda e: e.activation(out=Sbf_t, in_=S32_t, func=AF.Copy), reads=[r_S32], writes=[r_Sbf])
        yield

    import os as _os
    stop_after = int(_os.environ.get("B_UNITS", len(units)))
    pre_cut = int(_os.environ.get("B_PRE_CUT", 10000))
    do_seq = int(_os.environ.get("B_SEQ", 1))
    _pre = pre
    _seq = seq

    def pre(ui):
        for n_, _ in enumerate(_pre(ui)):
            if n_ + 1 >= pre_cut:
                return
            yield

    def seq(ui):
        if not do_seq:
            return
        yield from _seq(ui)
    loads(0)
    _drive(pre(0))
    for ui in range(stop_after):
        if ui + 1 < stop_after:
            loads(ui + 1)
            _drive(pre(ui + 1), seq(ui))
        else:
            _drive(seq(ui))
    k.b_state = (S32, Sbf)
    S.barrier()
    A.release()

def phase_c(k):
    S, A, nc, ins = k.S, k.A, k.nc, k.ins
    of_s, r_of_s = k.scr["of_s"]
    ob_s, r_ob_s = k.scr["ob_s"]
    zs_s, r_zs_s = k.scr["zs_s"]
    qmT_s, r_qmT_s = k.scr["qmT_s"]
    kmT_s, r_kmT_s = k.scr["kmT_s"]
    vm_s, r_vm_s = k.scr["vm_s"]
    yaT_s, r_yaT_s = k.scratch("yaT_s", [H, 128, TX], BF16)
    ybT_s, r_ybT_s = k.scratch("ybT_s", [H, 128, TX], BF16)
    A.mark()
    dng, r_dng = k.tile([128, 128], F32, "dng")
    k.load(dng, r_dng, ins["dng_rep"])
    NB = 2
    of_t = [k.tile([128, 8, 128], F32, "of") for _ in range(NB)]
    ob_t = [k.tile([128, 8, 128], F32, "ob") for _ in range(NB)]
    z_t = [k.tile([128, 8, 128], BF16, "z") for _ in range(NB)]
    sq, r_sq = k.tile([128, 8, 128], F32, "sq")
    ss8, r_ss8 = k.tile([128, 8], F32, "ss8")
    yb, r_yb = k.tile([128, 8, 128], BF16, "yb")
    yT = [k.tile([128, 8, 128], BF16, "yT") for _ in range(NB)]

    def c1_loads(xi):
        b = xi % NB
        rows = slice(xi * 128, (xi + 1) * 128)
        k.load(of_t[b][0], of_t[b][1], of_s[rows, :].rearrange("p (h d) -> p h d", h=8), r_of_s)
        k.load(ob_t[b][0], ob_t[b][1], ob_s[rows, :].rearrange("p (h d) -> p h d", h=8), r_ob_s, q="act")
        k.load(z_t[b][0], z_t[b][1], zs_s[rows, :].rearrange("p (h d) -> p h d", h=8), r_zs_s)

    c1_loads(0)
    for xi in range(32):
        b = xi % NB
        if xi + 1 < 32:
            c1_loads(xi + 1)
        o_, r_o = of_t[b]
        ob_, r_ob = ob_t[b]
        z_, r_z = z_t[b]
        S.op("dve", lambda e, o_=o_, ob_=ob_: e.tensor_tensor(out=o_, in0=o_, in1=ob_, op=ALU.add), reads=[r_o, r_ob], writes=[r_o])
        S.op("pool", lambda e, o_=o_: e.tensor_tensor(out=sq, in0=o_, in1=o_, op=ALU.mult), reads=[r_o], writes=[r_sq])
        S.op("dve", lambda e: e.tensor_reduce(out=ss8, in_=sq, axis=AX.X, op=ALU.add), reads=[r_sq], writes=[r_ss8])
        _rstd(k, ss8, r_ss8, 128, ss8, r_ss8)
        S.op("dve", lambda e, o_=o_: e.tensor_tensor(out=o_, in0=o_, in1=ss8.unsqueeze(2).to_broadcast([128, 8, 128]), op=ALU.mult),
             reads=[r_o, r_ss8], writes=[r_o])
        S.op("pool", lambda e, o_=o_: e.tensor_tensor(out=o_, in0=o_, in1=dng.unsqueeze(1).to_broadcast([128, 8, 128]), op=ALU.mult),
             reads=[r_o, r_dng], writes=[r_o])
        S.op("pool", lambda e, o_=o_, z_=z_: e.tensor_tensor(out=yb, in0=o_, in1=z_, op=ALU.mult), reads=[r_o, r_z], writes=[r_yb])
        pb, r_pb = _nb(k, BF16)
        pv = pb.rearrange("p (a b) -> p a b", b=128)

        def tr(e, pv=pv):
            for j in range(8):
                ins_ = e.transpose(out=pv[:, j, :], in_=yb[:, j, :], identity=k.ident_b)
            return ins_
        S.op("pe", tr, reads=[r_yb, k.r_ident_b], writes=[r_pb])
        yT_t, r_yT = yT[b]
        S.op("act", lambda e, pv=pv, yT_t=yT_t: e.activation(out=yT_t, in_=pv, func=AF.Copy), reads=[r_pb], writes=[r_yT])
        k.store(yaT_s[:, :, xi * 128:(xi + 1) * 128].rearrange("h d t -> d h t"), r_yaT_s, yT_t, r_yT)
    S.barrier()
    A.release()
    A.mark()
    Kn = [k.tile([128, T], BF16, "Kn") for _ in range(2)]
    Kr = [k.tile([64, T], BF16, "Kr") for _ in range(2)]
    Vh = [k.tile([128, NT, 128], BF16, "Vh") for _ in range(2)]
    Qn = [k.tile([128, 512], BF16, "Qn") for _ in range(2)]
    Qr = [k.tile([64, 512], BF16, "Qr") for _ in range(2)]
    NP = 4
    PT = [k.tile([128, 512], BF16, "PT") for _ in range(NP)]
    rinv, r_rinv = k.tile([128, 512], F32, "rinv")
    yo = [k.tile([128, 512], BF16, "yo") for _ in range(2)]
    vmv = vm_s.rearrange("(n p) c -> p n c", p=128)

    def head_loads(h):
        b = h % 2
        k.load(Kn[b][0], Kn[b][1], kmT_s[h, 0:128, :], r_kmT_s)
        k.load(Kr[b][0], Kr[b][1], kmT_s[h, 128:192, :], r_kmT_s, q="act")
        k.load(Vh[b][0], Vh[b][1], vmv[:, :, h * 128:(h + 1) * 128], r_vm_s)

    def q_loads(h, qg):
        b = (h * 8 + qg) % 2
        k.load(Qn[b][0], Qn[b][1], qmT_s[h, 0:128, qg * 512:(qg + 1) * 512], r_qmT_s)
        k.load(Qr[b][0], Qr[b][1], qmT_s[h, 128:192, qg * 512:(qg + 1) * 512], r_qmT_s, q="act")

    head_loads(0)
    q_loads(0, 0)
    sbank = [0]
    it = 0
    for h in range(H):
        if h + 1 < H:
            head_loads(h + 1)
        Kn_t, r_Kn = Kn[h % 2]
        Kr_t, r_Kr = Kr[h % 2]
        V_t, r_V = Vh[h % 2]
        for qg in range(8):
            gi = h * 8 + qg
            nxt = gi + 1
            if nxt < H * 8:
                q_loads(nxt // 8, nxt % 8)
            Qn_t, r_Qn = Qn[gi % 2]
            Qr_t, r_Qr = Qr[gi % 2]
            po, r_po = k.bank(4 + gi % 2)
            pr, r_pr = k.bank(6 + gi % 2)
            sb = {}

            def emit_s(kt):
                bnk = sbank[0]
                sbank[0] = (bnk + 1) % 4
                ps_, r_ps = k.bank(bnk)

                def f(e, ps_=ps_, kt=kt, Kn_t=Kn_t, Kr_t=Kr_t, Qn_t=Qn_t, Qr_t=Qr_t):
                    e.matmul(ps_, lhsT=Kn_t[:, kt * 128:(kt + 1) * 128], rhs=Qn_t, start=True, stop=False)
                    return e.matmul(ps_, lhsT=Kr_t[:, kt * 128:(kt + 1) * 128], rhs=Qr_t, start=False, stop=True)
                S.op("pe", f, reads=[r_Kn, r_Kr, r_Qn, r_Qr], writes=[r_ps])
                pt_, r_pt = PT[kt % NP]
                S.op("act", lambda e, ps_=ps_, pt_=pt_: e.activation(out=pt_, in_=ps_, func=AF.Exp), reads=[r_ps], writes=[r_pt])
                sb[kt] = (pt_, r_pt)

            def emit_pv(kt):
                pt_, r_pt = sb.pop(kt)

                def f(e, pt_=pt_, kt=kt, po=po, pr=pr, V_t=V_t):
                    e.matmul(po, lhsT=V_t[:, kt, :], rhs=pt_, start=(kt == 0), stop=(kt == NT - 1))
                    return e.matmul(pr, lhsT=k.ones_b, rhs=pt_, start=(kt == 0), stop=(kt == NT - 1))
                S.op("pe", f, reads=[r_V, r_pt, k.r_ones_b], writes=[r_po, r_pr])

            LOOK = 2
            for kt in range(min(LOOK, NT)):
                emit_s(kt)
            for kt in range(NT):
                if kt + LOOK < NT:
                    emit_s(kt + LOOK)
                emit_pv(kt)
            S.op("dve", lambda e, pr=pr: e.reciprocal(out=rinv, in_=pr), reads=[r_pr], writes=[r_rinv])
            yo_t, r_yo = yo[gi % 2]
            S.op("dve", lambda e, po=po, yo_t=yo_t: e.tensor_tensor(out=yo_t, in0=po, in1=rinv, op=ALU.mult), reads=[r_po, r_rinv], writes=[r_yo])
            k.store(ybT_s[h, :, qg * 512:(qg + 1) * 512], r_ybT_s, yo_t, r_yo)
    S.barrier()
    A.release()

def phase_d(k):
    S, A, nc, ins = k.S, k.A, k.nc, k.ins
    yaT_s, r_yaT_s = k.scr["yaT_s"]
    ybT_s, r_ybT_s = k.scr["ybT_s"]
    sgT_s, r_sgT_s = k.scr["sgT_s"]
    xmid_s, r_xmid_s = k.scratch("xmid_s", [TX, D], F32)
    h2_s, r_h2_s = k.scratch("h2_s", [TX, D], BF16)
    aff_s, r_aff_s = k.scratch("aff_s", [TX, 16], F32)
    affT_s, r_affT_s = k.scratch("affT_s", [16, TX], F32)
    A.mark()
    woa, r_woa = k.tile([128, 8, D], BF16, "woa")
    wob, r_wob = k.tile([128, 8, D], BF16, "wob")
    wo, r_wo = k.tile([128, 8, D], BF16, "wo")
    rw, r_rw = k.tile([128, 8, 16], F32, "rw")
    for (dst, rdst, nm) in ((woa, r_woa, "w_out_a"), (wob, r_wob, "w_out_b"), (wo, r_wo, "w_o")):
        src = ins[nm].rearrange("(kc p) n -> p kc n", p=128)
        for hf in range(2):
            k.wload(dst[:, 4 * hf:4 * hf + 4, :], rdst, src[:, 4 * hf:4 * hf + 4, :])
    k.load(rw, r_rw, ins["router_w"].rearrange("(kc p) n -> p kc n", p=128))
    NB = 2
    yaT = [k.tile([128, 8, 512], BF16, "yaT") for _ in range(NB)]
    ybT = [k.tile([128, 8, 512], BF16, "ybT") for _ in range(NB)]
    gA = [k.tile([128, 8, 512], BF16, "gA") for _ in range(NB)]
    gB = [k.tile([128, 8, 512], BF16, "gB") for _ in range(NB)]
    mg, r_mg = k.tile([128, 8, 512], BF16, "mg")
    t1 = [k.tile([128, 512], F32, "t1") for _ in range(2)]
    t2 = [k.tile([128, 512], F32, "t2") for _ in range(2)]
    xt = [k.tile([128, D], F32, "xt") for _ in range(NB)]
    xm = [k.tile([128, D], F32, "xm") for _ in range(NB)]
    junk, r_junk = k.tile([128, D], BF16, "junk")
    ssd = [k.tile([128, 8], F32, "ssd") for _ in range(NB)]
    h2f, r_h2f = k.tile([128, D], F32, "h2f")
    h2b = [k.tile([128, D], BF16, "h2b") for _ in range(NB)]
    h2T, r_h2T = k.tile([128, 8, 128], F32, "h2T")
    ex = [k.tile([128, 16], F32, "ex") for _ in range(NB)]
    affT = [k.tile([16, 128], F32, "affT") for _ in range(NB)]

    def g_loads(g):
        b = g % NB
        cols = slice(g * 512, (g + 1) * 512)
        k.load(yaT[b][0], yaT[b][1], yaT_s[:, :, cols].rearrange("h d t -> d h t"), r_yaT_s)
        k.load(ybT[b][0], ybT[b][1], ybT_s[:, :, cols].rearrange("h d t -> d h t"), r_ybT_s, q="act")
        k.load(gA[b][0], gA[b][1], sgT_s[0:8, :, cols].rearrange("h d t -> d h t"), r_sgT_s)
        k.load(gB[b][0], gB[b][1], sgT_s[8:16, :, cols].rearrange("h d t -> d h t"), r_sgT_s, q="act")

    g_loads(0)
    for g in range(8):
        b = g % NB
        if g + 1 < 8:
            g_loads(g + 1)
        ya_, r_ya = yaT[b]
        yb_, r_yb = ybT[b]
        gA_, r_gA = gA[b]
        gB_, r_gB = gB[b]
        for oc in range(8):
            pa, r_pa = _nb(k)
            pbk, r_pbk = _nb(k)

            def mma(e, pa=pa, oc=oc, ya_=ya_):
                for kc in range(8):
                    ins_ = e.matmul(pa, lhsT=woa[:, kc, oc * 128:(oc + 1) * 128], rhs=ya_[:, kc, :], start=(kc == 0), stop=(kc == 7))
                return ins_
            S.op("pe", mma, reads=[r_woa, r_ya], writes=[r_pa])

            def mmb(e, pbk=pbk, oc=oc, yb_=yb_):
                for kc in range(8):
                    ins_ = e.matmul(pbk, lhsT=wob[:, kc, oc * 128:(oc + 1) * 128], rhs=yb_[:, kc, :], start=(kc == 0), stop=(kc == 7))
                return ins_
            S.op("pe", mmb, reads=[r_wob, r_yb], writes=[r_pbk])
            t1_, r_t1 = t1[oc % 2]
            t2_, r_t2 = t2[oc % 2]
            S.op("dve", lambda e, pa=pa, oc=oc, t1_=t1_, gA_=gA_: e.tensor_tensor(out=t1_, in0=pa, in1=gA_[:, oc, :], op=ALU.mult), reads=[r_pa, r_gA], writes=[r_t1])
            S.op("dve", lambda e, pbk=pbk, oc=oc, t2_=t2_, gB_=gB_: e.tensor_tensor(out=t2_, in0=pbk, in1=gB_[:, oc, :], op=ALU.mult), reads=[r_pbk, r_gB], writes=[r_t2])
            S.op("pool", lambda e, oc=oc, t1_=t1_, t2_=t2_: e.tensor_tensor(out=mg[:, oc, :], in0=t1_, in1=t2_, op=ALU.add), reads=[r_t1, r_t2], writes=[r_mg])
        for tt in range(4):
            ti = g * 4 + tt
            tb = ti % NB
            rows = slice(ti * 128, (ti + 1) * 128)
            x_, r_x = xt[tb]
            xm_, r_xm = xm[tb]
            ss_, r_ss = ssd[tb]
            k.load(x_, r_x, ins["x"][rows, :])
            for hf in range(2):
                pm, r_pm = _nb(k)

                def mmo(e, pm=pm, hf=hf, tt=tt):
                    for kc in range(8):
                        ins_ = e.matmul(pm, lhsT=mg[:, kc, tt * 128:(tt + 1) * 128], rhs=wo[:, kc, hf * 512:(hf + 1) * 512], start=(kc == 0), stop=(kc == 7))
                    return ins_
                S.op("pe", mmo, reads=[r_mg, r_wo], writes=[r_pm])
                S.op("dve", lambda e, pm=pm, hf=hf, xm_=xm_: e.tensor_tensor(out=xm_[:, hf * 512:(hf + 1) * 512], in0=pm, in1=k.gate1_row[:, hf * 512:(hf + 1) * 512], op=ALU.mult),
                     reads=[r_pm, k.r_gate1], writes=[r_xm])
            S.op("pool", lambda e, xm_=xm_, x_=x_: e.tensor_tensor(out=xm_, in0=xm_, in1=x_, op=ALU.add), reads=[r_xm, r_x], writes=[r_xm])
            k.store(xmid_s[rows, :], r_xmid_s, xm_, r_xm)
            k.store(k.out[rows, :], k.out_res, xm_, r_xm)
            S.op("act", lambda e, xm_=xm_, ss_=ss_: e.activation(out=junk, in_=xm_, func=AF.Square, accum_out=ss_[:, 0:1]), reads=[r_xm], writes=[r_junk, r_ss])
            _rstd(k, ss_[:, 0:1], r_ss, D, ss_[:, 1:2], r_ss)
            S.op("act", lambda e, xm_=xm_, ss_=ss_: e.activation(out=h2f, in_=xm_, func=AF.Copy, scale=ss_[:, 1:2]), reads=[r_xm, r_ss], writes=[r_h2f])
            S.op("pool", lambda e: e.tensor_tensor(out=h2f, in0=h2f, in1=k.s2_row, op=ALU.mult), reads=[r_h2f, k.r_s2row], writes=[r_h2f])
            S.op("pool", lambda e: e.tensor_tensor(out=h2f, in0=h2f, in1=k.shift2_row, op=ALU.add), reads=[r_h2f, k.r_shift2], writes=[r_h2f])
            h2b_, r_h2b = h2b[tb]
            S.op("act", lambda e, h2b_=h2b_: e.activation(out=h2b_, in_=h2f, func=AF.Copy), reads=[r_h2f], writes=[r_h2b])
            k.store(h2_s[rows, :], r_h2_s, h2b_, r_h2b)
            for hf in range(2):
                pt, r_pt = _nb(k)
                pv = pt.rearrange("p (a b) -> p a b", b=128)

                def trh(e, pv=pv, hf=hf):
                    for j in range(4):
                        ins_ = e.transpose(out=pv[:, j, :], in_=h2f[:, (hf * 4 + j) * 128:(hf * 4 + j + 1) * 128], identity=k.ident_f)
                    return ins_
                S.op("pe", trh, reads=[r_h2f, k.r_ident_f], writes=[r_pt])
                if hf == 0:
                    S.op("act", lambda e, pv=pv: e.activation(out=h2T[:, 0:4, :], in_=pv, func=AF.Copy), reads=[r_pt], writes=[r_h2T])
                else:
                    S.op("dve", lambda e, pv=pv: e.tensor_copy(out=h2T[:, 4:8, :], in_=pv), reads=[r_pt], writes=[r_h2T])
            pl, r_pl = _nb(k)

            def mml(e, pl=pl):
                for kc in range(8):
                    ins_ = e.matmul(pl[:, 0:16], lhsT=h2T[:, kc, :], rhs=rw[:, kc, :], start=(kc == 0), stop=(kc == 7))
                return ins_
            S.op("pe", mml, reads=[r_h2T, r_rw], writes=[r_pl])
            ex_, r_ex = ex[tb]
            S.op("dve", lambda e, pl=pl, ss_=ss_: e.tensor_reduce(out=ss_[:, 2:3], in_=pl[:, 0:16], axis=AX.X, op=ALU.max), reads=[r_pl], writes=[r_ss])
            S.op("dve", lambda e, ss_=ss_: e.tensor_scalar(out=ss_[:, 3:4], in0=ss_[:, 2:3], scalar1=-1.0, scalar2=None, op0=ALU.mult), reads=[r_ss], writes=[r_ss])
            S.op("act", lambda e, pl=pl, ss_=ss_, ex_=ex_: e.activation(out=ex_, in_=pl[:, 0:16], func=AF.Exp, bias=ss_[:, 3:4], accum_out=ss_[:, 4:5]),
                 reads=[r_pl, r_ss], writes=[r_ex, r_ss])
            S.op("dve", lambda e, ss_=ss_: e.reciprocal(out=ss_[:, 5:6], in_=ss_[:, 4:5]), reads=[r_ss], writes=[r_ss])
            S.op("dve", lambda e, ss_=ss_, ex_=ex_: e.tensor_scalar(out=ex_, in0=ex_, scalar1=ss_[:, 5:6], scalar2=None, op0=ALU.mult), reads=[r_ex, r_ss], writes=[r_ex])
            k.store(aff_s[rows, :], r_aff_s, ex_, r_ex)
            pt2, r_pt2 = _nb(k)
            S.op("pe", lambda e, pt2=pt2, ex_=ex_: e.transpose(out=pt2[0:16, 0:128], in_=ex_, identity=k.ident_f), reads=[r_ex, k.r_ident_f], writes=[r_pt2])
            aT_, r_aT = affT[tb]
            S.op("act", lambda e, pt2=pt2, aT_=aT_: e.activation(out=aT_, in_=pt2[0:16, 0:128], func=AF.Copy), reads=[r_pt2], writes=[r_aT])
            k.store(affT_s[:, rows], r_affT_s, aT_, r_aT)
    S.barrier()
    A.release()

NE = 16
CAP = 512
FF = 1408
NFC = 11


def phase_e(k):
    S, A, nc, ins = k.S, k.A, k.nc, k.ins
    aff_s, r_aff_s = k.scr["aff_s"]
    affT_s, r_affT_s = k.scr["affT_s"]
    h2_s, r_h2_s = k.scr["h2_s"]
    xmid_s, r_xmid_s = k.scr["xmid_s"]
    posmT_s, r_posmT_s = k.scratch("posmT_s", [NE, TX], F32)
    gc_s, r_gc_s = k.scratch("gc_s", [NE, 128, 4], F32)
    idx_s, r_idx_s = k.scratch("idx_s", [NE, 128, 4], I32)
    A.off = k.off_after_gate2
    A.mark()
    cst, r_cst = k.tile([128, 1024], F32, "cst")
    k.load(cst, r_cst, ins["consts"])
    blk, r_blk = k.tile([128, 128], F32, "blk")
    k.load(blk, r_blk, ins["moe_blk"])
    sel8, r_sel8 = k.tile([128, 16], F32, "sel8")
    k.load(sel8, r_sel8, ins["moe_sel8"])
    tris, r_tris = k.tile([128, 128], BF16, "tris")
    k.wload(tris, r_tris, ins["moe_tris"])
    iota_c = cst[:, 0:512]
    A.mark()
    A8, r_A8 = k.tile([128, 512], F32, "A8")
    k.load(A8, r_A8, affT_s.rearrange("e (s t) -> (e s) t", s=8), r_affT_s)
    junk, r_junk = k.tile([128, 512], F32, "junk")
    sc, r_sc = k.tile([128, 16], F32, "sc")
    S.op("pool", lambda e: e.memset(sc, 0.0), writes=[r_sc])
    S.op("pool", lambda e: e.memset(sc[:, 1:2], 1.0), reads=[r_sc], writes=[r_sc])
    for it in range(30):
        S.op("dve", lambda e: e.tensor_tensor(out=sc[:, 2:3], in0=sc[:, 0:1], in1=sc[:, 1:2], op=ALU.add), reads=[r_sc], writes=[r_sc])
        S.op("dve", lambda e: e.tensor_scalar(out=sc[:, 2:3], in0=sc[:, 2:3], scalar1=0.5, scalar2=None, op0=ALU.mult), reads=[r_sc], writes=[r_sc])
        S.op("dve", lambda e: e.tensor_scalar(out=junk, in0=A8, scalar1=sc[:, 2:3], scalar2=0.0, op0=ALU.is_ge, op1=ALU.add, accum_out=sc[:, 3:4]),
             reads=[r_A8, r_sc], writes=[r_junk, r_sc])
        pb, r_pb = _nb(k)
        S.op("pe", lambda e, pb=pb: e.matmul(pb[:, 0:1], lhsT=blk, rhs=sc[:, 3:4], start=True, stop=True), reads=[r_blk, r_sc], writes=[r_pb])
        S.op("dve", lambda e, pb=pb: e.tensor_scalar(out=sc[:, 4:5], in0=pb[:, 0:1], scalar1=CAP - 0.5, scalar2=None, op0=ALU.is_ge), reads=[r_pb], writes=[r_sc])
        S.op("dve", lambda e: e.tensor_scalar(out=sc[:, 5:6], in0=sc[:, 4:5], scalar1=-1.0, scalar2=1.0, op0=ALU.mult, op1=ALU.add), reads=[r_sc], writes=[r_sc])
        S.op("dve", lambda e: e.tensor_tensor(out=sc[:, 6:7], in0=sc[:, 2:3], in1=sc[:, 0:1], op=ALU.subtract), reads=[r_sc], writes=[r_sc])
        S.op("dve", lambda e: e.tensor_tensor(out=sc[:, 7:8], in0=sc[:, 1:2], in1=sc[:, 2:3], op=ALU.subtract), reads=[r_sc], writes=[r_sc])
        S.op("dve", lambda e: e.scalar_tensor_tensor(out=sc[:, 0:1], in0=sc[:, 6:7], scalar=sc[:, 4:5], in1=sc[:, 0:1], op0=ALU.mult, op1=ALU.add), reads=[r_sc], writes=[r_sc])
        S.op("dve", lambda e: e.scalar_tensor_tensor(out=sc[:, 1:2], in0=sc[:, 7:8], scalar=sc[:, 4:5], in1=sc[:, 2:3], op0=ALU.mult, op1=ALU.add), reads=[r_sc], writes=[r_sc])
        S.op("dve", lambda e: e.memset(sc[:, 3:4], 0.0), reads=[r_sc], writes=[r_sc])
    thrrep, r_thrrep = k.tile([128, 128], F32, "thrrep")
    S.op("dve", lambda e: e.tensor_copy(out=thrrep, in_=sc[:, 0:1].to_broadcast([128, 128])), reads=[r_sc], writes=[r_thrrep])
    pb, r_pb = _nb(k)
    S.op("pe", lambda e, pb=pb: e.matmul(pb[:, 0:16], lhsT=thrrep, rhs=sel8, start=True, stop=True), reads=[r_thrrep, r_sel8], writes=[r_pb])
    thr_row, r_thr = k.tile([128, 16], F32, "thr_row")
    S.op("act", lambda e, pb=pb: e.activation(out=thr_row, in_=pb[:, 0:16], func=AF.Copy), reads=[r_pb], writes=[r_thr])
    import os as _os
    if _os.environ.get("E_DBG"):
        k.dump("sc", sc, r_sc, [128, 16])
        k.dump("thr_row", thr_row, r_thr, [128, 16])
        k.dump("A8", A8, r_A8, [128, 512])
        S.barrier()
        A.release()
        A.release()
        return
    aff, r_aff = k.tile([128, 32, 16], F32, "aff")
    k.load(aff, r_aff, aff_s.rearrange("(n p) e -> p n e", p=128), r_aff_s)
    maskf, r_maskf = k.tile([128, 32, 16], F32, "maskf")
    maskb, r_maskb = k.tile([128, 32, 16], BF16, "maskb")
    posm, r_posm = k.tile([128, 32, 16], F32, "posm")
    parts, r_parts = k.tile([128, 32, 16, 5], BF16, "parts")
    tokp, r_tokp = k.tile([128, 32, 2], F32, "tokp")
    k.load(tokp, r_tokp, ins["moe_tok"])
    rem, r_rem = k.tile([128, 32, 16], F32, "rem")
    S.op("dve", lambda e: e.tensor_tensor(out=maskf, in0=aff, in1=thr_row.unsqueeze(1).to_broadcast([128, 32, 16]), op=ALU.is_ge),
         reads=[r_aff, r_thr], writes=[r_maskf])
    S.op("act", lambda e: e.activation(out=maskb, in_=maskf, func=AF.Copy), reads=[r_maskf], writes=[r_maskb])
    pp, r_pp = _nb(k)
    ppv = pp.rearrange("p (n e) -> p n e", e=16)

    def mmpos(e):
        for n in range(32):
            for m in range(n):
                e.matmul(ppv[:, n, :], lhsT=k.ones_b, rhs=maskb[:, m, :], start=(m == 0), stop=False)
            ins_ = e.matmul(ppv[:, n, :], lhsT=tris, rhs=maskb[:, n, :], start=(n == 0), stop=True)
        return ins_
    S.op("pe", mmpos, reads=[r_maskb, k.r_ones_b, r_tris], writes=[r_pp])
    S.op("dve", lambda e: e.scalar_tensor_tensor(out=posm, in0=ppv, scalar=1.0, in1=maskf, op0=ALU.add, op1=ALU.mult), reads=[r_pp, r_maskf], writes=[r_posm])
    S.op("dve", lambda e: e.tensor_scalar(out=posm, in0=posm, scalar1=-1.0, scalar2=None, op0=ALU.add), reads=[r_posm], writes=[r_posm])
    S.op("act", lambda e: e.activation(out=parts[:, :, :, 0], in_=aff, func=AF.Copy), reads=[r_aff], writes=[r_parts])
    S.op("dve", lambda e: e.tensor_tensor(out=rem, in0=aff, in1=parts[:, :, :, 0], op=ALU.subtract), reads=[r_aff, r_parts], writes=[r_rem])
    S.op("act", lambda e: e.activation(out=parts[:, :, :, 1], in_=rem, func=AF.Copy), reads=[r_rem], writes=[r_parts])
    S.op("dve", lambda e: e.tensor_tensor(out=rem, in0=rem, in1=parts[:, :, :, 1], op=ALU.subtract), reads=[r_rem, r_parts], writes=[r_rem])
    S.op("act", lambda e: e.activation(out=parts[:, :, :, 2], in_=rem, func=AF.Copy), reads=[r_rem], writes=[r_parts])
    S.op("dve", lambda e: e.tensor_copy(out=parts[:, :, :, 3:5], in_=tokp.unsqueeze(2).to_broadcast([128, 32, 16, 2])), reads=[r_tokp, r_parts], writes=[r_parts])
    pmTs = [k.tile([16, 512], F32, "pmT") for _ in range(2)]
    for g in range(8):
        pt, r_pt = _nb(k)

        def trp(e, pt=pt, g=g):
            for j in range(4):
                ins_ = e.transpose(out=pt[0:16, j * 128:(j + 1) * 128], in_=posm[:, g * 4 + j, :], identity=k.ident_f)
            return ins_
        S.op("pe", trp, reads=[r_posm, k.r_ident_f], writes=[r_pt])
        pmT, r_pmT = pmTs[g % 2]
        S.op("act", lambda e, pt=pt, pmT=pmT: e.activation(out=pmT, in_=pt[0:16, :], func=AF.Copy), reads=[r_pt], writes=[r_pmT])
        k.store(posmT_s[:, g * 512:(g + 1) * 512], r_posmT_s, pmT, r_pmT)
    if _os.environ.get("E_STOP") == "e1":
        S.barrier(); A.release(); A.release(); return
    Sel = [k.tile([128, 32, CAP], BF16, "Sel") for _ in range(2)]
    Sel_res = [(Res("sel0"), Res("sel1")) for _ in range(2)]
    gcs = [k.tile([128, 4], F32, "gcs") for _ in range(2)]
    idf = [k.tile([128, 4], F32, "idf") for _ in range(2)]
    idi = [k.tile([128, 4], I32, "idi") for _ in range(2)]
    for ex in range(NE):
        Sel_t, _ = Sel[ex % 2]
        r_Sel0, r_Sel1 = Sel_res[ex % 2]
        gcs_t, r_gcs = gcs[ex % 2]
        idf_t, r_idf = idf[ex % 2]
        idi_t, r_idi = idi[ex % 2]

        def bsel(e, Sel_t=Sel_t, ex=ex, par=0):
            for n in range(par, 32, 2):
                ins_ = e.tensor_scalar(out=Sel_t[:, n, :], in0=iota_c, scalar1=posm[:, n, ex:ex + 1], scalar2=None, op0=ALU.is_equal)
            return ins_
        if _os.environ.get("E_X") != "nodve":
            S.op("dve", lambda e, f=bsel: f(e, par=0), reads=[r_cst, r_posm], writes=[r_Sel0])
        S.op("dve", lambda e, f=bsel: f(e, par=1), reads=[r_cst, r_posm], writes=[r_Sel1])
        pq, r_pq = _nb(k)

        def mmgate(e, pq=pq, Sel_t=Sel_t, ex=ex):
            for cc in range(4):
                for n in range(32):
                    ins_ = e.matmul(pq[:, cc * 8:cc * 8 + 5], lhsT=Sel_t[:, n, cc * 128:(cc + 1) * 128], rhs=parts[:, n, ex, :], start=(n == 0), stop=(n == 31))
            return ins_
        if _os.environ.get("E_X") != "nomm":
            S.op("pe", mmgate, reads=[r_Sel0, r_Sel1, r_parts], writes=[r_pq])
        pq3 = pq[:, 0:32].rearrange("p (a b) -> p a b", b=8)
        S.op("dve", lambda e, pq3=pq3, gcs_t=gcs_t: e.tensor_reduce(out=gcs_t, in_=pq3[:, :, 0:3], axis=AX.X, op=ALU.add), reads=[r_pq], writes=[r_gcs])
        S.op("dve", lambda e, pq3=pq3, idf_t=idf_t: e.tensor_scalar(out=idf_t, in0=pq3[:, :, 3], scalar1=128.0, scalar2=None, op0=ALU.mult), reads=[r_pq], writes=[r_idf])
        S.op("dve", lambda e, pq3=pq3, idf_t=idf_t: e.tensor_tensor(out=idf_t, in0=idf_t, in1=pq3[:, :, 4], op=ALU.add), reads=[r_pq, r_idf], writes=[r_idf])
        S.op("dve", lambda e, idf_t=idf_t, idi_t=idi_t: e.tensor_copy(out=idi_t, in_=idf_t), reads=[r_idf], writes=[r_idi])
        k.store(gc_s[ex], r_gc_s, gcs_t, r_gcs)
        k.store(idx_s[ex], r_idx_s, idi_t, r_idi)
    S.barrier()
    A.release()
    if _os.environ.get("E_STOP") == "ea1":
        A.release(); return
    A.mark()
    U32 = mybir.dt.uint32
    ig = S.pool("ig", 4)
    wg = [k.tile([128, 8, FF], BF16, "wg") for _ in range(2)]
    wu = [k.tile([128, 8, FF], BF16, "wu") for _ in range(2)]
    wd = [k.tile([128, NFC, D], BF16, "wd") for _ in range(1)]
    xg = [k.tile([128, 4, D], BF16, "xg") for _ in range(2)]
    idx2 = [k.tile([128, 4], I32, "idx2") for _ in range(2)]
    gc2 = [k.tile([128, 4], F32, "gc2") for _ in range(2)]
    xeT_t, r_xeT = k.tile([128, 8, CAP], BF16, "xeT")
    hid, r_hid = k.tile([128, NFC, CAP], BF16, "hid")
    sg = [k.tile([128, CAP], F32, "sg") for _ in range(2)]
    yef = [k.tile([128, D], F32, "yef") for _ in range(4)]
    r_outacc = Res("outacc")
    h2_rows = h2_s

    def w_loads(ex):
        b = ex % 2
        srcg = ins["w_gate"][ex].rearrange("(kc p) f -> p kc f", p=128)
        srcu = ins["w_up"][ex].rearrange("(kc p) f -> p kc f", p=128)
        for j in range(4):
            k.wload(wg[b][0][:, 2 * j:2 * j + 2, :], wg[b][1], srcg[:, 2 * j:2 * j + 2, :])
        for j in range(4):
            k.wload(wu[b][0][:, 2 * j:2 * j + 2, :], wu[b][1], srcu[:, 2 * j:2 * j + 2, :])

    def wd_loads(ex):
        srcd = ins["w_down"][ex].rearrange("(fc p) d -> p fc d", p=128)
        for (a0, a1) in ((0, 3), (3, 6), (6, 9), (9, 11)):
            k.wload(wd[0][0][:, a0:a1, :], wd[0][1], srcd[:, a0:a1, :])

    def g_loads(ex):
        b = ex % 2
        k.load(idx2[b][0], idx2[b][1], idx_s[ex], r_idx_s)
        k.load(gc2[b][0], gc2[b][1], gc_s[ex], r_gc_s, q="act")
        for cc in range(4):
            S.dma("pool", ig, lambda e, b=b, cc=cc: e.indirect_dma_start(out=xg[b][0][:, cc, :], out_offset=None, in_=h2_rows,
                                                                       in_offset=bass.IndirectOffsetOnAxis(idx2[b][0].bitcast(U32)[:, cc:cc + 1], 0)),
                  reads=[idx2[b][1], r_h2_s], writes=[xg[b][1]])

    g_loads(0)
    w_loads(0)
    for ex in range(NE):
        b = ex % 2
        if ex + 1 < NE:
            g_loads(ex + 1)
            w_loads(ex + 1)
        wd_loads(ex)
        wg_t, r_wg = wg[b]
        wu_t, r_wu = wu[b]
        wd_t, r_wd = wd[0]
        xg_t, r_xg = xg[b]
        gc_t, r_gc = gc2[b]
        id_t, r_id = idx2[b]
        for kc in range(8):
            pt, r_pt = _nb(k, BF16)

            def trx(e, pt=pt, kc=kc, xg_t=xg_t):
                for cc in range(4):
                    ins_ = e.transpose(out=pt[:, cc * 128:(cc + 1) * 128], in_=xg_t[:, cc, kc * 128:(kc + 1) * 128], identity=k.ident_b)
                return ins_
            S.op("pe", trx, reads=[r_xg, k.r_ident_b], writes=[r_pt])
            if kc % 2 == 0:
                S.op("act", lambda e, pt=pt, kc=kc: e.activation(out=xeT_t[:, kc, :], in_=pt[:, 0:512], func=AF.Copy), reads=[r_pt], writes=[r_xeT])
            else:
                S.op("dve", lambda e, pt=pt, kc=kc: e.tensor_copy(out=xeT_t[:, kc, :], in_=pt[:, 0:512]), reads=[r_pt], writes=[r_xeT])
        for fc in range(NFC):
            pg, r_pg = _nb(k)
            pu, r_pu = _nb(k)

            def mmG(e, pg=pg, fc=fc, wg_t=wg_t):
                for kc in range(8):
                    ins_ = e.matmul(pg, lhsT=wg_t[:, kc, fc * 128:(fc + 1) * 128], rhs=xeT_t[:, kc, :], start=(kc == 0), stop=(kc == 7))
                return ins_
            S.op("pe", mmG, reads=[r_wg, r_xeT], writes=[r_pg])

            def mmU(e, pu=pu, fc=fc, wu_t=wu_t):
                for kc in range(8):
                    ins_ = e.matmul(pu, lhsT=wu_t[:, kc, fc * 128:(fc + 1) * 128], rhs=xeT_t[:, kc, :], start=(kc == 0), stop=(kc == 7))
                return ins_
            S.op("pe", mmU, reads=[r_wu, r_xeT], writes=[r_pu])
            sg_t, r_sg = sg[fc % 2]
            S.op("act", lambda e, pg=pg, sg_t=sg_t: e.activation(out=sg_t, in_=pg, func=AF.Silu), reads=[r_pg], writes=[r_sg])
            S.op("dve", lambda e, pu=pu, sg_t=sg_t, fc=fc: e.tensor_tensor(out=hid[:, fc, :], in0=pu, in1=sg_t, op=ALU.mult), reads=[r_pu, r_sg], writes=[r_hid])
        for cc in range(4):
            y_t, r_y = yef[cc]
            for hf in range(2):
                pd, r_pd = _nb(k)

                def mmD(e, pd=pd, cc=cc, hf=hf):
                    for fc in range(NFC):
                        ins_ = e.matmul(pd, lhsT=hid[:, fc, cc * 128:(cc + 1) * 128], rhs=wd_t[:, fc, hf * 512:(hf + 1) * 512], start=(fc == 0), stop=(fc == NFC - 1))
                    return ins_
                S.op("pe", mmD, reads=[r_hid, r_wd], writes=[r_pd])
                S.op("act", lambda e, pd=pd, cc=cc, hf=hf, y_t=y_t, gc_t=gc_t: e.activation(out=y_t[:, hf * 512:(hf + 1) * 512], in_=pd, func=AF.Copy, scale=gc_t[:, cc:cc + 1]),
                     reads=[r_pd, r_gc], writes=[r_y])
            S.op("pool", lambda e, y_t=y_t: e.tensor_tensor(out=y_t, in0=y_t, in1=k.gate2_row, op=ALU.mult), reads=[r_y, k.r_gate2], writes=[r_y])
            S.dma("pool", ig, lambda e, y_t=y_t, id_t=id_t, cc=cc: e.indirect_dma_start(out=k.out, out_offset=bass.IndirectOffsetOnAxis(id_t.bitcast(U32)[:, cc:cc + 1], 0),
                                                                                   in_=y_t, in_offset=None, compute_op=ALU.add),
                  reads=[r_y, r_id, k.out_res], writes=[r_outacc])
    S.barrier()
    A.release()
    A.release()

def _rope_tables():
    rows, gw = 64, 64
    row = np.repeat(np.arange(rows), gw).astype(np.float32)
    col = np.tile(np.arange(gw), rows).astype(np.float32)
    n_freq = 16
    inv_freq = (10000.0 ** (-np.arange(n_freq, dtype=np.float32) / n_freq)).astype(np.float32)
    ang_r = row[:, None] * inv_freq
    ang_c = col[:, None] * inv_freq
    ang = np.concatenate([ang_r, ang_r, ang_c, ang_c], axis=-1).astype(np.float32)
    cos = np.cos(ang).astype(np.float32)
    sin = np.sin(ang).astype(np.float32)
    sgn = np.concatenate([-np.ones(16), np.ones(16), -np.ones(16), np.ones(16)]).astype(np.float32)
    return cos, (sin * sgn).astype(np.float32)


def prep_shared(inp):
    f = np.float32
    sh = {}
    sh["ada_w"] = np.ascontiguousarray(inp["ada_w"][0])
    sh["ada_b_row"] = np.ascontiguousarray(inp["ada_b"][0][None, :])
    sh["ada_bT"] = np.ascontiguousarray(inp["ada_b"][0].reshape(48, 128).T)
    sh["g1T"] = np.ascontiguousarray(inp["norm1_g"][0].reshape(8, 128).T)
    sh["g2T"] = np.ascontiguousarray(inp["norm2_g"][0].reshape(8, 128).T)
    sh["g2_rep"] = np.ascontiguousarray(np.broadcast_to(inp["norm2_g"][0].reshape(1, 1024), (128, 1024)))
    sh["w_in"] = np.ascontiguousarray(inp["w_in"][0])
    sh["convT"] = np.ascontiguousarray(inp["conv_w"][0].T.reshape(24, 128, 5).transpose(1, 0, 2))
    sh["alog_rep"] = np.ascontiguousarray(np.broadcast_to(inp["a_log"][0].reshape(1, 16), (128, 16)))
    sh["dtb_rep"] = np.ascontiguousarray(np.broadcast_to(inp["dt_bias"][0].reshape(1, 16), (128, 16)))
    sh["dng_rep"] = np.ascontiguousarray(np.broadcast_to(inp["dn_norm_g"][0].reshape(1, 128), (128, 128)))
    sh["gqaT"] = np.ascontiguousarray(inp["q_a_norm_g"][0].reshape(3, 128).T)
    sh["w_uq"] = np.ascontiguousarray(inp["w_uq"][0])
    sh["gkvaT"] = np.ascontiguousarray(inp["kv_a_norm_g"][0].reshape(2, 128).T)
    sh["w_ukv"] = np.ascontiguousarray(inp["w_ukv"][0])
    sh["gq_rep"] = np.ascontiguousarray(np.broadcast_to(inp["q_norm_g"][0].reshape(1, 192), (128, 192)))
    sh["gk_rep"] = np.ascontiguousarray(np.broadcast_to(inp["k_norm_g"][0].reshape(1, 192), (128, 192)))
    sh["w_out_a"] = np.ascontiguousarray(inp["w_out_a"][0])
    sh["w_out_b"] = np.ascontiguousarray(inp["w_out_b"][0])
    sh["w_o"] = np.ascontiguousarray(inp["w_o"][0])
    sh["router_w"] = np.ascontiguousarray(inp["router_w"][0])
    sh["w_gate"] = np.ascontiguousarray(inp["w_gate"][0])
    sh["w_up"] = np.ascontiguousarray(inp["w_up"][0])
    sh["w_down"] = np.ascontiguousarray(inp["w_down"][0])
    cos, sinS = _rope_tables()
    sh["rope_cs"] = np.ascontiguousarray(np.concatenate([cos, sinS], axis=1))
    sh["ident"] = np.eye(128, dtype=f)
    consts = np.zeros((128, 1024), f)
    consts[:, 0:512] = np.arange(512, dtype=f)[None, :]
    consts[:, 512] = np.arange(128, dtype=f)
    sh["consts"] = consts
    pp_ = np.arange(128)
    sh["moe_blk"] = (pp_[:, None] // 8 == pp_[None, :] // 8).astype(f)
    s8 = np.zeros((128, 16), f)
    s8[np.arange(16) * 8, np.arange(16)] = 1.0
    sh["moe_sel8"] = s8
    tk = np.zeros((128, 32, 2), f)
    tk[:, :, 0] = np.arange(32, dtype=f)[None, :]
    tk[:, :, 1] = np.arange(128, dtype=f)[:, None]
    sh["moe_tok"] = tk
    sh["moe_tris"] = (pp_[:, None] < pp_[None, :]).astype(f)
    ii = np.arange(128)
    P, Fr = ii[:, None], ii[None, :]
    NEGV = -30000.0
    mk = np.zeros((128, 9, 128), f)
    mk[:, 0] = (P <= Fr)
    mk[:, 1] = (P >= Fr)
    mk[:, 2] = np.where(P > Fr, 0.0, NEGV)
    mk[:, 3] = np.where(P < Fr, 0.0, NEGV)
    mk[:, 4] = np.where(Fr >= P, 0.0, NEGV)
    mk[:, 5] = np.where(Fr <= P, 0.0, NEGV)
    mk[:, 6] = (P // 32 == Fr // 32)
    mk[:, 7] = (P // 64 == Fr // 64) & (P // 32 != Fr // 32)
    mk[:, 8] = (P // 64 != Fr // 64)
    sh["dn_masks"] = mk
    es = np.zeros((64, 2, 8, 128), f)
    for hh in range(8):
        es[hh, 0, hh, :] = 1.0
        es[32 + hh, 0, hh, :] = 1.0
    es[:, 1] = -es[:, 0]
    sh["dn_esel"] = es
    li = np.zeros((64, 128), f)
    li[32:40] = 1.0
    sh["dn_linit"] = li
    return sh


def prep_core(inp, sh, b):
    m = dict(sh)
    m["x"] = np.ascontiguousarray(inp["x"][b])
    m["ctx"] = np.ascontiguousarray(inp["ctx"][b])
    cc = np.stack([inp["c"][b], inp["c_ctx"]], axis=-1).astype(np.float32)
    m["c2"] = np.ascontiguousarray(cc.reshape(8, 128, 2).transpose(1, 0, 2))
    return m

PHASES = ["a0", "a1", "a2", "b", "c", "d", "e"]


def build(upto="e", dbg=(), dumps=()):
    k = K(dbg=dbg)
    declare_inputs(k)
    setup_consts(k)
    k.dump_list = []

    def dump(name, ap, res, shape):
        t = k.nc.dram_tensor("dbg_" + name, list(shape), ap.dtype, kind="ExternalOutput").ap()
        k.store(t, None, ap, res)
        k.dump_list.append("dbg_" + name)
    k.dump = dump
    k.dumps = set(dumps)
    fns = {"a0": phase_a0}
    for nm in ("a1", "a2", "b", "c", "d", "e"):
        f = globals().get("phase_" + nm)
        if f is not None:
            fns[nm] = f
    for ph in PHASES:
        if ph in fns:
            fns[ph](k)
        if ph == upto:
            break
    k.S.emit()
    return k


_CACHE = {}


def kernel(**inputs):
    inp = {kk: np.asarray(v) for kk, v in inputs.items()}
    sh = prep_shared(inp)
    in_maps = [prep_core(inp, sh, b) for b in range(8)]
    k = build()
    res = run_bass_kernel_spmd(k.nc, in_maps, core_ids=list(range(8)))
    out = np.stack([np.asarray(r["out"]) for r in res.results], axis=0).astype(np.float32)
    return out
```

```python
import numpy as np
import concourse.bass as bass
import concourse.mybir as mybir
from concourse.bass_utils import run_bass_kernel_spmd

F32 = mybir.dt.float32
BF16 = mybir.dt.bfloat16
F32R = mybir.dt.float32r
I32 = mybir.dt.int32
AF = mybir.ActivationFunctionType
ALU = mybir.AluOpType
AX = mybir.AxisListType

ENGS = ("pe", "act", "dve", "pool", "sp")
EPOCH = 12000


class Res:
    __slots__ = ("name", "w", "rs", "multi", "ws", "excl")

    def __init__(self, name="", multi=False, excl=False):
        self.excl = excl
        self.name = name
        self.w = None
        self.rs = []
        self.multi = multi
        self.ws = []


class Tok:
    __slots__ = ("key", "val", "eng")

    def __init__(self, key, val, eng):
        self.key = key
        self.val = val
        self.eng = eng


class DmaPool:
    def __init__(self, sched, name, n):
        self.s = sched
        self.name = name
        self.n = n
        self.i = 0
        self.count = [0] * n
        self.last = [None] * n

    def keys(self):
        return [("dma", self.name, j) for j in range(self.n)]


class Sched:
    def __init__(self, nc):
        self.nc = nc
        self.ops = {e: [] for e in ENGS}
        self.cnt = {e: 0 for e in ENGS}
        self.pools = []
        self.last_tok = {e: None for e in ENGS}
        self.n_instr = 0

    def pool(self, name, n):
        p = DmaPool(self, name, n)
        self.pools.append(p)
        return p

    def _deps(self, eng, reads, writes):
        deps = []
        for r in reads:
            if r.multi:
                deps.extend(r.ws)
            elif r.w is not None:
                deps.append(r.w)
            if r.excl:
                deps.extend(t for t in r.rs if t.eng != eng)
        for w in writes:
            if w.multi:
                pass
            elif w.w is not None and w.w.eng != eng:
                deps.append(w.w)
            for t in w.rs:
                if t.eng != eng:
                    deps.append(t)
        return deps

    def _mark_w(self, writes, tok):
        for w in writes:
            if w.multi:
                w.ws.append(tok)
            else:
                w.w = tok
                w.rs = []

    def op(self, eng, fn, reads=(), writes=(), extra=()):
        deps = self._deps(eng, reads, writes) + list(extra)
        c = self.cnt[eng]
        tok = Tok(("eng", eng, c // EPOCH), c % EPOCH + 1, eng)
        self.cnt[eng] = c + 1
        for r in reads:
            r.rs.append(tok)
        self._mark_w(writes, tok)
        self.ops[eng].append((deps, fn, tok, 1))
        self.last_tok[eng] = tok
        return tok

    def dma(self, eng, pool, fn, reads=(), writes=(), extra=()):
        deps = self._deps('__dma__', reads, writes) + list(extra)
        j = pool.i
        pool.i = (pool.i + 1) % pool.n
        if pool.last[j] is not None:
            deps.append(pool.last[j])
        pool.count[j] += 16
        tok = Tok(("dma", pool.name, j), pool.count[j], None)
        pool.last[j] = tok
        for r in reads:
            r.rs.append(tok)
        self._mark_w(writes, tok)
        self.ops[eng].append((deps, fn, tok, 16))
        return tok

    def barrier(self):
        toks = [t for t in self.last_tok.values() if t is not None]
        for p in self.pools:
            toks += [t for t in p.last if t is not None]
        for e in ENGS:
            self.ops[e].append((list(toks), None, None, 0))

    def emit(self, final_waits_eng="sp"):
        nc = self.nc
        sems = {}

        def sem_of(key):
            if key not in sems:
                sems[key] = nc.alloc_semaphore("s_" + "_".join(str(k) for k in key))
            return sems[key]

        for e in ENGS:
            for ep in range((self.cnt[e] + EPOCH - 1) // EPOCH):
                sem_of(("eng", e, ep))
        for p in self.pools:
            for k in p.keys():
                sem_of(k)

        toks = [t for t in self.last_tok.values() if t is not None]
        for p in self.pools:
            toks += [t for t in p.last if t is not None]
        self.ops[final_waits_eng].append((list(toks), None, None, 0))

        engobj = {"pe": "tensor", "act": "scalar", "dve": "vector", "pool": "gpsimd", "sp": "sync"}
        sched = self

        def run(ename):
            def body(eng):
                seen = {}
                for deps, fn, tok, inc in sched.ops[ename]:
                    need = {}
                    for t in deps:
                        if t.val > need.get(t.key, 0):
                            need[t.key] = t.val
                    for k, v in need.items():
                        if seen.get(k, 0) >= v:
                            continue
                        seen[k] = v
                        eng.wait_ge(sem_of(k), v)
                        sched.n_instr += 1
                    if fn is not None:
                        ins = fn(eng)
                        ins.then_inc(sem_of(tok.key), inc)
                        sched.n_instr += 1
            return body

        with nc.Block() as block:
            for ename in ENGS:
                getattr(block, engobj[ename])(run(ename))


class Arena:
    def __init__(self, nc, kbytes=198):
        self.nc = nc
        self.words = kbytes * 256
        self.t = nc.alloc_sbuf_tensor("arena", [128, self.words], F32)
        self.ap = self.t.ap()
        self.off = 0
        self.marks = []
        self.peak = 0

    def tile(self, shape, dtype, name=None):
        esz = {F32: 4, BF16: 2, I32: 4}[dtype]
        n = int(np.prod(shape[1:]))
        nw = (n * esz + 3) // 4
        off = (self.off + 15) // 16 * 16
        assert off + nw <= self.words, f"SBUF overflow {off}+{nw} > {self.words}"
        self.off = off + nw
        self.peak = max(self.peak, self.off)
        a = self.ap[0:shape[0], off:off + nw]
        if dtype != F32:
            a = a.bitcast(dtype)
        a = a[:, 0:n]
        if len(shape) == 3:
            a = a.rearrange("p (a b) -> p a b", a=shape[1])
        elif len(shape) == 4:
            a = a.rearrange("p (a b c) -> p a b c", a=shape[1], b=shape[2])
        return a

    def mark(self):
        self.marks.append(self.off)

    def release(self):
        self.off = self.marks.pop()

D = 1024
T = 4352
NT = 34
TX = 4096
NCTX = 256
H = 8
OFF_Z = 3072
OFF_GATE = 4832
D_IN = 6880
NMID = 1760
EPS = 1e-6
NEG = -30000.0


class K:
    def __init__(self, dbg=()):
        self.nc = bass.Bass("TRN2", target_bir_lowering=False)
        self.S = Sched(self.nc)
        self.A = Arena(self.nc)
        self.dbg = set(dbg)
        self.ins = {}
        self.scr = {}
        nc = self.nc
        self.ps = nc.alloc_psum_tensor("ps", [128, 8, 512], F32).ap()
        self.psr = [Res(f"ps{b}", excl=True) for b in range(8)]
        self.ld = self.S.pool("ld", 8)
        self.st = self.S.pool("st", 8)
        self.wl = self.S.pool("wl", 6)

    def inp(self, name, shape, dtype=F32):
        t = self.nc.dram_tensor(name, list(shape), dtype, kind="ExternalInput").ap()
        self.ins[name] = t
        return t

    def scratch(self, name, shape, dtype):
        kind = "ExternalOutput" if name in self.dbg else "Internal"
        t = self.nc.dram_tensor(name, list(shape), dtype, kind=kind).ap()
        self.scr[name] = (t, Res(name, multi=True))
        return t, self.scr[name][1]

    def bank(self, b, dtype=F32):
        a = self.ps[:, b, :]
        if dtype == BF16:
            a = a.bitcast(BF16)
        return a, self.psr[b]

    def tile(self, shape, dtype, name=None):
        return self.A.tile(shape, dtype, name), Res(name or "t")

    def load(self, dst, dres, src, sres=None, q="sp", pool=None):
        return self.S.dma(q, pool or self.ld, lambda e: e.dma_start(out=dst, in_=src),
                          reads=[sres] if sres is not None else [], writes=[dres])

    def store(self, dst, dres, src, sres, q="sp", pool=None):
        return self.S.dma(q, pool or self.st, lambda e: e.dma_start(out=dst, in_=src),
                          reads=[sres], writes=[dres] if dres is not None else [])

    def wload(self, dst, dres, src):
        return self.S.dma("pool", self.wl, lambda e: e.dma_start(out=dst, in_=src), writes=[dres])


def declare_inputs(k):
    i = k.inp
    i("x", [TX, D]); i("ctx", [NCTX, D]); i("c2", [128, 8, 2])
    i("ada_w", [D, 6 * D]); i("ada_b_row", [1, 6 * D]); i("ada_bT", [128, 48])
    i("g1T", [128, 8]); i("g2T", [128, 8]); i("g2_rep", [128, D])
    i("w_in", [D, D_IN]); i("convT", [128, 24, 5])
    i("alog_rep", [128, 16]); i("dtb_rep", [128, 16]); i("dng_rep", [128, 128])
    i("gqaT", [128, 3]); i("w_uq", [384, 1536]); i("gkvaT", [128, 2]); i("w_ukv", [256, 2048])
    i("gq_rep", [128, 192]); i("gk_rep", [128, 192])
    i("w_out_a", [D, D]); i("w_out_b", [D, D]); i("w_o", [D, D])
    i("router_w", [D, 16]); i("w_gate", [16, D, 1408]); i("w_up", [16, D, 1408]); i("w_down", [16, 1408, D])
    i("rope_cs", [TX, 128])
    i("ident", [128, 128]); i("consts", [128, 1024])
    i("moe_tok", [128, 32, 2]); i("moe_blk", [128, 128]); i("moe_sel8", [128, 16]); i("moe_tris", [128, 128])
    i("dn_masks", [128, 9, 128]); i("dn_esel", [64, 2, 8, 128]); i("dn_linit", [64, 128])
    k.out = k.nc.dram_tensor("out", [TX, D], F32, kind="ExternalOutput").ap()
    k.out_res = Res("out", multi=True)


def setup_consts(k):
    S = k.S
    k.ident_f, k.r_ident_f = k.tile([128, 128], F32, "identf")
    k.ident_b, k.r_ident_b = k.tile([128, 128], BF16, "identb")
    k.load(k.ident_f, k.r_ident_f, k.ins["ident"])
    S.op("dve", lambda e: e.tensor_copy(out=k.ident_b, in_=k.ident_f), reads=[k.r_ident_f], writes=[k.r_ident_b])
    k.ones_f, k.r_ones_f = k.tile([128, 128], F32, "onesf")
    k.ones_b, k.r_ones_b = k.tile([128, 128], BF16, "onesb")
    S.op("pool", lambda e: e.memset(k.ones_f, 1.0), writes=[k.r_ones_f])
    S.op("pool", lambda e: e.memset(k.ones_b, 1.0), writes=[k.r_ones_b])


def phase_a0(k):
    S, A, nc = k.S, k.A, k.nc
    ins = k.ins
    k.modT, k.r_modT = k.tile([128, 48, 2], F32, "modT")
    k.s1, k.r_s1 = k.tile([128, 8, 2], F32, "s1")
    k.s2, k.r_s2 = k.tile([128, 8], F32, "s2")
    k.gate1_row, k.r_gate1 = k.tile([128, D], F32, "gate1row")
    k.gate2_row, k.r_gate2 = k.tile([128, D], F32, "gate2row")
    k.off_after_gate2 = A.off
    k.shift2_row, k.r_shift2 = k.tile([128, D], F32, "shift2row")
    k.s2_row, k.r_s2row = k.tile([128, D], F32, "s2row")
    A.mark()
    c2, r_c2 = k.tile([128, 8, 2], F32, "c2")
    sc, r_sc = k.tile([128, 8, 2], F32, "sc")
    screp, r_screp = k.tile([128, 8, 128], F32, "screp")
    abT, r_abT = k.tile([128, 48], F32, "abT")
    abrow, r_abrow = k.tile([1, 6 * D], F32, "abrow")
    g1T, r_g1T = k.tile([128, 8], F32, "g1T")
    g2T, r_g2T = k.tile([128, 8], F32, "g2T")
    wbuf = [k.tile([128, 8, D], F32, f"adaw{j}") for j in range(2)]
    k.load(c2, r_c2, ins["c2"])
    k.load(abT, r_abT, ins["ada_bT"])
    k.load(abrow, r_abrow, ins["ada_b_row"])
    k.load(g1T, r_g1T, ins["g1T"])
    k.load(g2T, r_g2T, ins["g2T"])
    S.op("act", lambda e: e.activation(out=sc, in_=c2, func=AF.Silu), reads=[r_c2], writes=[r_sc])
    S.op("dve", lambda e: e.tensor_copy(out=screp, in_=sc[:, :, 0:1].to_broadcast([128, 8, 128])),
         reads=[r_sc], writes=[r_screp])
    pm, r_pm = k.bank(0)
    aw = ins["ada_w"].rearrange("(kc p) n -> p kc n", p=128)
    for sec in range(6):
        wt, r_wt = wbuf[sec % 2]
        q = "sp" if sec % 2 == 0 else "act"
        S.dma(q, k.ld, lambda e, wt=wt, sec=sec: e.dma_start(out=wt, in_=aw[:, :, sec * D:(sec + 1) * D]), writes=[r_wt])

        def mm(e, wt=wt, sec=sec):
            for fc in range(8):
                for kc in range(8):
                    ins_ = e.matmul(pm[:, (sec * 8 + fc) * 2:(sec * 8 + fc) * 2 + 2], lhsT=wt[:, kc, fc * 128:(fc + 1) * 128],
                                    rhs=sc[:, kc, :], start=(kc == 0), stop=(kc == 7))
            return ins_
        S.op("pe", mm, reads=[r_wt, r_sc], writes=[r_pm])
        if sec in (2, 3, 4, 5):
            dst, r_dst = {2: (k.gate1_row, k.r_gate1), 5: (k.gate2_row, k.r_gate2), 3: (k.shift2_row, k.r_shift2), 4: (k.s2_row, k.r_s2row)}[sec]
            for hf in range(2):
                pb, r_pb = k.bank(1 + hf)

                def mmr(e, wt=wt, sec=sec, hf=hf, pb=pb):
                    for kc in range(8):
                        e.matmul(pb, lhsT=screp[:, kc, :], rhs=wt[:, kc, hf * 512:(hf + 1) * 512], start=(kc == 0), stop=False)
                    return e.matmul(pb, lhsT=k.ones_f[0:1, :], rhs=abrow[0:1, sec * D + hf * 512: sec * D + (hf + 1) * 512],
                                    start=False, stop=True)
                S.op("pe", mmr, reads=[r_wt, r_screp, k.r_ones_f, r_abrow], writes=[r_pb])
                S.op("act", lambda e, dst=dst, hf=hf, pb=pb: e.activation(out=dst[:, hf * 512:(hf + 1) * 512], in_=pb, func=AF.Copy),
                     reads=[r_pb], writes=[r_dst])
    S.op("dve", lambda e: e.tensor_tensor(out=k.modT, in0=pm[:, 0:96].rearrange("p (a b) -> p a b", b=2),
                                          in1=abT.unsqueeze(2).to_broadcast([128, 48, 2]), op=ALU.add),
         reads=[r_pm, r_abT], writes=[k.r_modT])
    S.op("dve", lambda e: e.scalar_tensor_tensor(out=k.s1, in0=k.modT[:, 8:16, :], scalar=1.0,
                                                 in1=g1T.unsqueeze(2).to_broadcast([128, 8, 2]), op0=ALU.add, op1=ALU.mult),
         reads=[k.r_modT, r_g1T], writes=[k.r_s1])
    S.op("dve", lambda e: e.scalar_tensor_tensor(out=k.s2, in0=k.modT[:, 32:40, 0], scalar=1.0,
                                                 in1=g2T, op0=ALU.add, op1=ALU.mult),
         reads=[k.r_modT, r_g2T], writes=[k.r_s2])
    g2rep, r_g2rep = k.tile([128, D], F32, "g2rep")
    k.load(g2rep, r_g2rep, ins["g2_rep"])
    S.op("dve", lambda e: e.scalar_tensor_tensor(out=k.s2_row, in0=k.s2_row, scalar=1.0, in1=g2rep, op0=ALU.add, op1=ALU.mult),
         reads=[k.r_s2row, r_g2rep], writes=[k.r_s2row])
    S.barrier()
    A.release()

def _nb(k, dtype=F32):
    b = getattr(k, "_bank_i", 0)
    k._bank_i = (b + 1) % 8
    return k.bank(b, dtype)


def _rstd(k, ss, r_ss, n, out, r_out):
    S = k.S
    S.op("act", lambda e: e.activation(out=out, in_=ss, func=AF.Sqrt, scale=1.0 / n, bias=EPS), reads=[r_ss], writes=[r_out])
    S.op("dve", lambda e: e.reciprocal(out=out, in_=out), reads=[r_out], writes=[r_out])


def _rope(k, pe, r_pe, cos_t, sin_t, r_tab, t1, t2, r_t1, r_t2):
    S = k.S
    cb = cos_t.unsqueeze(1).to_broadcast([128, 8, 64])
    S.op("pool", lambda e: e.tensor_tensor(out=t1, in0=pe, in1=cb, op=ALU.mult), reads=[r_pe, r_tab], writes=[r_t1])
    pe5 = pe.rearrange("p h (a s c) -> p h a s c", a=2, s=2)
    t25 = t2.rearrange("p h (a s c) -> p h a s c", a=2, s=2)
    sn5 = sin_t.rearrange("p (a s c) -> p a s c", a=2, s=2)

    def f(e):
        for s in range(2):
            ins_ = e.tensor_tensor(out=t25[:, :, :, s, :], in0=pe5[:, :, :, 1 - s, :],
                                   in1=sn5[:, :, s, :].unsqueeze(1).to_broadcast([128, 8, 2, 16]), op=ALU.mult)
        return ins_
    S.op("dve", f, reads=[r_pe, r_tab], writes=[r_t2])
    S.op("pool", lambda e: e.tensor_tensor(out=pe, in0=t1, in1=t2, op=ALU.add), reads=[r_t1, r_t2], writes=[r_pe])


def phase_a1(k):
    S, A, nc, ins = k.S, k.A, k.nc, k.ins
    zs_s, r_zs_s = k.scratch("zs_s", [TX, D], BF16)
    gb_s, r_gb_s = k.scratch("gb_s", [T, 48], F32)
    qmT_s, r_qmT_s = k.scratch("qmT_s", [H, 192, TX], BF16)
    kmT_s, r_kmT_s = k.scratch("kmT_s", [H, 192, T], BF16)
    vm_s, r_vm_s = k.scratch("vm_s", [T, D], BF16)
    A.mark()
    k.hT, _ = k.tile([128, 8, T], BF16, "hT")
    k.r_hT = [Res(f"hT{i}") for i in range(NT)]
    A.mark()
    wmid, r_wmid = k.tile([128, 8, NMID], BF16, "wmid")
    wuq, r_wuq = k.tile([128, 3, 1536], BF16, "wuq")
    wukv, r_wukv = k.tile([128, 2, 2048], BF16, "wukv")
    dtb, r_dtb = k.tile([128, 16], F32, "dtb")
    negA, r_negA = k.tile([128, 16], F32, "negA")
    gq, r_gq = k.tile([128, 192], F32, "gq")
    gk, r_gk = k.tile([128, 192], F32, "gk")
    gqaT, r_gqaT = k.tile([128, 3], F32, "gqaT")
    gkvaT, r_gkvaT = k.tile([128, 2], F32, "gkvaT")
    win = ins["w_in"].rearrange("(kc p) n -> p kc n", p=128)
    for j in range(4):
        k.wload(wmid[:, 2 * j:2 * j + 2, :], r_wmid, win[:, 2 * j:2 * j + 2, OFF_Z:OFF_GATE])
    k.wload(wuq, r_wuq, ins["w_uq"].rearrange("(kc p) n -> p kc n", p=128))
    k.wload(wukv, r_wukv, ins["w_ukv"].rearrange("(kc p) n -> p kc n", p=128))
    k.load(dtb, r_dtb, ins["dtb_rep"])
    k.load(negA, r_negA, ins["alog_rep"])
    k.load(gq, r_gq, ins["gq_rep"])
    k.load(gk, r_gk, ins["gk_rep"])
    k.load(gqaT, r_gqaT, ins["gqaT"])
    k.load(gkvaT, r_gkvaT, ins["gkvaT"])
    S.op("act", lambda e: e.activation(out=negA, in_=negA, func=AF.Exp), reads=[r_negA], writes=[r_negA])
    S.op("dve", lambda e: e.tensor_scalar(out=negA, in0=negA, scalar1=-1.0, scalar2=None, op0=ALU.mult), reads=[r_negA], writes=[r_negA])
    S.op("dve", lambda e: e.tensor_scalar(out=gq, in0=gq, scalar1=192.0 ** -0.5, scalar2=None, op0=ALU.mult), reads=[r_gq], writes=[r_gq])

    NB = 2
    xt = [k.tile([128, D], F32, "xt") for _ in range(NB)]
    junk, r_junk = k.tile([128, D], BF16, "junk")
    ss = [k.tile([128, 8], F32, "ss") for _ in range(NB)]
    xn = [k.tile([128, D], BF16, "xn") for _ in range(NB)]
    zs = [k.tile([128, D], BF16, "zs") for _ in range(NB)]
    gb = [k.tile([128, 48], F32, "gb") for _ in range(NB)]
    t16, r_t16 = k.tile([128, 16], F32, "t16")
    cqn, r_cqn = k.tile([128, 384], BF16, "cqn")
    ckvn, r_ckvn = k.tile([128, 256], BF16, "ckvn")
    cqnT, r_cqnT = k.tile([128, 3, 128], BF16, "cqnT")
    ckvnT, r_ckvnT = k.tile([128, 2, 128], BF16, "ckvnT")
    kr, r_kr = k.tile([128, 64], F32, "kr")
    qsb, r_qsb = k.tile([128, 8, 192], F32, "qsb")
    sq, r_sq = k.tile([128, 8, 192], F32, "sq")
    r8, r_r8 = k.tile([128, 8], F32, "r8")
    kvsb, r_kvsb = k.tile([128, 8, 2, 128], F32, "kvsb")
    tmpf, r_tmpf = kvsb.rearrange("p a b c -> p (a b c)")[:, 0:1024].rearrange("p (a b) -> p a b", a=8), r_kvsb
    kf, r_kf = sq, r_sq
    rk8, r_rk8 = k.tile([128, 8], F32, "rk8")
    sskr, r_sskr = k.tile([128, 1], F32, "sskr")
    rt1, r_rt1 = k.tile([128, 8, 64], F32, "rt1")
    rt2, r_rt2 = k.tile([128, 8, 64], F32, "rt2")
    cs_t = [k.tile([128, 128], F32, "cs") for _ in range(NB)]
    qf, r_qf = k.tile([128, 8, 192], BF16, "qf")
    kfb, r_kfb = k.tile([128, 8, 192], BF16, "kfb")
    vb = [k.tile([128, 8, 128], BF16, "vb") for _ in range(1)]
    qTn = [k.tile([128, 8, 128], BF16, "qTn") for _ in range(1)]
    qTr = [k.tile([64, 8, 128], BF16, "qTr") for _ in range(1)]
    kTn = [k.tile([128, 8, 128], BF16, "kTn") for _ in range(1)]
    kTr = [k.tile([64, 8, 128], BF16, "kTr") for _ in range(1)]

    def src_rows(i):
        return ins["ctx"][i * 128:(i + 1) * 128, :] if i < 2 else ins["x"][(i - 2) * 128:(i - 1) * 128, :]

    def prefetch(i):
        b = i % NB
        k.load(xt[b][0], xt[b][1], src_rows(i))
        if i >= 2:
            xi = i - 2
            S.dma("act", k.ld, lambda e: e.dma_start(out=cs_t[b][0], in_=ins["rope_cs"][xi * 128:(xi + 1) * 128, :]), writes=[cs_t[b][1]])

    def transposes(src, r_src, n, width=128, rows=128):
        pb, r_pb = _nb(k, BF16)
        pv = pb.rearrange("p (a b) -> p a b", b=128)[0:width, 0:n, :]

        def f(e):
            for j in range(n):
                ins_ = e.transpose(out=pv[:, j, :], in_=src(j), identity=k.ident_b)
            return ins_
        S.op("pe", f, reads=[r_src, k.r_ident_b], writes=[r_pb])
        return pv, r_pb

    prefetch(0)
    for i in range(NT):
        b = i % NB
        is_x = i >= 2
        xi = i - 2
        col = 0 if is_x else 1
        tok = slice(i * 128, (i + 1) * 128)
        if i + 1 < NT:
            prefetch(i + 1)
        x_t, r_x = xt[b]
        ss_t, r_ss = ss[b]
        xn_t, r_xn = xn[b]
        S.op("act", lambda e, x_t=x_t, ss_t=ss_t: e.activation(out=junk, in_=x_t, func=AF.Square, accum_out=ss_t[:, 0:1]),
             reads=[r_x], writes=[r_junk, r_ss])
        _rstd(k, ss_t[:, 0:1], r_ss, D, ss_t[:, 1:2], r_ss)
        S.op("act", lambda e, x_t=x_t, ss_t=ss_t, xn_t=xn_t: e.activation(out=xn_t, in_=x_t, func=AF.Copy, scale=ss_t[:, 1:2]),
             reads=[r_x, r_ss], writes=[r_xn])
        pv, r_pv = transposes(lambda j, xn_t=xn_t: xn_t[:, j * 128:(j + 1) * 128], r_xn, 8)
        S.op("dve", lambda e, pv=pv, col=col: e.tensor_tensor(out=tmpf, in0=pv, in1=k.s1[:, :, col:col + 1].to_broadcast([128, 8, 128]), op=ALU.mult),
             reads=[r_pv, k.r_s1], writes=[r_tmpf])
        S.op("pool", lambda e, col=col, tok=tok: e.tensor_tensor(out=k.hT[:, :, tok], in0=tmpf,
                                                                in1=k.modT[:, 0:8, col:col + 1].to_broadcast([128, 8, 128]), op=ALU.add),
             reads=[r_tmpf, k.r_modT], writes=[k.r_hT[i]])
        groups = [(0, 512), (512, 1024), (1024, 1440), (1440, 1760)]
        banks = []
        for g, (c0, c1) in enumerate(groups):
            if g < 2 and not is_x:
                banks.append(None)
                continue
            pb, r_pb = _nb(k)

            def mm(e, pb=pb, c0=c0, c1=c1, tok=tok):
                for kc in range(8):
                    ins_ = e.matmul(pb[:, 0:c1 - c0], lhsT=k.hT[:, kc, tok], rhs=wmid[:, kc, c0:c1], start=(kc == 0), stop=(kc == 7))
                return ins_
            S.op("pe", mm, reads=[k.r_hT[i], r_wmid], writes=[r_pb])
            banks.append((pb, r_pb))
        if is_x:
            z_t, r_z = zs[b]
            for g in range(2):
                pb, r_pb = banks[g]
                S.op("act", lambda e, pb=pb, g=g, z_t=z_t: e.activation(out=z_t[:, g * 512:(g + 1) * 512], in_=pb, func=AF.Silu),
                     reads=[r_pb], writes=[r_z])
            k.store(zs_s[xi * 128:(xi + 1) * 128, :], r_zs_s, z_t, r_z)
        p2, r_p2 = banks[2]
        p3, r_p3 = banks[3]
        gb_t, r_gb = gb[b]
        S.op("dve", lambda e, p2=p2: e.tensor_tensor(out=t16, in0=p2[:, 0:16], in1=dtb, op=ALU.add), reads=[r_p2, r_dtb], writes=[r_t16])
        S.op("act", lambda e: e.activation(out=t16, in_=t16, func=AF.Exp), reads=[r_t16], writes=[r_t16])
        S.op("act", lambda e: e.activation(out=t16, in_=t16, func=AF.Ln, bias=1.0), reads=[r_t16], writes=[r_t16])
        S.op("dve", lambda e, gb_t=gb_t: e.tensor_tensor(out=gb_t[:, 0:16], in0=t16, in1=negA, op=ALU.mult), reads=[r_t16, r_negA], writes=[r_gb])
        S.op("act", lambda e, gb_t=gb_t, p2=p2: e.activation(out=gb_t[:, 16:32], in_=p2[:, 16:32], func=AF.Sigmoid), reads=[r_p2], writes=[r_gb])
        S.op("act", lambda e, gb_t=gb_t: e.activation(out=gb_t[:, 32:48], in_=gb_t[:, 16:32], func=AF.Ln), reads=[r_gb], writes=[r_gb])
        k.store(gb_s[tok, :], r_gb_s, gb_t, r_gb)
        if is_x:
            S.op("act", lambda e, p2=p2, ss_t=ss_t: e.activation(out=junk[:, 0:384], in_=p2[:, 32:416], func=AF.Square, accum_out=ss_t[:, 4:5]),
                 reads=[r_p2], writes=[r_junk, r_ss])
            _rstd(k, ss_t[:, 4:5], r_ss, 384, ss_t[:, 5:6], r_ss)
            S.op("act", lambda e, p2=p2, ss_t=ss_t: e.activation(out=cqn, in_=p2[:, 32:416], func=AF.Copy, scale=ss_t[:, 5:6]),
                 reads=[r_p2, r_ss], writes=[r_cqn])

        S.op("act", lambda e, p3=p3, ss_t=ss_t: e.activation(out=junk[:, 0:256], in_=p3[:, 0:256], func=AF.Square, accum_out=ss_t[:, 2:3]),
             reads=[r_p3], writes=[r_junk, r_ss])
        _rstd(k, ss_t[:, 2:3], r_ss, 256, ss_t[:, 3:4], r_ss)
        S.op("act", lambda e, p3=p3, ss_t=ss_t: e.activation(out=ckvn, in_=p3[:, 0:256], func=AF.Copy, scale=ss_t[:, 3:4]),
             reads=[r_p3, r_ss], writes=[r_ckvn])
        S.op("dve", lambda e, p3=p3: e.tensor_copy(out=kr, in_=p3[:, 256:320]), reads=[r_p3], writes=[r_kr])
        pv, r_pv = transposes(lambda j: ckvn[:, j * 128:(j + 1) * 128], r_ckvn, 2)
        S.op("dve", lambda e, pv=pv: e.tensor_tensor(out=ckvnT, in0=pv, in1=gkvaT.unsqueeze(2).to_broadcast([128, 2, 128]), op=ALU.mult),
             reads=[r_pv, r_gkvaT], writes=[r_ckvnT])
        for b4 in range(4):
            pb, r_pb = _nb(k)

            def mmkv(e, pb=pb, b4=b4):
                for kc in range(2):
                    ins_ = e.matmul(pb, lhsT=ckvnT[:, kc, :], rhs=wukv[:, kc, b4 * 512:(b4 + 1) * 512], start=(kc == 0), stop=(kc == 1))
                return ins_
            S.op("pe", mmkv, reads=[r_ckvnT, r_wukv], writes=[r_pb])
            eng = "act" if b4 % 2 == 0 else "dve"
            dst = kvsb[:, 2 * b4:2 * b4 + 2, :, :].rearrange("p a b c -> p (a b c)")
            if eng == "act":
                S.op("act", lambda e, dst=dst, pb=pb: e.activation(out=dst, in_=pb, func=AF.Copy), reads=[r_pb], writes=[r_kvsb])
            else:
                S.op("dve", lambda e, dst=dst, pb=pb: e.tensor_copy(out=dst, in_=pb), reads=[r_pb], writes=[r_kvsb])
        vb_t, r_vb = vb[0]
        S.op("pool", lambda e, vb_t=vb_t: e.tensor_copy(out=vb_t, in_=kvsb[:, :, 1, :]), reads=[r_kvsb], writes=[r_vb])
        k.store(vm_s[tok, :], r_vm_s, vb_t.rearrange("p h d -> p (h d)"), r_vb)
        S.op("pool", lambda e: e.tensor_tensor(out=sq[:, :, 0:128], in0=kvsb[:, :, 0, :], in1=kvsb[:, :, 0, :], op=ALU.mult),
             reads=[r_kvsb], writes=[r_sq])
        S.op("dve", lambda e: e.tensor_reduce(out=rk8, in_=sq[:, :, 0:128], axis=AX.X, op=ALU.add), reads=[r_sq], writes=[r_rk8])
        S.op("act", lambda e: e.activation(out=junk[:, 0:64], in_=kr, func=AF.Square, accum_out=sskr), reads=[r_kr], writes=[r_junk, r_sskr])
        S.op("dve", lambda e: e.tensor_scalar(out=rk8, in0=rk8, scalar1=sskr, scalar2=None, op0=ALU.add), reads=[r_rk8, r_sskr], writes=[r_rk8])
        _rstd(k, rk8, r_rk8, 192, rk8, r_rk8)
        S.op("dve", lambda e: e.tensor_tensor(out=kf[:, :, 0:128], in0=kvsb[:, :, 0, :], in1=rk8.unsqueeze(2).to_broadcast([128, 8, 128]), op=ALU.mult),
             reads=[r_kvsb, r_rk8], writes=[r_kf])
        S.op("dve", lambda e: e.tensor_tensor(out=kf[:, :, 128:192], in0=kr.unsqueeze(1).to_broadcast([128, 8, 64]),
                                              in1=rk8.unsqueeze(2).to_broadcast([128, 8, 64]), op=ALU.mult),
             reads=[r_kr, r_rk8, r_kf], writes=[r_kf])
        S.op("pool", lambda e: e.tensor_tensor(out=kf, in0=kf, in1=gk.unsqueeze(1).to_broadcast([128, 8, 192]), op=ALU.mult),
             reads=[r_kf, r_gk], writes=[r_kf])
        if is_x:
            _rope(k, kf[:, :, 128:192], r_kf, cs_t[b][0][:, 0:64], cs_t[b][0][:, 64:128], cs_t[b][1], rt1, rt2, r_rt1, r_rt2)
        S.op("act", lambda e: e.activation(out=kfb, in_=kf, func=AF.Copy), reads=[r_kf], writes=[r_kfb])
        kTn_t, r_kTn = kTn[0]
        kTr_t, r_kTr = kTr[0]
        pv, r_pv = transposes(lambda j: kfb[:, j, 0:128], r_kfb, 8)
        S.op("dve", lambda e, pv=pv, kTn_t=kTn_t: e.tensor_copy(out=kTn_t, in_=pv), reads=[r_pv], writes=[r_kTn])
        pv, r_pv = transposes(lambda j: kfb[:, j, 128:192], r_kfb, 8, width=64)
        S.op("act", lambda e, pv=pv, kTr_t=kTr_t: e.activation(out=kTr_t, in_=pv, func=AF.Copy), reads=[r_pv], writes=[r_kTr])
        k.store(kmT_s[:, 0:128, tok].rearrange("h d t -> d h t"), r_kmT_s, kTn_t, r_kTn)
        k.store(kmT_s[:, 128:192, tok].rearrange("h d t -> d h t"), r_kmT_s, kTr_t, r_kTr)
        if not is_x:
            continue
        xtok = slice(xi * 128, (xi + 1) * 128)
        pv, r_pv = transposes(lambda j: cqn[:, j * 128:(j + 1) * 128], r_cqn, 3)
        S.op("dve", lambda e, pv=pv: e.tensor_tensor(out=cqnT, in0=pv, in1=gqaT.unsqueeze(2).to_broadcast([128, 3, 128]), op=ALU.mult),
             reads=[r_pv, r_gqaT], writes=[r_cqnT])
        qflat = qsb.rearrange("p h d -> p (h d)")
        for b3 in range(3):
            pb, r_pb = _nb(k)

            def mmq(e, pb=pb, b3=b3):
                for kc in range(3):
                    ins_ = e.matmul(pb, lhsT=cqnT[:, kc, :], rhs=wuq[:, kc, b3 * 512:(b3 + 1) * 512], start=(kc == 0), stop=(kc == 2))
                return ins_
            S.op("pe", mmq, reads=[r_cqnT, r_wuq], writes=[r_pb])
            if b3 % 2 == 0:
                S.op("act", lambda e, pb=pb, b3=b3: e.activation(out=qflat[:, b3 * 512:(b3 + 1) * 512], in_=pb, func=AF.Copy), reads=[r_pb], writes=[r_qsb])
            else:
                S.op("dve", lambda e, pb=pb, b3=b3: e.tensor_copy(out=qflat[:, b3 * 512:(b3 + 1) * 512], in_=pb), reads=[r_pb], writes=[r_qsb])
        S.op("pool", lambda e: e.tensor_tensor(out=sq, in0=qsb, in1=qsb, op=ALU.mult), reads=[r_qsb], writes=[r_sq])
        S.op("dve", lambda e: e.tensor_reduce(out=r8, in_=sq, axis=AX.X, op=ALU.add), reads=[r_sq], writes=[r_r8])
        _rstd(k, r8, r_r8, 192, r8, r_r8)
        S.op("dve", lambda e: e.tensor_tensor(out=qsb, in0=qsb, in1=r8.unsqueeze(2).to_broadcast([128, 8, 192]), op=ALU.mult),
             reads=[r_qsb, r_r8], writes=[r_qsb])
        S.op("pool", lambda e: e.tensor_tensor(out=qsb, in0=qsb, in1=gq.unsqueeze(1).to_broadcast([128, 8, 192]), op=ALU.mult),
             reads=[r_qsb, r_gq], writes=[r_qsb])
        _rope(k, qsb[:, :, 128:192], r_qsb, cs_t[b][0][:, 0:64], cs_t[b][0][:, 64:128], cs_t[b][1], rt1, rt2, r_rt1, r_rt2)
        S.op("act", lambda e: e.activation(out=qf, in_=qsb, func=AF.Copy), reads=[r_qsb], writes=[r_qf])
        qTn_t, r_qTn = qTn[0]
        qTr_t, r_qTr = qTr[0]
        pv, r_pv = transposes(lambda j: qf[:, j, 0:128], r_qf, 8)
        S.op("dve", lambda e, pv=pv, qTn_t=qTn_t: e.tensor_copy(out=qTn_t, in_=pv), reads=[r_pv], writes=[r_qTn])
        pv, r_pv = transposes(lambda j: qf[:, j, 128:192], r_qf, 8, width=64)
        S.op("act", lambda e, pv=pv, qTr_t=qTr_t: e.activation(out=qTr_t, in_=pv, func=AF.Copy), reads=[r_pv], writes=[r_qTr])
        k.store(qmT_s[:, 0:128, xtok].rearrange("h d t -> d h t"), r_qmT_s, qTn_t, r_qTn)
        k.store(qmT_s[:, 128:192, xtok].rearrange("h d t -> d h t"), r_qmT_s, qTr_t, r_qTr)
    S.barrier()
    A.release()

RW = 4364
NU = 4356


def phase_a2(k):
    S, A, nc, ins = k.S, k.A, k.nc, k.ins
    qdT_s, r_qdT_s = k.scratch("qdT_s", [H, 128, TX], BF16)
    kdT_s, r_kdT_s = k.scratch("kdT_s", [H, 128, T], BF16)
    kd_s, r_kd_s = k.scratch("kd_s", [T, H, 128], BF16)
    vd_s, r_vd_s = k.scratch("vd_s", [T, H, 128], BF16)
    sgT_s, r_sgT_s = k.scratch("sgT_s", [16, 128, TX], BF16)
    A.mark()
    convw, r_convw = k.tile([128, 24, 5], F32, "convw")
    k.load(convw, r_convw, ins["convT"])
    wc = [k.tile([128, 8, 512], BF16, "wc") for _ in range(2)]
    R = [k.tile([128, RW], F32, "R") for _ in range(2)]
    acc, r_acc = k.tile([128, NU], F32, "acc")
    sq, r_sq = k.tile([128, NU], BF16, "sq")
    Yb = [k.tile([128, NU], BF16, "Yb") for _ in range(2)]
    rn, r_rn = k.tile([128, 512], F32, "rn")
    tm = [k.tile([128, NT, 128], BF16, "tm") for _ in range(1)]
    sg = [k.tile([128, 512], BF16, "sg") for _ in range(2)]
    for j in range(2):
        S.op("pool", lambda e, j=j: e.memset(R[j][0], 0.0), writes=[R[j][1]])
    win = ins["w_in"].rearrange("(kc p) n -> p kc n", p=128)
    blocks = [(c * 512, "qkv", c * 4) for c in range(6)] + [(OFF_GATE + c * 512, "gate", c * 4) for c in range(4)]
    tgroups = [(0, 256, 2)] + [(256 + g * 512, 512, 262 + g * 512) for g in range(8)]
    all_hT = list(k.r_hT)

    def load_block(bi):
        c0, kind, _ = blocks[bi]
        w_t, r_w = wc[bi % 2]
        for hf in range(2):
            k.wload(w_t[:, 4 * hf:4 * hf + 4, :], r_w, win[:, 4 * hf:4 * hf + 4, c0:c0 + 512])

    load_block(0)
    ci = 0
    for bi, (c0, kind, chunk0) in enumerate(blocks):
        if bi + 1 < len(blocks):
            load_block(bi + 1)
        w_t, r_w = wc[bi % 2]
        for sub in range(4):
            cc = chunk0 + sub
            if kind == "gate":
                for g in range(8):
                    pb, r_pb = _nb(k)

                    def mm(e, pb=pb, g=g, sub=sub, w_t=w_t):
                        for kc in range(8):
                            ins_ = e.matmul(pb, lhsT=w_t[:, kc, sub * 128:(sub + 1) * 128], rhs=k.hT[:, kc, 256 + g * 512:256 + (g + 1) * 512],
                                            start=(kc == 0), stop=(kc == 7))
                        return ins_
                    S.op("pe", mm, reads=[r_w] + all_hT, writes=[r_pb])
                    s_t, r_s = sg[g % 2]
                    S.op("act", lambda e, pb=pb, s_t=s_t: e.activation(out=s_t, in_=pb, func=AF.Sigmoid), reads=[r_pb], writes=[r_s])
                    k.store(sgT_s[cc, :, g * 512:(g + 1) * 512], r_sgT_s, s_t, r_s)
                continue
            R_t, r_R = R[ci % 2]
            Y_t, r_Y = Yb[ci % 2]
            ci += 1
            for gi, (h0, n, ro) in enumerate(tgroups):
                pb, r_pb = _nb(k)

                def mm(e, pb=pb, h0=h0, n=n, sub=sub, w_t=w_t):
                    for kc in range(8):
                        ins_ = e.matmul(pb[:, 0:n], lhsT=w_t[:, kc, sub * 128:(sub + 1) * 128], rhs=k.hT[:, kc, h0:h0 + n],
                                        start=(kc == 0), stop=(kc == 7))
                    return ins_
                S.op("pe", mm, reads=[r_w] + all_hT, writes=[r_pb])
                if gi % 2 == 0:
                    S.op("act", lambda e, pb=pb, n=n, ro=ro, R_t=R_t: e.activation(out=R_t[:, ro:ro + n], in_=pb[:, 0:n], func=AF.Copy),
                         reads=[r_pb], writes=[r_R])
                else:
                    S.op("dve", lambda e, pb=pb, n=n, ro=ro, R_t=R_t: e.tensor_copy(out=R_t[:, ro:ro + n], in_=pb[:, 0:n]),
                         reads=[r_pb], writes=[r_R])
            ceng = "dve"

            def conv(e, R_t=R_t, cc=cc):
                e.tensor_scalar(out=acc, in0=R_t[:, 0:NU], scalar1=convw[:, cc, 0:1], scalar2=None, op0=ALU.mult)
                for j in range(1, 5):
                    ins_ = e.scalar_tensor_tensor(out=acc, in0=R_t[:, j:j + NU], scalar=convw[:, cc, j:j + 1], in1=acc,
                                                  op0=ALU.mult, op1=ALU.add)
                return ins_
            S.op(ceng, conv, reads=[r_R, r_convw], writes=[r_acc])
            head = cc % 8
            if cc >= 16:
                S.op("act", lambda e, Y_t=Y_t: e.activation(out=Y_t, in_=acc, func=AF.Silu), reads=[r_acc], writes=[r_Y])
            else:
                S.op("act", lambda e: e.activation(out=acc, in_=acc, func=AF.Silu), reads=[r_acc], writes=[r_acc])
                S.op("pool" if ceng == "dve" else "dve", lambda e: e.tensor_tensor(out=sq, in0=acc, in1=acc, op=ALU.mult), reads=[r_acc], writes=[r_sq])
                scale = (128.0 ** -0.5) if cc < 8 else 1.0
                for g in range(9):
                    u0 = g * 512
                    n = min(512, NU - u0)
                    pb, r_pb = _nb(k)
                    S.op("pe", lambda e, pb=pb, u0=u0, n=n: e.matmul(pb[:, 0:n], lhsT=k.ones_b, rhs=sq[:, u0:u0 + n], start=True, stop=True),
                         reads=[r_sq, k.r_ones_b], writes=[r_pb])
                    S.op("act", lambda e, pb=pb, n=n, scale=scale: e.activation(out=rn[:, 0:n], in_=pb[:, 0:n], func=AF.Sqrt,
                                                                              scale=1.0 / (scale * scale), bias=EPS / (scale * scale)),
                         reads=[r_pb], writes=[r_rn])
                    S.op("dve", lambda e, n=n: e.reciprocal(out=rn[:, 0:n], in_=rn[:, 0:n]), reads=[r_rn], writes=[r_rn])
                    S.op("dve", lambda e, u0=u0, n=n, Y_t=Y_t: e.tensor_tensor(out=Y_t[:, u0:u0 + n], in0=acc[:, u0:u0 + n], in1=rn[:, 0:n], op=ALU.mult),
                         reads=[r_acc, r_rn], writes=[r_Y])
            if cc < 8:
                k.store(qdT_s[head, :, :], r_qdT_s, Y_t[:, 260:260 + TX], r_Y)
            elif cc < 16:
                k.store(kdT_s[head, :, 0:256], r_kdT_s, Y_t[:, 0:256], r_Y)
                k.store(kdT_s[head, :, 256:T], r_kdT_s, Y_t[:, 260:260 + TX], r_Y)
            if cc >= 8:
                tm_t, r_tm = tm[0]
                for tb in range(5):
                    t0 = tb * 8
                    nt = min(8, NT - t0)
                    pb, r_pb = _nb(k, BF16)
                    pv = pb.rearrange("p (a b) -> p a b", b=128)[:, 0:nt, :]

                    def tr(e, pv=pv, t0=t0, nt=nt, Y_t=Y_t):
                        for j in range(nt):
                            ti = t0 + j
                            u = ti * 128 if ti < 2 else 260 + (ti - 2) * 128
                            ins_ = e.transpose(out=pv[:, j, :], in_=Y_t[:, u:u + 128], identity=k.ident_b)
                        return ins_
                    S.op("pe", tr, reads=[r_Y, k.r_ident_b], writes=[r_pb])
                    if tb % 2 == 0:
                        S.op("act", lambda e, pv=pv, t0=t0, nt=nt, tm_t=tm_t: e.activation(out=tm_t[:, t0:t0 + nt, :], in_=pv, func=AF.Copy),
                             reads=[r_pb], writes=[r_tm])
                    else:
                        S.op("dve", lambda e, pv=pv, t0=t0, nt=nt, tm_t=tm_t: e.tensor_copy(out=tm_t[:, t0:t0 + nt, :], in_=pv),
                             reads=[r_pb], writes=[r_tm])
                dst_s, r_dst = (kd_s, r_kd_s) if cc < 16 else (vd_s, r_vd_s)
                k.store(dst_s.rearrange("(n p) h d -> p n h d", p=128)[:, :, head, :], r_dst, tm_t, r_tm)
    S.barrier()
    A.release()
    A.release()

import os as _os


def _drive(*gens):
    gens = [g for g in gens if g is not None]
    while gens:
        for g in list(gens):
            try:
                next(g)
            except StopIteration:
                gens.remove(g)


def phase_b(k):
    S, A, nc, ins = k.S, k.A, k.nc, k.ins
    qdT_s, r_qdT_s = k.scr["qdT_s"]
    kdT_s, r_kdT_s = k.scr["kdT_s"]
    kd_s, r_kd_s = k.scr["kd_s"]
    vd_s, r_vd_s = k.scr["vd_s"]
    gb_s, r_gb_s = k.scr["gb_s"]
    o_s = [k.scratch("of_s", [TX, D], F32), k.scratch("ob_s", [TX, D], F32)]
    A.mark()
    msk, r_msk = k.tile([128, 9, 128], F32, "msk")
    k.load(msk, r_msk, ins["dn_masks"])
    EC, r_EC = k.tile([64, 2, 8, 128], F32, "EC")
    k.load(EC, r_EC, ins["dn_esel"])
    L1, r_L1 = k.tile([64, 128], F32, "L1")
    L2, r_L2 = k.tile([64, 128], F32, "L2")
    R1, r_R1 = k.tile([64, 8, 128], F32, "R1")
    R2, r_R2 = k.tile([64, 8, 128], F32, "R2")
    X, r_X = k.tile([128, 2, 64], F32, "X")
    k.load(L1, r_L1, ins["dn_linit"])
    k.load(L2, r_L2, ins["dn_linit"])
    S.op("dve", lambda e: e.tensor_copy(out=R1, in_=EC[:, 0, :, :]), reads=[r_EC], writes=[r_R1])
    S.op("dve", lambda e: e.tensor_copy(out=R2, in_=EC[:, 1, :, :]), reads=[r_EC], writes=[r_R2])
    S.op("pool", lambda e: e.memset(X, 0.0), writes=[r_X])
    S32 = [k.tile([128, 8, 128], F32, f"S32_{d}") for d in range(2)]
    Sbf = [k.tile([128, 8, 128], BF16, f"Sbf_{d}") for d in range(2)]
    for d in range(2):
        S.op("pool", lambda e, d=d: e.memset(S32[d][0], 0.0), writes=[S32[d][1]])
        S.op("pool", lambda e, d=d: e.memset(Sbf[d][0], 0.0), writes=[Sbf[d][1]])
    NB = 2
    gbt = [k.tile([128, 48], F32, "gbt") for _ in range(NB)]
    kT = [k.tile([128, 8, 128], BF16, "kT") for _ in range(NB)]
    qT = [k.tile([128, 8, 128], BF16, "qT") for _ in range(NB)]
    ktm = [k.tile([128, 8, 128], BF16, "ktm") for _ in range(NB)]
    vtm = [k.tile([128, 8, 128], BF16, "vtm") for _ in range(NB)]
    sm = [k.tile([128, 4, 8], F32, "sm") for _ in range(NB)]
    E1, r_E1 = k.tile([128, 8, 128], F32, "E1")
    M, r_M = k.tile([128, 8, 128], F32, "M")
    Mt, r_Mt = k.tile([128, 8, 128], BF16, "Mt")
    Md, r_Md = k.tile([128, 8, 128], BF16, "Md")
    Mo1, r_Mo1 = k.tile([128, 8, 128], BF16, "Mo1")
    Mo2, r_Mo2 = k.tile([128, 8, 128], BF16, "Mo2")
    PP = [k.tile([128, 8, 128], BF16, f"PP{j}") for j in range(2)]
    PT = [k.tile([128, 8, 128], BF16, f"PT{j}") for j in range(2)]
    Tt, r_Tt = k.tile([128, 8, 128], BF16, "Tt")
    TtB, r_TtB = k.tile([128, 8, 128], BF16, "TtB")
    Abuf, _ = k.tile([128, 8, 128], BF16, "Abuf")
    E1r, Mdr, Mo1r, Mo2r, Mtr, Ttr = (t_ for t_ in (Abuf, Md, Mo1, Mo2, Mt, Tt))
    PPr = [PP[j][0] for j in range(2)]
    PTr = [PT[j][0] for j in range(2)]
    kg, r_kg = k.tile([128, 8, 128], BF16, "kg")
    kdec = [k.tile([128, 8, 128], BF16, "kdec") for _ in range(NB)]
    wT = [k.tile([128, 8, 128], BF16, "wT") for _ in range(NB)]
    u = [k.tile([128, 8, 128], F32, "u") for _ in range(NB)]
    qkT = [k.tile([128, 8, 128], BF16, "qkT") for _ in range(NB)]
    vnew, r_vnew = k.tile([128, 8, 128], BF16, "vnew")
    tmpo, r_tmpo = k.tile([128, 8, 128], F32, "tmpo")
    o_t = [k.tile([128, 8, 128], F32, "o") for _ in range(NB)]

    RG = {nm: [Res(nm + "0"), Res(nm + "1")] for nm in ("Ab", "E1", "M", "Md", "Mo1", "Mo2", "Mt", "Tt", "TtB", "PP0", "PP1", "PT0", "PT1")}
    wT_res = [[Res("wT"), Res("wT")] for _ in range(NB)]
    u_res = [[Res("u"), Res("u")] for _ in range(NB)]
    qk_res = [[Res("qk"), Res("qk")] for _ in range(NB)]
    pre_i = [0]

    def nbp(dtype=F32):
        b = pre_i[0]
        pre_i[0] = (b + 1) % 4
        return k.bank(b, dtype)

    def b4(pb):
        return pb.rearrange("p (a b) -> p a b", b=128)

    def b4h(pb):
        return pb.rearrange("p (a b) -> p a b", b=128)[:, 0:4, :]

    units = []
    fwd = list(range(NT))
    bwd = [1, 0] + list(range(NT - 1, 1, -1))
    for s in range(NT):
        units.append((0, fwd[s]))
        units.append((1, bwd[s]))

    def loads(ui):
        d, ti = units[ui]
        b = ui % NB
        tok = slice(ti * 128, (ti + 1) * 128)
        k.load(gbt[b][0], gbt[b][1], gb_s[tok, :], r_gb_s)
        k.load(kT[b][0], kT[b][1], kdT_s[:, :, tok].rearrange("h d t -> d h t"), r_kdT_s)
        k.load(ktm[b][0], ktm[b][1], kd_s[tok, :, :], r_kd_s, q="act")
        k.load(vtm[b][0], vtm[b][1], vd_s[tok, :, :], r_vd_s, q="act")
        if ti >= 2:
            xt_ = slice((ti - 2) * 128, (ti - 1) * 128)
            k.load(qT[b][0], qT[b][1], qdT_s[:, :, xt_].rearrange("h d t -> d h t"), r_qdT_s)

    def pre(ui):
        d, ti = units[ui]
        b = ui % NB
        is_x = ti >= 2
        g_t, r_g = gbt[b]
        kT_t, r_kT = kT[b]
        qT_t, r_qT = qT[b]
        ktm_t, r_ktm = ktm[b]
        vtm_t, r_vtm = vtm[b]
        sm_t, r_sm = sm[b]
        gcol = g_t[:, d * 8:(d + 1) * 8]
        beta = g_t[:, 16 + d * 8:16 + (d + 1) * 8]
        lnb = g_t[:, 32 + d * 8:32 + (d + 1) * 8]
        pb, r_pb = nbp()

        def mm0(e):
            e.matmul(pb[:, 0:8], lhsT=msk[:, d, :], rhs=gcol, start=True, stop=True)
            return e.matmul(pb[:, 8:16], lhsT=k.ones_f, rhs=gcol, start=True, stop=True)
        S.op("pe", mm0, reads=[r_msk, r_g, k.r_ones_f], writes=[r_pb])
        S.op("dve", lambda e: e.tensor_copy(out=X[:, :, 32:40], in_=pb[:, 0:8].unsqueeze(1).to_broadcast([128, 2, 8])), reads=[r_pb], writes=[r_X])
        S.op("dve", lambda e: e.tensor_copy(out=X[:, 1, 0:8], in_=pb[:, 0:8]), reads=[r_pb], writes=[r_X])
        S.op("dve", lambda e: e.tensor_tensor(out=X[:, 0, 0:8], in0=pb[:, 0:8], in1=lnb, op=ALU.add), reads=[r_pb, r_g], writes=[r_X])
        S.op("act", lambda e: e.activation(out=sm_t[:, 0, :], in_=pb[:, 0:8], func=AF.Exp), reads=[r_pb], writes=[r_sm])
        S.op("act", lambda e: e.activation(out=sm_t[:, 1, :], in_=pb[:, 8:16], func=AF.Exp), reads=[r_pb], writes=[r_sm])
        S.op("dve", lambda e: e.tensor_tensor(out=sm_t[:, 3, :], in0=pb[:, 8:16], in1=X[:, 1, 0:8], op=ALU.subtract), reads=[r_pb, r_X], writes=[r_sm])
        S.op("act", lambda e: e.activation(out=sm_t[:, 2, :], in_=sm_t[:, 3, :], func=AF.Exp), reads=[r_sm], writes=[r_sm])
        pt, r_pt = nbp()

        def tr0(e):
            e.transpose(out=pt[0:64, 0:128], in_=X[:, 0, :], identity=k.ident_f)
            return e.transpose(out=pt[0:64, 128:256], in_=X[:, 1, :], identity=k.ident_f)
        S.op("pe", tr0, reads=[r_X, k.r_ident_f], writes=[r_pt])
        S.op("act", lambda e: e.activation(out=L1[0:8, :], in_=pt[0:8, 0:128], func=AF.Copy), reads=[r_pt], writes=[r_L1])
        S.op("act", lambda e: e.activation(out=L2[0:8, :], in_=pt[0:8, 128:256], func=AF.Copy), reads=[r_pt], writes=[r_L2])
        S.op("dve", lambda e: e.tensor_tensor(out=R1[32:40, :, :], in0=EC[32:40, 1, :, :], in1=pt[32:40, 0:128].unsqueeze(1).to_broadcast([8, 8, 128]), op=ALU.mult),
             reads=[r_pt, r_EC], writes=[r_R1])
        S.op("dve", lambda e: e.tensor_tensor(out=R2[32:40, :, :], in0=EC[32:40, 0, :, :], in1=pt[32:40, 128:256].unsqueeze(1).to_broadcast([8, 8, 128]), op=ALU.mult),
             reads=[r_pt, r_EC], writes=[r_R2])
        kd_t, r_kd = kdec[b]
        S.op("pool", lambda e: e.tensor_tensor(out=kg, in0=ktm_t, in1=sm_t[:, 0, :].unsqueeze(2).to_broadcast([128, 8, 128]), op=ALU.mult),
             reads=[r_ktm, r_sm], writes=[r_kg])
        S.op("pool", lambda e: e.tensor_tensor(out=kd_t, in0=ktm_t, in1=sm_t[:, 2, :].unsqueeze(2).to_broadcast([128, 8, 128]), op=ALU.mult),
             reads=[r_ktm, r_sm], writes=[r_kd])
        yield
        def do_group(grp):
            hs = range(4 * grp, 4 * grp + 4)
            gs = slice(4 * grp, 4 * grp + 4)
            r_E1, r_M, r_Md, r_Mo1, r_Mo2, r_Mt, r_Tt, r_TtB = (RG[nm][grp] for nm in ("E1", "M", "Md", "Mo1", "Mo2", "Mt", "Tt", "TtB"))
            r_Ab = RG["Ab"][grp]
            rPP = [RG["PP0"][grp], RG["PP1"][grp]]
            rPT = [RG["PT0"][grp], RG["PT1"][grp]]
            Mo_list = ((Mo1r, r_Mo1), (Mo2r, r_Mo2))
            Md_list = ((Mdr, r_Md, 6), (Mo1r, r_Mo1, 7), (Mo2r, r_Mo2, 8))
            pk, r_pk = nbp()
            pd, r_pd = nbp()

            def mmk(e):
                for hh, h in enumerate(hs):
                    ins_ = e.matmul(b4(pk)[:, hh, :], lhsT=kT_t[:, h, :], rhs=kT_t[:, h, :], start=True, stop=True)
                return ins_
            S.op("pe", mmk, reads=[r_kT], writes=[r_pk])

            def mmd(e):
                for hh, h in enumerate(hs):
                    ins_ = e.matmul(b4(pd)[:, hh, :], lhsT=L1, rhs=R1[:, h, :], start=True, stop=True)
                return ins_
            S.op("pe", mmd, reads=[r_L1, r_R1], writes=[r_pd])
            S.op("dve", lambda e: e.scalar_tensor_tensor(out=E1[:, gs, :], in0=b4(pd), scalar=0.0, in1=msk[:, 2 + d, :].unsqueeze(1).to_broadcast([128, 4, 128]),
                                                         op0=ALU.min, op1=ALU.add), reads=[r_pd, r_msk], writes=[r_E1])
            S.op("act", lambda e: e.activation(out=E1[:, gs, :], in_=E1[:, gs, :], func=AF.Exp), reads=[r_E1], writes=[r_E1])
            S.op("dve", lambda e: e.tensor_tensor(out=M[:, gs, :], in0=b4(pk), in1=E1[:, gs, :], op=ALU.mult), reads=[r_pk, r_E1], writes=[r_M])
            for (dst_, rdst_, mi_) in Md_list:
                S.op("pool", lambda e, dst_=dst_, mi_=mi_: e.tensor_tensor(out=dst_[:, gs, :], in0=M[:, gs, :],
                                                                        in1=msk[:, mi_, :].unsqueeze(1).to_broadcast([128, 4, 128]), op=ALU.mult),
                     reads=[r_M, r_msk], writes=[rdst_])
            pm, r_pm = nbp(BF16)

            def trm(e):
                for hh, h in enumerate(hs):
                    ins_ = e.transpose(out=b4h(pm)[:, hh, :], in_=Md[:, h, :], identity=k.ident_b)
                return ins_
            S.op("pe", trm, reads=[r_Md, k.r_ident_b], writes=[r_pm])
            S.op("act", lambda e: e.activation(out=Mtr[:, gs, :], in_=b4h(pm), func=AF.Copy), reads=[r_pm], writes=[r_Mt])
            S.op("dve", lambda e: e.scalar_tensor_tensor(out=Ttr[:, gs, :], in0=b4h(pm), scalar=-1.0, in1=k.ident_f.unsqueeze(1).to_broadcast([128, 4, 128]),
                                                         op0=ALU.mult, op1=ALU.add), reads=[r_pm, k.r_ident_f], writes=[r_Tt])
            yield
            P_prev, rP_prev, Pt_prev, rPt_prev = Mdr, r_Md, Mtr, r_Mt
            for lvl in range(1, 1 + int(_os.environ.get('B_LEVELS', 4))):
                P_new, rP_new = PPr[lvl % 2], rPP[lvl % 2]
                Pt_new, rPt_new = PTr[lvl % 2], rPT[lvl % 2]
                pa, r_pa = nbp()

                def mma(e, P_prev=P_prev, Pt_prev=Pt_prev, pa=pa):
                    for hh, h in enumerate(hs):
                        ins_ = e.matmul(b4(pa)[:, hh, :], lhsT=Pt_prev[:, h, :], rhs=P_prev[:, h, :], start=True, stop=True)
                    return ins_
                S.op("pe", mma, reads=[rP_prev, rPt_prev], writes=[r_pa])
                S.op("act", lambda e, P_new=P_new, pa=pa: e.activation(out=P_new[:, gs, :], in_=b4(pa), func=AF.Copy), reads=[r_pa], writes=[rP_new])
                if lvl < int(_os.environ.get('B_LEVELS', 4)):
                    pbk, r_pbk = nbp()

                    def mmb(e, P_prev=P_prev, Pt_prev=Pt_prev, pbk=pbk):
                        for hh, h in enumerate(hs):
                            ins_ = e.matmul(b4(pbk)[:, hh, :], lhsT=P_prev[:, h, :], rhs=Pt_prev[:, h, :], start=True, stop=True)
                        return ins_
                    S.op("pe", mmb, reads=[rP_prev, rPt_prev], writes=[r_pbk])
                    S.op("act", lambda e, Pt_new=Pt_new, pbk=pbk: e.activation(out=Pt_new[:, gs, :], in_=b4(pbk), func=AF.Copy), reads=[r_pbk], writes=[rPt_new])
                pc, r_pc = nbp()

                def mmc(e, P_new=P_new, pc=pc):
                    for hh, h in enumerate(hs):
                        ins_ = e.matmul(b4(pc)[:, hh, :], lhsT=P_new[:, h, :], rhs=Ttr[:, h, :], start=True, stop=True)
                    return ins_
                S.op("pe", mmc, reads=[rP_new, r_Tt], writes=[r_pc])
                S.op("dve", lambda e, pc=pc: e.tensor_tensor(out=Ttr[:, gs, :], in0=Tt[:, gs, :], in1=b4(pc), op=ALU.add), reads=[r_Tt, r_pc], writes=[r_Tt])
                P_prev, rP_prev, Pt_prev, rPt_prev = P_new, rP_new, Pt_new, rPt_new
                yield
            for (Mo_, rMo_) in Mo_list:
                ptd, r_ptd = nbp(BF16)

                def trt(e, ptd=ptd):
                    for hh, h in enumerate(hs):
                        ins_ = e.transpose(out=b4h(ptd)[:, hh, :], in_=Tt[:, h, :], identity=k.ident_b)
                    return ins_
                S.op("pe", trt, reads=[r_Tt, k.r_ident_b], writes=[r_ptd])
                S.op("act", lambda e, ptd=ptd: e.activation(out=Mtr[:, gs, :], in_=b4h(ptd), func=AF.Copy), reads=[r_ptd], writes=[r_Mt])
                pa2, r_pa2 = nbp()

                def mma2(e, pa2=pa2, Mo_=Mo_):
                    for hh, h in enumerate(hs):
                        ins_ = e.matmul(b4(pa2)[:, hh, :], lhsT=Mo_[:, h, :], rhs=Ttr[:, h, :], start=True, stop=True)
                    return ins_
                S.op("pe", mma2, reads=[rMo_, r_Tt], writes=[r_pa2])
                S.op("act", lambda e, pa2=pa2: e.activation(out=E1r[:, gs, :], in_=b4(pa2), func=AF.Copy), reads=[r_pa2], writes=[r_Ab])
                pc2, r_pc2 = nbp()

                def mmc2(e, pc2=pc2):
                    for hh, h in enumerate(hs):
                        ins_ = e.matmul(b4(pc2)[:, hh, :], lhsT=Mtr[:, h, :], rhs=E1r[:, h, :], start=True, stop=True)
                    return ins_
                S.op("pe", mmc2, reads=[r_Mt, r_Ab], writes=[r_pc2])
                S.op("dve", lambda e, pc2=pc2: e.tensor_tensor(out=Ttr[:, gs, :], in0=Tt[:, gs, :], in1=b4(pc2), op=ALU.subtract), reads=[r_Tt, r_pc2], writes=[r_Tt])
                yield
            S.op("pool", lambda e: e.tensor_tensor(out=TtB[:, gs, :], in0=Tt[:, gs, :], in1=beta[:, gs].unsqueeze(2).to_broadcast([128, 4, 128]), op=ALU.mult),
                 reads=[r_Tt, r_g], writes=[r_TtB])
            pw, r_pw = nbp()

            def mmw(e):
                for hh, h in enumerate(hs):
                    ins_ = e.matmul(b4(pw)[:, hh, :], lhsT=kg[:, h, :], rhs=TtB[:, h, :], start=True, stop=True)
                return ins_
            S.op("pe", mmw, reads=[r_kg, r_TtB], writes=[r_pw])
            S.op("act", lambda e: e.activation(out=wT[b][0][:, gs, :], in_=b4(pw), func=AF.Copy), reads=[r_pw], writes=[wT_res[b][grp]])
            pu, r_pu = nbp()

            def mmu(e):
                for hh, h in enumerate(hs):
                    ins_ = e.matmul(b4(pu)[:, hh, :], lhsT=TtB[:, h, :], rhs=vtm_t[:, h, :], start=True, stop=True)
                return ins_
            S.op("pe", mmu, reads=[r_TtB, r_vtm], writes=[r_pu])
            S.op("act", lambda e: e.activation(out=u[b][0][:, gs, :], in_=b4(pu), func=AF.Copy), reads=[r_pu], writes=[u_res[b][grp]])
            yield
            if is_x:
                pq, r_pq = nbp()
                pd2, r_pd2 = nbp()

                def mmq(e):
                    for hh, h in enumerate(hs):
                        ins_ = e.matmul(b4(pq)[:, hh, :], lhsT=kT_t[:, h, :], rhs=qT_t[:, h, :], start=True, stop=True)
                    return ins_
                S.op("pe", mmq, reads=[r_kT, r_qT], writes=[r_pq])

                def mmd2(e):
                    for hh, h in enumerate(hs):
                        ins_ = e.matmul(b4(pd2)[:, hh, :], lhsT=L2, rhs=R2[:, h, :], start=True, stop=True)
                    return ins_
                S.op("pe", mmd2, reads=[r_L2, r_R2], writes=[r_pd2])
                S.op("dve", lambda e: e.scalar_tensor_tensor(out=E1[:, gs, :], in0=b4(pd2), scalar=0.0, in1=msk[:, 4 + d, :].unsqueeze(1).to_broadcast([128, 4, 128]),
                                                             op0=ALU.min, op1=ALU.add), reads=[r_pd2, r_msk], writes=[r_E1])
                S.op("act", lambda e: e.activation(out=E1[:, gs, :], in_=E1[:, gs, :], func=AF.Exp), reads=[r_E1], writes=[r_E1])
                S.op("dve", lambda e: e.tensor_tensor(out=qkT[b][0][:, gs, :], in0=b4(pq), in1=E1[:, gs, :], op=ALU.mult), reads=[r_pq, r_E1], writes=[qk_res[b][grp]])
                yield
        gens_ = [do_group(0), do_group(1)]
        while gens_:
            for g_ in list(gens_):
                try:
                    next(g_)
                    yield
                except StopIteration:
                    gens_.remove(g_)

    def seq(ui):
        d, ti = units[ui]
        b = ui % NB
        is_x = ti >= 2
        S32_t, r_S32 = S32[d]
        Sbf_t, r_Sbf = Sbf[d]
        sm_t, r_sm = sm[b]
        wT_t, u_t, qk_t = wT[b][0], u[b][0], qkT[b][0]
        qT_t, r_qT = qT[b]
        kd_t, r_kd = kdec[b]
        banks = [k.bank(4 + j) for j in range(4)]

        def grp_mm(bank2, lhs_fn, rhs_fn, reads):
            for grp in range(2):
                pb, r_pb = bank2[grp]

                def f(e, grp=grp, pb=pb):
                    for hh in range(4):
                        h = 4 * grp + hh
                        ins_ = e.matmul(b4(pb)[:, hh, :], lhsT=lhs_fn(h), rhs=rhs_fn(h), start=True, stop=True)
                    return ins_
                S.op("pe", f, reads=reads, writes=[r_pb])
        grp_mm(banks[0:2], lambda h: wT_t[:, h, :], lambda h: Sbf_t[:, h, :], wT_res[b] + [r_Sbf])
        S.op("pool", lambda e: e.tensor_tensor(out=S32_t, in0=S32_t, in1=sm_t[:, 1, :].unsqueeze(2).to_broadcast([128, 8, 128]), op=ALU.mult),
             reads=[r_S32, r_sm], writes=[r_S32])
        yield
        for grp in range(2):
            gs = slice(4 * grp, 4 * grp + 4)
            pb, r_pb = banks[grp]
            S.op("dve", lambda e, pb=pb, gs=gs: e.tensor_tensor(out=vnew[:, gs, :], in0=u_t[:, gs, :], in1=b4(pb), op=ALU.subtract),
                 reads=u_res[b] + [r_pb], writes=[r_vnew])
        yield
        if is_x:
            grp_mm(banks[2:4], lambda h: qT_t[:, h, :], lambda h: Sbf_t[:, h, :], [r_qT, r_Sbf])
            grp_mm(banks[0:2], lambda h: qk_t[:, h, :], lambda h: vnew[:, h, :], qk_res[b] + [r_vnew])
            yield
            o_tt, r_o = o_t[b]
            for grp in range(2):
                gs = slice(4 * grp, 4 * grp + 4)
                pbq, r_pbq = banks[2 + grp]
                pbc, r_pbc = banks[grp]
                S.op("dve", lambda e, pbq=pbq, gs=gs: e.tensor_tensor(out=tmpo[:, gs, :], in0=b4(pbq), in1=sm_t[:, 0, gs].unsqueeze(2).to_broadcast([128, 4, 128]), op=ALU.mult),
                     reads=[r_pbq, r_sm], writes=[r_tmpo])
                S.op("dve", lambda e, pbc=pbc, gs=gs, o_tt=o_tt: e.tensor_tensor(out=o_tt[:, gs, :], in0=tmpo[:, gs, :], in1=b4(pbc), op=ALU.add),
                     reads=[r_tmpo, r_pbc], writes=[r_o])
            xi = ti - 2
            k.store(o_s[d][0][xi * 128:(xi + 1) * 128, :], o_s[d][1], o_tt.rearrange("p h d -> p (h d)"), r_o)
            yield
        grp_mm(banks[2:4], lambda h: kd_t[:, h, :], lambda h: vnew[:, h, :], [r_kd, r_vnew])
        yield
        for grp in range(2):
            gs = slice(4 * grp, 4 * grp + 4)
            pb, r_pb = banks[2 + grp]
            S.op("dve", lambda e, pb=pb, gs=gs: e.tensor_tensor(out=S32_t[:, gs, :], in0=S32_t[:, gs, :], in1=b4(pb), op=ALU.add),
                 reads=[r_S32, r_pb], writes=[r_S32])
        S.op("act", lambda e: e.activation(out=Sbf_t, in_=S32_t, func=AF.Copy), reads=[r_S32], writes=[r_Sbf])
        yield

    import os as _os
    stop_after = int(_os.environ.get("B_UNITS", len(units)))
    pre_cut = int(_os.environ.get("B_PRE_CUT", 10000))
    do_seq = int(_os.environ.get("B_SEQ", 1))
    _pre = pre
    _seq = seq

    def pre(ui):
        for n_, _ in enumerate(_pre(ui)):
            if n_ + 1 >= pre_cut:
                return
            yield

    def seq(ui):
        if not do_seq:
            return
        yield from _seq(ui)
    loads(0)
    _drive(pre(0))
    for ui in range(stop_after):
        if ui + 1 < stop_after:
            loads(ui + 1)
            _drive(pre(ui + 1), seq(ui))
        else:
            _drive(seq(ui))
    k.b_state = (S32, Sbf)
    S.barrier()
    A.release()

def phase_c(k):
    S, A, nc, ins = k.S, k.A, k.nc, k.ins
    of_s, r_of_s = k.scr["of_s"]
    ob_s, r_ob_s = k.scr["ob_s"]
    zs_s, r_zs_s = k.scr["zs_s"]
    qmT_s, r_qmT_s = k.scr["qmT_s"]
    kmT_s, r_kmT_s = k.scr["kmT_s"]
    vm_s, r_vm_s = k.scr["vm_s"]
    yaT_s, r_yaT_s = k.scratch("yaT_s", [H, 128, TX], BF16)
    ybT_s, r_ybT_s = k.scratch("ybT_s", [H, 128, TX], BF16)
    A.mark()
    dng, r_dng = k.tile([128, 128], F32, "dng")
    k.load(dng, r_dng, ins["dng_rep"])
    NB = 2
    of_t = [k.tile([128, 8, 128], F32, "of") for _ in range(NB)]
    ob_t = [k.tile([128, 8, 128], F32, "ob") for _ in range(NB)]
    z_t = [k.tile([128, 8, 128], BF16, "z") for _ in range(NB)]
    sq, r_sq = k.tile([128, 8, 128], F32, "sq")
    ss8, r_ss8 = k.tile([128, 8], F32, "ss8")
    yb, r_yb = k.tile([128, 8, 128], BF16, "yb")
    yT = [k.tile([128, 8, 128], BF16, "yT") for _ in range(NB)]

    def c1_loads(xi):
        b = xi % NB
        rows = slice(xi * 128, (xi + 1) * 128)
        k.load(of_t[b][0], of_t[b][1], of_s[rows, :].rearrange("p (h d) -> p h d", h=8), r_of_s)
        k.load(ob_t[b][0], ob_t[b][1], ob_s[rows, :].rearrange("p (h d) -> p h d", h=8), r_ob_s, q="act")
        k.load(z_t[b][0], z_t[b][1], zs_s[rows, :].rearrange("p (h d) -> p h d", h=8), r_zs_s)

    c1_loads(0)
    for xi in range(32):
        b = xi % NB
        if xi + 1 < 32:
            c1_loads(xi + 1)
        o_, r_o = of_t[b]
        ob_, r_ob = ob_t[b]
        z_, r_z = z_t[b]
        S.op("dve", lambda e, o_=o_, ob_=ob_: e.tensor_tensor(out=o_, in0=o_, in1=ob_, op=ALU.add), reads=[r_o, r_ob], writes=[r_o])
        S.op("pool", lambda e, o_=o_: e.tensor_tensor(out=sq, in0=o_, in1=o_, op=ALU.mult), reads=[r_o], writes=[r_sq])
        S.op("dve", lambda e: e.tensor_reduce(out=ss8, in_=sq, axis=AX.X, op=ALU.add), reads=[r_sq], writes=[r_ss8])
        _rstd(k, ss8, r_ss8, 128, ss8, r_ss8)
        S.op("dve", lambda e, o_=o_: e.tensor_tensor(out=o_, in0=o_, in1=ss8.unsqueeze(2).to_broadcast([128, 8, 128]), op=ALU.mult),
             reads=[r_o, r_ss8], writes=[r_o])
        S.op("pool", lambda e, o_=o_: e.tensor_tensor(out=o_, in0=o_, in1=dng.unsqueeze(1).to_broadcast([128, 8, 128]), op=ALU.mult),
             reads=[r_o, r_dng], writes=[r_o])
        S.op("pool", lambda e, o_=o_, z_=z_: e.tensor_tensor(out=yb, in0=o_, in1=z_, op=ALU.mult), reads=[r_o, r_z], writes=[r_yb])
        pb, r_pb = _nb(k, BF16)
        pv = pb.rearrange("p (a b) -> p a b", b=128)

        def tr(e, pv=pv):
            for j in range(8):
                ins_ = e.transpose(out=pv[:, j, :], in_=yb[:, j, :], identity=k.ident_b)
            return ins_
        S.op("pe", tr, reads=[r_yb, k.r_ident_b], writes=[r_pb])
        yT_t, r_yT = yT[b]
        S.op("act", lambda e, pv=pv, yT_t=yT_t: e.activation(out=yT_t, in_=pv, func=AF.Copy), reads=[r_pb], writes=[r_yT])
        k.store(yaT_s[:, :, xi * 128:(xi + 1) * 128].rearrange("h d t -> d h t"), r_yaT_s, yT_t, r_yT)
    S.barrier()
    A.release()
    A.mark()
    Kn = [k.tile([128, T], BF16, "Kn") for _ in range(2)]
    Kr = [k.tile([64, T], BF16, "Kr") for _ in range(2)]
    Vh = [k.tile([128, NT, 128], BF16, "Vh") for _ in range(2)]
    Qn = [k.tile([128, 512], BF16, "Qn") for _ in range(2)]
    Qr = [k.tile([64, 512], BF16, "Qr") for _ in range(2)]
    NP = 4
    PT = [k.tile([128, 512], BF16, "PT") for _ in range(NP)]
    rinv, r_rinv = k.tile([128, 512], F32, "rinv")
    yo = [k.tile([128, 512], BF16, "yo") for _ in range(2)]
    vmv = vm_s.rearrange("(n p) c -> p n c", p=128)

    def head_loads(h):
        b = h % 2
        k.load(Kn[b][0], Kn[b][1], kmT_s[h, 0:128, :], r_kmT_s)
        k.load(Kr[b][0], Kr[b][1], kmT_s[h, 128:192, :], r_kmT_s, q="act")
        k.load(Vh[b][0], Vh[b][1], vmv[:, :, h * 128:(h + 1) * 128], r_vm_s)

    def q_loads(h, qg):
        b = (h * 8 + qg) % 2
        k.load(Qn[b][0], Qn[b][1], qmT_s[h, 0:128, qg * 512:(qg + 1) * 512], r_qmT_s)
        k.load(Qr[b][0], Qr[b][1], qmT_s[h, 128:192, qg * 512:(qg + 1) * 512], r_qmT_s, q="act")

    head_loads(0)
    q_loads(0, 0)
    sbank = [0]
    it = 0
    for h in range(H):
        if h + 1 < H:
            head_loads(h + 1)
        Kn_t, r_Kn = Kn[h % 2]
        Kr_t, r_Kr = Kr[h % 2]
        V_t, r_V = Vh[h % 2]
        for qg in range(8):
            gi = h * 8 + qg
            nxt = gi + 1
            if nxt < H * 8:
                q_loads(nxt // 8, nxt % 8)
            Qn_t, r_Qn = Qn[gi % 2]
            Qr_t, r_Qr = Qr[gi % 2]
            po, r_po = k.bank(4 + gi % 2)
            pr, r_pr = k.bank(6 + gi % 2)
            sb = {}

            def emit_s(kt):
                bnk = sbank[0]
                sbank[0] = (bnk + 1) % 4
                ps_, r_ps = k.bank(bnk)

                def f(e, ps_=ps_, kt=kt, Kn_t=Kn_t, Kr_t=Kr_t, Qn_t=Qn_t, Qr_t=Qr_t):
                    e.matmul(ps_, lhsT=Kn_t[:, kt * 128:(kt + 1) * 128], rhs=Qn_t, start=True, stop=False)
                    return e.matmul(ps_, lhsT=Kr_t[:, kt * 128:(kt + 1) * 128], rhs=Qr_t, start=False, stop=True)
                S.op("pe", f, reads=[r_Kn, r_Kr, r_Qn, r_Qr], writes=[r_ps])
                pt_, r_pt = PT[kt % NP]
                S.op("act", lambda e, ps_=ps_, pt_=pt_: e.activation(out=pt_, in_=ps_, func=AF.Exp), reads=[r_ps], writes=[r_pt])
                sb[kt] = (pt_, r_pt)

            def emit_pv(kt):
                pt_, r_pt = sb.pop(kt)

                def f(e, pt_=pt_, kt=kt, po=po, pr=pr, V_t=V_t):
                    e.matmul(po, lhsT=V_t[:, kt, :], rhs=pt_, start=(kt == 0), stop=(kt == NT - 1))
                    return e.matmul(pr, lhsT=k.ones_b, rhs=pt_, start=(kt == 0), stop=(kt == NT - 1))
                S.op("pe", f, reads=[r_V, r_pt, k.r_ones_b], writes=[r_po, r_pr])

            LOOK = 2
            for kt in range(min(LOOK, NT)):
                emit_s(kt)
            for kt in range(NT):
                if kt + LOOK < NT:
                    emit_s(kt + LOOK)
                emit_pv(kt)
            S.op("dve", lambda e, pr=pr: e.reciprocal(out=rinv, in_=pr), reads=[r_pr], writes=[r_rinv])
            yo_t, r_yo = yo[gi % 2]
            S.op("dve", lambda e, po=po, yo_t=yo_t: e.tensor_tensor(out=yo_t, in0=po, in1=rinv, op=ALU.mult), reads=[r_po, r_rinv], writes=[r_yo])
            k.store(ybT_s[h, :, qg * 512:(qg + 1) * 512], r_ybT_s, yo_t, r_yo)
    S.barrier()
    A.release()

def phase_d(k):
    S, A, nc, ins = k.S, k.A, k.nc, k.ins
    yaT_s, r_yaT_s = k.scr["yaT_s"]
    ybT_s, r_ybT_s = k.scr["ybT_s"]
    sgT_s, r_sgT_s = k.scr["sgT_s"]
    xmid_s, r_xmid_s = k.scratch("xmid_s", [TX, D], F32)
    h2_s, r_h2_s = k.scratch("h2_s", [TX, D], BF16)
    aff_s, r_aff_s = k.scratch("aff_s", [TX, 16], F32)
    affT_s, r_affT_s = k.scratch("affT_s", [16, TX], F32)
    A.mark()
    woa, r_woa = k.tile([128, 8, D], BF16, "woa")
    wob, r_wob = k.tile([128, 8, D], BF16, "wob")
    wo, r_wo = k.tile([128, 8, D], BF16, "wo")
    rw, r_rw = k.tile([128, 8, 16], F32, "rw")
    for (dst, rdst, nm) in ((woa, r_woa, "w_out_a"), (wob, r_wob, "w_out_b"), (wo, r_wo, "w_o")):
        src = ins[nm].rearrange("(kc p) n -> p kc n", p=128)
        for hf in range(2):
            k.wload(dst[:, 4 * hf:4 * hf + 4, :], rdst, src[:, 4 * hf:4 * hf + 4, :])
    k.load(rw, r_rw, ins["router_w"].rearrange("(kc p) n -> p kc n", p=128))
    NB = 2
    yaT = [k.tile([128, 8, 512], BF16, "yaT") for _ in range(NB)]
    ybT = [k.tile([128, 8, 512], BF16, "ybT") for _ in range(NB)]
    gA = [k.tile([128, 8, 512], BF16, "gA") for _ in range(NB)]
    gB = [k.tile([128, 8, 512], BF16, "gB") for _ in range(NB)]
    mg, r_mg = k.tile([128, 8, 512], BF16, "mg")
    t1 = [k.tile([128, 512], F32, "t1") for _ in range(2)]
    t2 = [k.tile([128, 512], F32, "t2") for _ in range(2)]
    xt = [k.tile([128, D], F32, "xt") for _ in range(NB)]
    xm = [k.tile([128, D], F32, "xm") for _ in range(NB)]
    junk, r_junk = k.tile([128, D], BF16, "junk")
    ssd = [k.tile([128, 8], F32, "ssd") for _ in range(NB)]
    h2f, r_h2f = k.tile([128, D], F32, "h2f")
    h2b = [k.tile([128, D], BF16, "h2b") for _ in range(NB)]
    h2T, r_h2T = k.tile([128, 8, 128], F32, "h2T")
    ex = [k.tile([128, 16], F32, "ex") for _ in range(NB)]
    affT = [k.tile([16, 128], F32, "affT") for _ in range(NB)]

    def g_loads(g):
        b = g % NB
        cols = slice(g * 512, (g + 1) * 512)
        k.load(yaT[b][0], yaT[b][1], yaT_s[:, :, cols].rearrange("h d t -> d h t"), r_yaT_s)
        k.load(ybT[b][0], ybT[b][1], ybT_s[:, :, cols].rearrange("h d t -> d h t"), r_ybT_s, q="act")
        k.load(gA[b][0], gA[b][1], sgT_s[0:8, :, cols].rearrange("h d t -> d h t"), r_sgT_s)
        k.load(gB[b][0], gB[b][1], sgT_s[8:16, :, cols].rearrange("h d t -> d h t"), r_sgT_s, q="act")

    g_loads(0)
    for g in range(8):
        b = g % NB
        if g + 1 < 8:
            g_loads(g + 1)
        ya_, r_ya = yaT[b]
        yb_, r_yb = ybT[b]
        gA_, r_gA = gA[b]
        gB_, r_gB = gB[b]
        for oc in range(8):
            pa, r_pa = _nb(k)
            pbk, r_pbk = _nb(k)

            def mma(e, pa=pa, oc=oc, ya_=ya_):
                for kc in range(8):
                    ins_ = e.matmul(pa, lhsT=woa[:, kc, oc * 128:(oc + 1) * 128], rhs=ya_[:, kc, :], start=(kc == 0), stop=(kc == 7))
                return ins_
            S.op("pe", mma, reads=[r_woa, r_ya], writes=[r_pa])

            def mmb(e, pbk=pbk, oc=oc, yb_=yb_):
                for kc in range(8):
                    ins_ = e.matmul(pbk, lhsT=wob[:, kc, oc * 128:(oc + 1) * 128], rhs=yb_[:, kc, :], start=(kc == 0), stop=(kc == 7))
                return ins_
            S.op("pe", mmb, reads=[r_wob, r_yb], writes=[r_pbk])
            t1_, r_t1 = t1[oc % 2]
            t2_, r_t2 = t2[oc % 2]
            S.op("dve", lambda e, pa=pa, oc=oc, t1_=t1_, gA_=gA_: e.tensor_tensor(out=t1_, in0=pa, in1=gA_[:, oc, :], op=ALU.mult), reads=[r_pa, r_gA], writes=[r_t1])
            S.op("dve", lambda e, pbk=pbk, oc=oc, t2_=t2_, gB_=gB_: e.tensor_tensor(out=t2_, in0=pbk, in1=gB_[:, oc, :], op=ALU.mult), reads=[r_pbk, r_gB], writes=[r_t2])
            S.op("pool", lambda e, oc=oc, t1_=t1_, t2_=t2_: e.tensor_tensor(out=mg[:, oc, :], in0=t1_, in1=t2_, op=ALU.add), reads=[r_t1, r_t2], writes=[r_mg])
        for tt in range(4):
            ti = g * 4 + tt
            tb = ti % NB
            rows = slice(ti * 128, (ti + 1) * 128)
            x_, r_x = xt[tb]
            xm_, r_xm = xm[tb]
            ss_, r_ss = ssd[tb]
            k.load(x_, r_x, ins["x"][rows, :])
            for hf in range(2):
                pm, r_pm = _nb(k)

                def mmo(e, pm=pm, hf=hf, tt=tt):
                    for kc in range(8):
                        ins_ = e.matmul(pm, lhsT=mg[:, kc, tt * 128:(tt + 1) * 128], rhs=wo[:, kc, hf * 512:(hf + 1) * 512], start=(kc == 0), stop=(kc == 7))
                    return ins_
                S.op("pe", mmo, reads=[r_mg, r_wo], writes=[r_pm])
                S.op("dve", lambda e, pm=pm, hf=hf, xm_=xm_: e.tensor_tensor(out=xm_[:, hf * 512:(hf + 1) * 512], in0=pm, in1=k.gate1_row[:, hf * 512:(hf + 1) * 512], op=ALU.mult),
                     reads=[r_pm, k.r_gate1], writes=[r_xm])
            S.op("pool", lambda e, xm_=xm_, x_=x_: e.tensor_tensor(out=xm_, in0=xm_, in1=x_, op=ALU.add), reads=[r_xm, r_x], writes=[r_xm])
            k.store(xmid_s[rows, :], r_xmid_s, xm_, r_xm)
            k.store(k.out[rows, :], k.out_res, xm_, r_xm)
            S.op("act", lambda e, xm_=xm_, ss_=ss_: e.activation(out=junk, in_=xm_, func=AF.Square, accum_out=ss_[:, 0:1]), reads=[r_xm], writes=[r_junk, r_ss])
            _rstd(k, ss_[:, 0:1], r_ss, D, ss_[:, 1:2], r_ss)
            S.op("act", lambda e, xm_=xm_, ss_=ss_: e.activation(out=h2f, in_=xm_, func=AF.Copy, scale=ss_[:, 1:2]), reads=[r_xm, r_ss], writes=[r_h2f])
            S.op("pool", lambda e: e.tensor_tensor(out=h2f, in0=h2f, in1=k.s2_row, op=ALU.mult), reads=[r_h2f, k.r_s2row], writes=[r_h2f])
            S.op("pool", lambda e: e.tensor_tensor(out=h2f, in0=h2f, in1=k.shift2_row, op=ALU.add), reads=[r_h2f, k.r_shift2], writes=[r_h2f])
            h2b_, r_h2b = h2b[tb]
            S.op("act", lambda e, h2b_=h2b_: e.activation(out=h2b_, in_=h2f, func=AF.Copy), reads=[r_h2f], writes=[r_h2b])
            k.store(h2_s[rows, :], r_h2_s, h2b_, r_h2b)
            for hf in range(2):
                pt, r_pt = _nb(k)
                pv = pt.rearrange("p (a b) -> p a b", b=128)

                def trh(e, pv=pv, hf=hf):
                    for j in range(4):
                        ins_ = e.transpose(out=pv[:, j, :], in_=h2f[:, (hf * 4 + j) * 128:(hf * 4 + j + 1) * 128], identity=k.ident_f)
                    return ins_
                S.op("pe", trh, reads=[r_h2f, k.r_ident_f], writes=[r_pt])
                if hf == 0:
                    S.op("act", lambda e, pv=pv: e.activation(out=h2T[:, 0:4, :], in_=pv, func=AF.Copy), reads=[r_pt], writes=[r_h2T])
                else:
                    S.op("dve", lambda e, pv=pv: e.tensor_copy(out=h2T[:, 4:8, :], in_=pv), reads=[r_pt], writes=[r_h2T])
            pl, r_pl = _nb(k)

            def mml(e, pl=pl):
                for kc in range(8):
                    ins_ = e.matmul(pl[:, 0:16], lhsT=h2T[:, kc, :], rhs=rw[:, kc, :], start=(kc == 0), stop=(kc == 7))
                return ins_
            S.op("pe", mml, reads=[r_h2T, r_rw], writes=[r_pl])
            ex_, r_ex = ex[tb]
            S.op("dve", lambda e, pl=pl, ss_=ss_: e.tensor_reduce(out=ss_[:, 2:3], in_=pl[:, 0:16], axis=AX.X, op=ALU.max), reads=[r_pl], writes=[r_ss])
            S.op("dve", lambda e, ss_=ss_: e.tensor_scalar(out=ss_[:, 3:4], in0=ss_[:, 2:3], scalar1=-1.0, scalar2=None, op0=ALU.mult), reads=[r_ss], writes=[r_ss])
            S.op("act", lambda e, pl=pl, ss_=ss_, ex_=ex_: e.activation(out=ex_, in_=pl[:, 0:16], func=AF.Exp, bias=ss_[:, 3:4], accum_out=ss_[:, 4:5]),
                 reads=[r_pl, r_ss], writes=[r_ex, r_ss])
            S.op("dve", lambda e, ss_=ss_: e.reciprocal(out=ss_[:, 5:6], in_=ss_[:, 4:5]), reads=[r_ss], writes=[r_ss])
            S.op("dve", lambda e, ss_=ss_, ex_=ex_: e.tensor_scalar(out=ex_, in0=ex_, scalar1=ss_[:, 5:6], scalar2=None, op0=ALU.mult), reads=[r_ex, r_ss], writes=[r_ex])
            k.store(aff_s[rows, :], r_aff_s, ex_, r_ex)
            pt2, r_pt2 = _nb(k)
            S.op("pe", lambda e, pt2=pt2, ex_=ex_: e.transpose(out=pt2[0:16, 0:128], in_=ex_, identity=k.ident_f), reads=[r_ex, k.r_ident_f], writes=[r_pt2])
            aT_, r_aT = affT[tb]
            S.op("act", lambda e, pt2=pt2, aT_=aT_: e.activation(out=aT_, in_=pt2[0:16, 0:128], func=AF.Copy), reads=[r_pt2], writes=[r_aT])
            k.store(affT_s[:, rows], r_affT_s, aT_, r_aT)
    S.barrier()
    A.release()

NE = 16
CAP = 512
FF = 1408
NFC = 11


def phase_e(k):
    S, A, nc, ins = k.S, k.A, k.nc, k.ins
    aff_s, r_aff_s = k.scr["aff_s"]
    affT_s, r_affT_s = k.scr["affT_s"]
    h2_s, r_h2_s = k.scr["h2_s"]
    xmid_s, r_xmid_s = k.scr["xmid_s"]
    posmT_s, r_posmT_s = k.scratch("posmT_s", [NE, TX], F32)
    gc_s, r_gc_s = k.scratch("gc_s", [NE, 128, 4], F32)
    idx_s, r_idx_s = k.scratch("idx_s", [NE, 128, 4], I32)
    A.off = k.off_after_gate2
    A.mark()
    cst, r_cst = k.tile([128, 1024], F32, "cst")
    k.load(cst, r_cst, ins["consts"])
    blk, r_blk = k.tile([128, 128], F32, "blk")
    k.load(blk, r_blk, ins["moe_blk"])
    sel8, r_sel8 = k.tile([128, 16], F32, "sel8")
    k.load(sel8, r_sel8, ins["moe_sel8"])
    tris, r_tris = k.tile([128, 128], BF16, "tris")
    k.wload(tris, r_tris, ins["moe_tris"])
    iota_c = cst[:, 0:512]
    A.mark()
    A8, r_A8 = k.tile([128, 512], F32, "A8")
    k.load(A8, r_A8, affT_s.rearrange("e (s t) -> (e s) t", s=8), r_affT_s)
    junk, r_junk = k.tile([128, 512], F32, "junk")
    sc, r_sc = k.tile([128, 16], F32, "sc")
    S.op("pool", lambda e: e.memset(sc, 0.0), writes=[r_sc])
    S.op("pool", lambda e: e.memset(sc[:, 1:2], 1.0), reads=[r_sc], writes=[r_sc])
    for it in range(30):
        S.op("dve", lambda e: e.tensor_tensor(out=sc[:, 2:3], in0=sc[:, 0:1], in1=sc[:, 1:2], op=ALU.add), reads=[r_sc], writes=[r_sc])
        S.op("dve", lambda e: e.tensor_scalar(out=sc[:, 2:3], in0=sc[:, 2:3], scalar1=0.5, scalar2=None, op0=ALU.mult), reads=[r_sc], writes=[r_sc])
        S.op("dve", lambda e: e.tensor_scalar(out=junk, in0=A8, scalar1=sc[:, 2:3], scalar2=0.0, op0=ALU.is_ge, op1=ALU.add, accum_out=sc[:, 3:4]),
             reads=[r_A8, r_sc], writes=[r_junk, r_sc])
        pb, r_pb = _nb(k)
        S.op("pe", lambda e, pb=pb: e.matmul(pb[:, 0:1], lhsT=blk, rhs=sc[:, 3:4], start=True, stop=True), reads=[r_blk, r_sc], writes=[r_pb])
        S.op("dve", lambda e, pb=pb: e.tensor_scalar(out=sc[:, 4:5], in0=pb[:, 0:1], scalar1=CAP - 0.5, scalar2=None, op0=ALU.is_ge), reads=[r_pb], writes=[r_sc])
        S.op("dve", lambda e: e.tensor_scalar(out=sc[:, 5:6], in0=sc[:, 4:5], scalar1=-1.0, scalar2=1.0, op0=ALU.mult, op1=ALU.add), reads=[r_sc], writes=[r_sc])
        S.op("dve", lambda e: e.tensor_tensor(out=sc[:, 6:7], in0=sc[:, 2:3], in1=sc[:, 0:1], op=ALU.subtract), reads=[r_sc], writes=[r_sc])
        S.op("dve", lambda e: e.tensor_tensor(out=sc[:, 7:8], in0=sc[:, 1:2], in1=sc[:, 2:3], op=ALU.subtract), reads=[r_sc], writes=[r_sc])
        S.op("dve", lambda e: e.scalar_tensor_tensor(out=sc[:, 0:1], in0=sc[:, 6:7], scalar=sc[:, 4:5], in1=sc[:, 0:1], op0=ALU.mult, op1=ALU.add), reads=[r_sc], writes=[r_sc])
        S.op("dve", lambda e: e.scalar_tensor_tensor(out=sc[:, 1:2], in0=sc[:, 7:8], scalar=sc[:, 4:5], in1=sc[:, 2:3], op0=ALU.mult, op1=ALU.add), reads=[r_sc], writes=[r_sc])
        S.op("dve", lambda e: e.memset(sc[:, 3:4], 0.0), reads=[r_sc], writes=[r_sc])
    thrrep, r_thrrep = k.tile([128, 128], F32, "thrrep")
    S.op("dve", lambda e: e.tensor_copy(out=thrrep, in_=sc[:, 0:1].to_broadcast([128, 128])), reads=[r_sc], writes=[r_thrrep])
    pb, r_pb = _nb(k)
    S.op("pe", lambda e, pb=pb: e.matmul(pb[:, 0:16], lhsT=thrrep, rhs=sel8, start=True, stop=True), reads=[r_thrrep, r_sel8], writes=[r_pb])
    thr_row, r_thr = k.tile([128, 16], F32, "thr_row")
    S.op("act", lambda e, pb=pb: e.activation(out=thr_row, in_=pb[:, 0:16], func=AF.Copy), reads=[r_pb], writes=[r_thr])
    import os as _os
    if _os.environ.get("E_DBG"):
        k.dump("sc", sc, r_sc, [128, 16])
        k.dump("thr_row", thr_row, r_thr, [128, 16])
        k.dump("A8", A8, r_A8, [128, 512])
        S.barrier()
        A.release()
        A.release()
        return
    aff, r_aff = k.tile([128, 32, 16], F32, "aff")
    k.load(aff, r_aff, aff_s.rearrange("(n p) e -> p n e", p=128), r_aff_s)
    maskf, r_maskf = k.tile([128, 32, 16], F32, "maskf")
    maskb, r_maskb = k.tile([128, 32, 16], BF16, "maskb")
    posm, r_posm = k.tile([128, 32, 16], F32, "posm")
    parts, r_parts = k.tile([128, 32, 16, 5], BF16, "parts")
    tokp, r_tokp = k.tile([128, 32, 2], F32, "tokp")
    k.load(tokp, r_tokp, ins["moe_tok"])
    rem, r_rem = k.tile([128, 32, 16], F32, "rem")
    S.op("dve", lambda e: e.tensor_tensor(out=maskf, in0=aff, in1=thr_row.unsqueeze(1).to_broadcast([128, 32, 16]), op=ALU.is_ge),
         reads=[r_aff, r_thr], writes=[r_maskf])
    S.op("act", lambda e: e.activation(out=maskb, in_=maskf, func=AF.Copy), reads=[r_maskf], writes=[r_maskb])
    pp, r_pp = _nb(k)
    ppv = pp.rearrange("p (n e) -> p n e", e=16)

    def mmpos(e):
        for n in range(32):
            for m in range(n):
                e.matmul(ppv[:, n, :], lhsT=k.ones_b, rhs=maskb[:, m, :], start=(m == 0), stop=False)
            ins_ = e.matmul(ppv[:, n, :], lhsT=tris, rhs=maskb[:, n, :], start=(n == 0), stop=True)
        return ins_
    S.op("pe", mmpos, reads=[r_maskb, k.r_ones_b, r_tris], writes=[r_pp])
    S.op("dve", lambda e: e.scalar_tensor_tensor(out=posm, in0=ppv, scalar=1.0, in1=maskf, op0=ALU.add, op1=ALU.mult), reads=[r_pp, r_maskf], writes=[r_posm])
    S.op("dve", lambda e: e.tensor_scalar(out=posm, in0=posm, scalar1=-1.0, scalar2=None, op0=ALU.add), reads=[r_posm], writes=[r_posm])
    S.op("act", lambda e: e.activation(out=parts[:, :, :, 0], in_=aff, func=AF.Copy), reads=[r_aff], writes=[r_parts])
    S.op("dve", lambda e: e.tensor_tensor(out=rem, in0=aff, in1=parts[:, :, :, 0], op=ALU.subtract), reads=[r_aff, r_parts], writes=[r_rem])
    S.op("act", lambda e: e.activation(out=parts[:, :, :, 1], in_=rem, func=AF.Copy), reads=[r_rem], writes=[r_parts])
    S.op("dve", lambda e: e.tensor_tensor(out=rem, in0=rem, in1=parts[:, :, :, 1], op=ALU.subtract), reads=[r_rem, r_parts], writes=[r_rem])
    S.op("act", lambda e: e.activation(out=parts[:, :, :, 2], in_=rem, func=AF.Copy), reads=[r_rem], writes=[r_parts])
    S.op("dve", lambda e: e.tensor_copy(out=parts[:, :, :, 3:5], in_=tokp.unsqueeze(2).to_broadcast([128, 32, 16, 2])), reads=[r_tokp, r_parts], writes=[r_parts])
    pmTs = [k.tile([16, 512], F32, "pmT") for _ in range(2)]
    for g in range(8):
        pt, r_pt = _nb(k)

        def trp(e, pt=pt, g=g):
            for j in range(4):
                ins_ = e.transpose(out=pt[0:16, j * 128:(j + 1) * 128], in_=posm[:, g * 4 + j, :], identity=k.ident_f)
            return ins_
        S.op("pe", trp, reads=[r_posm, k.r_ident_f], writes=[r_pt])
        pmT, r_pmT = pmTs[g % 2]
        S.op("act", lambda e, pt=pt, pmT=pmT: e.activation(out=pmT, in_=pt[0:16, :], func=AF.Copy), reads=[r_pt], writes=[r_pmT])
        k.store(posmT_s[:, g * 512:(g + 1) * 512], r_posmT_s, pmT, r_pmT)
    if _os.environ.get("E_STOP") == "e1":
        S.barrier(); A.release(); A.release(); return
    Sel = [k.tile([128, 32, CAP], BF16, "Sel") for _ in range(2)]
    Sel_res = [(Res("sel0"), Res("sel1")) for _ in range(2)]
    gcs = [k.tile([128, 4], F32, "gcs") for _ in range(2)]
    idf = [k.tile([128, 4], F32, "idf") for _ in range(2)]
    idi = [k.tile([128, 4], I32, "idi") for _ in range(2)]
    for ex in range(NE):
        Sel_t, _ = Sel[ex % 2]
        r_Sel0, r_Sel1 = Sel_res[ex % 2]
        gcs_t, r_gcs = gcs[ex % 2]
        idf_t, r_idf = idf[ex % 2]
        idi_t, r_idi = idi[ex % 2]

        def bsel(e, Sel_t=Sel_t, ex=ex, par=0):
            for n in range(par, 32, 2):
                ins_ = e.tensor_scalar(out=Sel_t[:, n, :], in0=iota_c, scalar1=posm[:, n, ex:ex + 1], scalar2=None, op0=ALU.is_equal)
            return ins_
        if _os.environ.get("E_X") != "nodve":
            S.op("dve", lambda e, f=bsel: f(e, par=0), reads=[r_cst, r_posm], writes=[r_Sel0])
        S.op("dve", lambda e, f=bsel: f(e, par=1), reads=[r_cst, r_posm], writes=[r_Sel1])
        pq, r_pq = _nb(k)

        def mmgate(e, pq=pq, Sel_t=Sel_t, ex=ex):
            for cc in range(4):
                for n in range(32):
                    ins_ = e.matmul(pq[:, cc * 8:cc * 8 + 5], lhsT=Sel_t[:, n, cc * 128:(cc + 1) * 128], rhs=parts[:, n, ex, :], start=(n == 0), stop=(n == 31))
            return ins_
        if _os.environ.get("E_X") != "nomm":
            S.op("pe", mmgate, reads=[r_Sel0, r_Sel1, r_parts], writes=[r_pq])
        pq3 = pq[:, 0:32].rearrange("p (a b) -> p a b", b=8)
        S.op("dve", lambda e, pq3=pq3, gcs_t=gcs_t: e.tensor_reduce(out=gcs_t, in_=pq3[:, :, 0:3], axis=AX.X, op=ALU.add), reads=[r_pq], writes=[r_gcs])
        S.op("dve", lambda e, pq3=pq3, idf_t=idf_t: e.tensor_scalar(out=idf_t, in0=pq3[:, :, 3], scalar1=128.0, scalar2=None, op0=ALU.mult), reads=[r_pq], writes=[r_idf])
        S.op("dve", lambda e, pq3=pq3, idf_t=idf_t: e.tensor_tensor(out=idf_t, in0=idf_t, in1=pq3[:, :, 4], op=ALU.add), reads=[r_pq, r_idf], writes=[r_idf])
        S.op("dve", lambda e, idf_t=idf_t, idi_t=idi_t: e.tensor_copy(out=idi_t, in_=idf_t), reads=[r_idf], writes=[r_idi])
        k.store(gc_s[ex], r_gc_s, gcs_t, r_gcs)
        k.store(idx_s[ex], r_idx_s, idi_t, r_idi)
    S.barrier()
    A.release()
    if _os.environ.get("E_STOP") == "ea1":
        A.release(); return
    A.mark()
    U32 = mybir.dt.uint32
    ig = S.pool("ig", 4)
    wg = [k.tile([128, 8, FF], BF16, "wg") for _ in range(2)]
    wu = [k.tile([128, 8, FF], BF16, "wu") for _ in range(2)]
    wd = [k.tile([128, NFC, D], BF16, "wd") for _ in range(1)]
    xg = [k.tile([128, 4, D], BF16, "xg") for _ in range(2)]
    idx2 = [k.tile([128, 4], I32, "idx2") for _ in range(2)]
    gc2 = [k.tile([128, 4], F32, "gc2") for _ in range(2)]
    xeT_t, r_xeT = k.tile([128, 8, CAP], BF16, "xeT")
    hid, r_hid = k.tile([128, NFC, CAP], BF16, "hid")
    sg = [k.tile([128, CAP], F32, "sg") for _ in range(2)]
    yef = [k.tile([128, D], F32, "yef") for _ in range(4)]
    r_outacc = Res("outacc")
    h2_rows = h2_s

    def w_loads(ex):
        b = ex % 2
        srcg = ins["w_gate"][ex].rearrange("(kc p) f -> p kc f", p=128)
        srcu = ins["w_up"][ex].rearrange("(kc p) f -> p kc f", p=128)
        for j in range(4):
            k.wload(wg[b][0][:, 2 * j:2 * j + 2, :], wg[b][1], srcg[:, 2 * j:2 * j + 2, :])
        for j in range(4):
            k.wload(wu[b][0][:, 2 * j:2 * j + 2, :], wu[b][1], srcu[:, 2 * j:2 * j + 2, :])

    def wd_loads(ex):
        srcd = ins["w_down"][ex].rearrange("(fc p) d -> p fc d", p=128)
        for (a0, a1) in ((0, 3), (3, 6), (6, 9), (9, 11)):
            k.wload(wd[0][0][:, a0:a1, :], wd[0][1], srcd[:, a0:a1, :])

    def g_loads(ex):
        b = ex % 2
        k.load(idx2[b][0], idx2[b][1], idx_s[ex], r_idx_s)
        k.load(gc2[b][0], gc2[b][1], gc_s[ex], r_gc_s, q="act")
        for cc in range(4):
            S.dma("pool", ig, lambda e, b=b, cc=cc: e.indirect_dma_start(out=xg[b][0][:, cc, :], out_offset=None, in_=h2_rows,
                                                                       in_offset=bass.IndirectOffsetOnAxis(idx2[b][0].bitcast(U32)[:, cc:cc + 1], 0)),
                  reads=[idx2[b][1], r_h2_s], writes=[xg[b][1]])

    g_loads(0)
    w_loads(0)
    for ex in range(NE):
        b = ex % 2
        if ex + 1 < NE:
            g_loads(ex + 1)
            w_loads(ex + 1)
        wd_loads(ex)
        wg_t, r_wg = wg[b]
        wu_t, r_wu = wu[b]
        wd_t, r_wd = wd[0]
        xg_t, r_xg = xg[b]
        gc_t, r_gc = gc2[b]
        id_t, r_id = idx2[b]
        for kc in range(8):
            pt, r_pt = _nb(k, BF16)

            def trx(e, pt=pt, kc=kc, xg_t=xg_t):
                for cc in range(4):
                    ins_ = e.transpose(out=pt[:, cc * 128:(cc + 1) * 128], in_=xg_t[:, cc, kc * 128:(kc + 1) * 128], identity=k.ident_b)
                return ins_
            S.op("pe", trx, reads=[r_xg, k.r_ident_b], writes=[r_pt])
            if kc % 2 == 0:
                S.op("act", lambda e, pt=pt, kc=kc: e.activation(out=xeT_t[:, kc, :], in_=pt[:, 0:512], func=AF.Copy), reads=[r_pt], writes=[r_xeT])
            else:
                S.op("dve", lambda e, pt=pt, kc=kc: e.tensor_copy(out=xeT_t[:, kc, :], in_=pt[:, 0:512]), reads=[r_pt], writes=[r_xeT])
        for fc in range(NFC):
            pg, r_pg = _nb(k)
            pu, r_pu = _nb(k)

            def mmG(e, pg=pg, fc=fc, wg_t=wg_t):
                for kc in range(8):
                    ins_ = e.matmul(pg, lhsT=wg_t[:, kc, fc * 128:(fc + 1) * 128], rhs=xeT_t[:, kc, :], start=(kc == 0), stop=(kc == 7))
                return ins_
            S.op("pe", mmG, reads=[r_wg, r_xeT], writes=[r_pg])

            def mmU(e, pu=pu, fc=fc, wu_t=wu_t):
                for kc in range(8):
                    ins_ = e.matmul(pu, lhsT=wu_t[:, kc, fc * 128:(fc + 1) * 128], rhs=xeT_t[:, kc, :], start=(kc == 0), stop=(kc == 7))
                return ins_
            S.op("pe", mmU, reads=[r_wu, r_xeT], writes=[r_pu])
            sg_t, r_sg = sg[fc % 2]
            S.op("act", lambda e, pg=pg, sg_t=sg_t: e.activation(out=sg_t, in_=pg, func=AF.Silu), reads=[r_pg], writes=[r_sg])
            S.op("dve", lambda e, pu=pu, sg_t=sg_t, fc=fc: e.tensor_tensor(out=hid[:, fc, :], in0=pu, in1=sg_t, op=ALU.mult), reads=[r_pu, r_sg], writes=[r_hid])
        for cc in range(4):
            y_t, r_y = yef[cc]
            for hf in range(2):
                pd, r_pd = _nb(k)

                def mmD(e, pd=pd, cc=cc, hf=hf):
                    for fc in range(NFC):
                        ins_ = e.matmul(pd, lhsT=hid[:, fc, cc * 128:(cc + 1) * 128], rhs=wd_t[:, fc, hf * 512:(hf + 1) * 512], start=(fc == 0), stop=(fc == NFC - 1))
                    return ins_
                S.op("pe", mmD, reads=[r_hid, r_wd], writes=[r_pd])
                S.op("act", lambda e, pd=pd, cc=cc, hf=hf, y_t=y_t, gc_t=gc_t: e.activation(out=y_t[:, hf * 512:(hf + 1) * 512], in_=pd, func=AF.Copy, scale=gc_t[:, cc:cc + 1]),
                     reads=[r_pd, r_gc], writes=[r_y])
            S.op("pool", lambda e, y_t=y_t: e.tensor_tensor(out=y_t, in0=y_t, in1=k.gate2_row, op=ALU.mult), reads=[r_y, k.r_gate2], writes=[r_y])
            S.dma("pool", ig, lambda e, y_t=y_t, id_t=id_t, cc=cc: e.indirect_dma_start(out=k.out, out_offset=bass.IndirectOffsetOnAxis(id_t.bitcast(U32)[:, cc:cc + 1], 0),
                                                                                   in_=y_t, in_offset=None, compute_op=ALU.add),
                  reads=[r_y, r_id, k.out_res], writes=[r_outacc])
    S.barrier()
    A.release()
    A.release()

def _rope_tables():
    rows, gw = 64, 64
    row = np.repeat(np.arange(rows), gw).astype(np.float32)
    col = np.tile(np.arange(gw), rows).astype(np.float32)
    n_freq = 16
    inv_freq = (10000.0 ** (-np.arange(n_freq, dtype=np.float32) / n_freq)).astype(np.float32)
    ang_r = row[:, None] * inv_freq
    ang_c = col[:, None] * inv_freq
    ang = np.concatenate([ang_r, ang_r, ang_c, ang_c], axis=-1).astype(np.float32)
    cos = np.cos(ang).astype(np.float32)
    sin = np.sin(ang).astype(np.float32)
    sgn = np.concatenate([-np.ones(16), np.ones(16), -np.ones(16), np.ones(16)]).astype(np.float32)
    return cos, (sin * sgn).astype(np.float32)


def prep_shared(inp):
    f = np.float32
    sh = {}
    sh["ada_w"] = np.ascontiguousarray(inp["ada_w"][0])
    sh["ada_b_row"] = np.ascontiguousarray(inp["ada_b"][0][None, :])
    sh["ada_bT"] = np.ascontiguousarray(inp["ada_b"][0].reshape(48, 128).T)
    sh["g1T"] = np.ascontiguousarray(inp["norm1_g"][0].reshape(8, 128).T)
    sh["g2T"] = np.ascontiguousarray(inp["norm2_g"][0].reshape(8, 128).T)
    sh["g2_rep"] = np.ascontiguousarray(np.broadcast_to(inp["norm2_g"][0].reshape(1, 1024), (128, 1024)))
    sh["w_in"] = np.ascontiguousarray(inp["w_in"][0])
    sh["convT"] = np.ascontiguousarray(inp["conv_w"][0].T.reshape(24, 128, 5).transpose(1, 0, 2))
    sh["alog_rep"] = np.ascontiguousarray(np.broadcast_to(inp["a_log"][0].reshape(1, 16), (128, 16)))
    sh["dtb_rep"] = np.ascontiguousarray(np.broadcast_to(inp["dt_bias"][0].reshape(1, 16), (128, 16)))
    sh["dng_rep"] = np.ascontiguousarray(np.broadcast_to(inp["dn_norm_g"][0].reshape(1, 128), (128, 128)))
    sh["gqaT"] = np.ascontiguousarray(inp["q_a_norm_g"][0].reshape(3, 128).T)
    sh["w_uq"] = np.ascontiguousarray(inp["w_uq"][0])
    sh["gkvaT"] = np.ascontiguousarray(inp["kv_a_norm_g"][0].reshape(2, 128).T)
    sh["w_ukv"] = np.ascontiguousarray(inp["w_ukv"][0])
    sh["gq_rep"] = np.ascontiguousarray(np.broadcast_to(inp["q_norm_g"][0].reshape(1, 192), (128, 192)))
    sh["gk_rep"] = np.ascontiguousarray(np.broadcast_to(inp["k_norm_g"][0].reshape(1, 192), (128, 192)))
    sh["w_out_a"] = np.ascontiguousarray(inp["w_out_a"][0])
    sh["w_out_b"] = np.ascontiguousarray(inp["w_out_b"][0])
    sh["w_o"] = np.ascontiguousarray(inp["w_o"][0])
    sh["router_w"] = np.ascontiguousarray(inp["router_w"][0])
    sh["w_gate"] = np.ascontiguousarray(inp["w_gate"][0])
    sh["w_up"] = np.ascontiguousarray(inp["w_up"][0])
    sh["w_down"] = np.ascontiguousarray(inp["w_down"][0])
    cos, sinS = _rope_tables()
    sh["rope_cs"] = np.ascontiguousarray(np.concatenate([cos, sinS], axis=1))
    sh["ident"] = np.eye(128, dtype=f)
    consts = np.zeros((128, 1024), f)
    consts[:, 0:512] = np.arange(512, dtype=f)[None, :]
    consts[:, 512] = np.arange(128, dtype=f)
    sh["consts"] = consts
    pp_ = np.arange(128)
    sh["moe_blk"] = (pp_[:, None] // 8 == pp_[None, :] // 8).astype(f)
    s8 = np.zeros((128, 16), f)
    s8[np.arange(16) * 8, np.arange(16)] = 1.0
    sh["moe_sel8"] = s8
    tk = np.zeros((128, 32, 2), f)
    tk[:, :, 0] = np.arange(32, dtype=f)[None, :]
    tk[:, :, 1] = np.arange(128, dtype=f)[:, None]
    sh["moe_tok"] = tk
    sh["moe_tris"] = (pp_[:, None] < pp_[None, :]).astype(f)
    ii = np.arange(128)
    P, Fr = ii[:, None], ii[None, :]
    NEGV = -30000.0
    mk = np.zeros((128, 9, 128), f)
    mk[:, 0] = (P <= Fr)
    mk[:, 1] = (P >= Fr)
    mk[:, 2] = np.where(P > Fr, 0.0, NEGV)
    mk[:, 3] = np.where(P < Fr, 0.0, NEGV)
    mk[:, 4] = np.where(Fr >= P, 0.0, NEGV)
    mk[:, 5] = np.where(Fr <= P, 0.0, NEGV)
    mk[:, 6] = (P // 32 == Fr // 32)
    mk[:, 7] = (P // 64 == Fr // 64) & (P // 32 != Fr // 32)
    mk[:, 8] = (P // 64 != Fr // 64)
    sh["dn_masks"] = mk
    es = np.zeros((64, 2, 8, 128), f)
    for hh in range(8):
        es[hh, 0, hh, :] = 1.0
        es[32 + hh, 0, hh, :] = 1.0
    es[:, 1] = -es[:, 0]
    sh["dn_esel"] = es
    li = np.zeros((64, 128), f)
    li[32:40] = 1.0
    sh["dn_linit"] = li
    return sh


def prep_core(inp, sh, b):
    m = dict(sh)
    m["x"] = np.ascontiguousarray(inp["x"][b])
    m["ctx"] = np.ascontiguousarray(inp["ctx"][b])
    cc = np.stack([inp["c"][b], inp["c_ctx"]], axis=-1).astype(np.float32)
    m["c2"] = np.ascontiguousarray(cc.reshape(8, 128, 2).transpose(1, 0, 2))
    return m

PHASES = ["a0", "a1", "a2", "b", "c", "d", "e"]


def build(upto="e", dbg=(), dumps=()):
    k = K(dbg=dbg)
    declare_inputs(k)
    setup_consts(k)
    k.dump_list = []

    def dump(name, ap, res, shape):
        t = k.nc.dram_tensor("dbg_" + name, list(shape), ap.dtype, kind="ExternalOutput").ap()
        k.store(t, None, ap, res)
        k.dump_list.append("dbg_" + name)
    k.dump = dump
    k.dumps = set(dumps)
    fns = {"a0": phase_a0}
    for nm in ("a1", "a2", "b", "c", "d", "e"):
        f = globals().get("phase_" + nm)
        if f is not None:
            fns[nm] = f
    for ph in PHASES:
        if ph in fns:
            fns[ph](k)
        if ph == upto:
            break
    k.S.emit()
    return k


_CACHE = {}


def kernel(**inputs):
    inp = {kk: np.asarray(v) for kk, v in inputs.items()}
    sh = prep_shared(inp)
    in_maps = [prep_core(inp, sh, b) for b in range(8)]
    k = build()
    res = run_bass_kernel_spmd(k.nc, in_maps, core_ids=list(range(8)))
    out = np.stack([np.asarray(r["out"]) for r in res.results], axis=0).astype(np.float32)
    return out
```

```python
import numpy as np
import concourse.bass as bass
import concourse.mybir as mybir
from concourse.bass_utils import run_bass_kernel_spmd

F32 = mybir.dt.float32
BF16 = mybir.dt.bfloat16
F32R = mybir.dt.float32r
I32 = mybir.dt.int32
AF = mybir.ActivationFunctionType
ALU = mybir.AluOpType
AX = mybir.AxisListType

ENGS = ("pe", "act", "dve", "pool", "sp")
EPOCH = 12000


class Res:
    __slots__ = ("name", "w", "rs", "multi", "ws", "excl")

    def __init__(self, name="", multi=False, excl=False):
        self.excl = excl
        self.name = name
        self.w = None
        self.rs = []
        self.multi = multi
        self.ws = []


class Tok:
    __slots__ = ("key", "val", "eng")

    def __init__(self, key, val, eng):
        self.key = key
        self.val = val
        self.eng = eng


class DmaPool:
    def __init__(self, sched, name, n):
        self.s = sched
        self.name = name
        self.n = n
        self.i = 0
        self.count = [0] * n
        self.last = [None] * n

    def keys(self):
        return [("dma", self.name, j) for j in range(self.n)]


class Sched:
    def __init__(self, nc):
        self.nc = nc
        self.ops = {e: [] for e in ENGS}
        self.cnt = {e: 0 for e in ENGS}
        self.pools = []
        self.last_tok = {e: None for e in ENGS}
        self.n_instr = 0

    def pool(self, name, n):
        p = DmaPool(self, name, n)
        self.pools.append(p)
        return p

    def _deps(self, eng, reads, writes):
        deps = []
        for r in reads:
            if r.multi:
                deps.extend(r.ws)
            elif r.w is not None:
                deps.append(r.w)
            if r.excl:
                deps.extend(t for t in r.rs if t.eng != eng)
        for w in writes:
            if w.multi:
                pass
            elif w.w is not None and w.w.eng != eng:
                deps.append(w.w)
            for t in w.rs:
                if t.eng != eng:
                    deps.append(t)
        return deps

    def _mark_w(self, writes, tok):
        for w in writes:
            if w.multi:
                w.ws.append(tok)
            else:
                w.w = tok
                w.rs = []

    def op(self, eng, fn, reads=(), writes=(), extra=()):
        deps = self._deps(eng, reads, writes) + list(extra)
        c = self.cnt[eng]
        tok = Tok(("eng", eng, c // EPOCH), c % EPOCH + 1, eng)
        self.cnt[eng] = c + 1
        for r in reads:
            r.rs.append(tok)
        self._mark_w(writes, tok)
        self.ops[eng].append((deps, fn, tok, 1))
        self.last_tok[eng] = tok
        return tok

    def dma(self, eng, pool, fn, reads=(), writes=(), extra=()):
        deps = self._deps('__dma__', reads, writes) + list(extra)
        j = pool.i
        pool.i = (pool.i + 1) % pool.n
        if pool.last[j] is not None:
            deps.append(pool.last[j])
        pool.count[j] += 16
        tok = Tok(("dma", pool.name, j), pool.count[j], None)
        pool.last[j] = tok
        for r in reads:
            r.rs.append(tok)
        self._mark_w(writes, tok)
        self.ops[eng].append((deps, fn, tok, 16))
        return tok

    def barrier(self):
        toks = [t for t in self.last_tok.values() if t is not None]
        for p in self.pools:
            toks += [t for t in p.last if t is not None]
        for e in ENGS:
            self.ops[e].append((list(toks), None, None, 0))

    def emit(self, final_waits_eng="sp"):
        nc = self.nc
        sems = {}

        def sem_of(key):
            if key not in sems:
                sems[key] = nc.alloc_semaphore("s_" + "_".join(str(k) for k in key))
            return sems[key]

        for e in ENGS:
            for ep in range((self.cnt[e] + EPOCH - 1) // EPOCH):
                sem_of(("eng", e, ep))
        for p in self.pools:
            for k in p.keys():
                sem_of(k)

        toks = [t for t in self.last_tok.values() if t is not None]
        for p in self.pools:
            toks += [t for t in p.last if t is not None]
        self.ops[final_waits_eng].append((list(toks), None, None, 0))

        engobj = {"pe": "tensor", "act": "scalar", "dve": "vector", "pool": "gpsimd", "sp": "sync"}
        sched = self

        def run(ename):
            def body(eng):
                seen = {}
                for deps, fn, tok, inc in sched.ops[ename]:
                    need = {}
                    for t in deps:
                        if t.val > need.get(t.key, 0):
                            need[t.key] = t.val
                    for k, v in need.items():
                        if seen.get(k, 0) >= v:
                            continue
                        seen[k] = v
                        eng.wait_ge(sem_of(k), v)
                        sched.n_instr += 1
                    if fn is not None:
                        ins = fn(eng)
                        ins.then_inc(sem_of(tok.key), inc)
                        sched.n_instr += 1
            return body

        with nc.Block() as block:
            for ename in ENGS:
                getattr(block, engobj[ename])(run(ename))


class Arena:
    def __init__(self, nc, kbytes=198):
        self.nc = nc
        self.words = kbytes * 256
        self.t = nc.alloc_sbuf_tensor("arena", [128, self.words], F32)
        self.ap = self.t.ap()
        self.off = 0
        self.marks = []
        self.peak = 0

    def tile(self, shape, dtype, name=None):
        esz = {F32: 4, BF16: 2, I32: 4}[dtype]
        n = int(np.prod(shape[1:]))
        nw = (n * esz + 3) // 4
        off = (self.off + 15) // 16 * 16
        assert off + nw <= self.words, f"SBUF overflow {off}+{nw} > {self.words}"
        self.off = off + nw
        self.peak = max(self.peak, self.off)
        a = self.ap[0:shape[0], off:off + nw]
        if dtype != F32:
            a = a.bitcast(dtype)
        a = a[:, 0:n]
        if len(shape) == 3:
            a = a.rearrange("p (a b) -> p a b", a=shape[1])
        elif len(shape) == 4:
            a = a.rearrange("p (a b c) -> p a b c", a=shape[1], b=shape[2])
        return a

    def mark(self):
        self.marks.append(self.off)

    def release(self):
        self.off = self.marks.pop()

D = 1024
T = 4352
NT = 34
TX = 4096
NCTX = 256
H = 8
OFF_Z = 3072
OFF_GATE = 4832
D_IN = 6880
NMID = 1760
EPS = 1e-6
NEG = -30000.0


class K:
    def __init__(self, dbg=()):
        self.nc = bass.Bass("TRN2", target_bir_lowering=False)
        self.S = Sched(self.nc)
        self.A = Arena(self.nc)
        self.dbg = set(dbg)
        self.ins = {}
        self.scr = {}
        nc = self.nc
        self.ps = nc.alloc_psum_tensor("ps", [128, 8, 512], F32).ap()
        self.psr = [Res(f"ps{b}", excl=True) for b in range(8)]
        self.ld = self.S.pool("ld", 8)
        self.st = self.S.pool("st", 8)
        self.wl = self.S.pool("wl", 12)

    def inp(self, name, shape, dtype=F32):
        t = self.nc.dram_tensor(name, list(shape), dtype, kind="ExternalInput").ap()
        self.ins[name] = t
        return t

    def scratch(self, name, shape, dtype):
        kind = "ExternalOutput" if name in self.dbg else "Internal"
        t = self.nc.dram_tensor(name, list(shape), dtype, kind=kind).ap()
        self.scr[name] = (t, Res(name, multi=True))
        return t, self.scr[name][1]

    def bank(self, b, dtype=F32):
        a = self.ps[:, b, :]
        if dtype == BF16:
            a = a.bitcast(BF16)
        return a, self.psr[b]

    def tile(self, shape, dtype, name=None):
        return self.A.tile(shape, dtype, name), Res(name or "t")

    def load(self, dst, dres, src, sres=None, q="sp", pool=None):
        return self.S.dma(q, pool or self.ld, lambda e: e.dma_start(out=dst, in_=src),
                          reads=[sres] if sres is not None else [], writes=[dres])

    def store(self, dst, dres, src, sres, q="sp", pool=None):
        return self.S.dma(q, pool or self.st, lambda e: e.dma_start(out=dst, in_=src),
                          reads=[sres], writes=[dres] if dres is not None else [])

    def wload(self, dst, dres, src):
        return self.S.dma("pool", self.wl, lambda e: e.dma_start(out=dst, in_=src), writes=[dres])


def declare_inputs(k):
    i = k.inp
    i("x", [TX, D]); i("ctx", [NCTX, D]); i("c2", [128, 8, 2])
    i("ada_w", [D, 6 * D]); i("ada_b_row", [1, 6 * D]); i("ada_bT", [128, 48])
    i("g1T", [128, 8]); i("g2T", [128, 8]); i("g2_rep", [128, D])
    i("w_in", [D, D_IN]); i("convT", [128, 24, 5])
    i("alog_rep", [128, 16]); i("dtb_rep", [128, 16]); i("dng_rep", [128, 128])
    i("gqaT", [128, 3]); i("w_uq", [384, 1536]); i("gkvaT", [128, 2]); i("w_ukv", [256, 2048])
    i("gq_rep", [128, 192]); i("gk_rep", [128, 192])
    i("w_out_a", [D, D]); i("w_out_b", [D, D]); i("w_o", [D, D])
    i("router_w", [D, 16]); i("w_gate", [16, D, 1408]); i("w_up", [16, D, 1408]); i("w_down", [16, 1408, D])
    i("rope_cs", [TX, 128])
    i("ident", [128, 128]); i("consts", [128, 1024])
    i("moe_tok", [128, 32, 2]); i("moe_blk", [128, 128]); i("moe_sel8", [128, 16]); i("moe_tris", [128, 128])
    i("dn_masks", [128, 9, 128]); i("dn_esel", [64, 2, 8, 128]); i("dn_linit", [64, 128])
    k.out = k.nc.dram_tensor("out", [TX, D], F32, kind="ExternalOutput").ap()
    k.out_res = Res("out", multi=True)


def setup_consts(k):
    S = k.S
    k.ident_f, k.r_ident_f = k.tile([128, 128], F32, "identf")
    k.ident_b, k.r_ident_b = k.tile([128, 128], BF16, "identb")
    k.load(k.ident_f, k.r_ident_f, k.ins["ident"])
    S.op("dve", lambda e: e.tensor_copy(out=k.ident_b, in_=k.ident_f), reads=[k.r_ident_f], writes=[k.r_ident_b])
    k.ones_f, k.r_ones_f = k.tile([128, 128], F32, "onesf")
    k.ones_b, k.r_ones_b = k.tile([128, 128], BF16, "onesb")
    S.op("pool", lambda e: e.memset(k.ones_f, 1.0), writes=[k.r_ones_f])
    S.op("pool", lambda e: e.memset(k.ones_b, 1.0), writes=[k.r_ones_b])


def phase_a0(k):
    S, A, nc = k.S, k.A, k.nc
    ins = k.ins
    k.modT, k.r_modT = k.tile([128, 48, 2], F32, "modT")
    k.s1, k.r_s1 = k.tile([128, 8, 2], F32, "s1")
    k.s2, k.r_s2 = k.tile([128, 8], F32, "s2")
    k.gate1_row, k.r_gate1 = k.tile([128, D], F32, "gate1row")
    k.gate2_row, k.r_gate2 = k.tile([128, D], F32, "gate2row")
    k.off_after_gate2 = A.off
    k.shift2_row, k.r_shift2 = k.tile([128, D], F32, "shift2row")
    k.s2_row, k.r_s2row = k.tile([128, D], F32, "s2row")
    A.mark()
    c2, r_c2 = k.tile([128, 8, 2], F32, "c2")
    sc, r_sc = k.tile([128, 8, 2], F32, "sc")
    screp, r_screp = k.tile([128, 8, 128], F32, "screp")
    abT, r_abT = k.tile([128, 48], F32, "abT")
    abrow, r_abrow = k.tile([1, 6 * D], F32, "abrow")
    g1T, r_g1T = k.tile([128, 8], F32, "g1T")
    g2T, r_g2T = k.tile([128, 8], F32, "g2T")
    wbuf = [k.tile([128, 8, D], F32, f"adaw{j}") for j in range(2)]
    k.load(c2, r_c2, ins["c2"])
    k.load(abT, r_abT, ins["ada_bT"])
    k.load(abrow, r_abrow, ins["ada_b_row"])
    k.load(g1T, r_g1T, ins["g1T"])
    k.load(g2T, r_g2T, ins["g2T"])
    S.op("act", lambda e: e.activation(out=sc, in_=c2, func=AF.Silu), reads=[r_c2], writes=[r_sc])
    S.op("dve", lambda e: e.tensor_copy(out=screp, in_=sc[:, :, 0:1].to_broadcast([128, 8, 128])),
         reads=[r_sc], writes=[r_screp])
    pm, r_pm = k.bank(0)
    aw = ins["ada_w"].rearrange("(kc p) n -> p kc n", p=128)
    for sec in range(6):
        wt, r_wt = wbuf[sec % 2]
        q = "sp" if sec % 2 == 0 else "act"
        S.dma(q, k.ld, lambda e, wt=wt, sec=sec: e.dma_start(out=wt, in_=aw[:, :, sec * D:(sec + 1) * D]), writes=[r_wt])

        def mm(e, wt=wt, sec=sec):
            for fc in range(8):
                for kc in range(8):
                    ins_ = e.matmul(pm[:, (sec * 8 + fc) * 2:(sec * 8 + fc) * 2 + 2], lhsT=wt[:, kc, fc * 128:(fc + 1) * 128],
                                    rhs=sc[:, kc, :], start=(kc == 0), stop=(kc == 7))
            return ins_
        S.op("pe", mm, reads=[r_wt, r_sc], writes=[r_pm])
        if sec in (2, 3, 4, 5):
            dst, r_dst = {2: (k.gate1_row, k.r_gate1), 5: (k.gate2_row, k.r_gate2), 3: (k.shift2_row, k.r_shift2), 4: (k.s2_row, k.r_s2row)}[sec]
            for hf in range(2):
                pb, r_pb = k.bank(1 + hf)

                def mmr(e, wt=wt, sec=sec, hf=hf, pb=pb):
                    for kc in range(8):
                        e.matmul(pb, lhsT=screp[:, kc, :], rhs=wt[:, kc, hf * 512:(hf + 1) * 512], start=(kc == 0), stop=False)
                    return e.matmul(pb, lhsT=k.ones_f[0:1, :], rhs=abrow[0:1, sec * D + hf * 512: sec * D + (hf + 1) * 512],
                                    start=False, stop=True)
                S.op("pe", mmr, reads=[r_wt, r_screp, k.r_ones_f, r_abrow], writes=[r_pb])
                S.op("act", lambda e, dst=dst, hf=hf, pb=pb: e.activation(out=dst[:, hf * 512:(hf + 1) * 512], in_=pb, func=AF.Copy),
                     reads=[r_pb], writes=[r_dst])
    S.op("dve", lambda e: e.tensor_tensor(out=k.modT, in0=pm[:, 0:96].rearrange("p (a b) -> p a b", b=2),
                                          in1=abT.unsqueeze(2).to_broadcast([128, 48, 2]), op=ALU.add),
         reads=[r_pm, r_abT], writes=[k.r_modT])
    S.op("dve", lambda e: e.scalar_tensor_tensor(out=k.s1, in0=k.modT[:, 8:16, :], scalar=1.0,
                                                 in1=g1T.unsqueeze(2).to_broadcast([128, 8, 2]), op0=ALU.add, op1=ALU.mult),
         reads=[k.r_modT, r_g1T], writes=[k.r_s1])
    S.op("dve", lambda e: e.scalar_tensor_tensor(out=k.s2, in0=k.modT[:, 32:40, 0], scalar=1.0,
                                                 in1=g2T, op0=ALU.add, op1=ALU.mult),
         reads=[k.r_modT, r_g2T], writes=[k.r_s2])
    g2rep, r_g2rep = k.tile([128, D], F32, "g2rep")
    k.load(g2rep, r_g2rep, ins["g2_rep"])
    S.op("dve", lambda e: e.scalar_tensor_tensor(out=k.s2_row, in0=k.s2_row, scalar=1.0, in1=g2rep, op0=ALU.add, op1=ALU.mult),
         reads=[k.r_s2row, r_g2rep], writes=[k.r_s2row])
    S.barrier()
    A.release()

def _nb(k, dtype=F32):
    b = getattr(k, "_bank_i", 0)
    k._bank_i = (b + 1) % 8
    return k.bank(b, dtype)


def _rstd(k, ss, r_ss, n, out, r_out):
    S = k.S
    S.op("act", lambda e: e.activation(out=out, in_=ss, func=AF.Sqrt, scale=1.0 / n, bias=EPS), reads=[r_ss], writes=[r_out])
    S.op("dve", lambda e: e.reciprocal(out=out, in_=out), reads=[r_out], writes=[r_out])


def _rope(k, pe, r_pe, cos_t, sin_t, r_tab, t1, t2, r_t1, r_t2):
    S = k.S
    cb = cos_t.unsqueeze(1).to_broadcast([128, 8, 64])
    S.op("pool", lambda e: e.tensor_tensor(out=t1, in0=pe, in1=cb, op=ALU.mult), reads=[r_pe, r_tab], writes=[r_t1])
    pe5 = pe.rearrange("p h (a s c) -> p h a s c", a=2, s=2)
    t25 = t2.rearrange("p h (a s c) -> p h a s c", a=2, s=2)
    sn5 = sin_t.rearrange("p (a s c) -> p a s c", a=2, s=2)

    def f(e):
        for s in range(2):
            ins_ = e.tensor_tensor(out=t25[:, :, :, s, :], in0=pe5[:, :, :, 1 - s, :],
                                   in1=sn5[:, :, s, :].unsqueeze(1).to_broadcast([128, 8, 2, 16]), op=ALU.mult)
        return ins_
    S.op("dve", f, reads=[r_pe, r_tab], writes=[r_t2])
    S.op("pool", lambda e: e.tensor_tensor(out=pe, in0=t1, in1=t2, op=ALU.add), reads=[r_t1, r_t2], writes=[r_pe])


def phase_a1(k):
    S, A, nc, ins = k.S, k.A, k.nc, k.ins
    zs_s, r_zs_s = k.scratch("zs_s", [TX, D], BF16)
    gb_s, r_gb_s = k.scratch("gb_s", [T, 48], F32)
    qmT_s, r_qmT_s = k.scratch("qmT_s", [H, 192, TX], BF16)
    kmT_s, r_kmT_s = k.scratch("kmT_s", [H, 192, T], BF16)
    vm_s, r_vm_s = k.scratch("vm_s", [T, D], BF16)
    A.mark()
    k.hT, _ = k.tile([128, 8, T], BF16, "hT")
    k.r_hT = [Res(f"hT{i}") for i in range(NT)]
    A.mark()
    wmid, r_wmid = k.tile([128, 8, NMID], BF16, "wmid")
    wuq, r_wuq = k.tile([128, 3, 1536], BF16, "wuq")
    wukv, r_wukv = k.tile([128, 2, 2048], BF16, "wukv")
    dtb, r_dtb = k.tile([128, 16], F32, "dtb")
    negA, r_negA = k.tile([128, 16], F32, "negA")
    gq, r_gq = k.tile([128, 192], F32, "gq")
    gk, r_gk = k.tile([128, 192], F32, "gk")
    gqaT, r_gqaT = k.tile([128, 3], F32, "gqaT")
    gkvaT, r_gkvaT = k.tile([128, 2], F32, "gkvaT")
    win = ins["w_in"].rearrange("(kc p) n -> p kc n", p=128)
    for j in range(4):
        k.wload(wmid[:, 2 * j:2 * j + 2, :], r_wmid, win[:, 2 * j:2 * j + 2, OFF_Z:OFF_GATE])
    k.wload(wuq, r_wuq, ins["w_uq"].rearrange("(kc p) n -> p kc n", p=128))
    k.wload(wukv, r_wukv, ins["w_ukv"].rearrange("(kc p) n -> p kc n", p=128))
    k.load(dtb, r_dtb, ins["dtb_rep"])
    k.load(negA, r_negA, ins["alog_rep"])
    k.load(gq, r_gq, ins["gq_rep"])
    k.load(gk, r_gk, ins["gk_rep"])
    k.load(gqaT, r_gqaT, ins["gqaT"])
    k.load(gkvaT, r_gkvaT, ins["gkvaT"])
    S.op("act", lambda e: e.activation(out=negA, in_=negA, func=AF.Exp), reads=[r_negA], writes=[r_negA])
    S.op("dve", lambda e: e.tensor_scalar(out=negA, in0=negA, scalar1=-1.0, scalar2=None, op0=ALU.mult), reads=[r_negA], writes=[r_negA])
    S.op("dve", lambda e: e.tensor_scalar(out=gq, in0=gq, scalar1=192.0 ** -0.5, scalar2=None, op0=ALU.mult), reads=[r_gq], writes=[r_gq])

    NB = 2
    xt = [k.tile([128, D], F32, "xt") for _ in range(NB)]
    junk, r_junk = k.tile([128, D], BF16, "junk")
    ss = [k.tile([128, 8], F32, "ss") for _ in range(NB)]
    xn = [k.tile([128, D], BF16, "xn") for _ in range(NB)]
    zs = [k.tile([128, D], BF16, "zs") for _ in range(NB)]
    gb = [k.tile([128, 48], F32, "gb") for _ in range(NB)]
    t16, r_t16 = k.tile([128, 16], F32, "t16")
    cqn, r_cqn = k.tile([128, 384], BF16, "cqn")
    ckvn, r_ckvn = k.tile([128, 256], BF16, "ckvn")
    cqnT, r_cqnT = k.tile([128, 3, 128], BF16, "cqnT")
    ckvnT, r_ckvnT = k.tile([128, 2, 128], BF16, "ckvnT")
    kr, r_kr = k.tile([128, 64], F32, "kr")
    qsb, r_qsb = k.tile([128, 8, 192], F32, "qsb")
    sq, r_sq = k.tile([128, 8, 192], F32, "sq")
    r8, r_r8 = k.tile([128, 8], F32, "r8")
    kvsb, r_kvsb = k.tile([128, 8, 2, 128], F32, "kvsb")
    tmpf, r_tmpf = kvsb.rearrange("p a b c -> p (a b c)")[:, 0:1024].rearrange("p (a b) -> p a b", a=8), r_kvsb
    kf, r_kf = sq, r_sq
    rk8, r_rk8 = k.tile([128, 8], F32, "rk8")
    sskr, r_sskr = k.tile([128, 1], F32, "sskr")
    rt1, r_rt1 = k.tile([128, 8, 64], F32, "rt1")
    rt2, r_rt2 = k.tile([128, 8, 64], F32, "rt2")
    cs_t = [k.tile([128, 128], F32, "cs") for _ in range(NB)]
    qf, r_qf = k.tile([128, 8, 192], BF16, "qf")
    kfb, r_kfb = k.tile([128, 8, 192], BF16, "kfb")
    vb = [k.tile([128, 8, 128], BF16, "vb") for _ in range(1)]
    qTn = [k.tile([128, 8, 128], BF16, "qTn") for _ in range(1)]
    qTr = [k.tile([64, 8, 128], BF16, "qTr") for _ in range(1)]
    kTn = [k.tile([128, 8, 128], BF16, "kTn") for _ in range(1)]
    kTr = [k.tile([64, 8, 128], BF16, "kTr") for _ in range(1)]

    def src_rows(i):
        return ins["ctx"][i * 128:(i + 1) * 128, :] if i < 2 else ins["x"][(i - 2) * 128:(i - 1) * 128, :]

    def prefetch(i):
        b = i % NB
        k.load(xt[b][0], xt[b][1], src_rows(i))
        if i >= 2:
            xi = i - 2
            S.dma("act", k.ld, lambda e: e.dma_start(out=cs_t[b][0], in_=ins["rope_cs"][xi * 128:(xi + 1) * 128, :]), writes=[cs_t[b][1]])

    def transposes(src, r_src, n, width=128, rows=128):
        pb, r_pb = _nb(k, BF16)
        pv = pb.rearrange("p (a b) -> p a b", b=128)[0:width, 0:n, :]

        def f(e):
            for j in range(n):
                ins_ = e.transpose(out=pv[:, j, :], in_=src(j), identity=k.ident_b)
            return ins_
        S.op("pe", f, reads=[r_src, k.r_ident_b], writes=[r_pb])
        return pv, r_pb

    prefetch(0)
    for i in range(NT):
        b = i % NB
        is_x = i >= 2
        xi = i - 2
        col = 0 if is_x else 1
        tok = slice(i * 128, (i + 1) * 128)
        if i + 1 < NT:
            prefetch(i + 1)
        x_t, r_x = xt[b]
        ss_t, r_ss = ss[b]
        xn_t, r_xn = xn[b]
        S.op("act", lambda e, x_t=x_t, ss_t=ss_t: e.activation(out=junk, in_=x_t, func=AF.Square, accum_out=ss_t[:, 0:1]),
             reads=[r_x], writes=[r_junk, r_ss])
        _rstd(k, ss_t[:, 0:1], r_ss, D, ss_t[:, 1:2], r_ss)
        S.op("act", lambda e, x_t=x_t, ss_t=ss_t, xn_t=xn_t: e.activation(out=xn_t, in_=x_t, func=AF.Copy, scale=ss_t[:, 1:2]),
             reads=[r_x, r_ss], writes=[r_xn])
        pv, r_pv = transposes(lambda j, xn_t=xn_t: xn_t[:, j * 128:(j + 1) * 128], r_xn, 8)
        S.op("dve", lambda e, pv=pv, col=col: e.tensor_tensor(out=tmpf, in0=pv, in1=k.s1[:, :, col:col + 1].to_broadcast([128, 8, 128]), op=ALU.mult),
             reads=[r_pv, k.r_s1], writes=[r_tmpf])
        S.op("pool", lambda e, col=col, tok=tok: e.tensor_tensor(out=k.hT[:, :, tok], in0=tmpf,
                                                                in1=k.modT[:, 0:8, col:col + 1].to_broadcast([128, 8, 128]), op=ALU.add),
             reads=[r_tmpf, k.r_modT], writes=[k.r_hT[i]])
        groups = [(0, 512), (512, 1024), (1024, 1440), (1440, 1760)]
        banks = []
        for g, (c0, c1) in enumerate(groups):
            if g < 2 and not is_x:
                banks.append(None)
                continue
            pb, r_pb = _nb(k)

            def mm(e, pb=pb, c0=c0, c1=c1, tok=tok):
                for kc in range(8):
                    ins_ = e.matmul(pb[:, 0:c1 - c0], lhsT=k.hT[:, kc, tok], rhs=wmid[:, kc, c0:c1], start=(kc == 0), stop=(kc == 7))
                return ins_
            S.op("pe", mm, reads=[k.r_hT[i], r_wmid], writes=[r_pb])
            banks.append((pb, r_pb))
        if is_x:
            z_t, r_z = zs[b]
            for g in range(2):
                pb, r_pb = banks[g]
                S.op("act", lambda e, pb=pb, g=g, z_t=z_t: e.activation(out=z_t[:, g * 512:(g + 1) * 512], in_=pb, func=AF.Silu),
                     reads=[r_pb], writes=[r_z])
            k.store(zs_s[xi * 128:(xi + 1) * 128, :], r_zs_s, z_t, r_z)
        p2, r_p2 = banks[2]
        p3, r_p3 = banks[3]
        gb_t, r_gb = gb[b]
        S.op("dve", lambda e, p2=p2: e.tensor_tensor(out=t16, in0=p2[:, 0:16], in1=dtb, op=ALU.add), reads=[r_p2, r_dtb], writes=[r_t16])
        S.op("act", lambda e: e.activation(out=t16, in_=t16, func=AF.Exp), reads=[r_t16], writes=[r_t16])
        S.op("act", lambda e: e.activation(out=t16, in_=t16, func=AF.Ln, bias=1.0), reads=[r_t16], writes=[r_t16])
        S.op("dve", lambda e, gb_t=gb_t: e.tensor_tensor(out=gb_t[:, 0:16], in0=t16, in1=negA, op=ALU.mult), reads=[r_t16, r_negA], writes=[r_gb])
        S.op("act", lambda e, gb_t=gb_t, p2=p2: e.activation(out=gb_t[:, 16:32], in_=p2[:, 16:32], func=AF.Sigmoid), reads=[r_p2], writes=[r_gb])
        S.op("act", lambda e, gb_t=gb_t: e.activation(out=gb_t[:, 32:48], in_=gb_t[:, 16:32], func=AF.Ln), reads=[r_gb], writes=[r_gb])
        k.store(gb_s[tok, :], r_gb_s, gb_t, r_gb)
        if is_x:
            S.op("act", lambda e, p2=p2, ss_t=ss_t: e.activation(out=junk[:, 0:384], in_=p2[:, 32:416], func=AF.Square, accum_out=ss_t[:, 4:5]),
                 reads=[r_p2], writes=[r_junk, r_ss])
            _rstd(k, ss_t[:, 4:5], r_ss, 384, ss_t[:, 5:6], r_ss)
            S.op("act", lambda e, p2=p2, ss_t=ss_t: e.activation(out=cqn, in_=p2[:, 32:416], func=AF.Copy, scale=ss_t[:, 5:6]),
                 reads=[r_p2, r_ss], writes=[r_cqn])

        S.op("act", lambda e, p3=p3, ss_t=ss_t: e.activation(out=junk[:, 0:256], in_=p3[:, 0:256], func=AF.Square, accum_out=ss_t[:, 2:3]),
             reads=[r_p3], writes=[r_junk, r_ss])
        _rstd(k, ss_t[:, 2:3], r_ss, 256, ss_t[:, 3:4], r_ss)
        S.op("act", lambda e, p3=p3, ss_t=ss_t: e.activation(out=ckvn, in_=p3[:, 0:256], func=AF.Copy, scale=ss_t[:, 3:4]),
             reads=[r_p3, r_ss], writes=[r_ckvn])
        S.op("dve", lambda e, p3=p3: e.tensor_copy(out=kr, in_=p3[:, 256:320]), reads=[r_p3], writes=[r_kr])
        pv, r_pv = transposes(lambda j: ckvn[:, j * 128:(j + 1) * 128], r_ckvn, 2)
        S.op("dve", lambda e, pv=pv: e.tensor_tensor(out=ckvnT, in0=pv, in1=gkvaT.unsqueeze(2).to_broadcast([128, 2, 128]), op=ALU.mult),
             reads=[r_pv, r_gkvaT], writes=[r_ckvnT])
        for b4 in range(4):
            pb, r_pb = _nb(k)

            def mmkv(e, pb=pb, b4=b4):
                for kc in range(2):
                    ins_ = e.matmul(pb, lhsT=ckvnT[:, kc, :], rhs=wukv[:, kc, b4 * 512:(b4 + 1) * 512], start=(kc == 0), stop=(kc == 1))
                return ins_
            S.op("pe", mmkv, reads=[r_ckvnT, r_wukv], writes=[r_pb])
            eng = "act" if b4 % 2 == 0 else "dve"
            dst = kvsb[:, 2 * b4:2 * b4 + 2, :, :].rearrange("p a b c -> p (a b c)")
            if eng == "act":
                S.op("act", lambda e, dst=dst, pb=pb: e.activation(out=dst, in_=pb, func=AF.Copy), reads=[r_pb], writes=[r_kvsb])
            else:
                S.op("dve", lambda e, dst=dst, pb=pb: e.tensor_copy(out=dst, in_=pb), reads=[r_pb], writes=[r_kvsb])
        vb_t, r_vb = vb[0]
        S.op("pool", lambda e, vb_t=vb_t: e.tensor_copy(out=vb_t, in_=kvsb[:, :, 1, :]), reads=[r_kvsb], writes=[r_vb])
        k.store(vm_s[tok, :], r_vm_s, vb_t.rearrange("p h d -> p (h d)"), r_vb)
        S.op("pool", lambda e: e.tensor_tensor(out=sq[:, :, 0:128], in0=kvsb[:, :, 0, :], in1=kvsb[:, :, 0, :], op=ALU.mult),
             reads=[r_kvsb], writes=[r_sq])
        S.op("dve", lambda e: e.tensor_reduce(out=rk8, in_=sq[:, :, 0:128], axis=AX.X, op=ALU.add), reads=[r_sq], writes=[r_rk8])
        S.op("act", lambda e: e.activation(out=junk[:, 0:64], in_=kr, func=AF.Square, accum_out=sskr), reads=[r_kr], writes=[r_junk, r_sskr])
        S.op("dve", lambda e: e.tensor_scalar(out=rk8, in0=rk8, scalar1=sskr, scalar2=None, op0=ALU.add), reads=[r_rk8, r_sskr], writes=[r_rk8])
        _rstd(k, rk8, r_rk8, 192, rk8, r_rk8)
        S.op("dve", lambda e: e.tensor_tensor(out=kf[:, :, 0:128], in0=kvsb[:, :, 0, :], in1=rk8.unsqueeze(2).to_broadcast([128, 8, 128]), op=ALU.mult),
             reads=[r_kvsb, r_rk8], writes=[r_kf])
        S.op("dve", lambda e: e.tensor_tensor(out=kf[:, :, 128:192], in0=kr.unsqueeze(1).to_broadcast([128, 8, 64]),
                                              in1=rk8.unsqueeze(2).to_broadcast([128, 8, 64]), op=ALU.mult),
             reads=[r_kr, r_rk8, r_kf], writes=[r_kf])
        S.op("pool", lambda e: e.tensor_tensor(out=kf, in0=kf, in1=gk.unsqueeze(1).to_broadcast([128, 8, 192]), op=ALU.mult),
             reads=[r_kf, r_gk], writes=[r_kf])
        if is_x:
            _rope(k, kf[:, :, 128:192], r_kf, cs_t[b][0][:, 0:64], cs_t[b][0][:, 64:128], cs_t[b][1], rt1, rt2, r_rt1, r_rt2)
        S.op("act", lambda e: e.activation(out=kfb, in_=kf, func=AF.Copy), reads=[r_kf], writes=[r_kfb])
        kTn_t, r_kTn = kTn[0]
        kTr_t, r_kTr = kTr[0]
        pv, r_pv = transposes(lambda j: kfb[:, j, 0:128], r_kfb, 8)
        S.op("dve", lambda e, pv=pv, kTn_t=kTn_t: e.tensor_copy(out=kTn_t, in_=pv), reads=[r_pv], writes=[r_kTn])
        pv, r_pv = transposes(lambda j: kfb[:, j, 128:192], r_kfb, 8, width=64)
        S.op("act", lambda e, pv=pv, kTr_t=kTr_t: e.activation(out=kTr_t, in_=pv, func=AF.Copy), reads=[r_pv], writes=[r_kTr])
        k.store(kmT_s[:, 0:128, tok].rearrange("h d t -> d h t"), r_kmT_s, kTn_t, r_kTn)
        k.store(kmT_s[:, 128:192, tok].rearrange("h d t -> d h t"), r_kmT_s, kTr_t, r_kTr)
        if not is_x:
            continue
        xtok = slice(xi * 128, (xi + 1) * 128)
        pv, r_pv = transposes(lambda j: cqn[:, j * 128:(j + 1) * 128], r_cqn, 3)
        S.op("dve", lambda e, pv=pv: e.tensor_tensor(out=cqnT, in0=pv, in1=gqaT.unsqueeze(2).to_broadcast([128, 3, 128]), op=ALU.mult),
             reads=[r_pv, r_gqaT], writes=[r_cqnT])
        qflat = qsb.rearrange("p h d -> p (h d)")
        for b3 in range(3):
            pb, r_pb = _nb(k)

            def mmq(e, pb=pb, b3=b3):
                for kc in range(3):
                    ins_ = e.matmul(pb, lhsT=cqnT[:, kc, :], rhs=wuq[:, kc, b3 * 512:(b3 + 1) * 512], start=(kc == 0), stop=(kc == 2))
                return ins_
            S.op("pe", mmq, reads=[r_cqnT, r_wuq], writes=[r_pb])
            if b3 % 2 == 0:
                S.op("act", lambda e, pb=pb, b3=b3: e.activation(out=qflat[:, b3 * 512:(b3 + 1) * 512], in_=pb, func=AF.Copy), reads=[r_pb], writes=[r_qsb])
            else:
                S.op("dve", lambda e, pb=pb, b3=b3: e.tensor_copy(out=qflat[:, b3 * 512:(b3 + 1) * 512], in_=pb), reads=[r_pb], writes=[r_qsb])
        S.op("pool", lambda e: e.tensor_tensor(out=sq, in0=qsb, in1=qsb, op=ALU.mult), reads=[r_qsb], writes=[r_sq])
        S.op("dve", lambda e: e.tensor_reduce(out=r8, in_=sq, axis=AX.X, op=ALU.add), reads=[r_sq], writes=[r_r8])
        _rstd(k, r8, r_r8, 192, r8, r_r8)
        S.op("dve", lambda e: e.tensor_tensor(out=qsb, in0=qsb, in1=r8.unsqueeze(2).to_broadcast([128, 8, 192]), op=ALU.mult),
             reads=[r_qsb, r_r8], writes=[r_qsb])
        S.op("pool", lambda e: e.tensor_tensor(out=qsb, in0=qsb, in1=gq.unsqueeze(1).to_broadcast([128, 8, 192]), op=ALU.mult),
             reads=[r_qsb, r_gq], writes=[r_qsb])
        _rope(k, qsb[:, :, 128:192], r_qsb, cs_t[b][0][:, 0:64], cs_t[b][0][:, 64:128], cs_t[b][1], rt1, rt2, r_rt1, r_rt2)
        S.op("act", lambda e: e.activation(out=qf, in_=qsb, func=AF.Copy), reads=[r_qsb], writes=[r_qf])
        qTn_t, r_qTn = qTn[0]
        qTr_t, r_qTr = qTr[0]
        pv, r_pv = transposes(lambda j: qf[:, j, 0:128], r_qf, 8)
        S.op("dve", lambda e, pv=pv, qTn_t=qTn_t: e.tensor_copy(out=qTn_t, in_=pv), reads=[r_pv], writes=[r_qTn])
        pv, r_pv = transposes(lambda j: qf[:, j, 128:192], r_qf, 8, width=64)
        S.op("act", lambda e, pv=pv, qTr_t=qTr_t: e.activation(out=qTr_t, in_=pv, func=AF.Copy), reads=[r_pv], writes=[r_qTr])
        k.store(qmT_s[:, 0:128, xtok].rearrange("h d t -> d h t"), r_qmT_s, qTn_t, r_qTn)
        k.store(qmT_s[:, 128:192, xtok].rearrange("h d t -> d h t"), r_qmT_s, qTr_t, r_qTr)
    S.barrier()
    A.release()

RW = 4364
NU = 4356


def phase_a2(k):
    S, A, nc, ins = k.S, k.A, k.nc, k.ins
    qdT_s, r_qdT_s = k.scratch("qdT_s", [H, 128, TX], BF16)
    kdT_s, r_kdT_s = k.scratch("kdT_s", [H, 128, T], BF16)
    kd_s, r_kd_s = k.scratch("kd_s", [T, H, 128], BF16)
    vd_s, r_vd_s = k.scratch("vd_s", [T, H, 128], BF16)
    sgT_s, r_sgT_s = k.scratch("sgT_s", [16, 128, TX], BF16)
    A.mark()
    convw, r_convw = k.tile([128, 24, 5], F32, "convw")
    k.load(convw, r_convw, ins["convT"])
    wc = [k.tile([128, 8, 512], BF16, "wc") for _ in range(2)]
    R = [k.tile([128, RW], F32, "R") for _ in range(2)]
    acc, r_acc = k.tile([128, NU], F32, "acc")
    sq, r_sq = k.tile([128, NU], BF16, "sq")
    Yb = [k.tile([128, NU], BF16, "Yb") for _ in range(2)]
    rn, r_rn = k.tile([128, 512], F32, "rn")
    tm = [k.tile([128, NT, 128], BF16, "tm") for _ in range(1)]
    sg = [k.tile([128, 512], BF16, "sg") for _ in range(2)]
    for j in range(2):
        S.op("pool", lambda e, j=j: e.memset(R[j][0], 0.0), writes=[R[j][1]])
    win = ins["w_in"].rearrange("(kc p) n -> p kc n", p=128)
    blocks = [(c * 512, "qkv", c * 4) for c in range(6)] + [(OFF_GATE + c * 512, "gate", c * 4) for c in range(4)]
    tgroups = [(0, 256, 2)] + [(256 + g * 512, 512, 262 + g * 512) for g in range(8)]
    all_hT = list(k.r_hT)

    def load_block(bi):
        c0, kind, _ = blocks[bi]
        w_t, r_w = wc[bi % 2]
        for hf in range(2):
            k.wload(w_t[:, 4 * hf:4 * hf + 4, :], r_w, win[:, 4 * hf:4 * hf + 4, c0:c0 + 512])

    load_block(0)
    ci = 0
    for bi, (c0, kind, chunk0) in enumerate(blocks):
        if bi + 1 < len(blocks):
            load_block(bi + 1)
        w_t, r_w = wc[bi % 2]
        for sub in range(4):
            cc = chunk0 + sub
            if kind == "gate":
                for g in range(8):
                    pb, r_pb = _nb(k)

                    def mm(e, pb=pb, g=g, sub=sub, w_t=w_t):
                        for kc in range(8):
                            ins_ = e.matmul(pb, lhsT=w_t[:, kc, sub * 128:(sub + 1) * 128], rhs=k.hT[:, kc, 256 + g * 512:256 + (g + 1) * 512],
                                            start=(kc == 0), stop=(kc == 7))
                        return ins_
                    S.op("pe", mm, reads=[r_w] + all_hT, writes=[r_pb])
                    s_t, r_s = sg[g % 2]
                    S.op("act", lambda e, pb=pb, s_t=s_t: e.activation(out=s_t, in_=pb, func=AF.Sigmoid), reads=[r_pb], writes=[r_s])
                    k.store(sgT_s[cc, :, g * 512:(g + 1) * 512], r_sgT_s, s_t, r_s)
                continue
            R_t, r_R = R[ci % 2]
            Y_t, r_Y = Yb[ci % 2]
            ci += 1
            for gi, (h0, n, ro) in enumerate(tgroups):
                pb, r_pb = _nb(k)

                def mm(e, pb=pb, h0=h0, n=n, sub=sub, w_t=w_t):
                    for kc in range(8):
                        ins_ = e.matmul(pb[:, 0:n], lhsT=w_t[:, kc, sub * 128:(sub + 1) * 128], rhs=k.hT[:, kc, h0:h0 + n],
                                        start=(kc == 0), stop=(kc == 7))
                    return ins_
                S.op("pe", mm, reads=[r_w] + all_hT, writes=[r_pb])
                if gi % 2 == 0:
                    S.op("act", lambda e, pb=pb, n=n, ro=ro, R_t=R_t: e.activation(out=R_t[:, ro:ro + n], in_=pb[:, 0:n], func=AF.Copy),
                         reads=[r_pb], writes=[r_R])
                else:
                    S.op("dve", lambda e, pb=pb, n=n, ro=ro, R_t=R_t: e.tensor_copy(out=R_t[:, ro:ro + n], in_=pb[:, 0:n]),
                         reads=[r_pb], writes=[r_R])
            ceng = "dve"

            def conv(e, R_t=R_t, cc=cc):
                e.tensor_scalar(out=acc, in0=R_t[:, 0:NU], scalar1=convw[:, cc, 0:1], scalar2=None, op0=ALU.mult)
                for j in range(1, 5):
                    ins_ = e.scalar_tensor_tensor(out=acc, in0=R_t[:, j:j + NU], scalar=convw[:, cc, j:j + 1], in1=acc,
                                                  op0=ALU.mult, op1=ALU.add)
                return ins_
            S.op(ceng, conv, reads=[r_R, r_convw], writes=[r_acc])
            head = cc % 8
            if cc >= 16:
                S.op("act", lambda e, Y_t=Y_t: e.activation(out=Y_t, in_=acc, func=AF.Silu), reads=[r_acc], writes=[r_Y])
            else:
                S.op("act", lambda e: e.activation(out=acc, in_=acc, func=AF.Silu), reads=[r_acc], writes=[r_acc])
                S.op("pool" if ceng == "dve" else "dve", lambda e: e.tensor_tensor(out=sq, in0=acc, in1=acc, op=ALU.mult), reads=[r_acc], writes=[r_sq])
                scale = (128.0 ** -0.5) if cc < 8 else 1.0
                for g in range(9):
                    u0 = g * 512
                    n = min(512, NU - u0)
                    pb, r_pb = _nb(k)
                    S.op("pe", lambda e, pb=pb, u0=u0, n=n: e.matmul(pb[:, 0:n], lhsT=k.ones_b, rhs=sq[:, u0:u0 + n], start=True, stop=True),
                         reads=[r_sq, k.r_ones_b], writes=[r_pb])
                    S.op("act", lambda e, pb=pb, n=n, scale=scale: e.activation(out=rn[:, 0:n], in_=pb[:, 0:n], func=AF.Sqrt,
                                                                              scale=1.0 / (scale * scale), bias=EPS / (scale * scale)),
                         reads=[r_pb], writes=[r_rn])
                    S.op("dve", lambda e, n=n: e.reciprocal(out=rn[:, 0:n], in_=rn[:, 0:n]), reads=[r_rn], writes=[r_rn])
                    S.op("dve", lambda e, u0=u0, n=n, Y_t=Y_t: e.tensor_tensor(out=Y_t[:, u0:u0 + n], in0=acc[:, u0:u0 + n], in1=rn[:, 0:n], op=ALU.mult),
                         reads=[r_acc, r_rn], writes=[r_Y])
            if cc < 8:
                k.store(qdT_s[head, :, :], r_qdT_s, Y_t[:, 260:260 + TX], r_Y)
            elif cc < 16:
                k.store(kdT_s[head, :, 0:256], r_kdT_s, Y_t[:, 0:256], r_Y)
                k.store(kdT_s[head, :, 256:T], r_kdT_s, Y_t[:, 260:260 + TX], r_Y)
            if cc >= 8:
                tm_t, r_tm = tm[0]
                for tb in range(5):
                    t0 = tb * 8
                    nt = min(8, NT - t0)
                    pb, r_pb = _nb(k, BF16)
                    pv = pb.rearrange("p (a b) -> p a b", b=128)[:, 0:nt, :]

                    def tr(e, pv=pv, t0=t0, nt=nt, Y_t=Y_t):
                        for j in range(nt):
                            ti = t0 + j
                            u = ti * 128 if ti < 2 else 260 + (ti - 2) * 128
                            ins_ = e.transpose(out=pv[:, j, :], in_=Y_t[:, u:u + 128], identity=k.ident_b)
                        return ins_
                    S.op("pe", tr, reads=[r_Y, k.r_ident_b], writes=[r_pb])
                    if tb % 2 == 0:
                        S.op("act", lambda e, pv=pv, t0=t0, nt=nt, tm_t=tm_t: e.activation(out=tm_t[:, t0:t0 + nt, :], in_=pv, func=AF.Copy),
                             reads=[r_pb], writes=[r_tm])
                    else:
                        S.op("dve", lambda e, pv=pv, t0=t0, nt=nt, tm_t=tm_t: e.tensor_copy(out=tm_t[:, t0:t0 + nt, :], in_=pv),
                             reads=[r_pb], writes=[r_tm])
                dst_s, r_dst = (kd_s, r_kd_s) if cc < 16 else (vd_s, r_vd_s)
                k.store(dst_s.rearrange("(n p) h d -> p n h d", p=128)[:, :, head, :], r_dst, tm_t, r_tm)
    S.barrier()
    A.release()
    A.release()

import os as _os


def _drive(*gens):
    gens = [g for g in gens if g is not None]
    while gens:
        for g in list(gens):
            try:
                next(g)
            except StopIteration:
                gens.remove(g)


def phase_b(k):
    S, A, nc, ins = k.S, k.A, k.nc, k.ins
    qdT_s, r_qdT_s = k.scr["qdT_s"]
    kdT_s, r_kdT_s = k.scr["kdT_s"]
    kd_s, r_kd_s = k.scr["kd_s"]
    vd_s, r_vd_s = k.scr["vd_s"]
    gb_s, r_gb_s = k.scr["gb_s"]
    o_s = [k.scratch("of_s", [TX, D], F32), k.scratch("ob_s", [TX, D], F32)]
    A.mark()
    msk, r_msk = k.tile([128, 9, 128], F32, "msk")
    k.load(msk, r_msk, ins["dn_masks"])
    EC, r_EC = k.tile([64, 2, 8, 128], F32, "EC")
    k.load(EC, r_EC, ins["dn_esel"])
    L1, r_L1 = k.tile([64, 128], F32, "L1")
    L2, r_L2 = k.tile([64, 128], F32, "L2")
    R1, r_R1 = k.tile([64, 8, 128], F32, "R1")
    R2, r_R2 = k.tile([64, 8, 128], F32, "R2")
    X, r_X = k.tile([128, 2, 64], F32, "X")
    k.load(L1, r_L1, ins["dn_linit"])
    k.load(L2, r_L2, ins["dn_linit"])
    S.op("dve", lambda e: e.tensor_copy(out=R1, in_=EC[:, 0, :, :]), reads=[r_EC], writes=[r_R1])
    S.op("dve", lambda e: e.tensor_copy(out=R2, in_=EC[:, 1, :, :]), reads=[r_EC], writes=[r_R2])
    S.op("pool", lambda e: e.memset(X, 0.0), writes=[r_X])
    S32 = [k.tile([128, 8, 128], F32, f"S32_{d}") for d in range(2)]
    Sbf = [k.tile([128, 8, 128], BF16, f"Sbf_{d}") for d in range(2)]
    for d in range(2):
        S.op("pool", lambda e, d=d: e.memset(S32[d][0], 0.0), writes=[S32[d][1]])
        S.op("pool", lambda e, d=d: e.memset(Sbf[d][0], 0.0), writes=[Sbf[d][1]])
    NB = 2
    gbt = [k.tile([128, 48], F32, "gbt") for _ in range(NB)]
    kT = [k.tile([128, 8, 128], BF16, "kT") for _ in range(NB)]
    qT = [k.tile([128, 8, 128], BF16, "qT") for _ in range(NB)]
    ktm = [k.tile([128, 8, 128], BF16, "ktm") for _ in range(NB)]
    vtm = [k.tile([128, 8, 128], BF16, "vtm") for _ in range(NB)]
    sm = [k.tile([128, 4, 8], F32, "sm") for _ in range(NB)]
    E1, r_E1 = k.tile([128, 8, 128], F32, "E1")
    M, r_M = k.tile([128, 8, 128], F32, "M")
    Mt, r_Mt = k.tile([128, 8, 128], BF16, "Mt")
    Md, r_Md = k.tile([128, 8, 128], BF16, "Md")
    Mo1, r_Mo1 = k.tile([128, 8, 128], BF16, "Mo1")
    Mo2, r_Mo2 = k.tile([128, 8, 128], BF16, "Mo2")
    PP = [k.tile([128, 8, 128], BF16, f"PP{j}") for j in range(2)]
    PT = [k.tile([128, 8, 128], BF16, f"PT{j}") for j in range(2)]
    Tt, r_Tt = k.tile([128, 8, 128], BF16, "Tt")
    TtB, r_TtB = k.tile([128, 8, 128], BF16, "TtB")
    Abuf, _ = k.tile([128, 8, 128], BF16, "Abuf")
    E1r, Mdr, Mo1r, Mo2r, Mtr, Ttr = (t_ for t_ in (Abuf, Md, Mo1, Mo2, Mt, Tt))
    PPr = [PP[j][0] for j in range(2)]
    PTr = [PT[j][0] for j in range(2)]
    kg, r_kg = k.tile([128, 8, 128], BF16, "kg")
    kdec = [k.tile([128, 8, 128], BF16, "kdec") for _ in range(NB)]
    wT = [k.tile([128, 8, 128], BF16, "wT") for _ in range(NB)]
    u = [k.tile([128, 8, 128], F32, "u") for _ in range(NB)]
    qkT = [k.tile([128, 8, 128], BF16, "qkT") for _ in range(NB)]
    vnew, r_vnew = k.tile([128, 8, 128], BF16, "vnew")
    tmpo, r_tmpo = k.tile([128, 8, 128], F32, "tmpo")
    o_t = [k.tile([128, 8, 128], F32, "o") for _ in range(NB)]

    RG = {nm: [Res(nm + "0"), Res(nm + "1")] for nm in ("Ab", "E1", "M", "Md", "Mo1", "Mo2", "Mt", "Tt", "TtB", "PP0", "PP1", "PT0", "PT1")}
    wT_res = [[Res("wT"), Res("wT")] for _ in range(NB)]
    u_res = [[Res("u"), Res("u")] for _ in range(NB)]
    qk_res = [[Res("qk"), Res("qk")] for _ in range(NB)]
    pre_i = [0]

    def nbp(dtype=F32):
        b = pre_i[0]
        pre_i[0] = (b + 1) % 4
        return k.bank(b, dtype)

    def b4(pb):
        return pb.rearrange("p (a b) -> p a b", b=128)

    def b4h(pb):
        return pb.rearrange("p (a b) -> p a b", b=128)[:, 0:4, :]

    units = []
    fwd = list(range(NT))
    bwd = [1, 0] + list(range(NT - 1, 1, -1))
    for s in range(NT):
        units.append((0, fwd[s]))
        units.append((1, bwd[s]))

    def loads(ui):
        d, ti = units[ui]
        b = ui % NB
        tok = slice(ti * 128, (ti + 1) * 128)
        k.load(gbt[b][0], gbt[b][1], gb_s[tok, :], r_gb_s)
        k.load(kT[b][0], kT[b][1], kdT_s[:, :, tok].rearrange("h d t -> d h t"), r_kdT_s)
        k.load(ktm[b][0], ktm[b][1], kd_s[tok, :, :], r_kd_s, q="act")
        k.load(vtm[b][0], vtm[b][1], vd_s[tok, :, :], r_vd_s, q="act")
        if ti >= 2:
            xt_ = slice((ti - 2) * 128, (ti - 1) * 128)
            k.load(qT[b][0], qT[b][1], qdT_s[:, :, xt_].rearrange("h d t -> d h t"), r_qdT_s)

    def pre(ui):
        d, ti = units[ui]
        b = ui % NB
        is_x = ti >= 2
        g_t, r_g = gbt[b]
        kT_t, r_kT = kT[b]
        qT_t, r_qT = qT[b]
        ktm_t, r_ktm = ktm[b]
        vtm_t, r_vtm = vtm[b]
        sm_t, r_sm = sm[b]
        gcol = g_t[:, d * 8:(d + 1) * 8]
        beta = g_t[:, 16 + d * 8:16 + (d + 1) * 8]
        lnb = g_t[:, 32 + d * 8:32 + (d + 1) * 8]
        pb, r_pb = nbp()

        def mm0(e):
            e.matmul(pb[:, 0:8], lhsT=msk[:, d, :], rhs=gcol, start=True, stop=True)
            return e.matmul(pb[:, 8:16], lhsT=k.ones_f, rhs=gcol, start=True, stop=True)
        S.op("pe", mm0, reads=[r_msk, r_g, k.r_ones_f], writes=[r_pb])
        S.op("dve", lambda e: e.tensor_copy(out=X[:, :, 32:40], in_=pb[:, 0:8].unsqueeze(1).to_broadcast([128, 2, 8])), reads=[r_pb], writes=[r_X])
        S.op("dve", lambda e: e.tensor_copy(out=X[:, 1, 0:8], in_=pb[:, 0:8]), reads=[r_pb], writes=[r_X])
        S.op("dve", lambda e: e.tensor_tensor(out=X[:, 0, 0:8], in0=pb[:, 0:8], in1=lnb, op=ALU.add), reads=[r_pb, r_g], writes=[r_X])
        S.op("act", lambda e: e.activation(out=sm_t[:, 0, :], in_=pb[:, 0:8], func=AF.Exp), reads=[r_pb], writes=[r_sm])
        S.op("act", lambda e: e.activation(out=sm_t[:, 1, :], in_=pb[:, 8:16], func=AF.Exp), reads=[r_pb], writes=[r_sm])
        S.op("dve", lambda e: e.tensor_tensor(out=sm_t[:, 3, :], in0=pb[:, 8:16], in1=X[:, 1, 0:8], op=ALU.subtract), reads=[r_pb, r_X], writes=[r_sm])
        S.op("act", lambda e: e.activation(out=sm_t[:, 2, :], in_=sm_t[:, 3, :], func=AF.Exp), reads=[r_sm], writes=[r_sm])
        pt, r_pt = nbp()

        def tr0(e):
            e.transpose(out=pt[0:64, 0:128], in_=X[:, 0, :], identity=k.ident_f)
            return e.transpose(out=pt[0:64, 128:256], in_=X[:, 1, :], identity=k.ident_f)
        S.op("pe", tr0, reads=[r_X, k.r_ident_f], writes=[r_pt])
        S.op("act", lambda e: e.activation(out=L1[0:8, :], in_=pt[0:8, 0:128], func=AF.Copy), reads=[r_pt], writes=[r_L1])
        S.op("act", lambda e: e.activation(out=L2[0:8, :], in_=pt[0:8, 128:256], func=AF.Copy), reads=[r_pt], writes=[r_L2])
        S.op("dve", lambda e: e.tensor_tensor(out=R1[32:40, :, :], in0=EC[32:40, 1, :, :], in1=pt[32:40, 0:128].unsqueeze(1).to_broadcast([8, 8, 128]), op=ALU.mult),
             reads=[r_pt, r_EC], writes=[r_R1])
        S.op("dve", lambda e: e.tensor_tensor(out=R2[32:40, :, :], in0=EC[32:40, 0, :, :], in1=pt[32:40, 128:256].unsqueeze(1).to_broadcast([8, 8, 128]), op=ALU.mult),
             reads=[r_pt, r_EC], writes=[r_R2])
        kd_t, r_kd = kdec[b]
        S.op("pool", lambda e: e.tensor_tensor(out=kg, in0=ktm_t, in1=sm_t[:, 0, :].unsqueeze(2).to_broadcast([128, 8, 128]), op=ALU.mult),
             reads=[r_ktm, r_sm], writes=[r_kg])
        S.op("pool", lambda e: e.tensor_tensor(out=kd_t, in0=ktm_t, in1=sm_t[:, 2, :].unsqueeze(2).to_broadcast([128, 8, 128]), op=ALU.mult),
             reads=[r_ktm, r_sm], writes=[r_kd])
        yield
        def do_group(grp):
            hs = range(4 * grp, 4 * grp + 4)
            gs = slice(4 * grp, 4 * grp + 4)
            r_E1, r_M, r_Md, r_Mo1, r_Mo2, r_Mt, r_Tt, r_TtB = (RG[nm][grp] for nm in ("E1", "M", "Md", "Mo1", "Mo2", "Mt", "Tt", "TtB"))
            r_Ab = RG["Ab"][grp]
            rPP = [RG["PP0"][grp], RG["PP1"][grp]]
            rPT = [RG["PT0"][grp], RG["PT1"][grp]]
            Mo_list = ((Mo1r, r_Mo1), (Mo2r, r_Mo2))
            Md_list = ((Mdr, r_Md, 6), (Mo1r, r_Mo1, 7), (Mo2r, r_Mo2, 8))
            pk, r_pk = nbp()
            pd, r_pd = nbp()

            def mmk(e):
                for hh, h in enumerate(hs):
                    ins_ = e.matmul(b4(pk)[:, hh, :], lhsT=kT_t[:, h, :], rhs=kT_t[:, h, :], start=True, stop=True)
                return ins_
            S.op("pe", mmk, reads=[r_kT], writes=[r_pk])

            def mmd(e):
                for hh, h in enumerate(hs):
                    ins_ = e.matmul(b4(pd)[:, hh, :], lhsT=L1, rhs=R1[:, h, :], start=True, stop=True)
                return ins_
            S.op("pe", mmd, reads=[r_L1, r_R1], writes=[r_pd])
            S.op("dve", lambda e: e.scalar_tensor_tensor(out=E1[:, gs, :], in0=b4(pd), scalar=0.0, in1=msk[:, 2 + d, :].unsqueeze(1).to_broadcast([128, 4, 128]),
                                                         op0=ALU.min, op1=ALU.add), reads=[r_pd, r_msk], writes=[r_E1])
            S.op("act", lambda e: e.activation(out=E1[:, gs, :], in_=E1[:, gs, :], func=AF.Exp), reads=[r_E1], writes=[r_E1])
            S.op("dve", lambda e: e.tensor_tensor(out=M[:, gs, :], in0=b4(pk), in1=E1[:, gs, :], op=ALU.mult), reads=[r_pk, r_E1], writes=[r_M])
            for (dst_, rdst_, mi_) in Md_list:
                S.op("pool", lambda e, dst_=dst_, mi_=mi_: e.tensor_tensor(out=dst_[:, gs, :], in0=M[:, gs, :],
                                                                        in1=msk[:, mi_, :].unsqueeze(1).to_broadcast([128, 4, 128]), op=ALU.mult),
                     reads=[r_M, r_msk], writes=[rdst_])
            pm, r_pm = nbp(BF16)

            def trm(e):
                for hh, h in enumerate(hs):
                    ins_ = e.transpose(out=b4h(pm)[:, hh, :], in_=Md[:, h, :], identity=k.ident_b)
                return ins_
            S.op("pe", trm, reads=[r_Md, k.r_ident_b], writes=[r_pm])
            S.op("act", lambda e: e.activation(out=Mtr[:, gs, :], in_=b4h(pm), func=AF.Copy), reads=[r_pm], writes=[r_Mt])
            S.op("dve", lambda e: e.scalar_tensor_tensor(out=Ttr[:, gs, :], in0=b4h(pm), scalar=-1.0, in1=k.ident_f.unsqueeze(1).to_broadcast([128, 4, 128]),
                                                         op0=ALU.mult, op1=ALU.add), reads=[r_pm, k.r_ident_f], writes=[r_Tt])
            yield
            P_prev, rP_prev, Pt_prev, rPt_prev = Mdr, r_Md, Mtr, r_Mt
            for lvl in range(1, 1 + int(_os.environ.get('B_LEVELS', 4))):
                P_new, rP_new = PPr[lvl % 2], rPP[lvl % 2]
                Pt_new, rPt_new = PTr[lvl % 2], rPT[lvl % 2]
                pa, r_pa = nbp()

                def mma(e, P_prev=P_prev, Pt_prev=Pt_prev, pa=pa):
                    for hh, h in enumerate(hs):
                        ins_ = e.matmul(b4(pa)[:, hh, :], lhsT=Pt_prev[:, h, :], rhs=P_prev[:, h, :], start=True, stop=True)
                    return ins_
                S.op("pe", mma, reads=[rP_prev, rPt_prev], writes=[r_pa])
                S.op("act", lambda e, P_new=P_new, pa=pa: e.activation(out=P_new[:, gs, :], in_=b4(pa), func=AF.Copy), reads=[r_pa], writes=[rP_new])
                if lvl < int(_os.environ.get('B_LEVELS', 4)):
                    pbk, r_pbk = nbp()

                    def mmb(e, P_prev=P_prev, Pt_prev=Pt_prev, pbk=pbk):
                        for hh, h in enumerate(hs):
                            ins_ = e.matmul(b4(pbk)[:, hh, :], lhsT=P_prev[:, h, :], rhs=Pt_prev[:, h, :], start=True, stop=True)
                        return ins_
                    S.op("pe", mmb, reads=[rP_prev, rPt_prev], writes=[r_pbk])
                    S.op("act", lambda e, Pt_new=Pt_new, pbk=pbk: e.activation(out=Pt_new[:, gs, :], in_=b4(pbk), func=AF.Copy), reads=[r_pbk], writes=[rPt_new])
                pc, r_pc = nbp()

                def mmc(e, P_new=P_new, pc=pc):
                    for hh, h in enumerate(hs):
                        ins_ = e.matmul(b4(pc)[:, hh, :], lhsT=P_new[:, h, :], rhs=Ttr[:, h, :], start=True, stop=True)
                    return ins_
                S.op("pe", mmc, reads=[rP_new, r_Tt], writes=[r_pc])
                S.op("dve", lambda e, pc=pc: e.tensor_tensor(out=Ttr[:, gs, :], in0=Tt[:, gs, :], in1=b4(pc), op=ALU.add), reads=[r_Tt, r_pc], writes=[r_Tt])
                P_prev, rP_prev, Pt_prev, rPt_prev = P_new, rP_new, Pt_new, rPt_new
                yield
            for (Mo_, rMo_) in Mo_list:
                ptd, r_ptd = nbp(BF16)

                def trt(e, ptd=ptd):
                    for hh, h in enumerate(hs):
                        ins_ = e.transpose(out=b4h(ptd)[:, hh, :], in_=Tt[:, h, :], identity=k.ident_b)
                    return ins_
                S.op("pe", trt, reads=[r_Tt, k.r_ident_b], writes=[r_ptd])
                S.op("act", lambda e, ptd=ptd: e.activation(out=Mtr[:, gs, :], in_=b4h(ptd), func=AF.Copy), reads=[r_ptd], writes=[r_Mt])
                pa2, r_pa2 = nbp()

                def mma2(e, pa2=pa2, Mo_=Mo_):
                    for hh, h in enumerate(hs):
                        ins_ = e.matmul(b4(pa2)[:, hh, :], lhsT=Mo_[:, h, :], rhs=Ttr[:, h, :], start=True, stop=True)
                    return ins_
                S.op("pe", mma2, reads=[rMo_, r_Tt], writes=[r_pa2])
                S.op("act", lambda e, pa2=pa2: e.activation(out=E1r[:, gs, :], in_=b4(pa2), func=AF.Copy), reads=[r_pa2], writes=[r_Ab])
                pc2, r_pc2 = nbp()

                def mmc2(e, pc2=pc2):
                    for hh, h in enumerate(hs):
                        ins_ = e.matmul(b4(pc2)[:, hh, :], lhsT=Mtr[:, h, :], rhs=E1r[:, h, :], start=True, stop=True)
                    return ins_
                S.op("pe", mmc2, reads=[r_Mt, r_Ab], writes=[r_pc2])
                S.op("dve", lambda e, pc2=pc2: e.tensor_tensor(out=Ttr[:, gs, :], in0=Tt[:, gs, :], in1=b4(pc2), op=ALU.subtract), reads=[r_Tt, r_pc2], writes=[r_Tt])
                yield
            S.op("pool", lambda e: e.tensor_tensor(out=TtB[:, gs, :], in0=Tt[:, gs, :], in1=beta[:, gs].unsqueeze(2).to_broadcast([128, 4, 128]), op=ALU.mult),
                 reads=[r_Tt, r_g], writes=[r_TtB])
            pw, r_pw = nbp()

            def mmw(e):
                for hh, h in enumerate(hs):
                    ins_ = e.matmul(b4(pw)[:, hh, :], lhsT=kg[:, h, :], rhs=TtB[:, h, :], start=True, stop=True)
                return ins_
            S.op("pe", mmw, reads=[r_kg, r_TtB], writes=[r_pw])
            S.op("act", lambda e: e.activation(out=wT[b][0][:, gs, :], in_=b4(pw), func=AF.Copy), reads=[r_pw], writes=[wT_res[b][grp]])
            pu, r_pu = nbp()

            def mmu(e):
                for hh, h in enumerate(hs):
                    ins_ = e.matmul(b4(pu)[:, hh, :], lhsT=TtB[:, h, :], rhs=vtm_t[:, h, :], start=True, stop=True)
                return ins_
            S.op("pe", mmu, reads=[r_TtB, r_vtm], writes=[r_pu])
            S.op("act", lambda e: e.activation(out=u[b][0][:, gs, :], in_=b4(pu), func=AF.Copy), reads=[r_pu], writes=[u_res[b][grp]])
            yield
            if is_x:
                pq, r_pq = nbp()
                pd2, r_pd2 = nbp()

                def mmq(e):
                    for hh, h in enumerate(hs):
                        ins_ = e.matmul(b4(pq)[:, hh, :], lhsT=kT_t[:, h, :], rhs=qT_t[:, h, :], start=True, stop=True)
                    return ins_
                S.op("pe", mmq, reads=[r_kT, r_qT], writes=[r_pq])

                def mmd2(e):
                    for hh, h in enumerate(hs):
                        ins_ = e.matmul(b4(pd2)[:, hh, :], lhsT=L2, rhs=R2[:, h, :], start=True, stop=True)
                    return ins_
                S.op("pe", mmd2, reads=[r_L2, r_R2], writes=[r_pd2])
                S.op("dve", lambda e: e.scalar_tensor_tensor(out=E1[:, gs, :], in0=b4(pd2), scalar=0.0, in1=msk[:, 4 + d, :].unsqueeze(1).to_broadcast([128, 4, 128]),
                                                             op0=ALU.min, op1=ALU.add), reads=[r_pd2, r_msk], writes=[r_E1])
                S.op("act", lambda e: e.activation(out=E1[:, gs, :], in_=E1[:, gs, :], func=AF.Exp), reads=[r_E1], writes=[r_E1])
                S.op("dve", lambda e: e.tensor_tensor(out=qkT[b][0][:, gs, :], in0=b4(pq), in1=E1[:, gs, :], op=ALU.mult), reads=[r_pq, r_E1], writes=[qk_res[b][grp]])
                yield
        gens_ = [do_group(0), do_group(1)]
        while gens_:
            for g_ in list(gens_):
                try:
                    next(g_)
                    yield
                except StopIteration:
                    gens_.remove(g_)

    def seq(ui):
        d, ti = units[ui]
        b = ui % NB
        is_x = ti >= 2
        S32_t, r_S32 = S32[d]
        Sbf_t, r_Sbf = Sbf[d]
        sm_t, r_sm = sm[b]
        wT_t, u_t, qk_t = wT[b][0], u[b][0], qkT[b][0]
        qT_t, r_qT = qT[b]
        kd_t, r_kd = kdec[b]
        banks = [k.bank(4 + j) for j in range(4)]

        def grp_mm(bank2, lhs_fn, rhs_fn, reads):
            for grp in range(2):
                pb, r_pb = bank2[grp]

                def f(e, grp=grp, pb=pb):
                    for hh in range(4):
                        h = 4 * grp + hh
                        ins_ = e.matmul(b4(pb)[:, hh, :], lhsT=lhs_fn(h), rhs=rhs_fn(h), start=True, stop=True)
                    return ins_
                S.op("pe", f, reads=reads, writes=[r_pb])
        grp_mm(banks[0:2], lambda h: wT_t[:, h, :], lambda h: Sbf_t[:, h, :], wT_res[b] + [r_Sbf])
        S.op("pool", lambda e: e.tensor_tensor(out=S32_t, in0=S32_t, in1=sm_t[:, 1, :].unsqueeze(2).to_broadcast([128, 8, 128]), op=ALU.mult),
             reads=[r_S32, r_sm], writes=[r_S32])
        yield
        for grp in range(2):
            gs = slice(4 * grp, 4 * grp + 4)
            pb, r_pb = banks[grp]
            S.op("dve", lambda e, pb=pb, gs=gs: e.tensor_tensor(out=vnew[:, gs, :], in0=u_t[:, gs, :], in1=b4(pb), op=ALU.subtract),
                 reads=u_res[b] + [r_pb], writes=[r_vnew])
        yield
        if is_x:
            grp_mm(banks[2:4], lambda h: qT_t[:, h, :], lambda h: Sbf_t[:, h, :], [r_qT, r_Sbf])
            grp_mm(banks[0:2], lambda h: qk_t[:, h, :], lambda h: vnew[:, h, :], qk_res[b] + [r_vnew])
            yield
            o_tt, r_o = o_t[b]
            for grp in range(2):
                gs = slice(4 * grp, 4 * grp + 4)
                pbq, r_pbq = banks[2 + grp]
                pbc, r_pbc = banks[grp]
                S.op("dve", lambda e, pbq=pbq, gs=gs: e.tensor_tensor(out=tmpo[:, gs, :], in0=b4(pbq), in1=sm_t[:, 0, gs].unsqueeze(2).to_broadcast([128, 4, 128]), op=ALU.mult),
                     reads=[r_pbq, r_sm], writes=[r_tmpo])
                S.op("dve", lambda e, pbc=pbc, gs=gs, o_tt=o_tt: e.tensor_tensor(out=o_tt[:, gs, :], in0=tmpo[:, gs, :], in1=b4(pbc), op=ALU.add),
                     reads=[r_tmpo, r_pbc], writes=[r_o])
            xi = ti - 2
            k.store(o_s[d][0][xi * 128:(xi + 1) * 128, :], o_s[d][1], o_tt.rearrange("p h d -> p (h d)"), r_o)
            yield
        grp_mm(banks[2:4], lambda h: kd_t[:, h, :], lambda h: vnew[:, h, :], [r_kd, r_vnew])
        yield
        for grp in range(2):
            gs = slice(4 * grp, 4 * grp + 4)
            pb, r_pb = banks[2 + grp]
            S.op("dve", lambda e, pb=pb, gs=gs: e.tensor_tensor(out=S32_t[:, gs, :], in0=S32_t[:, gs, :], in1=b4(pb), op=ALU.add),
                 reads=[r_S32, r_pb], writes=[r_S32])
        S.op("act", lambda e: e.activation(out=Sbf_t, in_=S32_t, func=AF.Copy), reads=[r_S32], writes=[r_Sbf])
        yield

    import os as _os
    stop_after = int(_os.environ.get("B_UNITS", len(units)))
    pre_cut = int(_os.environ.get("B_PRE_CUT", 10000))
    do_seq = int(_os.environ.get("B_SEQ", 1))
    _pre = pre
    _seq = seq

    def pre(ui):
        for n_, _ in enumerate(_pre(ui)):
            if n_ + 1 >= pre_cut:
                return
            yield

    def seq(ui):
        if not do_seq:
            return
        yield from _seq(ui)
    loads(0)
    _drive(pre(0))
    for ui in range(stop_after):
        if ui + 1 < stop_after:
            loads(ui + 1)
            _drive(pre(ui + 1), seq(ui))
        else:
            _drive(seq(ui))
    k.b_state = (S32, Sbf)
    S.barrier()
    A.release()

def phase_c(k):
    S, A, nc, ins = k.S, k.A, k.nc, k.ins
    of_s, r_of_s = k.scr["of_s"]
    ob_s, r_ob_s = k.scr["ob_s"]
    zs_s, r_zs_s = k.scr["zs_s"]
    qmT_s, r_qmT_s = k.scr["qmT_s"]
    kmT_s, r_kmT_s = k.scr["kmT_s"]
    vm_s, r_vm_s = k.scr["vm_s"]
    yaT_s, r_yaT_s = k.scratch("yaT_s", [H, 128, TX], BF16)
    ybT_s, r_ybT_s = k.scratch("ybT_s", [H, 128, TX], BF16)
    A.mark()
    dng, r_dng = k.tile([128, 128], F32, "dng")
    k.load(dng, r_dng, ins["dng_rep"])
    NB = 2
    of_t = [k.tile([128, 8, 128], F32, "of") for _ in range(NB)]
    ob_t = [k.tile([128, 8, 128], F32, "ob") for _ in range(NB)]
    z_t = [k.tile([128, 8, 128], BF16, "z") for _ in range(NB)]
    sq, r_sq = k.tile([128, 8, 128], F32, "sq")
    ss8, r_ss8 = k.tile([128, 8], F32, "ss8")
    yb, r_yb = k.tile([128, 8, 128], BF16, "yb")
    yT = [k.tile([128, 8, 128], BF16, "yT") for _ in range(NB)]

    def c1_loads(xi):
        b = xi % NB
        rows = slice(xi * 128, (xi + 1) * 128)
        k.load(of_t[b][0], of_t[b][1], of_s[rows, :].rearrange("p (h d) -> p h d", h=8), r_of_s)
        k.load(ob_t[b][0], ob_t[b][1], ob_s[rows, :].rearrange("p (h d) -> p h d", h=8), r_ob_s, q="act")
        k.load(z_t[b][0], z_t[b][1], zs_s[rows, :].rearrange("p (h d) -> p h d", h=8), r_zs_s)

    c1_loads(0)
    for xi in range(32):
        b = xi % NB
        if xi + 1 < 32:
            c1_loads(xi + 1)
        o_, r_o = of_t[b]
        ob_, r_ob = ob_t[b]
        z_, r_z = z_t[b]
        S.op("dve", lambda e, o_=o_, ob_=ob_: e.tensor_tensor(out=o_, in0=o_, in1=ob_, op=ALU.add), reads=[r_o, r_ob], writes=[r_o])
        S.op("pool", lambda e, o_=o_: e.tensor_tensor(out=sq, in0=o_, in1=o_, op=ALU.mult), reads=[r_o], writes=[r_sq])
        S.op("dve", lambda e: e.tensor_reduce(out=ss8, in_=sq, axis=AX.X, op=ALU.add), reads=[r_sq], writes=[r_ss8])
        _rstd(k, ss8, r_ss8, 128, ss8, r_ss8)
        S.op("dve", lambda e, o_=o_: e.tensor_tensor(out=o_, in0=o_, in1=ss8.unsqueeze(2).to_broadcast([128, 8, 128]), op=ALU.mult),
             reads=[r_o, r_ss8], writes=[r_o])
        S.op("pool", lambda e, o_=o_: e.tensor_tensor(out=o_, in0=o_, in1=dng.unsqueeze(1).to_broadcast([128, 8, 128]), op=ALU.mult),
             reads=[r_o, r_dng], writes=[r_o])
        S.op("pool", lambda e, o_=o_, z_=z_: e.tensor_tensor(out=yb, in0=o_, in1=z_, op=ALU.mult), reads=[r_o, r_z], writes=[r_yb])
        pb, r_pb = _nb(k, BF16)
        pv = pb.rearrange("p (a b) -> p a b", b=128)

        def tr(e, pv=pv):
            for j in range(8):
                ins_ = e.transpose(out=pv[:, j, :], in_=yb[:, j, :], identity=k.ident_b)
            return ins_
        S.op("pe", tr, reads=[r_yb, k.r_ident_b], writes=[r_pb])
        yT_t, r_yT = yT[b]
        S.op("act", lambda e, pv=pv, yT_t=yT_t: e.activation(out=yT_t, in_=pv, func=AF.Copy), reads=[r_pb], writes=[r_yT])
        k.store(yaT_s[:, :, xi * 128:(xi + 1) * 128].rearrange("h d t -> d h t"), r_yaT_s, yT_t, r_yT)
    S.barrier()
    A.release()
    A.mark()
    Kn = [k.tile([128, T], BF16, "Kn") for _ in range(2)]
    Kr = [k.tile([64, T], BF16, "Kr") for _ in range(2)]
    Vh = [k.tile([128, NT, 128], BF16, "Vh") for _ in range(2)]
    Qn = [k.tile([128, 512], BF16, "Qn") for _ in range(2)]
    Qr = [k.tile([64, 512], BF16, "Qr") for _ in range(2)]
    NP = 6
    PT = [k.tile([128, 512], BF16, "PT") for _ in range(NP)]
    rinv, r_rinv = k.tile([128, 512], F32, "rinv")
    yo = [k.tile([128, 512], BF16, "yo") for _ in range(2)]
    vmv = vm_s.rearrange("(n p) c -> p n c", p=128)

    def head_loads(h):
        b = h % 2
        k.load(Kn[b][0], Kn[b][1], kmT_s[h, 0:128, :], r_kmT_s)
        k.load(Kr[b][0], Kr[b][1], kmT_s[h, 128:192, :], r_kmT_s, q="act")
        k.load(Vh[b][0], Vh[b][1], vmv[:, :, h * 128:(h + 1) * 128], r_vm_s)

    def q_loads(h, qg):
        b = (h * 8 + qg) % 2
        k.load(Qn[b][0], Qn[b][1], qmT_s[h, 0:128, qg * 512:(qg + 1) * 512], r_qmT_s)
        k.load(Qr[b][0], Qr[b][1], qmT_s[h, 128:192, qg * 512:(qg + 1) * 512], r_qmT_s, q="act")

    head_loads(0)
    q_loads(0, 0)
    sbank = [0]
    it = 0
    for h in range(H):
        if h + 1 < H:
            head_loads(h + 1)
        Kn_t, r_Kn = Kn[h % 2]
        Kr_t, r_Kr = Kr[h % 2]
        V_t, r_V = Vh[h % 2]
        for qg in range(8):
            gi = h * 8 + qg
            nxt = gi + 1
            if nxt < H * 8:
                q_loads(nxt // 8, nxt % 8)
            Qn_t, r_Qn = Qn[gi % 2]
            Qr_t, r_Qr = Qr[gi % 2]
            po, r_po = k.bank(4 + gi % 2)
            pr, r_pr = k.bank(6 + gi % 2)
            sb = {}

            def emit_s(kt):
                bnk = sbank[0]
                sbank[0] = (bnk + 1) % 4
                ps_, r_ps = k.bank(bnk)

                def f(e, ps_=ps_, kt=kt, Kn_t=Kn_t, Kr_t=Kr_t, Qn_t=Qn_t, Qr_t=Qr_t):
                    e.matmul(ps_, lhsT=Kn_t[:, kt * 128:(kt + 1) * 128], rhs=Qn_t, start=True, stop=False)
                    return e.matmul(ps_, lhsT=Kr_t[:, kt * 128:(kt + 1) * 128], rhs=Qr_t, start=False, stop=True)
                S.op("pe", f, reads=[r_Kn, r_Kr, r_Qn, r_Qr], writes=[r_ps])
                pt_, r_pt = PT[kt % NP]
                S.op("act", lambda e, ps_=ps_, pt_=pt_: e.activation(out=pt_, in_=ps_, func=AF.Exp), reads=[r_ps], writes=[r_pt])
                sb[kt] = (pt_, r_pt)

            def emit_pv(kt):
                pt_, r_pt = sb.pop(kt)

                def f(e, pt_=pt_, kt=kt, po=po, pr=pr, V_t=V_t):
                    e.matmul(po, lhsT=V_t[:, kt, :], rhs=pt_, start=(kt == 0), stop=(kt == NT - 1))
                    return e.matmul(pr, lhsT=k.ones_b, rhs=pt_, start=(kt == 0), stop=(kt == NT - 1))
                S.op("pe", f, reads=[r_V, r_pt, k.r_ones_b], writes=[r_po, r_pr])

            LOOK = 3
            for kt in range(min(LOOK, NT)):
                emit_s(kt)
            for kt in range(NT):
                if kt + LOOK < NT:
                    emit_s(kt + LOOK)
                emit_pv(kt)
            S.op("dve", lambda e, pr=pr: e.reciprocal(out=rinv, in_=pr), reads=[r_pr], writes=[r_rinv])
            yo_t, r_yo = yo[gi % 2]
            S.op("dve", lambda e, po=po, yo_t=yo_t: e.tensor_tensor(out=yo_t, in0=po, in1=rinv, op=ALU.mult), reads=[r_po, r_rinv], writes=[r_yo])
            k.store(ybT_s[h, :, qg * 512:(qg + 1) * 512], r_ybT_s, yo_t, r_yo)
    S.barrier()
    A.release()

def phase_d(k):
    S, A, nc, ins = k.S, k.A, k.nc, k.ins
    yaT_s, r_yaT_s = k.scr["yaT_s"]
    ybT_s, r_ybT_s = k.scr["ybT_s"]
    sgT_s, r_sgT_s = k.scr["sgT_s"]
    xmid_s, r_xmid_s = k.scratch("xmid_s", [TX, D], F32)
    h2_s, r_h2_s = k.scratch("h2_s", [TX, D], BF16)
    aff_s, r_aff_s = k.scratch("aff_s", [TX, 16], F32)
    affT_s, r_affT_s = k.scratch("affT_s", [16, TX], F32)
    A.mark()
    woa, r_woa = k.tile([128, 8, D], BF16, "woa")
    wob, r_wob = k.tile([128, 8, D], BF16, "wob")
    wo, r_wo = k.tile([128, 8, D], BF16, "wo")
    rw, r_rw = k.tile([128, 8, 16], F32, "rw")
    for (dst, rdst, nm) in ((woa, r_woa, "w_out_a"), (wob, r_wob, "w_out_b"), (wo, r_wo, "w_o")):
        src = ins[nm].rearrange("(kc p) n -> p kc n", p=128)
        for hf in range(2):
            k.wload(dst[:, 4 * hf:4 * hf + 4, :], rdst, src[:, 4 * hf:4 * hf + 4, :])
    k.load(rw, r_rw, ins["router_w"].rearrange("(kc p) n -> p kc n", p=128))
    NB = 2
    yaT = [k.tile([128, 8, 512], BF16, "yaT") for _ in range(NB)]
    ybT = [k.tile([128, 8, 512], BF16, "ybT") for _ in range(NB)]
    gA = [k.tile([128, 8, 512], BF16, "gA") for _ in range(NB)]
    gB = [k.tile([128, 8, 512], BF16, "gB") for _ in range(NB)]
    mg, r_mg = k.tile([128, 8, 512], BF16, "mg")
    t1 = [k.tile([128, 512], F32, "t1") for _ in range(2)]
    t2 = [k.tile([128, 512], F32, "t2") for _ in range(2)]
    xt = [k.tile([128, D], F32, "xt") for _ in range(NB)]
    xm = [k.tile([128, D], F32, "xm") for _ in range(NB)]
    junk, r_junk = k.tile([128, D], BF16, "junk")
    ssd = [k.tile([128, 8], F32, "ssd") for _ in range(NB)]
    h2f, r_h2f = k.tile([128, D], F32, "h2f")
    h2b = [k.tile([128, D], BF16, "h2b") for _ in range(NB)]
    h2T, r_h2T = k.tile([128, 8, 128], F32, "h2T")
    ex = [k.tile([128, 16], F32, "ex") for _ in range(NB)]
    affT = [k.tile([16, 128], F32, "affT") for _ in range(NB)]

    def g_loads(g):
        b = g % NB
        cols = slice(g * 512, (g + 1) * 512)
        k.load(yaT[b][0], yaT[b][1], yaT_s[:, :, cols].rearrange("h d t -> d h t"), r_yaT_s)
        k.load(ybT[b][0], ybT[b][1], ybT_s[:, :, cols].rearrange("h d t -> d h t"), r_ybT_s, q="act")
        k.load(gA[b][0], gA[b][1], sgT_s[0:8, :, cols].rearrange("h d t -> d h t"), r_sgT_s)
        k.load(gB[b][0], gB[b][1], sgT_s[8:16, :, cols].rearrange("h d t -> d h t"), r_sgT_s, q="act")

    g_loads(0)
    for g in range(8):
        b = g % NB
        if g + 1 < 8:
            g_loads(g + 1)
        ya_, r_ya = yaT[b]
        yb_, r_yb = ybT[b]
        gA_, r_gA = gA[b]
        gB_, r_gB = gB[b]
        for oc in range(8):
            pa, r_pa = _nb(k)
            pbk, r_pbk = _nb(k)

            def mma(e, pa=pa, oc=oc, ya_=ya_):
                for kc in range(8):
                    ins_ = e.matmul(pa, lhsT=woa[:, kc, oc * 128:(oc + 1) * 128], rhs=ya_[:, kc, :], start=(kc == 0), stop=(kc == 7))
                return ins_
            S.op("pe", mma, reads=[r_woa, r_ya], writes=[r_pa])

            def mmb(e, pbk=pbk, oc=oc, yb_=yb_):
                for kc in range(8):
                    ins_ = e.matmul(pbk, lhsT=wob[:, kc, oc * 128:(oc + 1) * 128], rhs=yb_[:, kc, :], start=(kc == 0), stop=(kc == 7))
                return ins_
            S.op("pe", mmb, reads=[r_wob, r_yb], writes=[r_pbk])
            t1_, r_t1 = t1[oc % 2]
            t2_, r_t2 = t2[oc % 2]
            S.op("dve", lambda e, pa=pa, oc=oc, t1_=t1_, gA_=gA_: e.tensor_tensor(out=t1_, in0=pa, in1=gA_[:, oc, :], op=ALU.mult), reads=[r_pa, r_gA], writes=[r_t1])
            S.op("dve", lambda e, pbk=pbk, oc=oc, t2_=t2_, gB_=gB_: e.tensor_tensor(out=t2_, in0=pbk, in1=gB_[:, oc, :], op=ALU.mult), reads=[r_pbk, r_gB], writes=[r_t2])
            S.op("pool", lambda e, oc=oc, t1_=t1_, t2_=t2_: e.tensor_tensor(out=mg[:, oc, :], in0=t1_, in1=t2_, op=ALU.add), reads=[r_t1, r_t2], writes=[r_mg])
        for tt in range(4):
            ti = g * 4 + tt
            tb = ti % NB
            rows = slice(ti * 128, (ti + 1) * 128)
            x_, r_x = xt[tb]
            xm_, r_xm = xm[tb]
            ss_, r_ss = ssd[tb]
            k.load(x_, r_x, ins["x"][rows, :])
            for hf in range(2):
                pm, r_pm = _nb(k)

                def mmo(e, pm=pm, hf=hf, tt=tt):
                    for kc in range(8):
                        ins_ = e.matmul(pm, lhsT=mg[:, kc, tt * 128:(tt + 1) * 128], rhs=wo[:, kc, hf * 512:(hf + 1) * 512], start=(kc == 0), stop=(kc == 7))
                    return ins_
                S.op("pe", mmo, reads=[r_mg, r_wo], writes=[r_pm])
                S.op("dve", lambda e, pm=pm, hf=hf, xm_=xm_: e.tensor_tensor(out=xm_[:, hf * 512:(hf + 1) * 512], in0=pm, in1=k.gate1_row[:, hf * 512:(hf + 1) * 512], op=ALU.mult),
                     reads=[r_pm, k.r_gate1], writes=[r_xm])
            S.op("pool", lambda e, xm_=xm_, x_=x_: e.tensor_tensor(out=xm_, in0=xm_, in1=x_, op=ALU.add), reads=[r_xm, r_x], writes=[r_xm])
            k.store(xmid_s[rows, :], r_xmid_s, xm_, r_xm)
            k.store(k.out[rows, :], k.out_res, xm_, r_xm)
            S.op("act", lambda e, xm_=xm_, ss_=ss_: e.activation(out=junk, in_=xm_, func=AF.Square, accum_out=ss_[:, 0:1]), reads=[r_xm], writes=[r_junk, r_ss])
            _rstd(k, ss_[:, 0:1], r_ss, D, ss_[:, 1:2], r_ss)
            S.op("act", lambda e, xm_=xm_, ss_=ss_: e.activation(out=h2f, in_=xm_, func=AF.Copy, scale=ss_[:, 1:2]), reads=[r_xm, r_ss], writes=[r_h2f])
            S.op("pool", lambda e: e.tensor_tensor(out=h2f, in0=h2f, in1=k.s2_row, op=ALU.mult), reads=[r_h2f, k.r_s2row], writes=[r_h2f])
            S.op("pool", lambda e: e.tensor_tensor(out=h2f, in0=h2f, in1=k.shift2_row, op=ALU.add), reads=[r_h2f, k.r_shift2], writes=[r_h2f])
            h2b_, r_h2b = h2b[tb]
            S.op("act", lambda e, h2b_=h2b_: e.activation(out=h2b_, in_=h2f, func=AF.Copy), reads=[r_h2f], writes=[r_h2b])
            k.store(h2_s[rows, :], r_h2_s, h2b_, r_h2b)
            for hf in range(2):
                pt, r_pt = _nb(k)
                pv = pt.rearrange("p (a b) -> p a b", b=128)

                def trh(e, pv=pv, hf=hf):
                    for j in range(4):
                        ins_ = e.transpose(out=pv[:, j, :], in_=h2f[:, (hf * 4 + j) * 128:(hf * 4 + j + 1) * 128], identity=k.ident_f)
                    return ins_
                S.op("pe", trh, reads=[r_h2f, k.r_ident_f], writes=[r_pt])
                if hf == 0:
                    S.op("act", lambda e, pv=pv: e.activation(out=h2T[:, 0:4, :], in_=pv, func=AF.Copy), reads=[r_pt], writes=[r_h2T])
                else:
                    S.op("dve", lambda e, pv=pv: e.tensor_copy(out=h2T[:, 4:8, :], in_=pv), reads=[r_pt], writes=[r_h2T])
            pl, r_pl = _nb(k)

            def mml(e, pl=pl):
                for kc in range(8):
                    ins_ = e.matmul(pl[:, 0:16], lhsT=h2T[:, kc, :], rhs=rw[:, kc, :], start=(kc == 0), stop=(kc == 7))
                return ins_
            S.op("pe", mml, reads=[r_h2T, r_rw], writes=[r_pl])
            ex_, r_ex = ex[tb]
            S.op("dve", lambda e, pl=pl, ss_=ss_: e.tensor_reduce(out=ss_[:, 2:3], in_=pl[:, 0:16], axis=AX.X, op=ALU.max), reads=[r_pl], writes=[r_ss])
            S.op("dve", lambda e, ss_=ss_: e.tensor_scalar(out=ss_[:, 3:4], in0=ss_[:, 2:3], scalar1=-1.0, scalar2=None, op0=ALU.mult), reads=[r_ss], writes=[r_ss])
            S.op("act", lambda e, pl=pl, ss_=ss_, ex_=ex_: e.activation(out=ex_, in_=pl[:, 0:16], func=AF.Exp, bias=ss_[:, 3:4], accum_out=ss_[:, 4:5]),
                 reads=[r_pl, r_ss], writes=[r_ex, r_ss])
            S.op("dve", lambda e, ss_=ss_: e.reciprocal(out=ss_[:, 5:6], in_=ss_[:, 4:5]), reads=[r_ss], writes=[r_ss])
            S.op("dve", lambda e, ss_=ss_, ex_=ex_: e.tensor_scalar(out=ex_, in0=ex_, scalar1=ss_[:, 5:6], scalar2=None, op0=ALU.mult), reads=[r_ex, r_ss], writes=[r_ex])
            k.store(aff_s[rows, :], r_aff_s, ex_, r_ex)
            pt2, r_pt2 = _nb(k)
            S.op("pe", lambda e, pt2=pt2, ex_=ex_: e.transpose(out=pt2[0:16, 0:128], in_=ex_, identity=k.ident_f), reads=[r_ex, k.r_ident_f], writes=[r_pt2])
            aT_, r_aT = affT[tb]
            S.op("act", lambda e, pt2=pt2, aT_=aT_: e.activation(out=aT_, in_=pt2[0:16, 0:128], func=AF.Copy), reads=[r_pt2], writes=[r_aT])
            k.store(affT_s[:, rows], r_affT_s, aT_, r_aT)
    S.barrier()
    A.release()

NE = 16
CAP = 512
FF = 1408
NFC = 11


def phase_e(k):
    S, A, nc, ins = k.S, k.A, k.nc, k.ins
    aff_s, r_aff_s = k.scr["aff_s"]
    affT_s, r_affT_s = k.scr["affT_s"]
    h2_s, r_h2_s = k.scr["h2_s"]
    xmid_s, r_xmid_s = k.scr["xmid_s"]
    posmT_s, r_posmT_s = k.scratch("posmT_s", [NE, TX], F32)
    gc_s, r_gc_s = k.scratch("gc_s", [NE, 128, 4], F32)
    idx_s, r_idx_s = k.scratch("idx_s", [NE, 128, 4], I32)
    A.off = k.off_after_gate2
    A.mark()
    cst, r_cst = k.tile([128, 1024], F32, "cst")
    k.load(cst, r_cst, ins["consts"])
    blk, r_blk = k.tile([128, 128], F32, "blk")
    k.load(blk, r_blk, ins["moe_blk"])
    sel8, r_sel8 = k.tile([128, 16], F32, "sel8")
    k.load(sel8, r_sel8, ins["moe_sel8"])
    tris, r_tris = k.tile([128, 128], BF16, "tris")
    k.wload(tris, r_tris, ins["moe_tris"])
    iota_c = cst[:, 0:512]
    A.mark()
    A8, r_A8 = k.tile([128, 512], F32, "A8")
    k.load(A8, r_A8, affT_s.rearrange("e (s t) -> (e s) t", s=8), r_affT_s)
    junk, r_junk = k.tile([128, 512], F32, "junk")
    sc, r_sc = k.tile([128, 16], F32, "sc")
    S.op("pool", lambda e: e.memset(sc, 0.0), writes=[r_sc])
    S.op("pool", lambda e: e.memset(sc[:, 1:2], 1.0), reads=[r_sc], writes=[r_sc])
    for it in range(30):
        S.op("dve", lambda e: e.tensor_tensor(out=sc[:, 2:3], in0=sc[:, 0:1], in1=sc[:, 1:2], op=ALU.add), reads=[r_sc], writes=[r_sc])
        S.op("dve", lambda e: e.tensor_scalar(out=sc[:, 2:3], in0=sc[:, 2:3], scalar1=0.5, scalar2=None, op0=ALU.mult), reads=[r_sc], writes=[r_sc])
        S.op("dve", lambda e: e.tensor_scalar(out=junk, in0=A8, scalar1=sc[:, 2:3], scalar2=0.0, op0=ALU.is_ge, op1=ALU.add, accum_out=sc[:, 3:4]),
             reads=[r_A8, r_sc], writes=[r_junk, r_sc])
        pb, r_pb = _nb(k)
        S.op("pe", lambda e, pb=pb: e.matmul(pb[:, 0:1], lhsT=blk, rhs=sc[:, 3:4], start=True, stop=True), reads=[r_blk, r_sc], writes=[r_pb])
        S.op("dve", lambda e, pb=pb: e.tensor_scalar(out=sc[:, 4:5], in0=pb[:, 0:1], scalar1=CAP - 0.5, scalar2=None, op0=ALU.is_ge), reads=[r_pb], writes=[r_sc])
        S.op("dve", lambda e: e.tensor_scalar(out=sc[:, 5:6], in0=sc[:, 4:5], scalar1=-1.0, scalar2=1.0, op0=ALU.mult, op1=ALU.add), reads=[r_sc], writes=[r_sc])
        S.op("dve", lambda e: e.tensor_tensor(out=sc[:, 6:7], in0=sc[:, 2:3], in1=sc[:, 0:1], op=ALU.subtract), reads=[r_sc], writes=[r_sc])
        S.op("dve", lambda e: e.tensor_tensor(out=sc[:, 7:8], in0=sc[:, 1:2], in1=sc[:, 2:3], op=ALU.subtract), reads=[r_sc], writes=[r_sc])
        S.op("dve", lambda e: e.scalar_tensor_tensor(out=sc[:, 0:1], in0=sc[:, 6:7], scalar=sc[:, 4:5], in1=sc[:, 0:1], op0=ALU.mult, op1=ALU.add), reads=[r_sc], writes=[r_sc])
        S.op("dve", lambda e: e.scalar_tensor_tensor(out=sc[:, 1:2], in0=sc[:, 7:8], scalar=sc[:, 4:5], in1=sc[:, 2:3], op0=ALU.mult, op1=ALU.add), reads=[r_sc], writes=[r_sc])
        S.op("dve", lambda e: e.memset(sc[:, 3:4], 0.0), reads=[r_sc], writes=[r_sc])
    thrrep, r_thrrep = k.tile([128, 128], F32, "thrrep")
    S.op("dve", lambda e: e.tensor_copy(out=thrrep, in_=sc[:, 0:1].to_broadcast([128, 128])), reads=[r_sc], writes=[r_thrrep])
    pb, r_pb = _nb(k)
    S.op("pe", lambda e, pb=pb: e.matmul(pb[:, 0:16], lhsT=thrrep, rhs=sel8, start=True, stop=True), reads=[r_thrrep, r_sel8], writes=[r_pb])
    thr_row, r_thr = k.tile([128, 16], F32, "thr_row")
    S.op("act", lambda e, pb=pb: e.activation(out=thr_row, in_=pb[:, 0:16], func=AF.Copy), reads=[r_pb], writes=[r_thr])
    import os as _os
    if _os.environ.get("E_DBG"):
        k.dump("sc", sc, r_sc, [128, 16])
        k.dump("thr_row", thr_row, r_thr, [128, 16])
        k.dump("A8", A8, r_A8, [128, 512])
        S.barrier()
        A.release()
        A.release()
        return
    aff, r_aff = k.tile([128, 32, 16], F32, "aff")
    k.load(aff, r_aff, aff_s.rearrange("(n p) e -> p n e", p=128), r_aff_s)
    maskf, r_maskf = k.tile([128, 32, 16], F32, "maskf")
    maskb, r_maskb = k.tile([128, 32, 16], BF16, "maskb")
    posm, r_posm = k.tile([128, 32, 16], F32, "posm")
    parts, r_parts = k.tile([128, 32, 16, 5], BF16, "parts")
    tokp, r_tokp = k.tile([128, 32, 2], F32, "tokp")
    k.load(tokp, r_tokp, ins["moe_tok"])
    rem, r_rem = k.tile([128, 32, 16], F32, "rem")
    S.op("dve", lambda e: e.tensor_tensor(out=maskf, in0=aff, in1=thr_row.unsqueeze(1).to_broadcast([128, 32, 16]), op=ALU.is_ge),
         reads=[r_aff, r_thr], writes=[r_maskf])
    S.op("act", lambda e: e.activation(out=maskb, in_=maskf, func=AF.Copy), reads=[r_maskf], writes=[r_maskb])
    pp, r_pp = _nb(k)
    ppv = pp.rearrange("p (n e) -> p n e", e=16)

    def mmpos(e):
        for n in range(32):
            for m in range(n):
                e.matmul(ppv[:, n, :], lhsT=k.ones_b, rhs=maskb[:, m, :], start=(m == 0), stop=False)
            ins_ = e.matmul(ppv[:, n, :], lhsT=tris, rhs=maskb[:, n, :], start=(n == 0), stop=True)
        return ins_
    S.op("pe", mmpos, reads=[r_maskb, k.r_ones_b, r_tris], writes=[r_pp])
    S.op("dve", lambda e: e.scalar_tensor_tensor(out=posm, in0=ppv, scalar=1.0, in1=maskf, op0=ALU.add, op1=ALU.mult), reads=[r_pp, r_maskf], writes=[r_posm])
    S.op("dve", lambda e: e.tensor_scalar(out=posm, in0=posm, scalar1=-1.0, scalar2=None, op0=ALU.add), reads=[r_posm], writes=[r_posm])
    S.op("act", lambda e: e.activation(out=parts[:, :, :, 0], in_=aff, func=AF.Copy), reads=[r_aff], writes=[r_parts])
    S.op("dve", lambda e: e.tensor_tensor(out=rem, in0=aff, in1=parts[:, :, :, 0], op=ALU.subtract), reads=[r_aff, r_parts], writes=[r_rem])
    S.op("act", lambda e: e.activation(out=parts[:, :, :, 1], in_=rem, func=AF.Copy), reads=[r_rem], writes=[r_parts])
    S.op("dve", lambda e: e.tensor_tensor(out=rem, in0=rem, in1=parts[:, :, :, 1], op=ALU.subtract), reads=[r_rem, r_parts], writes=[r_rem])
    S.op("act", lambda e: e.activation(out=parts[:, :, :, 2], in_=rem, func=AF.Copy), reads=[r_rem], writes=[r_parts])
    S.op("dve", lambda e: e.tensor_copy(out=parts[:, :, :, 3:5], in_=tokp.unsqueeze(2).to_broadcast([128, 32, 16, 2])), reads=[r_tokp, r_parts], writes=[r_parts])
    pmTs = [k.tile([16, 512], F32, "pmT") for _ in range(2)]
    for g in range(8):
        pt, r_pt = _nb(k)

        def trp(e, pt=pt, g=g):
            for j in range(4):
                ins_ = e.transpose(out=pt[0:16, j * 128:(j + 1) * 128], in_=posm[:, g * 4 + j, :], identity=k.ident_f)
            return ins_
        S.op("pe", trp, reads=[r_posm, k.r_ident_f], writes=[r_pt])
        pmT, r_pmT = pmTs[g % 2]
        S.op("act", lambda e, pt=pt, pmT=pmT: e.activation(out=pmT, in_=pt[0:16, :], func=AF.Copy), reads=[r_pt], writes=[r_pmT])
        k.store(posmT_s[:, g * 512:(g + 1) * 512], r_posmT_s, pmT, r_pmT)
    if _os.environ.get("E_STOP") == "e1":
        S.barrier(); A.release(); A.release(); return
    Sel = [k.tile([128, 32, CAP], BF16, "Sel") for _ in range(2)]
    Sel_res = [(Res("sel0"), Res("sel1")) for _ in range(2)]
    gcs = [k.tile([128, 4], F32, "gcs") for _ in range(2)]
    idf = [k.tile([128, 4], F32, "idf") for _ in range(2)]
    idi = [k.tile([128, 4], I32, "idi") for _ in range(2)]
    for ex in range(NE):
        Sel_t, _ = Sel[ex % 2]
        r_Sel0, r_Sel1 = Sel_res[ex % 2]
        gcs_t, r_gcs = gcs[ex % 2]
        idf_t, r_idf = idf[ex % 2]
        idi_t, r_idi = idi[ex % 2]

        def bsel(e, Sel_t=Sel_t, ex=ex, par=0):
            for n in range(par, 32, 2):
                ins_ = e.tensor_scalar(out=Sel_t[:, n, :], in0=iota_c, scalar1=posm[:, n, ex:ex + 1], scalar2=None, op0=ALU.is_equal)
            return ins_
        if _os.environ.get("E_X") != "nodve":
            S.op("dve", lambda e, f=bsel: f(e, par=0), reads=[r_cst, r_posm], writes=[r_Sel0])
        S.op("dve", lambda e, f=bsel: f(e, par=1), reads=[r_cst, r_posm], writes=[r_Sel1])
        pq, r_pq = _nb(k)

        def mmgate(e, pq=pq, Sel_t=Sel_t, ex=ex):
            for cc in range(4):
                for n in range(32):
                    ins_ = e.matmul(pq[:, cc * 8:cc * 8 + 5], lhsT=Sel_t[:, n, cc * 128:(cc + 1) * 128], rhs=parts[:, n, ex, :], start=(n == 0), stop=(n == 31))
            return ins_
        if _os.environ.get("E_X") != "nomm":
            S.op("pe", mmgate, reads=[r_Sel0, r_Sel1, r_parts], writes=[r_pq])
        pq3 = pq[:, 0:32].rearrange("p (a b) -> p a b", b=8)
        S.op("dve", lambda e, pq3=pq3, gcs_t=gcs_t: e.tensor_reduce(out=gcs_t, in_=pq3[:, :, 0:3], axis=AX.X, op=ALU.add), reads=[r_pq], writes=[r_gcs])
        S.op("dve", lambda e, pq3=pq3, idf_t=idf_t: e.tensor_scalar(out=idf_t, in0=pq3[:, :, 3], scalar1=128.0, scalar2=None, op0=ALU.mult), reads=[r_pq], writes=[r_idf])
        S.op("dve", lambda e, pq3=pq3, idf_t=idf_t: e.tensor_tensor(out=idf_t, in0=idf_t, in1=pq3[:, :, 4], op=ALU.add), reads=[r_pq, r_idf], writes=[r_idf])
        S.op("dve", lambda e, idf_t=idf_t, idi_t=idi_t: e.tensor_copy(out=idi_t, in_=idf_t), reads=[r_idf], writes=[r_idi])
        k.store(gc_s[ex], r_gc_s, gcs_t, r_gcs)
        k.store(idx_s[ex], r_idx_s, idi_t, r_idi)
    S.barrier()
    A.release()
    if _os.environ.get("E_STOP") == "ea1":
        A.release(); return
    A.mark()
    U32 = mybir.dt.uint32
    ig = S.pool("ig", 8)
    wg = [k.tile([128, 8, FF], BF16, "wg") for _ in range(2)]
    wu = [k.tile([128, 8, FF], BF16, "wu") for _ in range(2)]
    wd = [k.tile([128, NFC, D], BF16, "wd") for _ in range(1)]
    xg = [k.tile([128, 4, D], BF16, "xg") for _ in range(2)]
    idx2 = [k.tile([128, 4], I32, "idx2") for _ in range(2)]
    gc2 = [k.tile([128, 4], F32, "gc2") for _ in range(2)]
    xeT_t, r_xeT = k.tile([128, 8, CAP], BF16, "xeT")
    hid, r_hid = k.tile([128, NFC, CAP], BF16, "hid")
    sg = [k.tile([128, CAP], F32, "sg") for _ in range(2)]
    yef = [k.tile([128, D], F32, "yef") for _ in range(4)]
    r_outacc = Res("outacc")
    h2_rows = h2_s

    def w_loads(ex):
        b = ex % 2
        srcg = ins["w_gate"][ex].rearrange("(kc p) f -> p kc f", p=128)
        srcu = ins["w_up"][ex].rearrange("(kc p) f -> p kc f", p=128)
        for j in range(4):
            k.wload(wg[b][0][:, 2 * j:2 * j + 2, :], wg[b][1], srcg[:, 2 * j:2 * j + 2, :])
        for j in range(4):
            k.wload(wu[b][0][:, 2 * j:2 * j + 2, :], wu[b][1], srcu[:, 2 * j:2 * j + 2, :])

    def wd_loads(ex):
        srcd = ins["w_down"][ex].rearrange("(fc p) d -> p fc d", p=128)
        for (a0, a1) in ((0, 3), (3, 6), (6, 9), (9, 11)):
            k.wload(wd[0][0][:, a0:a1, :], wd[0][1], srcd[:, a0:a1, :])

    def g_loads(ex):
        b = ex % 2
        k.load(idx2[b][0], idx2[b][1], idx_s[ex], r_idx_s)
        k.load(gc2[b][0], gc2[b][1], gc_s[ex], r_gc_s, q="act")
        for cc in range(4):
            S.dma("pool", ig, lambda e, b=b, cc=cc: e.indirect_dma_start(out=xg[b][0][:, cc, :], out_offset=None, in_=h2_rows,
                                                                       in_offset=bass.IndirectOffsetOnAxis(idx2[b][0].bitcast(U32)[:, cc:cc + 1], 0)),
                  reads=[idx2[b][1], r_h2_s], writes=[xg[b][1]])

    g_loads(0)
    w_loads(0)
    prev_sc = []
    for ex in range(NE):
        b = ex % 2
        wd_loads(ex)
        if ex + 1 < NE:
            g_loads(ex + 1)
            w_loads(ex + 1)
        wg_t, r_wg = wg[b]
        wu_t, r_wu = wu[b]
        wd_t, r_wd = wd[0]
        xg_t, r_xg = xg[b]
        gc_t, r_gc = gc2[b]
        id_t, r_id = idx2[b]
        for kc in range(8):
            pt, r_pt = _nb(k, BF16)

            def trx(e, pt=pt, kc=kc, xg_t=xg_t):
                for cc in range(4):
                    ins_ = e.transpose(out=pt[:, cc * 128:(cc + 1) * 128], in_=xg_t[:, cc, kc * 128:(kc + 1) * 128], identity=k.ident_b)
                return ins_
            S.op("pe", trx, reads=[r_xg, k.r_ident_b], writes=[r_pt])
            if kc % 2 == 0:
                S.op("act", lambda e, pt=pt, kc=kc: e.activation(out=xeT_t[:, kc, :], in_=pt[:, 0:512], func=AF.Copy), reads=[r_pt], writes=[r_xeT])
            else:
                S.op("dve", lambda e, pt=pt, kc=kc: e.tensor_copy(out=xeT_t[:, kc, :], in_=pt[:, 0:512]), reads=[r_pt], writes=[r_xeT])
        for fc in range(NFC):
            pg, r_pg = _nb(k)
            pu, r_pu = _nb(k)

            def mmG(e, pg=pg, fc=fc, wg_t=wg_t):
                for kc in range(8):
                    ins_ = e.matmul(pg, lhsT=wg_t[:, kc, fc * 128:(fc + 1) * 128], rhs=xeT_t[:, kc, :], start=(kc == 0), stop=(kc == 7))
                return ins_
            S.op("pe", mmG, reads=[r_wg, r_xeT], writes=[r_pg])

            def mmU(e, pu=pu, fc=fc, wu_t=wu_t):
                for kc in range(8):
                    ins_ = e.matmul(pu, lhsT=wu_t[:, kc, fc * 128:(fc + 1) * 128], rhs=xeT_t[:, kc, :], start=(kc == 0), stop=(kc == 7))
                return ins_
            S.op("pe", mmU, reads=[r_wu, r_xeT], writes=[r_pu])
            sg_t, r_sg = sg[fc % 2]
            S.op("act", lambda e, pg=pg, sg_t=sg_t: e.activation(out=sg_t, in_=pg, func=AF.Silu), reads=[r_pg], writes=[r_sg])
            S.op("dve", lambda e, pu=pu, sg_t=sg_t, fc=fc: e.tensor_tensor(out=hid[:, fc, :], in0=pu, in1=sg_t, op=ALU.mult), reads=[r_pu, r_sg], writes=[r_hid])
        cur_sc = []
        for cc in range(4):
            y_t, r_y = yef[cc]
            for hf in range(2):
                pd, r_pd = _nb(k)

                def mmD(e, pd=pd, cc=cc, hf=hf):
                    for fc in range(NFC):
                        ins_ = e.matmul(pd, lhsT=hid[:, fc, cc * 128:(cc + 1) * 128], rhs=wd_t[:, fc, hf * 512:(hf + 1) * 512], start=(fc == 0), stop=(fc == NFC - 1))
                    return ins_
                S.op("pe", mmD, reads=[r_hid, r_wd], writes=[r_pd])
                S.op("act", lambda e, pd=pd, cc=cc, hf=hf, y_t=y_t, gc_t=gc_t: e.activation(out=y_t[:, hf * 512:(hf + 1) * 512], in_=pd, func=AF.Copy, scale=gc_t[:, cc:cc + 1]),
                     reads=[r_pd, r_gc], writes=[r_y])
            S.op("pool", lambda e, y_t=y_t: e.tensor_tensor(out=y_t, in0=y_t, in1=k.gate2_row, op=ALU.mult), reads=[r_y, k.r_gate2], writes=[r_y])
            tok_ = S.dma("pool", ig, lambda e, y_t=y_t, id_t=id_t, cc=cc: e.indirect_dma_start(out=k.out, out_offset=bass.IndirectOffsetOnAxis(id_t.bitcast(U32)[:, cc:cc + 1], 0),
                                                                                          in_=y_t, in_offset=None, compute_op=ALU.add),
                         reads=[r_y, r_id, k.out_res], writes=[], extra=prev_sc)
            cur_sc.append(tok_)
            if cc == 3:
                prev_sc = cur_sc
    S.barrier()
    A.release()
    A.release()

def _rope_tables():
    rows, gw = 64, 64
    row = np.repeat(np.arange(rows), gw).astype(np.float32)
    col = np.tile(np.arange(gw), rows).astype(np.float32)
    n_freq = 16
    inv_freq = (10000.0 ** (-np.arange(n_freq, dtype=np.float32) / n_freq)).astype(np.float32)
    ang_r = row[:, None] * inv_freq
    ang_c = col[:, None] * inv_freq
    ang = np.concatenate([ang_r, ang_r, ang_c, ang_c], axis=-1).astype(np.float32)
    cos = np.cos(ang).astype(np.float32)
    sin = np.sin(ang).astype(np.float32)
    sgn = np.concatenate([-np.ones(16), np.ones(16), -np.ones(16), np.ones(16)]).astype(np.float32)
    return cos, (sin * sgn).astype(np.float32)


def prep_shared(inp):
    f = np.float32
    sh = {}
    sh["ada_w"] = np.ascontiguousarray(inp["ada_w"][0])
    sh["ada_b_row"] = np.ascontiguousarray(inp["ada_b"][0][None, :])
    sh["ada_bT"] = np.ascontiguousarray(inp["ada_b"][0].reshape(48, 128).T)
    sh["g1T"] = np.ascontiguousarray(inp["norm1_g"][0].reshape(8, 128).T)
    sh["g2T"] = np.ascontiguousarray(inp["norm2_g"][0].reshape(8, 128).T)
    sh["g2_rep"] = np.ascontiguousarray(np.broadcast_to(inp["norm2_g"][0].reshape(1, 1024), (128, 1024)))
    sh["w_in"] = np.ascontiguousarray(inp["w_in"][0])
    sh["convT"] = np.ascontiguousarray(inp["conv_w"][0].T.reshape(24, 128, 5).transpose(1, 0, 2))
    sh["alog_rep"] = np.ascontiguousarray(np.broadcast_to(inp["a_log"][0].reshape(1, 16), (128, 16)))
    sh["dtb_rep"] = np.ascontiguousarray(np.broadcast_to(inp["dt_bias"][0].reshape(1, 16), (128, 16)))
    sh["dng_rep"] = np.ascontiguousarray(np.broadcast_to(inp["dn_norm_g"][0].reshape(1, 128), (128, 128)))
    sh["gqaT"] = np.ascontiguousarray(inp["q_a_norm_g"][0].reshape(3, 128).T)
    sh["w_uq"] = np.ascontiguousarray(inp["w_uq"][0])
    sh["gkvaT"] = np.ascontiguousarray(inp["kv_a_norm_g"][0].reshape(2, 128).T)
    sh["w_ukv"] = np.ascontiguousarray(inp["w_ukv"][0])
    sh["gq_rep"] = np.ascontiguousarray(np.broadcast_to(inp["q_norm_g"][0].reshape(1, 192), (128, 192)))
    sh["gk_rep"] = np.ascontiguousarray(np.broadcast_to(inp["k_norm_g"][0].reshape(1, 192), (128, 192)))
    sh["w_out_a"] = np.ascontiguousarray(inp["w_out_a"][0])
    sh["w_out_b"] = np.ascontiguousarray(inp["w_out_b"][0])
    sh["w_o"] = np.ascontiguousarray(inp["w_o"][0])
    sh["router_w"] = np.ascontiguousarray(inp["router_w"][0])
    sh["w_gate"] = np.ascontiguousarray(inp["w_gate"][0])
    sh["w_up"] = np.ascontiguousarray(inp["w_up"][0])
    sh["w_down"] = np.ascontiguousarray(inp["w_down"][0])
    cos, sinS = _rope_tables()
    sh["rope_cs"] = np.ascontiguousarray(np.concatenate([cos, sinS], axis=1))
    sh["ident"] = np.eye(128, dtype=f)
    consts = np.zeros((128, 1024), f)
    consts[:, 0:512] = np.arange(512, dtype=f)[None, :]
    consts[:, 512] = np.arange(128, dtype=f)
    sh["consts"] = consts
    pp_ = np.arange(128)
    sh["moe_blk"] = (pp_[:, None] // 8 == pp_[None, :] // 8).astype(f)
    s8 = np.zeros((128, 16), f)
    s8[np.arange(16) * 8, np.arange(16)] = 1.0
    sh["moe_sel8"] = s8
    tk = np.zeros((128, 32, 2), f)
    tk[:, :, 0] = np.arange(32, dtype=f)[None, :]
    tk[:, :, 1] = np.arange(128, dtype=f)[:, None]
    sh["moe_tok"] = tk
    sh["moe_tris"] = (pp_[:, None] < pp_[None, :]).astype(f)
    ii = np.arange(128)
    P, Fr = ii[:, None], ii[None, :]
    NEGV = -30000.0
    mk = np.zeros((128, 9, 128), f)
    mk[:, 0] = (P <= Fr)
    mk[:, 1] = (P >= Fr)
    mk[:, 2] = np.where(P > Fr, 0.0, NEGV)
    mk[:, 3] = np.where(P < Fr, 0.0, NEGV)
    mk[:, 4] = np.where(Fr >= P, 0.0, NEGV)
    mk[:, 5] = np.where(Fr <= P, 0.0, NEGV)
    mk[:, 6] = (P // 32 == Fr // 32)
    mk[:, 7] = (P // 64 == Fr // 64) & (P // 32 != Fr // 32)
    mk[:, 8] = (P // 64 != Fr // 64)
    sh["dn_masks"] = mk
    es = np.zeros((64, 2, 8, 128), f)
    for hh in range(8):
        es[hh, 0, hh, :] = 1.0
        es[32 + hh, 0, hh, :] = 1.0
    es[:, 1] = -es[:, 0]
    sh["dn_esel"] = es
    li = np.zeros((64, 128), f)
    li[32:40] = 1.0
    sh["dn_linit"] = li
    return sh


def prep_core(inp, sh, b):
    m = dict(sh)
    m["x"] = np.ascontiguousarray(inp["x"][b])
    m["ctx"] = np.ascontiguousarray(inp["ctx"][b])
    cc = np.stack([inp["c"][b], inp["c_ctx"]], axis=-1).astype(np.float32)
    m["c2"] = np.ascontiguousarray(cc.reshape(8, 128, 2).transpose(1, 0, 2))
    return m

PHASES = ["a0", "a1", "a2", "b", "c", "d", "e"]


def build(upto="e", dbg=(), dumps=()):
    k = K(dbg=dbg)
    declare_inputs(k)
    setup_consts(k)
    k.dump_list = []

    def dump(name, ap, res, shape):
        t = k.nc.dram_tensor("dbg_" + name, list(shape), ap.dtype, kind="ExternalOutput").ap()
        k.store(t, None, ap, res)
        k.dump_list.append("dbg_" + name)
    k.dump = dump
    k.dumps = set(dumps)
    fns = {"a0": phase_a0}
    for nm in ("a1", "a2", "b", "c", "d", "e"):
        f = globals().get("phase_" + nm)
        if f is not None:
            fns[nm] = f
    for ph in PHASES:
        if ph in fns:
            fns[ph](k)
        if ph == upto:
            break
    k.S.emit()
    return k


_CACHE = {}


def kernel(**inputs):
    inp = {kk: np.asarray(v) for kk, v in inputs.items()}
    sh = prep_shared(inp)
    in_maps = [prep_core(inp, sh, b) for b in range(8)]
    k = build()
    res = run_bass_kernel_spmd(k.nc, in_maps, core_ids=list(range(8)))
    out = np.stack([np.asarray(r["out"]) for r in res.results], axis=0).astype(np.float32)
    return out
```

```python
import numpy as np
import concourse.bass as bass
import concourse.mybir as mybir
from concourse.bass_utils import run_bass_kernel_spmd

F32 = mybir.dt.float32
BF16 = mybir.dt.bfloat16
F32R = mybir.dt.float32r
I32 = mybir.dt.int32
AF = mybir.ActivationFunctionType
ALU = mybir.AluOpType
AX = mybir.AxisListType

ENGS = ("pe", "act", "dve", "pool", "sp")
EPOCH = 12000


class Res:
    __slots__ = ("name", "w", "rs", "multi", "ws", "excl")

    def __init__(self, name="", multi=False, excl=False):
        self.excl = excl
        self.name = name
        self.w = None
        self.rs = []
        self.multi = multi
        self.ws = []


class Tok:
    __slots__ = ("key", "val", "eng")

    def __init__(self, key, val, eng):
        self.key = key
        self.val = val
        self.eng = eng


class DmaPool:
    def __init__(self, sched, name, n):
        self.s = sched
        self.name = name
        self.n = n
        self.i = 0
        self.count = [0] * n
        self.last = [None] * n

    def keys(self):
        return [("dma", self.name, j) for j in range(self.n)]


class Sched:
    def __init__(self, nc):
        self.nc = nc
        self.ops = {e: [] for e in ENGS}
        self.cnt = {e: 0 for e in ENGS}
        self.pools = []
        self.last_tok = {e: None for e in ENGS}
        self.n_instr = 0

    def pool(self, name, n):
        p = DmaPool(self, name, n)
        self.pools.append(p)
        return p

    def _deps(self, eng, reads, writes):
        deps = []
        for r in reads:
            if r.multi:
                deps.extend(r.ws)
            elif r.w is not None:
                deps.append(r.w)
            if r.excl:
                deps.extend(t for t in r.rs if t.eng != eng)
        for w in writes:
            if w.multi:
                pass
            elif w.w is not None and w.w.eng != eng:
                deps.append(w.w)
            for t in w.rs:
                if t.eng != eng:
                    deps.append(t)
        return deps

    def _mark_w(self, writes, tok):
        for w in writes:
            if w.multi:
                w.ws.append(tok)
            else:
                w.w = tok
                w.rs = []

    def op(self, eng, fn, reads=(), writes=(), extra=()):
        deps = self._deps(eng, reads, writes) + list(extra)
        c = self.cnt[eng]
        tok = Tok(("eng", eng, c // EPOCH), c % EPOCH + 1, eng)
        self.cnt[eng] = c + 1
        for r in reads:
            r.rs.append(tok)
        self._mark_w(writes, tok)
        self.ops[eng].append((deps, fn, tok, 1))
        self.last_tok[eng] = tok
        return tok

    def dma(self, eng, pool, fn, reads=(), writes=(), extra=()):
        deps = self._deps('__dma__', reads, writes) + list(extra)
        j = pool.i
        pool.i = (pool.i + 1) % pool.n
        if pool.last[j] is not None:
            deps.append(pool.last[j])
        pool.count[j] += 16
        tok = Tok(("dma", pool.name, j), pool.count[j], None)
        pool.last[j] = tok
        for r in reads:
            r.rs.append(tok)
        self._mark_w(writes, tok)
        self.ops[eng].append((deps, fn, tok, 16))
        return tok

    def barrier(self):
        toks = [t for t in self.last_tok.values() if t is not None]
        for p in self.pools:
            toks += [t for t in p.last if t is not None]
        for e in ENGS:
            self.ops[e].append((list(toks), None, None, 0))

    def emit(self, final_waits_eng="sp"):
        nc = self.nc
        sems = {}

        def sem_of(key):
            if key not in sems:
                sems[key] = nc.alloc_semaphore("s_" + "_".join(str(k) for k in key))
            return sems[key]

        for e in ENGS:
            for ep in range((self.cnt[e] + EPOCH - 1) // EPOCH):
                sem_of(("eng", e, ep))
        for p in self.pools:
            for k in p.keys():
                sem_of(k)

        toks = [t for t in self.last_tok.values() if t is not None]
        for p in self.pools:
            toks += [t for t in p.last if t is not None]
        self.ops[final_waits_eng].append((list(toks), None, None, 0))

        engobj = {"pe": "tensor", "act": "scalar", "dve": "vector", "pool": "gpsimd", "sp": "sync"}
        sched = self

        def run(ename):
            def body(eng):
                seen = {}
                for deps, fn, tok, inc in sched.ops[ename]:
                    need = {}
                    for t in deps:
                        if t.val > need.get(t.key, 0):
                            need[t.key] = t.val
                    for k, v in need.items():
                        if seen.get(k, 0) >= v:
                            continue
                        seen[k] = v
                        eng.wait_ge(sem_of(k), v)
                        sched.n_instr += 1
                    if fn is not None:
                        ins = fn(eng)
                        ins.then_inc(sem_of(tok.key), inc)
                        sched.n_instr += 1
            return body

        with nc.Block() as block:
            for ename in ENGS:
                getattr(block, engobj[ename])(run(ename))


class Arena:
    def __init__(self, nc, kbytes=198):
        self.nc = nc
        self.words = kbytes * 256
        self.t = nc.alloc_sbuf_tensor("arena", [128, self.words], F32)
        self.ap = self.t.ap()
        self.off = 0
        self.marks = []
        self.peak = 0

    def tile(self, shape, dtype, name=None):
        esz = {F32: 4, BF16: 2, I32: 4}[dtype]
        n = int(np.prod(shape[1:]))
        nw = (n * esz + 3) // 4
        off = (self.off + 15) // 16 * 16
        assert off + nw <= self.words, f"SBUF overflow {off}+{nw} > {self.words}"
        self.off = off + nw
        self.peak = max(self.peak, self.off)
        a = self.ap[0:shape[0], off:off + nw]
        if dtype != F32:
            a = a.bitcast(dtype)
        a = a[:, 0:n]
        if len(shape) == 3:
            a = a.rearrange("p (a b) -> p a b", a=shape[1])
        elif len(shape) == 4:
            a = a.rearrange("p (a b c) -> p a b c", a=shape[1], b=shape[2])
        return a

    def mark(self):
        self.marks.append(self.off)

    def release(self):
        self.off = self.marks.pop()

D = 1024
T = 4352
NT = 34
TX = 4096
NCTX = 256
H = 8
OFF_Z = 3072
OFF_GATE = 4832
D_IN = 6880
NMID = 1760
EPS = 1e-6
NEG = -30000.0


class K:
    def __init__(self, dbg=()):
        self.nc = bass.Bass("TRN2", target_bir_lowering=False)
        self.S = Sched(self.nc)
        self.A = Arena(self.nc)
        self.dbg = set(dbg)
        self.ins = {}
        self.scr = {}
        nc = self.nc
        self.ps = nc.alloc_psum_tensor("ps", [128, 8, 512], F32).ap()
        self.psr = [Res(f"ps{b}", excl=True) for b in range(8)]
        self.ld = self.S.pool("ld", 8)
        self.st = self.S.pool("st", 8)
        self.wl = self.S.pool("wl", 12)

    def inp(self, name, shape, dtype=F32):
        t = self.nc.dram_tensor(name, list(shape), dtype, kind="ExternalInput").ap()
        self.ins[name] = t
        return t

    def scratch(self, name, shape, dtype):
        kind = "ExternalOutput" if name in self.dbg else "Internal"
        t = self.nc.dram_tensor(name, list(shape), dtype, kind=kind).ap()
        self.scr[name] = (t, Res(name, multi=True))
        return t, self.scr[name][1]

    def bank(self, b, dtype=F32):
        a = self.ps[:, b, :]
        if dtype == BF16:
            a = a.bitcast(BF16)
        return a, self.psr[b]

    def tile(self, shape, dtype, name=None):
        return self.A.tile(shape, dtype, name), Res(name or "t")

    def load(self, dst, dres, src, sres=None, q="sp", pool=None):
        return self.S.dma(q, pool or self.ld, lambda e: e.dma_start(out=dst, in_=src),
                          reads=[sres] if sres is not None else [], writes=[dres])

    def store(self, dst, dres, src, sres, q="sp", pool=None):
        return self.S.dma(q, pool or self.st, lambda e: e.dma_start(out=dst, in_=src),
                          reads=[sres], writes=[dres] if dres is not None else [])

    def wload(self, dst, dres, src):
        return self.S.dma("pool", self.wl, lambda e: e.dma_start(out=dst, in_=src), writes=[dres])


def declare_inputs(k):
    i = k.inp
    i("x", [TX, D]); i("ctx", [NCTX, D]); i("c2", [128, 8, 2])
    i("ada_w", [D, 6 * D]); i("ada_b_row", [1, 6 * D]); i("ada_bT", [128, 48])
    i("g1T", [128, 8]); i("g2T", [128, 8]); i("g2_rep", [128, D])
    i("w_in", [D, D_IN]); i("convT", [128, 24, 5])
    i("alog_rep", [128, 16]); i("dtb_rep", [128, 16]); i("dng_rep", [128, 128])
    i("gqaT", [128, 3]); i("w_uq", [384, 1536]); i("gkvaT", [128, 2]); i("w_ukv", [256, 2048])
    i("gq_rep", [128, 192]); i("gk_rep", [128, 192])
    i("w_out_a", [D, D]); i("w_out_b", [D, D]); i("w_o", [D, D])
    i("router_w", [D, 16]); i("w_gate", [16, D, 1408]); i("w_up", [16, D, 1408]); i("w_down", [16, 1408, D])
    i("rope_cs", [TX, 128])
    i("ident", [128, 128]); i("consts", [128, 1024])
    i("moe_tok", [128, 32, 2]); i("moe_blk", [128, 128]); i("moe_sel8", [128, 16]); i("moe_tris", [128, 128])
    i("dn_masks", [128, 9, 128]); i("dn_esel", [64, 2, 8, 128]); i("dn_linit", [64, 128])
    k.out = k.nc.dram_tensor("out", [TX, D], F32, kind="ExternalOutput").ap()
    k.out_res = Res("out", multi=True)


def setup_consts(k):
    S = k.S
    k.ident_f, k.r_ident_f = k.tile([128, 128], F32, "identf")
    k.ident_b, k.r_ident_b = k.tile([128, 128], BF16, "identb")
    k.load(k.ident_f, k.r_ident_f, k.ins["ident"])
    S.op("dve", lambda e: e.tensor_copy(out=k.ident_b, in_=k.ident_f), reads=[k.r_ident_f], writes=[k.r_ident_b])
    k.ones_f, k.r_ones_f = k.tile([128, 128], F32, "onesf")
    k.ones_b, k.r_ones_b = k.tile([128, 128], BF16, "onesb")
    S.op("pool", lambda e: e.memset(k.ones_f, 1.0), writes=[k.r_ones_f])
    S.op("pool", lambda e: e.memset(k.ones_b, 1.0), writes=[k.r_ones_b])


def phase_a0(k):
    S, A, nc = k.S, k.A, k.nc
    ins = k.ins
    k.modT, k.r_modT = k.tile([128, 48, 2], F32, "modT")
    k.s1, k.r_s1 = k.tile([128, 8, 2], F32, "s1")
    k.s2, k.r_s2 = k.tile([128, 8], F32, "s2")
    k.gate1_row, k.r_gate1 = k.tile([128, D], F32, "gate1row")
    k.gate2_row, k.r_gate2 = k.tile([128, D], F32, "gate2row")
    k.off_after_gate2 = A.off
    k.shift2_row, k.r_shift2 = k.tile([128, D], F32, "shift2row")
    k.s2_row, k.r_s2row = k.tile([128, D], F32, "s2row")
    A.mark()
    c2, r_c2 = k.tile([128, 8, 2], F32, "c2")
    sc, r_sc = k.tile([128, 8, 2], F32, "sc")
    screp, r_screp = k.tile([128, 8, 128], F32, "screp")
    abT, r_abT = k.tile([128, 48], F32, "abT")
    abrow, r_abrow = k.tile([1, 6 * D], F32, "abrow")
    g1T, r_g1T = k.tile([128, 8], F32, "g1T")
    g2T, r_g2T = k.tile([128, 8], F32, "g2T")
    wbuf = [k.tile([128, 8, D], F32, f"adaw{j}") for j in range(2)]
    k.load(c2, r_c2, ins["c2"])
    k.load(abT, r_abT, ins["ada_bT"])
    k.load(abrow, r_abrow, ins["ada_b_row"])
    k.load(g1T, r_g1T, ins["g1T"])
    k.load(g2T, r_g2T, ins["g2T"])
    S.op("act", lambda e: e.activation(out=sc, in_=c2, func=AF.Silu), reads=[r_c2], writes=[r_sc])
    S.op("dve", lambda e: e.tensor_copy(out=screp, in_=sc[:, :, 0:1].to_broadcast([128, 8, 128])),
         reads=[r_sc], writes=[r_screp])
    pm, r_pm = k.bank(0)
    aw = ins["ada_w"].rearrange("(kc p) n -> p kc n", p=128)
    for sec in range(6):
        wt, r_wt = wbuf[sec % 2]
        q = "sp" if sec % 2 == 0 else "act"
        S.dma(q, k.ld, lambda e, wt=wt, sec=sec: e.dma_start(out=wt, in_=aw[:, :, sec * D:(sec + 1) * D]), writes=[r_wt])

        def mm(e, wt=wt, sec=sec):
            for fc in range(8):
                for kc in range(8):
                    ins_ = e.matmul(pm[:, (sec * 8 + fc) * 2:(sec * 8 + fc) * 2 + 2], lhsT=wt[:, kc, fc * 128:(fc + 1) * 128],
                                    rhs=sc[:, kc, :], start=(kc == 0), stop=(kc == 7))
            return ins_
        S.op("pe", mm, reads=[r_wt, r_sc], writes=[r_pm])
        if sec in (2, 3, 4, 5):
            dst, r_dst = {2: (k.gate1_row, k.r_gate1), 5: (k.gate2_row, k.r_gate2), 3: (k.shift2_row, k.r_shift2), 4: (k.s2_row, k.r_s2row)}[sec]
            for hf in range(2):
                pb, r_pb = k.bank(1 + hf)

                def mmr(e, wt=wt, sec=sec, hf=hf, pb=pb):
                    for kc in range(8):
                        e.matmul(pb, lhsT=screp[:, kc, :], rhs=wt[:, kc, hf * 512:(hf + 1) * 512], start=(kc == 0), stop=False)
                    return e.matmul(pb, lhsT=k.ones_f[0:1, :], rhs=abrow[0:1, sec * D + hf * 512: sec * D + (hf + 1) * 512],
                                    start=False, stop=True)
                S.op("pe", mmr, reads=[r_wt, r_screp, k.r_ones_f, r_abrow], writes=[r_pb])
                S.op("act", lambda e, dst=dst, hf=hf, pb=pb: e.activation(out=dst[:, hf * 512:(hf + 1) * 512], in_=pb, func=AF.Copy),
                     reads=[r_pb], writes=[r_dst])
    S.op("dve", lambda e: e.tensor_tensor(out=k.modT, in0=pm[:, 0:96].rearrange("p (a b) -> p a b", b=2),
                                          in1=abT.unsqueeze(2).to_broadcast([128, 48, 2]), op=ALU.add),
         reads=[r_pm, r_abT], writes=[k.r_modT])
    S.op("dve", lambda e: e.scalar_tensor_tensor(out=k.s1, in0=k.modT[:, 8:16, :], scalar=1.0,
                                                 in1=g1T.unsqueeze(2).to_broadcast([128, 8, 2]), op0=ALU.add, op1=ALU.mult),
         reads=[k.r_modT, r_g1T], writes=[k.r_s1])
    S.op("dve", lambda e: e.scalar_tensor_tensor(out=k.s2, in0=k.modT[:, 32:40, 0], scalar=1.0,
                                                 in1=g2T, op0=ALU.add, op1=ALU.mult),
         reads=[k.r_modT, r_g2T], writes=[k.r_s2])
    g2rep, r_g2rep = k.tile([128, D], F32, "g2rep")
    k.load(g2rep, r_g2rep, ins["g2_rep"])
    S.op("dve", lambda e: e.scalar_tensor_tensor(out=k.s2_row, in0=k.s2_row, scalar=1.0, in1=g2rep, op0=ALU.add, op1=ALU.mult),
         reads=[k.r_s2row, r_g2rep], writes=[k.r_s2row])
    S.barrier()
    A.release()

def _nb(k, dtype=F32):
    b = getattr(k, "_bank_i", 0)
    k._bank_i = (b + 1) % 8
    return k.bank(b, dtype)


def _rstd(k, ss, r_ss, n, out, r_out):
    S = k.S
    S.op("act", lambda e: e.activation(out=out, in_=ss, func=AF.Sqrt, scale=1.0 / n, bias=EPS), reads=[r_ss], writes=[r_out])
    S.op("dve", lambda e: e.reciprocal(out=out, in_=out), reads=[r_out], writes=[r_out])


def _rope(k, pe, r_pe, cos_t, sin_t, r_tab, t1, t2, r_t1, r_t2):
    S = k.S
    cb = cos_t.unsqueeze(1).to_broadcast([128, 8, 64])
    S.op("pool", lambda e: e.tensor_tensor(out=t1, in0=pe, in1=cb, op=ALU.mult), reads=[r_pe, r_tab], writes=[r_t1])
    pe5 = pe.rearrange("p h (a s c) -> p h a s c", a=2, s=2)
    t25 = t2.rearrange("p h (a s c) -> p h a s c", a=2, s=2)
    sn5 = sin_t.rearrange("p (a s c) -> p a s c", a=2, s=2)

    def f(e):
        for s in range(2):
            ins_ = e.tensor_tensor(out=t25[:, :, :, s, :], in0=pe5[:, :, :, 1 - s, :],
                                   in1=sn5[:, :, s, :].unsqueeze(1).to_broadcast([128, 8, 2, 16]), op=ALU.mult)
        return ins_
    S.op("dve", f, reads=[r_pe, r_tab], writes=[r_t2])
    S.op("pool", lambda e: e.tensor_tensor(out=pe, in0=t1, in1=t2, op=ALU.add), reads=[r_t1, r_t2], writes=[r_pe])


def phase_a1(k):
    S, A, nc, ins = k.S, k.A, k.nc, k.ins
    zs_s, r_zs_s = k.scratch("zs_s", [TX, D], BF16)
    gb_s, r_gb_s = k.scratch("gb_s", [T, 48], F32)
    qmT_s, r_qmT_s = k.scratch("qmT_s", [H, 192, TX], BF16)
    kmT_s, r_kmT_s = k.scratch("kmT_s", [H, 192, T], BF16)
    vm_s, r_vm_s = k.scratch("vm_s", [T, D], BF16)
    A.mark()
    k.hT, _ = k.tile([128, 8, T], BF16, "hT")
    k.r_hT = [Res(f"hT{i}") for i in range(NT)]
    A.mark()
    wmid, r_wmid = k.tile([128, 8, NMID], BF16, "wmid")
    wuq, r_wuq = k.tile([128, 3, 1536], BF16, "wuq")
    wukv, r_wukv = k.tile([128, 2, 2048], BF16, "wukv")
    dtb, r_dtb = k.tile([128, 16], F32, "dtb")
    negA, r_negA = k.tile([128, 16], F32, "negA")
    gq, r_gq = k.tile([128, 192], F32, "gq")
    gk, r_gk = k.tile([128, 192], F32, "gk")
    gqaT, r_gqaT = k.tile([128, 3], F32, "gqaT")
    gkvaT, r_gkvaT = k.tile([128, 2], F32, "gkvaT")
    win = ins["w_in"].rearrange("(kc p) n -> p kc n", p=128)
    for j in range(4):
        k.wload(wmid[:, 2 * j:2 * j + 2, :], r_wmid, win[:, 2 * j:2 * j + 2, OFF_Z:OFF_GATE])
    k.wload(wuq, r_wuq, ins["w_uq"].rearrange("(kc p) n -> p kc n", p=128))
    k.wload(wukv, r_wukv, ins["w_ukv"].rearrange("(kc p) n -> p kc n", p=128))
    k.load(dtb, r_dtb, ins["dtb_rep"])
    k.load(negA, r_negA, ins["alog_rep"])
    k.load(gq, r_gq, ins["gq_rep"])
    k.load(gk, r_gk, ins["gk_rep"])
    k.load(gqaT, r_gqaT, ins["gqaT"])
    k.load(gkvaT, r_gkvaT, ins["gkvaT"])
    S.op("act", lambda e: e.activation(out=negA, in_=negA, func=AF.Exp), reads=[r_negA], writes=[r_negA])
    S.op("dve", lambda e: e.tensor_scalar(out=negA, in0=negA, scalar1=-1.0, scalar2=None, op0=ALU.mult), reads=[r_negA], writes=[r_negA])
    S.op("dve", lambda e: e.tensor_scalar(out=gq, in0=gq, scalar1=192.0 ** -0.5, scalar2=None, op0=ALU.mult), reads=[r_gq], writes=[r_gq])

    NB = 2
    xt = [k.tile([128, D], F32, "xt") for _ in range(NB)]
    junk, r_junk = k.tile([128, D], BF16, "junk")
    ss = [k.tile([128, 8], F32, "ss") for _ in range(NB)]
    xn = [k.tile([128, D], BF16, "xn") for _ in range(NB)]
    zs = [k.tile([128, D], BF16, "zs") for _ in range(NB)]
    gb = [k.tile([128, 48], F32, "gb") for _ in range(NB)]
    t16, r_t16 = k.tile([128, 16], F32, "t16")
    cqn, r_cqn = k.tile([128, 384], BF16, "cqn")
    ckvn, r_ckvn = k.tile([128, 256], BF16, "ckvn")
    cqnT, r_cqnT = k.tile([128, 3, 128], BF16, "cqnT")
    ckvnT, r_ckvnT = k.tile([128, 2, 128], BF16, "ckvnT")
    kr, r_kr = k.tile([128, 64], F32, "kr")
    qsb, r_qsb = k.tile([128, 8, 192], F32, "qsb")
    sq, r_sq = k.tile([128, 8, 192], F32, "sq")
    r8, r_r8 = k.tile([128, 8], F32, "r8")
    kvsb, r_kvsb = k.tile([128, 8, 2, 128], F32, "kvsb")
    tmpf, r_tmpf = kvsb.rearrange("p a b c -> p (a b c)")[:, 0:1024].rearrange("p (a b) -> p a b", a=8), r_kvsb
    kf, r_kf = sq, r_sq
    rk8, r_rk8 = k.tile([128, 8], F32, "rk8")
    sskr, r_sskr = k.tile([128, 1], F32, "sskr")
    rt1, r_rt1 = k.tile([128, 8, 64], F32, "rt1")
    rt2, r_rt2 = k.tile([128, 8, 64], F32, "rt2")
    cs_t = [k.tile([128, 128], F32, "cs") for _ in range(NB)]
    qf, r_qf = k.tile([128, 8, 192], BF16, "qf")
    kfb, r_kfb = k.tile([128, 8, 192], BF16, "kfb")
    vb = [k.tile([128, 8, 128], BF16, "vb") for _ in range(1)]
    qTn = [k.tile([128, 8, 128], BF16, "qTn") for _ in range(1)]
    qTr = [k.tile([64, 8, 128], BF16, "qTr") for _ in range(1)]
    kTn = [k.tile([128, 8, 128], BF16, "kTn") for _ in range(1)]
    kTr = [k.tile([64, 8, 128], BF16, "kTr") for _ in range(1)]

    def src_rows(i):
        return ins["ctx"][i * 128:(i + 1) * 128, :] if i < 2 else ins["x"][(i - 2) * 128:(i - 1) * 128, :]

    def prefetch(i):
        b = i % NB
        k.load(xt[b][0], xt[b][1], src_rows(i))
        if i >= 2:
            xi = i - 2
            S.dma("act", k.ld, lambda e: e.dma_start(out=cs_t[b][0], in_=ins["rope_cs"][xi * 128:(xi + 1) * 128, :]), writes=[cs_t[b][1]])

    def transposes(src, r_src, n, width=128, rows=128):
        pb, r_pb = _nb(k, BF16)
        pv = pb.rearrange("p (a b) -> p a b", b=128)[0:width, 0:n, :]

        def f(e):
            for j in range(n):
                ins_ = e.transpose(out=pv[:, j, :], in_=src(j), identity=k.ident_b)
            return ins_
        S.op("pe", f, reads=[r_src, k.r_ident_b], writes=[r_pb])
        return pv, r_pb

    prefetch(0)
    for i in range(NT):
        b = i % NB
        is_x = i >= 2
        xi = i - 2
        col = 0 if is_x else 1
        tok = slice(i * 128, (i + 1) * 128)
        if i + 1 < NT:
            prefetch(i + 1)
        x_t, r_x = xt[b]
        ss_t, r_ss = ss[b]
        xn_t, r_xn = xn[b]
        S.op("act", lambda e, x_t=x_t, ss_t=ss_t: e.activation(out=junk, in_=x_t, func=AF.Square, accum_out=ss_t[:, 0:1]),
             reads=[r_x], writes=[r_junk, r_ss])
        _rstd(k, ss_t[:, 0:1], r_ss, D, ss_t[:, 1:2], r_ss)
        S.op("act", lambda e, x_t=x_t, ss_t=ss_t, xn_t=xn_t: e.activation(out=xn_t, in_=x_t, func=AF.Copy, scale=ss_t[:, 1:2]),
             reads=[r_x, r_ss], writes=[r_xn])
        pv, r_pv = transposes(lambda j, xn_t=xn_t: xn_t[:, j * 128:(j + 1) * 128], r_xn, 8)
        S.op("dve", lambda e, pv=pv, col=col: e.tensor_tensor(out=tmpf, in0=pv, in1=k.s1[:, :, col:col + 1].to_broadcast([128, 8, 128]), op=ALU.mult),
             reads=[r_pv, k.r_s1], writes=[r_tmpf])
        S.op("pool", lambda e, col=col, tok=tok: e.tensor_tensor(out=k.hT[:, :, tok], in0=tmpf,
                                                                in1=k.modT[:, 0:8, col:col + 1].to_broadcast([128, 8, 128]), op=ALU.add),
             reads=[r_tmpf, k.r_modT], writes=[k.r_hT[i]])
        groups = [(0, 512), (512, 1024), (1024, 1440), (1440, 1760)]
        banks = []
        for g, (c0, c1) in enumerate(groups):
            if g < 2 and not is_x:
                banks.append(None)
                continue
            pb, r_pb = _nb(k)

            def mm(e, pb=pb, c0=c0, c1=c1, tok=tok):
                for kc in range(8):
                    ins_ = e.matmul(pb[:, 0:c1 - c0], lhsT=k.hT[:, kc, tok], rhs=wmid[:, kc, c0:c1], start=(kc == 0), stop=(kc == 7))
                return ins_
            S.op("pe", mm, reads=[k.r_hT[i], r_wmid], writes=[r_pb])
            banks.append((pb, r_pb))
        if is_x:
            z_t, r_z = zs[b]
            for g in range(2):
                pb, r_pb = banks[g]
                S.op("act", lambda e, pb=pb, g=g, z_t=z_t: e.activation(out=z_t[:, g * 512:(g + 1) * 512], in_=pb, func=AF.Silu),
                     reads=[r_pb], writes=[r_z])
            k.store(zs_s[xi * 128:(xi + 1) * 128, :], r_zs_s, z_t, r_z)
        p2, r_p2 = banks[2]
        p3, r_p3 = banks[3]
        gb_t, r_gb = gb[b]
        S.op("dve", lambda e, p2=p2: e.tensor_tensor(out=t16, in0=p2[:, 0:16], in1=dtb, op=ALU.add), reads=[r_p2, r_dtb], writes=[r_t16])
        S.op("act", lambda e: e.activation(out=t16, in_=t16, func=AF.Exp), reads=[r_t16], writes=[r_t16])
        S.op("act", lambda e: e.activation(out=t16, in_=t16, func=AF.Ln, bias=1.0), reads=[r_t16], writes=[r_t16])
        S.op("dve", lambda e, gb_t=gb_t: e.tensor_tensor(out=gb_t[:, 0:16], in0=t16, in1=negA, op=ALU.mult), reads=[r_t16, r_negA], writes=[r_gb])
        S.op("act", lambda e, gb_t=gb_t, p2=p2: e.activation(out=gb_t[:, 16:32], in_=p2[:, 16:32], func=AF.Sigmoid), reads=[r_p2], writes=[r_gb])
        S.op("act", lambda e, gb_t=gb_t: e.activation(out=gb_t[:, 32:48], in_=gb_t[:, 16:32], func=AF.Ln), reads=[r_gb], writes=[r_gb])
        k.store(gb_s[tok, :], r_gb_s, gb_t, r_gb)
        if is_x:
            S.op("act", lambda e, p2=p2, ss_t=ss_t: e.activation(out=junk[:, 0:384], in_=p2[:, 32:416], func=AF.Square, accum_out=ss_t[:, 4:5]),
                 reads=[r_p2], writes=[r_junk, r_ss])
            _rstd(k, ss_t[:, 4:5], r_ss, 384, ss_t[:, 5:6], r_ss)
            S.op("act", lambda e, p2=p2, ss_t=ss_t: e.activation(out=cqn, in_=p2[:, 32:416], func=AF.Copy, scale=ss_t[:, 5:6]),
                 reads=[r_p2, r_ss], writes=[r_cqn])

        S.op("act", lambda e, p3=p3, ss_t=ss_t: e.activation(out=junk[:, 0:256], in_=p3[:, 0:256], func=AF.Square, accum_out=ss_t[:, 2:3]),
             reads=[r_p3], writes=[r_junk, r_ss])
        _rstd(k, ss_t[:, 2:3], r_ss, 256, ss_t[:, 3:4], r_ss)
        S.op("act", lambda e, p3=p3, ss_t=ss_t: e.activation(out=ckvn, in_=p3[:, 0:256], func=AF.Copy, scale=ss_t[:, 3:4]),
             reads=[r_p3, r_ss], writes=[r_ckvn])
        S.op("dve", lambda e, p3=p3: e.tensor_copy(out=kr, in_=p3[:, 256:320]), reads=[r_p3], writes=[r_kr])
        pv, r_pv = transposes(lambda j: ckvn[:, j * 128:(j + 1) * 128], r_ckvn, 2)
        S.op("dve", lambda e, pv=pv: e.tensor_tensor(out=ckvnT, in0=pv, in1=gkvaT.unsqueeze(2).to_broadcast([128, 2, 128]), op=ALU.mult),
             reads=[r_pv, r_gkvaT], writes=[r_ckvnT])
        for b4 in range(4):
            pb, r_pb = _nb(k)

            def mmkv(e, pb=pb, b4=b4):
                for kc in range(2):
                    ins_ = e.matmul(pb, lhsT=ckvnT[:, kc, :], rhs=wukv[:, kc, b4 * 512:(b4 + 1) * 512], start=(kc == 0), stop=(kc == 1))
                return ins_
            S.op("pe", mmkv, reads=[r_ckvnT, r_wukv], writes=[r_pb])
            eng = "act" if b4 % 2 == 0 else "dve"
            dst = kvsb[:, 2 * b4:2 * b4 + 2, :, :].rearrange("p a b c -> p (a b c)")
            if eng == "act":
                S.op("act", lambda e, dst=dst, pb=pb: e.activation(out=dst, in_=pb, func=AF.Copy), reads=[r_pb], writes=[r_kvsb])
            else:
                S.op("dve", lambda e, dst=dst, pb=pb: e.tensor_copy(out=dst, in_=pb), reads=[r_pb], writes=[r_kvsb])
        vb_t, r_vb = vb[0]
        S.op("pool", lambda e, vb_t=vb_t: e.tensor_copy(out=vb_t, in_=kvsb[:, :, 1, :]), reads=[r_kvsb], writes=[r_vb])
        k.store(vm_s[tok, :], r_vm_s, vb_t.rearrange("p h d -> p (h d)"), r_vb)
        S.op("pool", lambda e: e.tensor_tensor(out=sq[:, :, 0:128], in0=kvsb[:, :, 0, :], in1=kvsb[:, :, 0, :], op=ALU.mult),
             reads=[r_kvsb], writes=[r_sq])
        S.op("dve", lambda e: e.tensor_reduce(out=rk8, in_=sq[:, :, 0:128], axis=AX.X, op=ALU.add), reads=[r_sq], writes=[r_rk8])
        S.op("act", lambda e: e.activation(out=junk[:, 0:64], in_=kr, func=AF.Square, accum_out=sskr), reads=[r_kr], writes=[r_junk, r_sskr])
        S.op("dve", lambda e: e.tensor_scalar(out=rk8, in0=rk8, scalar1=sskr, scalar2=None, op0=ALU.add), reads=[r_rk8, r_sskr], writes=[r_rk8])
        _rstd(k, rk8, r_rk8, 192, rk8, r_rk8)
        S.op("dve", lambda e: e.tensor_tensor(out=kf[:, :, 0:128], in0=kvsb[:, :, 0, :], in1=rk8.unsqueeze(2).to_broadcast([128, 8, 128]), op=ALU.mult),
             reads=[r_kvsb, r_rk8], writes=[r_kf])
        S.op("dve", lambda e: e.tensor_tensor(out=kf[:, :, 128:192], in0=kr.unsqueeze(1).to_broadcast([128, 8, 64]),
                                              in1=rk8.unsqueeze(2).to_broadcast([128, 8, 64]), op=ALU.mult),
             reads=[r_kr, r_rk8, r_kf], writes=[r_kf])
        S.op("pool", lambda e: e.tensor_tensor(out=kf, in0=kf, in1=gk.unsqueeze(1).to_broadcast([128, 8, 192]), op=ALU.mult),
             reads=[r_kf, r_gk], writes=[r_kf])
        if is_x:
            _rope(k, kf[:, :, 128:192], r_kf, cs_t[b][0][:, 0:64], cs_t[b][0][:, 64:128], cs_t[b][1], rt1, rt2, r_rt1, r_rt2)
        S.op("act", lambda e: e.activation(out=kfb, in_=kf, func=AF.Copy), reads=[r_kf], writes=[r_kfb])
        kTn_t, r_kTn = kTn[0]
        kTr_t, r_kTr = kTr[0]
        pv, r_pv = transposes(lambda j: kfb[:, j, 0:128], r_kfb, 8)
        S.op("dve", lambda e, pv=pv, kTn_t=kTn_t: e.tensor_copy(out=kTn_t, in_=pv), reads=[r_pv], writes=[r_kTn])
        pv, r_pv = transposes(lambda j: kfb[:, j, 128:192], r_kfb, 8, width=64)
        S.op("act", lambda e, pv=pv, kTr_t=kTr_t: e.activation(out=kTr_t, in_=pv, func=AF.Copy), reads=[r_pv], writes=[r_kTr])
        k.store(kmT_s[:, 0:128, tok].rearrange("h d t -> d h t"), r_kmT_s, kTn_t, r_kTn)
        k.store(kmT_s[:, 128:192, tok].rearrange("h d t -> d h t"), r_kmT_s, kTr_t, r_kTr)
        if not is_x:
            continue
        xtok = slice(xi * 128, (xi + 1) * 128)
        pv, r_pv = transposes(lambda j: cqn[:, j * 128:(j + 1) * 128], r_cqn, 3)
        S.op("dve", lambda e, pv=pv: e.tensor_tensor(out=cqnT, in0=pv, in1=gqaT.unsqueeze(2).to_broadcast([128, 3, 128]), op=ALU.mult),
             reads=[r_pv, r_gqaT], writes=[r_cqnT])
        qflat = qsb.rearrange("p h d -> p (h d)")
        for b3 in range(3):
            pb, r_pb = _nb(k)

            def mmq(e, pb=pb, b3=b3):
                for kc in range(3):
                    ins_ = e.matmul(pb, lhsT=cqnT[:, kc, :], rhs=wuq[:, kc, b3 * 512:(b3 + 1) * 512], start=(kc == 0), stop=(kc == 2))
                return ins_
            S.op("pe", mmq, reads=[r_cqnT, r_wuq], writes=[r_pb])
            if b3 % 2 == 0:
                S.op("act", lambda e, pb=pb, b3=b3: e.activation(out=qflat[:, b3 * 512:(b3 + 1) * 512], in_=pb, func=AF.Copy), reads=[r_pb], writes=[r_qsb])
            else:
                S.op("dve", lambda e, pb=pb, b3=b3: e.tensor_copy(out=qflat[:, b3 * 512:(b3 + 1) * 512], in_=pb), reads=[r_pb], writes=[r_qsb])
        S.op("pool", lambda e: e.tensor_tensor(out=sq, in0=qsb, in1=qsb, op=ALU.mult), reads=[r_qsb], writes=[r_sq])
        S.op("dve", lambda e: e.tensor_reduce(out=r8, in_=sq, axis=AX.X, op=ALU.add), reads=[r_sq], writes=[r_r8])
        _rstd(k, r8, r_r8, 192, r8, r_r8)
        S.op("dve", lambda e: e.tensor_tensor(out=qsb, in0=qsb, in1=r8.unsqueeze(2).to_broadcast([128, 8, 192]), op=ALU.mult),
             reads=[r_qsb, r_r8], writes=[r_qsb])
        S.op("pool", lambda e: e.tensor_tensor(out=qsb, in0=qsb, in1=gq.unsqueeze(1).to_broadcast([128, 8, 192]), op=ALU.mult),
             reads=[r_qsb, r_gq], writes=[r_qsb])
        _rope(k, qsb[:, :, 128:192], r_qsb, cs_t[b][0][:, 0:64], cs_t[b][0][:, 64:128], cs_t[b][1], rt1, rt2, r_rt1, r_rt2)
        S.op("act", lambda e: e.activation(out=qf, in_=qsb, func=AF.Copy), reads=[r_qsb], writes=[r_qf])
        qTn_t, r_qTn = qTn[0]
        qTr_t, r_qTr = qTr[0]
        pv, r_pv = transposes(lambda j: qf[:, j, 0:128], r_qf, 8)
        S.op("dve", lambda e, pv=pv, qTn_t=qTn_t: e.tensor_copy(out=qTn_t, in_=pv), reads=[r_pv], writes=[r_qTn])
        pv, r_pv = transposes(lambda j: qf[:, j, 128:192], r_qf, 8, width=64)
        S.op("act", lambda e, pv=pv, qTr_t=qTr_t: e.activation(out=qTr_t, in_=pv, func=AF.Copy), reads=[r_pv], writes=[r_qTr])
        k.store(qmT_s[:, 0:128, xtok].rearrange("h d t -> d h t"), r_qmT_s, qTn_t, r_qTn)
        k.store(qmT_s[:, 128:192, xtok].rearrange("h d t -> d h t"), r_qmT_s, qTr_t, r_qTr)
    S.barrier()
    A.release()

RW = 4364
NU = 4356


def phase_a2(k):
    S, A, nc, ins = k.S, k.A, k.nc, k.ins
    qdT_s, r_qdT_s = k.scratch("qdT_s", [H, 128, TX], BF16)
    kdT_s, r_kdT_s = k.scratch("kdT_s", [H, 128, T], BF16)
    kd_s, r_kd_s = k.scratch("kd_s", [T, H, 128], BF16)
    vd_s, r_vd_s = k.scratch("vd_s", [T, H, 128], BF16)
    sgT_s, r_sgT_s = k.scratch("sgT_s", [16, 128, TX], BF16)
    A.mark()
    convw, r_convw = k.tile([128, 24, 5], F32, "convw")
    k.load(convw, r_convw, ins["convT"])
    wc = [k.tile([128, 8, 512], BF16, "wc") for _ in range(2)]
    R = [k.tile([128, RW], BF16, "R") for _ in range(2)]
    accs = [k.tile([128, NU], F32, "acc") for _ in range(2)]
    sq, r_sq = k.tile([128, NU], BF16, "sq")
    Yb = [k.tile([128, NU], BF16, "Yb") for _ in range(2)]
    rn, r_rn = k.tile([128, 512], F32, "rn")
    tm = [k.tile([128, NT, 128], BF16, "tm") for _ in range(1)]
    sg = [k.tile([128, 512], BF16, "sg") for _ in range(2)]
    for j in range(2):
        S.op("pool", lambda e, j=j: e.memset(R[j][0], 0.0), writes=[R[j][1]])
    win = ins["w_in"].rearrange("(kc p) n -> p kc n", p=128)
    blocks = [(c * 512, "qkv", c * 4) for c in range(6)] + [(OFF_GATE + c * 512, "gate", c * 4) for c in range(4)]
    tgroups = [(0, 256, 2)] + [(256 + g * 512, 512, 262 + g * 512) for g in range(8)]
    all_hT = list(k.r_hT)

    def load_block(bi):
        c0, kind, _ = blocks[bi]
        w_t, r_w = wc[bi % 2]
        for hf in range(2):
            k.wload(w_t[:, 4 * hf:4 * hf + 4, :], r_w, win[:, 4 * hf:4 * hf + 4, c0:c0 + 512])

    load_block(0)
    ci = 0
    pending = []
    for bi, (c0, kind, chunk0) in enumerate(blocks):
        if bi + 1 < len(blocks):
            load_block(bi + 1)
        w_t, r_w = wc[bi % 2]
        for sub in range(4):
            cc = chunk0 + sub
            if kind == "gate":
                while pending:
                    pending.pop(0)()
                for g in range(8):
                    pb, r_pb = _nb(k)

                    def mm(e, pb=pb, g=g, sub=sub, w_t=w_t):
                        for kc in range(8):
                            ins_ = e.matmul(pb, lhsT=w_t[:, kc, sub * 128:(sub + 1) * 128], rhs=k.hT[:, kc, 256 + g * 512:256 + (g + 1) * 512],
                                            start=(kc == 0), stop=(kc == 7))
                        return ins_
                    S.op("pe", mm, reads=[r_w] + all_hT, writes=[r_pb])
                    s_t, r_s = sg[g % 2]
                    S.op("act", lambda e, pb=pb, s_t=s_t: e.activation(out=s_t, in_=pb, func=AF.Sigmoid), reads=[r_pb], writes=[r_s])
                    k.store(sgT_s[cc, :, g * 512:(g + 1) * 512], r_sgT_s, s_t, r_s)
                continue
            R_t, r_R = R[ci % 2]
            Y_t, r_Y = Yb[ci % 2]
            acc, r_acc = accs[ci % 2]
            ci += 1
            for gi, (h0, n, ro) in enumerate(tgroups):
                pb, r_pb = _nb(k)

                def mm(e, pb=pb, h0=h0, n=n, sub=sub, w_t=w_t):
                    for kc in range(8):
                        ins_ = e.matmul(pb[:, 0:n], lhsT=w_t[:, kc, sub * 128:(sub + 1) * 128], rhs=k.hT[:, kc, h0:h0 + n],
                                        start=(kc == 0), stop=(kc == 7))
                    return ins_
                S.op("pe", mm, reads=[r_w] + all_hT, writes=[r_pb])
                if gi % 2 == 0:
                    S.op("act", lambda e, pb=pb, n=n, ro=ro, R_t=R_t: e.activation(out=R_t[:, ro:ro + n], in_=pb[:, 0:n], func=AF.Copy),
                         reads=[r_pb], writes=[r_R])
                else:
                    S.op("dve", lambda e, pb=pb, n=n, ro=ro, R_t=R_t: e.tensor_copy(out=R_t[:, ro:ro + n], in_=pb[:, 0:n]),
                         reads=[r_pb], writes=[r_R])
            ceng = "dve"

            def conv(e, R_t=R_t, cc=cc, acc=acc):
                e.tensor_scalar(out=acc, in0=R_t[:, 0:NU], scalar1=convw[:, cc, 0:1], scalar2=None, op0=ALU.mult)
                for j in range(1, 5):
                    ins_ = e.scalar_tensor_tensor(out=acc, in0=R_t[:, j:j + NU], scalar=convw[:, cc, j:j + 1], in1=acc,
                                                  op0=ALU.mult, op1=ALU.add)
                return ins_
            S.op(ceng, conv, reads=[r_R, r_convw], writes=[r_acc])
            head = cc % 8
            if cc >= 16:
                S.op("act", lambda e, Y_t=Y_t, acc=acc: e.activation(out=Y_t, in_=acc, func=AF.Silu), reads=[r_acc], writes=[r_Y])
            else:
                S.op("act", lambda e, acc=acc: e.activation(out=acc, in_=acc, func=AF.Silu), reads=[r_acc], writes=[r_acc])

            def stage2(cc=cc, head=head, Y_t=Y_t, r_Y=r_Y, acc=acc, r_acc=r_acc):
                if cc < 16:
                    S.op("pool", lambda e: e.tensor_tensor(out=sq, in0=acc, in1=acc, op=ALU.mult), reads=[r_acc], writes=[r_sq])
                    scale = (128.0 ** -0.5) if cc < 8 else 1.0
                    for g in range(9):
                        u0 = g * 512
                        n = min(512, NU - u0)
                        pb, r_pb = _nb(k)
                        S.op("pe", lambda e, pb=pb, u0=u0, n=n: e.matmul(pb[:, 0:n], lhsT=k.ones_b, rhs=sq[:, u0:u0 + n], start=True, stop=True),
                             reads=[r_sq, k.r_ones_b], writes=[r_pb])
                        S.op("act", lambda e, pb=pb, n=n, scale=scale: e.activation(out=rn[:, 0:n], in_=pb[:, 0:n], func=AF.Sqrt,
                                                                                  scale=1.0 / (scale * scale), bias=EPS / (scale * scale)),
                             reads=[r_pb], writes=[r_rn])
                        S.op("dve", lambda e, n=n: e.reciprocal(out=rn[:, 0:n], in_=rn[:, 0:n]), reads=[r_rn], writes=[r_rn])
                        S.op("dve", lambda e, u0=u0, n=n, Y_t=Y_t, acc=acc: e.tensor_tensor(out=Y_t[:, u0:u0 + n], in0=acc[:, u0:u0 + n], in1=rn[:, 0:n], op=ALU.mult),
                             reads=[r_acc, r_rn], writes=[r_Y])
                if cc < 8:
                    k.store(qdT_s[head, :, :], r_qdT_s, Y_t[:, 260:260 + TX], r_Y)
                elif cc < 16:
                    k.store(kdT_s[head, :, 0:256], r_kdT_s, Y_t[:, 0:256], r_Y)
                    k.store(kdT_s[head, :, 256:T], r_kdT_s, Y_t[:, 260:260 + TX], r_Y)
                if cc >= 8:
                    tm_t, r_tm = tm[0]
                    for tb in range(5):
                        t0 = tb * 8
                        nt = min(8, NT - t0)
                        pb, r_pb = _nb(k, BF16)
                        pv = pb.rearrange("p (a b) -> p a b", b=128)[:, 0:nt, :]

                        def tr(e, pv=pv, t0=t0, nt=nt, Y_t=Y_t):
                            for j in range(nt):
                                ti = t0 + j
                                u = ti * 128 if ti < 2 else 260 + (ti - 2) * 128
                                ins_ = e.transpose(out=pv[:, j, :], in_=Y_t[:, u:u + 128], identity=k.ident_b)
                            return ins_
                        S.op("pe", tr, reads=[r_Y, k.r_ident_b], writes=[r_pb])
                        if tb % 2 == 0:
                            S.op("act", lambda e, pv=pv, t0=t0, nt=nt, tm_t=tm_t: e.activation(out=tm_t[:, t0:t0 + nt, :], in_=pv, func=AF.Copy),
                                 reads=[r_pb], writes=[r_tm])
                        else:
                            S.op("dve", lambda e, pv=pv, t0=t0, nt=nt, tm_t=tm_t: e.tensor_copy(out=tm_t[:, t0:t0 + nt, :], in_=pv),
                                 reads=[r_pb], writes=[r_tm])
                    dst_s, r_dst = (kd_s, r_kd_s) if cc < 16 else (vd_s, r_vd_s)
                    k.store(dst_s.rearrange("(n p) h d -> p n h d", p=128)[:, :, head, :], r_dst, tm_t, r_tm)
            pending.append(stage2)
            if len(pending) > 1:
                pending.pop(0)()
    while pending:
        pending.pop(0)()
    S.barrier()
    A.release()
    A.release()

import os as _os


def _drive(*gens):
    gens = [g for g in gens if g is not None]
    while gens:
        for g in list(gens):
            try:
                next(g)
            except StopIteration:
                gens.remove(g)


def phase_b(k):
    S, A, nc, ins = k.S, k.A, k.nc, k.ins
    qdT_s, r_qdT_s = k.scr["qdT_s"]
    kdT_s, r_kdT_s = k.scr["kdT_s"]
    kd_s, r_kd_s = k.scr["kd_s"]
    vd_s, r_vd_s = k.scr["vd_s"]
    gb_s, r_gb_s = k.scr["gb_s"]
    o_s = [k.scratch("of_s", [TX, D], F32), k.scratch("ob_s", [TX, D], F32)]
    A.mark()
    msk, r_msk = k.tile([128, 9, 128], F32, "msk")
    k.load(msk, r_msk, ins["dn_masks"])
    EC, r_EC = k.tile([64, 2, 8, 128], F32, "EC")
    k.load(EC, r_EC, ins["dn_esel"])
    L1, r_L1 = k.tile([64, 128], F32, "L1")
    L2, r_L2 = k.tile([64, 128], F32, "L2")
    R1, r_R1 = k.tile([64, 8, 128], F32, "R1")
    R2, r_R2 = k.tile([64, 8, 128], F32, "R2")
    X, r_X = k.tile([128, 2, 64], F32, "X")
    k.load(L1, r_L1, ins["dn_linit"])
    k.load(L2, r_L2, ins["dn_linit"])
    S.op("dve", lambda e: e.tensor_copy(out=R1, in_=EC[:, 0, :, :]), reads=[r_EC], writes=[r_R1])
    S.op("dve", lambda e: e.tensor_copy(out=R2, in_=EC[:, 1, :, :]), reads=[r_EC], writes=[r_R2])
    S.op("pool", lambda e: e.memset(X, 0.0), writes=[r_X])
    S32 = [k.tile([128, 8, 128], F32, f"S32_{d}") for d in range(2)]
    Sbf = [k.tile([128, 8, 128], BF16, f"Sbf_{d}") for d in range(2)]
    for d in range(2):
        S.op("pool", lambda e, d=d: e.memset(S32[d][0], 0.0), writes=[S32[d][1]])
        S.op("pool", lambda e, d=d: e.memset(Sbf[d][0], 0.0), writes=[Sbf[d][1]])
    NB = 2
    gbt = [k.tile([128, 48], F32, "gbt") for _ in range(NB)]
    kT = [k.tile([128, 8, 128], BF16, "kT") for _ in range(NB)]
    qT = [k.tile([128, 8, 128], BF16, "qT") for _ in range(NB)]
    ktm = [k.tile([128, 8, 128], BF16, "ktm") for _ in range(NB)]
    vtm = [k.tile([128, 8, 128], BF16, "vtm") for _ in range(NB)]
    sm = [k.tile([128, 4, 8], F32, "sm") for _ in range(NB)]
    E1, r_E1 = k.tile([128, 8, 128], F32, "E1")
    M, r_M = k.tile([128, 8, 128], F32, "M")
    Mt, r_Mt = k.tile([128, 8, 128], BF16, "Mt")
    Md, r_Md = k.tile([128, 8, 128], BF16, "Md")
    Mo1, r_Mo1 = k.tile([128, 8, 128], BF16, "Mo1")
    Mo2, r_Mo2 = k.tile([128, 8, 128], BF16, "Mo2")
    PP = [k.tile([128, 8, 128], BF16, f"PP{j}") for j in range(2)]
    PT = [k.tile([128, 8, 128], BF16, f"PT{j}") for j in range(2)]
    Tt, r_Tt = k.tile([128, 8, 128], BF16, "Tt")
    TtB, r_TtB = k.tile([128, 8, 128], BF16, "TtB")
    Abuf, _ = k.tile([128, 8, 128], BF16, "Abuf")
    E1r, Mdr, Mo1r, Mo2r, Mtr, Ttr = (t_ for t_ in (Abuf, Md, Mo1, Mo2, Mt, Tt))
    PPr = [PP[j][0] for j in range(2)]
    PTr = [PT[j][0] for j in range(2)]
    kg, r_kg = k.tile([128, 8, 128], BF16, "kg")
    kdec = [k.tile([128, 8, 128], BF16, "kdec") for _ in range(NB)]
    wT = [k.tile([128, 8, 128], BF16, "wT") for _ in range(NB)]
    u = [k.tile([128, 8, 128], F32, "u") for _ in range(NB)]
    qkT = [k.tile([128, 8, 128], BF16, "qkT") for _ in range(NB)]
    vnew, r_vnew = k.tile([128, 8, 128], BF16, "vnew")
    tmpo, r_tmpo = k.tile([128, 8, 128], F32, "tmpo")
    o_t = [k.tile([128, 8, 128], F32, "o") for _ in range(NB)]

    RG = {nm: [Res(nm + "0"), Res(nm + "1")] for nm in ("Ab", "E1", "M", "Md", "Mo1", "Mo2", "Mt", "Tt", "TtB", "PP0", "PP1", "PT0", "PT1")}
    wT_res = [[Res("wT"), Res("wT")] for _ in range(NB)]
    u_res = [[Res("u"), Res("u")] for _ in range(NB)]
    qk_res = [[Res("qk"), Res("qk")] for _ in range(NB)]
    pre_i = [0]

    def nbp(dtype=F32):
        b = pre_i[0]
        pre_i[0] = (b + 1) % 4
        return k.bank(b, dtype)

    def b4(pb):
        return pb.rearrange("p (a b) -> p a b", b=128)

    def b4h(pb):
        return pb.rearrange("p (a b) -> p a b", b=128)[:, 0:4, :]

    units = []
    fwd = list(range(NT))
    bwd = [1, 0] + list(range(NT - 1, 1, -1))
    for s in range(NT):
        units.append((0, fwd[s]))
        units.append((1, bwd[s]))

    def loads(ui):
        d, ti = units[ui]
        b = ui % NB
        tok = slice(ti * 128, (ti + 1) * 128)
        k.load(gbt[b][0], gbt[b][1], gb_s[tok, :], r_gb_s)
        k.load(kT[b][0], kT[b][1], kdT_s[:, :, tok].rearrange("h d t -> d h t"), r_kdT_s)
        k.load(ktm[b][0], ktm[b][1], kd_s[tok, :, :], r_kd_s, q="act")
        k.load(vtm[b][0], vtm[b][1], vd_s[tok, :, :], r_vd_s, q="act")
        if ti >= 2:
            xt_ = slice((ti - 2) * 128, (ti - 1) * 128)
            k.load(qT[b][0], qT[b][1], qdT_s[:, :, xt_].rearrange("h d t -> d h t"), r_qdT_s)

    def pre(ui):
        d, ti = units[ui]
        b = ui % NB
        is_x = ti >= 2
        g_t, r_g = gbt[b]
        kT_t, r_kT = kT[b]
        qT_t, r_qT = qT[b]
        ktm_t, r_ktm = ktm[b]
        vtm_t, r_vtm = vtm[b]
        sm_t, r_sm = sm[b]
        gcol = g_t[:, d * 8:(d + 1) * 8]
        beta = g_t[:, 16 + d * 8:16 + (d + 1) * 8]
        lnb = g_t[:, 32 + d * 8:32 + (d + 1) * 8]
        pb, r_pb = nbp()

        def mm0(e):
            e.matmul(pb[:, 0:8], lhsT=msk[:, d, :], rhs=gcol, start=True, stop=True)
            return e.matmul(pb[:, 8:16], lhsT=k.ones_f, rhs=gcol, start=True, stop=True)
        S.op("pe", mm0, reads=[r_msk, r_g, k.r_ones_f], writes=[r_pb])
        S.op("dve", lambda e: e.tensor_copy(out=X[:, :, 32:40], in_=pb[:, 0:8].unsqueeze(1).to_broadcast([128, 2, 8])), reads=[r_pb], writes=[r_X])
        S.op("dve", lambda e: e.tensor_copy(out=X[:, 1, 0:8], in_=pb[:, 0:8]), reads=[r_pb], writes=[r_X])
        S.op("dve", lambda e: e.tensor_tensor(out=X[:, 0, 0:8], in0=pb[:, 0:8], in1=lnb, op=ALU.add), reads=[r_pb, r_g], writes=[r_X])
        S.op("act", lambda e: e.activation(out=sm_t[:, 0, :], in_=pb[:, 0:8], func=AF.Exp), reads=[r_pb], writes=[r_sm])
        S.op("act", lambda e: e.activation(out=sm_t[:, 1, :], in_=pb[:, 8:16], func=AF.Exp), reads=[r_pb], writes=[r_sm])
        S.op("dve", lambda e: e.tensor_tensor(out=sm_t[:, 3, :], in0=pb[:, 8:16], in1=X[:, 1, 0:8], op=ALU.subtract), reads=[r_pb, r_X], writes=[r_sm])
        S.op("act", lambda e: e.activation(out=sm_t[:, 2, :], in_=sm_t[:, 3, :], func=AF.Exp), reads=[r_sm], writes=[r_sm])
        pt, r_pt = nbp()

        def tr0(e):
            e.transpose(out=pt[0:64, 0:128], in_=X[:, 0, :], identity=k.ident_f)
            return e.transpose(out=pt[0:64, 128:256], in_=X[:, 1, :], identity=k.ident_f)
        S.op("pe", tr0, reads=[r_X, k.r_ident_f], writes=[r_pt])
        S.op("act", lambda e: e.activation(out=L1[0:8, :], in_=pt[0:8, 0:128], func=AF.Copy), reads=[r_pt], writes=[r_L1])
        S.op("act", lambda e: e.activation(out=L2[0:8, :], in_=pt[0:8, 128:256], func=AF.Copy), reads=[r_pt], writes=[r_L2])
        S.op("dve", lambda e: e.tensor_tensor(out=R1[32:40, :, :], in0=EC[32:40, 1, :, :], in1=pt[32:40, 0:128].unsqueeze(1).to_broadcast([8, 8, 128]), op=ALU.mult),
             reads=[r_pt, r_EC], writes=[r_R1])
        S.op("dve", lambda e: e.tensor_tensor(out=R2[32:40, :, :], in0=EC[32:40, 0, :, :], in1=pt[32:40, 128:256].unsqueeze(1).to_broadcast([8, 8, 128]), op=ALU.mult),
             reads=[r_pt, r_EC], writes=[r_R2])
        kd_t, r_kd = kdec[b]
        S.op("pool", lambda e: e.tensor_tensor(out=kg, in0=ktm_t, in1=sm_t[:, 0, :].unsqueeze(2).to_broadcast([128, 8, 128]), op=ALU.mult),
             reads=[r_ktm, r_sm], writes=[r_kg])
        S.op("pool", lambda e: e.tensor_tensor(out=kd_t, in0=ktm_t, in1=sm_t[:, 2, :].unsqueeze(2).to_broadcast([128, 8, 128]), op=ALU.mult),
             reads=[r_ktm, r_sm], writes=[r_kd])
        yield
        def do_group(grp):
            hs = range(4 * grp, 4 * grp + 4)
            gs = slice(4 * grp, 4 * grp + 4)
            r_E1, r_M, r_Md, r_Mo1, r_Mo2, r_Mt, r_Tt, r_TtB = (RG[nm][grp] for nm in ("E1", "M", "Md", "Mo1", "Mo2", "Mt", "Tt", "TtB"))
            r_Ab = RG["Ab"][grp]
            rPP = [RG["PP0"][grp], RG["PP1"][grp]]
            rPT = [RG["PT0"][grp], RG["PT1"][grp]]
            Mo_list = ((Mo1r, r_Mo1), (Mo2r, r_Mo2))
            Md_list = ((Mdr, r_Md, 6), (Mo1r, r_Mo1, 7), (Mo2r, r_Mo2, 8))
            pk, r_pk = nbp()
            pd, r_pd = nbp()

            def mmk(e):
                for hh, h in enumerate(hs):
                    ins_ = e.matmul(b4(pk)[:, hh, :], lhsT=kT_t[:, h, :], rhs=kT_t[:, h, :], start=True, stop=True)
                return ins_
            S.op("pe", mmk, reads=[r_kT], writes=[r_pk])

            def mmd(e):
                for hh, h in enumerate(hs):
                    ins_ = e.matmul(b4(pd)[:, hh, :], lhsT=L1, rhs=R1[:, h, :], start=True, stop=True)
                return ins_
            S.op("pe", mmd, reads=[r_L1, r_R1], writes=[r_pd])
            S.op("dve", lambda e: e.scalar_tensor_tensor(out=E1[:, gs, :], in0=b4(pd), scalar=0.0, in1=msk[:, 2 + d, :].unsqueeze(1).to_broadcast([128, 4, 128]),
                                                         op0=ALU.min, op1=ALU.add), reads=[r_pd, r_msk], writes=[r_E1])
            S.op("act", lambda e: e.activation(out=E1[:, gs, :], in_=E1[:, gs, :], func=AF.Exp), reads=[r_E1], writes=[r_E1])
            S.op("dve", lambda e: e.tensor_tensor(out=M[:, gs, :], in0=b4(pk), in1=E1[:, gs, :], op=ALU.mult), reads=[r_pk, r_E1], writes=[r_M])
            for (dst_, rdst_, mi_) in Md_list:
                S.op("pool", lambda e, dst_=dst_, mi_=mi_: e.tensor_tensor(out=dst_[:, gs, :], in0=M[:, gs, :],
                                                                        in1=msk[:, mi_, :].unsqueeze(1).to_broadcast([128, 4, 128]), op=ALU.mult),
                     reads=[r_M, r_msk], writes=[rdst_])
            pm, r_pm = nbp(BF16)

            def trm(e):
                for hh, h in enumerate(hs):
                    ins_ = e.transpose(out=b4h(pm)[:, hh, :], in_=Md[:, h, :], identity=k.ident_b)
                return ins_
            S.op("pe", trm, reads=[r_Md, k.r_ident_b], writes=[r_pm])
            S.op("act", lambda e: e.activation(out=Mtr[:, gs, :], in_=b4h(pm), func=AF.Copy), reads=[r_pm], writes=[r_Mt])
            S.op("dve", lambda e: e.scalar_tensor_tensor(out=Ttr[:, gs, :], in0=b4h(pm), scalar=-1.0, in1=k.ident_f.unsqueeze(1).to_broadcast([128, 4, 128]),
                                                         op0=ALU.mult, op1=ALU.add), reads=[r_pm, k.r_ident_f], writes=[r_Tt])
            yield
            P_prev, rP_prev, Pt_prev, rPt_prev = Mdr, r_Md, Mtr, r_Mt
            for lvl in range(1, 1 + int(_os.environ.get('B_LEVELS', 4))):
                P_new, rP_new = PPr[lvl % 2], rPP[lvl % 2]
                Pt_new, rPt_new = PTr[lvl % 2], rPT[lvl % 2]
                pa, r_pa = nbp()

                def mma(e, P_prev=P_prev, Pt_prev=Pt_prev, pa=pa):
                    for hh, h in enumerate(hs):
                        ins_ = e.matmul(b4(pa)[:, hh, :], lhsT=Pt_prev[:, h, :], rhs=P_prev[:, h, :], start=True, stop=True)
                    return ins_
                S.op("pe", mma, reads=[rP_prev, rPt_prev], writes=[r_pa])
                S.op("act", lambda e, P_new=P_new, pa=pa: e.activation(out=P_new[:, gs, :], in_=b4(pa), func=AF.Copy), reads=[r_pa], writes=[rP_new])
                if lvl < int(_os.environ.get('B_LEVELS', 4)):
                    pbk, r_pbk = nbp()

                    def mmb(e, P_prev=P_prev, Pt_prev=Pt_prev, pbk=pbk):
                        for hh, h in enumerate(hs):
                            ins_ = e.matmul(b4(pbk)[:, hh, :], lhsT=P_prev[:, h, :], rhs=Pt_prev[:, h, :], start=True, stop=True)
                        return ins_
                    S.op("pe", mmb, reads=[rP_prev, rPt_prev], writes=[r_pbk])
                    S.op("act", lambda e, Pt_new=Pt_new, pbk=pbk: e.activation(out=Pt_new[:, gs, :], in_=b4(pbk), func=AF.Copy), reads=[r_pbk], writes=[rPt_new])
                pc, r_pc = nbp()

                def mmc(e, P_new=P_new, pc=pc):
                    for hh, h in enumerate(hs):
                        ins_ = e.matmul(b4(pc)[:, hh, :], lhsT=P_new[:, h, :], rhs=Ttr[:, h, :], start=True, stop=True)
                    return ins_
                S.op("pe", mmc, reads=[rP_new, r_Tt], writes=[r_pc])
                S.op("dve", lambda e, pc=pc: e.tensor_tensor(out=Ttr[:, gs, :], in0=Tt[:, gs, :], in1=b4(pc), op=ALU.add), reads=[r_Tt, r_pc], writes=[r_Tt])
                P_prev, rP_prev, Pt_prev, rPt_prev = P_new, rP_new, Pt_new, rPt_new
                yield
            for (Mo_, rMo_) in Mo_list:
                ptd, r_ptd = nbp(BF16)

                def trt(e, ptd=ptd):
                    for hh, h in enumerate(hs):
                        ins_ = e.transpose(out=b4h(ptd)[:, hh, :], in_=Tt[:, h, :], identity=k.ident_b)
                    return ins_
                S.op("pe", trt, reads=[r_Tt, k.r_ident_b], writes=[r_ptd])
                S.op("act", lambda e, ptd=ptd: e.activation(out=Mtr[:, gs, :], in_=b4h(ptd), func=AF.Copy), reads=[r_ptd], writes=[r_Mt])
                pa2, r_pa2 = nbp()

                def mma2(e, pa2=pa2, Mo_=Mo_):
                    for hh, h in enumerate(hs):
                        ins_ = e.matmul(b4(pa2)[:, hh, :], lhsT=Mo_[:, h, :], rhs=Ttr[:, h, :], start=True, stop=True)
                    return ins_
                S.op("pe", mma2, reads=[rMo_, r_Tt], writes=[r_pa2])
                S.op("act", lambda e, pa2=pa2: e.activation(out=E1r[:, gs, :], in_=b4(pa2), func=AF.Copy), reads=[r_pa2], writes=[r_Ab])
                pc2, r_pc2 = nbp()

                def mmc2(e, pc2=pc2):
                    for hh, h in enumerate(hs):
                        ins_ = e.matmul(b4(pc2)[:, hh, :], lhsT=Mtr[:, h, :], rhs=E1r[:, h, :], start=True, stop=True)
                    return ins_
                S.op("pe", mmc2, reads=[r_Mt, r_Ab], writes=[r_pc2])
                S.op("dve", lambda e, pc2=pc2: e.tensor_tensor(out=Ttr[:, gs, :], in0=Tt[:, gs, :], in1=b4(pc2), op=ALU.subtract), reads=[r_Tt, r_pc2], writes=[r_Tt])
                yield
            S.op("pool", lambda e: e.tensor_tensor(out=TtB[:, gs, :], in0=Tt[:, gs, :], in1=beta[:, gs].unsqueeze(2).to_broadcast([128, 4, 128]), op=ALU.mult),
                 reads=[r_Tt, r_g], writes=[r_TtB])
            pw, r_pw = nbp()

            def mmw(e):
                for hh, h in enumerate(hs):
                    ins_ = e.matmul(b4(pw)[:, hh, :], lhsT=kg[:, h, :], rhs=TtB[:, h, :], start=True, stop=True)
                return ins_
            S.op("pe", mmw, reads=[r_kg, r_TtB], writes=[r_pw])
            S.op("act", lambda e: e.activation(out=wT[b][0][:, gs, :], in_=b4(pw), func=AF.Copy), reads=[r_pw], writes=[wT_res[b][grp]])
            pu, r_pu = nbp()

            def mmu(e):
                for hh, h in enumerate(hs):
                    ins_ = e.matmul(b4(pu)[:, hh, :], lhsT=TtB[:, h, :], rhs=vtm_t[:, h, :], start=True, stop=True)
                return ins_
            S.op("pe", mmu, reads=[r_TtB, r_vtm], writes=[r_pu])
            S.op("act", lambda e: e.activation(out=u[b][0][:, gs, :], in_=b4(pu), func=AF.Copy), reads=[r_pu], writes=[u_res[b][grp]])
            yield
            if is_x:
                pq, r_pq = nbp()
                pd2, r_pd2 = nbp()

                def mmq(e):
                    for hh, h in enumerate(hs):
                        ins_ = e.matmul(b4(pq)[:, hh, :], lhsT=kT_t[:, h, :], rhs=qT_t[:, h, :], start=True, stop=True)
                    return ins_
                S.op("pe", mmq, reads=[r_kT, r_qT], writes=[r_pq])

                def mmd2(e):
                    for hh, h in enumerate(hs):
                        ins_ = e.matmul(b4(pd2)[:, hh, :], lhsT=L2, rhs=R2[:, h, :], start=True, stop=True)
                    return ins_
                S.op("pe", mmd2, reads=[r_L2, r_R2], writes=[r_pd2])
                S.op("dve", lambda e: e.scalar_tensor_tensor(out=E1[:, gs, :], in0=b4(pd2), scalar=0.0, in1=msk[:, 4 + d, :].unsqueeze(1).to_broadcast([128, 4, 128]),
                                                             op0=ALU.min, op1=ALU.add), reads=[r_pd2, r_msk], writes=[r_E1])
                S.op("act", lambda e: e.activation(out=E1[:, gs, :], in_=E1[:, gs, :], func=AF.Exp), reads=[r_E1], writes=[r_E1])
                S.op("dve", lambda e: e.tensor_tensor(out=qkT[b][0][:, gs, :], in0=b4(pq), in1=E1[:, gs, :], op=ALU.mult), reads=[r_pq, r_E1], writes=[qk_res[b][grp]])
                yield
        gens_ = [do_group(0), do_group(1)]
        while gens_:
            for g_ in list(gens_):
                try:
                    next(g_)
                    yield
                except StopIteration:
                    gens_.remove(g_)

    def seq(ui):
        d, ti = units[ui]
        b = ui % NB
        is_x = ti >= 2
        S32_t, r_S32 = S32[d]
        Sbf_t, r_Sbf = Sbf[d]
        sm_t, r_sm = sm[b]
        wT_t, u_t, qk_t = wT[b][0], u[b][0], qkT[b][0]
        qT_t, r_qT = qT[b]
        kd_t, r_kd = kdec[b]
        banks = [k.bank(4 + j) for j in range(4)]

        def grp_mm(bank2, lhs_fn, rhs_fn, reads):
            for grp in range(2):
                pb, r_pb = bank2[grp]

                def f(e, grp=grp, pb=pb):
                    for hh in range(4):
                        h = 4 * grp + hh
                        ins_ = e.matmul(b4(pb)[:, hh, :], lhsT=lhs_fn(h), rhs=rhs_fn(h), start=True, stop=True)
                    return ins_
                S.op("pe", f, reads=reads, writes=[r_pb])
        grp_mm(banks[0:2], lambda h: wT_t[:, h, :], lambda h: Sbf_t[:, h, :], wT_res[b] + [r_Sbf])
        S.op("pool", lambda e: e.tensor_tensor(out=S32_t, in0=S32_t, in1=sm_t[:, 1, :].unsqueeze(2).to_broadcast([128, 8, 128]), op=ALU.mult),
             reads=[r_S32, r_sm], writes=[r_S32])
        yield
        for grp in range(2):
            gs = slice(4 * grp, 4 * grp + 4)
            pb, r_pb = banks[grp]
            S.op("dve", lambda e, pb=pb, gs=gs: e.tensor_tensor(out=vnew[:, gs, :], in0=u_t[:, gs, :], in1=b4(pb), op=ALU.subtract),
                 reads=u_res[b] + [r_pb], writes=[r_vnew])
        yield
        if is_x:
            grp_mm(banks[2:4], lambda h: qT_t[:, h, :], lambda h: Sbf_t[:, h, :], [r_qT, r_Sbf])
            grp_mm(banks[0:2], lambda h: qk_t[:, h, :], lambda h: vnew[:, h, :], qk_res[b] + [r_vnew])
            yield
            o_tt, r_o = o_t[b]
            for grp in range(2):
                gs = slice(4 * grp, 4 * grp + 4)
                pbq, r_pbq = banks[2 + grp]
                pbc, r_pbc = banks[grp]
                S.op("dve", lambda e, pbq=pbq, gs=gs: e.tensor_tensor(out=tmpo[:, gs, :], in0=b4(pbq), in1=sm_t[:, 0, gs].unsqueeze(2).to_broadcast([128, 4, 128]), op=ALU.mult),
                     reads=[r_pbq, r_sm], writes=[r_tmpo])
                S.op("dve", lambda e, pbc=pbc, gs=gs, o_tt=o_tt: e.tensor_tensor(out=o_tt[:, gs, :], in0=tmpo[:, gs, :], in1=b4(pbc), op=ALU.add),
                     reads=[r_tmpo, r_pbc], writes=[r_o])
            xi = ti - 2
            k.store(o_s[d][0][xi * 128:(xi + 1) * 128, :], o_s[d][1], o_tt.rearrange("p h d -> p (h d)"), r_o)
            yield
        grp_mm(banks[2:4], lambda h: kd_t[:, h, :], lambda h: vnew[:, h, :], [r_kd, r_vnew])
        yield
        for grp in range(2):
            gs = slice(4 * grp, 4 * grp + 4)
            pb, r_pb = banks[2 + grp]
            S.op("dve", lambda e, pb=pb, gs=gs: e.tensor_tensor(out=S32_t[:, gs, :], in0=S32_t[:, gs, :], in1=b4(pb), op=ALU.add),
                 reads=[r_S32, r_pb], writes=[r_S32])
        S.op("act", lambda e: e.activation(out=Sbf_t, in_=S32_t, func=AF.Copy), reads=[r_S32], writes=[r_Sbf])
        yield

    import os as _os
    stop_after = int(_os.environ.get("B_UNITS", len(units)))
    pre_cut = int(_os.environ.get("B_PRE_CUT", 10000))
    do_seq = int(_os.environ.get("B_SEQ", 1))
    _pre = pre
    _seq = seq

    def pre(ui):
        for n_, _ in enumerate(_pre(ui)):
            if n_ + 1 >= pre_cut:
                return
            yield

    def seq(ui):
        if not do_seq:
            return
        yield from _seq(ui)
    loads(0)
    _drive(pre(0))
    for ui in range(stop_after):
        if ui + 1 < stop_after:
            loads(ui + 1)
            _drive(pre(ui + 1), seq(ui))
        else:
            _drive(seq(ui))
    k.b_state = (S32, Sbf)
    S.barrier()
    A.release()

def phase_c(k):
    S, A, nc, ins = k.S, k.A, k.nc, k.ins
    of_s, r_of_s = k.scr["of_s"]
    ob_s, r_ob_s = k.scr["ob_s"]
    zs_s, r_zs_s = k.scr["zs_s"]
    qmT_s, r_qmT_s = k.scr["qmT_s"]
    kmT_s, r_kmT_s = k.scr["kmT_s"]
    vm_s, r_vm_s = k.scr["vm_s"]
    yaT_s, r_yaT_s = k.scratch("yaT_s", [H, 128, TX], BF16)
    ybT_s, r_ybT_s = k.scratch("ybT_s", [H, 128, TX], BF16)
    A.mark()
    dng, r_dng = k.tile([128, 128], F32, "dng")
    k.load(dng, r_dng, ins["dng_rep"])
    NB = 2
    of_t = [k.tile([128, 8, 128], F32, "of") for _ in range(NB)]
    ob_t = [k.tile([128, 8, 128], F32, "ob") for _ in range(NB)]
    z_t = [k.tile([128, 8, 128], BF16, "z") for _ in range(NB)]
    sq, r_sq = k.tile([128, 8, 128], F32, "sq")
    ss8, r_ss8 = k.tile([128, 8], F32, "ss8")
    yb, r_yb = k.tile([128, 8, 128], BF16, "yb")
    yT = [k.tile([128, 8, 128], BF16, "yT") for _ in range(NB)]

    def c1_loads(xi):
        b = xi % NB
        rows = slice(xi * 128, (xi + 1) * 128)
        k.load(of_t[b][0], of_t[b][1], of_s[rows, :].rearrange("p (h d) -> p h d", h=8), r_of_s)
        k.load(ob_t[b][0], ob_t[b][1], ob_s[rows, :].rearrange("p (h d) -> p h d", h=8), r_ob_s, q="act")
        k.load(z_t[b][0], z_t[b][1], zs_s[rows, :].rearrange("p (h d) -> p h d", h=8), r_zs_s)

    c1_loads(0)
    for xi in range(32):
        b = xi % NB
        if xi + 1 < 32:
            c1_loads(xi + 1)
        o_, r_o = of_t[b]
        ob_, r_ob = ob_t[b]
        z_, r_z = z_t[b]
        S.op("dve", lambda e, o_=o_, ob_=ob_: e.tensor_tensor(out=o_, in0=o_, in1=ob_, op=ALU.add), reads=[r_o, r_ob], writes=[r_o])
        S.op("pool", lambda e, o_=o_: e.tensor_tensor(out=sq, in0=o_, in1=o_, op=ALU.mult), reads=[r_o], writes=[r_sq])
        S.op("dve", lambda e: e.tensor_reduce(out=ss8, in_=sq, axis=AX.X, op=ALU.add), reads=[r_sq], writes=[r_ss8])
        _rstd(k, ss8, r_ss8, 128, ss8, r_ss8)
        S.op("dve", lambda e, o_=o_: e.tensor_tensor(out=o_, in0=o_, in1=ss8.unsqueeze(2).to_broadcast([128, 8, 128]), op=ALU.mult),
             reads=[r_o, r_ss8], writes=[r_o])
        S.op("pool", lambda e, o_=o_: e.tensor_tensor(out=o_, in0=o_, in1=dng.unsqueeze(1).to_broadcast([128, 8, 128]), op=ALU.mult),
             reads=[r_o, r_dng], writes=[r_o])
        S.op("pool", lambda e, o_=o_, z_=z_: e.tensor_tensor(out=yb, in0=o_, in1=z_, op=ALU.mult), reads=[r_o, r_z], writes=[r_yb])
        pb, r_pb = _nb(k, BF16)
        pv = pb.rearrange("p (a b) -> p a b", b=128)

        def tr(e, pv=pv):
            for j in range(8):
                ins_ = e.transpose(out=pv[:, j, :], in_=yb[:, j, :], identity=k.ident_b)
            return ins_
        S.op("pe", tr, reads=[r_yb, k.r_ident_b], writes=[r_pb])
        yT_t, r_yT = yT[b]
        S.op("act", lambda e, pv=pv, yT_t=yT_t: e.activation(out=yT_t, in_=pv, func=AF.Copy), reads=[r_pb], writes=[r_yT])
        k.store(yaT_s[:, :, xi * 128:(xi + 1) * 128].rearrange("h d t -> d h t"), r_yaT_s, yT_t, r_yT)
    S.barrier()
    A.release()
    A.mark()
    Kn = [k.tile([128, T], BF16, "Kn") for _ in range(2)]
    Kr = [k.tile([64, T], BF16, "Kr") for _ in range(2)]
    Vh = [k.tile([128, NT, 128], BF16, "Vh") for _ in range(2)]
    Qn = [k.tile([128, 512], BF16, "Qn") for _ in range(2)]
    Qr = [k.tile([64, 512], BF16, "Qr") for _ in range(2)]
    NP = 6
    PT = [k.tile([128, 512], BF16, "PT") for _ in range(NP)]
    rinv, r_rinv = k.tile([128, 512], F32, "rinv")
    yo = [k.tile([128, 512], BF16, "yo") for _ in range(2)]
    vmv = vm_s.rearrange("(n p) c -> p n c", p=128)

    def head_loads(h):
        b = h % 2
        k.load(Kn[b][0], Kn[b][1], kmT_s[h, 0:128, :], r_kmT_s)
        k.load(Kr[b][0], Kr[b][1], kmT_s[h, 128:192, :], r_kmT_s, q="act")
        k.load(Vh[b][0], Vh[b][1], vmv[:, :, h * 128:(h + 1) * 128], r_vm_s)

    def q_loads(h, qg):
        b = (h * 8 + qg) % 2
        k.load(Qn[b][0], Qn[b][1], qmT_s[h, 0:128, qg * 512:(qg + 1) * 512], r_qmT_s)
        k.load(Qr[b][0], Qr[b][1], qmT_s[h, 128:192, qg * 512:(qg + 1) * 512], r_qmT_s, q="act")

    head_loads(0)
    q_loads(0, 0)
    sbank = [0]
    it = 0
    for h in range(H):
        if h + 1 < H:
            head_loads(h + 1)
        Kn_t, r_Kn = Kn[h % 2]
        Kr_t, r_Kr = Kr[h % 2]
        V_t, r_V = Vh[h % 2]
        for qg in range(8):
            gi = h * 8 + qg
            nxt = gi + 1
            if nxt < H * 8:
                q_loads(nxt // 8, nxt % 8)
            Qn_t, r_Qn = Qn[gi % 2]
            Qr_t, r_Qr = Qr[gi % 2]
            po, r_po = k.bank(4 + gi % 2)
            pr, r_pr = k.bank(6 + gi % 2)
            sb = {}

            def emit_s(kt):
                bnk = sbank[0]
                sbank[0] = (bnk + 1) % 4
                ps_, r_ps = k.bank(bnk)

                def f(e, ps_=ps_, kt=kt, Kn_t=Kn_t, Kr_t=Kr_t, Qn_t=Qn_t, Qr_t=Qr_t):
                    e.matmul(ps_, lhsT=Kn_t[:, kt * 128:(kt + 1) * 128], rhs=Qn_t, start=True, stop=False)
                    return e.matmul(ps_, lhsT=Kr_t[:, kt * 128:(kt + 1) * 128], rhs=Qr_t, start=False, stop=True)
                S.op("pe", f, reads=[r_Kn, r_Kr, r_Qn, r_Qr], writes=[r_ps])
                pt_, r_pt = PT[kt % NP]
                S.op("act", lambda e, ps_=ps_, pt_=pt_: e.activation(out=pt_, in_=ps_, func=AF.Exp), reads=[r_ps], writes=[r_pt])
                sb[kt] = (pt_, r_pt)

            def emit_pv(kt):
                pt_, r_pt = sb.pop(kt)

                def f(e, pt_=pt_, kt=kt, po=po, pr=pr, V_t=V_t):
                    e.matmul(po, lhsT=V_t[:, kt, :], rhs=pt_, start=(kt == 0), stop=(kt == NT - 1))
                    return e.matmul(pr, lhsT=k.ones_b, rhs=pt_, start=(kt == 0), stop=(kt == NT - 1))
                S.op("pe", f, reads=[r_V, r_pt, k.r_ones_b], writes=[r_po, r_pr])

            LOOK = 3
            for kt in range(min(LOOK, NT)):
                emit_s(kt)
            for kt in range(NT):
                if kt + LOOK < NT:
                    emit_s(kt + LOOK)
                emit_pv(kt)
            S.op("dve", lambda e, pr=pr: e.reciprocal(out=rinv, in_=pr), reads=[r_pr], writes=[r_rinv])
            yo_t, r_yo = yo[gi % 2]
            S.op("dve", lambda e, po=po, yo_t=yo_t: e.tensor_tensor(out=yo_t, in0=po, in1=rinv, op=ALU.mult), reads=[r_po, r_rinv], writes=[r_yo])
            k.store(ybT_s[h, :, qg * 512:(qg + 1) * 512], r_ybT_s, yo_t, r_yo)
    S.barrier()
    A.release()

def phase_d(k):
    S, A, nc, ins = k.S, k.A, k.nc, k.ins
    yaT_s, r_yaT_s = k.scr["yaT_s"]
    ybT_s, r_ybT_s = k.scr["ybT_s"]
    sgT_s, r_sgT_s = k.scr["sgT_s"]
    xmid_s, r_xmid_s = k.scratch("xmid_s", [TX, D], F32)
    h2_s, r_h2_s = k.scratch("h2_s", [TX, D], BF16)
    aff_s, r_aff_s = k.scratch("aff_s", [TX, 16], F32)
    affT_s, r_affT_s = k.scratch("affT_s", [16, TX], F32)
    A.mark()
    woa, r_woa = k.tile([128, 8, D], BF16, "woa")
    wob, r_wob = k.tile([128, 8, D], BF16, "wob")
    wo, r_wo = k.tile([128, 8, D], BF16, "wo")
    rw, r_rw = k.tile([128, 8, 16], F32, "rw")
    for (dst, rdst, nm) in ((woa, r_woa, "w_out_a"), (wob, r_wob, "w_out_b"), (wo, r_wo, "w_o")):
        src = ins[nm].rearrange("(kc p) n -> p kc n", p=128)
        for hf in range(2):
            k.wload(dst[:, 4 * hf:4 * hf + 4, :], rdst, src[:, 4 * hf:4 * hf + 4, :])
    k.load(rw, r_rw, ins["router_w"].rearrange("(kc p) n -> p kc n", p=128))
    NB = 2
    yaT = [k.tile([128, 8, 512], BF16, "yaT") for _ in range(NB)]
    ybT = [k.tile([128, 8, 512], BF16, "ybT") for _ in range(NB)]
    gA = [k.tile([128, 8, 512], BF16, "gA") for _ in range(NB)]
    gB = [k.tile([128, 8, 512], BF16, "gB") for _ in range(NB)]
    mg, r_mg = k.tile([128, 8, 512], BF16, "mg")
    t1 = [k.tile([128, 512], F32, "t1") for _ in range(2)]
    t2 = [k.tile([128, 512], F32, "t2") for _ in range(2)]
    xt = [k.tile([128, D], F32, "xt") for _ in range(NB)]
    xm = [k.tile([128, D], F32, "xm") for _ in range(NB)]
    junk, r_junk = k.tile([128, D], BF16, "junk")
    ssd = [k.tile([128, 8], F32, "ssd") for _ in range(NB)]
    h2f, r_h2f = k.tile([128, D], F32, "h2f")
    h2b = [k.tile([128, D], BF16, "h2b") for _ in range(NB)]
    h2T, r_h2T = k.tile([128, 8, 128], F32, "h2T")
    ex = [k.tile([128, 16], F32, "ex") for _ in range(NB)]
    affT = [k.tile([16, 128], F32, "affT") for _ in range(NB)]

    def g_loads(g):
        b = g % NB
        cols = slice(g * 512, (g + 1) * 512)
        k.load(yaT[b][0], yaT[b][1], yaT_s[:, :, cols].rearrange("h d t -> d h t"), r_yaT_s)
        k.load(ybT[b][0], ybT[b][1], ybT_s[:, :, cols].rearrange("h d t -> d h t"), r_ybT_s, q="act")
        k.load(gA[b][0], gA[b][1], sgT_s[0:8, :, cols].rearrange("h d t -> d h t"), r_sgT_s)
        k.load(gB[b][0], gB[b][1], sgT_s[8:16, :, cols].rearrange("h d t -> d h t"), r_sgT_s, q="act")

    g_loads(0)
    for g in range(8):
        b = g % NB
        if g + 1 < 8:
            g_loads(g + 1)
        ya_, r_ya = yaT[b]
        yb_, r_yb = ybT[b]
        gA_, r_gA = gA[b]
        gB_, r_gB = gB[b]
        for oc in range(8):
            pa, r_pa = _nb(k)
            pbk, r_pbk = _nb(k)

            def mma(e, pa=pa, oc=oc, ya_=ya_):
                for kc in range(8):
                    ins_ = e.matmul(pa, lhsT=woa[:, kc, oc * 128:(oc + 1) * 128], rhs=ya_[:, kc, :], start=(kc == 0), stop=(kc == 7))
                return ins_
            S.op("pe", mma, reads=[r_woa, r_ya], writes=[r_pa])

            def mmb(e, pbk=pbk, oc=oc, yb_=yb_):
                for kc in range(8):
                    ins_ = e.matmul(pbk, lhsT=wob[:, kc, oc * 128:(oc + 1) * 128], rhs=yb_[:, kc, :], start=(kc == 0), stop=(kc == 7))
                return ins_
            S.op("pe", mmb, reads=[r_wob, r_yb], writes=[r_pbk])
            t1_, r_t1 = t1[oc % 2]
            t2_, r_t2 = t2[oc % 2]
            S.op("dve", lambda e, pa=pa, oc=oc, t1_=t1_, gA_=gA_: e.tensor_tensor(out=t1_, in0=pa, in1=gA_[:, oc, :], op=ALU.mult), reads=[r_pa, r_gA], writes=[r_t1])
            S.op("dve", lambda e, pbk=pbk, oc=oc, t2_=t2_, gB_=gB_: e.tensor_tensor(out=t2_, in0=pbk, in1=gB_[:, oc, :], op=ALU.mult), reads=[r_pbk, r_gB], writes=[r_t2])
            S.op("pool", lambda e, oc=oc, t1_=t1_, t2_=t2_: e.tensor_tensor(out=mg[:, oc, :], in0=t1_, in1=t2_, op=ALU.add), reads=[r_t1, r_t2], writes=[r_mg])
        for tt in range(4):
            ti = g * 4 + tt
            tb = ti % NB
            rows = slice(ti * 128, (ti + 1) * 128)
            x_, r_x = xt[tb]
            xm_, r_xm = xm[tb]
            ss_, r_ss = ssd[tb]
            k.load(x_, r_x, ins["x"][rows, :])
            for hf in range(2):
                pm, r_pm = _nb(k)

                def mmo(e, pm=pm, hf=hf, tt=tt):
                    for kc in range(8):
                        ins_ = e.matmul(pm, lhsT=mg[:, kc, tt * 128:(tt + 1) * 128], rhs=wo[:, kc, hf * 512:(hf + 1) * 512], start=(kc == 0), stop=(kc == 7))
                    return ins_
                S.op("pe", mmo, reads=[r_mg, r_wo], writes=[r_pm])
                S.op("dve", lambda e, pm=pm, hf=hf, xm_=xm_: e.tensor_tensor(out=xm_[:, hf * 512:(hf + 1) * 512], in0=pm, in1=k.gate1_row[:, hf * 512:(hf + 1) * 512], op=ALU.mult),
                     reads=[r_pm, k.r_gate1], writes=[r_xm])
            S.op("pool", lambda e, xm_=xm_, x_=x_: e.tensor_tensor(out=xm_, in0=xm_, in1=x_, op=ALU.add), reads=[r_xm, r_x], writes=[r_xm])
            k.store(xmid_s[rows, :], r_xmid_s, xm_, r_xm)
            k.store(k.out[rows, :], k.out_res, xm_, r_xm)
            S.op("act", lambda e, xm_=xm_, ss_=ss_: e.activation(out=junk, in_=xm_, func=AF.Square, accum_out=ss_[:, 0:1]), reads=[r_xm], writes=[r_junk, r_ss])
            _rstd(k, ss_[:, 0:1], r_ss, D, ss_[:, 1:2], r_ss)
            S.op("act", lambda e, xm_=xm_, ss_=ss_: e.activation(out=h2f, in_=xm_, func=AF.Copy, scale=ss_[:, 1:2]), reads=[r_xm, r_ss], writes=[r_h2f])
            S.op("pool", lambda e: e.tensor_tensor(out=h2f, in0=h2f, in1=k.s2_row, op=ALU.mult), reads=[r_h2f, k.r_s2row], writes=[r_h2f])
            S.op("pool", lambda e: e.tensor_tensor(out=h2f, in0=h2f, in1=k.shift2_row, op=ALU.add), reads=[r_h2f, k.r_shift2], writes=[r_h2f])
            h2b_, r_h2b = h2b[tb]
            S.op("act", lambda e, h2b_=h2b_: e.activation(out=h2b_, in_=h2f, func=AF.Copy), reads=[r_h2f], writes=[r_h2b])
            k.store(h2_s[rows, :], r_h2_s, h2b_, r_h2b)
            for hf in range(2):
                pt, r_pt = _nb(k)
                pv = pt.rearrange("p (a b) -> p a b", b=128)

                def trh(e, pv=pv, hf=hf):
                    for j in range(4):
                        ins_ = e.transpose(out=pv[:, j, :], in_=h2f[:, (hf * 4 + j) * 128:(hf * 4 + j + 1) * 128], identity=k.ident_f)
                    return ins_
                S.op("pe", trh, reads=[r_h2f, k.r_ident_f], writes=[r_pt])
                if hf == 0:
                    S.op("act", lambda e, pv=pv: e.activation(out=h2T[:, 0:4, :], in_=pv, func=AF.Copy), reads=[r_pt], writes=[r_h2T])
                else:
                    S.op("dve", lambda e, pv=pv: e.tensor_copy(out=h2T[:, 4:8, :], in_=pv), reads=[r_pt], writes=[r_h2T])
            pl, r_pl = _nb(k)

            def mml(e, pl=pl):
                for kc in range(8):
                    ins_ = e.matmul(pl[:, 0:16], lhsT=h2T[:, kc, :], rhs=rw[:, kc, :], start=(kc == 0), stop=(kc == 7))
                return ins_
            S.op("pe", mml, reads=[r_h2T, r_rw], writes=[r_pl])
            ex_, r_ex = ex[tb]
            S.op("dve", lambda e, pl=pl, ss_=ss_: e.tensor_reduce(out=ss_[:, 2:3], in_=pl[:, 0:16], axis=AX.X, op=ALU.max), reads=[r_pl], writes=[r_ss])
            S.op("dve", lambda e, ss_=ss_: e.tensor_scalar(out=ss_[:, 3:4], in0=ss_[:, 2:3], scalar1=-1.0, scalar2=None, op0=ALU.mult), reads=[r_ss], writes=[r_ss])
            S.op("act", lambda e, pl=pl, ss_=ss_, ex_=ex_: e.activation(out=ex_, in_=pl[:, 0:16], func=AF.Exp, bias=ss_[:, 3:4], accum_out=ss_[:, 4:5]),
                 reads=[r_pl, r_ss], writes=[r_ex, r_ss])
            S.op("dve", lambda e, ss_=ss_: e.reciprocal(out=ss_[:, 5:6], in_=ss_[:, 4:5]), reads=[r_ss], writes=[r_ss])
            S.op("dve", lambda e, ss_=ss_, ex_=ex_: e.tensor_scalar(out=ex_, in0=ex_, scalar1=ss_[:, 5:6], scalar2=None, op0=ALU.mult), reads=[r_ex, r_ss], writes=[r_ex])
            k.store(aff_s[rows, :], r_aff_s, ex_, r_ex)
            pt2, r_pt2 = _nb(k)
            S.op("pe", lambda e, pt2=pt2, ex_=ex_: e.transpose(out=pt2[0:16, 0:128], in_=ex_, identity=k.ident_f), reads=[r_ex, k.r_ident_f], writes=[r_pt2])
            aT_, r_aT = affT[tb]
            S.op("act", lambda e, pt2=pt2, aT_=aT_: e.activation(out=aT_, in_=pt2[0:16, 0:128], func=AF.Copy), reads=[r_pt2], writes=[r_aT])
            k.store(affT_s[:, rows], r_affT_s, aT_, r_aT)
    S.barrier()
    A.release()

NE = 16
CAP = 512
FF = 1408
NFC = 11


def phase_e(k):
    S, A, nc, ins = k.S, k.A, k.nc, k.ins
    aff_s, r_aff_s = k.scr["aff_s"]
    affT_s, r_affT_s = k.scr["affT_s"]
    h2_s, r_h2_s = k.scr["h2_s"]
    xmid_s, r_xmid_s = k.scr["xmid_s"]
    posmT_s, r_posmT_s = k.scratch("posmT_s", [NE, TX], F32)
    gc_s, r_gc_s = k.scratch("gc_s", [NE, 128, 4], F32)
    idx_s, r_idx_s = k.scratch("idx_s", [NE, 128, 4], I32)
    A.off = k.off_after_gate2
    A.mark()
    cst, r_cst = k.tile([128, 1024], F32, "cst")
    k.load(cst, r_cst, ins["consts"])
    blk, r_blk = k.tile([128, 128], F32, "blk")
    k.load(blk, r_blk, ins["moe_blk"])
    sel8, r_sel8 = k.tile([128, 16], F32, "sel8")
    k.load(sel8, r_sel8, ins["moe_sel8"])
    tris, r_tris = k.tile([128, 128], BF16, "tris")
    k.wload(tris, r_tris, ins["moe_tris"])
    iota_c = cst[:, 0:512]
    A.mark()
    A8, r_A8 = k.tile([128, 512], F32, "A8")
    k.load(A8, r_A8, affT_s.rearrange("e (s t) -> (e s) t", s=8), r_affT_s)
    junk, r_junk = k.tile([128, 512], F32, "junk")
    sc, r_sc = k.tile([128, 16], F32, "sc")
    S.op("pool", lambda e: e.memset(sc, 0.0), writes=[r_sc])
    S.op("pool", lambda e: e.memset(sc[:, 1:2], 1.0), reads=[r_sc], writes=[r_sc])
    for it in range(30):
        S.op("dve", lambda e: e.tensor_tensor(out=sc[:, 2:3], in0=sc[:, 0:1], in1=sc[:, 1:2], op=ALU.add), reads=[r_sc], writes=[r_sc])
        S.op("dve", lambda e: e.tensor_scalar(out=sc[:, 2:3], in0=sc[:, 2:3], scalar1=0.5, scalar2=None, op0=ALU.mult), reads=[r_sc], writes=[r_sc])
        S.op("dve", lambda e: e.tensor_scalar(out=junk, in0=A8, scalar1=sc[:, 2:3], scalar2=0.0, op0=ALU.is_ge, op1=ALU.add, accum_out=sc[:, 3:4]),
             reads=[r_A8, r_sc], writes=[r_junk, r_sc])
        pb, r_pb = _nb(k)
        S.op("pe", lambda e, pb=pb: e.matmul(pb[:, 0:1], lhsT=blk, rhs=sc[:, 3:4], start=True, stop=True), reads=[r_blk, r_sc], writes=[r_pb])
        S.op("dve", lambda e, pb=pb: e.tensor_scalar(out=sc[:, 4:5], in0=pb[:, 0:1], scalar1=CAP - 0.5, scalar2=None, op0=ALU.is_ge), reads=[r_pb], writes=[r_sc])
        S.op("dve", lambda e: e.tensor_scalar(out=sc[:, 5:6], in0=sc[:, 4:5], scalar1=-1.0, scalar2=1.0, op0=ALU.mult, op1=ALU.add), reads=[r_sc], writes=[r_sc])
        S.op("dve", lambda e: e.tensor_tensor(out=sc[:, 6:7], in0=sc[:, 2:3], in1=sc[:, 0:1], op=ALU.subtract), reads=[r_sc], writes=[r_sc])
        S.op("dve", lambda e: e.tensor_tensor(out=sc[:, 7:8], in0=sc[:, 1:2], in1=sc[:, 2:3], op=ALU.subtract), reads=[r_sc], writes=[r_sc])
        S.op("dve", lambda e: e.scalar_tensor_tensor(out=sc[:, 0:1], in0=sc[:, 6:7], scalar=sc[:, 4:5], in1=sc[:, 0:1], op0=ALU.mult, op1=ALU.add), reads=[r_sc], writes=[r_sc])
        S.op("dve", lambda e: e.scalar_tensor_tensor(out=sc[:, 1:2], in0=sc[:, 7:8], scalar=sc[:, 4:5], in1=sc[:, 2:3], op0=ALU.mult, op1=ALU.add), reads=[r_sc], writes=[r_sc])
        S.op("dve", lambda e: e.memset(sc[:, 3:4], 0.0), reads=[r_sc], writes=[r_sc])
    thrrep, r_thrrep = k.tile([128, 128], F32, "thrrep")
    S.op("dve", lambda e: e.tensor_copy(out=thrrep, in_=sc[:, 0:1].to_broadcast([128, 128])), reads=[r_sc], writes=[r_thrrep])
    pb, r_pb = _nb(k)
    S.op("pe", lambda e, pb=pb: e.matmul(pb[:, 0:16], lhsT=thrrep, rhs=sel8, start=True, stop=True), reads=[r_thrrep, r_sel8], writes=[r_pb])
    thr_row, r_thr = k.tile([128, 16], F32, "thr_row")
    S.op("act", lambda e, pb=pb: e.activation(out=thr_row, in_=pb[:, 0:16], func=AF.Copy), reads=[r_pb], writes=[r_thr])
    import os as _os
    if _os.environ.get("E_DBG"):
        k.dump("sc", sc, r_sc, [128, 16])
        k.dump("thr_row", thr_row, r_thr, [128, 16])
        k.dump("A8", A8, r_A8, [128, 512])
        S.barrier()
        A.release()
        A.release()
        return
    aff, r_aff = k.tile([128, 32, 16], F32, "aff")
    k.load(aff, r_aff, aff_s.rearrange("(n p) e -> p n e", p=128), r_aff_s)
    maskf, r_maskf = k.tile([128, 32, 16], F32, "maskf")
    maskb, r_maskb = k.tile([128, 32, 16], BF16, "maskb")
    posm, r_posm = k.tile([128, 32, 16], F32, "posm")
    parts, r_parts = k.tile([128, 32, 16, 5], BF16, "parts")
    tokp, r_tokp = k.tile([128, 32, 2], F32, "tokp")
    k.load(tokp, r_tokp, ins["moe_tok"])
    rem, r_rem = k.tile([128, 32, 16], F32, "rem")
    S.op("dve", lambda e: e.tensor_tensor(out=maskf, in0=aff, in1=thr_row.unsqueeze(1).to_broadcast([128, 32, 16]), op=ALU.is_ge),
         reads=[r_aff, r_thr], writes=[r_maskf])
    S.op("act", lambda e: e.activation(out=maskb, in_=maskf, func=AF.Copy), reads=[r_maskf], writes=[r_maskb])
    pp, r_pp = _nb(k)
    ppv = pp.rearrange("p (n e) -> p n e", e=16)

    def mmpos(e):
        for n in range(32):
            for m in range(n):
                e.matmul(ppv[:, n, :], lhsT=k.ones_b, rhs=maskb[:, m, :], start=(m == 0), stop=False)
            ins_ = e.matmul(ppv[:, n, :], lhsT=tris, rhs=maskb[:, n, :], start=(n == 0), stop=True)
        return ins_
    S.op("pe", mmpos, reads=[r_maskb, k.r_ones_b, r_tris], writes=[r_pp])
    S.op("dve", lambda e: e.scalar_tensor_tensor(out=posm, in0=ppv, scalar=1.0, in1=maskf, op0=ALU.add, op1=ALU.mult), reads=[r_pp, r_maskf], writes=[r_posm])
    S.op("dve", lambda e: e.tensor_scalar(out=posm, in0=posm, scalar1=-1.0, scalar2=None, op0=ALU.add), reads=[r_posm], writes=[r_posm])
    S.op("act", lambda e: e.activation(out=parts[:, :, :, 0], in_=aff, func=AF.Copy), reads=[r_aff], writes=[r_parts])
    S.op("dve", lambda e: e.tensor_tensor(out=rem, in0=aff, in1=parts[:, :, :, 0], op=ALU.subtract), reads=[r_aff, r_parts], writes=[r_rem])
    S.op("act", lambda e: e.activation(out=parts[:, :, :, 1], in_=rem, func=AF.Copy), reads=[r_rem], writes=[r_parts])
    S.op("dve", lambda e: e.tensor_tensor(out=rem, in0=rem, in1=parts[:, :, :, 1], op=ALU.subtract), reads=[r_rem, r_parts], writes=[r_rem])
    S.op("act", lambda e: e.activation(out=parts[:, :, :, 2], in_=rem, func=AF.Copy), reads=[r_rem], writes=[r_parts])
    S.op("dve", lambda e: e.tensor_copy(out=parts[:, :, :, 3:5], in_=tokp.unsqueeze(2).to_broadcast([128, 32, 16, 2])), reads=[r_tokp, r_parts], writes=[r_parts])
    pmTs = [k.tile([16, 512], F32, "pmT") for _ in range(2)]
    for g in range(8):
        pt, r_pt = _nb(k)

        def trp(e, pt=pt, g=g):
            for j in range(4):
                ins_ = e.transpose(out=pt[0:16, j * 128:(j + 1) * 128], in_=posm[:, g * 4 + j, :], identity=k.ident_f)
            return ins_
        S.op("pe", trp, reads=[r_posm, k.r_ident_f], writes=[r_pt])
        pmT, r_pmT = pmTs[g % 2]
        S.op("act", lambda e, pt=pt, pmT=pmT: e.activation(out=pmT, in_=pt[0:16, :], func=AF.Copy), reads=[r_pt], writes=[r_pmT])
        k.store(posmT_s[:, g * 512:(g + 1) * 512], r_posmT_s, pmT, r_pmT)
    if _os.environ.get("E_STOP") == "e1":
        S.barrier(); A.release(); A.release(); return
    Sel = [k.tile([128, 32, CAP], BF16, "Sel") for _ in range(2)]
    Sel_res = [(Res("sel0"), Res("sel1")) for _ in range(2)]
    gcs = [k.tile([128, 4], F32, "gcs") for _ in range(2)]
    idf = [k.tile([128, 4], F32, "idf") for _ in range(2)]
    idi = [k.tile([128, 4], I32, "idi") for _ in range(2)]
    for ex in range(NE):
        Sel_t, _ = Sel[ex % 2]
        r_Sel0, r_Sel1 = Sel_res[ex % 2]
        gcs_t, r_gcs = gcs[ex % 2]
        idf_t, r_idf = idf[ex % 2]
        idi_t, r_idi = idi[ex % 2]

        def bsel(e, Sel_t=Sel_t, ex=ex, par=0):
            for n in range(par, 32, 2):
                ins_ = e.tensor_scalar(out=Sel_t[:, n, :], in0=iota_c, scalar1=posm[:, n, ex:ex + 1], scalar2=None, op0=ALU.is_equal)
            return ins_
        if _os.environ.get("E_X") != "nodve":
            S.op("dve", lambda e, f=bsel: f(e, par=0), reads=[r_cst, r_posm], writes=[r_Sel0])
        S.op("dve", lambda e, f=bsel: f(e, par=1), reads=[r_cst, r_posm], writes=[r_Sel1])
        pq, r_pq = _nb(k)

        def mmgate(e, pq=pq, Sel_t=Sel_t, ex=ex):
            for cc in range(4):
                for n in range(32):
                    ins_ = e.matmul(pq[:, cc * 8:cc * 8 + 5], lhsT=Sel_t[:, n, cc * 128:(cc + 1) * 128], rhs=parts[:, n, ex, :], start=(n == 0), stop=(n == 31))
            return ins_
        if _os.environ.get("E_X") != "nomm":
            S.op("pe", mmgate, reads=[r_Sel0, r_Sel1, r_parts], writes=[r_pq])
        pq3 = pq[:, 0:32].rearrange("p (a b) -> p a b", b=8)
        S.op("dve", lambda e, pq3=pq3, gcs_t=gcs_t: e.tensor_reduce(out=gcs_t, in_=pq3[:, :, 0:3], axis=AX.X, op=ALU.add), reads=[r_pq], writes=[r_gcs])
        S.op("dve", lambda e, pq3=pq3, idf_t=idf_t: e.tensor_scalar(out=idf_t, in0=pq3[:, :, 3], scalar1=128.0, scalar2=None, op0=ALU.mult), reads=[r_pq], writes=[r_idf])
        S.op("dve", lambda e, pq3=pq3, idf_t=idf_t: e.tensor_tensor(out=idf_t, in0=idf_t, in1=pq3[:, :, 4], op=ALU.add), reads=[r_pq, r_idf], writes=[r_idf])
        S.op("dve", lambda e, idf_t=idf_t, idi_t=idi_t: e.tensor_copy(out=idi_t, in_=idf_t), reads=[r_idf], writes=[r_idi])
        k.store(gc_s[ex], r_gc_s, gcs_t, r_gcs)
        k.store(idx_s[ex], r_idx_s, idi_t, r_idi)
    S.barrier()
    A.release()
    if _os.environ.get("E_STOP") == "ea1":
        A.release(); return
    A.mark()
    U32 = mybir.dt.uint32
    ig = S.pool("ig", 8)
    wg = [k.tile([128, 8, FF], BF16, "wg") for _ in range(2)]
    wu = [k.tile([128, 8, FF], BF16, "wu") for _ in range(2)]
    wd = [k.tile([128, NFC, D], BF16, "wd") for _ in range(1)]
    xg = [k.tile([128, 4, D], BF16, "xg") for _ in range(2)]
    idx2 = [k.tile([128, 4], I32, "idx2") for _ in range(2)]
    gc2 = [k.tile([128, 4], F32, "gc2") for _ in range(2)]
    xeT_t, r_xeT = k.tile([128, 8, CAP], BF16, "xeT")
    hid, r_hid = k.tile([128, NFC, CAP], BF16, "hid")
    sg = [k.tile([128, CAP], F32, "sg") for _ in range(2)]
    yef = [k.tile([128, D], F32, "yef") for _ in range(4)]
    r_outacc = Res("outacc")
    h2_rows = h2_s

    def w_loads(ex):
        b = ex % 2
        srcg = ins["w_gate"][ex].rearrange("(kc p) f -> p kc f", p=128)
        srcu = ins["w_up"][ex].rearrange("(kc p) f -> p kc f", p=128)
        for j in range(4):
            k.wload(wg[b][0][:, 2 * j:2 * j + 2, :], wg[b][1], srcg[:, 2 * j:2 * j + 2, :])
        for j in range(4):
            k.wload(wu[b][0][:, 2 * j:2 * j + 2, :], wu[b][1], srcu[:, 2 * j:2 * j + 2, :])

    def wd_loads(ex):
        srcd = ins["w_down"][ex].rearrange("(fc p) d -> p fc d", p=128)
        for (a0, a1) in ((0, 3), (3, 6), (6, 9), (9, 11)):
            k.wload(wd[0][0][:, a0:a1, :], wd[0][1], srcd[:, a0:a1, :])

    def g_loads(ex):
        b = ex % 2
        k.load(idx2[b][0], idx2[b][1], idx_s[ex], r_idx_s)
        k.load(gc2[b][0], gc2[b][1], gc_s[ex], r_gc_s, q="act")
        for cc in range(4):
            S.dma("pool", ig, lambda e, b=b, cc=cc: e.indirect_dma_start(out=xg[b][0][:, cc, :], out_offset=None, in_=h2_rows,
                                                                       in_offset=bass.IndirectOffsetOnAxis(idx2[b][0].bitcast(U32)[:, cc:cc + 1], 0)),
                  reads=[idx2[b][1], r_h2_s], writes=[xg[b][1]])

    g_loads(0)
    w_loads(0)
    prev_sc = []
    for ex in range(NE):
        b = ex % 2
        wd_loads(ex)
        if ex + 1 < NE:
            g_loads(ex + 1)
            w_loads(ex + 1)
        wg_t, r_wg = wg[b]
        wu_t, r_wu = wu[b]
        wd_t, r_wd = wd[0]
        xg_t, r_xg = xg[b]
        gc_t, r_gc = gc2[b]
        id_t, r_id = idx2[b]
        for kc in range(8):
            pt, r_pt = _nb(k, BF16)

            def trx(e, pt=pt, kc=kc, xg_t=xg_t):
                for cc in range(4):
                    ins_ = e.transpose(out=pt[:, cc * 128:(cc + 1) * 128], in_=xg_t[:, cc, kc * 128:(kc + 1) * 128], identity=k.ident_b)
                return ins_
            S.op("pe", trx, reads=[r_xg, k.r_ident_b], writes=[r_pt])
            if kc % 2 == 0:
                S.op("act", lambda e, pt=pt, kc=kc: e.activation(out=xeT_t[:, kc, :], in_=pt[:, 0:512], func=AF.Copy), reads=[r_pt], writes=[r_xeT])
            else:
                S.op("dve", lambda e, pt=pt, kc=kc: e.tensor_copy(out=xeT_t[:, kc, :], in_=pt[:, 0:512]), reads=[r_pt], writes=[r_xeT])
        for fc in range(NFC):
            pg, r_pg = _nb(k)
            pu, r_pu = _nb(k)

            def mmG(e, pg=pg, fc=fc, wg_t=wg_t):
                for kc in range(8):
                    ins_ = e.matmul(pg, lhsT=wg_t[:, kc, fc * 128:(fc + 1) * 128], rhs=xeT_t[:, kc, :], start=(kc == 0), stop=(kc == 7))
                return ins_
            S.op("pe", mmG, reads=[r_wg, r_xeT], writes=[r_pg])

            def mmU(e, pu=pu, fc=fc, wu_t=wu_t):
                for kc in range(8):
                    ins_ = e.matmul(pu, lhsT=wu_t[:, kc, fc * 128:(fc + 1) * 128], rhs=xeT_t[:, kc, :], start=(kc == 0), stop=(kc == 7))
                return ins_
            S.op("pe", mmU, reads=[r_wu, r_xeT], writes=[r_pu])
            sg_t, r_sg = sg[fc % 2]
            S.op("act", lambda e, pg=pg, sg_t=sg_t: e.activation(out=sg_t, in_=pg, func=AF.Silu), reads=[r_pg], writes=[r_sg])
            S.op("dve", lambda e, pu=pu, sg_t=sg_t, fc=fc: e.tensor_tensor(out=hid[:, fc, :], in0=pu, in1=sg_t, op=ALU.mult), reads=[r_pu, r_sg], writes=[r_hid])
        cur_sc = []
        for cc in range(4):
            y_t, r_y = yef[cc]
            for hf in range(2):
                pd, r_pd = _nb(k)

                def mmD(e, pd=pd, cc=cc, hf=hf):
                    for fc in range(NFC):
                        ins_ = e.matmul(pd, lhsT=hid[:, fc, cc * 128:(cc + 1) * 128], rhs=wd_t[:, fc, hf * 512:(hf + 1) * 512], start=(fc == 0), stop=(fc == NFC - 1))
                    return ins_
                S.op("pe", mmD, reads=[r_hid, r_wd], writes=[r_pd])
                S.op("act", lambda e, pd=pd, cc=cc, hf=hf, y_t=y_t, gc_t=gc_t: e.activation(out=y_t[:, hf * 512:(hf + 1) * 512], in_=pd, func=AF.Copy, scale=gc_t[:, cc:cc + 1]),
                     reads=[r_pd, r_gc], writes=[r_y])
            S.op("pool", lambda e, y_t=y_t: e.tensor_tensor(out=y_t, in0=y_t, in1=k.gate2_row, op=ALU.mult), reads=[r_y, k.r_gate2], writes=[r_y])
            tok_ = S.dma("pool", ig, lambda e, y_t=y_t, id_t=id_t, cc=cc: e.indirect_dma_start(out=k.out, out_offset=bass.IndirectOffsetOnAxis(id_t.bitcast(U32)[:, cc:cc + 1], 0),
                                                                                          in_=y_t, in_offset=None, compute_op=ALU.add),
                         reads=[r_y, r_id, k.out_res], writes=[], extra=prev_sc)
            cur_sc.append(tok_)
            if cc == 3:
                prev_sc = cur_sc
    S.barrier()
    A.release()
    A.release()

def _rope_tables():
    rows, gw = 64, 64
    row = np.repeat(np.arange(rows), gw).astype(np.float32)
    col = np.tile(np.arange(gw), rows).astype(np.float32)
    n_freq = 16
    inv_freq = (10000.0 ** (-np.arange(n_freq, dtype=np.float32) / n_freq)).astype(np.float32)
    ang_r = row[:, None] * inv_freq
    ang_c = col[:, None] * inv_freq
    ang = np.concatenate([ang_r, ang_r, ang_c, ang_c], axis=-1).astype(np.float32)
    cos = np.cos(ang).astype(np.float32)
    sin = np.sin(ang).astype(np.float32)
    sgn = np.concatenate([-np.ones(16), np.ones(16), -np.ones(16), np.ones(16)]).astype(np.float32)
    return cos, (sin * sgn).astype(np.float32)


def prep_shared(inp):
    f = np.float32
    sh = {}
    sh["ada_w"] = np.ascontiguousarray(inp["ada_w"][0])
    sh["ada_b_row"] = np.ascontiguousarray(inp["ada_b"][0][None, :])
    sh["ada_bT"] = np.ascontiguousarray(inp["ada_b"][0].reshape(48, 128).T)
    sh["g1T"] = np.ascontiguousarray(inp["norm1_g"][0].reshape(8, 128).T)
    sh["g2T"] = np.ascontiguousarray(inp["norm2_g"][0].reshape(8, 128).T)
    sh["g2_rep"] = np.ascontiguousarray(np.broadcast_to(inp["norm2_g"][0].reshape(1, 1024), (128, 1024)))
    sh["w_in"] = np.ascontiguousarray(inp["w_in"][0])
    sh["convT"] = np.ascontiguousarray(inp["conv_w"][0].T.reshape(24, 128, 5).transpose(1, 0, 2))
    sh["alog_rep"] = np.ascontiguousarray(np.broadcast_to(inp["a_log"][0].reshape(1, 16), (128, 16)))
    sh["dtb_rep"] = np.ascontiguousarray(np.broadcast_to(inp["dt_bias"][0].reshape(1, 16), (128, 16)))
    sh["dng_rep"] = np.ascontiguousarray(np.broadcast_to(inp["dn_norm_g"][0].reshape(1, 128), (128, 128)))
    sh["gqaT"] = np.ascontiguousarray(inp["q_a_norm_g"][0].reshape(3, 128).T)
    sh["w_uq"] = np.ascontiguousarray(inp["w_uq"][0])
    sh["gkvaT"] = np.ascontiguousarray(inp["kv_a_norm_g"][0].reshape(2, 128).T)
    sh["w_ukv"] = np.ascontiguousarray(inp["w_ukv"][0])
    sh["gq_rep"] = np.ascontiguousarray(np.broadcast_to(inp["q_norm_g"][0].reshape(1, 192), (128, 192)))
    sh["gk_rep"] = np.ascontiguousarray(np.broadcast_to(inp["k_norm_g"][0].reshape(1, 192), (128, 192)))
    sh["w_out_a"] = np.ascontiguousarray(inp["w_out_a"][0])
    sh["w_out_b"] = np.ascontiguousarray(inp["w_out_b"][0])
    sh["w_o"] = np.ascontiguousarray(inp["w_o"][0])
    sh["router_w"] = np.ascontiguousarray(inp["router_w"][0])
    sh["w_gate"] = np.ascontiguousarray(inp["w_gate"][0])
    sh["w_up"] = np.ascontiguousarray(inp["w_up"][0])
    sh["w_down"] = np.ascontiguousarray(inp["w_down"][0])
    cos, sinS = _rope_tables()
    sh["rope_cs"] = np.ascontiguousarray(np.concatenate([cos, sinS], axis=1))
    sh["ident"] = np.eye(128, dtype=f)
    consts = np.zeros((128, 1024), f)
    consts[:, 0:512] = np.arange(512, dtype=f)[None, :]
    consts[:, 512] = np.arange(128, dtype=f)
    sh["consts"] = consts
    pp_ = np.arange(128)
    sh["moe_blk"] = (pp_[:, None] // 8 == pp_[None, :] // 8).astype(f)
    s8 = np.zeros((128, 16), f)
    s8[np.arange(16) * 8, np.arange(16)] = 1.0
    sh["moe_sel8"] = s8
    tk = np.zeros((128, 32, 2), f)
    tk[:, :, 0] = np.arange(32, dtype=f)[None, :]
    tk[:, :, 1] = np.arange(128, dtype=f)[:, None]
    sh["moe_tok"] = tk
    sh["moe_tris"] = (pp_[:, None] < pp_[None, :]).astype(f)
    ii = np.arange(128)
    P, Fr = ii[:, None], ii[None, :]
    NEGV = -30000.0
    mk = np.zeros((128, 9, 128), f)
    mk[:, 0] = (P <= Fr)
    mk[:, 1] = (P >= Fr)
    mk[:, 2] = np.where(P > Fr, 0.0, NEGV)
    mk[:, 3] = np.where(P < Fr, 0.0, NEGV)
    mk[:, 4] = np.where(Fr >= P, 0.0, NEGV)
    mk[:, 5] = np.where(Fr <= P, 0.0, NEGV)
    mk[:, 6] = (P // 32 == Fr // 32)
    mk[:, 7] = (P // 64 == Fr // 64) & (P // 32 != Fr // 32)
    mk[:, 8] = (P // 64 != Fr // 64)
    sh["dn_masks"] = mk
    es = np.zeros((64, 2, 8, 128), f)
    for hh in range(8):
        es[hh, 0, hh, :] = 1.0
        es[32 + hh, 0, hh, :] = 1.0
    es[:, 1] = -es[:, 0]
    sh["dn_esel"] = es
    li = np.zeros((64, 128), f)
    li[32:40] = 1.0
    sh["dn_linit"] = li
    return sh


def prep_core(inp, sh, b):
    m = dict(sh)
    m["x"] = np.ascontiguousarray(inp["x"][b])
    m["ctx"] = np.ascontiguousarray(inp["ctx"][b])
    cc = np.stack([inp["c"][b], inp["c_ctx"]], axis=-1).astype(np.float32)
    m["c2"] = np.ascontiguousarray(cc.reshape(8, 128, 2).transpose(1, 0, 2))
    return m

PHASES = ["a0", "a1", "a2", "b", "c", "d", "e"]


def build(upto="e", dbg=(), dumps=()):
    k = K(dbg=dbg)
    declare_inputs(k)
    setup_consts(k)
    k.dump_list = []

    def dump(name, ap, res, shape):
        t = k.nc.dram_tensor("dbg_" + name, list(shape), ap.dtype, kind="ExternalOutput").ap()
        k.store(t, None, ap, res)
        k.dump_list.append("dbg_" + name)
    k.dump = dump
    k.dumps = set(dumps)
    fns = {"a0": phase_a0}
    for nm in ("a1", "a2", "b", "c", "d", "e"):
        f = globals().get("phase_" + nm)
        if f is not None:
            fns[nm] = f
    for ph in PHASES:
        if ph in fns:
            fns[ph](k)
        if ph == upto:
            break
    k.S.emit()
    return k


_CACHE = {}


def kernel(**inputs):
    inp = {kk: np.asarray(v) for kk, v in inputs.items()}
    sh = prep_shared(inp)
    in_maps = [prep_core(inp, sh, b) for b in range(8)]
    k = build()
    res = run_bass_kernel_spmd(k.nc, in_maps, core_ids=list(range(8)))
    out = np.stack([np.asarray(r["out"]) for r in res.results], axis=0).astype(np.float32)
    return out
```

```python
import numpy as np
import concourse.bass as bass
import concourse.mybir as mybir
from concourse.bass_utils import run_bass_kernel_spmd

F32 = mybir.dt.float32
BF16 = mybir.dt.bfloat16
F32R = mybir.dt.float32r
I32 = mybir.dt.int32
AF = mybir.ActivationFunctionType
ALU = mybir.AluOpType
AX = mybir.AxisListType

ENGS = ("pe", "act", "dve", "pool", "sp")
EPOCH = 12000


class Res:
    __slots__ = ("name", "w", "rs", "multi", "ws", "excl")

    def __init__(self, name="", multi=False, excl=False):
        self.excl = excl
        self.name = name
        self.w = None
        self.rs = []
        self.multi = multi
        self.ws = []


class Tok:
    __slots__ = ("key", "val", "eng")

    def __init__(self, key, val, eng):
        self.key = key
        self.val = val
        self.eng = eng


class DmaPool:
    def __init__(self, sched, name, n):
        self.s = sched
        self.name = name
        self.n = n
        self.i = 0
        self.count = [0] * n
        self.last = [None] * n

    def keys(self):
        return [("dma", self.name, j) for j in range(self.n)]


class Sched:
    def __init__(self, nc):
        self.nc = nc
        self.ops = {e: [] for e in ENGS}
        self.cnt = {e: 0 for e in ENGS}
        self.pools = []
        self.last_tok = {e: None for e in ENGS}
        self.n_instr = 0

    def pool(self, name, n):
        p = DmaPool(self, name, n)
        self.pools.append(p)
        return p

    def _deps(self, eng, reads, writes):
        deps = []
        for r in reads:
            if r.multi:
                deps.extend(r.ws)
            elif r.w is not None:
                deps.append(r.w)
            if r.excl:
                deps.extend(t for t in r.rs if t.eng != eng)
        for w in writes:
            if w.multi:
                pass
            elif w.w is not None and w.w.eng != eng:
                deps.append(w.w)
            for t in w.rs:
                if t.eng != eng:
                    deps.append(t)
        return deps

    def _mark_w(self, writes, tok):
        for w in writes:
            if w.multi:
                w.ws.append(tok)
            else:
                w.w = tok
                w.rs = []

    def op(self, eng, fn, reads=(), writes=(), extra=()):
        deps = self._deps(eng, reads, writes) + list(extra)
        c = self.cnt[eng]
        tok = Tok(("eng", eng, c // EPOCH), c % EPOCH + 1, eng)
        self.cnt[eng] = c + 1
        for r in reads:
            r.rs.append(tok)
        self._mark_w(writes, tok)
        self.ops[eng].append((deps, fn, tok, 1))
        self.last_tok[eng] = tok
        return tok

    def dma(self, eng, pool, fn, reads=(), writes=(), extra=()):
        deps = self._deps('__dma__', reads, writes) + list(extra)
        j = pool.i
        pool.i = (pool.i + 1) % pool.n
        if pool.last[j] is not None:
            deps.append(pool.last[j])
        pool.count[j] += 16
        tok = Tok(("dma", pool.name, j), pool.count[j], None)
        pool.last[j] = tok
        for r in reads:
            r.rs.append(tok)
        self._mark_w(writes, tok)
        self.ops[eng].append((deps, fn, tok, 16))
        return tok

    def barrier(self):
        toks = [t for t in self.last_tok.values() if t is not None]
        for p in self.pools:
            toks += [t for t in p.last if t is not None]
        for e in ENGS:
            self.ops[e].append((list(toks), None, None, 0))

    def emit(self, final_waits_eng="sp"):
        nc = self.nc
        sems = {}

        def sem_of(key):
            if key not in sems:
                sems[key] = nc.alloc_semaphore("s_" + "_".join(str(k) for k in key))
            return sems[key]

        for e in ENGS:
            for ep in range((self.cnt[e] + EPOCH - 1) // EPOCH):
                sem_of(("eng", e, ep))
        for p in self.pools:
            for k in p.keys():
                sem_of(k)

        toks = [t for t in self.last_tok.values() if t is not None]
        for p in self.pools:
            toks += [t for t in p.last if t is not None]
        self.ops[final_waits_eng].append((list(toks), None, None, 0))

        engobj = {"pe": "tensor", "act": "scalar", "dve": "vector", "pool": "gpsimd", "sp": "sync"}
        sched = self

        def run(ename):
            def body(eng):
                seen = {}
                for deps, fn, tok, inc in sched.ops[ename]:
                    need = {}
                    for t in deps:
                        if t.val > need.get(t.key, 0):
                            need[t.key] = t.val
                    for k, v in need.items():
                        if seen.get(k, 0) >= v:
                            continue
                        seen[k] = v
                        eng.wait_ge(sem_of(k), v)
                        sched.n_instr += 1
                    if fn is not None:
                        ins = fn(eng)
                        ins.then_inc(sem_of(tok.key), inc)
                        sched.n_instr += 1
            return body

        with nc.Block() as block:
            for ename in ENGS:
                getattr(block, engobj[ename])(run(ename))


class Arena:
    def __init__(self, nc, kbytes=198):
        self.nc = nc
        self.words = kbytes * 256
        self.t = nc.alloc_sbuf_tensor("arena", [128, self.words], F32)
        self.ap = self.t.ap()
        self.off = 0
        self.marks = []
        self.peak = 0

    def tile(self, shape, dtype, name=None):
        esz = {F32: 4, BF16: 2, I32: 4}[dtype]
        n = int(np.prod(shape[1:]))
        nw = (n * esz + 3) // 4
        off = (self.off + 15) // 16 * 16
        assert off + nw <= self.words, f"SBUF overflow {off}+{nw} > {self.words}"
        self.off = off + nw
        self.peak = max(self.peak, self.off)
        a = self.ap[0:shape[0], off:off + nw]
        if dtype != F32:
            a = a.bitcast(dtype)
        a = a[:, 0:n]
        if len(shape) == 3:
            a = a.rearrange("p (a b) -> p a b", a=shape[1])
        elif len(shape) == 4:
            a = a.rearrange("p (a b c) -> p a b c", a=shape[1], b=shape[2])
        return a

    def mark(self):
        self.marks.append(self.off)

    def release(self):
        self.off = self.marks.pop()

D = 1024
T = 4352
NT = 34
TX = 4096
NCTX = 256
H = 8
OFF_Z = 3072
OFF_GATE = 4832
D_IN = 6880
NMID = 1760
EPS = 1e-6
NEG = -30000.0


class K:
    def __init__(self, dbg=()):
        self.nc = bass.Bass("TRN2", target_bir_lowering=False)
        self.S = Sched(self.nc)
        self.A = Arena(self.nc)
        self.dbg = set(dbg)
        self.ins = {}
        self.scr = {}
        nc = self.nc
        self.ps = nc.alloc_psum_tensor("ps", [128, 8, 512], F32).ap()
        self.psr = [Res(f"ps{b}", excl=True) for b in range(8)]
        self.ld = self.S.pool("ld", 8)
        self.st = self.S.pool("st", 8)
        self.wl = self.S.pool("wl", 12)

    def inp(self, name, shape, dtype=F32):
        t = self.nc.dram_tensor(name, list(shape), dtype, kind="ExternalInput").ap()
        self.ins[name] = t
        return t

    def scratch(self, name, shape, dtype):
        kind = "ExternalOutput" if name in self.dbg else "Internal"
        t = self.nc.dram_tensor(name, list(shape), dtype, kind=kind).ap()
        self.scr[name] = (t, Res(name, multi=True))
        return t, self.scr[name][1]

    def bank(self, b, dtype=F32):
        a = self.ps[:, b, :]
        if dtype == BF16:
            a = a.bitcast(BF16)
        return a, self.psr[b]

    def tile(self, shape, dtype, name=None):
        return self.A.tile(shape, dtype, name), Res(name or "t")

    def load(self, dst, dres, src, sres=None, q="sp", pool=None):
        return self.S.dma(q, pool or self.ld, lambda e: e.dma_start(out=dst, in_=src),
                          reads=[sres] if sres is not None else [], writes=[dres])

    def store(self, dst, dres, src, sres, q="sp", pool=None):
        return self.S.dma(q, pool or self.st, lambda e: e.dma_start(out=dst, in_=src),
                          reads=[sres], writes=[dres] if dres is not None else [])

    def wload(self, dst, dres, src):
        return self.S.dma("pool", self.wl, lambda e: e.dma_start(out=dst, in_=src), writes=[dres])


def declare_inputs(k):
    i = k.inp
    i("x", [TX, D]); i("ctx", [NCTX, D]); i("c2", [128, 8, 2])
    i("ada_w", [D, 6 * D]); i("ada_b_row", [1, 6 * D]); i("ada_bT", [128, 48])
    i("g1T", [128, 8]); i("g2T", [128, 8]); i("g2_rep", [128, D])
    i("w_in", [D, D_IN]); i("convT", [128, 24, 5])
    i("alog_rep", [128, 16]); i("dtb_rep", [128, 16]); i("dng_rep", [128, 128])
    i("gqaT", [128, 3]); i("w_uq", [384, 1536]); i("gkvaT", [128, 2]); i("w_ukv", [256, 2048])
    i("gq_rep", [128, 192]); i("gk_rep", [128, 192])
    i("w_out_a", [D, D]); i("w_out_b", [D, D]); i("w_o", [D, D])
    i("router_w", [D, 16]); i("w_gate", [16, D, 1408]); i("w_up", [16, D, 1408]); i("w_down", [16, 1408, D])
    i("rope_cs", [TX, 128])
    i("ident", [128, 128]); i("consts", [128, 1024])
    i("moe_tok", [128, 32, 2]); i("moe_blk", [128, 128]); i("moe_sel8", [128, 16]); i("moe_tris", [128, 128])
    i("dn_masks", [128, 9, 128]); i("dn_esel", [64, 2, 8, 128]); i("dn_linit", [64, 128])
    k.out = k.nc.dram_tensor("out", [TX, D], F32, kind="ExternalOutput").ap()
    k.out_res = Res("out", multi=True)


def setup_consts(k):
    S = k.S
    k.ident_f, k.r_ident_f = k.tile([128, 128], F32, "identf")
    k.ident_b, k.r_ident_b = k.tile([128, 128], BF16, "identb")
    k.load(k.ident_f, k.r_ident_f, k.ins["ident"])
    S.op("dve", lambda e: e.tensor_copy(out=k.ident_b, in_=k.ident_f), reads=[k.r_ident_f], writes=[k.r_ident_b])
    k.ones_f, k.r_ones_f = k.tile([128, 128], F32, "onesf")
    k.ones_b, k.r_ones_b = k.tile([128, 128], BF16, "onesb")
    S.op("pool", lambda e: e.memset(k.ones_f, 1.0), writes=[k.r_ones_f])
    S.op("pool", lambda e: e.memset(k.ones_b, 1.0), writes=[k.r_ones_b])


def phase_a0(k):
    S, A, nc = k.S, k.A, k.nc
    ins = k.ins
    k.modT, k.r_modT = k.tile([128, 48, 2], F32, "modT")
    k.s1, k.r_s1 = k.tile([128, 8, 2], F32, "s1")
    k.s2, k.r_s2 = k.tile([128, 8], F32, "s2")
    k.gate1_row, k.r_gate1 = k.tile([128, D], F32, "gate1row")
    k.gate2_row, k.r_gate2 = k.tile([128, D], F32, "gate2row")
    k.off_after_gate2 = A.off
    k.shift2_row, k.r_shift2 = k.tile([128, D], F32, "shift2row")
    k.s2_row, k.r_s2row = k.tile([128, D], F32, "s2row")
    A.mark()
    c2, r_c2 = k.tile([128, 8, 2], F32, "c2")
    sc, r_sc = k.tile([128, 8, 2], F32, "sc")
    screp, r_screp = k.tile([128, 8, 128], F32, "screp")
    abT, r_abT = k.tile([128, 48], F32, "abT")
    abrow, r_abrow = k.tile([1, 6 * D], F32, "abrow")
    g1T, r_g1T = k.tile([128, 8], F32, "g1T")
    g2T, r_g2T = k.tile([128, 8], F32, "g2T")
    wbuf = [k.tile([128, 8, D], F32, f"adaw{j}") for j in range(2)]
    k.load(c2, r_c2, ins["c2"])
    k.load(abT, r_abT, ins["ada_bT"])
    k.load(abrow, r_abrow, ins["ada_b_row"])
    k.load(g1T, r_g1T, ins["g1T"])
    k.load(g2T, r_g2T, ins["g2T"])
    S.op("act", lambda e: e.activation(out=sc, in_=c2, func=AF.Silu), reads=[r_c2], writes=[r_sc])
    S.op("dve", lambda e: e.tensor_copy(out=screp, in_=sc[:, :, 0:1].to_broadcast([128, 8, 128])),
         reads=[r_sc], writes=[r_screp])
    pm, r_pm = k.bank(0)
    aw = ins["ada_w"].rearrange("(kc p) n -> p kc n", p=128)
    for sec in range(6):
        wt, r_wt = wbuf[sec % 2]
        q = "sp" if sec % 2 == 0 else "act"
        S.dma(q, k.ld, lambda e, wt=wt, sec=sec: e.dma_start(out=wt, in_=aw[:, :, sec * D:(sec + 1) * D]), writes=[r_wt])

        def mm(e, wt=wt, sec=sec):
            for fc in range(8):
                for kc in range(8):
                    ins_ = e.matmul(pm[:, (sec * 8 + fc) * 2:(sec * 8 + fc) * 2 + 2], lhsT=wt[:, kc, fc * 128:(fc + 1) * 128],
                                    rhs=sc[:, kc, :], start=(kc == 0), stop=(kc == 7))
            return ins_
        S.op("pe", mm, reads=[r_wt, r_sc], writes=[r_pm])
        if sec in (2, 3, 4, 5):
            dst, r_dst = {2: (k.gate1_row, k.r_gate1), 5: (k.gate2_row, k.r_gate2), 3: (k.shift2_row, k.r_shift2), 4: (k.s2_row, k.r_s2row)}[sec]
            for hf in range(2):
                pb, r_pb = k.bank(1 + hf)

                def mmr(e, wt=wt, sec=sec, hf=hf, pb=pb):
                    for kc in range(8):
                        e.matmul(pb, lhsT=screp[:, kc, :], rhs=wt[:, kc, hf * 512:(hf + 1) * 512], start=(kc == 0), stop=False)
                    return e.matmul(pb, lhsT=k.ones_f[0:1, :], rhs=abrow[0:1, sec * D + hf * 512: sec * D + (hf + 1) * 512],
                                    start=False, stop=True)
                S.op("pe", mmr, reads=[r_wt, r_screp, k.r_ones_f, r_abrow], writes=[r_pb])
                S.op("act", lambda e, dst=dst, hf=hf, pb=pb: e.activation(out=dst[:, hf * 512:(hf + 1) * 512], in_=pb, func=AF.Copy),
                     reads=[r_pb], writes=[r_dst])
    S.op("dve", lambda e: e.tensor_tensor(out=k.modT, in0=pm[:, 0:96].rearrange("p (a b) -> p a b", b=2),
                                          in1=abT.unsqueeze(2).to_broadcast([128, 48, 2]), op=ALU.add),
         reads=[r_pm, r_abT], writes=[k.r_modT])
    S.op("dve", lambda e: e.scalar_tensor_tensor(out=k.s1, in0=k.modT[:, 8:16, :], scalar=1.0,
                                                 in1=g1T.unsqueeze(2).to_broadcast([128, 8, 2]), op0=ALU.add, op1=ALU.mult),
         reads=[k.r_modT, r_g1T], writes=[k.r_s1])
    S.op("dve", lambda e: e.scalar_tensor_tensor(out=k.s2, in0=k.modT[:, 32:40, 0], scalar=1.0,
                                                 in1=g2T, op0=ALU.add, op1=ALU.mult),
         reads=[k.r_modT, r_g2T], writes=[k.r_s2])
    g2rep, r_g2rep = k.tile([128, D], F32, "g2rep")
    k.load(g2rep, r_g2rep, ins["g2_rep"])
    S.op("dve", lambda e: e.scalar_tensor_tensor(out=k.s2_row, in0=k.s2_row, scalar=1.0, in1=g2rep, op0=ALU.add, op1=ALU.mult),
         reads=[k.r_s2row, r_g2rep], writes=[k.r_s2row])
    S.barrier()
    A.release()

def _nb(k, dtype=F32):
    b = getattr(k, "_bank_i", 0)
    k._bank_i = (b + 1) % 8
    return k.bank(b, dtype)


def _rstd(k, ss, r_ss, n, out, r_out):
    S = k.S
    S.op("act", lambda e: e.activation(out=out, in_=ss, func=AF.Sqrt, scale=1.0 / n, bias=EPS), reads=[r_ss], writes=[r_out])
    S.op("dve", lambda e: e.reciprocal(out=out, in_=out), reads=[r_out], writes=[r_out])


def _rope(k, pe, r_pe, cos_t, sin_t, r_tab, t1, t2, r_t1, r_t2):
    S = k.S
    cb = cos_t.unsqueeze(1).to_broadcast([128, 8, 64])
    S.op("pool", lambda e: e.tensor_tensor(out=t1, in0=pe, in1=cb, op=ALU.mult), reads=[r_pe, r_tab], writes=[r_t1])
    pe5 = pe.rearrange("p h (a s c) -> p h a s c", a=2, s=2)
    t25 = t2.rearrange("p h (a s c) -> p h a s c", a=2, s=2)
    sn5 = sin_t.rearrange("p (a s c) -> p a s c", a=2, s=2)

    def f(e):
        for s in range(2):
            ins_ = e.tensor_tensor(out=t25[:, :, :, s, :], in0=pe5[:, :, :, 1 - s, :],
                                   in1=sn5[:, :, s, :].unsqueeze(1).to_broadcast([128, 8, 2, 16]), op=ALU.mult)
        return ins_
    S.op("dve", f, reads=[r_pe, r_tab], writes=[r_t2])
    S.op("pool", lambda e: e.tensor_tensor(out=pe, in0=t1, in1=t2, op=ALU.add), reads=[r_t1, r_t2], writes=[r_pe])


def phase_a1(k):
    S, A, nc, ins = k.S, k.A, k.nc, k.ins
    zs_s, r_zs_s = k.scratch("zs_s", [TX, D], BF16)
    gb_s, r_gb_s = k.scratch("gb_s", [T, 48], F32)
    qmT_s, r_qmT_s = k.scratch("qmT_s", [H, 192, TX], BF16)
    kmT_s, r_kmT_s = k.scratch("kmT_s", [H, 192, T], BF16)
    vm_s, r_vm_s = k.scratch("vm_s", [T, D], BF16)
    A.mark()
    k.hT, _ = k.tile([128, 8, T], BF16, "hT")
    k.r_hT = [Res(f"hT{i}") for i in range(NT)]
    A.mark()
    wmid, r_wmid = k.tile([128, 8, NMID], BF16, "wmid")
    wuq, r_wuq = k.tile([128, 3, 1536], BF16, "wuq")
    wukv, r_wukv = k.tile([128, 2, 2048], BF16, "wukv")
    dtb, r_dtb = k.tile([128, 16], F32, "dtb")
    negA, r_negA = k.tile([128, 16], F32, "negA")
    gq, r_gq = k.tile([128, 192], F32, "gq")
    gk, r_gk = k.tile([128, 192], F32, "gk")
    gqaT, r_gqaT = k.tile([128, 3], F32, "gqaT")
    gkvaT, r_gkvaT = k.tile([128, 2], F32, "gkvaT")
    win = ins["w_in"].rearrange("(kc p) n -> p kc n", p=128)
    for j in range(4):
        k.wload(wmid[:, 2 * j:2 * j + 2, :], r_wmid, win[:, 2 * j:2 * j + 2, OFF_Z:OFF_GATE])
    k.wload(wuq, r_wuq, ins["w_uq"].rearrange("(kc p) n -> p kc n", p=128))
    k.wload(wukv, r_wukv, ins["w_ukv"].rearrange("(kc p) n -> p kc n", p=128))
    k.load(dtb, r_dtb, ins["dtb_rep"])
    k.load(negA, r_negA, ins["alog_rep"])
    k.load(gq, r_gq, ins["gq_rep"])
    k.load(gk, r_gk, ins["gk_rep"])
    k.load(gqaT, r_gqaT, ins["gqaT"])
    k.load(gkvaT, r_gkvaT, ins["gkvaT"])
    S.op("act", lambda e: e.activation(out=negA, in_=negA, func=AF.Exp), reads=[r_negA], writes=[r_negA])
    S.op("dve", lambda e: e.tensor_scalar(out=negA, in0=negA, scalar1=-1.0, scalar2=None, op0=ALU.mult), reads=[r_negA], writes=[r_negA])
    S.op("dve", lambda e: e.tensor_scalar(out=gq, in0=gq, scalar1=192.0 ** -0.5, scalar2=None, op0=ALU.mult), reads=[r_gq], writes=[r_gq])

    NB = 2
    xt = [k.tile([128, D], F32, "xt") for _ in range(NB)]
    junk, r_junk = k.tile([128, D], BF16, "junk")
    ss = [k.tile([128, 8], F32, "ss") for _ in range(NB)]
    xn = [k.tile([128, D], BF16, "xn") for _ in range(NB)]
    zs = [k.tile([128, D], BF16, "zs") for _ in range(NB)]
    gb = [k.tile([128, 48], F32, "gb") for _ in range(NB)]
    t16, r_t16 = k.tile([128, 16], F32, "t16")
    cqn, r_cqn = k.tile([128, 384], BF16, "cqn")
    ckvn, r_ckvn = k.tile([128, 256], BF16, "ckvn")
    cqnT, r_cqnT = k.tile([128, 3, 128], BF16, "cqnT")
    ckvnT, r_ckvnT = k.tile([128, 2, 128], BF16, "ckvnT")
    kr, r_kr = k.tile([128, 64], F32, "kr")
    qsb, r_qsb = k.tile([128, 8, 192], F32, "qsb")
    sq, r_sq = k.tile([128, 8, 192], F32, "sq")
    r8, r_r8 = k.tile([128, 8], F32, "r8")
    kvsb, r_kvsb = k.tile([128, 8, 2, 128], F32, "kvsb")
    tmpf, r_tmpf = kvsb.rearrange("p a b c -> p (a b c)")[:, 0:1024].rearrange("p (a b) -> p a b", a=8), r_kvsb
    kf, r_kf = sq, r_sq
    rk8, r_rk8 = k.tile([128, 8], F32, "rk8")
    sskr, r_sskr = k.tile([128, 1], F32, "sskr")
    rt1, r_rt1 = k.tile([128, 8, 64], F32, "rt1")
    rt2, r_rt2 = k.tile([128, 8, 64], F32, "rt2")
    cs_t = [k.tile([128, 128], F32, "cs") for _ in range(NB)]
    qf, r_qf = k.tile([128, 8, 192], BF16, "qf")
    kfb, r_kfb = k.tile([128, 8, 192], BF16, "kfb")
    vb = [k.tile([128, 8, 128], BF16, "vb") for _ in range(1)]
    qTn = [k.tile([128, 8, 128], BF16, "qTn") for _ in range(1)]
    qTr = [k.tile([64, 8, 128], BF16, "qTr") for _ in range(1)]
    kTn = [k.tile([128, 8, 128], BF16, "kTn") for _ in range(1)]
    kTr = [k.tile([64, 8, 128], BF16, "kTr") for _ in range(1)]

    def src_rows(i):
        return ins["ctx"][i * 128:(i + 1) * 128, :] if i < 2 else ins["x"][(i - 2) * 128:(i - 1) * 128, :]

    def prefetch(i):
        b = i % NB
        k.load(xt[b][0], xt[b][1], src_rows(i))
        if i >= 2:
            xi = i - 2
            S.dma("act", k.ld, lambda e: e.dma_start(out=cs_t[b][0], in_=ins["rope_cs"][xi * 128:(xi + 1) * 128, :]), writes=[cs_t[b][1]])

    def transposes(src, r_src, n, width=128, rows=128):
        pb, r_pb = _nb(k, BF16)
        pv = pb.rearrange("p (a b) -> p a b", b=128)[0:width, 0:n, :]

        def f(e):
            for j in range(n):
                ins_ = e.transpose(out=pv[:, j, :], in_=src(j), identity=k.ident_b)
            return ins_
        S.op("pe", f, reads=[r_src, k.r_ident_b], writes=[r_pb])
        return pv, r_pb

    prefetch(0)
    for i in range(NT):
        b = i % NB
        is_x = i >= 2
        xi = i - 2
        col = 0 if is_x else 1
        tok = slice(i * 128, (i + 1) * 128)
        if i + 1 < NT:
            prefetch(i + 1)
        x_t, r_x = xt[b]
        ss_t, r_ss = ss[b]
        xn_t, r_xn = xn[b]
        S.op("act", lambda e, x_t=x_t, ss_t=ss_t: e.activation(out=junk, in_=x_t, func=AF.Square, accum_out=ss_t[:, 0:1]),
             reads=[r_x], writes=[r_junk, r_ss])
        _rstd(k, ss_t[:, 0:1], r_ss, D, ss_t[:, 1:2], r_ss)
        S.op("act", lambda e, x_t=x_t, ss_t=ss_t, xn_t=xn_t: e.activation(out=xn_t, in_=x_t, func=AF.Copy, scale=ss_t[:, 1:2]),
             reads=[r_x, r_ss], writes=[r_xn])
        pv, r_pv = transposes(lambda j, xn_t=xn_t: xn_t[:, j * 128:(j + 1) * 128], r_xn, 8)
        S.op("dve", lambda e, pv=pv, col=col: e.tensor_tensor(out=tmpf, in0=pv, in1=k.s1[:, :, col:col + 1].to_broadcast([128, 8, 128]), op=ALU.mult),
             reads=[r_pv, k.r_s1], writes=[r_tmpf])
        S.op("pool", lambda e, col=col, tok=tok: e.tensor_tensor(out=k.hT[:, :, tok], in0=tmpf,
                                                                in1=k.modT[:, 0:8, col:col + 1].to_broadcast([128, 8, 128]), op=ALU.add),
             reads=[r_tmpf, k.r_modT], writes=[k.r_hT[i]])
        groups = [(0, 512), (512, 1024), (1024, 1440), (1440, 1760)]
        banks = []
        for g, (c0, c1) in enumerate(groups):
            if g < 2 and not is_x:
                banks.append(None)
                continue
            pb, r_pb = _nb(k)

            def mm(e, pb=pb, c0=c0, c1=c1, tok=tok):
                for kc in range(8):
                    ins_ = e.matmul(pb[:, 0:c1 - c0], lhsT=k.hT[:, kc, tok], rhs=wmid[:, kc, c0:c1], start=(kc == 0), stop=(kc == 7))
                return ins_
            S.op("pe", mm, reads=[k.r_hT[i], r_wmid], writes=[r_pb])
            banks.append((pb, r_pb))
        if is_x:
            z_t, r_z = zs[b]
            for g in range(2):
                pb, r_pb = banks[g]
                S.op("act", lambda e, pb=pb, g=g, z_t=z_t: e.activation(out=z_t[:, g * 512:(g + 1) * 512], in_=pb, func=AF.Silu),
                     reads=[r_pb], writes=[r_z])
            k.store(zs_s[xi * 128:(xi + 1) * 128, :], r_zs_s, z_t, r_z)
        p2, r_p2 = banks[2]
        p3, r_p3 = banks[3]
        gb_t, r_gb = gb[b]
        S.op("dve", lambda e, p2=p2: e.tensor_tensor(out=t16, in0=p2[:, 0:16], in1=dtb, op=ALU.add), reads=[r_p2, r_dtb], writes=[r_t16])
        S.op("act", lambda e: e.activation(out=t16, in_=t16, func=AF.Exp), reads=[r_t16], writes=[r_t16])
        S.op("act", lambda e: e.activation(out=t16, in_=t16, func=AF.Ln, bias=1.0), reads=[r_t16], writes=[r_t16])
        S.op("dve", lambda e, gb_t=gb_t: e.tensor_tensor(out=gb_t[:, 0:16], in0=t16, in1=negA, op=ALU.mult), reads=[r_t16, r_negA], writes=[r_gb])
        S.op("act", lambda e, gb_t=gb_t, p2=p2: e.activation(out=gb_t[:, 16:32], in_=p2[:, 16:32], func=AF.Sigmoid), reads=[r_p2], writes=[r_gb])
        S.op("act", lambda e, gb_t=gb_t: e.activation(out=gb_t[:, 32:48], in_=gb_t[:, 16:32], func=AF.Ln), reads=[r_gb], writes=[r_gb])
        k.store(gb_s[tok, :], r_gb_s, gb_t, r_gb)
        if is_x:
            S.op("act", lambda e, p2=p2, ss_t=ss_t: e.activation(out=junk[:, 0:384], in_=p2[:, 32:416], func=AF.Square, accum_out=ss_t[:, 4:5]),
                 reads=[r_p2], writes=[r_junk, r_ss])
            _rstd(k, ss_t[:, 4:5], r_ss, 384, ss_t[:, 5:6], r_ss)
            S.op("act", lambda e, p2=p2, ss_t=ss_t: e.activation(out=cqn, in_=p2[:, 32:416], func=AF.Copy, scale=ss_t[:, 5:6]),
                 reads=[r_p2, r_ss], writes=[r_cqn])

        S.op("act", lambda e, p3=p3, ss_t=ss_t: e.activation(out=junk[:, 0:256], in_=p3[:, 0:256], func=AF.Square, accum_out=ss_t[:, 2:3]),
             reads=[r_p3], writes=[r_junk, r_ss])
        _rstd(k, ss_t[:, 2:3], r_ss, 256, ss_t[:, 3:4], r_ss)
        S.op("act", lambda e, p3=p3, ss_t=ss_t: e.activation(out=ckvn, in_=p3[:, 0:256], func=AF.Copy, scale=ss_t[:, 3:4]),
             reads=[r_p3, r_ss], writes=[r_ckvn])
        S.op("dve", lambda e, p3=p3: e.tensor_copy(out=kr, in_=p3[:, 256:320]), reads=[r_p3], writes=[r_kr])
        pv, r_pv = transposes(lambda j: ckvn[:, j * 128:(j + 1) * 128], r_ckvn, 2)
        S.op("dve", lambda e, pv=pv: e.tensor_tensor(out=ckvnT, in0=pv, in1=gkvaT.unsqueeze(2).to_broadcast([128, 2, 128]), op=ALU.mult),
             reads=[r_pv, r_gkvaT], writes=[r_ckvnT])
        for b4 in range(4):
            pb, r_pb = _nb(k)

            def mmkv(e, pb=pb, b4=b4):
                for kc in range(2):
                    ins_ = e.matmul(pb, lhsT=ckvnT[:, kc, :], rhs=wukv[:, kc, b4 * 512:(b4 + 1) * 512], start=(kc == 0), stop=(kc == 1))
                return ins_
            S.op("pe", mmkv, reads=[r_ckvnT, r_wukv], writes=[r_pb])
            eng = "act" if b4 % 2 == 0 else "dve"
            dst = kvsb[:, 2 * b4:2 * b4 + 2, :, :].rearrange("p a b c -> p (a b c)")
            if eng == "act":
                S.op("act", lambda e, dst=dst, pb=pb: e.activation(out=dst, in_=pb, func=AF.Copy), reads=[r_pb], writes=[r_kvsb])
            else:
                S.op("dve", lambda e, dst=dst, pb=pb: e.tensor_copy(out=dst, in_=pb), reads=[r_pb], writes=[r_kvsb])
        vb_t, r_vb = vb[0]
        S.op("pool", lambda e, vb_t=vb_t: e.tensor_copy(out=vb_t, in_=kvsb[:, :, 1, :]), reads=[r_kvsb], writes=[r_vb])
        k.store(vm_s[tok, :], r_vm_s, vb_t.rearrange("p h d -> p (h d)"), r_vb)
        S.op("pool", lambda e: e.tensor_tensor(out=sq[:, :, 0:128], in0=kvsb[:, :, 0, :], in1=kvsb[:, :, 0, :], op=ALU.mult),
             reads=[r_kvsb], writes=[r_sq])
        S.op("dve", lambda e: e.tensor_reduce(out=rk8, in_=sq[:, :, 0:128], axis=AX.X, op=ALU.add), reads=[r_sq], writes=[r_rk8])
        S.op("act", lambda e: e.activation(out=junk[:, 0:64], in_=kr, func=AF.Square, accum_out=sskr), reads=[r_kr], writes=[r_junk, r_sskr])
        S.op("dve", lambda e: e.tensor_scalar(out=rk8, in0=rk8, scalar1=sskr, scalar2=None, op0=ALU.add), reads=[r_rk8, r_sskr], writes=[r_rk8])
        _rstd(k, rk8, r_rk8, 192, rk8, r_rk8)
        S.op("dve", lambda e: e.tensor_tensor(out=kf[:, :, 0:128], in0=kvsb[:, :, 0, :], in1=rk8.unsqueeze(2).to_broadcast([128, 8, 128]), op=ALU.mult),
             reads=[r_kvsb, r_rk8], writes=[r_kf])
        S.op("dve", lambda e: e.tensor_tensor(out=kf[:, :, 128:192], in0=kr.unsqueeze(1).to_broadcast([128, 8, 64]),
                                              in1=rk8.unsqueeze(2).to_broadcast([128, 8, 64]), op=ALU.mult),
             reads=[r_kr, r_rk8, r_kf], writes=[r_kf])
        S.op("pool", lambda e: e.tensor_tensor(out=kf, in0=kf, in1=gk.unsqueeze(1).to_broadcast([128, 8, 192]), op=ALU.mult),
             reads=[r_kf, r_gk], writes=[r_kf])
        if is_x:
            _rope(k, kf[:, :, 128:192], r_kf, cs_t[b][0][:, 0:64], cs_t[b][0][:, 64:128], cs_t[b][1], rt1, rt2, r_rt1, r_rt2)
        S.op("act", lambda e: e.activation(out=kfb, in_=kf, func=AF.Copy), reads=[r_kf], writes=[r_kfb])
        kTn_t, r_kTn = kTn[0]
        kTr_t, r_kTr = kTr[0]
        pv, r_pv = transposes(lambda j: kfb[:, j, 0:128], r_kfb, 8)
        S.op("dve", lambda e, pv=pv, kTn_t=kTn_t: e.tensor_copy(out=kTn_t, in_=pv), reads=[r_pv], writes=[r_kTn])
        pv, r_pv = transposes(lambda j: kfb[:, j, 128:192], r_kfb, 8, width=64)
        S.op("act", lambda e, pv=pv, kTr_t=kTr_t: e.activation(out=kTr_t, in_=pv, func=AF.Copy), reads=[r_pv], writes=[r_kTr])
        k.store(kmT_s[:, 0:128, tok].rearrange("h d t -> d h t"), r_kmT_s, kTn_t, r_kTn)
        k.store(kmT_s[:, 128:192, tok].rearrange("h d t -> d h t"), r_kmT_s, kTr_t, r_kTr)
        if not is_x:
            continue
        xtok = slice(xi * 128, (xi + 1) * 128)
        pv, r_pv = transposes(lambda j: cqn[:, j * 128:(j + 1) * 128], r_cqn, 3)
        S.op("dve", lambda e, pv=pv: e.tensor_tensor(out=cqnT, in0=pv, in1=gqaT.unsqueeze(2).to_broadcast([128, 3, 128]), op=ALU.mult),
             reads=[r_pv, r_gqaT], writes=[r_cqnT])
        qflat = qsb.rearrange("p h d -> p (h d)")
        for b3 in range(3):
            pb, r_pb = _nb(k)

            def mmq(e, pb=pb, b3=b3):
                for kc in range(3):
                    ins_ = e.matmul(pb, lhsT=cqnT[:, kc, :], rhs=wuq[:, kc, b3 * 512:(b3 + 1) * 512], start=(kc == 0), stop=(kc == 2))
                return ins_
            S.op("pe", mmq, reads=[r_cqnT, r_wuq], writes=[r_pb])
            if b3 % 2 == 0:
                S.op("act", lambda e, pb=pb, b3=b3: e.activation(out=qflat[:, b3 * 512:(b3 + 1) * 512], in_=pb, func=AF.Copy), reads=[r_pb], writes=[r_qsb])
            else:
                S.op("dve", lambda e, pb=pb, b3=b3: e.tensor_copy(out=qflat[:, b3 * 512:(b3 + 1) * 512], in_=pb), reads=[r_pb], writes=[r_qsb])
        S.op("pool", lambda e: e.tensor_tensor(out=sq, in0=qsb, in1=qsb, op=ALU.mult), reads=[r_qsb], writes=[r_sq])
        S.op("dve", lambda e: e.tensor_reduce(out=r8, in_=sq, axis=AX.X, op=ALU.add), reads=[r_sq], writes=[r_r8])
        _rstd(k, r8, r_r8, 192, r8, r_r8)
        S.op("dve", lambda e: e.tensor_tensor(out=qsb, in0=qsb, in1=r8.unsqueeze(2).to_broadcast([128, 8, 192]), op=ALU.mult),
             reads=[r_qsb, r_r8], writes=[r_qsb])
        S.op("pool", lambda e: e.tensor_tensor(out=qsb, in0=qsb, in1=gq.unsqueeze(1).to_broadcast([128, 8, 192]), op=ALU.mult),
             reads=[r_qsb, r_gq], writes=[r_qsb])
        _rope(k, qsb[:, :, 128:192], r_qsb, cs_t[b][0][:, 0:64], cs_t[b][0][:, 64:128], cs_t[b][1], rt1, rt2, r_rt1, r_rt2)
        S.op("act", lambda e: e.activation(out=qf, in_=qsb, func=AF.Copy), reads=[r_qsb], writes=[r_qf])
        qTn_t, r_qTn = qTn[0]
        qTr_t, r_qTr = qTr[0]
        pv, r_pv = transposes(lambda j: qf[:, j, 0:128], r_qf, 8)
        S.op("dve", lambda e, pv=pv, qTn_t=qTn_t: e.tensor_copy(out=qTn_t, in_=pv), reads=[r_pv], writes=[r_qTn])
        pv, r_pv = transposes(lambda j: qf[:, j, 128:192], r_qf, 8, width=64)
        S.op("act", lambda e, pv=pv, qTr_t=qTr_t: e.activation(out=qTr_t, in_=pv, func=AF.Copy), reads=[r_pv], writes=[r_qTr])
        k.store(qmT_s[:, 0:128, xtok].rearrange("h d t -> d h t"), r_qmT_s, qTn_t, r_qTn)
        k.store(qmT_s[:, 128:192, xtok].rearrange("h d t -> d h t"), r_qmT_s, qTr_t, r_qTr)
    S.barrier()
    A.release()

RW = 4364
NU = 4356


def phase_a2(k):
    S, A, nc, ins = k.S, k.A, k.nc, k.ins
    qdT_s, r_qdT_s = k.scratch("qdT_s", [H, 128, TX], BF16)
    kdT_s, r_kdT_s = k.scratch("kdT_s", [H, 128, T], BF16)
    kd_s, r_kd_s = k.scratch("kd_s", [T, H, 128], BF16)
    vd_s, r_vd_s = k.scratch("vd_s", [T, H, 128], BF16)
    sgT_s, r_sgT_s = k.scratch("sgT_s", [16, 128, TX], BF16)
    A.mark()
    convw, r_convw = k.tile([128, 24, 5], F32, "convw")
    k.load(convw, r_convw, ins["convT"])
    wc = [k.tile([128, 8, 512], BF16, "wc") for _ in range(2)]
    R = [k.tile([128, RW], BF16, "R") for _ in range(2)]
    accs = [k.tile([128, NU], F32, "acc") for _ in range(2)]
    sq, r_sq = k.tile([128, NU], BF16, "sq")
    Yb = [k.tile([128, NU], BF16, "Yb") for _ in range(2)]
    rn, r_rn = k.tile([128, 512], F32, "rn")
    tm = [k.tile([128, NT, 128], BF16, "tm") for _ in range(1)]
    sg = [k.tile([128, 512], BF16, "sg") for _ in range(2)]
    for j in range(2):
        S.op("pool", lambda e, j=j: e.memset(R[j][0], 0.0), writes=[R[j][1]])
    win = ins["w_in"].rearrange("(kc p) n -> p kc n", p=128)
    blocks = [(c * 512, "qkv", c * 4) for c in range(6)] + [(OFF_GATE + c * 512, "gate", c * 4) for c in range(4)]
    tgroups = [(0, 256, 2)] + [(256 + g * 512, 512, 262 + g * 512) for g in range(8)]
    all_hT = list(k.r_hT)

    def load_block(bi):
        c0, kind, _ = blocks[bi]
        w_t, r_w = wc[bi % 2]
        for hf in range(2):
            k.wload(w_t[:, 4 * hf:4 * hf + 4, :], r_w, win[:, 4 * hf:4 * hf + 4, c0:c0 + 512])

    load_block(0)
    ci = 0
    pending = []
    for bi, (c0, kind, chunk0) in enumerate(blocks):
        if bi + 1 < len(blocks):
            load_block(bi + 1)
        w_t, r_w = wc[bi % 2]
        for sub in range(4):
            cc = chunk0 + sub
            if kind == "gate":
                while pending:
                    pending.pop(0)()
                for g in range(8):
                    pb, r_pb = _nb(k)

                    def mm(e, pb=pb, g=g, sub=sub, w_t=w_t):
                        for kc in range(8):
                            ins_ = e.matmul(pb, lhsT=w_t[:, kc, sub * 128:(sub + 1) * 128], rhs=k.hT[:, kc, 256 + g * 512:256 + (g + 1) * 512],
                                            start=(kc == 0), stop=(kc == 7))
                        return ins_
                    S.op("pe", mm, reads=[r_w] + all_hT, writes=[r_pb])
                    s_t, r_s = sg[g % 2]
                    S.op("act", lambda e, pb=pb, s_t=s_t: e.activation(out=s_t, in_=pb, func=AF.Sigmoid), reads=[r_pb], writes=[r_s])
                    k.store(sgT_s[cc, :, g * 512:(g + 1) * 512], r_sgT_s, s_t, r_s)
                continue
            R_t, r_R = R[ci % 2]
            Y_t, r_Y = Yb[ci % 2]
            acc, r_acc = accs[ci % 2]
            ci += 1
            for gi, (h0, n, ro) in enumerate(tgroups):
                pb, r_pb = _nb(k)

                def mm(e, pb=pb, h0=h0, n=n, sub=sub, w_t=w_t):
                    for kc in range(8):
                        ins_ = e.matmul(pb[:, 0:n], lhsT=w_t[:, kc, sub * 128:(sub + 1) * 128], rhs=k.hT[:, kc, h0:h0 + n],
                                        start=(kc == 0), stop=(kc == 7))
                    return ins_
                S.op("pe", mm, reads=[r_w] + all_hT, writes=[r_pb])
                if gi % 2 == 0:
                    S.op("act", lambda e, pb=pb, n=n, ro=ro, R_t=R_t: e.activation(out=R_t[:, ro:ro + n], in_=pb[:, 0:n], func=AF.Copy),
                         reads=[r_pb], writes=[r_R])
                else:
                    S.op("dve", lambda e, pb=pb, n=n, ro=ro, R_t=R_t: e.tensor_copy(out=R_t[:, ro:ro + n], in_=pb[:, 0:n]),
                         reads=[r_pb], writes=[r_R])
            ceng = "dve"

            S.op(ceng, lambda e, R_t=R_t, cc=cc, acc=acc: e.tensor_scalar(out=acc, in0=R_t[:, 0:NU], scalar1=convw[:, cc, 0:1], scalar2=None, op0=ALU.mult),
                 reads=[r_R, r_convw], writes=[r_acc])
            for j in range(1, 5):
                S.op(ceng, lambda e, R_t=R_t, cc=cc, acc=acc, j=j: e.scalar_tensor_tensor(out=acc, in0=R_t[:, j:j + NU], scalar=convw[:, cc, j:j + 1], in1=acc,
                                                                                     op0=ALU.mult, op1=ALU.add),
                     reads=[r_R, r_convw, r_acc], writes=[r_acc])
            head = cc % 8
            if cc >= 16:
                S.op("act", lambda e, Y_t=Y_t, acc=acc: e.activation(out=Y_t, in_=acc, func=AF.Silu), reads=[r_acc], writes=[r_Y])
            else:
                S.op("act", lambda e, acc=acc: e.activation(out=acc, in_=acc, func=AF.Silu), reads=[r_acc], writes=[r_acc])

            def stage2(cc=cc, head=head, Y_t=Y_t, r_Y=r_Y, acc=acc, r_acc=r_acc):
                if cc < 16:
                    S.op("pool", lambda e: e.tensor_tensor(out=sq, in0=acc, in1=acc, op=ALU.mult), reads=[r_acc], writes=[r_sq])
                    scale = (128.0 ** -0.5) if cc < 8 else 1.0
                    for g in range(9):
                        u0 = g * 512
                        n = min(512, NU - u0)
                        pb, r_pb = _nb(k)
                        S.op("pe", lambda e, pb=pb, u0=u0, n=n: e.matmul(pb[:, 0:n], lhsT=k.ones_b, rhs=sq[:, u0:u0 + n], start=True, stop=True),
                             reads=[r_sq, k.r_ones_b], writes=[r_pb])
                        S.op("act", lambda e, pb=pb, n=n, scale=scale: e.activation(out=rn[:, 0:n], in_=pb[:, 0:n], func=AF.Sqrt,
                                                                                  scale=1.0 / (scale * scale), bias=EPS / (scale * scale)),
                             reads=[r_pb], writes=[r_rn])
                        S.op("dve", lambda e, n=n: e.reciprocal(out=rn[:, 0:n], in_=rn[:, 0:n]), reads=[r_rn], writes=[r_rn])
                        S.op("dve", lambda e, u0=u0, n=n, Y_t=Y_t, acc=acc: e.tensor_tensor(out=Y_t[:, u0:u0 + n], in0=acc[:, u0:u0 + n], in1=rn[:, 0:n], op=ALU.mult),
                             reads=[r_acc, r_rn], writes=[r_Y])
                if cc < 8:
                    k.store(qdT_s[head, :, :], r_qdT_s, Y_t[:, 260:260 + TX], r_Y)
                elif cc < 16:
                    k.store(kdT_s[head, :, 0:256], r_kdT_s, Y_t[:, 0:256], r_Y)
                    k.store(kdT_s[head, :, 256:T], r_kdT_s, Y_t[:, 260:260 + TX], r_Y)
                if cc >= 8:
                    tm_t, r_tm = tm[0]
                    for tb in range(5):
                        t0 = tb * 8
                        nt = min(8, NT - t0)
                        pb, r_pb = _nb(k, BF16)
                        pv = pb.rearrange("p (a b) -> p a b", b=128)[:, 0:nt, :]

                        def tr(e, pv=pv, t0=t0, nt=nt, Y_t=Y_t):
                            for j in range(nt):
                                ti = t0 + j
                                u = ti * 128 if ti < 2 else 260 + (ti - 2) * 128
                                ins_ = e.transpose(out=pv[:, j, :], in_=Y_t[:, u:u + 128], identity=k.ident_b)
                            return ins_
                        S.op("pe", tr, reads=[r_Y, k.r_ident_b], writes=[r_pb])
                        if tb % 2 == 0:
                            S.op("act", lambda e, pv=pv, t0=t0, nt=nt, tm_t=tm_t: e.activation(out=tm_t[:, t0:t0 + nt, :], in_=pv, func=AF.Copy),
                                 reads=[r_pb], writes=[r_tm])
                        else:
                            S.op("dve", lambda e, pv=pv, t0=t0, nt=nt, tm_t=tm_t: e.tensor_copy(out=tm_t[:, t0:t0 + nt, :], in_=pv),
                                 reads=[r_pb], writes=[r_tm])
                    dst_s, r_dst = (kd_s, r_kd_s) if cc < 16 else (vd_s, r_vd_s)
                    k.store(dst_s.rearrange("(n p) h d -> p n h d", p=128)[:, :, head, :], r_dst, tm_t, r_tm)
            pending.append(stage2)
            if len(pending) > 1:
                pending.pop(0)()
    while pending:
        pending.pop(0)()
    S.barrier()
    A.release()
    A.release()

import os as _os


def _drive(*gens):
    gens = [g for g in gens if g is not None]
    while gens:
        for g in list(gens):
            try:
                next(g)
            except StopIteration:
                gens.remove(g)


def phase_b(k):
    S, A, nc, ins = k.S, k.A, k.nc, k.ins
    qdT_s, r_qdT_s = k.scr["qdT_s"]
    kdT_s, r_kdT_s = k.scr["kdT_s"]
    kd_s, r_kd_s = k.scr["kd_s"]
    vd_s, r_vd_s = k.scr["vd_s"]
    gb_s, r_gb_s = k.scr["gb_s"]
    o_s = [k.scratch("of_s", [TX, D], F32), k.scratch("ob_s", [TX, D], F32)]
    A.mark()
    msk, r_msk = k.tile([128, 9, 128], F32, "msk")
    k.load(msk, r_msk, ins["dn_masks"])
    EC, r_EC = k.tile([64, 2, 8, 128], F32, "EC")
    k.load(EC, r_EC, ins["dn_esel"])
    L1, r_L1 = k.tile([64, 128], F32, "L1")
    L2, r_L2 = k.tile([64, 128], F32, "L2")
    R1, r_R1 = k.tile([64, 8, 128], F32, "R1")
    R2, r_R2 = k.tile([64, 8, 128], F32, "R2")
    X, r_X = k.tile([128, 2, 64], F32, "X")
    k.load(L1, r_L1, ins["dn_linit"])
    k.load(L2, r_L2, ins["dn_linit"])
    S.op("dve", lambda e: e.tensor_copy(out=R1, in_=EC[:, 0, :, :]), reads=[r_EC], writes=[r_R1])
    S.op("dve", lambda e: e.tensor_copy(out=R2, in_=EC[:, 1, :, :]), reads=[r_EC], writes=[r_R2])
    S.op("pool", lambda e: e.memset(X, 0.0), writes=[r_X])
    S32 = [k.tile([128, 8, 128], F32, f"S32_{d}") for d in range(2)]
    Sbf = [k.tile([128, 8, 128], BF16, f"Sbf_{d}") for d in range(2)]
    for d in range(2):
        S.op("pool", lambda e, d=d: e.memset(S32[d][0], 0.0), writes=[S32[d][1]])
        S.op("pool", lambda e, d=d: e.memset(Sbf[d][0], 0.0), writes=[Sbf[d][1]])
    NB = 2
    gbt = [k.tile([128, 48], F32, "gbt") for _ in range(NB)]
    kT = [k.tile([128, 8, 128], BF16, "kT") for _ in range(NB)]
    qT = [k.tile([128, 8, 128], BF16, "qT") for _ in range(NB)]
    ktm = [k.tile([128, 8, 128], BF16, "ktm") for _ in range(NB)]
    vtm = [k.tile([128, 8, 128], BF16, "vtm") for _ in range(NB)]
    sm = [k.tile([128, 4, 8], F32, "sm") for _ in range(NB)]
    E1, r_E1 = k.tile([128, 8, 128], F32, "E1")
    M, r_M = k.tile([128, 8, 128], F32, "M")
    Mt, r_Mt = k.tile([128, 8, 128], BF16, "Mt")
    Md, r_Md = k.tile([128, 8, 128], BF16, "Md")
    Mo1, r_Mo1 = k.tile([128, 8, 128], BF16, "Mo1")
    Mo2, r_Mo2 = k.tile([128, 8, 128], BF16, "Mo2")
    PP = [k.tile([128, 8, 128], BF16, f"PP{j}") for j in range(2)]
    PT = [k.tile([128, 8, 128], BF16, f"PT{j}") for j in range(2)]
    Tt, r_Tt = k.tile([128, 8, 128], BF16, "Tt")
    TtB, r_TtB = k.tile([128, 8, 128], BF16, "TtB")
    Abuf, _ = k.tile([128, 8, 128], BF16, "Abuf")
    E1r, Mdr, Mo1r, Mo2r, Mtr, Ttr = (t_ for t_ in (Abuf, Md, Mo1, Mo2, Mt, Tt))
    PPr = [PP[j][0] for j in range(2)]
    PTr = [PT[j][0] for j in range(2)]
    kg, r_kg = k.tile([128, 8, 128], BF16, "kg")
    kdec = [k.tile([128, 8, 128], BF16, "kdec") for _ in range(NB)]
    wT = [k.tile([128, 8, 128], BF16, "wT") for _ in range(NB)]
    u = [k.tile([128, 8, 128], F32, "u") for _ in range(NB)]
    qkT = [k.tile([128, 8, 128], BF16, "qkT") for _ in range(NB)]
    vnew, r_vnew = k.tile([128, 8, 128], BF16, "vnew")
    tmpo, r_tmpo = k.tile([128, 8, 128], F32, "tmpo")
    o_t = [k.tile([128, 8, 128], F32, "o") for _ in range(NB)]

    RG = {nm: [Res(nm + "0"), Res(nm + "1")] for nm in ("Ab", "E1", "M", "Md", "Mo1", "Mo2", "Mt", "Tt", "TtB", "PP0", "PP1", "PT0", "PT1")}
    wT_res = [[Res("wT"), Res("wT")] for _ in range(NB)]
    u_res = [[Res("u"), Res("u")] for _ in range(NB)]
    qk_res = [[Res("qk"), Res("qk")] for _ in range(NB)]
    pre_i = [0]

    def nbp(dtype=F32):
        b = pre_i[0]
        pre_i[0] = (b + 1) % 4
        return k.bank(b, dtype)

    def b4(pb):
        return pb.rearrange("p (a b) -> p a b", b=128)

    def b4h(pb):
        return pb.rearrange("p (a b) -> p a b", b=128)[:, 0:4, :]

    units = []
    fwd = list(range(NT))
    bwd = [1, 0] + list(range(NT - 1, 1, -1))
    for s in range(NT):
        units.append((0, fwd[s]))
        units.append((1, bwd[s]))

    def loads(ui):
        d, ti = units[ui]
        b = ui % NB
        tok = slice(ti * 128, (ti + 1) * 128)
        k.load(gbt[b][0], gbt[b][1], gb_s[tok, :], r_gb_s)
        k.load(kT[b][0], kT[b][1], kdT_s[:, :, tok].rearrange("h d t -> d h t"), r_kdT_s)
        k.load(ktm[b][0], ktm[b][1], kd_s[tok, :, :], r_kd_s, q="act")
        k.load(vtm[b][0], vtm[b][1], vd_s[tok, :, :], r_vd_s, q="act")
        if ti >= 2:
            xt_ = slice((ti - 2) * 128, (ti - 1) * 128)
            k.load(qT[b][0], qT[b][1], qdT_s[:, :, xt_].rearrange("h d t -> d h t"), r_qdT_s)

    def pre(ui):
        d, ti = units[ui]
        b = ui % NB
        is_x = ti >= 2
        g_t, r_g = gbt[b]
        kT_t, r_kT = kT[b]
        qT_t, r_qT = qT[b]
        ktm_t, r_ktm = ktm[b]
        vtm_t, r_vtm = vtm[b]
        sm_t, r_sm = sm[b]
        gcol = g_t[:, d * 8:(d + 1) * 8]
        beta = g_t[:, 16 + d * 8:16 + (d + 1) * 8]
        lnb = g_t[:, 32 + d * 8:32 + (d + 1) * 8]
        pb, r_pb = nbp()

        def mm0(e):
            e.matmul(pb[:, 0:8], lhsT=msk[:, d, :], rhs=gcol, start=True, stop=True)
            return e.matmul(pb[:, 8:16], lhsT=k.ones_f, rhs=gcol, start=True, stop=True)
        S.op("pe", mm0, reads=[r_msk, r_g, k.r_ones_f], writes=[r_pb])
        S.op("dve", lambda e: e.tensor_copy(out=X[:, :, 32:40], in_=pb[:, 0:8].unsqueeze(1).to_broadcast([128, 2, 8])), reads=[r_pb], writes=[r_X])
        S.op("dve", lambda e: e.tensor_copy(out=X[:, 1, 0:8], in_=pb[:, 0:8]), reads=[r_pb], writes=[r_X])
        S.op("dve", lambda e: e.tensor_tensor(out=X[:, 0, 0:8], in0=pb[:, 0:8], in1=lnb, op=ALU.add), reads=[r_pb, r_g], writes=[r_X])
        S.op("act", lambda e: e.activation(out=sm_t[:, 0, :], in_=pb[:, 0:8], func=AF.Exp), reads=[r_pb], writes=[r_sm])
        S.op("act", lambda e: e.activation(out=sm_t[:, 1, :], in_=pb[:, 8:16], func=AF.Exp), reads=[r_pb], writes=[r_sm])
        S.op("dve", lambda e: e.tensor_tensor(out=sm_t[:, 3, :], in0=pb[:, 8:16], in1=X[:, 1, 0:8], op=ALU.subtract), reads=[r_pb, r_X], writes=[r_sm])
        S.op("act", lambda e: e.activation(out=sm_t[:, 2, :], in_=sm_t[:, 3, :], func=AF.Exp), reads=[r_sm], writes=[r_sm])
        pt, r_pt = nbp()

        def tr0(e):
            e.transpose(out=pt[0:64, 0:128], in_=X[:, 0, :], identity=k.ident_f)
            return e.transpose(out=pt[0:64, 128:256], in_=X[:, 1, :], identity=k.ident_f)
        S.op("pe", tr0, reads=[r_X, k.r_ident_f], writes=[r_pt])
        S.op("act", lambda e: e.activation(out=L1[0:8, :], in_=pt[0:8, 0:128], func=AF.Copy), reads=[r_pt], writes=[r_L1])
        S.op("act", lambda e: e.activation(out=L2[0:8, :], in_=pt[0:8, 128:256], func=AF.Copy), reads=[r_pt], writes=[r_L2])
        S.op("dve", lambda e: e.tensor_tensor(out=R1[32:40, :, :], in0=EC[32:40, 1, :, :], in1=pt[32:40, 0:128].unsqueeze(1).to_broadcast([8, 8, 128]), op=ALU.mult),
             reads=[r_pt, r_EC], writes=[r_R1])
        S.op("dve", lambda e: e.tensor_tensor(out=R2[32:40, :, :], in0=EC[32:40, 0, :, :], in1=pt[32:40, 128:256].unsqueeze(1).to_broadcast([8, 8, 128]), op=ALU.mult),
             reads=[r_pt, r_EC], writes=[r_R2])
        kd_t, r_kd = kdec[b]
        S.op("pool", lambda e: e.tensor_tensor(out=kg, in0=ktm_t, in1=sm_t[:, 0, :].unsqueeze(2).to_broadcast([128, 8, 128]), op=ALU.mult),
             reads=[r_ktm, r_sm], writes=[r_kg])
        S.op("pool", lambda e: e.tensor_tensor(out=kd_t, in0=ktm_t, in1=sm_t[:, 2, :].unsqueeze(2).to_broadcast([128, 8, 128]), op=ALU.mult),
             reads=[r_ktm, r_sm], writes=[r_kd])
        yield
        def do_group(grp):
            hs = range(4 * grp, 4 * grp + 4)
            gs = slice(4 * grp, 4 * grp + 4)
            r_E1, r_M, r_Md, r_Mo1, r_Mo2, r_Mt, r_Tt, r_TtB = (RG[nm][grp] for nm in ("E1", "M", "Md", "Mo1", "Mo2", "Mt", "Tt", "TtB"))
            r_Ab = RG["Ab"][grp]
            rPP = [RG["PP0"][grp], RG["PP1"][grp]]
            rPT = [RG["PT0"][grp], RG["PT1"][grp]]
            Mo_list = ((Mo1r, r_Mo1), (Mo2r, r_Mo2))
            Md_list = ((Mdr, r_Md, 6), (Mo1r, r_Mo1, 7), (Mo2r, r_Mo2, 8))
            pk, r_pk = nbp()
            pd, r_pd = nbp()

            def mmk(e):
                for hh, h in enumerate(hs):
                    ins_ = e.matmul(b4(pk)[:, hh, :], lhsT=kT_t[:, h, :], rhs=kT_t[:, h, :], start=True, stop=True)
                return ins_
            S.op("pe", mmk, reads=[r_kT], writes=[r_pk])

            def mmd(e):
                for hh, h in enumerate(hs):
                    ins_ = e.matmul(b4(pd)[:, hh, :], lhsT=L1, rhs=R1[:, h, :], start=True, stop=True)
                return ins_
            S.op("pe", mmd, reads=[r_L1, r_R1], writes=[r_pd])
            S.op("dve", lambda e: e.scalar_tensor_tensor(out=E1[:, gs, :], in0=b4(pd), scalar=0.0, in1=msk[:, 2 + d, :].unsqueeze(1).to_broadcast([128, 4, 128]),
                                                         op0=ALU.min, op1=ALU.add), reads=[r_pd, r_msk], writes=[r_E1])
            S.op("act", lambda e: e.activation(out=E1[:, gs, :], in_=E1[:, gs, :], func=AF.Exp), reads=[r_E1], writes=[r_E1])
            S.op("dve", lambda e: e.tensor_tensor(out=M[:, gs, :], in0=b4(pk), in1=E1[:, gs, :], op=ALU.mult), reads=[r_pk, r_E1], writes=[r_M])
            for (dst_, rdst_, mi_) in Md_list:
                S.op("pool", lambda e, dst_=dst_, mi_=mi_: e.tensor_tensor(out=dst_[:, gs, :], in0=M[:, gs, :],
                                                                        in1=msk[:, mi_, :].unsqueeze(1).to_broadcast([128, 4, 128]), op=ALU.mult),
                     reads=[r_M, r_msk], writes=[rdst_])
            pm, r_pm = nbp(BF16)

            def trm(e):
                for hh, h in enumerate(hs):
                    ins_ = e.transpose(out=b4h(pm)[:, hh, :], in_=Md[:, h, :], identity=k.ident_b)
                return ins_
            S.op("pe", trm, reads=[r_Md, k.r_ident_b], writes=[r_pm])
            S.op("act", lambda e: e.activation(out=Mtr[:, gs, :], in_=b4h(pm), func=AF.Copy), reads=[r_pm], writes=[r_Mt])
            S.op("dve", lambda e: e.scalar_tensor_tensor(out=Ttr[:, gs, :], in0=b4h(pm), scalar=-1.0, in1=k.ident_f.unsqueeze(1).to_broadcast([128, 4, 128]),
                                                         op0=ALU.mult, op1=ALU.add), reads=[r_pm, k.r_ident_f], writes=[r_Tt])
            yield
            P_prev, rP_prev, Pt_prev, rPt_prev = Mdr, r_Md, Mtr, r_Mt
            for lvl in range(1, 1 + int(_os.environ.get('B_LEVELS', 4))):
                P_new, rP_new = PPr[lvl % 2], rPP[lvl % 2]
                Pt_new, rPt_new = PTr[lvl % 2], rPT[lvl % 2]
                pa, r_pa = nbp()

                def mma(e, P_prev=P_prev, Pt_prev=Pt_prev, pa=pa):
                    for hh, h in enumerate(hs):
                        ins_ = e.matmul(b4(pa)[:, hh, :], lhsT=Pt_prev[:, h, :], rhs=P_prev[:, h, :], start=True, stop=True)
                    return ins_
                S.op("pe", mma, reads=[rP_prev, rPt_prev], writes=[r_pa])
                S.op("act", lambda e, P_new=P_new, pa=pa: e.activation(out=P_new[:, gs, :], in_=b4(pa), func=AF.Copy), reads=[r_pa], writes=[rP_new])
                if lvl < int(_os.environ.get('B_LEVELS', 4)):
                    pbk, r_pbk = nbp()

                    def mmb(e, P_prev=P_prev, Pt_prev=Pt_prev, pbk=pbk):
                        for hh, h in enumerate(hs):
                            ins_ = e.matmul(b4(pbk)[:, hh, :], lhsT=P_prev[:, h, :], rhs=Pt_prev[:, h, :], start=True, stop=True)
                        return ins_
                    S.op("pe", mmb, reads=[rP_prev, rPt_prev], writes=[r_pbk])
                    S.op("act", lambda e, Pt_new=Pt_new, pbk=pbk: e.activation(out=Pt_new[:, gs, :], in_=b4(pbk), func=AF.Copy), reads=[r_pbk], writes=[rPt_new])
                pc, r_pc = nbp()

                def mmc(e, P_new=P_new, pc=pc):
                    for hh, h in enumerate(hs):
                        ins_ = e.matmul(b4(pc)[:, hh, :], lhsT=P_new[:, h, :], rhs=Ttr[:, h, :], start=True, stop=True)
                    return ins_
                S.op("pe", mmc, reads=[rP_new, r_Tt], writes=[r_pc])
                S.op("dve", lambda e, pc=pc: e.tensor_tensor(out=Ttr[:, gs, :], in0=Tt[:, gs, :], in1=b4(pc), op=ALU.add), reads=[r_Tt, r_pc], writes=[r_Tt])
                P_prev, rP_prev, Pt_prev, rPt_prev = P_new, rP_new, Pt_new, rPt_new
                yield
            for (Mo_, rMo_) in Mo_list:
                ptd, r_ptd = nbp(BF16)

                def trt(e, ptd=ptd):
                    for hh, h in enumerate(hs):
                        ins_ = e.transpose(out=b4h(ptd)[:, hh, :], in_=Tt[:, h, :], identity=k.ident_b)
                    return ins_
                S.op("pe", trt, reads=[r_Tt, k.r_ident_b], writes=[r_ptd])
                S.op("act", lambda e, ptd=ptd: e.activation(out=Mtr[:, gs, :], in_=b4h(ptd), func=AF.Copy), reads=[r_ptd], writes=[r_Mt])
                pa2, r_pa2 = nbp()

                def mma2(e, pa2=pa2, Mo_=Mo_):
                    for hh, h in enumerate(hs):
                        ins_ = e.matmul(b4(pa2)[:, hh, :], lhsT=Mo_[:, h, :], rhs=Ttr[:, h, :], start=True, stop=True)
                    return ins_
                S.op("pe", mma2, reads=[rMo_, r_Tt], writes=[r_pa2])
                S.op("act", lambda e, pa2=pa2: e.activation(out=E1r[:, gs, :], in_=b4(pa2), func=AF.Copy), reads=[r_pa2], writes=[r_Ab])
                pc2, r_pc2 = nbp()

                def mmc2(e, pc2=pc2):
                    for hh, h in enumerate(hs):
                        ins_ = e.matmul(b4(pc2)[:, hh, :], lhsT=Mtr[:, h, :], rhs=E1r[:, h, :], start=True, stop=True)
                    return ins_
                S.op("pe", mmc2, reads=[r_Mt, r_Ab], writes=[r_pc2])
                S.op("dve", lambda e, pc2=pc2: e.tensor_tensor(out=Ttr[:, gs, :], in0=Tt[:, gs, :], in1=b4(pc2), op=ALU.subtract), reads=[r_Tt, r_pc2], writes=[r_Tt])
                yield
            S.op("pool", lambda e: e.tensor_tensor(out=TtB[:, gs, :], in0=Tt[:, gs, :], in1=beta[:, gs].unsqueeze(2).to_broadcast([128, 4, 128]), op=ALU.mult),
                 reads=[r_Tt, r_g], writes=[r_TtB])
            pw, r_pw = nbp()

            def mmw(e):
                for hh, h in enumerate(hs):
                    ins_ = e.matmul(b4(pw)[:, hh, :], lhsT=kg[:, h, :], rhs=TtB[:, h, :], start=True, stop=True)
                return ins_
            S.op("pe", mmw, reads=[r_kg, r_TtB], writes=[r_pw])
            S.op("act", lambda e: e.activation(out=wT[b][0][:, gs, :], in_=b4(pw), func=AF.Copy), reads=[r_pw], writes=[wT_res[b][grp]])
            pu, r_pu = nbp()

            def mmu(e):
                for hh, h in enumerate(hs):
                    ins_ = e.matmul(b4(pu)[:, hh, :], lhsT=TtB[:, h, :], rhs=vtm_t[:, h, :], start=True, stop=True)
                return ins_
            S.op("pe", mmu, reads=[r_TtB, r_vtm], writes=[r_pu])
            S.op("act", lambda e: e.activation(out=u[b][0][:, gs, :], in_=b4(pu), func=AF.Copy), reads=[r_pu], writes=[u_res[b][grp]])
            yield
            if is_x:
                pq, r_pq = nbp()
                pd2, r_pd2 = nbp()

                def mmq(e):
                    for hh, h in enumerate(hs):
                        ins_ = e.matmul(b4(pq)[:, hh, :], lhsT=kT_t[:, h, :], rhs=qT_t[:, h, :], start=True, stop=True)
                    return ins_
                S.op("pe", mmq, reads=[r_kT, r_qT], writes=[r_pq])

                def mmd2(e):
                    for hh, h in enumerate(hs):
                        ins_ = e.matmul(b4(pd2)[:, hh, :], lhsT=L2, rhs=R2[:, h, :], start=True, stop=True)
                    return ins_
                S.op("pe", mmd2, reads=[r_L2, r_R2], writes=[r_pd2])
                S.op("dve", lambda e: e.scalar_tensor_tensor(out=E1[:, gs, :], in0=b4(pd2), scalar=0.0, in1=msk[:, 4 + d, :].unsqueeze(1).to_broadcast([128, 4, 128]),
                                                             op0=ALU.min, op1=ALU.add), reads=[r_pd2, r_msk], writes=[r_E1])
                S.op("act", lambda e: e.activation(out=E1[:, gs, :], in_=E1[:, gs, :], func=AF.Exp), reads=[r_E1], writes=[r_E1])
                S.op("dve", lambda e: e.tensor_tensor(out=qkT[b][0][:, gs, :], in0=b4(pq), in1=E1[:, gs, :], op=ALU.mult), reads=[r_pq, r_E1], writes=[qk_res[b][grp]])
                yield
        gens_ = [do_group(0), do_group(1)]
        while gens_:
            for g_ in list(gens_):
                try:
                    next(g_)
                    yield
                except StopIteration:
                    gens_.remove(g_)

    def seq(ui):
        d, ti = units[ui]
        b = ui % NB
        is_x = ti >= 2
        S32_t, r_S32 = S32[d]
        Sbf_t, r_Sbf = Sbf[d]
        sm_t, r_sm = sm[b]
        wT_t, u_t, qk_t = wT[b][0], u[b][0], qkT[b][0]
        qT_t, r_qT = qT[b]
        kd_t, r_kd = kdec[b]
        banks = [k.bank(4 + j) for j in range(4)]

        def grp_mm(bank2, lhs_fn, rhs_fn, reads):
            for grp in range(2):
                pb, r_pb = bank2[grp]

                def f(e, grp=grp, pb=pb):
                    for hh in range(4):
                        h = 4 * grp + hh
                        ins_ = e.matmul(b4(pb)[:, hh, :], lhsT=lhs_fn(h), rhs=rhs_fn(h), start=True, stop=True)
                    return ins_
                S.op("pe", f, reads=reads, writes=[r_pb])
        grp_mm(banks[0:2], lambda h: wT_t[:, h, :], lambda h: Sbf_t[:, h, :], wT_res[b] + [r_Sbf])
        S.op("pool", lambda e: e.tensor_tensor(out=S32_t, in0=S32_t, in1=sm_t[:, 1, :].unsqueeze(2).to_broadcast([128, 8, 128]), op=ALU.mult),
             reads=[r_S32, r_sm], writes=[r_S32])
        yield
        for grp in range(2):
            gs = slice(4 * grp, 4 * grp + 4)
            pb, r_pb = banks[grp]
            S.op("dve", lambda e, pb=pb, gs=gs: e.tensor_tensor(out=vnew[:, gs, :], in0=u_t[:, gs, :], in1=b4(pb), op=ALU.subtract),
                 reads=u_res[b] + [r_pb], writes=[r_vnew])
        yield
        if is_x:
            grp_mm(banks[2:4], lambda h: qT_t[:, h, :], lambda h: Sbf_t[:, h, :], [r_qT, r_Sbf])
            grp_mm(banks[0:2], lambda h: qk_t[:, h, :], lambda h: vnew[:, h, :], qk_res[b] + [r_vnew])
            yield
            o_tt, r_o = o_t[b]
            for grp in range(2):
                gs = slice(4 * grp, 4 * grp + 4)
                pbq, r_pbq = banks[2 + grp]
                pbc, r_pbc = banks[grp]
                S.op("dve", lambda e, pbq=pbq, gs=gs: e.tensor_tensor(out=tmpo[:, gs, :], in0=b4(pbq), in1=sm_t[:, 0, gs].unsqueeze(2).to_broadcast([128, 4, 128]), op=ALU.mult),
                     reads=[r_pbq, r_sm], writes=[r_tmpo])
                S.op("dve", lambda e, pbc=pbc, gs=gs, o_tt=o_tt: e.tensor_tensor(out=o_tt[:, gs, :], in0=tmpo[:, gs, :], in1=b4(pbc), op=ALU.add),
                     reads=[r_tmpo, r_pbc], writes=[r_o])
            xi = ti - 2
            k.store(o_s[d][0][xi * 128:(xi + 1) * 128, :], o_s[d][1], o_tt.rearrange("p h d -> p (h d)"), r_o)
            yield
        grp_mm(banks[2:4], lambda h: kd_t[:, h, :], lambda h: vnew[:, h, :], [r_kd, r_vnew])
        yield
        for grp in range(2):
            gs = slice(4 * grp, 4 * grp + 4)
            pb, r_pb = banks[2 + grp]
            S.op("dve", lambda e, pb=pb, gs=gs: e.tensor_tensor(out=S32_t[:, gs, :], in0=S32_t[:, gs, :], in1=b4(pb), op=ALU.add),
                 reads=[r_S32, r_pb], writes=[r_S32])
        S.op("act", lambda e: e.activation(out=Sbf_t, in_=S32_t, func=AF.Copy), reads=[r_S32], writes=[r_Sbf])
        yield

    import os as _os
    stop_after = int(_os.environ.get("B_UNITS", len(units)))
    pre_cut = int(_os.environ.get("B_PRE_CUT", 10000))
    do_seq = int(_os.environ.get("B_SEQ", 1))
    _pre = pre
    _seq = seq

    def pre(ui):
        for n_, _ in enumerate(_pre(ui)):
            if n_ + 1 >= pre_cut:
                return
            yield

    def seq(ui):
        if not do_seq:
            return
        yield from _seq(ui)
    loads(0)
    _drive(pre(0))
    for ui in range(stop_after):
        if ui + 1 < stop_after:
            loads(ui + 1)
            _drive(pre(ui + 1), seq(ui))
        else:
            _drive(seq(ui))
    k.b_state = (S32, Sbf)
    S.barrier()
    A.release()

def phase_c(k):
    S, A, nc, ins = k.S, k.A, k.nc, k.ins
    of_s, r_of_s = k.scr["of_s"]
    ob_s, r_ob_s = k.scr["ob_s"]
    zs_s, r_zs_s = k.scr["zs_s"]
    qmT_s, r_qmT_s = k.scr["qmT_s"]
    kmT_s, r_kmT_s = k.scr["kmT_s"]
    vm_s, r_vm_s = k.scr["vm_s"]
    yaT_s, r_yaT_s = k.scratch("yaT_s", [H, 128, TX], BF16)
    ybT_s, r_ybT_s = k.scratch("ybT_s", [H, 128, TX], BF16)
    A.mark()
    dng, r_dng = k.tile([128, 128], F32, "dng")
    k.load(dng, r_dng, ins["dng_rep"])
    NB = 2
    of_t = [k.tile([128, 8, 128], F32, "of") for _ in range(NB)]
    ob_t = [k.tile([128, 8, 128], F32, "ob") for _ in range(NB)]
    z_t = [k.tile([128, 8, 128], BF16, "z") for _ in range(NB)]
    sq, r_sq = k.tile([128, 8, 128], F32, "sq")
    ss8, r_ss8 = k.tile([128, 8], F32, "ss8")
    yb, r_yb = k.tile([128, 8, 128], BF16, "yb")
    yT = [k.tile([128, 8, 128], BF16, "yT") for _ in range(NB)]

    def c1_loads(xi):
        b = xi % NB
        rows = slice(xi * 128, (xi + 1) * 128)
        k.load(of_t[b][0], of_t[b][1], of_s[rows, :].rearrange("p (h d) -> p h d", h=8), r_of_s)
        k.load(ob_t[b][0], ob_t[b][1], ob_s[rows, :].rearrange("p (h d) -> p h d", h=8), r_ob_s, q="act")
        k.load(z_t[b][0], z_t[b][1], zs_s[rows, :].rearrange("p (h d) -> p h d", h=8), r_zs_s)

    c1_loads(0)
    for xi in range(32):
        b = xi % NB
        if xi + 1 < 32:
            c1_loads(xi + 1)
        o_, r_o = of_t[b]
        ob_, r_ob = ob_t[b]
        z_, r_z = z_t[b]
        S.op("dve", lambda e, o_=o_, ob_=ob_: e.tensor_tensor(out=o_, in0=o_, in1=ob_, op=ALU.add), reads=[r_o, r_ob], writes=[r_o])
        S.op("pool", lambda e, o_=o_: e.tensor_tensor(out=sq, in0=o_, in1=o_, op=ALU.mult), reads=[r_o], writes=[r_sq])
        S.op("dve", lambda e: e.tensor_reduce(out=ss8, in_=sq, axis=AX.X, op=ALU.add), reads=[r_sq], writes=[r_ss8])
        _rstd(k, ss8, r_ss8, 128, ss8, r_ss8)
        S.op("dve", lambda e, o_=o_: e.tensor_tensor(out=o_, in0=o_, in1=ss8.unsqueeze(2).to_broadcast([128, 8, 128]), op=ALU.mult),
             reads=[r_o, r_ss8], writes=[r_o])
        S.op("pool", lambda e, o_=o_: e.tensor_tensor(out=o_, in0=o_, in1=dng.unsqueeze(1).to_broadcast([128, 8, 128]), op=ALU.mult),
             reads=[r_o, r_dng], writes=[r_o])
        S.op("pool", lambda e, o_=o_, z_=z_: e.tensor_tensor(out=yb, in0=o_, in1=z_, op=ALU.mult), reads=[r_o, r_z], writes=[r_yb])
        pb, r_pb = _nb(k, BF16)
        pv = pb.rearrange("p (a b) -> p a b", b=128)

        def tr(e, pv=pv):
            for j in range(8):
                ins_ = e.transpose(out=pv[:, j, :], in_=yb[:, j, :], identity=k.ident_b)
            return ins_
        S.op("pe", tr, reads=[r_yb, k.r_ident_b], writes=[r_pb])
        yT_t, r_yT = yT[b]
        S.op("act", lambda e, pv=pv, yT_t=yT_t: e.activation(out=yT_t, in_=pv, func=AF.Copy), reads=[r_pb], writes=[r_yT])
        k.store(yaT_s[:, :, xi * 128:(xi + 1) * 128].rearrange("h d t -> d h t"), r_yaT_s, yT_t, r_yT)
    S.barrier()
    A.release()
    A.mark()
    Kn = [k.tile([128, T], BF16, "Kn") for _ in range(2)]
    Kr = [k.tile([64, T], BF16, "Kr") for _ in range(2)]
    Vh = [k.tile([128, NT, 128], BF16, "Vh") for _ in range(2)]
    Qn = [k.tile([128, 512], BF16, "Qn") for _ in range(2)]
    Qr = [k.tile([64, 512], BF16, "Qr") for _ in range(2)]
    NP = 6
    PT = [k.tile([128, 512], BF16, "PT") for _ in range(NP)]
    rinv, r_rinv = k.tile([128, 512], F32, "rinv")
    yo = [k.tile([128, 512], BF16, "yo") for _ in range(2)]
    vmv = vm_s.rearrange("(n p) c -> p n c", p=128)

    def head_loads(h):
        b = h % 2
        k.load(Kn[b][0], Kn[b][1], kmT_s[h, 0:128, :], r_kmT_s)
        k.load(Kr[b][0], Kr[b][1], kmT_s[h, 128:192, :], r_kmT_s, q="act")
        k.load(Vh[b][0], Vh[b][1], vmv[:, :, h * 128:(h + 1) * 128], r_vm_s)

    def q_loads(h, qg):
        b = (h * 8 + qg) % 2
        k.load(Qn[b][0], Qn[b][1], qmT_s[h, 0:128, qg * 512:(qg + 1) * 512], r_qmT_s)
        k.load(Qr[b][0], Qr[b][1], qmT_s[h, 128:192, qg * 512:(qg + 1) * 512], r_qmT_s, q="act")

    head_loads(0)
    q_loads(0, 0)
    sbank = [0]
    it = 0
    for h in range(H):
        if h + 1 < H:
            head_loads(h + 1)
        Kn_t, r_Kn = Kn[h % 2]
        Kr_t, r_Kr = Kr[h % 2]
        V_t, r_V = Vh[h % 2]
        for qg in range(8):
            gi = h * 8 + qg
            nxt = gi + 1
            if nxt < H * 8:
                q_loads(nxt // 8, nxt % 8)
            Qn_t, r_Qn = Qn[gi % 2]
            Qr_t, r_Qr = Qr[gi % 2]
            po, r_po = k.bank(4 + gi % 2)
            pr, r_pr = k.bank(6 + gi % 2)
            sb = {}

            def emit_s(kt):
                bnk = sbank[0]
                sbank[0] = (bnk + 1) % 4
                ps_, r_ps = k.bank(bnk)

                def f(e, ps_=ps_, kt=kt, Kn_t=Kn_t, Kr_t=Kr_t, Qn_t=Qn_t, Qr_t=Qr_t):
                    e.matmul(ps_, lhsT=Kn_t[:, kt * 128:(kt + 1) * 128], rhs=Qn_t, start=True, stop=False)
                    return e.matmul(ps_, lhsT=Kr_t[:, kt * 128:(kt + 1) * 128], rhs=Qr_t, start=False, stop=True)
                S.op("pe", f, reads=[r_Kn, r_Kr, r_Qn, r_Qr], writes=[r_ps])
                pt_, r_pt = PT[kt % NP]
                S.op("act", lambda e, ps_=ps_, pt_=pt_: e.activation(out=pt_, in_=ps_, func=AF.Exp), reads=[r_ps], writes=[r_pt])
                sb[kt] = (pt_, r_pt)

            def emit_pv(kt):
                pt_, r_pt = sb.pop(kt)

                def f(e, pt_=pt_, kt=kt, po=po, pr=pr, V_t=V_t):
                    e.matmul(po, lhsT=V_t[:, kt, :], rhs=pt_, start=(kt == 0), stop=(kt == NT - 1))
                    return e.matmul(pr, lhsT=k.ones_b, rhs=pt_, start=(kt == 0), stop=(kt == NT - 1))
                S.op("pe", f, reads=[r_V, r_pt, k.r_ones_b], writes=[r_po, r_pr])

            LOOK = 3
            for kt in range(min(LOOK, NT)):
                emit_s(kt)
            for kt in range(NT):
                if kt + LOOK < NT:
                    emit_s(kt + LOOK)
                emit_pv(kt)
            S.op("dve", lambda e, pr=pr: e.reciprocal(out=rinv, in_=pr), reads=[r_pr], writes=[r_rinv])
            yo_t, r_yo = yo[gi % 2]
            S.op("dve", lambda e, po=po, yo_t=yo_t: e.tensor_tensor(out=yo_t, in0=po, in1=rinv, op=ALU.mult), reads=[r_po, r_rinv], writes=[r_yo])
            k.store(ybT_s[h, :, qg * 512:(qg + 1) * 512], r_ybT_s, yo_t, r_yo)
    S.barrier()
    A.release()

def phase_d(k):
    S, A, nc, ins = k.S, k.A, k.nc, k.ins
    yaT_s, r_yaT_s = k.scr["yaT_s"]
    ybT_s, r_ybT_s = k.scr["ybT_s"]
    sgT_s, r_sgT_s = k.scr["sgT_s"]
    xmid_s, r_xmid_s = k.scratch("xmid_s", [TX, D], F32)
    h2_s, r_h2_s = k.scratch("h2_s", [TX, D], BF16)
    aff_s, r_aff_s = k.scratch("aff_s", [TX, 16], F32)
    affT_s, r_affT_s = k.scratch("affT_s", [16, TX], F32)
    A.mark()
    woa, r_woa = k.tile([128, 8, D], BF16, "woa")
    wob, r_wob = k.tile([128, 8, D], BF16, "wob")
    wo, r_wo = k.tile([128, 8, D], BF16, "wo")
    rw, r_rw = k.tile([128, 8, 16], F32, "rw")
    for (dst, rdst, nm) in ((woa, r_woa, "w_out_a"), (wob, r_wob, "w_out_b"), (wo, r_wo, "w_o")):
        src = ins[nm].rearrange("(kc p) n -> p kc n", p=128)
        for hf in range(2):
            k.wload(dst[:, 4 * hf:4 * hf + 4, :], rdst, src[:, 4 * hf:4 * hf + 4, :])
    k.load(rw, r_rw, ins["router_w"].rearrange("(kc p) n -> p kc n", p=128))
    NB = 2
    yaT = [k.tile([128, 8, 512], BF16, "yaT") for _ in range(NB)]
    ybT = [k.tile([128, 8, 512], BF16, "ybT") for _ in range(NB)]
    gA = [k.tile([128, 8, 512], BF16, "gA") for _ in range(NB)]
    gB = [k.tile([128, 8, 512], BF16, "gB") for _ in range(NB)]
    mg, r_mg = k.tile([128, 8, 512], BF16, "mg")
    t1 = [k.tile([128, 512], F32, "t1") for _ in range(2)]
    t2 = [k.tile([128, 512], F32, "t2") for _ in range(2)]
    xt = [k.tile([128, D], F32, "xt") for _ in range(NB)]
    xm = [k.tile([128, D], F32, "xm") for _ in range(NB)]
    junk, r_junk = k.tile([128, D], BF16, "junk")
    ssd = [k.tile([128, 8], F32, "ssd") for _ in range(NB)]
    h2f, r_h2f = k.tile([128, D], F32, "h2f")
    h2b = [k.tile([128, D], BF16, "h2b") for _ in range(NB)]
    h2T, r_h2T = k.tile([128, 8, 128], F32, "h2T")
    ex = [k.tile([128, 16], F32, "ex") for _ in range(NB)]
    affT = [k.tile([16, 128], F32, "affT") for _ in range(NB)]

    def g_loads(g):
        b = g % NB
        cols = slice(g * 512, (g + 1) * 512)
        k.load(yaT[b][0], yaT[b][1], yaT_s[:, :, cols].rearrange("h d t -> d h t"), r_yaT_s)
        k.load(ybT[b][0], ybT[b][1], ybT_s[:, :, cols].rearrange("h d t -> d h t"), r_ybT_s, q="act")
        k.load(gA[b][0], gA[b][1], sgT_s[0:8, :, cols].rearrange("h d t -> d h t"), r_sgT_s)
        k.load(gB[b][0], gB[b][1], sgT_s[8:16, :, cols].rearrange("h d t -> d h t"), r_sgT_s, q="act")

    g_loads(0)
    for g in range(8):
        b = g % NB
        if g + 1 < 8:
            g_loads(g + 1)
        ya_, r_ya = yaT[b]
        yb_, r_yb = ybT[b]
        gA_, r_gA = gA[b]
        gB_, r_gB = gB[b]
        for oc in range(8):
            pa, r_pa = _nb(k)
            pbk, r_pbk = _nb(k)

            def mma(e, pa=pa, oc=oc, ya_=ya_):
                for kc in range(8):
                    ins_ = e.matmul(pa, lhsT=woa[:, kc, oc * 128:(oc + 1) * 128], rhs=ya_[:, kc, :], start=(kc == 0), stop=(kc == 7))
                return ins_
            S.op("pe", mma, reads=[r_woa, r_ya], writes=[r_pa])

            def mmb(e, pbk=pbk, oc=oc, yb_=yb_):
                for kc in range(8):
                    ins_ = e.matmul(pbk, lhsT=wob[:, kc, oc * 128:(oc + 1) * 128], rhs=yb_[:, kc, :], start=(kc == 0), stop=(kc == 7))
                return ins_
            S.op("pe", mmb, reads=[r_wob, r_yb], writes=[r_pbk])
            t1_, r_t1 = t1[oc % 2]
            t2_, r_t2 = t2[oc % 2]
            S.op("dve", lambda e, pa=pa, oc=oc, t1_=t1_, gA_=gA_: e.tensor_tensor(out=t1_, in0=pa, in1=gA_[:, oc, :], op=ALU.mult), reads=[r_pa, r_gA], writes=[r_t1])
            S.op("dve", lambda e, pbk=pbk, oc=oc, t2_=t2_, gB_=gB_: e.tensor_tensor(out=t2_, in0=pbk, in1=gB_[:, oc, :], op=ALU.mult), reads=[r_pbk, r_gB], writes=[r_t2])
            S.op("pool", lambda e, oc=oc, t1_=t1_, t2_=t2_: e.tensor_tensor(out=mg[:, oc, :], in0=t1_, in1=t2_, op=ALU.add), reads=[r_t1, r_t2], writes=[r_mg])
        for tt in range(4):
            ti = g * 4 + tt
            tb = ti % NB
            rows = slice(ti * 128, (ti + 1) * 128)
            x_, r_x = xt[tb]
            xm_, r_xm = xm[tb]
            ss_, r_ss = ssd[tb]
            k.load(x_, r_x, ins["x"][rows, :])
            for hf in range(2):
                pm, r_pm = _nb(k)

                def mmo(e, pm=pm, hf=hf, tt=tt):
                    for kc in range(8):
                        ins_ = e.matmul(pm, lhsT=mg[:, kc, tt * 128:(tt + 1) * 128], rhs=wo[:, kc, hf * 512:(hf + 1) * 512], start=(kc == 0), stop=(kc == 7))
                    return ins_
                S.op("pe", mmo, reads=[r_mg, r_wo], writes=[r_pm])
                S.op("dve", lambda e, pm=pm, hf=hf, xm_=xm_: e.tensor_tensor(out=xm_[:, hf * 512:(hf + 1) * 512], in0=pm, in1=k.gate1_row[:, hf * 512:(hf + 1) * 512], op=ALU.mult),
                     reads=[r_pm, k.r_gate1], writes=[r_xm])
            S.op("pool", lambda e, xm_=xm_, x_=x_: e.tensor_tensor(out=xm_, in0=xm_, in1=x_, op=ALU.add), reads=[r_xm, r_x], writes=[r_xm])
            k.store(xmid_s[rows, :], r_xmid_s, xm_, r_xm)
            k.store(k.out[rows, :], k.out_res, xm_, r_xm)
            S.op("act", lambda e, xm_=xm_, ss_=ss_: e.activation(out=junk, in_=xm_, func=AF.Square, accum_out=ss_[:, 0:1]), reads=[r_xm], writes=[r_junk, r_ss])
            _rstd(k, ss_[:, 0:1], r_ss, D, ss_[:, 1:2], r_ss)
            S.op("act", lambda e, xm_=xm_, ss_=ss_: e.activation(out=h2f, in_=xm_, func=AF.Copy, scale=ss_[:, 1:2]), reads=[r_xm, r_ss], writes=[r_h2f])
            S.op("pool", lambda e: e.tensor_tensor(out=h2f, in0=h2f, in1=k.s2_row, op=ALU.mult), reads=[r_h2f, k.r_s2row], writes=[r_h2f])
            S.op("pool", lambda e: e.tensor_tensor(out=h2f, in0=h2f, in1=k.shift2_row, op=ALU.add), reads=[r_h2f, k.r_shift2], writes=[r_h2f])
            h2b_, r_h2b = h2b[tb]
            S.op("act", lambda e, h2b_=h2b_: e.activation(out=h2b_, in_=h2f, func=AF.Copy), reads=[r_h2f], writes=[r_h2b])
            k.store(h2_s[rows, :], r_h2_s, h2b_, r_h2b)
            for hf in range(2):
                pt, r_pt = _nb(k)
                pv = pt.rearrange("p (a b) -> p a b", b=128)

                def trh(e, pv=pv, hf=hf):
                    for j in range(4):
                        ins_ = e.transpose(out=pv[:, j, :], in_=h2f[:, (hf * 4 + j) * 128:(hf * 4 + j + 1) * 128], identity=k.ident_f)
                    return ins_
                S.op("pe", trh, reads=[r_h2f, k.r_ident_f], writes=[r_pt])
                if hf == 0:
                    S.op("act", lambda e, pv=pv: e.activation(out=h2T[:, 0:4, :], in_=pv, func=AF.Copy), reads=[r_pt], writes=[r_h2T])
                else:
                    S.op("dve", lambda e, pv=pv: e.tensor_copy(out=h2T[:, 4:8, :], in_=pv), reads=[r_pt], writes=[r_h2T])
            pl, r_pl = _nb(k)

            def mml(e, pl=pl):
                for kc in range(8):
                    ins_ = e.matmul(pl[:, 0:16], lhsT=h2T[:, kc, :], rhs=rw[:, kc, :], start=(kc == 0), stop=(kc == 7))
                return ins_
            S.op("pe", mml, reads=[r_h2T, r_rw], writes=[r_pl])
            ex_, r_ex = ex[tb]
            S.op("dve", lambda e, pl=pl, ss_=ss_: e.tensor_reduce(out=ss_[:, 2:3], in_=pl[:, 0:16], axis=AX.X, op=ALU.max), reads=[r_pl], writes=[r_ss])
            S.op("dve", lambda e, ss_=ss_: e.tensor_scalar(out=ss_[:, 3:4], in0=ss_[:, 2:3], scalar1=-1.0, scalar2=None, op0=ALU.mult), reads=[r_ss], writes=[r_ss])
            S.op("act", lambda e, pl=pl, ss_=ss_, ex_=ex_: e.activation(out=ex_, in_=pl[:, 0:16], func=AF.Exp, bias=ss_[:, 3:4], accum_out=ss_[:, 4:5]),
                 reads=[r_pl, r_ss], writes=[r_ex, r_ss])
            S.op("dve", lambda e, ss_=ss_: e.reciprocal(out=ss_[:, 5:6], in_=ss_[:, 4:5]), reads=[r_ss], writes=[r_ss])
            S.op("dve", lambda e, ss_=ss_, ex_=ex_: e.tensor_scalar(out=ex_, in0=ex_, scalar1=ss_[:, 5:6], scalar2=None, op0=ALU.mult), reads=[r_ex, r_ss], writes=[r_ex])
            k.store(aff_s[rows, :], r_aff_s, ex_, r_ex)
            pt2, r_pt2 = _nb(k)
            S.op("pe", lambda e, pt2=pt2, ex_=ex_: e.transpose(out=pt2[0:16, 0:128], in_=ex_, identity=k.ident_f), reads=[r_ex, k.r_ident_f], writes=[r_pt2])
            aT_, r_aT = affT[tb]
            S.op("act", lambda e, pt2=pt2, aT_=aT_: e.activation(out=aT_, in_=pt2[0:16, 0:128], func=AF.Copy), reads=[r_pt2], writes=[r_aT])
            k.store(affT_s[:, rows], r_affT_s, aT_, r_aT)
    S.barrier()
    A.release()

NE = 16
CAP = 512
FF = 1408
NFC = 11


def phase_e(k):
    S, A, nc, ins = k.S, k.A, k.nc, k.ins
    aff_s, r_aff_s = k.scr["aff_s"]
    affT_s, r_affT_s = k.scr["affT_s"]
    h2_s, r_h2_s = k.scr["h2_s"]
    xmid_s, r_xmid_s = k.scr["xmid_s"]
    posmT_s, r_posmT_s = k.scratch("posmT_s", [NE, TX], F32)
    gc_s, r_gc_s = k.scratch("gc_s", [NE, 128, 4], F32)
    idx_s, r_idx_s = k.scratch("idx_s", [NE, 128, 4], I32)
    A.off = k.off_after_gate2
    A.mark()
    cst, r_cst = k.tile([128, 1024], F32, "cst")
    k.load(cst, r_cst, ins["consts"])
    blk, r_blk = k.tile([128, 128], F32, "blk")
    k.load(blk, r_blk, ins["moe_blk"])
    sel8, r_sel8 = k.tile([128, 16], F32, "sel8")
    k.load(sel8, r_sel8, ins["moe_sel8"])
    tris, r_tris = k.tile([128, 128], BF16, "tris")
    k.wload(tris, r_tris, ins["moe_tris"])
    iota_c = cst[:, 0:512]
    A.mark()
    A8, r_A8 = k.tile([128, 512], F32, "A8")
    k.load(A8, r_A8, affT_s.rearrange("e (s t) -> (e s) t", s=8), r_affT_s)
    junk, r_junk = k.tile([128, 512], F32, "junk")
    sc, r_sc = k.tile([128, 16], F32, "sc")
    S.op("pool", lambda e: e.memset(sc, 0.0), writes=[r_sc])
    S.op("pool", lambda e: e.memset(sc[:, 1:2], 1.0), reads=[r_sc], writes=[r_sc])
    for it in range(30):
        S.op("dve", lambda e: e.tensor_tensor(out=sc[:, 2:3], in0=sc[:, 0:1], in1=sc[:, 1:2], op=ALU.add), reads=[r_sc], writes=[r_sc])
        S.op("dve", lambda e: e.tensor_scalar(out=sc[:, 2:3], in0=sc[:, 2:3], scalar1=0.5, scalar2=None, op0=ALU.mult), reads=[r_sc], writes=[r_sc])
        S.op("dve", lambda e: e.tensor_scalar(out=junk, in0=A8, scalar1=sc[:, 2:3], scalar2=0.0, op0=ALU.is_ge, op1=ALU.add, accum_out=sc[:, 3:4]),
             reads=[r_A8, r_sc], writes=[r_junk, r_sc])
        pb, r_pb = _nb(k)
        S.op("pe", lambda e, pb=pb: e.matmul(pb[:, 0:1], lhsT=blk, rhs=sc[:, 3:4], start=True, stop=True), reads=[r_blk, r_sc], writes=[r_pb])
        S.op("dve", lambda e, pb=pb: e.tensor_scalar(out=sc[:, 4:5], in0=pb[:, 0:1], scalar1=CAP - 0.5, scalar2=None, op0=ALU.is_ge), reads=[r_pb], writes=[r_sc])
        S.op("dve", lambda e: e.tensor_scalar(out=sc[:, 5:6], in0=sc[:, 4:5], scalar1=-1.0, scalar2=1.0, op0=ALU.mult, op1=ALU.add), reads=[r_sc], writes=[r_sc])
        S.op("dve", lambda e: e.tensor_tensor(out=sc[:, 6:7], in0=sc[:, 2:3], in1=sc[:, 0:1], op=ALU.subtract), reads=[r_sc], writes=[r_sc])
        S.op("dve", lambda e: e.tensor_tensor(out=sc[:, 7:8], in0=sc[:, 1:2], in1=sc[:, 2:3], op=ALU.subtract), reads=[r_sc], writes=[r_sc])
        S.op("dve", lambda e: e.scalar_tensor_tensor(out=sc[:, 0:1], in0=sc[:, 6:7], scalar=sc[:, 4:5], in1=sc[:, 0:1], op0=ALU.mult, op1=ALU.add), reads=[r_sc], writes=[r_sc])
        S.op("dve", lambda e: e.scalar_tensor_tensor(out=sc[:, 1:2], in0=sc[:, 7:8], scalar=sc[:, 4:5], in1=sc[:, 2:3], op0=ALU.mult, op1=ALU.add), reads=[r_sc], writes=[r_sc])
        S.op("dve", lambda e: e.memset(sc[:, 3:4], 0.0), reads=[r_sc], writes=[r_sc])
    thrrep, r_thrrep = k.tile([128, 128], F32, "thrrep")
    S.op("dve", lambda e: e.tensor_copy(out=thrrep, in_=sc[:, 0:1].to_broadcast([128, 128])), reads=[r_sc], writes=[r_thrrep])
    pb, r_pb = _nb(k)
    S.op("pe", lambda e, pb=pb: e.matmul(pb[:, 0:16], lhsT=thrrep, rhs=sel8, start=True, stop=True), reads=[r_thrrep, r_sel8], writes=[r_pb])
    thr_row, r_thr = k.tile([128, 16], F32, "thr_row")
    S.op("act", lambda e, pb=pb: e.activation(out=thr_row, in_=pb[:, 0:16], func=AF.Copy), reads=[r_pb], writes=[r_thr])
    import os as _os
    if _os.environ.get("E_DBG"):
        k.dump("sc", sc, r_sc, [128, 16])
        k.dump("thr_row", thr_row, r_thr, [128, 16])
        k.dump("A8", A8, r_A8, [128, 512])
        S.barrier()
        A.release()
        A.release()
        return
    aff, r_aff = k.tile([128, 32, 16], F32, "aff")
    k.load(aff, r_aff, aff_s.rearrange("(n p) e -> p n e", p=128), r_aff_s)
    maskf, r_maskf = k.tile([128, 32, 16], F32, "maskf")
    maskb, r_maskb = k.tile([128, 32, 16], BF16, "maskb")
    posm, r_posm = k.tile([128, 32, 16], F32, "posm")
    parts, r_parts = k.tile([128, 32, 16, 5], BF16, "parts")
    tokp, r_tokp = k.tile([128, 32, 2], F32, "tokp")
    k.load(tokp, r_tokp, ins["moe_tok"])
    rem, r_rem = k.tile([128, 32, 16], F32, "rem")
    S.op("dve", lambda e: e.tensor_tensor(out=maskf, in0=aff, in1=thr_row.unsqueeze(1).to_broadcast([128, 32, 16]), op=ALU.is_ge),
         reads=[r_aff, r_thr], writes=[r_maskf])
    S.op("act", lambda e: e.activation(out=maskb, in_=maskf, func=AF.Copy), reads=[r_maskf], writes=[r_maskb])
    pp, r_pp = _nb(k)
    ppv = pp.rearrange("p (n e) -> p n e", e=16)

    def mmpos(e):
        for n in range(32):
            for m in range(n):
                e.matmul(ppv[:, n, :], lhsT=k.ones_b, rhs=maskb[:, m, :], start=(m == 0), stop=False)
            ins_ = e.matmul(ppv[:, n, :], lhsT=tris, rhs=maskb[:, n, :], start=(n == 0), stop=True)
        return ins_
    S.op("pe", mmpos, reads=[r_maskb, k.r_ones_b, r_tris], writes=[r_pp])
    S.op("dve", lambda e: e.scalar_tensor_tensor(out=posm, in0=ppv, scalar=1.0, in1=maskf, op0=ALU.add, op1=ALU.mult), reads=[r_pp, r_maskf], writes=[r_posm])
    S.op("dve", lambda e: e.tensor_scalar(out=posm, in0=posm, scalar1=-1.0, scalar2=None, op0=ALU.add), reads=[r_posm], writes=[r_posm])
    S.op("act", lambda e: e.activation(out=parts[:, :, :, 0], in_=aff, func=AF.Copy), reads=[r_aff], writes=[r_parts])
    S.op("dve", lambda e: e.tensor_tensor(out=rem, in0=aff, in1=parts[:, :, :, 0], op=ALU.subtract), reads=[r_aff, r_parts], writes=[r_rem])
    S.op("act", lambda e: e.activation(out=parts[:, :, :, 1], in_=rem, func=AF.Copy), reads=[r_rem], writes=[r_parts])
    S.op("dve", lambda e: e.tensor_tensor(out=rem, in0=rem, in1=parts[:, :, :, 1], op=ALU.subtract), reads=[r_rem, r_parts], writes=[r_rem])
    S.op("act", lambda e: e.activation(out=parts[:, :, :, 2], in_=rem, func=AF.Copy), reads=[r_rem], writes=[r_parts])
    S.op("dve", lambda e: e.tensor_copy(out=parts[:, :, :, 3:5], in_=tokp.unsqueeze(2).to_broadcast([128, 32, 16, 2])), reads=[r_tokp, r_parts], writes=[r_parts])
    pmTs = [k.tile([16, 512], F32, "pmT") for _ in range(2)]
    for g in range(8):
        pt, r_pt = _nb(k)

        def trp(e, pt=pt, g=g):
            for j in range(4):
                ins_ = e.transpose(out=pt[0:16, j * 128:(j + 1) * 128], in_=posm[:, g * 4 + j, :], identity=k.ident_f)
            return ins_
        S.op("pe", trp, reads=[r_posm, k.r_ident_f], writes=[r_pt])
        pmT, r_pmT = pmTs[g % 2]
        S.op("act", lambda e, pt=pt, pmT=pmT: e.activation(out=pmT, in_=pt[0:16, :], func=AF.Copy), reads=[r_pt], writes=[r_pmT])
        k.store(posmT_s[:, g * 512:(g + 1) * 512], r_posmT_s, pmT, r_pmT)
    if _os.environ.get("E_STOP") == "e1":
        S.barrier(); A.release(); A.release(); return
    Sel = [k.tile([128, 32, CAP], BF16, "Sel") for _ in range(2)]
    Sel_res = [(Res("sel0"), Res("sel1")) for _ in range(2)]
    gcs = [k.tile([128, 4], F32, "gcs") for _ in range(2)]
    idf = [k.tile([128, 4], F32, "idf") for _ in range(2)]
    idi = [k.tile([128, 4], I32, "idi") for _ in range(2)]
    for ex in range(NE):
        Sel_t, _ = Sel[ex % 2]
        r_Sel0, r_Sel1 = Sel_res[ex % 2]
        gcs_t, r_gcs = gcs[ex % 2]
        idf_t, r_idf = idf[ex % 2]
        idi_t, r_idi = idi[ex % 2]

        def bsel(e, Sel_t=Sel_t, ex=ex, par=0):
            for n in range(par, 32, 2):
                ins_ = e.tensor_scalar(out=Sel_t[:, n, :], in0=iota_c, scalar1=posm[:, n, ex:ex + 1], scalar2=None, op0=ALU.is_equal)
            return ins_
        if _os.environ.get("E_X") != "nodve":
            S.op("dve", lambda e, f=bsel: f(e, par=0), reads=[r_cst, r_posm], writes=[r_Sel0])
        S.op("dve", lambda e, f=bsel: f(e, par=1), reads=[r_cst, r_posm], writes=[r_Sel1])
        pq, r_pq = _nb(k)

        def mmgate(e, pq=pq, Sel_t=Sel_t, ex=ex):
            for cc in range(4):
                for n in range(32):
                    ins_ = e.matmul(pq[:, cc * 8:cc * 8 + 5], lhsT=Sel_t[:, n, cc * 128:(cc + 1) * 128], rhs=parts[:, n, ex, :], start=(n == 0), stop=(n == 31))
            return ins_
        if _os.environ.get("E_X") != "nomm":
            S.op("pe", mmgate, reads=[r_Sel0, r_Sel1, r_parts], writes=[r_pq])
        pq3 = pq[:, 0:32].rearrange("p (a b) -> p a b", b=8)
        S.op("dve", lambda e, pq3=pq3, gcs_t=gcs_t: e.tensor_reduce(out=gcs_t, in_=pq3[:, :, 0:3], axis=AX.X, op=ALU.add), reads=[r_pq], writes=[r_gcs])
        S.op("dve", lambda e, pq3=pq3, idf_t=idf_t: e.tensor_scalar(out=idf_t, in0=pq3[:, :, 3], scalar1=128.0, scalar2=None, op0=ALU.mult), reads=[r_pq], writes=[r_idf])
        S.op("dve", lambda e, pq3=pq3, idf_t=idf_t: e.tensor_tensor(out=idf_t, in0=idf_t, in1=pq3[:, :, 4], op=ALU.add), reads=[r_pq, r_idf], writes=[r_idf])
        S.op("dve", lambda e, idf_t=idf_t, idi_t=idi_t: e.tensor_copy(out=idi_t, in_=idf_t), reads=[r_idf], writes=[r_idi])
        k.store(gc_s[ex], r_gc_s, gcs_t, r_gcs)
        k.store(idx_s[ex], r_idx_s, idi_t, r_idi)
    S.barrier()
    A.release()
    if _os.environ.get("E_STOP") == "ea1":
        A.release(); return
    A.mark()
    U32 = mybir.dt.uint32
    ig = S.pool("ig", 8)
    wg = [k.tile([128, 8, FF], BF16, "wg") for _ in range(2)]
    wu = [k.tile([128, 8, FF], BF16, "wu") for _ in range(2)]
    wd = [k.tile([128, NFC, D], BF16, "wd") for _ in range(1)]
    xg = [k.tile([128, 4, D], BF16, "xg") for _ in range(2)]
    idx2 = [k.tile([128, 4], I32, "idx2") for _ in range(2)]
    gc2 = [k.tile([128, 4], F32, "gc2") for _ in range(2)]
    xeT_t, r_xeT = k.tile([128, 8, CAP], BF16, "xeT")
    hid, r_hid = k.tile([128, NFC, CAP], BF16, "hid")
    sg = [k.tile([128, CAP], F32, "sg") for _ in range(2)]
    yef = [k.tile([128, D], F32, "yef") for _ in range(4)]
    r_outacc = Res("outacc")
    h2_rows = h2_s

    def w_loads(ex):
        b = ex % 2
        srcg = ins["w_gate"][ex].rearrange("(kc p) f -> p kc f", p=128)
        srcu = ins["w_up"][ex].rearrange("(kc p) f -> p kc f", p=128)
        for j in range(4):
            k.wload(wg[b][0][:, 2 * j:2 * j + 2, :], wg[b][1], srcg[:, 2 * j:2 * j + 2, :])
        for j in range(4):
            k.wload(wu[b][0][:, 2 * j:2 * j + 2, :], wu[b][1], srcu[:, 2 * j:2 * j + 2, :])

    def wd_loads(ex):
        srcd = ins["w_down"][ex].rearrange("(fc p) d -> p fc d", p=128)
        for (a0, a1) in ((0, 3), (3, 6), (6, 9), (9, 11)):
            k.wload(wd[0][0][:, a0:a1, :], wd[0][1], srcd[:, a0:a1, :])

    def g_loads(ex):
        b = ex % 2
        k.load(idx2[b][0], idx2[b][1], idx_s[ex], r_idx_s)
        k.load(gc2[b][0], gc2[b][1], gc_s[ex], r_gc_s, q="act")
        for cc in range(4):
            S.dma("pool", ig, lambda e, b=b, cc=cc: e.indirect_dma_start(out=xg[b][0][:, cc, :], out_offset=None, in_=h2_rows,
                                                                       in_offset=bass.IndirectOffsetOnAxis(idx2[b][0].bitcast(U32)[:, cc:cc + 1], 0)),
                  reads=[idx2[b][1], r_h2_s], writes=[xg[b][1]])

    g_loads(0)
    w_loads(0)
    prev_sc = []
    for ex in range(NE):
        b = ex % 2
        wd_loads(ex)
        if ex + 1 < NE:
            g_loads(ex + 1)
            w_loads(ex + 1)
        wg_t, r_wg = wg[b]
        wu_t, r_wu = wu[b]
        wd_t, r_wd = wd[0]
        xg_t, r_xg = xg[b]
        gc_t, r_gc = gc2[b]
        id_t, r_id = idx2[b]
        for kc in range(8):
            pt, r_pt = _nb(k, BF16)

            def trx(e, pt=pt, kc=kc, xg_t=xg_t):
                for cc in range(4):
                    ins_ = e.transpose(out=pt[:, cc * 128:(cc + 1) * 128], in_=xg_t[:, cc, kc * 128:(kc + 1) * 128], identity=k.ident_b)
                return ins_
            S.op("pe", trx, reads=[r_xg, k.r_ident_b], writes=[r_pt])
            if kc % 2 == 0:
                S.op("act", lambda e, pt=pt, kc=kc: e.activation(out=xeT_t[:, kc, :], in_=pt[:, 0:512], func=AF.Copy), reads=[r_pt], writes=[r_xeT])
            else:
                S.op("dve", lambda e, pt=pt, kc=kc: e.tensor_copy(out=xeT_t[:, kc, :], in_=pt[:, 0:512]), reads=[r_pt], writes=[r_xeT])
        for fc in range(NFC):
            pg, r_pg = _nb(k)
            pu, r_pu = _nb(k)

            def mmG(e, pg=pg, fc=fc, wg_t=wg_t):
                for kc in range(8):
                    ins_ = e.matmul(pg, lhsT=wg_t[:, kc, fc * 128:(fc + 1) * 128], rhs=xeT_t[:, kc, :], start=(kc == 0), stop=(kc == 7))
                return ins_
            S.op("pe", mmG, reads=[r_wg, r_xeT], writes=[r_pg])

            def mmU(e, pu=pu, fc=fc, wu_t=wu_t):
                for kc in range(8):
                    ins_ = e.matmul(pu, lhsT=wu_t[:, kc, fc * 128:(fc + 1) * 128], rhs=xeT_t[:, kc, :], start=(kc == 0), stop=(kc == 7))
                return ins_
            S.op("pe", mmU, reads=[r_wu, r_xeT], writes=[r_pu])
            sg_t, r_sg = sg[fc % 2]
            S.op("act", lambda e, pg=pg, sg_t=sg_t: e.activation(out=sg_t, in_=pg, func=AF.Silu), reads=[r_pg], writes=[r_sg])
            S.op("dve", lambda e, pu=pu, sg_t=sg_t, fc=fc: e.tensor_tensor(out=hid[:, fc, :], in0=pu, in1=sg_t, op=ALU.mult), reads=[r_pu, r_sg], writes=[r_hid])
        cur_sc = []
        for cc in range(4):
            y_t, r_y = yef[cc]
            for hf in range(2):
                pd, r_pd = _nb(k)

                def mmD(e, pd=pd, cc=cc, hf=hf):
                    for fc in range(NFC):
                        ins_ = e.matmul(pd, lhsT=hid[:, fc, cc * 128:(cc + 1) * 128], rhs=wd_t[:, fc, hf * 512:(hf + 1) * 512], start=(fc == 0), stop=(fc == NFC - 1))
                    return ins_
                S.op("pe", mmD, reads=[r_hid, r_wd], writes=[r_pd])
                S.op("act", lambda e, pd=pd, cc=cc, hf=hf, y_t=y_t, gc_t=gc_t: e.activation(out=y_t[:, hf * 512:(hf + 1) * 512], in_=pd, func=AF.Copy, scale=gc_t[:, cc:cc + 1]),
                     reads=[r_pd, r_gc], writes=[r_y])
            S.op("pool", lambda e, y_t=y_t: e.tensor_tensor(out=y_t, in0=y_t, in1=k.gate2_row, op=ALU.mult), reads=[r_y, k.r_gate2], writes=[r_y])
            tok_ = S.dma("pool", ig, lambda e, y_t=y_t, id_t=id_t, cc=cc: e.indirect_dma_start(out=k.out, out_offset=bass.IndirectOffsetOnAxis(id_t.bitcast(U32)[:, cc:cc + 1], 0),
                                                                                          in_=y_t, in_offset=None, compute_op=ALU.add),
                         reads=[r_y, r_id, k.out_res], writes=[], extra=prev_sc)
            cur_sc.append(tok_)
            if cc == 3:
                prev_sc = cur_sc
    S.barrier()
    A.release()
    A.release()

def _rope_tables():
    rows, gw = 64, 64
    row = np.repeat(np.arange(rows), gw).astype(np.float32)
    col = np.tile(np.arange(gw), rows).astype(np.float32)
    n_freq = 16
    inv_freq = (10000.0 ** (-np.arange(n_freq, dtype=np.float32) / n_freq)).astype(np.float32)
    ang_r = row[:, None] * inv_freq
    ang_c = col[:, None] * inv_freq
    ang = np.concatenate([ang_r, ang_r, ang_c, ang_c], axis=-1).astype(np.float32)
    cos = np.cos(ang).astype(np.float32)
    sin = np.sin(ang).astype(np.float32)
    sgn = np.concatenate([-np.ones(16), np.ones(16), -np.ones(16), np.ones(16)]).astype(np.float32)
    return cos, (sin * sgn).astype(np.float32)


def prep_shared(inp):
    f = np.float32
    sh = {}
    sh["ada_w"] = np.ascontiguousarray(inp["ada_w"][0])
    sh["ada_b_row"] = np.ascontiguousarray(inp["ada_b"][0][None, :])
    sh["ada_bT"] = np.ascontiguousarray(inp["ada_b"][0].reshape(48, 128).T)
    sh["g1T"] = np.ascontiguousarray(inp["norm1_g"][0].reshape(8, 128).T)
    sh["g2T"] = np.ascontiguousarray(inp["norm2_g"][0].reshape(8, 128).T)
    sh["g2_rep"] = np.ascontiguousarray(np.broadcast_to(inp["norm2_g"][0].reshape(1, 1024), (128, 1024)))
    sh["w_in"] = np.ascontiguousarray(inp["w_in"][0])
    sh["convT"] = np.ascontiguousarray(inp["conv_w"][0].T.reshape(24, 128, 5).transpose(1, 0, 2))
    sh["alog_rep"] = np.ascontiguousarray(np.broadcast_to(inp["a_log"][0].reshape(1, 16), (128, 16)))
    sh["dtb_rep"] = np.ascontiguousarray(np.broadcast_to(inp["dt_bias"][0].reshape(1, 16), (128, 16)))
    sh["dng_rep"] = np.ascontiguousarray(np.broadcast_to(inp["dn_norm_g"][0].reshape(1, 128), (128, 128)))
    sh["gqaT"] = np.ascontiguousarray(inp["q_a_norm_g"][0].reshape(3, 128).T)
    sh["w_uq"] = np.ascontiguousarray(inp["w_uq"][0])
    sh["gkvaT"] = np.ascontiguousarray(inp["kv_a_norm_g"][0].reshape(2, 128).T)
    sh["w_ukv"] = np.ascontiguousarray(inp["w_ukv"][0])
    sh["gq_rep"] = np.ascontiguousarray(np.broadcast_to(inp["q_norm_g"][0].reshape(1, 192), (128, 192)))
    sh["gk_rep"] = np.ascontiguousarray(np.broadcast_to(inp["k_norm_g"][0].reshape(1, 192), (128, 192)))
    sh["w_out_a"] = np.ascontiguousarray(inp["w_out_a"][0])
    sh["w_out_b"] = np.ascontiguousarray(inp["w_out_b"][0])
    sh["w_o"] = np.ascontiguousarray(inp["w_o"][0])
    sh["router_w"] = np.ascontiguousarray(inp["router_w"][0])
    sh["w_gate"] = np.ascontiguousarray(inp["w_gate"][0])
    sh["w_up"] = np.ascontiguousarray(inp["w_up"][0])
    sh["w_down"] = np.ascontiguousarray(inp["w_down"][0])
    cos, sinS = _rope_tables()
    sh["rope_cs"] = np.ascontiguousarray(np.concatenate([cos, sinS], axis=1))
    sh["ident"] = np.eye(128, dtype=f)
    consts = np.zeros((128, 1024), f)
    consts[:, 0:512] = np.arange(512, dtype=f)[None, :]
    consts[:, 512] = np.arange(128, dtype=f)
    sh["consts"] = consts
    pp_ = np.arange(128)
    sh["moe_blk"] = (pp_[:, None] // 8 == pp_[None, :] // 8).astype(f)
    s8 = np.zeros((128, 16), f)
    s8[np.arange(16) * 8, np.arange(16)] = 1.0
    sh["moe_sel8"] = s8
    tk = np.zeros((128, 32, 2), f)
    tk[:, :, 0] = np.arange(32, dtype=f)[None, :]
    tk[:, :, 1] = np.arange(128, dtype=f)[:, None]
    sh["moe_tok"] = tk
    sh["moe_tris"] = (pp_[:, None] < pp_[None, :]).astype(f)
    ii = np.arange(128)
    P, Fr = ii[:, None], ii[None, :]
    NEGV = -30000.0
    mk = np.zeros((128, 9, 128), f)
    mk[:, 0] = (P <= Fr)
    mk[:, 1] = (P >= Fr)
    mk[:, 2] = np.where(P > Fr, 0.0, NEGV)
    mk[:, 3] = np.where(P < Fr, 0.0, NEGV)
    mk[:, 4] = np.where(Fr >= P, 0.0, NEGV)
    mk[:, 5] = np.where(Fr <= P, 0.0, NEGV)
    mk[:, 6] = (P // 32 == Fr // 32)
    mk[:, 7] = (P // 64 == Fr // 64) & (P // 32 != Fr // 32)
    mk[:, 8] = (P // 64 != Fr // 64)
    sh["dn_masks"] = mk
    es = np.zeros((64, 2, 8, 128), f)
    for hh in range(8):
        es[hh, 0, hh, :] = 1.0
        es[32 + hh, 0, hh, :] = 1.0
    es[:, 1] = -es[:, 0]
    sh["dn_esel"] = es
    li = np.zeros((64, 128), f)
    li[32:40] = 1.0
    sh["dn_linit"] = li
    return sh


def prep_core(inp, sh, b):
    m = dict(sh)
    m["x"] = np.ascontiguousarray(inp["x"][b])
    m["ctx"] = np.ascontiguousarray(inp["ctx"][b])
    cc = np.stack([inp["c"][b], inp["c_ctx"]], axis=-1).astype(np.float32)
    m["c2"] = np.ascontiguousarray(cc.reshape(8, 128, 2).transpose(1, 0, 2))
    return m

PHASES = ["a0", "a1", "a2", "b", "c", "d", "e"]


def build(upto="e", dbg=(), dumps=()):
    k = K(dbg=dbg)
    declare_inputs(k)
    setup_consts(k)
    k.dump_list = []

    def dump(name, ap, res, shape):
        t = k.nc.dram_tensor("dbg_" + name, list(shape), ap.dtype, kind="ExternalOutput").ap()
        k.store(t, None, ap, res)
        k.dump_list.append("dbg_" + name)
    k.dump = dump
    k.dumps = set(dumps)
    fns = {"a0": phase_a0}
    for nm in ("a1", "a2", "b", "c", "d", "e"):
        f = globals().get("phase_" + nm)
        if f is not None:
            fns[nm] = f
    for ph in PHASES:
        if ph in fns:
            fns[ph](k)
        if ph == upto:
            break
    k.S.emit()
    return k


_CACHE = {}


def kernel(**inputs):
    inp = {kk: np.asarray(v) for kk, v in inputs.items()}
    sh = prep_shared(inp)
    in_maps = [prep_core(inp, sh, b) for b in range(8)]
    k = build()
    res = run_bass_kernel_spmd(k.nc, in_maps, core_ids=list(range(8)))
    out = np.stack([np.asarray(r["out"]) for r in res.results], axis=0).astype(np.float32)
    return out
```
